# Optimizing a Trainium2 kernel written in Bass

```python
import math
import jax
import jax.numpy as jnp
from jax import lax
import numpy as np

D_MODEL = 1024
BATCH = 4
SEQ = 8192
DEPTH = 1

MEM_LEN = 256
EPS = 1e-6
NEG_INF = -1e30
BIG = 1e30

GLA_HEADS = 4
GLA_DK = 64
GLA_DV = 128
GLA_RANK = 16
GLA_TAU = 16.0
GLA_CHUNK = 64

NSA_HEADS = 8
NSA_KV_GROUPS = 2
NSA_HPG = NSA_HEADS // NSA_KV_GROUPS
NSA_DH = 64
CMP_BLOCK = 32
CMP_STRIDE = 16
CMP_HIDDEN = 128
SLC_BLOCK = 64
SLC_TOPK = 16
WINDOW = 512
Q_BLOCK = 128

REL_BUCKETS = 32
REL_MAX_DIST = 2048

XA_HEADS = 4
XA_DH = D_MODEL // XA_HEADS

PEER_HEADS = 8
PEER_NKEYS = 128
PEER_EXPERTS = PEER_NKEYS * PEER_NKEYS
PEER_TOPK = 16
PEER_DKEY = 128
PEER_TOKEN_CHUNK = 128

IN_SIZES = (GLA_HEADS * GLA_DK, GLA_HEADS * GLA_DK, GLA_HEADS * GLA_DV, GLA_HEADS * GLA_DV, GLA_RANK,
            NSA_HEADS * NSA_DH,
            NSA_KV_GROUPS * NSA_DH, NSA_KV_GROUPS * NSA_DH,
            NSA_KV_GROUPS * NSA_DH, NSA_KV_GROUPS * NSA_DH,
            NSA_KV_GROUPS * NSA_DH, NSA_KV_GROUPS * NSA_DH,
            NSA_HEADS * 3)
IN_COLS = sum(IN_SIZES)

kernel_name = 'hybrid_gla_nsa_peer_block'


def rms_norm(x, g):
    xf = x.astype(jnp.float32)
    y = xf * lax.rsqrt(jnp.mean(xf * xf, axis=-1, keepdims=True) + EPS)
    return (y * g.astype(jnp.float32)).astype(x.dtype)


def _masked_softmax(s, mask):
    s = jnp.where(mask, s.astype(jnp.float32), NEG_INF)
    p = jax.nn.softmax(s, axis=-1)
    return p * jnp.any(mask, axis=-1, keepdims=True)


def _t5_bucket(dist):
    n = jnp.maximum(dist, 0)
    max_exact = REL_BUCKETS // 2
    nf = jnp.maximum(n, 1).astype(jnp.float32)
    log_ratio = jnp.log(nf / max_exact) / math.log(REL_MAX_DIST / max_exact)
    large = max_exact + (log_ratio * (REL_BUCKETS - max_exact)).astype(jnp.int32)
    large = jnp.minimum(large, REL_BUCKETS - 1)
    return jnp.where(n < max_exact, n, large)


def _head_bias(dist, rel_bias):
    b = jnp.moveaxis(rel_bias[_t5_bucket(dist)], -1, 0)
    return b.reshape((NSA_KV_GROUPS, NSA_HPG) + dist.shape).astype(jnp.float32)


def _split_cols(z):
    offs = np.cumsum(np.array(IN_SIZES))[:-1].tolist()
    return jnp.split(z, offs, axis=-1)


def _gla_chunked(q, k, v, log_a):
    B, T, H, dk = q.shape
    dv = v.shape[-1]
    C = GLA_CHUNK
    nc = T // C

    def to_chunks(a):
        return a.astype(jnp.float32).reshape(B, nc, C, H, a.shape[-1]).transpose(1, 0, 3, 2, 4)

    qc, kc, vc, ac = to_chunks(q), to_chunks(k), to_chunks(v), to_chunks(log_a)
    causal = jnp.tril(jnp.ones((C, C), dtype=bool))[..., None]

    def step(S, inp):
        qi, ki, vi, ai = inp
        b = jnp.cumsum(ai, axis=-2)
        diff = b[..., :, None, :] - b[..., None, :, :]
        decay = jnp.exp(jnp.where(causal, diff, NEG_INF))
        A = jnp.einsum('bhtd,bhsd,bhtsd->bhts', qi, ki, decay)
        o = jnp.einsum('bhts,bhsv->bhtv', A, vi) + jnp.einsum('bhtd,bhdv->bhtv', qi * jnp.exp(b), S)
        b_last = b[..., -1:, :]
        S_new = jnp.exp(b_last[..., 0, :])[..., None] * S + jnp.einsum(
            'bhsd,bhsv->bhdv', ki * jnp.exp(b_last - b), vi)
        return S_new, o

    S0 = jnp.zeros((B, H, dk, dv), jnp.float32)
    _, o = lax.scan(step, S0, (qc, kc, vc, ac))
    return o.transpose(1, 0, 3, 2, 4).reshape(B, T, H, dv)


def gla_mixer(gq, gk, gv, gr, glr, gate_w2, gate_b, out_norm):
    B, T, _ = gq.shape
    q = gq.reshape(B, T, GLA_HEADS, GLA_DK) * (GLA_DK ** -0.5)
    k = gk.reshape(B, T, GLA_HEADS, GLA_DK)
    v = gv.reshape(B, T, GLA_HEADS, GLA_DV)
    log_a = jax.nn.log_sigmoid((glr @ gate_w2 + gate_b).astype(jnp.float32)) / GLA_TAU
    log_a = log_a.reshape(B, T, GLA_HEADS, GLA_DK)
    o = _gla_chunked(q, k, v, log_a)
    o = rms_norm(o, out_norm).reshape(B, T, GLA_HEADS * GLA_DV)
    return (o * jax.nn.silu(gr.astype(jnp.float32))).astype(gq.dtype)


def _compress(kv, pos, w1, w2):
    B, T, G, dh = kv.shape
    n_cmp = (T - CMP_BLOCK) // CMP_STRIDE + 1
    idx = jnp.arange(n_cmp)[:, None] * CMP_STRIDE + jnp.arange(CMP_BLOCK)[None, :]
    blocks = kv[:, idx] + pos[None, None, :, None, :]
    blocks = blocks.transpose(0, 3, 1, 2, 4).reshape(B, G, n_cmp, CMP_BLOCK * dh)
    return jax.nn.gelu(blocks @ w1) @ w2


def _slc_importance(p_grp, n_slc):
    n_cmp = p_grp.shape[-1]
    ratio = SLC_BLOCK // CMP_STRIDE
    back = CMP_BLOCK // CMP_STRIDE - 1
    pad_end = max(ratio * n_slc - n_cmp, 0)
    pp = jnp.pad(p_grp, [(0, 0)] * (p_grp.ndim - 1) + [(back, pad_end)])
    base = jnp.arange(n_slc) * ratio + back
    imp = 0.0
    for r in range(-back, ratio):
        lo = CMP_STRIDE * r
        overlap = min(lo + CMP_BLOCK, SLC_BLOCK) - max(lo, 0)
        imp = imp + (overlap / CMP_BLOCK) * pp[..., base + r]
    return imp


def nsa_mixer(nq, kc, vc, ks, vs, kw, vw, ng, rel_bias, pos_k, pos_v, ck_w1, ck_w2, cv_w1, cv_w2):
    B, T, _ = nq.shape
    G, HPG, dh = NSA_KV_GROUPS, NSA_HPG, NSA_DH
    n_cmp = (T - CMP_BLOCK) // CMP_STRIDE + 1
    n_slc = T // SLC_BLOCK
    n_sel = min(SLC_TOPK, n_slc)
    n_qblk = T // Q_BLOCK
    kv_shape = (B, T, G, dh)

    q = (nq.reshape(B, T, G, HPG, dh) * (dh ** -0.5)).transpose(0, 2, 3, 1, 4)
    gates = jax.nn.sigmoid(ng.astype(jnp.float32)).reshape(B, T, G, HPG, 3).transpose(0, 2, 3, 1, 4)

    k_cmp = _compress(kc.reshape(kv_shape), pos_k, ck_w1, ck_w2)
    v_cmp = _compress(vc.reshape(kv_shape), pos_v, cv_w1, cv_w2)
    k_slc = ks.reshape(kv_shape).transpose(0, 2, 1, 3).reshape(B, G, n_slc, SLC_BLOCK, dh)
    v_slc = vs.reshape(kv_shape).transpose(0, 2, 1, 3).reshape(B, G, n_slc, SLC_BLOCK, dh)
    pad = ((0, 0), (0, 0), (WINDOW, 0), (0, 0))
    k_win = jnp.pad(kw.reshape(kv_shape).transpose(0, 2, 1, 3), pad)
    v_win = jnp.pad(vw.reshape(kv_shape).transpose(0, 2, 1, 3), pad)

    qi = jnp.arange(Q_BLOCK)
    kj = jnp.arange(WINDOW + Q_BLOCK)
    dist_w = qi[:, None] + WINDOW - kj[None, :]
    band = (dist_w >= 0) & (dist_w < WINDOW)
    bias_w = _head_bias(dist_w, rel_bias)
    cmp_end = jnp.arange(n_cmp) * CMP_STRIDE + (CMP_BLOCK - 1)
    slc_ids = jnp.arange(n_slc)
    bidx = jnp.arange(B)[:, None, None, None]
    gidx = jnp.arange(G)[None, :, None, None]
    table_g = rel_bias.reshape(REL_BUCKETS, G, HPG).transpose(1, 0, 2)

    def block(i):
        s0 = i * Q_BLOCK
        t = s0 + qi
        qb = lax.dynamic_slice_in_dim(q, s0, Q_BLOCK, axis=3)
        gb = lax.dynamic_slice_in_dim(gates, s0, Q_BLOCK, axis=3)

        dist_c = t[:, None] - cmp_end[None, :]
        s_c = jnp.einsum('bghqd,bgnd->bghqn', qb, k_cmp) + _head_bias(dist_c, rel_bias)
        p_c = _masked_softmax(s_c, dist_c >= 0)
        o_c = jnp.einsum('bghqn,bgnd->bghqd', p_c.astype(v_cmp.dtype), v_cmp)

        imp = _slc_importance(p_c.sum(axis=2), n_slc)
        cur = (t // SLC_BLOCK)[:, None]
        forced = (slc_ids == 0) | (slc_ids == cur) | (slc_ids == cur - 1)
        visible = (slc_ids * SLC_BLOCK)[None, :] <= t[:, None]
        imp = jnp.where(forced, BIG, jnp.where(visible, imp, NEG_INF))
        _, sel = lax.top_k(imp, n_sel)
        k_sel = k_slc[bidx, gidx, sel].reshape(B, G, Q_BLOCK, n_sel * SLC_BLOCK, dh)
        v_sel = v_slc[bidx, gidx, sel].reshape(B, G, Q_BLOCK, n_sel * SLC_BLOCK, dh)
        kpos = (sel[..., None] * SLC_BLOCK + jnp.arange(SLC_BLOCK)).reshape(B, G, Q_BLOCK, -1)
        dist_s = t[:, None] - kpos
        bias_s = jnp.moveaxis(table_g[gidx, _t5_bucket(dist_s)], -1, 2).astype(jnp.float32)
        s_s = jnp.einsum('bghqd,bgqkd->bghqk', qb, k_sel) + bias_s
        p_s = _masked_softmax(s_s, (dist_s >= 0)[:, :, None])
        o_s = jnp.einsum('bghqk,bgqkd->bghqd', p_s.astype(v_sel.dtype), v_sel)

        k_w = lax.dynamic_slice_in_dim(k_win, s0, WINDOW + Q_BLOCK, axis=2)
        v_w = lax.dynamic_slice_in_dim(v_win, s0, WINDOW + Q_BLOCK, axis=2)
        valid_w = band & ((s0 - WINDOW + kj) >= 0)[None, :]
        s_w = jnp.einsum('bghqd,bgkd->bghqk', qb, k_w) + bias_w
        p_w = _masked_softmax(s_w, valid_w)
        o_w = jnp.einsum('bghqk,bgkd->bghqd', p_w.astype(v_w.dtype), v_w)

        o = gb[..., 0:1] * o_c + gb[..., 1:2] * o_s + gb[..., 2:3] * o_w
        return o.astype(nq.dtype)

    o = lax.map(block, jnp.arange(n_qblk))
    return o.transpose(1, 0, 4, 2, 3, 5).reshape(B, T, NSA_HEADS * dh)


def cross_attn(hn, memn, wq, wk, wv, wo):
    B, T, _ = hn.shape
    M = memn.shape[1]
    q = (hn @ wq).reshape(B, T, XA_HEADS, XA_DH) * (XA_DH ** -0.5)
    k = (memn @ wk).reshape(B, M, XA_HEADS, XA_DH)
    v = (memn @ wv).reshape(B, M, XA_HEADS, XA_DH)
    s = jnp.einsum('bqhd,bmhd->bhqm', q, k).astype(jnp.float32)
    p = jax.nn.softmax(s, axis=-1).astype(v.dtype)
    o = jnp.einsum('bhqm,bmhd->bqhd', p, v).reshape(B, T, XA_HEADS * XA_DH)
    return o @ wo


def peer_ffn(hn, wq, subkeys, down, up):
    B, T, D = hn.shape
    N = B * T
    H, K = PEER_HEADS, PEER_TOPK
    xt = hn.reshape(N, D)
    q = (xt @ wq).reshape(N, H, 2, PEER_DKEY // 2)
    s = jnp.einsum('nhpd,hpkd->nhpk', q, subkeys).astype(jnp.float32)
    s_half, i_half = lax.top_k(s, K)
    cand_s = (s_half[:, :, 0, :, None] + s_half[:, :, 1, None, :]).reshape(N, H, K * K)
    cand_i = (i_half[:, :, 0, :, None] * PEER_NKEYS + i_half[:, :, 1, None, :]).reshape(N, H, K * K)
    top_s, pos = lax.top_k(cand_s, K)
    eidx = jnp.take_along_axis(cand_i, pos, axis=-1)
    gate = jax.nn.softmax(top_s, axis=-1)
    n_chunks = N // PEER_TOKEN_CHUNK

    def chunk(args):
        xc, ec, gc = args
        u = down[ec]
        act = jax.nn.gelu(jnp.einsum('cd,chkd->chk', xc, u).astype(jnp.float32)) * gc
        return jnp.einsum('chk,chkd->cd', act.astype(up.dtype), up[ec])

    y = lax.map(chunk, (xt.reshape(n_chunks, PEER_TOKEN_CHUNK, D),
                        eidx.reshape(n_chunks, PEER_TOKEN_CHUNK, H, K),
                        gate.reshape(n_chunks, PEER_TOKEN_CHUNK, H, K)))
    return y.reshape(B, T, D).astype(hn.dtype)


def setup_inputs(seed: int = 0) -> dict:
    key = jax.random.key(seed)
    ks = jax.random.split(key, 32)
    f32 = jnp.float32
    L, D = DEPTH, D_MODEL

    def nrm(k, shape, scale):
        return jax.random.normal(k, shape, f32) * scale

    def gain(k, shape):
        return 1.0 + 0.02 * jax.random.normal(k, shape, f32)

    return {
        'x': nrm(ks[0], (BATCH, SEQ, D), 1.0),
        'mem': nrm(ks[1], (BATCH, MEM_LEN, D), 1.0),
        'rel_bias': nrm(ks[2], (REL_BUCKETS, NSA_HEADS), 0.5),
        'norm_mix': gain(ks[3], (L, D)),
        'w_in': nrm(ks[4], (L, D, IN_COLS), D ** -0.5),
        'gla_gate_w2': nrm(ks[5], (L, GLA_RANK, GLA_HEADS * GLA_DK), GLA_RANK ** -0.5),
        'gla_gate_b': nrm(ks[6], (L, GLA_HEADS * GLA_DK), 0.1),
        'gla_out_norm': gain(ks[7], (L, GLA_DV)),
        'cmp_pos_k': nrm(ks[8], (L, CMP_BLOCK, NSA_DH), 0.1),
        'cmp_pos_v': nrm(ks[9], (L, CMP_BLOCK, NSA_DH), 0.1),
        'cmp_k_w1': nrm(ks[10], (L, CMP_BLOCK * NSA_DH, CMP_HIDDEN), (CMP_BLOCK * NSA_DH) ** -0.5),
        'cmp_k_w2': nrm(ks[11], (L, CMP_HIDDEN, NSA_DH), CMP_HIDDEN ** -0.5),
        'cmp_v_w1': nrm(ks[12], (L, CMP_BLOCK * NSA_DH, CMP_HIDDEN), (CMP_BLOCK * NSA_DH) ** -0.5),
        'cmp_v_w2': nrm(ks[13], (L, CMP_HIDDEN, NSA_DH), CMP_HIDDEN ** -0.5),
        'w_out': nrm(ks[14], (L, D, D), D ** -0.5),
        'norm_xattn': gain(ks[15], (L, D)),
        'norm_mem': gain(ks[16], (L, D)),
        'xa_wq': nrm(ks[17], (L, D, D), D ** -0.5),
        'xa_wk': nrm(ks[18], (L, D, D), D ** -0.5),
        'xa_wv': nrm(ks[19], (L, D, D), D ** -0.5),
        'xa_wo': nrm(ks[20], (L, D, D), D ** -0.5),
        'norm_ffn': gain(ks[21], (L, D)),
        'peer_wq': nrm(ks[22], (L, D, PEER_HEADS * PEER_DKEY), D ** -0.5),
        'peer_subkeys': nrm(ks[23], (L, PEER_HEADS, 2, PEER_NKEYS, PEER_DKEY // 2), (PEER_DKEY // 2) ** -0.5),
        'peer_down': nrm(ks[24], (L, PEER_EXPERTS, D), D ** -0.5),
        'peer_up': nrm(ks[25], (L, PEER_EXPERTS, D), PEER_HEADS ** -0.5),
        'norm_final': gain(ks[26], (D,)),
    }


def reference(x, mem, rel_bias, norm_mix, w_in, gla_gate_w2, gla_gate_b, gla_out_norm,
              cmp_pos_k, cmp_pos_v, cmp_k_w1, cmp_k_w2, cmp_v_w1, cmp_v_w2, w_out,
              norm_xattn, norm_mem, xa_wq, xa_wk, xa_wv, xa_wo,
              norm_ffn, peer_wq, peer_subkeys, peer_down, peer_up, norm_final):
    h = x
    for l in range(DEPTH):
        hn = rms_norm(h, norm_mix[l])
        (gq, gk, gv, gr, glr, nq, kc, vc, ks_, vs_, kw, vw, ng) = _split_cols(hn @ w_in[l])
        o_gla = gla_mixer(gq, gk, gv, gr, glr, gla_gate_w2[l], gla_gate_b[l], gla_out_norm[l])
        o_nsa = nsa_mixer(nq, kc, vc, ks_, vs_, kw, vw, ng, rel_bias,
                          cmp_pos_k[l], cmp_pos_v[l], cmp_k_w1[l], cmp_k_w2[l], cmp_v_w1[l], cmp_v_w2[l])
        h = h + jnp.concatenate([o_gla, o_nsa], axis=-1) @ w_out[l]
        h = h + cross_attn(rms_norm(h, norm_xattn[l]), rms_norm(mem, norm_mem[l]),
                           xa_wq[l], xa_wk[l], xa_wv[l], xa_wo[l])
        h = h + peer_ffn(rms_norm(h, norm_ffn[l]), peer_wq[l], peer_subkeys[l], peer_down[l], peer_up[l])
    return rms_norm(h, norm_final)
```

```python
import math
import numpy as np
import ml_dtypes
import concourse.bass as bass
import concourse.mybir as mybir
from concourse.bass_utils import run_bass_kernel_spmd
from contextlib import ExitStack

F32 = mybir.dt.float32
BF16 = mybir.dt.bfloat16
AF = mybir.ActivationFunctionType
ALU = mybir.AluOpType
AX = mybir.AxisListType
NPBF = ml_dtypes.bfloat16

D = 1024
NEG = -30000.0


class T:
    def __init__(self, h, name):
        self.h = h
        self.name = name
        self.w = None
        self.r = []
        self.psum = False

    def __getitem__(self, k):
        return self.h[k]


class Prog:
    def __init__(self, nc, n_dma_sems=48):
        self.nc = nc
        self.es = ExitStack()
        self.scopes = []
        self.eng = {'pe': nc.tensor, 'dve': nc.vector, 'act': nc.scalar,
                    'pool': nc.gpsimd, 'sp': nc.sync}
        self.sems = {}
        for k in self.eng:
            self.sems['e_' + k] = self.es.enter_context(nc.semaphore('e_' + k))
        self.cnt = {k: 0 for k in self.sems}
        self.ndma = n_dma_sems
        for i in range(n_dma_sems):
            key = 'd_%d' % i
            self.sems[key] = self.es.enter_context(nc.semaphore(key))
            self.cnt[key] = 0
        self.dma_rr = 0
        self.known = {k: {} for k in self.eng}
        self.ntile = 0
        self.ninst = 0

    def push(self):
        self.scopes.append(ExitStack())

    def pop(self):
        self.barrier()
        self.scopes.pop().close()

    def _stack(self):
        return self.scopes[-1] if self.scopes else self.es

    def sb(self, shape, dt, name=None):
        self.ntile += 1
        name = (name or 't') + '_%d' % self.ntile
        h = self._stack().enter_context(self.nc.sbuf_tensor(name, list(shape), dt))
        return T(h, name)

    def ps(self, shape, dt, name=None):
        self.ntile += 1
        name = (name or 'p') + '_%d' % self.ntile
        h = self._stack().enter_context(self.nc.psum_tensor(name, list(shape), dt))
        t = T(h, name)
        t.psum = True
        return t

    def dram(self, name, shape, dt, kind="Internal"):
        h = self.nc.dram_tensor(name, list(shape), dt, kind=kind).ap()
        return T(h, name)

    def _deps(self, reads, writes, e=None):
        deps = []
        for t in reads:
            if t.w is not None:
                deps.append(t.w)
            if t.psum:
                deps.extend([tok for tok in t.r if tok[0] != 'e_' + str(e)])
        for t in writes:
            if t.w is not None:
                deps.append(t.w)
            deps.extend(t.r)
        return deps

    def _waits(self, e, deps):
        kn = self.known[e]
        need = {}
        for (k, v) in deps:
            if kn.get(k, 0) >= v:
                continue
            if need.get(k, 0) < v:
                need[k] = v
        for k, v in need.items():
            kn[k] = v
        return list(need.items())

    def _emit(self, e, waits, fn, inc):
        eng = self.eng[e]
        for (k, v) in waits:
            eng.wait_ge(self.sems[k], v)
        ins = fn(eng)
        ins.then_inc(self.sems[inc[0]], inc[1])
        self.ninst += 1

    def op(self, e, fn, reads=(), writes=()):
        deps = self._deps(reads, writes, e)
        key = 'e_' + e
        if e == 'pe':
            deps = [d for d in deps if d[0] != key]
        waits = self._waits(e, deps)
        self.cnt[key] += 1
        tok = (key, self.cnt[key])
        self._emit(e, waits, fn, (key, 1))
        for t in reads:
            t.r.append(tok)
        for t in writes:
            t.w = tok
            t.r = []
        return tok

    def dma(self, e, out_t, out_ap, in_t, in_ap, **kw):
        reads = [in_t]
        writes = [out_t]
        deps = self._deps(reads, writes)
        key = 'd_%d' % self.dma_rr
        self.dma_rr = (self.dma_rr + 1) % self.ndma
        if self.cnt[key] > 0:
            deps.append((key, self.cnt[key]))
        waits = self._waits(e, deps)
        self.cnt[key] += 16
        tok = (key, self.cnt[key])
        self._emit(e, waits, lambda eng: eng.dma_start(out=out_ap, in_=in_ap, **kw), (key, 16))
        in_t.r.append(tok)
        out_t.w = tok
        out_t.r = []
        return tok

    def barrier(self):
        allt = [(k, v) for k, v in self.cnt.items() if v > 0]
        for e in self.eng:
            for (k, v) in self._waits(e, allt):
                self.eng[e].wait_ge(self.sems[k], v)

    def finish(self):
        self.barrier()
        while self.scopes:
            self.scopes.pop().close()
        self.es.close()


ORIG = dict(gq=(0, 256), gk=(256, 512), gv=(512, 1024), gr=(1024, 1536), glr=(1536, 1552),
            nq=(1552, 2064), kc=(2064, 2192), vc=(2192, 2320), ks=(2320, 2448), vs=(2448, 2576),
            kw=(2576, 2704), vw=(2704, 2832), ng=(2832, 2856))
PERM_ORDER = ['gq', 'gk', 'gv', 'gr', 'nq', 'kc', 'vc', 'ks', 'vs', 'kw', 'vw', 'ng', 'glr']
NCOL = 2856


def cmp_nchunks(jj):
    return min(4, (16 * jj + 15 + 127) // 128)


def build(T, debug=False, upto=9, cut=99):
    NT = T // 128
    NO = NT // 2
    NS = T // 64
    TO = T // 2
    NCMP = (T - 32) // 16 + 1
    NCC = (NCMP + 127) // 128
    pair_off = []
    npair = 0
    for jj in range(NO):
        pair_off.append(npair)
        npair += min(NCC, cmp_nchunks(jj))

    nc = bass.Bass("TRN2", target_bir_lowering=False)
    P = Prog(nc)
    SK = "ExternalOutput" if debug else "Internal"

    def inp(name, shape, dt=F32):
        return P.dram(name, shape, dt, kind="ExternalInput")

    xb = inp("xb", [T, D]); xo = inp("xo", [TO, D]); memb = inp("mem", [256, D])
    w_in = inp("w_in", [D, NCOL]); gw2 = inp("gw2", [16, 256]); gb = inp("gb", [1, 256])
    gnorm = inp("gnorm", [1, 128])
    norms = inp("norms", [5, D])
    cw1 = inp("cw1", [2, 64, 32, 128]); cpos = inp("cpos", [2, 64, 32]); cw2 = inp("cw2", [2, 128, 64])
    w_out = inp("w_out", [D, D]); xa_w = inp("xa_w", [4, D, D])
    pwq = inp("pwq", [D, D]); skd = inp("skd", [8, 128, 256])
    downT = inp("downT", [D, 16384]); up = inp("up", [16384, D])
    c_ident = inp("c_ident", [128, 128]); c_ucs = inp("c_ucs", [128, 128]); c_urev = inp("c_urev", [128, 128])
    c_causal = inp("c_causal", [128, 128]); c_selu = inp("c_selu", [2, 128, 128]); c_pv = inp("c_pv", [128, 2])
    c_selb = inp("c_selb", [2, 128, 15, 512]); c_winb = inp("c_winb", [2, 128, 6, 512])
    c_cmpb = inp("c_cmpb", [2, npair, 128, 512])
    c_far = inp("c_far", [2, 128, 512])
    c_wimp = inp("c_wimp", [NCC, 128, 128]); c_m12 = inp("c_m12", [NO, 128, 2, 128])
    c_ex = inp("c_ex", [128, T], BF16)
    out = P.dram("out", [TO, D], F32, kind="ExternalOutput")

    d_nq = P.dram("d_nq", [T, 512], BF16, kind=SK)
    d_ng = P.dram("d_ng", [T, 24], F32, kind=SK)
    d_ogla = P.dram("d_ogla", [T, 512], BF16, kind=SK)
    d_kT = P.dram("d_kT", [4, 128, T], BF16, kind=SK)
    d_vs = P.dram("d_vs", [T, 128], BF16, kind=SK)
    d_vw = P.dram("d_vw", [T, 128], BF16, kind=SK)
    d_omix = P.dram("d_omix", [TO, D], BF16, kind=SK)
    d_h = P.dram("d_h", [TO, D], F32, kind=SK)
    d_hn3T = P.dram("d_hn3T", [128, 8, TO], BF16, kind=SK)
    d_route = P.dram("d_route", [TO, 2064], F32, kind=SK)
    d_downT = P.dram("d_downT", [D, 16384], BF16, kind="Internal")
    d_up = P.dram("d_up", [16384, D], BF16, kind="Internal")
    d_cmp = P.dram("d_cmp", [2, 64, 512], BF16, kind=SK)
    d_dbg = P.dram("d_dbg", [3, TO, 512], F32, kind=SK)
    d_imp = P.dram("d_imp", [2, TO, 128], F32, kind=SK)

    def mm(ot, o_ap, lt, l_ap, rt, r_ap, start=True, stop=True):
        P.op('pe', lambda e: e.matmul(o_ap, lhsT=l_ap, rhs=r_ap, start=start, stop=stop), [lt, rt], [ot])

    def tr(ot, o_ap, it, i_ap, idt):
        P.op('pe', lambda e: e.transpose(out=o_ap, in_=i_ap, identity=idt[:]), [it, idt], [ot])

    def act(ot, o_ap, it, i_ap, func, bias=None, scale=None, accum=None, rd=(), wr=()):
        kw = {}
        if bias is not None:
            kw['bias'] = bias
        if scale is not None:
            kw['scale'] = scale
        if accum is not None:
            kw['accum_out'] = accum
        P.op('act', lambda e: e.activation(out=o_ap, in_=i_ap, func=func, **kw), [it] + list(rd), [ot] + list(wr))

    def cp(eng, ot, o_ap, it, i_ap):
        if eng == 'act':
            P.op('act', lambda e: e.copy(out=o_ap, in_=i_ap), [it], [ot])
        else:
            P.op(eng, lambda e: e.tensor_copy(out=o_ap, in_=i_ap), [it], [ot])

    def tt(eng, ot, o_ap, at, a_ap, bt, b_ap, op):
        P.op(eng, lambda e: e.tensor_tensor(out=o_ap, in0=a_ap, in1=b_ap, op=op), [at, bt], [ot])

    def ts(eng, ot, o_ap, at, a_ap, s1, s2, op0, op1=None, rd=()):
        if op1 is None:
            P.op(eng, lambda e: e.tensor_scalar(out=o_ap, in0=a_ap, scalar1=s1, scalar2=None, op0=op0), [at] + list(rd), [ot])
        else:
            P.op(eng, lambda e: e.tensor_scalar(out=o_ap, in0=a_ap, scalar1=s1, scalar2=s2, op0=op0, op1=op1), [at] + list(rd), [ot])

    def stt(eng, ot, o_ap, at, a_ap, sc, bt, b_ap, op0, op1, rd=()):
        P.op(eng, lambda e: e.scalar_tensor_tensor(out=o_ap, in0=a_ap, scalar=sc, in1=b_ap, op0=op0, op1=op1),
             [at, bt] + list(rd), [ot])

    def memset(eng, t, ap, v):
        P.op(eng, lambda e: e.memset(ap, v), [], [t])

    def recip(ot, o_ap, it, i_ap):
        P.op('dve', lambda e: e.reciprocal(out=o_ap, in_=i_ap), [it], [ot])

    dmaq = ['sp', 'act', 'pool']
    dq = [0]

    def dma(ot, o_ap, it, i_ap, q=None, **kw):
        if q is None:
            q = dmaq[dq[0] % 2]
            dq[0] += 1
        return P.dma(q, ot, o_ap, it, i_ap, **kw)

    ident = P.sb([128, 128], BF16, "ident"); identf = P.sb([128, 128], F32, "identf")
    ones = P.sb([128, 128], BF16, "ones")
    pv = P.sb([128, 2], F32, "pv")
    nrm = P.sb([128, 5, D], F32, "nrm")
    dma(identf, identf[:], c_ident, c_ident[:])
    dma(pv, pv[:], c_pv, c_pv[:])
    for k in range(5):
        dma(nrm, nrm[:, k, :], norms, norms[k:k + 1, :].partition_broadcast(128))
    cp('dve', ident, ident[:], identf, identf[:])
    memset('pool', ones, ones[:], 1.0)
    eps_t = P.sb([128, 1], F32, "eps_t")
    memset('dve', eps_t, eps_t[:], 1e-6)
    kcmpT = [P.sb([64, 512], BF16, "kcmpT%d" % g) for g in range(2)]
    vcmp = [P.sb([128, 4, 65], BF16, "vcmp%d" % g) for g in range(2)]

    def rmsnorm(xt, x_ap, gain_k, hn, hn_ap, junk, ss, rstd, n=D, eps=1e-6):
        memset('dve', ss, ss[:], 0.0)
        act(junk, junk[:, 0:n], xt, x_ap, AF.Square, accum=ss[:], rd=[ss], wr=[ss])
        act(rstd, rstd[:], ss, ss[:], AF.Ln, scale=1.0 / n, bias=eps_t[:, 0:1], rd=[eps_t])
        act(rstd, rstd[:], rstd, rstd[:], AF.Exp, scale=-0.5)
        stt('dve', hn, hn_ap, xt, x_ap, rstd[:, 0:1], nrm, nrm[:, gain_k, 0:n], ALU.mult, ALU.mult, rd=[rstd])

    if upto >= 1:
        P.push()
        winb = P.sb([128, 8, NCOL], BF16, "winb")
        wst = [P.sb([128, NCOL], F32, "wst%d" % i) for i in range(2)]
        w_in_v = w_in[:].rearrange("(c p) n -> c p n", p=128)
        for c in range(8):
            dma(wst[c % 2], wst[c % 2][:], w_in, w_in_v[c])
            cp(['dve', 'pool'][c % 2], winb, winb[:, c, :], wst[c % 2], wst[c % 2][:])
        gw2t = P.sb([16, 256], F32, "gw2t"); gbt = P.sb([1, 256], F32, "gbt"); onesrow = P.sb([1, 128], F32, "onesrow")
        gnt = P.sb([128, 128], F32, "gnt")
        ucs = P.sb([128, 128], F32, "ucs"); urev = P.sb([128, 128], F32, "urev"); causal = P.sb([128, 128], F32, "causal")
        m16 = P.sb([128, 1], F32, "m16")
        dma(gw2t, gw2t[:], gw2, gw2[:]); dma(gbt, gbt[:], gb, gb[:])
        dma(gnt, gnt[:], gnorm, gnorm[0:1, :].partition_broadcast(128))
        dma(ucs, ucs[:], c_ucs, c_ucs[:]); dma(urev, urev[:], c_urev, c_urev[:]); dma(causal, causal[:], c_causal, c_causal[:])
        memset('dve', onesrow, onesrow[:], 1.0)
        memset('dve', m16, m16[:], -1.0 / 16)
        S = P.sb([128, 2, 128], F32, "S"); Sb = P.sb([128, 2, 128], BF16, "Sb")
        memset('dve', S, S[:], 0.0); memset('pool', Sb, Sb[:], 0.0)

        xt = [P.sb([128, D], F32, "xt%d" % i) for i in range(2)]
        junk = P.sb([128, D], BF16, "junk"); ss = P.sb([128, 1], F32, "ss"); rstd = P.sb([128, 1], F32, "rstd")
        hn = P.sb([128, D], BF16, "hn"); hnT = P.sb([128, 8, 128], BF16, "hnT")
        pt = P.ps([128, 8, 128], BF16, "pt")
        pz = [P.ps([128, 512], F32, "pz%d" % i) for i in range(3)]
        pg1 = P.ps([128, 512], F32, "pg1"); pg2 = P.ps([128, 512], F32, "pg2")
        po = P.ps([128, 512], F32, "po"); ptb = P.ps([128, 8, 128], BF16, "ptb")
        glr = P.sb([128, 16], F32, "glr"); glrT = P.sb([16, 128], F32, "glrT")
        e1 = P.sb([128, 256], F32, "e1"); L = P.sb([128, 256], F32, "L")
        eb = P.sb([128, 256], F32, "eb"); enb = P.sb([128, 256], F32, "enb"); ec = P.sb([128, 256], F32, "ec")
        ebl = P.sb([128, 2], F32, "ebl")
        qk = P.sb([128, 3, 256], BF16, "qk")
        qkT = P.sb([128, 4, 128], BF16, "qkT")
        vv = P.sb([128, 512], BF16, "vv"); sg = P.sb([128, 512], F32, "sg")
        AT = P.sb([128, 128], BF16, "AT")
        ssq = P.sb([128, 4], F32, "ssq"); rs4 = P.sb([128, 4], F32, "rs4"); tmpn = P.sb([128, 128], F32, "tmpn")
        junk2 = P.sb([128, 128], F32, "junk2")
        og = P.sb([128, 512], BF16, "og"); otmp4 = P.sb([128, 512], F32, "otmp4")
        nqs = P.sb([128, 512], BF16, "nqs"); ngs = P.sb([128, 24], F32, "ngs")
        kk = P.sb([128, 4, 128], BF16, "kk"); kkT = P.sb([128, 4, 128], BF16, "kkT")
        vsw = P.sb([128, 2, 128], BF16, "vsw")
        cast_jobs = []
        if upto >= 4:
            stg = [P.sb([128, 4096], F32, "stg%d" % i) for i in range(2)]
            stb = [P.sb([128, 4096], BF16, "stb%d" % i) for i in range(2)]
            for r in range(8):
                for c in range(4):
                    cast_jobs.append((downT, downT[r * 128:(r + 1) * 128, c * 4096:(c + 1) * 4096],
                                      d_downT, d_downT[r * 128:(r + 1) * 128, c * 4096:(c + 1) * 4096]))
            sv = up[:].rearrange("(a p f) d -> a p (f d)", p=128, f=4)
            dv = d_up[:].rearrange("(a p f) d -> a p (f d)", p=128, f=4)
            for a in range(32):
                cast_jobs.append((up, sv[a], d_up, dv[a]))
        cj = [0]

        def cast_job():
            if cj[0] >= len(cast_jobs):
                return
            src_t, s_ap, dst_t, d_ap = cast_jobs[cj[0]]
            a, b_ = stg[cj[0] % 2], stb[cj[0] % 2]
            P.dma('sp', a, a[:], src_t, s_ap)
            for q_ in range(4):
                cp('act', b_, b_[:, q_ * 1024:(q_ + 1) * 1024], a, a[:, q_ * 1024:(q_ + 1) * 1024])
            P.dma('sp', dst_t, d_ap, b_, b_[:])
            cj[0] += 1
        cA, cB, cC, cD, cE, cF = 0, 512, 1024, 1536, 2048, 2560
        for i in range(NT):
            x_t = xt[i % 2]
            dma(x_t, x_t[:], xb, xb[i * 128:(i + 1) * 128, :])
            rmsnorm(x_t, x_t[:], 0, hn, hn[:], junk, ss, rstd)
            for c in range(8):
                tr(pt, pt[:, c, :], hn, hn[:, c * 128:(c + 1) * 128], ident)
            cp('act', hnT, hnT[:], pt, pt[:])

            def zgroup(pz_t, c0, n):
                for c in range(8):
                    mm(pz_t, pz_t[:, 0:n], hnT, hnT[:, c, :], winb, winb[:, c, c0:c0 + n], start=(c == 0), stop=(c == 7))
            if cut <= 1:
                continue
            zgroup(pz[0], cF, 296)
            cp('act', kk, kk[:, 3, :], pz[0], pz[0][:, 0:128])
            cp('act', vsw, vsw[:, 1, :], pz[0], pz[0][:, 128:256])
            if cut <= 1.05:
                continue
            cp('act', glr, glr[:], pz[0], pz[0][:, 280:296])
            if cut <= 1.07:
                continue
            act(ngs, ngs[:], pz[0], pz[0][:, 256:280], AF.Exp, scale=-1.0)
            if cut <= 1.1:
                continue
            ts('dve', ngs, ngs[:], ngs, ngs[:], 1.0, None, ALU.add)
            recip(ngs, ngs[:], ngs, ngs[:])
            dma(d_ng, d_ng[i * 128:(i + 1) * 128, :], ngs, ngs[:])
            if cut <= 1.2:
                continue
            zgroup(pz[1], cE, 512)
            cp('act', kk, kk[:, 0:3, :], pz[1], pz[1][:, 0:384].rearrange("p (a b) -> p a b", a=3))
            cp('dve', vsw, vsw[:, 0, :], pz[1], pz[1][:, 384:512])
            dma(d_vs, d_vs[i * 128:(i + 1) * 128, :], vsw, vsw[:, 0, :])
            dma(d_vw, d_vw[i * 128:(i + 1) * 128, :], vsw, vsw[:, 1, :])
            if cut <= 1.4:
                continue
            for a in range(4):
                tr(ptb, ptb[:, a, :], kk, kk[:, a, :], ident)
            cp('act', kkT, kkT[:], ptb, ptb[:, 0:4, :])
            dma(d_kT, d_kT[:, :, i * 128:(i + 1) * 128].rearrange("a p t -> p a t"), kkT, kkT[:])
            if cut <= 1.6:
                continue
            zgroup(pz[2], cD, 512)
            P.op('act', lambda e, pzt=pz[2]: e.mul(out=nqs[:], in_=pzt[:], mul=0.125), [pz[2]], [nqs])
            dma(d_nq, d_nq[i * 128:(i + 1) * 128, :], nqs, nqs[:])
            if cut <= 2:
                continue
            tr(pg1, pg1[0:16, 0:128], glr, glr[:], identf)
            cp('dve', glrT, glrT[:], pg1, pg1[0:16, 0:128])
            mm(pg1, pg1[:, 256:512], glrT, glrT[:], gw2t, gw2t[:], start=True, stop=False)
            mm(pg1, pg1[:, 256:512], onesrow, onesrow[:], gbt, gbt[:], start=False, stop=True)
            zgroup(pz[0], cA, 512)
            zgroup(pz[1], cB, 512)
            zgroup(pz[2], cC, 512)
            act(e1, e1[:], pg1, pg1[:, 256:512], AF.Exp, scale=-1.0)
            act(L, L[:], e1, e1[:], AF.Ln, bias=1.0)
            cp('act', vv, vv[:], pz[1], pz[1][:])
            act(sg, sg[:], pz[2], pz[2][:], AF.Exp, scale=-1.0)
            ts('dve', sg, sg[:], sg, sg[:], 1.0, None, ALU.add)
            recip(sg, sg[:], sg, sg[:])
            tt('dve', sg, sg[:], sg, sg[:], pz[2], pz[2][:], ALU.mult)
            tt('pool', sg, sg[:].rearrange("p (h d) -> p h d", h=4), sg, sg[:].rearrange("p (h d) -> p h d", h=4),
               gnt, gnt[:].unsqueeze(1).to_broadcast([128, 4, 128]), ALU.mult)
            if cut <= 3:
                continue
            mm(pg2, pg2[:, 0:256], ucs, ucs[:], L, L[:])
            mm(pg2, pg2[:, 256:512], urev, urev[:], L, L[:])
            for hp in range(2):
                mm(pg1, pg1[:, hp:hp + 1], L, L[:, hp * 128:(hp + 1) * 128], m16, m16[:])
            act(eb, eb[:], pg2, pg2[:, 0:256], AF.Exp)
            act(enb, enb[:], pg2, pg2[:, 0:256], AF.Exp, scale=-1.0)
            act(ec, ec[:], pg2, pg2[:, 256:512], AF.Exp)
            act(ebl, ebl[:], pg1, pg1[:, 0:2], AF.Exp)
            if cut <= 4:
                continue
            stt('dve', qk, qk[:, 0, :], pz[0], pz[0][:, 0:256], 0.125, eb, eb[:], ALU.mult, ALU.mult)
            tt('dve', qk, qk[:, 1, :], pz[0], pz[0][:, 256:512], enb, enb[:], ALU.mult)
            tt('dve', qk, qk[:, 2, :], pz[0], pz[0][:, 256:512], ec, ec[:], ALU.mult)
            for a in range(4):
                tr(ptb, ptb[:, 4 + a, :], qk, qk[:, a // 2, (a % 2) * 128:(a % 2 + 1) * 128], ident)
            cp('act', qkT, qkT[:], ptb, ptb[:, 4:8, :])
            if cut <= 6:
                continue
            memset('dve', ssq, ssq[:], 0.0)
            for h in range(4):
                hp, hh = h // 2, h % 2
                pr = slice(hh * 64, hh * 64 + 64)
                mm(pg2, pg2[:, 0:128], qkT, qkT[pr, 2 + hp, :], qkT, qkT[pr, hp, :])
                tt('dve', AT, AT[:], pg2, pg2[:, 0:128], causal, causal[:], ALU.mult)
                o_ap = po[:, h * 128:(h + 1) * 128]
                mm(po, o_ap, AT, AT[:], vv, vv[:, h * 128:(h + 1) * 128], start=True, stop=False)
                mm(po, o_ap, qkT, qkT[pr, hp, :], Sb, Sb[pr, hp, :], start=False, stop=True)
                mm(pg2, pg2[:, 128:256], qk, qk[:, 2, hp * 128:(hp + 1) * 128], vv, vv[:, h * 128:(h + 1) * 128])
                stt('dve', Sb, Sb[pr, hp, :], S, S[pr, hp, :], ebl[pr, hp:hp + 1], pg2, pg2[pr, 128:256], ALU.mult, ALU.add, rd=[ebl])
                stt('dve', S, S[pr, hp, :], S, S[pr, hp, :], ebl[pr, hp:hp + 1], pg2, pg2[pr, 128:256], ALU.mult, ALU.add, rd=[ebl])
                act(junk2, junk2[:], po, o_ap, AF.Square, accum=ssq[:, h:h + 1], rd=[ssq], wr=[ssq])
            act(rs4, rs4[:], ssq, ssq[:], AF.Ln, scale=1.0 / 128, bias=eps_t[:, 0:1], rd=[eps_t])
            act(rs4, rs4[:], rs4, rs4[:], AF.Exp, scale=-0.5)
            tt('dve', otmp4, otmp4[:], po, po[:], sg, sg[:], ALU.mult)
            tt('dve', og, og[:].rearrange("p (h d) -> p h d", h=4), otmp4, otmp4[:].rearrange("p (h d) -> p h d", h=4),
               rs4, rs4[:].unsqueeze(2).to_broadcast([128, 4, 128]), ALU.mult)
            dma(d_ogla, d_ogla[i * 128:(i + 1) * 128, :], og, og[:])
            for _ in range((len(cast_jobs) + NT - 1) // NT):
                cast_job()
        while cj[0] < len(cast_jobs):
            cast_job()
        P.pop()

        P.push()
        w1f = P.sb([64, 32, 128], F32, "w1f"); w1b = P.sb([64, 32, 128], BF16, "w1b")
        posf = P.sb([64, 32], F32, "posf"); posb = P.sb([64, 32], BF16, "posb")
        w2f = P.sb([128, 64], F32, "w2f"); w2b = P.sb([128, 64], BF16, "w2b")
        kTg = P.sb([64, T], BF16, "kTg")
        ph = P.ps([128, 512], F32, "ph"); pb = P.ps([128, 512], F32, "pb"); pc = P.ps([128, 512], F32, "pc")
        bias_h = P.sb([128, 1], F32, "bias_h")
        H = P.sb([128, 512], BF16, "H")
        for g in range(2):
            memset('dve', kcmpT[g], kcmpT[g][:], 0.0)
            memset('dve', vcmp[g], vcmp[g][:], 0.0)
            memset('dve', vcmp[g], vcmp[g][:, :, 64:65], 1.0)
        for kv in range(2):
            dma(w1f, w1f[:], cw1, cw1[kv]); dma(posf, posf[:], cpos, cpos[kv]); dma(w2f, w2f[:], cw2, cw2[kv])
            cp('dve', w1b, w1b[:], w1f, w1f[:]); cp('dve', posb, posb[:], posf, posf[:]); cp('dve', w2b, w2b[:], w2f, w2f[:])
            for l in range(32):
                mm(pb, pb[:, 0:1], w1b, w1b[:, l, :], posb, posb[:, l:l + 1], start=(l == 0), stop=(l == 31))
            cp('dve', bias_h, bias_h[:], pb, pb[:, 0:1])
            for g in range(2):
                dma(kTg, kTg[:], d_kT, d_kT[kv, g * 64:(g + 1) * 64, :])
                for l in range(32):
                    mm(ph, ph[:, 0:NCMP], w1b, w1b[:, l, :], kTg, kTg[:, l:l + 16 * (NCMP - 1) + 1:16],
                       start=(l == 0), stop=(l == 31))
                memset('dve', H, H[:], 0.0)
                act(H, H[:, 0:NCMP], ph, ph[:, 0:NCMP], AF.Gelu_apprx_tanh, bias=bias_h[:, 0:1], rd=[bias_h])
                if kv == 0:
                    mm(pc, pc[0:64, 0:NCMP], w2b, w2b[:], H, H[:, 0:NCMP])
                    cp('act', kcmpT[g], kcmpT[g][:, 0:NCMP], pc, pc[0:64, 0:NCMP])
                    if debug:
                        dma(d_cmp, d_cmp[g], kcmpT[g], kcmpT[g][:])
                else:
                    for c in range(NCC):
                        mm(pc, pc[:, c * 64:(c + 1) * 64], H, H[:, c * 128:(c + 1) * 128], w2b, w2b[:])
                    cp('act', vcmp[g], vcmp[g][:, 0:NCC, 0:64], pc, pc[:, 0:NCC * 64].rearrange("p (c d) -> p c d", d=64))
        P.pop()

    if upto >= 2:
        P.push()
        selu = P.sb([128, 2, 128], BF16, "selu"); seluf = P.sb([128, 2, 128], F32, "seluf")
        dma(seluf, seluf[:], c_selu, c_selu[:].rearrange("a p t -> p a t"))
        cp('dve', selu, selu[:], seluf, seluf[:])
        exm = P.sb([128, T], BF16, "exm")
        dma(exm, exm[:], c_ex, c_ex[:])
        wimpf = P.sb([128, NCC, 128], F32, "wimpf"); wimp = P.sb([128, NCC, 128], BF16, "wimp")
        dma(wimpf, wimpf[:], c_wimp, c_wimp[:].rearrange("c p s -> p c s"))
        cp('dve', wimp, wimp[:], wimpf, wimpf[:])
        ksT = P.sb([65, T], BF16, "ksT"); kwT = P.sb([64, T], BF16, "kwT")
        memset('dve', ksT, ksT[64:65, :], 1.0)
        farf = P.sb([128, 512], F32, "farf")
        vs = P.sb([128, NT, 65], BF16, "vs"); vw = P.sb([128, NT, 65], BF16, "vw")
        selb = P.sb([128, 15, 512], BF16, "selb"); winb2 = P.sb([128, 6, 512], BF16, "winb2")
        bst = [P.sb([128, 512], F32, "bst%d" % i) for i in range(2)]
        cbt = [P.sb([128, 512], BF16, "cbt%d" % i) for i in range(2)]
        qrows_ = [P.sb([128, 2, 256], BF16, "qrows%d" % i) for i in range(2)]; grows_ = [P.sb([128, 2, 24], F32, "grows%d" % i) for i in range(2)]
        gown_ = [P.sb([128, 12], F32, "gown%d" % i) for i in range(2)]; gtmp = P.sb([128, 12], F32, "gtmp")
        qT_ = [P.sb([65, 4, 128], BF16, "qT%d" % i) for i in range(2)]
        Ec = [P.sb([128, 4, 128], BF16, "Ec%d" % i) for i in range(4)]
        Eb = [P.sb([128, 4, 128], BF16, "Eb%d" % i) for i in range(3)]
        m12_ = [P.sb([128, 2, 128], F32, "m12_%d" % i) for i in range(2)]
        imp = P.sb([128, 128], F32, "imp"); imp2 = P.sb([128, 128], F32, "imp2"); m8 = P.sb([128, 16], F32, "m8")
        mk = P.sb([128, 128], BF16, "mk"); maskT4 = P.sb([128, 4, 128], BF16, "maskT4")
        rden = P.sb([128, 4], F32, "rden"); coef = P.sb([128, 4], F32, "coef")
        oacc = P.sb([128, 4, 64], F32, "oacc"); otmp = P.sb([128, 4, 64], F32, "otmp"); onsa = P.sb([128, 256], BF16, "onsa")
        pq = P.ps([64, 4, 128], F32, "pq")
        psc = [P.ps([128, 4, 128], F32, "psc%d" % i) for i in range(2)]
        pn = P.ps([128, 4, 128], F32, "pn")
        pnT = P.ps([65, 4, 128], F32, "pnT")
        pnT2 = P.ps([65, 4, 128], F32, "pnT2")
        numTs = P.sb([65, 4, 128], F32, "numTs")
        pimp = P.ps([128, 4, 128], F32, "pimp")
        pmt = P.ps([128, 128], BF16, "pmt")
        for g in range(2):
            dma(ksT, ksT[0:64, :], d_kT, d_kT[2, g * 64:(g + 1) * 64, :])
            dma(farf, farf[:], c_far, c_far[g])
            for i_ in range(2):
                cp('dve', qT_[i_], qT_[i_][64:65, :, :], farf, farf[64:65, :].rearrange("p (h q) -> p h q", h=4))
            dma(kwT, kwT[:], d_kT, d_kT[3, g * 64:(g + 1) * 64, :])
            for n0 in range(0, NT, 16):
                n1 = min(NT, n0 + 16)
                dma(vs, vs[:, n0:n1, 0:64], d_vs, d_vs[n0 * 128:n1 * 128, g * 64:(g + 1) * 64].rearrange("(n p) c -> p n c", p=128))
                dma(vw, vw[:, n0:n1, 0:64], d_vw, d_vw[n0 * 128:n1 * 128, g * 64:(g + 1) * 64].rearrange("(n p) c -> p n c", p=128))
            memset('dve', vs, vs[:, :, 64:65], 1.0)
            memset('dve', vw, vw[:, :, 64:65], 1.0)
            k = 0
            for m in range(15):
                dma(bst[k % 2], bst[k % 2][:], c_selb, c_selb[g, :, m, :])
                tt('pool', selb, selb[:, m, :], bst[k % 2], bst[k % 2][:], farf, farf[:], ALU.subtract); k += 1
            for m in range(6):
                dma(bst[k % 2], bst[k % 2][:], c_winb, c_winb[g, :, m, :])
                cp('pool', winb2, winb2[:, m, :], bst[k % 2], bst[k % 2][:]); k += 1
            ne = 0
            ne_ = [0]
            for jj in range(NO):
                qrows, grows, gown, qT, m12 = qrows_[jj % 2], grows_[jj % 2], gown_[jj % 2], qT_[jj % 2], m12_[jj % 2]
                dma(qrows, qrows[:], d_nq, d_nq[jj * 256:(jj + 1) * 256, g * 256:(g + 1) * 256].rearrange("(u p) c -> p u c", p=128))
                dma(grows, grows[:], d_ng, d_ng[jj * 256:(jj + 1) * 256, :].rearrange("(u p) c -> p u c", p=128))
                dma(m12, m12[:], c_m12, c_m12[jj])
                for h in range(4):
                    for u in range(2):
                        mm(pq, pq[:, h, :], qrows, qrows[:, u, h * 64:(h + 1) * 64], selu, selu[:, u, :], start=(u == 0), stop=(u == 1))
                cp('act', qT, qT[0:64], pq, pq[:])
                ts('dve', gtmp, gtmp[:], grows, grows[:, 0, g * 12:(g + 1) * 12], pv[:, 0:1], None, ALU.mult, rd=[pv])
                stt('dve', gown, gown[:], grows, grows[:, 1, g * 12:(g + 1) * 12], pv[:, 1:2], gtmp, gtmp[:], ALU.mult, ALU.add, rd=[pv])
                gv3 = gown[:].rearrange("p (h k) -> p h k", k=3)
                qT_all = qT[0:64].rearrange("p h q -> p (h q)")
                qT_aug = qT[:].rearrange("p h q -> p (h q)")

                def finish_branch(pn, br, first):
                    ts('dve', rden, rden[:], pn, pn[:, :, 64], 1e-30, None, ALU.max)
                    recip(rden, rden[:], rden, rden[:])
                    tt('dve', coef, coef[:], rden, rden[:], gown, gv3[:, :, br], ALU.mult)
                    dst = oacc if first else otmp
                    tt('dve', dst, dst[:], pn, pn[:, :, 0:64], coef, coef[:].unsqueeze(2).to_broadcast([128, 4, 64]), ALU.mult)
                    if not first:
                        tt('pool', oacc, oacc[:], oacc, oacc[:], otmp, otmp[:], ALU.add)
                    if debug:
                        dma(d_dbg, d_dbg[br, jj * 128:(jj + 1) * 128, g * 256:(g + 1) * 256], oacc, oacc[:].rearrange("p h d -> p (h d)"))

                def back_T(src):
                    cp('act', numTs, numTs[:], src, src[:])
                    for h in range(4):
                        P.op('pe', lambda e, h=h: e.transpose(out=pn[:, h, 0:65], in_=numTs[:, h, :], identity=identf[0:65, 0:65]), [numTs, identf], [pn])

                ncc = min(NCC, cmp_nchunks(jj))
                for c in range(ncc):
                    pi = pair_off[jj] + c
                    dma(bst[k % 2], bst[k % 2][:], c_cmpb, c_cmpb[g, pi])
                    cp('pool', cbt[k % 2], cbt[k % 2][:], bst[k % 2], bst[k % 2][:])
                    sc = psc[ne % 2]; ne += 1
                    sc_all = sc[:].rearrange("p h q -> p (h q)")
                    mm(sc, sc_all, kcmpT[g], kcmpT[g][:, c * 128:(c + 1) * 128], qT, qT_all, start=True, stop=False)
                    mm(sc, sc_all, ident, ident[:], cbt[k % 2], cbt[k % 2][:], start=False, stop=True)
                    k += 1
                    act(Ec[c], Ec[c][:], sc, sc[:], AF.Exp)
                for h in range(4):
                    for c in range(ncc):
                        mm(pn, pn[:, h, 0:65], Ec[c], Ec[c][:, h, :], vcmp[g], vcmp[g][:, c, :], start=(c == 0), stop=(c == ncc - 1))
                    for c in range(ncc):
                        mm(pimp, pimp[:, h, :], Ec[c], Ec[c][:, h, :], wimp, wimp[:, c, :], start=(c == 0), stop=(c == ncc - 1))
                nk = 2 * jj + 2
                k0 = max(0, 2 * jj - 4)
                bufs = {}

                def score(kind, kc, n):
                    sc = psc[ne_[0] % 2]; E = Eb[ne_[0] % 3]; ne_[0] += 1
                    bufs[n] = E
                    sc_all = sc[:].rearrange("p h q -> p (h q)")
                    if kind == 's':
                        m = 2 * jj + 1 - kc
                        mm(sc, sc_all, ksT, ksT[:, kc * 128:(kc + 1) * 128], qT, qT_aug, start=True, stop=False)
                        if m < 14:
                            mm(sc, sc_all, ident, ident[:], selb, selb[:, m, :], start=False, stop=False)
                        mm(sc, sc_all, exm, exm[:, kc * 128:(kc + 1) * 128], maskT4, mT_all, start=False, stop=True)
                    else:
                        m = 2 * jj + 1 - kc
                        mm(sc, sc_all, kwT, kwT[:, kc * 128:(kc + 1) * 128], qT, qT_all, start=True, stop=False)
                        mm(sc, sc_all, ident, ident[:], winb2, winb2[:, m, :], start=False, stop=True)
                    act(E, E[:], sc, sc[:], AF.Exp)

                def pvs(kind, kc, n):
                    E = bufs.pop(n)
                    if kind == 's':
                        mm(pnT, pnT[:].rearrange("p h q -> p (h q)"), vs, vs[:, kc, :], E, E[:].rearrange("p h q -> p (h q)"), start=(kc == 0), stop=(kc == nk - 1))
                    else:
                        mm(pnT2, pnT2[:].rearrange("p h q -> p (h q)"), vw, vw[:, kc, :], E, E[:].rearrange("p h q -> p (h q)"), start=(kc == k0), stop=(kc == nk - 1))

                def run_items(items):
                    NI = len(items)
                    for n in range(NI + 1):
                        if n < NI:
                            score(items[n][0], items[n][1], n)
                        if n >= 1:
                            pvs(items[n - 1][0], items[n - 1][1], n - 1)
                mT_all = maskT4[:].rearrange("p h q -> p (h q)")
                run_items([('w', kc) for kc in range(k0, nk)])
                finish_branch(pn, 0, True)
                for h in range(4):
                    if h == 0:
                        ts('dve', imp, imp[:], pimp, pimp[:, 0, :], rden[:, 0:1], None, ALU.mult, rd=[rden])
                    else:
                        stt('dve', imp, imp[:], pimp, pimp[:, h, :], rden[:, h:h + 1], imp, imp[:], ALU.mult, ALU.add, rd=[rden])
                tt('dve', imp, imp[:], imp, imp[:], m12, m12[:, 0, :], ALU.mult)
                tt('dve', imp, imp[:], imp, imp[:], m12, m12[:, 1, :], ALU.add)
                if debug:
                    dma(d_imp, d_imp[g, jj * 128:(jj + 1) * 128, :], imp, imp[:])
                P.op('dve', lambda e: e.max(out=m8[:, 0:8], in_=imp[:]), [imp], [m8])
                P.op('dve', lambda e: e.match_replace(out=imp2[:], in_to_replace=m8[:, 0:8], in_values=imp[:], imm_value=-1e30), [imp, m8], [imp2])
                P.op('dve', lambda e: e.max(out=m8[:, 8:16], in_=imp2[:]), [imp2], [m8])
                ts('dve', mk, mk[:], imp, imp[:], m8[:, 15:16], 1.0, ALU.is_ge, ALU.subtract, rd=[m8])
                tr(pmt, pmt[:], mk, mk[:], ident)
                P.op('act', lambda e: e.mul(out=maskT4[:], in_=pmt[:].unsqueeze(1).to_broadcast([128, 4, 128]), mul=30000.0), [pmt], [maskT4])
                run_items([('s', kc) for kc in range(nk)])
                back_T(pnT)
                finish_branch(pn, 1, False)
                back_T(pnT2)
                finish_branch(pn, 2, False)
                cp('act', onsa, onsa[:], oacc, oacc[:].rearrange("p h d -> p (h d)"))
                dma(d_omix, d_omix[jj * 128:(jj + 1) * 128, 512 + g * 256:512 + (g + 1) * 256], onsa, onsa[:])
        P.pop()

    if upto >= 3:
        P.push()
        woutb = P.sb([128, 8, D], BF16, "woutb"); wqb = P.sb([128, 8, D], BF16, "wqb"); wob = P.sb([128, 8, D], BF16, "wob")
        pwqb = P.sb([128, 8, D], BF16, "pwqb")
        skb = P.sb([128, 8, 256], BF16, "skb")
        kTm = P.sb([128, 8, 256], BF16, "kTm")
        vm = P.sb([128, 2, 4, 257], BF16, "vm")
        xt = [P.sb([128, D], F32, "x3_%d" % i) for i in range(2)]
        junk = P.sb([128, D], BF16, "junk3"); ss = P.sb([128, 1], F32, "ss3"); rstd = P.sb([128, 1], F32, "rstd3")
        hn = P.sb([128, D], BF16, "hn3"); hnT = P.sb([128, 8, 128], BF16, "hnT3")
        pt = P.ps([128, 8, 128], BF16, "pt3")
        pa = [P.ps([128, 512], F32, "pa%d" % i) for i in range(4)]
        pxa = P.ps([128, 2, 512], F32, "pxa")
        P.push()
        wst = [P.sb([128, D], F32, "wst3_%d" % i) for i in range(2)]
        wtmp = P.sb([128, 8, D], BF16, "wtmp"); skf = P.sb([128, 8, 256], F32, "skf")
        memT = P.sb([128, 8, 256], BF16, "memT")
        k = [0]

        def load_w(dst, src_t, src_ap3):
            for c in range(8):
                a = wst[k[0] % 2]
                dma(a, a[:], src_t, src_ap3[c])
                cp(['dve', 'pool'][k[0] % 2], dst, dst[:, c, :], a, a[:]); k[0] += 1
        load_w(woutb, w_out, w_out[:].rearrange("(c p) n -> c p n", p=128))
        load_w(wqb, xa_w, xa_w[0].rearrange("(c p) n -> c p n", p=128))
        load_w(wob, xa_w, xa_w[3].rearrange("(c p) n -> c p n", p=128))
        load_w(pwqb, pwq, pwq[:].rearrange("(c p) n -> c p n", p=128))
        dma(skf, skf[:], skd, skd[:].rearrange("c p n -> p c n"))
        cp('dve', skb, skb[:], skf, skf[:])
        memset('dve', vm, vm[:, :, :, 256:257], 1.0)
        for mc in range(2):
            x_t = xt[mc % 2]
            dma(x_t, x_t[:], memb, memb[mc * 128:(mc + 1) * 128, :])
            rmsnorm(x_t, x_t[:], 2, hn, hn[:], junk, ss, rstd)
            for c in range(8):
                tr(pt, pt[:, c, :], hn, hn[:, c * 128:(c + 1) * 128], ident)
            cp('act', memT, memT[:, :, mc * 128:(mc + 1) * 128], pt, pt[:])
        load_w(wtmp, xa_w, xa_w[1].rearrange("(c p) n -> c p n", p=128))
        for oc in range(8):
            for c in range(8):
                mm(pa[0], pa[0][:, 0:256], wtmp, wtmp[:, c, oc * 128:(oc + 1) * 128], memT, memT[:, c, :], start=(c == 0), stop=(c == 7))
            cp('act', kTm, kTm[:, oc, :], pa[0], pa[0][:, 0:256])
        load_w(wtmp, xa_w, xa_w[2].rearrange("(c p) n -> c p n", p=128))
        for mc in range(2):
            for half in range(2):
                for c in range(8):
                    mm(pa[half], pa[half][:], memT, memT[:, c, mc * 128:(mc + 1) * 128], wtmp, wtmp[:, c, half * 512:(half + 1) * 512], start=(c == 0), stop=(c == 7))
                cp('act', vm, vm[:, mc, half * 2:half * 2 + 2, 0:256], pa[half], pa[half][:].rearrange("p (h d) -> p h d", d=256))
        P.pop()
        og2 = P.sb([128, 2, 512], BF16, "og2"); omx = P.sb([128, D], BF16, "omx"); otm = P.sb([128, 512], F32, "otm")
        h1 = P.sb([128, D], F32, "h1"); qTx = P.sb([128, 8, 128], BF16, "qTx")
        Ex = P.sb([128, 2, 4, 128], BF16, "Ex"); rdx = P.sb([128, 4], F32, "rdx")
        oxa = P.sb([128, D], BF16, "oxa")
        hn3T = P.sb([128, 8, 128], BF16, "hn3Ts"); qpT = P.sb([128, 8, 128], BF16, "qpT")
        sc_ = [P.sb([128, 16, 128], F32, "scr%d" % i) for i in range(2)]; ab_ = [P.sb([128, 16, 128], F32, "ab%d" % i) for i in range(2)]
        negm = P.sb([128, 16], F32, "negm"); t16 = P.sb([128, 16, 16], F32, "t16"); scr2_ = [P.sb([128, 128], F32, "scr2_%d" % i) for i in range(4)]
        candall = P.sb([128, 8, 256], F32, "candall"); cand2_ = [P.sb([128, 256], F32, "cand2_%d" % i) for i in range(4)]; c16 = P.sb([128, 8, 16], F32, "c16")
        route = P.sb([128, 16], F32, "route"); zs = P.sb([128, 8], F32, "zs")
        memset('dve', route, route[:], 0.0)
        def Xgen(jj):
            sc = sc_[jj % 2]
            x_t = xt[jj % 2]
            dma(x_t, x_t[:], xo, xo[jj * 128:(jj + 1) * 128, :])
            dma(og2, og2[:], d_ogla, d_ogla[jj * 256:(jj + 1) * 256, :].rearrange("(u p) c -> p u c", p=128))
            dma(omx, omx[:, 512:1024], d_omix, d_omix[jj * 128:(jj + 1) * 128, 512:1024])
            ts('dve', otm, otm[:], og2, og2[:, 0, :], pv[:, 0:1], None, ALU.mult, rd=[pv])
            stt('dve', omx, omx[:, 0:512], og2, og2[:, 1, :], pv[:, 1:2], otm, otm[:], ALU.mult, ALU.add, rd=[pv])
            for c in range(8):
                tr(pt, pt[:, c, :], omx, omx[:, c * 128:(c + 1) * 128], ident)
            cp('act', hnT, hnT[:], pt, pt[:])
            for half in range(2):
                for c in range(8):
                    mm(pa[half], pa[half][:], hnT, hnT[:, c, :], woutb, woutb[:, c, half * 512:(half + 1) * 512], start=(c == 0), stop=(c == 7))
                tt('dve', h1, h1[:, half * 512:(half + 1) * 512], pa[half], pa[half][:], x_t, x_t[:, half * 512:(half + 1) * 512], ALU.add)
            yield
            rmsnorm(h1, h1[:], 1, hn, hn[:], junk, ss, rstd)
            for c in range(8):
                tr(pt, pt[:, c, :], hn, hn[:, c * 128:(c + 1) * 128], ident)
            cp('act', hnT, hnT[:], pt, pt[:])
            for oc in range(8):
                pq_ = pa[2 + oc % 2]
                for c in range(8):
                    mm(pq_, pq_[:, 0:128], wqb, wqb[:, c, oc * 128:(oc + 1) * 128], hnT, hnT[:, c, :], start=(c == 0), stop=(c == 7))
                cp(['act', 'dve'][oc % 2], qTx, qTx[:, oc, :], pq_, pq_[:, 0:128])
                yield
            yield
            for mc in range(2):
                for h in range(4):
                    for dc in range(2):
                        mm(pxa, pxa[:, mc, h * 128:(h + 1) * 128], kTm, kTm[:, h * 2 + dc, mc * 128:(mc + 1) * 128], qTx, qTx[:, h * 2 + dc, :], start=(dc == 0), stop=(dc == 1))
            act(Ex, Ex[:].rearrange("p a h q -> p a (h q)"), pxa, pxa[:], AF.Exp, scale=1.0 / 16)
            for h in range(4):
                o_ap = pxa[:, h // 2, (h % 2) * 256:(h % 2) * 256 + 256]
                for mc in range(2):
                    mm(pa[h % 2], pa[h % 2][:, 0:257], Ex, Ex[:, mc, h, :], vm, vm[:, mc, h, :], start=(mc == 0), stop=(mc == 1))
                ts('dve', rdx, rdx[:, h:h + 1], pa[h % 2], pa[h % 2][:, 256:257], 1e-30, None, ALU.max)
                recip(rdx, rdx[:, h:h + 1], rdx, rdx[:, h:h + 1])
                ts('dve', oxa, oxa[:, h * 256:(h + 1) * 256], pa[h % 2], pa[h % 2][:, 0:256], rdx[:, h:h + 1], None, ALU.mult, rd=[rdx])
            yield
            for c in range(8):
                tr(pt, pt[:, c, :], oxa, oxa[:, c * 128:(c + 1) * 128], ident)
            cp('act', hnT, hnT[:], pt, pt[:])
            for half in range(2):
                for c in range(8):
                    mm(pa[half], pa[half][:], hnT, hnT[:, c, :], wob, wob[:, c, half * 512:(half + 1) * 512], start=(c == 0), stop=(c == 7))
                tt('dve', h1, h1[:, half * 512:(half + 1) * 512], pa[half], pa[half][:], h1, h1[:, half * 512:(half + 1) * 512], ALU.add)
            dma(d_h, d_h[jj * 128:(jj + 1) * 128, :], h1, h1[:])
            yield
            rmsnorm(h1, h1[:], 3, hn, hn[:], junk, ss, rstd)
            for c in range(8):
                tr(pt, pt[:, c, :], hn, hn[:, c * 128:(c + 1) * 128], ident)
            cp('act', hn3T, hn3T[:], pt, pt[:])
            dma(d_hn3T, d_hn3T[:, :, jj * 128:(jj + 1) * 128], hn3T, hn3T[:])
            for oc in range(8):
                pq_ = pa[2 + oc % 2]
                for c in range(8):
                    mm(pq_, pq_[:, 0:128], pwqb, pwqb[:, c, oc * 128:(oc + 1) * 128], hn3T, hn3T[:, c, :], start=(c == 0), stop=(c == 7))
                cp(['act', 'dve'][oc % 2], qpT, qpT[:, oc, :], pq_, pq_[:, 0:128])
                yield
            yield
            for oc in range(8):
                pq_ = pa[oc % 2]
                mm(pq_, pq_[:, 0:256], qpT, qpT[:, oc, :], skb, skb[:, oc, :])
                cp(['act', 'dve'][oc % 2], sc, sc[:, 2 * oc:2 * oc + 2, :], pq_, pq_[:, 0:256].rearrange("p (a k) -> p a k", a=2))
            yield

        def Rgen(jj):
            sc = sc_[jj % 2]; ab = ab_[jj % 2]
            P.op('dve', lambda e: e.tensor_reduce(out=negm[:], in_=sc[:], axis=AX.X, op=ALU.max), [sc], [negm])
            ts('dve', negm, negm[:], negm, negm[:], -1.0, None, ALU.mult)
            for r in range(16):
                act(ab, ab[:, r, :], sc, sc[:, r, :], AF.Exp, bias=negm[:, r:r + 1], rd=[negm])
            yield
            for r0 in range(0, 16, 4):
                for r in range(r0, r0 + 4):
                    P.op('dve', lambda e, r=r: e.max(out=t16[:, r, 0:8], in_=ab[:, r, :]), [ab], [t16])
                for r in range(r0, r0 + 4):
                    P.op('dve', lambda e, r=r: e.match_replace(out=scr2_[r % 4][:], in_to_replace=t16[:, r, 0:8], in_values=ab[:, r, :], imm_value=-1.0), [ab, t16], [scr2_[r % 4]])
                for r in range(r0, r0 + 4):
                    P.op('dve', lambda e, r=r: e.max(out=t16[:, r, 8:16], in_=scr2_[r % 4][:]), [scr2_[r % 4]], [t16])
                yield
            t16v = t16[:].rearrange("p (h a) k -> p h a k", a=2)
            abv = ab[:].rearrange("p (h a) k -> p h a k", a=2)

            def cand_top16():
                yield
                tt('dve', candall, candall[:].rearrange("p h (a b) -> p h a b", a=16),
                   t16, t16v[:, :, 0, :].unsqueeze(3).to_broadcast([128, 8, 16, 16]),
                   t16, t16v[:, :, 1, :].unsqueeze(2).to_broadcast([128, 8, 16, 16]), ALU.mult)
                for h0 in range(0, 8, 4):
                    for h in range(h0, h0 + 4):
                        P.op('dve', lambda e, h=h: e.max(out=c16[:, h, 0:8], in_=candall[:, h, :]), [candall], [c16])
                    for h in range(h0, h0 + 4):
                        P.op('dve', lambda e, h=h: e.match_replace(out=cand2_[h % 4][:], in_to_replace=c16[:, h, 0:8], in_values=candall[:, h, :], imm_value=-1.0), [candall, c16], [cand2_[h % 4]])
                    for h in range(h0, h0 + 4):
                        P.op('dve', lambda e, h=h: e.max(out=c16[:, h, 8:16], in_=cand2_[h % 4][:]), [cand2_[h % 4]], [c16])
                    yield
            yield from cand_top16()
            P.op('dve', lambda e: e.tensor_reduce(out=zs[:], in_=c16[:], axis=AX.X, op=ALU.add), [c16], [zs])
            recip(zs, zs[:], zs, zs[:])
            tt('dve', ab, abv[:, :, 1, :], ab, abv[:, :, 1, :], zs, zs[:].unsqueeze(2).to_broadcast([128, 8, 128]), ALU.mult)
            stt('dve', route, route[:, 0:8], c16, c16[:, :, 15], 1.0 - 1e-6, zs, zs[:], ALU.mult, ALU.mult)
            dma(d_route, d_route[jj * 128:(jj + 1) * 128, 0:2048], ab, ab[:].rearrange("p r k -> p (r k)"))
            dma(d_route, d_route[jj * 128:(jj + 1) * 128, 2048:2064], route, route[:])
            yield

        def drain(g):
            for _ in g:
                pass
        drain(Xgen(0))
        for jj in range(NO):
            gr = Rgen(jj)
            gx = Xgen(jj + 1) if jj + 1 < NO else iter(())
            ra = xa_ = True
            while ra or xa_:
                if xa_:
                    xa_ = next(gx, 'END') != 'END'
                if ra:
                    ra = next(gr, 'END') != 'END'
        P.pop()

    if upto >= 4:
        P.push()
        TG = 2
        IC = 16
        NCH = 128 // IC
        ACT_HEADS = (2, 4, 6)
        hT = P.sb([128, 8, TG * 128], BF16, "hT")
        ab = [P.sb([128, 16, 128], F32, "ab4_%d" % u) for u in range(TG)]
        rt = [P.sb([128, 16], F32, "rt%d" % u) for u in range(TG)]
        Wc = [[P.sb([128, IC * 128], BF16, "Wc%d_%d" % (i, u)) for u in range(TG)] for i in range(2)]
        et = [P.sb([128, IC, 128], F32, "et%d" % i) for i in range(4)]
        mt = [P.sb([128, IC, 128], BF16, "mt%d" % i) for i in range(2)]
        dnb = [P.sb([128, 8, 512], BF16, "dnb%d" % i) for i in range(3)]
        upb = [P.sb([128, 4, D], BF16, "upb%d" % i) for i in range(3)]
        Gs = [P.sb([128, 512], BF16, "G%d" % i) for i in range(2)]
        GT = [P.sb([128, 4, 128], BF16, "GT%d" % i) for i in range(2)]
        py = [P.ps([128, 2, 512], F32, "py%d" % u) for u in range(TG)]
        pd = [P.ps([128, 512], F32, "pd%d" % i) for i in range(2)]
        ptg = [P.ps([128, 8, 128], BF16, "ptg%d" % i) for i in range(2)]
        h2 = P.sb([128, D], F32, "h2"); yo = P.sb([128, D], F32, "yo")
        junk = P.sb([128, D], BF16, "junk4"); ss = P.sb([128, 1], F32, "ss4"); rstd = P.sb([128, 1], F32, "rstd4")
        qn = [0]
        for tg in range(NO // TG):
            dma(hT, hT[:], d_hn3T, d_hn3T[:, :, tg * TG * 128:(tg + 1) * TG * 128])
            for u in range(TG):
                j = tg * TG + u
                dma(ab[u], ab[u][:].rearrange("p r k -> p (r k)"), d_route, d_route[j * 128:(j + 1) * 128, 0:2048])
                dma(rt[u], rt[u][:], d_route, d_route[j * 128:(j + 1) * 128, 2048:2064])

            def wgen(c):
                its = [(u, h) for u in range(TG) for h in range(8)]
                K = len(its)
                eb_ = {}; mb_ = {}

                def E_(n):
                    u, h = its[n]
                    e_ = et[qn[0] % 4]; qn[0] += 1
                    eb_[n] = e_
                    if h in ACT_HEADS:
                        for i_ in range(IC):
                            P.op('act', lambda e, e_=e_, i_=i_, u=u, h=h: e.activation(out=e_[:, i_, :], in_=ab[u][:, 2 * h + 1, :], func=AF.Copy,
                                                                                  scale=ab[u][:, 2 * h, c * IC + i_:c * IC + i_ + 1]), [ab[u]], [e_])
                    else:
                        tt('dve', e_, e_[:], ab[u], ab[u][:, 2 * h, c * IC:(c + 1) * IC].unsqueeze(2).to_broadcast([128, IC, 128]),
                           ab[u], ab[u][:, 2 * h + 1, :].unsqueeze(1).to_broadcast([128, IC, 128]), ALU.mult)

                def S_(n):
                    u, h = its[n]
                    e_ = eb_.pop(n)
                    w_ap = Wc[c % 2][u][:].rearrange("p (a b) -> p a b", a=IC)
                    if h == 0:
                        stt('dve', Wc[c % 2][u], w_ap, e_, e_[:], rt[u][:, h:h + 1], e_, e_[:], ALU.is_ge, ALU.mult, rd=[rt[u]])
                    else:
                        m_ = mt[n % 2]
                        mb_[n] = m_
                        stt('dve', m_, m_[:], e_, e_[:], rt[u][:, h:h + 1], e_, e_[:], ALU.is_ge, ALU.mult, rd=[rt[u]])

                def A_(n):
                    u, h = its[n]
                    if h == 0:
                        return
                    m_ = mb_.pop(n)
                    w_ap = Wc[c % 2][u][:].rearrange("p (a b) -> p a b", a=IC)
                    tt('dve', Wc[c % 2][u], w_ap, Wc[c % 2][u], w_ap, m_, m_[:], ALU.add)
                for n in range(K + 2):
                    if n < K:
                        E_(n)
                    if 1 <= n <= K:
                        S_(n - 1)
                    if n >= 2:
                        A_(n - 2)
                    yield

            def load_w(ecx):
                dn, ub = dnb[ecx % 3], upb[ecx % 3]
                dma(dn, dn[:], d_downT, d_downT[:, ecx * 512:(ecx + 1) * 512].rearrange("(c p) e -> p c e", p=128))
                dma(ub, ub[:], d_up, d_up[ecx * 512:(ecx + 1) * 512, :].rearrange("(s p) d -> p s d", p=128))

            items = [(ecx, u) for ecx in range(32) for u in range(TG)]
            N = len(items)

            def stA(n):
                ecx, u = items[n]
                if u == 0:
                    if ecx == 0:
                        load_w(0)
                    if ecx + 1 < 32:
                        load_w(ecx + 1)
                    if ecx % 4 == 0:
                        for _ in wg[0]:
                            pass
                        wg[0] = wgen(ecx // 4 + 1) if ecx // 4 + 1 < NCH else iter(())
                for _ in range(3):
                    next(wg[0], None)
                dn = dnb[ecx % 3]
                pdt = pd[n % 2]; G = Gs[n % 2]
                for c in range(8):
                    mm(pdt, pdt[:], hT, hT[:, c, u * 128:(u + 1) * 128], dn, dn[:, c, :], start=(c == 0), stop=(c == 7))
                act(G, G[:], pdt, pdt[:], AF.Gelu_apprx_tanh)
                wch = Wc[(ecx // 4) % 2][u]
                tt('dve', G, G[:], G, G[:], wch, wch[:, (ecx % 4) * 512:(ecx % 4 + 1) * 512], ALU.mult)

            def stB(n):
                G = Gs[n % 2]; gt_ = GT[n % 2]; pt_ = ptg[n % 2]
                for s_ in range(4):
                    tr(pt_, pt_[:, s_, :], G, G[:, s_ * 128:(s_ + 1) * 128], ident)
                cp('act', gt_, gt_[:], pt_, pt_[:, 0:4, :])

            def stC(n):
                ecx, u = items[n]
                gt_ = GT[n % 2]; ub = upb[ecx % 3]
                for half in range(2):
                    for s_ in range(4):
                        mm(py[u], py[u][:, half, :], gt_, gt_[:, s_, :], ub, ub[:, s_, half * 512:(half + 1) * 512],
                           start=(ecx == 0 and s_ == 0), stop=(ecx == 31 and s_ == 3))

            wg = [iter(())]
            for _ in wgen(0):
                pass
            for n in range(N + 2):
                if n < N:
                    stA(n)
                if 1 <= n <= N:
                    stB(n - 1)
                if n >= 2:
                    stC(n - 2)
            for _ in wg[0]:
                pass
            for u in range(TG):
                j = tg * TG + u
                dma(h2, h2[:], d_h, d_h[j * 128:(j + 1) * 128, :])
                tt('dve', h2, h2[:], h2, h2[:], py[u], py[u][:].rearrange("p a b -> p (a b)"), ALU.add)
                rmsnorm(h2, h2[:], 4, yo, yo[:], junk, ss, rstd)
                dma(out, out[j * 128:(j + 1) * 128, :], yo, yo[:])
        P.pop()

    P.finish()
    return nc, dict(npair=npair, pair_off=pair_off, NCC=NCC, NCMP=NCMP, ninst=P.ninst)


def _t5_bucket_np(dist):
    n = np.maximum(dist, 0)
    nf = np.maximum(n, 1).astype(np.float32)
    log_ratio = (np.log(nf / np.float32(16)) / np.float32(math.log(2048 / 16))).astype(np.float32)
    large = 16 + (log_ratio * np.float32(16)).astype(np.int32)
    large = np.minimum(large, 31)
    return np.where(n < 16, n, large).astype(np.int64)


def _bias_tile(rel_bias, g, dist, valid):
    bk = _t5_bucket_np(dist)
    outt = np.empty((128, 4, 128), np.float32)
    for h in range(4):
        outt[:, h, :] = np.where(valid, rel_bias[bk, g * 4 + h], np.float32(NEG))
    return outt.reshape(128, 512)


def make_core_inputs(inputs, T, b, p, meta):
    NT = T // 128; NO = NT // 2; NS = T // 64; NCMP = meta['NCMP']; NCC = meta['NCC']
    f = lambda a: np.ascontiguousarray(np.asarray(a, dtype=np.float32))
    x = f(inputs['x'][b]); rel_bias = f(inputs['rel_bias'])
    m = {}
    m['xb'] = x
    m['xo'] = np.ascontiguousarray(x.reshape(NO, 2, 128, D)[:, p].reshape(NO * 128, D))
    m['mem'] = f(inputs['mem'][b])
    w_in = f(inputs['w_in'][0])
    m['w_in'] = np.ascontiguousarray(np.concatenate([w_in[:, ORIG[k][0]:ORIG[k][1]] for k in PERM_ORDER], axis=1))
    m['gw2'] = f(inputs['gla_gate_w2'][0]); m['gb'] = f(inputs['gla_gate_b'][0]).reshape(1, 256)
    m['gnorm'] = f(inputs['gla_out_norm'][0]).reshape(1, 128)
    m['norms'] = np.stack([f(inputs['norm_mix'][0]), f(inputs['norm_xattn'][0]), f(inputs['norm_mem'][0]),
                           f(inputs['norm_ffn'][0]), f(inputs['norm_final'])], 0)
    cw1 = np.stack([f(inputs['cmp_k_w1'][0]), f(inputs['cmp_v_w1'][0])], 0)
    m['cw1'] = np.ascontiguousarray(cw1.reshape(2, 32, 64, 128).transpose(0, 2, 1, 3))
    cpos = np.stack([f(inputs['cmp_pos_k'][0]), f(inputs['cmp_pos_v'][0])], 0)
    m['cpos'] = np.ascontiguousarray(cpos.transpose(0, 2, 1))
    m['cw2'] = np.stack([f(inputs['cmp_k_w2'][0]), f(inputs['cmp_v_w2'][0])], 0)
    m['w_out'] = f(inputs['w_out'][0])
    m['xa_w'] = np.stack([f(inputs['xa_wq'][0]), f(inputs['xa_wk'][0]), f(inputs['xa_wv'][0]), f(inputs['xa_wo'][0])], 0)
    m['pwq'] = f(inputs['peer_wq'][0])
    sk = f(inputs['peer_subkeys'][0])
    skd = np.zeros((8, 128, 256), np.float32)
    for h in range(8):
        for pp in range(2):
            skd[h, pp * 64:(pp + 1) * 64, pp * 128:(pp + 1) * 128] = sk[h, pp].T
    m['skd'] = skd
    m['downT'] = np.ascontiguousarray(f(inputs['peer_down'][0]).T)
    m['up'] = f(inputs['peer_up'][0])
    m['c_ident'] = np.eye(128, dtype=np.float32)
    s_ = np.arange(128)[:, None]; t_ = np.arange(128)[None, :]
    m['c_ucs'] = np.where(s_ <= t_, -1.0 / 16, 0.0).astype(np.float32)
    m['c_urev'] = np.where(s_ > t_, -1.0 / 16, 0.0).astype(np.float32)
    m['c_causal'] = (s_ <= t_).astype(np.float32)
    m['c_selu'] = np.stack([np.eye(128) * (1 - p), np.eye(128) * p], 0).astype(np.float32)
    m['c_pv'] = np.tile(np.array([[1.0 - p, float(p)]], np.float32), (128, 1))
    kk = np.arange(128)[:, None]; qq = np.arange(128)[None, :]
    selb = np.empty((2, 128, 15, 512), np.float32); winb = np.empty((2, 128, 6, 512), np.float32)
    for g in range(2):
        for mm_ in range(15):
            j = mm_ - 1 + p
            dist = 128 * j + qq - kk
            selb[g, :, mm_, :] = _bias_tile(rel_bias, g, dist, (dist >= 0) & (j >= 0))
        for mm_ in range(6):
            j = mm_ - 1 + p
            dist = 128 * j + qq - kk
            winb[g, :, mm_, :] = _bias_tile(rel_bias, g, dist, (dist >= 0) & (dist < 512) & (j >= 0))
    m['c_selb'] = selb; m['c_winb'] = winb
    cmpb = np.empty((2, meta['npair'], 128, 512), np.float32)
    for jj in range(NO):
        for c in range(min(NCC, cmp_nchunks(jj))):
            n = 128 * c + kk
            t = (2 * jj + p) * 128 + qq
            dist = t - (16 * n + 31)
            for g in range(2):
                cmpb[g, meta['pair_off'][jj] + c] = _bias_tile(rel_bias, g, dist, (dist >= 0) & (n < NCMP))
    m['c_cmpb'] = cmpb
    far = np.empty((2, 128, 4, 128), np.float32)
    for g in range(2):
        for h in range(4):
            far[g, :, h, :] = rel_bias[31, g * 4 + h]
    m['c_far'] = far.reshape(2, 128, 512)
    wimp = np.zeros((NCC * 128, 128), np.float32)
    for s in range(NS):
        for r in range(-1, 4):
            n = 4 * s + r
            lo = 16 * r
            ov = min(lo + 32, 64) - max(lo, 0)
            if 0 <= n < NCMP:
                wimp[n, s] += ov / 32.0
    m['c_wimp'] = wimp.reshape(NCC, 128, 128)
    m12 = np.zeros((NO, 128, 2, 128), np.float32)
    sid = np.arange(128)[None, :]
    for jj in range(NO):
        t = (2 * jj + p) * 128 + np.arange(128)[:, None]
        cur = t // 64
        visible = (sid * 64 <= t) & (sid < NS)
        f0 = (sid == 0); f1 = (sid == cur); f2 = (sid == cur - 1)
        forced = f0 | f1 | f2
        m12[jj, :, 0, :] = (visible & ~forced)
        add = np.where(~visible, -100.0 - sid, 0.0)
        add = np.where(f2, 100.0, add); add = np.where(f1, 101.0, add); add = np.where(f0 & (sid < NS), 102.0, add)
        m12[jj, :, 1, :] = add
    m['c_m12'] = m12
    ex = np.zeros((128, T), np.float32)
    ex[np.arange(T) // 64, np.arange(T)] = 1.0
    m['c_ex'] = ex.astype(NPBF)
    return m


_CACHE = {}


def kernel(**inputs):
    T = inputs['x'].shape[1]
    B = inputs['x'].shape[0]
    if T not in _CACHE:
        _CACHE[T] = build(T)
    nc, meta = _CACHE[T]
    in_maps = []
    for c in range(2 * B):
        in_maps.append(make_core_inputs(inputs, T, c // 2, c % 2, meta))
    res = run_bass_kernel_spmd(nc, in_maps, core_ids=list(range(2 * B)))
    NO = T // 256
    outp = np.empty((B, T // 128, 128, D), np.float32)
    for c in range(2 * B):
        o = np.asarray(res.results[c]["out"], dtype=np.float32).reshape(NO, 128, D)
        outp[c // 2, (c % 2)::2] = o
    return outp.reshape(B, T, D)
```

```python
import math
import numpy as np
import ml_dtypes
import concourse.bass as bass
import concourse.mybir as mybir
from concourse.bass_utils import run_bass_kernel_spmd
from contextlib import ExitStack

F32 = mybir.dt.float32
BF16 = mybir.dt.bfloat16
AF = mybir.ActivationFunctionType
ALU = mybir.AluOpType
AX = mybir.AxisListType
NPBF = ml_dtypes.bfloat16

D = 1024
NEG = -30000.0


class T:
    def __init__(self, h, name):
        self.h = h
        self.name = name
        self.w = None
        self.r = []
        self.psum = False

    def __getitem__(self, k):
        return self.h[k]


class Prog:
    def __init__(self, nc, n_dma_sems=48):
        self.nc = nc
        self.es = ExitStack()
        self.scopes = []
        self.eng = {'pe': nc.tensor, 'dve': nc.vector, 'act': nc.scalar,
                    'pool': nc.gpsimd, 'sp': nc.sync}
        self.sems = {}
        for k in self.eng:
            self.sems['e_' + k] = self.es.enter_context(nc.semaphore('e_' + k))
        self.cnt = {k: 0 for k in self.sems}
        self.ndma = n_dma_sems
        for i in range(n_dma_sems):
            key = 'd_%d' % i
            self.sems[key] = self.es.enter_context(nc.semaphore(key))
            self.cnt[key] = 0
        self.dma_rr = 0
        self.known = {k: {} for k in self.eng}
        self.ntile = 0
        self.ninst = 0

    def push(self):
        self.scopes.append(ExitStack())

    def pop(self):
        self.barrier()
        self.scopes.pop().close()

    def _stack(self):
        return self.scopes[-1] if self.scopes else self.es

    def sb(self, shape, dt, name=None):
        self.ntile += 1
        name = (name or 't') + '_%d' % self.ntile
        h = self._stack().enter_context(self.nc.sbuf_tensor(name, list(shape), dt))
        return T(h, name)

    def ps(self, shape, dt, name=None):
        self.ntile += 1
        name = (name or 'p') + '_%d' % self.ntile
        h = self._stack().enter_context(self.nc.psum_tensor(name, list(shape), dt))
        t = T(h, name)
        t.psum = True
        return t

    def dram(self, name, shape, dt, kind="Internal"):
        h = self.nc.dram_tensor(name, list(shape), dt, kind=kind).ap()
        return T(h, name)

    def _deps(self, reads, writes, e=None):
        deps = []
        for t in reads:
            if t.w is not None:
                deps.append(t.w)
            if t.psum:
                deps.extend([tok for tok in t.r if tok[0] != 'e_' + str(e)])
        for t in writes:
            if t.w is not None:
                deps.append(t.w)
            deps.extend(t.r)
        return deps

    def _waits(self, e, deps):
        kn = self.known[e]
        need = {}
        for (k, v) in deps:
            if kn.get(k, 0) >= v:
                continue
            if need.get(k, 0) < v:
                need[k] = v
        for k, v in need.items():
            kn[k] = v
        return list(need.items())

    def _emit(self, e, waits, fn, inc):
        eng = self.eng[e]
        for (k, v) in waits:
            eng.wait_ge(self.sems[k], v)
        ins = fn(eng)
        ins.then_inc(self.sems[inc[0]], inc[1])
        self.ninst += 1

    def op(self, e, fn, reads=(), writes=()):
        deps = self._deps(reads, writes, e)
        key = 'e_' + e
        if e == 'pe':
            deps = [d for d in deps if d[0] != key]
        waits = self._waits(e, deps)
        self.cnt[key] += 1
        tok = (key, self.cnt[key])
        self._emit(e, waits, fn, (key, 1))
        for t in reads:
            t.r.append(tok)
        for t in writes:
            t.w = tok
            t.r = []
        return tok

    def dma(self, e, out_t, out_ap, in_t, in_ap, **kw):
        reads = [in_t]
        writes = [out_t]
        deps = self._deps(reads, writes)
        key = 'd_%d' % self.dma_rr
        self.dma_rr = (self.dma_rr + 1) % self.ndma
        if self.cnt[key] > 0:
            deps.append((key, self.cnt[key]))
        waits = self._waits(e, deps)
        self.cnt[key] += 16
        tok = (key, self.cnt[key])
        self._emit(e, waits, lambda eng: eng.dma_start(out=out_ap, in_=in_ap, **kw), (key, 16))
        in_t.r.append(tok)
        out_t.w = tok
        out_t.r = []
        return tok

    def barrier(self):
        allt = [(k, v) for k, v in self.cnt.items() if v > 0]
        for e in self.eng:
            for (k, v) in self._waits(e, allt):
                self.eng[e].wait_ge(self.sems[k], v)

    def finish(self):
        self.barrier()
        while self.scopes:
            self.scopes.pop().close()
        self.es.close()


ORIG = dict(gq=(0, 256), gk=(256, 512), gv=(512, 1024), gr=(1024, 1536), glr=(1536, 1552),
            nq=(1552, 2064), kc=(2064, 2192), vc=(2192, 2320), ks=(2320, 2448), vs=(2448, 2576),
            kw=(2576, 2704), vw=(2704, 2832), ng=(2832, 2856))
PERM_ORDER = ['gq', 'gk', 'gv', 'gr', 'nq', 'kc', 'vc', 'ks', 'vs', 'kw', 'vw', 'ng', 'glr']
NCOL = 2856


def cmp_nchunks(jj):
    return min(4, (16 * jj + 15 + 127) // 128)


def build(T, debug=False, upto=9, cut=99):
    NT = T // 128
    NO = NT // 2
    NS = T // 64
    TO = T // 2
    NCMP = (T - 32) // 16 + 1
    NCC = (NCMP + 127) // 128
    pair_off = []
    npair = 0
    for jj in range(NO):
        pair_off.append(npair)
        npair += min(NCC, cmp_nchunks(jj))

    nc = bass.Bass("TRN2", target_bir_lowering=False)
    P = Prog(nc)
    SK = "ExternalOutput" if debug else "Internal"

    def inp(name, shape, dt=F32):
        return P.dram(name, shape, dt, kind="ExternalInput")

    xb = inp("xb", [T, D]); xo = inp("xo", [TO, D]); memb = inp("mem", [256, D])
    w_in = inp("w_in", [D, NCOL]); gw2 = inp("gw2", [16, 256]); gb = inp("gb", [1, 256])
    gnorm = inp("gnorm", [1, 128])
    norms = inp("norms", [5, D])
    cw1 = inp("cw1", [2, 64, 32, 128]); cpos = inp("cpos", [2, 64, 32]); cw2 = inp("cw2", [2, 128, 64])
    w_out = inp("w_out", [D, D]); xa_w = inp("xa_w", [4, D, D])
    pwq = inp("pwq", [D, D]); skd = inp("skd", [8, 128, 256])
    downT = inp("downT", [D, 16384]); up = inp("up", [16384, D])
    c_ident = inp("c_ident", [128, 128]); c_ucs = inp("c_ucs", [128, 128]); c_urev = inp("c_urev", [128, 128])
    c_causal = inp("c_causal", [128, 128]); c_selu = inp("c_selu", [2, 128, 128]); c_pv = inp("c_pv", [128, 2])
    c_selb = inp("c_selb", [2, 128, 15, 512]); c_winb = inp("c_winb", [2, 128, 6, 512])
    c_cmpb = inp("c_cmpb", [2, npair, 128, 512])
    c_far = inp("c_far", [2, 128, 512])
    c_wimp = inp("c_wimp", [NCC, 128, 128]); c_m12 = inp("c_m12", [NO, 128, 2, 128])
    c_ex = inp("c_ex", [128, T], BF16)
    out = P.dram("out", [TO, D], F32, kind="ExternalOutput")

    d_nq = P.dram("d_nq", [T, 512], BF16, kind=SK)
    d_ng = P.dram("d_ng", [T, 24], F32, kind=SK)
    d_ogla = P.dram("d_ogla", [T, 512], BF16, kind=SK)
    d_kT = P.dram("d_kT", [4, 128, T], BF16, kind=SK)
    d_vs = P.dram("d_vs", [T, 128], BF16, kind=SK)
    d_vw = P.dram("d_vw", [T, 128], BF16, kind=SK)
    d_omix = P.dram("d_omix", [TO, D], BF16, kind=SK)
    d_h = P.dram("d_h", [TO, D], F32, kind=SK)
    d_hn3T = P.dram("d_hn3T", [128, 8, TO], BF16, kind=SK)
    d_route = P.dram("d_route", [TO, 2064], F32, kind=SK)
    d_downT = P.dram("d_downT", [D, 16384], BF16, kind="Internal")
    d_up = P.dram("d_up", [16384, D], BF16, kind="Internal")
    d_cmp = P.dram("d_cmp", [2, 64, 512], BF16, kind=SK)
    d_dbg = P.dram("d_dbg", [3, TO, 512], F32, kind=SK)
    d_imp = P.dram("d_imp", [2, TO, 128], F32, kind=SK)

    def mm(ot, o_ap, lt, l_ap, rt, r_ap, start=True, stop=True):
        P.op('pe', lambda e: e.matmul(o_ap, lhsT=l_ap, rhs=r_ap, start=start, stop=stop), [lt, rt], [ot])

    def tr(ot, o_ap, it, i_ap, idt):
        P.op('pe', lambda e: e.transpose(out=o_ap, in_=i_ap, identity=idt[:]), [it, idt], [ot])

    def act(ot, o_ap, it, i_ap, func, bias=None, scale=None, accum=None, rd=(), wr=()):
        kw = {}
        if bias is not None:
            kw['bias'] = bias
        if scale is not None:
            kw['scale'] = scale
        if accum is not None:
            kw['accum_out'] = accum
        P.op('act', lambda e: e.activation(out=o_ap, in_=i_ap, func=func, **kw), [it] + list(rd), [ot] + list(wr))

    def cp(eng, ot, o_ap, it, i_ap):
        if eng == 'act':
            P.op('act', lambda e: e.copy(out=o_ap, in_=i_ap), [it], [ot])
        else:
            P.op(eng, lambda e: e.tensor_copy(out=o_ap, in_=i_ap), [it], [ot])

    def tt(eng, ot, o_ap, at, a_ap, bt, b_ap, op):
        P.op(eng, lambda e: e.tensor_tensor(out=o_ap, in0=a_ap, in1=b_ap, op=op), [at, bt], [ot])

    def ts(eng, ot, o_ap, at, a_ap, s1, s2, op0, op1=None, rd=()):
        if op1 is None:
            P.op(eng, lambda e: e.tensor_scalar(out=o_ap, in0=a_ap, scalar1=s1, scalar2=None, op0=op0), [at] + list(rd), [ot])
        else:
            P.op(eng, lambda e: e.tensor_scalar(out=o_ap, in0=a_ap, scalar1=s1, scalar2=s2, op0=op0, op1=op1), [at] + list(rd), [ot])

    def stt(eng, ot, o_ap, at, a_ap, sc, bt, b_ap, op0, op1, rd=()):
        P.op(eng, lambda e: e.scalar_tensor_tensor(out=o_ap, in0=a_ap, scalar=sc, in1=b_ap, op0=op0, op1=op1),
             [at, bt] + list(rd), [ot])

    def memset(eng, t, ap, v):
        P.op(eng, lambda e: e.memset(ap, v), [], [t])

    def recip(ot, o_ap, it, i_ap):
        P.op('dve', lambda e: e.reciprocal(out=o_ap, in_=i_ap), [it], [ot])

    dmaq = ['sp', 'act', 'pool']
    dq = [0]

    def dma(ot, o_ap, it, i_ap, q=None, **kw):
        if q is None:
            q = dmaq[dq[0] % 2]
            dq[0] += 1
        return P.dma(q, ot, o_ap, it, i_ap, **kw)

    ident = P.sb([128, 128], BF16, "ident"); identf = P.sb([128, 128], F32, "identf")
    ones = P.sb([128, 128], BF16, "ones")
    pv = P.sb([128, 2], F32, "pv")
    nrm = P.sb([128, 5, D], F32, "nrm")
    dma(identf, identf[:], c_ident, c_ident[:])
    dma(pv, pv[:], c_pv, c_pv[:])
    for k in range(5):
        dma(nrm, nrm[:, k, :], norms, norms[k:k + 1, :].partition_broadcast(128))
    cp('dve', ident, ident[:], identf, identf[:])
    memset('pool', ones, ones[:], 1.0)
    eps_t = P.sb([128, 1], F32, "eps_t")
    memset('dve', eps_t, eps_t[:], 1e-6)
    kcmpT = [P.sb([64, 512], BF16, "kcmpT%d" % g) for g in range(2)]
    vcmp = [P.sb([128, 4, 65], BF16, "vcmp%d" % g) for g in range(2)]

    def rmsnorm(xt, x_ap, gain_k, hn, hn_ap, junk, ss, rstd, n=D, eps=1e-6):
        memset('dve', ss, ss[:], 0.0)
        act(junk, junk[:, 0:n], xt, x_ap, AF.Square, accum=ss[:], rd=[ss], wr=[ss])
        act(rstd, rstd[:], ss, ss[:], AF.Ln, scale=1.0 / n, bias=eps_t[:, 0:1], rd=[eps_t])
        act(rstd, rstd[:], rstd, rstd[:], AF.Exp, scale=-0.5)
        stt('dve', hn, hn_ap, xt, x_ap, rstd[:, 0:1], nrm, nrm[:, gain_k, 0:n], ALU.mult, ALU.mult, rd=[rstd])

    if upto >= 1:
        P.push()
        winb = P.sb([128, 8, NCOL], BF16, "winb")
        wst = [P.sb([128, NCOL], F32, "wst%d" % i) for i in range(2)]
        w_in_v = w_in[:].rearrange("(c p) n -> c p n", p=128)
        for c in range(8):
            dma(wst[c % 2], wst[c % 2][:], w_in, w_in_v[c])
            cp(['dve', 'pool'][c % 2], winb, winb[:, c, :], wst[c % 2], wst[c % 2][:])
        gw2t = P.sb([16, 256], F32, "gw2t"); gbt = P.sb([1, 256], F32, "gbt"); onesrow = P.sb([1, 128], F32, "onesrow")
        gnt = P.sb([128, 128], F32, "gnt")
        ucs = P.sb([128, 128], F32, "ucs"); urev = P.sb([128, 128], F32, "urev"); causal = P.sb([128, 128], F32, "causal")
        m16 = P.sb([128, 1], F32, "m16")
        dma(gw2t, gw2t[:], gw2, gw2[:]); dma(gbt, gbt[:], gb, gb[:])
        dma(gnt, gnt[:], gnorm, gnorm[0:1, :].partition_broadcast(128))
        dma(ucs, ucs[:], c_ucs, c_ucs[:]); dma(urev, urev[:], c_urev, c_urev[:]); dma(causal, causal[:], c_causal, c_causal[:])
        memset('dve', onesrow, onesrow[:], 1.0)
        memset('dve', m16, m16[:], -1.0 / 16)
        S = P.sb([128, 2, 128], F32, "S"); Sb = P.sb([128, 2, 128], BF16, "Sb")
        memset('dve', S, S[:], 0.0); memset('pool', Sb, Sb[:], 0.0)

        xt = [P.sb([128, D], F32, "xt%d" % i) for i in range(2)]
        junk = P.sb([128, D], BF16, "junk"); ss = P.sb([128, 1], F32, "ss"); rstd = P.sb([128, 1], F32, "rstd")
        hn = P.sb([128, D], BF16, "hn"); hnT = P.sb([128, 8, 128], BF16, "hnT")
        pt = P.ps([128, 8, 128], BF16, "pt")
        pz = [P.ps([128, 512], F32, "pz%d" % i) for i in range(3)]
        pg1 = P.ps([128, 512], F32, "pg1"); pg2 = P.ps([128, 512], F32, "pg2")
        po = P.ps([128, 512], F32, "po"); ptb = P.ps([128, 8, 128], BF16, "ptb")
        glr = P.sb([128, 16], F32, "glr"); glrT = P.sb([16, 128], F32, "glrT")
        e1 = P.sb([128, 256], F32, "e1"); L = P.sb([128, 256], F32, "L")
        eb = P.sb([128, 256], F32, "eb"); enb = P.sb([128, 256], F32, "enb"); ec = P.sb([128, 256], F32, "ec")
        ebl = P.sb([128, 2], F32, "ebl")
        qk = P.sb([128, 3, 256], BF16, "qk")
        qkT = P.sb([128, 4, 128], BF16, "qkT")
        vv = P.sb([128, 512], BF16, "vv"); sg = P.sb([128, 512], F32, "sg")
        AT = P.sb([128, 128], BF16, "AT")
        ssq = P.sb([128, 4], F32, "ssq"); rs4 = P.sb([128, 4], F32, "rs4"); tmpn = P.sb([128, 128], F32, "tmpn")
        junk2 = P.sb([128, 128], F32, "junk2")
        og = P.sb([128, 512], BF16, "og"); otmp4 = P.sb([128, 512], F32, "otmp4")
        nqs = P.sb([128, 512], BF16, "nqs"); ngs = P.sb([128, 24], F32, "ngs")
        kk = P.sb([128, 4, 128], BF16, "kk"); kkT = P.sb([128, 4, 128], BF16, "kkT")
        vsw = P.sb([128, 2, 128], BF16, "vsw")
        cast_jobs = []
        if upto >= 4:
            stg = [P.sb([128, 4096], F32, "stg%d" % i) for i in range(2)]
            stb = [P.sb([128, 4096], BF16, "stb%d" % i) for i in range(2)]
            for r in range(8):
                for c in range(4):
                    cast_jobs.append((downT, downT[r * 128:(r + 1) * 128, c * 4096:(c + 1) * 4096],
                                      d_downT, d_downT[r * 128:(r + 1) * 128, c * 4096:(c + 1) * 4096]))
            sv = up[:].rearrange("(a p f) d -> a p (f d)", p=128, f=4)
            dv = d_up[:].rearrange("(a p f) d -> a p (f d)", p=128, f=4)
            for a in range(32):
                cast_jobs.append((up, sv[a], d_up, dv[a]))
        cj = [0]

        def cast_job():
            if cj[0] >= len(cast_jobs):
                return
            src_t, s_ap, dst_t, d_ap = cast_jobs[cj[0]]
            a, b_ = stg[cj[0] % 2], stb[cj[0] % 2]
            P.dma('pool', a, a[:], src_t, s_ap)
            for q_ in range(4):
                cp('act', b_, b_[:, q_ * 1024:(q_ + 1) * 1024], a, a[:, q_ * 1024:(q_ + 1) * 1024])
            P.dma('pool', dst_t, d_ap, b_, b_[:])
            cj[0] += 1
        cA, cB, cC, cD, cE, cF = 0, 512, 1024, 1536, 2048, 2560
        for i in range(NT):
            x_t = xt[i % 2]
            dma(x_t, x_t[:], xb, xb[i * 128:(i + 1) * 128, :])
            rmsnorm(x_t, x_t[:], 0, hn, hn[:], junk, ss, rstd)
            for c in range(8):
                tr(pt, pt[:, c, :], hn, hn[:, c * 128:(c + 1) * 128], ident)
            cp('act', hnT, hnT[:], pt, pt[:])

            def zgroup(pz_t, c0, n):
                for c in range(8):
                    mm(pz_t, pz_t[:, 0:n], hnT, hnT[:, c, :], winb, winb[:, c, c0:c0 + n], start=(c == 0), stop=(c == 7))
            if cut <= 1:
                continue
            zgroup(pz[0], cF, 296)
            cp('act', kk, kk[:, 3, :], pz[0], pz[0][:, 0:128])
            cp('act', vsw, vsw[:, 1, :], pz[0], pz[0][:, 128:256])
            if cut <= 1.05:
                continue
            cp('act', glr, glr[:], pz[0], pz[0][:, 280:296])
            if cut <= 1.07:
                continue
            act(ngs, ngs[:], pz[0], pz[0][:, 256:280], AF.Exp, scale=-1.0)
            if cut <= 1.1:
                continue
            ts('dve', ngs, ngs[:], ngs, ngs[:], 1.0, None, ALU.add)
            recip(ngs, ngs[:], ngs, ngs[:])
            dma(d_ng, d_ng[i * 128:(i + 1) * 128, :], ngs, ngs[:])
            if cut <= 1.2:
                continue
            zgroup(pz[1], cE, 512)
            cp('act', kk, kk[:, 0:3, :], pz[1], pz[1][:, 0:384].rearrange("p (a b) -> p a b", a=3))
            cp('dve', vsw, vsw[:, 0, :], pz[1], pz[1][:, 384:512])
            dma(d_vs, d_vs[i * 128:(i + 1) * 128, :], vsw, vsw[:, 0, :])
            dma(d_vw, d_vw[i * 128:(i + 1) * 128, :], vsw, vsw[:, 1, :])
            if cut <= 1.4:
                continue
            for a in range(4):
                tr(ptb, ptb[:, a, :], kk, kk[:, a, :], ident)
            cp('act', kkT, kkT[:], ptb, ptb[:, 0:4, :])
            dma(d_kT, d_kT[:, :, i * 128:(i + 1) * 128].rearrange("a p t -> p a t"), kkT, kkT[:])
            if cut <= 1.6:
                continue
            zgroup(pz[2], cD, 512)
            P.op('act', lambda e, pzt=pz[2]: e.mul(out=nqs[:], in_=pzt[:], mul=0.125), [pz[2]], [nqs])
            dma(d_nq, d_nq[i * 128:(i + 1) * 128, :], nqs, nqs[:])
            if cut <= 2:
                continue
            tr(pg1, pg1[0:16, 0:128], glr, glr[:], identf)
            cp('dve', glrT, glrT[:], pg1, pg1[0:16, 0:128])
            mm(pg1, pg1[:, 256:512], glrT, glrT[:], gw2t, gw2t[:], start=True, stop=False)
            mm(pg1, pg1[:, 256:512], onesrow, onesrow[:], gbt, gbt[:], start=False, stop=True)
            zgroup(pz[0], cA, 512)
            zgroup(pz[1], cB, 512)
            zgroup(pz[2], cC, 512)
            act(e1, e1[:], pg1, pg1[:, 256:512], AF.Exp, scale=-1.0)
            act(L, L[:], e1, e1[:], AF.Ln, bias=1.0)
            cp('act', vv, vv[:], pz[1], pz[1][:])
            act(sg, sg[:], pz[2], pz[2][:], AF.Exp, scale=-1.0)
            ts('dve', sg, sg[:], sg, sg[:], 1.0, None, ALU.add)
            recip(sg, sg[:], sg, sg[:])
            tt('dve', sg, sg[:], sg, sg[:], pz[2], pz[2][:], ALU.mult)
            tt('pool', sg, sg[:].rearrange("p (h d) -> p h d", h=4), sg, sg[:].rearrange("p (h d) -> p h d", h=4),
               gnt, gnt[:].unsqueeze(1).to_broadcast([128, 4, 128]), ALU.mult)
            if cut <= 3:
                continue
            mm(pg2, pg2[:, 0:256], ucs, ucs[:], L, L[:])
            mm(pg2, pg2[:, 256:512], urev, urev[:], L, L[:])
            for hp in range(2):
                mm(pg1, pg1[:, hp:hp + 1], L, L[:, hp * 128:(hp + 1) * 128], m16, m16[:])
            act(eb, eb[:], pg2, pg2[:, 0:256], AF.Exp)
            act(enb, enb[:], pg2, pg2[:, 0:256], AF.Exp, scale=-1.0)
            act(ec, ec[:], pg2, pg2[:, 256:512], AF.Exp)
            act(ebl, ebl[:], pg1, pg1[:, 0:2], AF.Exp)
            if cut <= 4:
                continue
            stt('dve', qk, qk[:, 0, :], pz[0], pz[0][:, 0:256], 0.125, eb, eb[:], ALU.mult, ALU.mult)
            tt('dve', qk, qk[:, 1, :], pz[0], pz[0][:, 256:512], enb, enb[:], ALU.mult)
            tt('dve', qk, qk[:, 2, :], pz[0], pz[0][:, 256:512], ec, ec[:], ALU.mult)
            for a in range(4):
                tr(ptb, ptb[:, 4 + a, :], qk, qk[:, a // 2, (a % 2) * 128:(a % 2 + 1) * 128], ident)
            cp('act', qkT, qkT[:], ptb, ptb[:, 4:8, :])
            if cut <= 6:
                continue
            memset('dve', ssq, ssq[:], 0.0)
            for h in range(4):
                hp, hh = h // 2, h % 2
                pr = slice(hh * 64, hh * 64 + 64)
                mm(pg2, pg2[:, 0:128], qkT, qkT[pr, 2 + hp, :], qkT, qkT[pr, hp, :])
                tt('dve', AT, AT[:], pg2, pg2[:, 0:128], causal, causal[:], ALU.mult)
                o_ap = po[:, h * 128:(h + 1) * 128]
                mm(po, o_ap, AT, AT[:], vv, vv[:, h * 128:(h + 1) * 128], start=True, stop=False)
                mm(po, o_ap, qkT, qkT[pr, hp, :], Sb, Sb[pr, hp, :], start=False, stop=True)
                mm(pg2, pg2[:, 128:256], qk, qk[:, 2, hp * 128:(hp + 1) * 128], vv, vv[:, h * 128:(h + 1) * 128])
                stt('dve', Sb, Sb[pr, hp, :], S, S[pr, hp, :], ebl[pr, hp:hp + 1], pg2, pg2[pr, 128:256], ALU.mult, ALU.add, rd=[ebl])
                stt('dve', S, S[pr, hp, :], S, S[pr, hp, :], ebl[pr, hp:hp + 1], pg2, pg2[pr, 128:256], ALU.mult, ALU.add, rd=[ebl])
                act(junk2, junk2[:], po, o_ap, AF.Square, accum=ssq[:, h:h + 1], rd=[ssq], wr=[ssq])
            act(rs4, rs4[:], ssq, ssq[:], AF.Ln, scale=1.0 / 128, bias=eps_t[:, 0:1], rd=[eps_t])
            act(rs4, rs4[:], rs4, rs4[:], AF.Exp, scale=-0.5)
            tt('dve', otmp4, otmp4[:], po, po[:], sg, sg[:], ALU.mult)
            tt('dve', og, og[:].rearrange("p (h d) -> p h d", h=4), otmp4, otmp4[:].rearrange("p (h d) -> p h d", h=4),
               rs4, rs4[:].unsqueeze(2).to_broadcast([128, 4, 128]), ALU.mult)
            dma(d_ogla, d_ogla[i * 128:(i + 1) * 128, :], og, og[:])
            for _ in range((len(cast_jobs) + NT - 1) // NT):
                cast_job()
        while cj[0] < len(cast_jobs):
            cast_job()
        P.pop()

        P.push()
        w1f = P.sb([64, 32, 128], F32, "w1f"); w1b = P.sb([64, 32, 128], BF16, "w1b")
        posf = P.sb([64, 32], F32, "posf"); posb = P.sb([64, 32], BF16, "posb")
        w2f = P.sb([128, 64], F32, "w2f"); w2b = P.sb([128, 64], BF16, "w2b")
        kTg = P.sb([64, T], BF16, "kTg")
        ph = P.ps([128, 512], F32, "ph"); pb = P.ps([128, 512], F32, "pb"); pc = P.ps([128, 512], F32, "pc")
        bias_h = P.sb([128, 1], F32, "bias_h")
        H = P.sb([128, 512], BF16, "H")
        for g in range(2):
            memset('dve', kcmpT[g], kcmpT[g][:], 0.0)
            memset('dve', vcmp[g], vcmp[g][:], 0.0)
            memset('dve', vcmp[g], vcmp[g][:, :, 64:65], 1.0)
        for kv in range(2):
            dma(w1f, w1f[:], cw1, cw1[kv]); dma(posf, posf[:], cpos, cpos[kv]); dma(w2f, w2f[:], cw2, cw2[kv])
            cp('dve', w1b, w1b[:], w1f, w1f[:]); cp('dve', posb, posb[:], posf, posf[:]); cp('dve', w2b, w2b[:], w2f, w2f[:])
            for l in range(32):
                mm(pb, pb[:, 0:1], w1b, w1b[:, l, :], posb, posb[:, l:l + 1], start=(l == 0), stop=(l == 31))
            cp('dve', bias_h, bias_h[:], pb, pb[:, 0:1])
            for g in range(2):
                dma(kTg, kTg[:], d_kT, d_kT[kv, g * 64:(g + 1) * 64, :])
                for l in range(32):
                    mm(ph, ph[:, 0:NCMP], w1b, w1b[:, l, :], kTg, kTg[:, l:l + 16 * (NCMP - 1) + 1:16],
                       start=(l == 0), stop=(l == 31))
                memset('dve', H, H[:], 0.0)
                act(H, H[:, 0:NCMP], ph, ph[:, 0:NCMP], AF.Gelu_apprx_tanh, bias=bias_h[:, 0:1], rd=[bias_h])
                if kv == 0:
                    mm(pc, pc[0:64, 0:NCMP], w2b, w2b[:], H, H[:, 0:NCMP])
                    cp('act', kcmpT[g], kcmpT[g][:, 0:NCMP], pc, pc[0:64, 0:NCMP])
                    if debug:
                        dma(d_cmp, d_cmp[g], kcmpT[g], kcmpT[g][:])
                else:
                    for c in range(NCC):
                        mm(pc, pc[:, c * 64:(c + 1) * 64], H, H[:, c * 128:(c + 1) * 128], w2b, w2b[:])
                    cp('act', vcmp[g], vcmp[g][:, 0:NCC, 0:64], pc, pc[:, 0:NCC * 64].rearrange("p (c d) -> p c d", d=64))
        P.pop()

    if upto >= 2:
        P.push()
        selu = P.sb([128, 2, 128], BF16, "selu"); seluf = P.sb([128, 2, 128], F32, "seluf")
        dma(seluf, seluf[:], c_selu, c_selu[:].rearrange("a p t -> p a t"))
        cp('dve', selu, selu[:], seluf, seluf[:])
        exm = P.sb([128, T], BF16, "exm")
        dma(exm, exm[:], c_ex, c_ex[:])
        wimpf = P.sb([128, NCC, 128], F32, "wimpf"); wimp = P.sb([128, NCC, 128], BF16, "wimp")
        dma(wimpf, wimpf[:], c_wimp, c_wimp[:].rearrange("c p s -> p c s"))
        cp('dve', wimp, wimp[:], wimpf, wimpf[:])
        ksT = P.sb([65, T], BF16, "ksT"); kwT = P.sb([64, T], BF16, "kwT")
        memset('dve', ksT, ksT[64:65, :], 1.0)
        farf = P.sb([128, 512], F32, "farf")
        vs = P.sb([128, NT, 65], BF16, "vs"); vw = P.sb([128, NT, 65], BF16, "vw")
        selb = P.sb([128, 15, 512], BF16, "selb"); winb2 = P.sb([128, 6, 512], BF16, "winb2")
        bst = [P.sb([128, 512], F32, "bst%d" % i) for i in range(2)]
        cbt = [P.sb([128, 512], BF16, "cbt%d" % i) for i in range(2)]
        qrows_ = [P.sb([128, 2, 256], BF16, "qrows%d" % i) for i in range(2)]; grows_ = [P.sb([128, 2, 24], F32, "grows%d" % i) for i in range(2)]
        gown_ = [P.sb([128, 12], F32, "gown%d" % i) for i in range(2)]; gtmp = P.sb([128, 12], F32, "gtmp")
        qT_ = [P.sb([65, 4, 128], BF16, "qT%d" % i) for i in range(2)]
        Ec = [P.sb([128, 4, 128], BF16, "Ec%d" % i) for i in range(4)]
        Eb = [P.sb([128, 4, 128], BF16, "Eb%d" % i) for i in range(3)]
        m12_ = [P.sb([128, 2, 128], F32, "m12_%d" % i) for i in range(2)]
        imp = P.sb([128, 128], F32, "imp"); imp2 = P.sb([128, 128], F32, "imp2"); m8 = P.sb([128, 16], F32, "m8")
        mk = P.sb([128, 128], BF16, "mk"); maskT4 = P.sb([128, 4, 128], BF16, "maskT4")
        rden = P.sb([128, 4], F32, "rden"); coef = P.sb([128, 4], F32, "coef")
        oacc = P.sb([128, 4, 64], F32, "oacc"); otmp = P.sb([128, 4, 64], F32, "otmp"); onsa = P.sb([128, 256], BF16, "onsa")
        pq = P.ps([64, 4, 128], F32, "pq")
        psc = [P.ps([128, 4, 128], F32, "psc%d" % i) for i in range(2)]
        pn = P.ps([128, 4, 128], F32, "pn")
        pnT = P.ps([65, 4, 128], F32, "pnT")
        pnT2 = P.ps([65, 4, 128], F32, "pnT2")
        numTs = P.sb([65, 4, 128], F32, "numTs")
        pimp = P.ps([128, 4, 128], F32, "pimp")
        pmt = P.ps([128, 128], BF16, "pmt")
        for g in range(2):
            dma(ksT, ksT[0:64, :], d_kT, d_kT[2, g * 64:(g + 1) * 64, :])
            dma(farf, farf[:], c_far, c_far[g])
            for i_ in range(2):
                cp('dve', qT_[i_], qT_[i_][64:65, :, :], farf, farf[64:65, :].rearrange("p (h q) -> p h q", h=4))
            dma(kwT, kwT[:], d_kT, d_kT[3, g * 64:(g + 1) * 64, :])
            for n0 in range(0, NT, 16):
                n1 = min(NT, n0 + 16)
                dma(vs, vs[:, n0:n1, 0:64], d_vs, d_vs[n0 * 128:n1 * 128, g * 64:(g + 1) * 64].rearrange("(n p) c -> p n c", p=128))
                dma(vw, vw[:, n0:n1, 0:64], d_vw, d_vw[n0 * 128:n1 * 128, g * 64:(g + 1) * 64].rearrange("(n p) c -> p n c", p=128))
            memset('dve', vs, vs[:, :, 64:65], 1.0)
            memset('dve', vw, vw[:, :, 64:65], 1.0)
            k = 0
            for m in range(15):
                dma(bst[k % 2], bst[k % 2][:], c_selb, c_selb[g, :, m, :])
                tt('pool', selb, selb[:, m, :], bst[k % 2], bst[k % 2][:], farf, farf[:], ALU.subtract); k += 1
            for m in range(6):
                dma(bst[k % 2], bst[k % 2][:], c_winb, c_winb[g, :, m, :])
                cp('pool', winb2, winb2[:, m, :], bst[k % 2], bst[k % 2][:]); k += 1
            ne = 0
            ne_ = [0]
            for jj in range(NO):
                qrows, grows, gown, qT, m12 = qrows_[jj % 2], grows_[jj % 2], gown_[jj % 2], qT_[jj % 2], m12_[jj % 2]
                dma(qrows, qrows[:], d_nq, d_nq[jj * 256:(jj + 1) * 256, g * 256:(g + 1) * 256].rearrange("(u p) c -> p u c", p=128))
                dma(grows, grows[:], d_ng, d_ng[jj * 256:(jj + 1) * 256, :].rearrange("(u p) c -> p u c", p=128))
                dma(m12, m12[:], c_m12, c_m12[jj])
                for h in range(4):
                    for u in range(2):
                        mm(pq, pq[:, h, :], qrows, qrows[:, u, h * 64:(h + 1) * 64], selu, selu[:, u, :], start=(u == 0), stop=(u == 1))
                cp('act', qT, qT[0:64], pq, pq[:])
                ts('dve', gtmp, gtmp[:], grows, grows[:, 0, g * 12:(g + 1) * 12], pv[:, 0:1], None, ALU.mult, rd=[pv])
                stt('dve', gown, gown[:], grows, grows[:, 1, g * 12:(g + 1) * 12], pv[:, 1:2], gtmp, gtmp[:], ALU.mult, ALU.add, rd=[pv])
                gv3 = gown[:].rearrange("p (h k) -> p h k", k=3)
                qT_all = qT[0:64].rearrange("p h q -> p (h q)")
                qT_aug = qT[:].rearrange("p h q -> p (h q)")

                def finish_branch(pn, br, first):
                    ts('dve', rden, rden[:], pn, pn[:, :, 64], 1e-30, None, ALU.max)
                    recip(rden, rden[:], rden, rden[:])
                    tt('dve', coef, coef[:], rden, rden[:], gown, gv3[:, :, br], ALU.mult)
                    dst = oacc if first else otmp
                    tt('dve', dst, dst[:], pn, pn[:, :, 0:64], coef, coef[:].unsqueeze(2).to_broadcast([128, 4, 64]), ALU.mult)
                    if not first:
                        tt('pool', oacc, oacc[:], oacc, oacc[:], otmp, otmp[:], ALU.add)
                    if debug:
                        dma(d_dbg, d_dbg[br, jj * 128:(jj + 1) * 128, g * 256:(g + 1) * 256], oacc, oacc[:].rearrange("p h d -> p (h d)"))

                def back_T(src):
                    cp('act', numTs, numTs[:], src, src[:])
                    for h in range(4):
                        P.op('pe', lambda e, h=h: e.transpose(out=pn[:, h, 0:65], in_=numTs[:, h, :], identity=identf[0:65, 0:65]), [numTs, identf], [pn])

                ncc = min(NCC, cmp_nchunks(jj))
                for c in range(ncc):
                    pi = pair_off[jj] + c
                    dma(bst[k % 2], bst[k % 2][:], c_cmpb, c_cmpb[g, pi])
                    cp('pool', cbt[k % 2], cbt[k % 2][:], bst[k % 2], bst[k % 2][:])
                    sc = psc[ne % 2]; ne += 1
                    sc_all = sc[:].rearrange("p h q -> p (h q)")
                    mm(sc, sc_all, kcmpT[g], kcmpT[g][:, c * 128:(c + 1) * 128], qT, qT_all, start=True, stop=False)
                    mm(sc, sc_all, ident, ident[:], cbt[k % 2], cbt[k % 2][:], start=False, stop=True)
                    k += 1
                    act(Ec[c], Ec[c][:], sc, sc[:], AF.Exp)
                for h in range(4):
                    for c in range(ncc):
                        mm(pn, pn[:, h, 0:65], Ec[c], Ec[c][:, h, :], vcmp[g], vcmp[g][:, c, :], start=(c == 0), stop=(c == ncc - 1))
                    for c in range(ncc):
                        mm(pimp, pimp[:, h, :], Ec[c], Ec[c][:, h, :], wimp, wimp[:, c, :], start=(c == 0), stop=(c == ncc - 1))
                nk = 2 * jj + 2
                k0 = max(0, 2 * jj - 4)
                bufs = {}

                def score(kind, kc, n):
                    sc = psc[ne_[0] % 2]; E = Eb[ne_[0] % 3]; ne_[0] += 1
                    bufs[n] = E
                    sc_all = sc[:].rearrange("p h q -> p (h q)")
                    if kind == 's':
                        m = 2 * jj + 1 - kc
                        mm(sc, sc_all, ksT, ksT[:, kc * 128:(kc + 1) * 128], qT, qT_aug, start=True, stop=False)
                        if m < 14:
                            mm(sc, sc_all, ident, ident[:], selb, selb[:, m, :], start=False, stop=False)
                        mm(sc, sc_all, exm, exm[:, kc * 128:(kc + 1) * 128], maskT4, mT_all, start=False, stop=True)
                    else:
                        m = 2 * jj + 1 - kc
                        mm(sc, sc_all, kwT, kwT[:, kc * 128:(kc + 1) * 128], qT, qT_all, start=True, stop=False)
                        mm(sc, sc_all, ident, ident[:], winb2, winb2[:, m, :], start=False, stop=True)
                    act(E, E[:], sc, sc[:], AF.Exp)

                def pvs(kind, kc, n):
                    E = bufs.pop(n)
                    if kind == 's':
                        mm(pnT, pnT[:].rearrange("p h q -> p (h q)"), vs, vs[:, kc, :], E, E[:].rearrange("p h q -> p (h q)"), start=(kc == 0), stop=(kc == nk - 1))
                    else:
                        mm(pnT2, pnT2[:].rearrange("p h q -> p (h q)"), vw, vw[:, kc, :], E, E[:].rearrange("p h q -> p (h q)"), start=(kc == k0), stop=(kc == nk - 1))

                def run_items(items):
                    NI = len(items)
                    for n in range(NI + 1):
                        if n < NI:
                            score(items[n][0], items[n][1], n)
                        if n >= 1:
                            pvs(items[n - 1][0], items[n - 1][1], n - 1)
                mT_all = maskT4[:].rearrange("p h q -> p (h q)")
                run_items([('w', kc) for kc in range(k0, nk)])
                finish_branch(pn, 0, True)
                for h in range(4):
                    if h == 0:
                        ts('dve', imp, imp[:], pimp, pimp[:, 0, :], rden[:, 0:1], None, ALU.mult, rd=[rden])
                    else:
                        stt('dve', imp, imp[:], pimp, pimp[:, h, :], rden[:, h:h + 1], imp, imp[:], ALU.mult, ALU.add, rd=[rden])
                tt('dve', imp, imp[:], imp, imp[:], m12, m12[:, 0, :], ALU.mult)
                tt('dve', imp, imp[:], imp, imp[:], m12, m12[:, 1, :], ALU.add)
                if debug:
                    dma(d_imp, d_imp[g, jj * 128:(jj + 1) * 128, :], imp, imp[:])
                P.op('dve', lambda e: e.max(out=m8[:, 0:8], in_=imp[:]), [imp], [m8])
                P.op('dve', lambda e: e.match_replace(out=imp2[:], in_to_replace=m8[:, 0:8], in_values=imp[:], imm_value=-1e30), [imp, m8], [imp2])
                P.op('dve', lambda e: e.max(out=m8[:, 8:16], in_=imp2[:]), [imp2], [m8])
                ts('dve', mk, mk[:], imp, imp[:], m8[:, 15:16], 1.0, ALU.is_ge, ALU.subtract, rd=[m8])
                tr(pmt, pmt[:], mk, mk[:], ident)
                P.op('act', lambda e: e.mul(out=maskT4[:], in_=pmt[:].unsqueeze(1).to_broadcast([128, 4, 128]), mul=30000.0), [pmt], [maskT4])
                run_items([('s', kc) for kc in range(nk)])
                back_T(pnT)
                finish_branch(pn, 1, False)
                back_T(pnT2)
                finish_branch(pn, 2, False)
                cp('act', onsa, onsa[:], oacc, oacc[:].rearrange("p h d -> p (h d)"))
                dma(d_omix, d_omix[jj * 128:(jj + 1) * 128, 512 + g * 256:512 + (g + 1) * 256], onsa, onsa[:])
        P.pop()

    if upto >= 3:
        P.push()
        woutb = P.sb([128, 8, D], BF16, "woutb"); wqb = P.sb([128, 8, D], BF16, "wqb"); wob = P.sb([128, 8, D], BF16, "wob")
        pwqb = P.sb([128, 8, D], BF16, "pwqb")
        skb = P.sb([128, 8, 256], BF16, "skb")
        kTm = P.sb([128, 8, 256], BF16, "kTm")
        vm = P.sb([128, 2, 4, 257], BF16, "vm")
        xt = [P.sb([128, D], F32, "x3_%d" % i) for i in range(2)]
        junk = P.sb([128, D], BF16, "junk3"); ss = P.sb([128, 1], F32, "ss3"); rstd = P.sb([128, 1], F32, "rstd3")
        hn = P.sb([128, D], BF16, "hn3"); hnT = P.sb([128, 8, 128], BF16, "hnT3")
        pt = P.ps([128, 8, 128], BF16, "pt3")
        pa = [P.ps([128, 512], F32, "pa%d" % i) for i in range(4)]
        pxa = P.ps([128, 2, 512], F32, "pxa")
        P.push()
        wst = [P.sb([128, D], F32, "wst3_%d" % i) for i in range(2)]
        wtmp = P.sb([128, 8, D], BF16, "wtmp"); skf = P.sb([128, 8, 256], F32, "skf")
        memT = P.sb([128, 8, 256], BF16, "memT")
        k = [0]

        def load_w(dst, src_t, src_ap3):
            for c in range(8):
                a = wst[k[0] % 2]
                dma(a, a[:], src_t, src_ap3[c])
                cp(['dve', 'pool'][k[0] % 2], dst, dst[:, c, :], a, a[:]); k[0] += 1
        load_w(woutb, w_out, w_out[:].rearrange("(c p) n -> c p n", p=128))
        load_w(wqb, xa_w, xa_w[0].rearrange("(c p) n -> c p n", p=128))
        load_w(wob, xa_w, xa_w[3].rearrange("(c p) n -> c p n", p=128))
        load_w(pwqb, pwq, pwq[:].rearrange("(c p) n -> c p n", p=128))
        dma(skf, skf[:], skd, skd[:].rearrange("c p n -> p c n"))
        cp('dve', skb, skb[:], skf, skf[:])
        memset('dve', vm, vm[:, :, :, 256:257], 1.0)
        for mc in range(2):
            x_t = xt[mc % 2]
            dma(x_t, x_t[:], memb, memb[mc * 128:(mc + 1) * 128, :])
            rmsnorm(x_t, x_t[:], 2, hn, hn[:], junk, ss, rstd)
            for c in range(8):
                tr(pt, pt[:, c, :], hn, hn[:, c * 128:(c + 1) * 128], ident)
            cp('act', memT, memT[:, :, mc * 128:(mc + 1) * 128], pt, pt[:])
        load_w(wtmp, xa_w, xa_w[1].rearrange("(c p) n -> c p n", p=128))
        for oc in range(8):
            for c in range(8):
                mm(pa[0], pa[0][:, 0:256], wtmp, wtmp[:, c, oc * 128:(oc + 1) * 128], memT, memT[:, c, :], start=(c == 0), stop=(c == 7))
            cp('act', kTm, kTm[:, oc, :], pa[0], pa[0][:, 0:256])
        load_w(wtmp, xa_w, xa_w[2].rearrange("(c p) n -> c p n", p=128))
        for mc in range(2):
            for half in range(2):
                for c in range(8):
                    mm(pa[half], pa[half][:], memT, memT[:, c, mc * 128:(mc + 1) * 128], wtmp, wtmp[:, c, half * 512:(half + 1) * 512], start=(c == 0), stop=(c == 7))
                cp('act', vm, vm[:, mc, half * 2:half * 2 + 2, 0:256], pa[half], pa[half][:].rearrange("p (h d) -> p h d", d=256))
        P.pop()
        og2 = P.sb([128, 2, 512], BF16, "og2"); omx = P.sb([128, D], BF16, "omx"); otm = P.sb([128, 512], F32, "otm")
        h1 = P.sb([128, D], F32, "h1"); qTx = P.sb([128, 8, 128], BF16, "qTx")
        Ex = P.sb([128, 2, 4, 128], BF16, "Ex"); rdx = P.sb([128, 4], F32, "rdx")
        oxa = P.sb([128, D], BF16, "oxa")
        hn3T = P.sb([128, 8, 128], BF16, "hn3Ts"); qpT = P.sb([128, 8, 128], BF16, "qpT")
        sc_ = [P.sb([128, 16, 128], F32, "scr%d" % i) for i in range(2)]; ab_ = [P.sb([128, 16, 128], F32, "ab%d" % i) for i in range(2)]
        negm = P.sb([128, 16], F32, "negm"); t16 = P.sb([128, 16, 16], F32, "t16"); scr2_ = [P.sb([128, 128], F32, "scr2_%d" % i) for i in range(4)]
        candall = P.sb([128, 8, 256], F32, "candall"); cand2_ = [P.sb([128, 256], F32, "cand2_%d" % i) for i in range(4)]; c16 = P.sb([128, 8, 16], F32, "c16")
        route = P.sb([128, 16], F32, "route"); zs = P.sb([128, 8], F32, "zs")
        memset('dve', route, route[:], 0.0)
        def Xgen(jj):
            sc = sc_[jj % 2]
            x_t = xt[jj % 2]
            dma(x_t, x_t[:], xo, xo[jj * 128:(jj + 1) * 128, :])
            dma(og2, og2[:], d_ogla, d_ogla[jj * 256:(jj + 1) * 256, :].rearrange("(u p) c -> p u c", p=128))
            dma(omx, omx[:, 512:1024], d_omix, d_omix[jj * 128:(jj + 1) * 128, 512:1024])
            ts('dve', otm, otm[:], og2, og2[:, 0, :], pv[:, 0:1], None, ALU.mult, rd=[pv])
            stt('dve', omx, omx[:, 0:512], og2, og2[:, 1, :], pv[:, 1:2], otm, otm[:], ALU.mult, ALU.add, rd=[pv])
            for c in range(8):
                tr(pt, pt[:, c, :], omx, omx[:, c * 128:(c + 1) * 128], ident)
            cp('act', hnT, hnT[:], pt, pt[:])
            for half in range(2):
                for c in range(8):
                    mm(pa[half], pa[half][:], hnT, hnT[:, c, :], woutb, woutb[:, c, half * 512:(half + 1) * 512], start=(c == 0), stop=(c == 7))
                tt('dve', h1, h1[:, half * 512:(half + 1) * 512], pa[half], pa[half][:], x_t, x_t[:, half * 512:(half + 1) * 512], ALU.add)
            yield
            rmsnorm(h1, h1[:], 1, hn, hn[:], junk, ss, rstd)
            for c in range(8):
                tr(pt, pt[:, c, :], hn, hn[:, c * 128:(c + 1) * 128], ident)
            cp('act', hnT, hnT[:], pt, pt[:])
            for oc in range(8):
                pq_ = pa[2 + oc % 2]
                for c in range(8):
                    mm(pq_, pq_[:, 0:128], wqb, wqb[:, c, oc * 128:(oc + 1) * 128], hnT, hnT[:, c, :], start=(c == 0), stop=(c == 7))
                cp(['act', 'dve'][oc % 2], qTx, qTx[:, oc, :], pq_, pq_[:, 0:128])
                yield
            yield
            for mc in range(2):
                for h in range(4):
                    for dc in range(2):
                        mm(pxa, pxa[:, mc, h * 128:(h + 1) * 128], kTm, kTm[:, h * 2 + dc, mc * 128:(mc + 1) * 128], qTx, qTx[:, h * 2 + dc, :], start=(dc == 0), stop=(dc == 1))
            act(Ex, Ex[:].rearrange("p a h q -> p a (h q)"), pxa, pxa[:], AF.Exp, scale=1.0 / 16)
            for h in range(4):
                o_ap = pxa[:, h // 2, (h % 2) * 256:(h % 2) * 256 + 256]
                for mc in range(2):
                    mm(pa[h % 2], pa[h % 2][:, 0:257], Ex, Ex[:, mc, h, :], vm, vm[:, mc, h, :], start=(mc == 0), stop=(mc == 1))
                ts('dve', rdx, rdx[:, h:h + 1], pa[h % 2], pa[h % 2][:, 256:257], 1e-30, None, ALU.max)
                recip(rdx, rdx[:, h:h + 1], rdx, rdx[:, h:h + 1])
                ts('dve', oxa, oxa[:, h * 256:(h + 1) * 256], pa[h % 2], pa[h % 2][:, 0:256], rdx[:, h:h + 1], None, ALU.mult, rd=[rdx])
            yield
            for c in range(8):
                tr(pt, pt[:, c, :], oxa, oxa[:, c * 128:(c + 1) * 128], ident)
            cp('act', hnT, hnT[:], pt, pt[:])
            for half in range(2):
                for c in range(8):
                    mm(pa[half], pa[half][:], hnT, hnT[:, c, :], wob, wob[:, c, half * 512:(half + 1) * 512], start=(c == 0), stop=(c == 7))
                tt('dve', h1, h1[:, half * 512:(half + 1) * 512], pa[half], pa[half][:], h1, h1[:, half * 512:(half + 1) * 512], ALU.add)
            dma(d_h, d_h[jj * 128:(jj + 1) * 128, :], h1, h1[:])
            yield
            rmsnorm(h1, h1[:], 3, hn, hn[:], junk, ss, rstd)
            for c in range(8):
                tr(pt, pt[:, c, :], hn, hn[:, c * 128:(c + 1) * 128], ident)
            cp('act', hn3T, hn3T[:], pt, pt[:])
            dma(d_hn3T, d_hn3T[:, :, jj * 128:(jj + 1) * 128], hn3T, hn3T[:])
            for oc in range(8):
                pq_ = pa[2 + oc % 2]
                for c in range(8):
                    mm(pq_, pq_[:, 0:128], pwqb, pwqb[:, c, oc * 128:(oc + 1) * 128], hn3T, hn3T[:, c, :], start=(c == 0), stop=(c == 7))
                cp(['act', 'dve'][oc % 2], qpT, qpT[:, oc, :], pq_, pq_[:, 0:128])
                yield
            yield
            for oc in range(8):
                pq_ = pa[oc % 2]
                mm(pq_, pq_[:, 0:256], qpT, qpT[:, oc, :], skb, skb[:, oc, :])
                cp(['act', 'dve'][oc % 2], sc, sc[:, 2 * oc:2 * oc + 2, :], pq_, pq_[:, 0:256].rearrange("p (a k) -> p a k", a=2))
            yield

        def Rgen(jj):
            sc = sc_[jj % 2]; ab = ab_[jj % 2]
            P.op('dve', lambda e: e.tensor_reduce(out=negm[:], in_=sc[:], axis=AX.X, op=ALU.max), [sc], [negm])
            ts('dve', negm, negm[:], negm, negm[:], -1.0, None, ALU.mult)
            for r in range(16):
                act(ab, ab[:, r, :], sc, sc[:, r, :], AF.Exp, bias=negm[:, r:r + 1], rd=[negm])
            yield
            for r0 in range(0, 16, 4):
                for r in range(r0, r0 + 4):
                    P.op('dve', lambda e, r=r: e.max(out=t16[:, r, 0:8], in_=ab[:, r, :]), [ab], [t16])
                for r in range(r0, r0 + 4):
                    P.op('dve', lambda e, r=r: e.match_replace(out=scr2_[r % 4][:], in_to_replace=t16[:, r, 0:8], in_values=ab[:, r, :], imm_value=-1.0), [ab, t16], [scr2_[r % 4]])
                for r in range(r0, r0 + 4):
                    P.op('dve', lambda e, r=r: e.max(out=t16[:, r, 8:16], in_=scr2_[r % 4][:]), [scr2_[r % 4]], [t16])
                yield
            t16v = t16[:].rearrange("p (h a) k -> p h a k", a=2)
            abv = ab[:].rearrange("p (h a) k -> p h a k", a=2)

            def cand_top16():
                yield
                tt('dve', candall, candall[:].rearrange("p h (a b) -> p h a b", a=16),
                   t16, t16v[:, :, 0, :].unsqueeze(3).to_broadcast([128, 8, 16, 16]),
                   t16, t16v[:, :, 1, :].unsqueeze(2).to_broadcast([128, 8, 16, 16]), ALU.mult)
                for h0 in range(0, 8, 4):
                    for h in range(h0, h0 + 4):
                        P.op('dve', lambda e, h=h: e.max(out=c16[:, h, 0:8], in_=candall[:, h, :]), [candall], [c16])
                    for h in range(h0, h0 + 4):
                        P.op('dve', lambda e, h=h: e.match_replace(out=cand2_[h % 4][:], in_to_replace=c16[:, h, 0:8], in_values=candall[:, h, :], imm_value=-1.0), [candall, c16], [cand2_[h % 4]])
                    for h in range(h0, h0 + 4):
                        P.op('dve', lambda e, h=h: e.max(out=c16[:, h, 8:16], in_=cand2_[h % 4][:]), [cand2_[h % 4]], [c16])
                    yield
            yield from cand_top16()
            P.op('dve', lambda e: e.tensor_reduce(out=zs[:], in_=c16[:], axis=AX.X, op=ALU.add), [c16], [zs])
            recip(zs, zs[:], zs, zs[:])
            tt('dve', ab, abv[:, :, 1, :], ab, abv[:, :, 1, :], zs, zs[:].unsqueeze(2).to_broadcast([128, 8, 128]), ALU.mult)
            stt('dve', route, route[:, 0:8], c16, c16[:, :, 15], 1.0 - 1e-6, zs, zs[:], ALU.mult, ALU.mult)
            dma(d_route, d_route[jj * 128:(jj + 1) * 128, 0:2048], ab, ab[:].rearrange("p r k -> p (r k)"))
            dma(d_route, d_route[jj * 128:(jj + 1) * 128, 2048:2064], route, route[:])
            yield

        def drain(g):
            for _ in g:
                pass
        drain(Xgen(0))
        for jj in range(NO):
            gr = Rgen(jj)
            gx = Xgen(jj + 1) if jj + 1 < NO else iter(())
            ra = xa_ = True
            while ra or xa_:
                if xa_:
                    xa_ = next(gx, 'END') != 'END'
                if ra:
                    ra = next(gr, 'END') != 'END'
        P.pop()

    if upto >= 4:
        P.push()
        TG = 2
        IC = 16
        NCH = 128 // IC
        ACT_HEADS = (2, 4, 6)
        hT = P.sb([128, 8, TG * 128], BF16, "hT")
        ab = [P.sb([128, 16, 128], F32, "ab4_%d" % u) for u in range(TG)]
        rt = [P.sb([128, 16], F32, "rt%d" % u) for u in range(TG)]
        Wc = [[P.sb([128, IC * 128], BF16, "Wc%d_%d" % (i, u)) for u in range(TG)] for i in range(2)]
        et = [P.sb([128, IC, 128], F32, "et%d" % i) for i in range(4)]
        mt = [P.sb([128, IC, 128], BF16, "mt%d" % i) for i in range(2)]
        dnb = [P.sb([128, 8, 512], BF16, "dnb%d" % i) for i in range(3)]
        upb = [P.sb([128, 4, D], BF16, "upb%d" % i) for i in range(3)]
        Gs = [P.sb([128, 512], BF16, "G%d" % i) for i in range(2)]
        GT = [P.sb([128, 4, 128], BF16, "GT%d" % i) for i in range(2)]
        py = [P.ps([128, 2, 512], F32, "py%d" % u) for u in range(TG)]
        pd = [P.ps([128, 512], F32, "pd%d" % i) for i in range(2)]
        ptg = [P.ps([128, 8, 128], BF16, "ptg%d" % i) for i in range(2)]
        h2 = P.sb([128, D], F32, "h2"); yo = P.sb([128, D], F32, "yo")
        junk = P.sb([128, D], BF16, "junk4"); ss = P.sb([128, 1], F32, "ss4"); rstd = P.sb([128, 1], F32, "rstd4")
        qn = [0]
        for tg in range(NO // TG):
            dma(hT, hT[:], d_hn3T, d_hn3T[:, :, tg * TG * 128:(tg + 1) * TG * 128])
            for u in range(TG):
                j = tg * TG + u
                dma(ab[u], ab[u][:].rearrange("p r k -> p (r k)"), d_route, d_route[j * 128:(j + 1) * 128, 0:2048])
                dma(rt[u], rt[u][:], d_route, d_route[j * 128:(j + 1) * 128, 2048:2064])

            def wgen(c):
                its = [(u, h) for u in range(TG) for h in range(8)]
                K = len(its)
                eb_ = {}; mb_ = {}

                def E_(n):
                    u, h = its[n]
                    e_ = et[qn[0] % 4]; qn[0] += 1
                    eb_[n] = e_
                    if h in ACT_HEADS:
                        for i_ in range(IC):
                            P.op('act', lambda e, e_=e_, i_=i_, u=u, h=h: e.activation(out=e_[:, i_, :], in_=ab[u][:, 2 * h + 1, :], func=AF.Copy,
                                                                                  scale=ab[u][:, 2 * h, c * IC + i_:c * IC + i_ + 1]), [ab[u]], [e_])
                    else:
                        tt('dve', e_, e_[:], ab[u], ab[u][:, 2 * h, c * IC:(c + 1) * IC].unsqueeze(2).to_broadcast([128, IC, 128]),
                           ab[u], ab[u][:, 2 * h + 1, :].unsqueeze(1).to_broadcast([128, IC, 128]), ALU.mult)

                def S_(n):
                    u, h = its[n]
                    e_ = eb_.pop(n)
                    w_ap = Wc[c % 2][u][:].rearrange("p (a b) -> p a b", a=IC)
                    if h == 0:
                        stt('dve', Wc[c % 2][u], w_ap, e_, e_[:], rt[u][:, h:h + 1], e_, e_[:], ALU.is_ge, ALU.mult, rd=[rt[u]])
                    else:
                        m_ = mt[n % 2]
                        mb_[n] = m_
                        stt('dve', m_, m_[:], e_, e_[:], rt[u][:, h:h + 1], e_, e_[:], ALU.is_ge, ALU.mult, rd=[rt[u]])

                def A_(n):
                    u, h = its[n]
                    if h == 0:
                        return
                    m_ = mb_.pop(n)
                    w_ap = Wc[c % 2][u][:].rearrange("p (a b) -> p a b", a=IC)
                    tt('dve', Wc[c % 2][u], w_ap, Wc[c % 2][u], w_ap, m_, m_[:], ALU.add)
                for n in range(K + 2):
                    if n < K:
                        E_(n)
                    if 1 <= n <= K:
                        S_(n - 1)
                    if n >= 2:
                        A_(n - 2)
                    yield

            def load_w(ecx):
                dn, ub = dnb[ecx % 3], upb[ecx % 3]
                dma(dn, dn[:], d_downT, d_downT[:, ecx * 512:(ecx + 1) * 512].rearrange("(c p) e -> p c e", p=128))
                dma(ub, ub[:], d_up, d_up[ecx * 512:(ecx + 1) * 512, :].rearrange("(s p) d -> p s d", p=128))

            items = [(ecx, u) for ecx in range(32) for u in range(TG)]
            N = len(items)

            def stA(n):
                ecx, u = items[n]
                if u == 0:
                    if ecx == 0:
                        load_w(0)
                    if ecx + 1 < 32:
                        load_w(ecx + 1)
                    if ecx % 4 == 0:
                        for _ in wg[0]:
                            pass
                        wg[0] = wgen(ecx // 4 + 1) if ecx // 4 + 1 < NCH else iter(())
                for _ in range(3):
                    next(wg[0], None)
                dn = dnb[ecx % 3]
                pdt = pd[n % 2]; G = Gs[n % 2]
                for c in range(8):
                    mm(pdt, pdt[:], hT, hT[:, c, u * 128:(u + 1) * 128], dn, dn[:, c, :], start=(c == 0), stop=(c == 7))
                act(G, G[:], pdt, pdt[:], AF.Gelu_apprx_tanh)
                wch = Wc[(ecx // 4) % 2][u]
                tt('dve', G, G[:], G, G[:], wch, wch[:, (ecx % 4) * 512:(ecx % 4 + 1) * 512], ALU.mult)

            def stB(n):
                G = Gs[n % 2]; gt_ = GT[n % 2]; pt_ = ptg[n % 2]
                for s_ in range(4):
                    tr(pt_, pt_[:, s_, :], G, G[:, s_ * 128:(s_ + 1) * 128], ident)
                cp('act', gt_, gt_[:], pt_, pt_[:, 0:4, :])

            def stC(n):
                ecx, u = items[n]
                gt_ = GT[n % 2]; ub = upb[ecx % 3]
                for half in range(2):
                    for s_ in range(4):
                        mm(py[u], py[u][:, half, :], gt_, gt_[:, s_, :], ub, ub[:, s_, half * 512:(half + 1) * 512],
                           start=(ecx == 0 and s_ == 0), stop=(ecx == 31 and s_ == 3))

            wg = [iter(())]
            for _ in wgen(0):
                pass
            for n in range(N + 2):
                if n < N:
                    stA(n)
                if 1 <= n <= N:
                    stB(n - 1)
                if n >= 2:
                    stC(n - 2)
            for _ in wg[0]:
                pass
            for u in range(TG):
                j = tg * TG + u
                dma(h2, h2[:], d_h, d_h[j * 128:(j + 1) * 128, :])
                tt('dve', h2, h2[:], h2, h2[:], py[u], py[u][:].rearrange("p a b -> p (a b)"), ALU.add)
                rmsnorm(h2, h2[:], 4, yo, yo[:], junk, ss, rstd)
                dma(out, out[j * 128:(j + 1) * 128, :], yo, yo[:])
        P.pop()

    P.finish()
    return nc, dict(npair=npair, pair_off=pair_off, NCC=NCC, NCMP=NCMP, ninst=P.ninst)


def _t5_bucket_np(dist):
    n = np.maximum(dist, 0)
    nf = np.maximum(n, 1).astype(np.float32)
    log_ratio = (np.log(nf / np.float32(16)) / np.float32(math.log(2048 / 16))).astype(np.float32)
    large = 16 + (log_ratio * np.float32(16)).astype(np.int32)
    large = np.minimum(large, 31)
    return np.where(n < 16, n, large).astype(np.int64)


def _bias_tile(rel_bias, g, dist, valid):
    bk = _t5_bucket_np(dist)
    outt = np.empty((128, 4, 128), np.float32)
    for h in range(4):
        outt[:, h, :] = np.where(valid, rel_bias[bk, g * 4 + h], np.float32(NEG))
    return outt.reshape(128, 512)


def make_core_inputs(inputs, T, b, p, meta):
    NT = T // 128; NO = NT // 2; NS = T // 64; NCMP = meta['NCMP']; NCC = meta['NCC']
    f = lambda a: np.ascontiguousarray(np.asarray(a, dtype=np.float32))
    x = f(inputs['x'][b]); rel_bias = f(inputs['rel_bias'])
    m = {}
    m['xb'] = x
    m['xo'] = np.ascontiguousarray(x.reshape(NO, 2, 128, D)[:, p].reshape(NO * 128, D))
    m['mem'] = f(inputs['mem'][b])
    w_in = f(inputs['w_in'][0])
    m['w_in'] = np.ascontiguousarray(np.concatenate([w_in[:, ORIG[k][0]:ORIG[k][1]] for k in PERM_ORDER], axis=1))
    m['gw2'] = f(inputs['gla_gate_w2'][0]); m['gb'] = f(inputs['gla_gate_b'][0]).reshape(1, 256)
    m['gnorm'] = f(inputs['gla_out_norm'][0]).reshape(1, 128)
    m['norms'] = np.stack([f(inputs['norm_mix'][0]), f(inputs['norm_xattn'][0]), f(inputs['norm_mem'][0]),
                           f(inputs['norm_ffn'][0]), f(inputs['norm_final'])], 0)
    cw1 = np.stack([f(inputs['cmp_k_w1'][0]), f(inputs['cmp_v_w1'][0])], 0)
    m['cw1'] = np.ascontiguousarray(cw1.reshape(2, 32, 64, 128).transpose(0, 2, 1, 3))
    cpos = np.stack([f(inputs['cmp_pos_k'][0]), f(inputs['cmp_pos_v'][0])], 0)
    m['cpos'] = np.ascontiguousarray(cpos.transpose(0, 2, 1))
    m['cw2'] = np.stack([f(inputs['cmp_k_w2'][0]), f(inputs['cmp_v_w2'][0])], 0)
    m['w_out'] = f(inputs['w_out'][0])
    m['xa_w'] = np.stack([f(inputs['xa_wq'][0]), f(inputs['xa_wk'][0]), f(inputs['xa_wv'][0]), f(inputs['xa_wo'][0])], 0)
    m['pwq'] = f(inputs['peer_wq'][0])
    sk = f(inputs['peer_subkeys'][0])
    skd = np.zeros((8, 128, 256), np.float32)
    for h in range(8):
        for pp in range(2):
            skd[h, pp * 64:(pp + 1) * 64, pp * 128:(pp + 1) * 128] = sk[h, pp].T
    m['skd'] = skd
    m['downT'] = np.ascontiguousarray(f(inputs['peer_down'][0]).T)
    m['up'] = f(inputs['peer_up'][0])
    m['c_ident'] = np.eye(128, dtype=np.float32)
    s_ = np.arange(128)[:, None]; t_ = np.arange(128)[None, :]
    m['c_ucs'] = np.where(s_ <= t_, -1.0 / 16, 0.0).astype(np.float32)
    m['c_urev'] = np.where(s_ > t_, -1.0 / 16, 0.0).astype(np.float32)
    m['c_causal'] = (s_ <= t_).astype(np.float32)
    m['c_selu'] = np.stack([np.eye(128) * (1 - p), np.eye(128) * p], 0).astype(np.float32)
    m['c_pv'] = np.tile(np.array([[1.0 - p, float(p)]], np.float32), (128, 1))
    kk = np.arange(128)[:, None]; qq = np.arange(128)[None, :]
    selb = np.empty((2, 128, 15, 512), np.float32); winb = np.empty((2, 128, 6, 512), np.float32)
    for g in range(2):
        for mm_ in range(15):
            j = mm_ - 1 + p
            dist = 128 * j + qq - kk
            selb[g, :, mm_, :] = _bias_tile(rel_bias, g, dist, (dist >= 0) & (j >= 0))
        for mm_ in range(6):
            j = mm_ - 1 + p
            dist = 128 * j + qq - kk
            winb[g, :, mm_, :] = _bias_tile(rel_bias, g, dist, (dist >= 0) & (dist < 512) & (j >= 0))
    m['c_selb'] = selb; m['c_winb'] = winb
    cmpb = np.empty((2, meta['npair'], 128, 512), np.float32)
    for jj in range(NO):
        for c in range(min(NCC, cmp_nchunks(jj))):
            n = 128 * c + kk
            t = (2 * jj + p) * 128 + qq
            dist = t - (16 * n + 31)
            for g in range(2):
                cmpb[g, meta['pair_off'][jj] + c] = _bias_tile(rel_bias, g, dist, (dist >= 0) & (n < NCMP))
    m['c_cmpb'] = cmpb
    far = np.empty((2, 128, 4, 128), np.float32)
    for g in range(2):
        for h in range(4):
            far[g, :, h, :] = rel_bias[31, g * 4 + h]
    m['c_far'] = far.reshape(2, 128, 512)
    wimp = np.zeros((NCC * 128, 128), np.float32)
    for s in range(NS):
        for r in range(-1, 4):
            n = 4 * s + r
            lo = 16 * r
            ov = min(lo + 32, 64) - max(lo, 0)
            if 0 <= n < NCMP:
                wimp[n, s] += ov / 32.0
    m['c_wimp'] = wimp.reshape(NCC, 128, 128)
    m12 = np.zeros((NO, 128, 2, 128), np.float32)
    sid = np.arange(128)[None, :]
    for jj in range(NO):
        t = (2 * jj + p) * 128 + np.arange(128)[:, None]
        cur = t // 64
        visible = (sid * 64 <= t) & (sid < NS)
        f0 = (sid == 0); f1 = (sid == cur); f2 = (sid == cur - 1)
        forced = f0 | f1 | f2
        m12[jj, :, 0, :] = (visible & ~forced)
        add = np.where(~visible, -100.0 - sid, 0.0)
        add = np.where(f2, 100.0, add); add = np.where(f1, 101.0, add); add = np.where(f0 & (sid < NS), 102.0, add)
        m12[jj, :, 1, :] = add
    m['c_m12'] = m12
    ex = np.zeros((128, T), np.float32)
    ex[np.arange(T) // 64, np.arange(T)] = 1.0
    m['c_ex'] = ex.astype(NPBF)
    return m


_CACHE = {}


def kernel(**inputs):
    T = inputs['x'].shape[1]
    B = inputs['x'].shape[0]
    if T not in _CACHE:
        _CACHE[T] = build(T)
    nc, meta = _CACHE[T]
    in_maps = []
    for c in range(2 * B):
        in_maps.append(make_core_inputs(inputs, T, c // 2, c % 2, meta))
    res = run_bass_kernel_spmd(nc, in_maps, core_ids=list(range(2 * B)))
    NO = T // 256
    outp = np.empty((B, T // 128, 128, D), np.float32)
    for c in range(2 * B):
        o = np.asarray(res.results[c]["out"], dtype=np.float32).reshape(NO, 128, D)
        outp[c // 2, (c % 2)::2] = o
    return outp.reshape(B, T, D)
```

```python
import math
import numpy as np
import ml_dtypes
import concourse.bass as bass
import concourse.mybir as mybir
from concourse.bass_utils import run_bass_kernel_spmd
from contextlib import ExitStack

F32 = mybir.dt.float32
BF16 = mybir.dt.bfloat16
AF = mybir.ActivationFunctionType
ALU = mybir.AluOpType
AX = mybir.AxisListType
NPBF = ml_dtypes.bfloat16

D = 1024
NEG = -30000.0


class T:
    def __init__(self, h, name):
        self.h = h
        self.name = name
        self.w = None
        self.r = []
        self.psum = False

    def __getitem__(self, k):
        return self.h[k]


class Prog:
    def __init__(self, nc, n_dma_sems=48):
        self.nc = nc
        self.es = ExitStack()
        self.scopes = []
        self.eng = {'pe': nc.tensor, 'dve': nc.vector, 'act': nc.scalar,
                    'pool': nc.gpsimd, 'sp': nc.sync}
        self.sems = {}
        for k in self.eng:
            self.sems['e_' + k] = self.es.enter_context(nc.semaphore('e_' + k))
        self.cnt = {k: 0 for k in self.sems}
        self.ndma = n_dma_sems
        for i in range(n_dma_sems):
            key = 'd_%d' % i
            self.sems[key] = self.es.enter_context(nc.semaphore(key))
            self.cnt[key] = 0
        self.dma_rr = 0
        self.known = {k: {} for k in self.eng}
        self.ntile = 0
        self.ninst = 0

    def push(self):
        self.scopes.append(ExitStack())

    def pop(self):
        self.barrier()
        self.scopes.pop().close()

    def _stack(self):
        return self.scopes[-1] if self.scopes else self.es

    def sb(self, shape, dt, name=None):
        self.ntile += 1
        name = (name or 't') + '_%d' % self.ntile
        h = self._stack().enter_context(self.nc.sbuf_tensor(name, list(shape), dt))
        return T(h, name)

    def ps(self, shape, dt, name=None):
        self.ntile += 1
        name = (name or 'p') + '_%d' % self.ntile
        h = self._stack().enter_context(self.nc.psum_tensor(name, list(shape), dt))
        t = T(h, name)
        t.psum = True
        return t

    def dram(self, name, shape, dt, kind="Internal"):
        h = self.nc.dram_tensor(name, list(shape), dt, kind=kind).ap()
        return T(h, name)

    def _deps(self, reads, writes, e=None):
        deps = []
        for t in reads:
            if t.w is not None:
                deps.append(t.w)
            if t.psum:
                deps.extend([tok for tok in t.r if tok[0] != 'e_' + str(e)])
        for t in writes:
            if t.w is not None:
                deps.append(t.w)
            deps.extend(t.r)
        return deps

    def _waits(self, e, deps):
        kn = self.known[e]
        need = {}
        for (k, v) in deps:
            if kn.get(k, 0) >= v:
                continue
            if need.get(k, 0) < v:
                need[k] = v
        for k, v in need.items():
            kn[k] = v
        return list(need.items())

    def _emit(self, e, waits, fn, inc):
        eng = self.eng[e]
        for (k, v) in waits:
            eng.wait_ge(self.sems[k], v)
        ins = fn(eng)
        ins.then_inc(self.sems[inc[0]], inc[1])
        self.ninst += 1

    def op(self, e, fn, reads=(), writes=()):
        deps = self._deps(reads, writes, e)
        key = 'e_' + e
        if e == 'pe':
            deps = [d for d in deps if d[0] != key]
        waits = self._waits(e, deps)
        self.cnt[key] += 1
        tok = (key, self.cnt[key])
        self._emit(e, waits, fn, (key, 1))
        for t in reads:
            t.r.append(tok)
        for t in writes:
            t.w = tok
            t.r = []
        return tok

    def dma(self, e, out_t, out_ap, in_t, in_ap, **kw):
        reads = [in_t]
        writes = [out_t]
        deps = self._deps(reads, writes)
        key = 'd_%d' % self.dma_rr
        self.dma_rr = (self.dma_rr + 1) % self.ndma
        if self.cnt[key] > 0:
            deps.append((key, self.cnt[key]))
        waits = self._waits(e, deps)
        self.cnt[key] += 16
        tok = (key, self.cnt[key])
        self._emit(e, waits, lambda eng: eng.dma_start(out=out_ap, in_=in_ap, **kw), (key, 16))
        in_t.r.append(tok)
        out_t.w = tok
        out_t.r = []
        return tok

    def barrier(self):
        allt = [(k, v) for k, v in self.cnt.items() if v > 0]
        for e in self.eng:
            for (k, v) in self._waits(e, allt):
                self.eng[e].wait_ge(self.sems[k], v)

    def finish(self):
        self.barrier()
        while self.scopes:
            self.scopes.pop().close()
        self.es.close()


ORIG = dict(gq=(0, 256), gk=(256, 512), gv=(512, 1024), gr=(1024, 1536), glr=(1536, 1552),
            nq=(1552, 2064), kc=(2064, 2192), vc=(2192, 2320), ks=(2320, 2448), vs=(2448, 2576),
            kw=(2576, 2704), vw=(2704, 2832), ng=(2832, 2856))
PERM_ORDER = ['gq', 'gk', 'gv', 'gr', 'nq', 'kc', 'vc', 'ks', 'vs', 'kw', 'vw', 'ng', 'glr']
NCOL = 2856


def cmp_nchunks(jj):
    return min(4, (16 * jj + 15 + 127) // 128)


def build(T, debug=False, upto=9, cut=99):
    NT = T // 128
    NO = NT // 2
    NS = T // 64
    TO = T // 2
    NCMP = (T - 32) // 16 + 1
    NCC = (NCMP + 127) // 128
    pair_off = []
    npair = 0
    for jj in range(NO):
        pair_off.append(npair)
        npair += min(NCC, cmp_nchunks(jj))

    nc = bass.Bass("TRN2", target_bir_lowering=False)
    P = Prog(nc)
    SK = "ExternalOutput" if debug else "Internal"

    def inp(name, shape, dt=F32):
        return P.dram(name, shape, dt, kind="ExternalInput")

    xb = inp("xb", [T, D]); xo = inp("xo", [TO, D]); memb = inp("mem", [256, D])
    w_in = inp("w_in", [D, NCOL]); gw2 = inp("gw2", [16, 256]); gb = inp("gb", [1, 256])
    gnorm = inp("gnorm", [1, 128])
    norms = inp("norms", [5, D])
    cw1 = inp("cw1", [2, 64, 32, 128]); cpos = inp("cpos", [2, 64, 32]); cw2 = inp("cw2", [2, 128, 64])
    w_out = inp("w_out", [D, D]); xa_w = inp("xa_w", [4, D, D])
    pwq = inp("pwq", [D, D]); skd = inp("skd", [8, 128, 256])
    downT = inp("downT", [D, 16384]); up = inp("up", [16384, D])
    c_ident = inp("c_ident", [128, 128]); c_ucs = inp("c_ucs", [128, 128]); c_urev = inp("c_urev", [128, 128])
    c_causal = inp("c_causal", [128, 128]); c_selu = inp("c_selu", [2, 128, 128]); c_pv = inp("c_pv", [128, 2])
    c_selb = inp("c_selb", [2, 128, 15, 512]); c_winb = inp("c_winb", [2, 128, 6, 512])
    c_cmpb = inp("c_cmpb", [2, npair, 128, 512])
    c_far = inp("c_far", [2, 128, 512])
    c_wimp = inp("c_wimp", [NCC, 128, 128]); c_m12 = inp("c_m12", [NO, 128, 2, 128])
    c_ex = inp("c_ex", [128, T], BF16)
    out = P.dram("out", [TO, D], F32, kind="ExternalOutput")

    d_nq = P.dram("d_nq", [T, 512], BF16, kind=SK)
    d_ng = P.dram("d_ng", [T, 24], F32, kind=SK)
    d_ogla = P.dram("d_ogla", [T, 512], BF16, kind=SK)
    d_kT = P.dram("d_kT", [4, 128, T], BF16, kind=SK)
    d_vs = P.dram("d_vs", [T, 128], BF16, kind=SK)
    d_vw = P.dram("d_vw", [T, 128], BF16, kind=SK)
    d_omix = P.dram("d_omix", [TO, D], BF16, kind=SK)
    d_h = P.dram("d_h", [TO, D], F32, kind=SK)
    d_hn3T = P.dram("d_hn3T", [128, 8, TO], BF16, kind=SK)
    d_route = P.dram("d_route", [TO, 2064], F32, kind=SK)
    d_downT = P.dram("d_downT", [D, 16384], BF16, kind="Internal")
    d_up = P.dram("d_up", [16384, D], BF16, kind="Internal")
    d_cmp = P.dram("d_cmp", [2, 64, 512], BF16, kind=SK)
    d_dbg = P.dram("d_dbg", [3, TO, 512], F32, kind=SK)
    d_imp = P.dram("d_imp", [2, TO, 128], F32, kind=SK)

    def mm(ot, o_ap, lt, l_ap, rt, r_ap, start=True, stop=True):
        P.op('pe', lambda e: e.matmul(o_ap, lhsT=l_ap, rhs=r_ap, start=start, stop=stop), [lt, rt], [ot])

    def tr(ot, o_ap, it, i_ap, idt):
        P.op('pe', lambda e: e.transpose(out=o_ap, in_=i_ap, identity=idt[:]), [it, idt], [ot])

    def act(ot, o_ap, it, i_ap, func, bias=None, scale=None, accum=None, rd=(), wr=()):
        kw = {}
        if bias is not None:
            kw['bias'] = bias
        if scale is not None:
            kw['scale'] = scale
        if accum is not None:
            kw['accum_out'] = accum
        P.op('act', lambda e: e.activation(out=o_ap, in_=i_ap, func=func, **kw), [it] + list(rd), [ot] + list(wr))

    def cp(eng, ot, o_ap, it, i_ap):
        if eng == 'act':
            P.op('act', lambda e: e.copy(out=o_ap, in_=i_ap), [it], [ot])
        else:
            P.op(eng, lambda e: e.tensor_copy(out=o_ap, in_=i_ap), [it], [ot])

    def tt(eng, ot, o_ap, at, a_ap, bt, b_ap, op):
        P.op(eng, lambda e: e.tensor_tensor(out=o_ap, in0=a_ap, in1=b_ap, op=op), [at, bt], [ot])

    def ts(eng, ot, o_ap, at, a_ap, s1, s2, op0, op1=None, rd=()):
        if op1 is None:
            P.op(eng, lambda e: e.tensor_scalar(out=o_ap, in0=a_ap, scalar1=s1, scalar2=None, op0=op0), [at] + list(rd), [ot])
        else:
            P.op(eng, lambda e: e.tensor_scalar(out=o_ap, in0=a_ap, scalar1=s1, scalar2=s2, op0=op0, op1=op1), [at] + list(rd), [ot])

    def stt(eng, ot, o_ap, at, a_ap, sc, bt, b_ap, op0, op1, rd=()):
        P.op(eng, lambda e: e.scalar_tensor_tensor(out=o_ap, in0=a_ap, scalar=sc, in1=b_ap, op0=op0, op1=op1),
             [at, bt] + list(rd), [ot])

    def memset(eng, t, ap, v):
        P.op(eng, lambda e: e.memset(ap, v), [], [t])

    def recip(ot, o_ap, it, i_ap):
        P.op('dve', lambda e: e.reciprocal(out=o_ap, in_=i_ap), [it], [ot])

    dmaq = ['sp', 'act', 'pool']
    dq = [0]

    def dma(ot, o_ap, it, i_ap, q=None, **kw):
        if q is None:
            q = dmaq[dq[0] % 2]
            dq[0] += 1
        return P.dma(q, ot, o_ap, it, i_ap, **kw)

    ident = P.sb([128, 128], BF16, "ident"); identf = P.sb([128, 128], F32, "identf")
    ones = P.sb([128, 128], BF16, "ones")
    pv = P.sb([128, 2], F32, "pv")
    nrm = P.sb([128, 5, D], F32, "nrm")
    dma(identf, identf[:], c_ident, c_ident[:])
    dma(pv, pv[:], c_pv, c_pv[:])
    for k in range(5):
        dma(nrm, nrm[:, k, :], norms, norms[k:k + 1, :].partition_broadcast(128))
    cp('dve', ident, ident[:], identf, identf[:])
    memset('pool', ones, ones[:], 1.0)
    eps_t = P.sb([128, 1], F32, "eps_t")
    memset('dve', eps_t, eps_t[:], 1e-6)
    kcmpT = [P.sb([128, 512], BF16, "kcmpT%d" % g) for g in range(2)]
    vcmp = [P.sb([128, 4, 65], BF16, "vcmp%d" % g) for g in range(2)]

    def rmsnorm(xt, x_ap, gain_k, hn, hn_ap, junk, ss, rstd, n=D, eps=1e-6):
        memset('dve', ss, ss[:], 0.0)
        act(junk, junk[:, 0:n], xt, x_ap, AF.Square, accum=ss[:], rd=[ss], wr=[ss])
        act(rstd, rstd[:], ss, ss[:], AF.Ln, scale=1.0 / n, bias=eps_t[:, 0:1], rd=[eps_t])
        act(rstd, rstd[:], rstd, rstd[:], AF.Exp, scale=-0.5)
        stt('dve', hn, hn_ap, xt, x_ap, rstd[:, 0:1], nrm, nrm[:, gain_k, 0:n], ALU.mult, ALU.mult, rd=[rstd])

    if upto >= 1:
        P.push()
        winb = P.sb([128, 8, NCOL], BF16, "winb")
        wst = [P.sb([128, NCOL], F32, "wst%d" % i) for i in range(2)]
        w_in_v = w_in[:].rearrange("(c p) n -> c p n", p=128)
        for c in range(8):
            dma(wst[c % 2], wst[c % 2][:], w_in, w_in_v[c])
            cp(['dve', 'pool'][c % 2], winb, winb[:, c, :], wst[c % 2], wst[c % 2][:])
        gw2t = P.sb([16, 256], F32, "gw2t"); gbt = P.sb([1, 256], F32, "gbt"); onesrow = P.sb([1, 128], F32, "onesrow")
        gnt = P.sb([128, 128], F32, "gnt")
        ucs = P.sb([128, 128], F32, "ucs"); urev = P.sb([128, 128], F32, "urev"); causal = P.sb([128, 128], F32, "causal")
        m16 = P.sb([128, 1], F32, "m16")
        dma(gw2t, gw2t[:], gw2, gw2[:]); dma(gbt, gbt[:], gb, gb[:])
        dma(gnt, gnt[:], gnorm, gnorm[0:1, :].partition_broadcast(128))
        dma(ucs, ucs[:], c_ucs, c_ucs[:]); dma(urev, urev[:], c_urev, c_urev[:]); dma(causal, causal[:], c_causal, c_causal[:])
        memset('dve', onesrow, onesrow[:], 1.0)
        memset('dve', m16, m16[:], -1.0 / 16)
        S = P.sb([128, 2, 128], F32, "S"); Sb = P.sb([128, 2, 128], BF16, "Sb")
        memset('dve', S, S[:], 0.0); memset('pool', Sb, Sb[:], 0.0)

        xt = [P.sb([128, D], F32, "xt%d" % i) for i in range(2)]
        junk = P.sb([128, D], BF16, "junk"); ss = P.sb([128, 1], F32, "ss"); rstd = P.sb([128, 1], F32, "rstd")
        hn = P.sb([128, D], BF16, "hn"); hnT = P.sb([128, 8, 128], BF16, "hnT")
        pt = P.ps([128, 8, 128], BF16, "pt")
        pz = [P.ps([128, 512], F32, "pz%d" % i) for i in range(3)]
        pg1 = P.ps([128, 512], F32, "pg1"); pg2 = P.ps([128, 512], F32, "pg2")
        po = P.ps([128, 512], F32, "po"); ptb = P.ps([128, 8, 128], BF16, "ptb")
        glr = P.sb([128, 16], F32, "glr"); glrT = P.sb([16, 128], F32, "glrT")
        e1 = P.sb([128, 256], F32, "e1"); L = P.sb([128, 256], F32, "L")
        eb = P.sb([128, 256], F32, "eb"); enb = P.sb([128, 256], F32, "enb"); ec = P.sb([128, 256], F32, "ec")
        ebl = P.sb([128, 2], F32, "ebl")
        qk = P.sb([128, 3, 256], BF16, "qk")
        qkT = P.sb([128, 4, 128], BF16, "qkT")
        vv = P.sb([128, 512], BF16, "vv"); sg = P.sb([128, 512], F32, "sg")
        AT = P.sb([128, 128], BF16, "AT")
        ssq = P.sb([128, 4], F32, "ssq"); rs4 = P.sb([128, 4], F32, "rs4"); tmpn = P.sb([128, 128], F32, "tmpn")
        junk2 = P.sb([128, 128], F32, "junk2")
        og = P.sb([128, 512], BF16, "og"); otmp4 = P.sb([128, 512], F32, "otmp4")
        nqs = P.sb([128, 512], BF16, "nqs"); ngs = P.sb([128, 24], F32, "ngs")
        kk = P.sb([128, 4, 128], BF16, "kk"); kkT = P.sb([128, 4, 128], BF16, "kkT")
        vsw = P.sb([128, 2, 128], BF16, "vsw")
        cast_jobs = []
        if upto >= 4:
            stg = [P.sb([128, 4096], F32, "stg%d" % i) for i in range(2)]
            stb = [P.sb([128, 4096], BF16, "stb%d" % i) for i in range(2)]
            for r in range(8):
                for c in range(4):
                    cast_jobs.append((downT, downT[r * 128:(r + 1) * 128, c * 4096:(c + 1) * 4096],
                                      d_downT, d_downT[r * 128:(r + 1) * 128, c * 4096:(c + 1) * 4096]))
            sv = up[:].rearrange("(a p f) d -> a p (f d)", p=128, f=4)
            dv = d_up[:].rearrange("(a p f) d -> a p (f d)", p=128, f=4)
            for a in range(32):
                cast_jobs.append((up, sv[a], d_up, dv[a]))
        cj = [0]

        def cast_job():
            if cj[0] >= len(cast_jobs):
                return
            src_t, s_ap, dst_t, d_ap = cast_jobs[cj[0]]
            a, b_ = stg[cj[0] % 2], stb[cj[0] % 2]
            P.dma('pool', a, a[:], src_t, s_ap)
            for q_ in range(4):
                cp('act', b_, b_[:, q_ * 1024:(q_ + 1) * 1024], a, a[:, q_ * 1024:(q_ + 1) * 1024])
            P.dma('pool', dst_t, d_ap, b_, b_[:])
            cj[0] += 1
        cA, cB, cC, cD, cE, cF = 0, 512, 1024, 1536, 2048, 2560
        for i in range(NT):
            x_t = xt[i % 2]
            dma(x_t, x_t[:], xb, xb[i * 128:(i + 1) * 128, :])
            rmsnorm(x_t, x_t[:], 0, hn, hn[:], junk, ss, rstd)
            for c in range(8):
                tr(pt, pt[:, c, :], hn, hn[:, c * 128:(c + 1) * 128], ident)
            cp('act', hnT, hnT[:], pt, pt[:])

            def zgroup(pz_t, c0, n):
                for c in range(8):
                    mm(pz_t, pz_t[:, 0:n], hnT, hnT[:, c, :], winb, winb[:, c, c0:c0 + n], start=(c == 0), stop=(c == 7))
            if cut <= 1:
                continue
            zgroup(pz[0], cF, 296)
            cp('act', kk, kk[:, 3, :], pz[0], pz[0][:, 0:128])
            cp('act', vsw, vsw[:, 1, :], pz[0], pz[0][:, 128:256])
            if cut <= 1.05:
                continue
            cp('act', glr, glr[:], pz[0], pz[0][:, 280:296])
            if cut <= 1.07:
                continue
            act(ngs, ngs[:], pz[0], pz[0][:, 256:280], AF.Exp, scale=-1.0)
            if cut <= 1.1:
                continue
            ts('dve', ngs, ngs[:], ngs, ngs[:], 1.0, None, ALU.add)
            recip(ngs, ngs[:], ngs, ngs[:])
            dma(d_ng, d_ng[i * 128:(i + 1) * 128, :], ngs, ngs[:])
            if cut <= 1.2:
                continue
            zgroup(pz[1], cE, 512)
            cp('act', kk, kk[:, 0:3, :], pz[1], pz[1][:, 0:384].rearrange("p (a b) -> p a b", a=3))
            cp('dve', vsw, vsw[:, 0, :], pz[1], pz[1][:, 384:512])
            dma(d_vs, d_vs[i * 128:(i + 1) * 128, :], vsw, vsw[:, 0, :])
            dma(d_vw, d_vw[i * 128:(i + 1) * 128, :], vsw, vsw[:, 1, :])
            if cut <= 1.4:
                continue
            for a in range(4):
                tr(ptb, ptb[:, a, :], kk, kk[:, a, :], ident)
            cp('act', kkT, kkT[:], ptb, ptb[:, 0:4, :])
            dma(d_kT, d_kT[:, :, i * 128:(i + 1) * 128].rearrange("a p t -> p a t"), kkT, kkT[:])
            if cut <= 1.6:
                continue
            zgroup(pz[2], cD, 512)
            P.op('act', lambda e, pzt=pz[2]: e.mul(out=nqs[:], in_=pzt[:], mul=0.125), [pz[2]], [nqs])
            dma(d_nq, d_nq[i * 128:(i + 1) * 128, :], nqs, nqs[:])
            if cut <= 2:
                continue
            tr(pg1, pg1[0:16, 0:128], glr, glr[:], identf)
            cp('dve', glrT, glrT[:], pg1, pg1[0:16, 0:128])
            mm(pg1, pg1[:, 256:512], glrT, glrT[:], gw2t, gw2t[:], start=True, stop=False)
            mm(pg1, pg1[:, 256:512], onesrow, onesrow[:], gbt, gbt[:], start=False, stop=True)
            zgroup(pz[0], cA, 512)
            zgroup(pz[1], cB, 512)
            zgroup(pz[2], cC, 512)
            act(e1, e1[:], pg1, pg1[:, 256:512], AF.Exp, scale=-1.0)
            act(L, L[:], e1, e1[:], AF.Ln, bias=1.0)
            cp('act', vv, vv[:], pz[1], pz[1][:])
            act(sg, sg[:], pz[2], pz[2][:], AF.Exp, scale=-1.0)
            ts('dve', sg, sg[:], sg, sg[:], 1.0, None, ALU.add)
            recip(sg, sg[:], sg, sg[:])
            tt('dve', sg, sg[:], sg, sg[:], pz[2], pz[2][:], ALU.mult)
            tt('pool', sg, sg[:].rearrange("p (h d) -> p h d", h=4), sg, sg[:].rearrange("p (h d) -> p h d", h=4),
               gnt, gnt[:].unsqueeze(1).to_broadcast([128, 4, 128]), ALU.mult)
            if cut <= 3:
                continue
            mm(pg2, pg2[:, 0:256], ucs, ucs[:], L, L[:])
            mm(pg2, pg2[:, 256:512], urev, urev[:], L, L[:])
            for hp in range(2):
                mm(pg1, pg1[:, hp:hp + 1], L, L[:, hp * 128:(hp + 1) * 128], m16, m16[:])
            act(eb, eb[:], pg2, pg2[:, 0:256], AF.Exp)
            act(enb, enb[:], pg2, pg2[:, 0:256], AF.Exp, scale=-1.0)
            act(ec, ec[:], pg2, pg2[:, 256:512], AF.Exp)
            act(ebl, ebl[:], pg1, pg1[:, 0:2], AF.Exp)
            if cut <= 4:
                continue
            stt('dve', qk, qk[:, 0, :], pz[0], pz[0][:, 0:256], 0.125, eb, eb[:], ALU.mult, ALU.mult)
            tt('dve', qk, qk[:, 1, :], pz[0], pz[0][:, 256:512], enb, enb[:], ALU.mult)
            tt('dve', qk, qk[:, 2, :], pz[0], pz[0][:, 256:512], ec, ec[:], ALU.mult)
            for a in range(4):
                tr(ptb, ptb[:, 4 + a, :], qk, qk[:, a // 2, (a % 2) * 128:(a % 2 + 1) * 128], ident)
            cp('act', qkT, qkT[:], ptb, ptb[:, 4:8, :])
            if cut <= 6:
                continue
            memset('dve', ssq, ssq[:], 0.0)
            for h in range(4):
                hp, hh = h // 2, h % 2
                pr = slice(hh * 64, hh * 64 + 64)
                mm(pg2, pg2[:, 0:128], qkT, qkT[pr, 2 + hp, :], qkT, qkT[pr, hp, :])
                tt('dve', AT, AT[:], pg2, pg2[:, 0:128], causal, causal[:], ALU.mult)
                o_ap = po[:, h * 128:(h + 1) * 128]
                mm(po, o_ap, AT, AT[:], vv, vv[:, h * 128:(h + 1) * 128], start=True, stop=False)
                mm(po, o_ap, qkT, qkT[pr, hp, :], Sb, Sb[pr, hp, :], start=False, stop=True)
                mm(pg2, pg2[:, 128:256], qk, qk[:, 2, hp * 128:(hp + 1) * 128], vv, vv[:, h * 128:(h + 1) * 128])
                stt('dve', Sb, Sb[pr, hp, :], S, S[pr, hp, :], ebl[pr, hp:hp + 1], pg2, pg2[pr, 128:256], ALU.mult, ALU.add, rd=[ebl])
                stt('dve', S, S[pr, hp, :], S, S[pr, hp, :], ebl[pr, hp:hp + 1], pg2, pg2[pr, 128:256], ALU.mult, ALU.add, rd=[ebl])
                act(junk2, junk2[:], po, o_ap, AF.Square, accum=ssq[:, h:h + 1], rd=[ssq], wr=[ssq])
            act(rs4, rs4[:], ssq, ssq[:], AF.Ln, scale=1.0 / 128, bias=eps_t[:, 0:1], rd=[eps_t])
            act(rs4, rs4[:], rs4, rs4[:], AF.Exp, scale=-0.5)
            tt('dve', otmp4, otmp4[:], po, po[:], sg, sg[:], ALU.mult)
            tt('dve', og, og[:].rearrange("p (h d) -> p h d", h=4), otmp4, otmp4[:].rearrange("p (h d) -> p h d", h=4),
               rs4, rs4[:].unsqueeze(2).to_broadcast([128, 4, 128]), ALU.mult)
            dma(d_ogla, d_ogla[i * 128:(i + 1) * 128, :], og, og[:])
            for _ in range((len(cast_jobs) + NT - 1) // NT):
                cast_job()
        while cj[0] < len(cast_jobs):
            cast_job()
        P.pop()

        P.push()
        w1f = P.sb([64, 32, 128], F32, "w1f"); w1b = P.sb([64, 32, 128], BF16, "w1b")
        posf = P.sb([64, 32], F32, "posf"); posb = P.sb([64, 32], BF16, "posb")
        w2f = P.sb([128, 64], F32, "w2f"); w2b = P.sb([128, 64], BF16, "w2b")
        kTg = P.sb([64, T], BF16, "kTg")
        ph = P.ps([128, 512], F32, "ph"); pb = P.ps([128, 512], F32, "pb"); pc = P.ps([128, 512], F32, "pc")
        bias_h = P.sb([128, 1], F32, "bias_h")
        H = P.sb([128, 512], BF16, "H")
        for g in range(2):
            memset('dve', kcmpT[g], kcmpT[g][:], 0.0)
            memset('dve', vcmp[g], vcmp[g][:], 0.0)
            memset('dve', vcmp[g], vcmp[g][:, :, 64:65], 1.0)
        for kv in range(2):
            dma(w1f, w1f[:], cw1, cw1[kv]); dma(posf, posf[:], cpos, cpos[kv]); dma(w2f, w2f[:], cw2, cw2[kv])
            cp('dve', w1b, w1b[:], w1f, w1f[:]); cp('dve', posb, posb[:], posf, posf[:]); cp('dve', w2b, w2b[:], w2f, w2f[:])
            for l in range(32):
                mm(pb, pb[:, 0:1], w1b, w1b[:, l, :], posb, posb[:, l:l + 1], start=(l == 0), stop=(l == 31))
            cp('dve', bias_h, bias_h[:], pb, pb[:, 0:1])
            for g in range(2):
                dma(kTg, kTg[:], d_kT, d_kT[kv, g * 64:(g + 1) * 64, :])
                for l in range(32):
                    mm(ph, ph[:, 0:NCMP], w1b, w1b[:, l, :], kTg, kTg[:, l:l + 16 * (NCMP - 1) + 1:16],
                       start=(l == 0), stop=(l == 31))
                memset('dve', H, H[:], 0.0)
                act(H, H[:, 0:NCMP], ph, ph[:, 0:NCMP], AF.Gelu_apprx_tanh, bias=bias_h[:, 0:1], rd=[bias_h])
                if kv == 0:
                    mm(pc, pc[0:64, 0:NCMP], w2b, w2b[:], H, H[:, 0:NCMP])
                    cp('act', kcmpT[g], kcmpT[g][0:64, 0:NCMP], pc, pc[0:64, 0:NCMP])
                    if debug:
                        dma(d_cmp, d_cmp[g], kcmpT[g], kcmpT[g][0:64, :])
                else:
                    for c in range(NCC):
                        mm(pc, pc[:, c * 64:(c + 1) * 64], H, H[:, c * 128:(c + 1) * 128], w2b, w2b[:])
                    cp('act', vcmp[g], vcmp[g][:, 0:NCC, 0:64], pc, pc[:, 0:NCC * 64].rearrange("p (c d) -> p c d", d=64))
        P.pop()

    if upto >= 2:
        P.push()
        selu = P.sb([128, 2, 128], BF16, "selu"); seluf = P.sb([128, 2, 128], F32, "seluf")
        dma(seluf, seluf[:], c_selu, c_selu[:].rearrange("a p t -> p a t"))
        cp('dve', selu, selu[:], seluf, seluf[:])
        exm = P.sb([128, T], BF16, "exm")
        dma(exm, exm[:], c_ex, c_ex[:])
        wimpf = P.sb([128, NCC, 128], F32, "wimpf"); wimp = P.sb([128, NCC, 128], BF16, "wimp")
        dma(wimpf, wimpf[:], c_wimp, c_wimp[:].rearrange("c p s -> p c s"))
        cp('dve', wimp, wimp[:], wimpf, wimpf[:])
        ksT = P.sb([128, T], BF16, "ksT"); kwT = P.sb([128, T], BF16, "kwT")
        memset('dve', ksT, ksT[64:128, :], 0.0)
        memset('pool', kwT, kwT[64:128, :], 0.0)
        memset('dve', ksT, ksT[64:65, :], 1.0)
        farf = P.sb([128, 512], F32, "farf")
        vs = P.sb([128, NT, 65], BF16, "vs"); vw = P.sb([128, NT, 65], BF16, "vw")
        selb = P.sb([128, 15, 512], BF16, "selb"); winb2 = P.sb([128, 6, 512], BF16, "winb2")
        bst = [P.sb([128, 512], F32, "bst%d" % i) for i in range(2)]
        cbt = [P.sb([128, 512], BF16, "cbt%d" % i) for i in range(2)]
        qrows_ = [P.sb([128, 2, 256], BF16, "qrows%d" % i) for i in range(2)]; grows_ = [P.sb([128, 2, 24], F32, "grows%d" % i) for i in range(2)]
        gown_ = [P.sb([128, 12], F32, "gown%d" % i) for i in range(2)]; gtmp = P.sb([128, 12], F32, "gtmp")
        qT_ = [P.sb([128, 4, 128], BF16, "qT%d" % i) for i in range(2)]
        for i_ in range(2):
            memset('dve', qT_[i_], qT_[i_][64:128, :, :], 0.0)
        Ec = [P.sb([128, 4, 128], BF16, "Ec%d" % i) for i in range(4)]
        Eb = [P.sb([128, 4, 128], BF16, "Eb%d" % i) for i in range(3)]
        m12_ = [P.sb([128, 2, 128], F32, "m12_%d" % i) for i in range(2)]
        imp = P.sb([128, 128], F32, "imp"); imp2 = P.sb([128, 128], F32, "imp2"); m8 = P.sb([128, 16], F32, "m8")
        mk = P.sb([128, 128], BF16, "mk"); maskT4 = P.sb([128, 4, 128], BF16, "maskT4")
        rden = P.sb([128, 4], F32, "rden"); coef = P.sb([128, 4], F32, "coef")
        oacc = P.sb([128, 4, 64], F32, "oacc"); otmp = P.sb([128, 4, 64], F32, "otmp"); onsa = P.sb([128, 256], BF16, "onsa")
        pq = P.ps([64, 4, 128], F32, "pq")
        psc = [P.ps([128, 4, 128], F32, "psc%d" % i) for i in range(2)]
        pn = P.ps([128, 4, 128], F32, "pn")
        pnT = P.ps([65, 4, 128], F32, "pnT")
        pnT2 = P.ps([65, 4, 128], F32, "pnT2")
        numTs = P.sb([65, 4, 128], F32, "numTs")
        pimp = P.ps([128, 4, 128], F32, "pimp")
        pmt = P.ps([128, 128], BF16, "pmt")
        for g in range(2):
            dma(ksT, ksT[0:64, :], d_kT, d_kT[2, g * 64:(g + 1) * 64, :])
            dma(farf, farf[:], c_far, c_far[g])
            for i_ in range(2):
                cp('dve', qT_[i_], qT_[i_][64:65, :, :], farf, farf[64:65, :].rearrange("p (h q) -> p h q", h=4))
            dma(kwT, kwT[0:64, :], d_kT, d_kT[3, g * 64:(g + 1) * 64, :])
            for n0 in range(0, NT, 16):
                n1 = min(NT, n0 + 16)
                dma(vs, vs[:, n0:n1, 0:64], d_vs, d_vs[n0 * 128:n1 * 128, g * 64:(g + 1) * 64].rearrange("(n p) c -> p n c", p=128))
                dma(vw, vw[:, n0:n1, 0:64], d_vw, d_vw[n0 * 128:n1 * 128, g * 64:(g + 1) * 64].rearrange("(n p) c -> p n c", p=128))
            memset('dve', vs, vs[:, :, 64:65], 1.0)
            memset('dve', vw, vw[:, :, 64:65], 1.0)
            k = 0
            for m in range(15):
                dma(bst[k % 2], bst[k % 2][:], c_selb, c_selb[g, :, m, :])
                tt('pool', selb, selb[:, m, :], bst[k % 2], bst[k % 2][:], farf, farf[:], ALU.subtract); k += 1
            for m in range(6):
                dma(bst[k % 2], bst[k % 2][:], c_winb, c_winb[g, :, m, :])
                cp('pool', winb2, winb2[:, m, :], bst[k % 2], bst[k % 2][:]); k += 1
            ne = 0
            ne_ = [0]
            for jj in range(NO):
                qrows, grows, gown, qT, m12 = qrows_[jj % 2], grows_[jj % 2], gown_[jj % 2], qT_[jj % 2], m12_[jj % 2]
                dma(qrows, qrows[:], d_nq, d_nq[jj * 256:(jj + 1) * 256, g * 256:(g + 1) * 256].rearrange("(u p) c -> p u c", p=128))
                dma(grows, grows[:], d_ng, d_ng[jj * 256:(jj + 1) * 256, :].rearrange("(u p) c -> p u c", p=128))
                dma(m12, m12[:], c_m12, c_m12[jj])
                for h in range(4):
                    for u in range(2):
                        mm(pq, pq[:, h, :], qrows, qrows[:, u, h * 64:(h + 1) * 64], selu, selu[:, u, :], start=(u == 0), stop=(u == 1))
                cp('act', qT, qT[0:64], pq, pq[:])
                ts('dve', gtmp, gtmp[:], grows, grows[:, 0, g * 12:(g + 1) * 12], pv[:, 0:1], None, ALU.mult, rd=[pv])
                stt('dve', gown, gown[:], grows, grows[:, 1, g * 12:(g + 1) * 12], pv[:, 1:2], gtmp, gtmp[:], ALU.mult, ALU.add, rd=[pv])
                gv3 = gown[:].rearrange("p (h k) -> p h k", k=3)
                qT_all = qT[:].rearrange("p h q -> p (h q)")
                qT_aug = qT_all

                def finish_branch(pn, br, first):
                    ts('dve', rden, rden[:], pn, pn[:, :, 64], 1e-30, None, ALU.max)
                    recip(rden, rden[:], rden, rden[:])
                    tt('dve', coef, coef[:], rden, rden[:], gown, gv3[:, :, br], ALU.mult)
                    dst = oacc if first else otmp
                    tt('dve', dst, dst[:], pn, pn[:, :, 0:64], coef, coef[:].unsqueeze(2).to_broadcast([128, 4, 64]), ALU.mult)
                    if not first:
                        tt('pool', oacc, oacc[:], oacc, oacc[:], otmp, otmp[:], ALU.add)
                    if debug:
                        dma(d_dbg, d_dbg[br, jj * 128:(jj + 1) * 128, g * 256:(g + 1) * 256], oacc, oacc[:].rearrange("p h d -> p (h d)"))

                def back_T(src):
                    cp('act', numTs, numTs[:], src, src[:])
                    for h in range(4):
                        P.op('pe', lambda e, h=h: e.transpose(out=pn[:, h, 0:65], in_=numTs[:, h, :], identity=identf[0:65, 0:65]), [numTs, identf], [pn])

                ncc = min(NCC, cmp_nchunks(jj))
                for c in range(ncc):
                    pi = pair_off[jj] + c
                    dma(bst[k % 2], bst[k % 2][:], c_cmpb, c_cmpb[g, pi])
                    cp('pool', cbt[k % 2], cbt[k % 2][:], bst[k % 2], bst[k % 2][:])
                    sc = psc[ne % 2]; ne += 1
                    sc_all = sc[:].rearrange("p h q -> p (h q)")
                    mm(sc, sc_all, kcmpT[g], kcmpT[g][:, c * 128:(c + 1) * 128], qT, qT_all, start=True, stop=False)
                    mm(sc, sc_all, ident, ident[:], cbt[k % 2], cbt[k % 2][:], start=False, stop=True)
                    k += 1
                    act(Ec[c], Ec[c][:], sc, sc[:], AF.Exp)
                for h in range(4):
                    for c in range(ncc):
                        mm(pn, pn[:, h, 0:65], Ec[c], Ec[c][:, h, :], vcmp[g], vcmp[g][:, c, :], start=(c == 0), stop=(c == ncc - 1))
                    for c in range(ncc):
                        mm(pimp, pimp[:, h, :], Ec[c], Ec[c][:, h, :], wimp, wimp[:, c, :], start=(c == 0), stop=(c == ncc - 1))
                nk = 2 * jj + 2
                k0 = max(0, 2 * jj - 4)
                bufs = {}

                def score(kind, kc, n):
                    sc = psc[ne_[0] % 2]; E = Eb[ne_[0] % 3]; ne_[0] += 1
                    bufs[n] = E
                    sc_all = sc[:].rearrange("p h q -> p (h q)")
                    if kind == 's':
                        m = 2 * jj + 1 - kc
                        mm(sc, sc_all, ksT, ksT[:, kc * 128:(kc + 1) * 128], qT, qT_aug, start=True, stop=False)
                        if m < 14:
                            mm(sc, sc_all, ident, ident[:], selb, selb[:, m, :], start=False, stop=False)
                        mm(sc, sc_all, exm, exm[:, kc * 128:(kc + 1) * 128], maskT4, mT_all, start=False, stop=True)
                    else:
                        m = 2 * jj + 1 - kc
                        mm(sc, sc_all, kwT, kwT[:, kc * 128:(kc + 1) * 128], qT, qT_all, start=True, stop=False)
                        mm(sc, sc_all, ident, ident[:], winb2, winb2[:, m, :], start=False, stop=True)
                    act(E, E[:], sc, sc[:], AF.Exp)

                def pvs(kind, kc, n):
                    E = bufs.pop(n)
                    if kind == 's':
                        mm(pnT, pnT[:].rearrange("p h q -> p (h q)"), vs, vs[:, kc, :], E, E[:].rearrange("p h q -> p (h q)"), start=(kc == 0), stop=(kc == nk - 1))
                    else:
                        mm(pnT2, pnT2[:].rearrange("p h q -> p (h q)"), vw, vw[:, kc, :], E, E[:].rearrange("p h q -> p (h q)"), start=(kc == k0), stop=(kc == nk - 1))

                def run_items(items):
                    NI = len(items)
                    for n in range(NI + 1):
                        if n < NI:
                            score(items[n][0], items[n][1], n)
                        if n >= 1:
                            pvs(items[n - 1][0], items[n - 1][1], n - 1)
                mT_all = maskT4[:].rearrange("p h q -> p (h q)")
                run_items([('w', kc) for kc in range(k0, nk)])
                finish_branch(pn, 0, True)
                for h in range(4):
                    if h == 0:
                        ts('dve', imp, imp[:], pimp, pimp[:, 0, :], rden[:, 0:1], None, ALU.mult, rd=[rden])
                    else:
                        stt('dve', imp, imp[:], pimp, pimp[:, h, :], rden[:, h:h + 1], imp, imp[:], ALU.mult, ALU.add, rd=[rden])
                tt('dve', imp, imp[:], imp, imp[:], m12, m12[:, 0, :], ALU.mult)
                tt('dve', imp, imp[:], imp, imp[:], m12, m12[:, 1, :], ALU.add)
                if debug:
                    dma(d_imp, d_imp[g, jj * 128:(jj + 1) * 128, :], imp, imp[:])
                P.op('dve', lambda e: e.max(out=m8[:, 0:8], in_=imp[:]), [imp], [m8])
                P.op('dve', lambda e: e.match_replace(out=imp2[:], in_to_replace=m8[:, 0:8], in_values=imp[:], imm_value=-1e30), [imp, m8], [imp2])
                P.op('dve', lambda e: e.max(out=m8[:, 8:16], in_=imp2[:]), [imp2], [m8])
                ts('dve', mk, mk[:], imp, imp[:], m8[:, 15:16], 1.0, ALU.is_ge, ALU.subtract, rd=[m8])
                tr(pmt, pmt[:], mk, mk[:], ident)
                P.op('act', lambda e: e.mul(out=maskT4[:], in_=pmt[:].unsqueeze(1).to_broadcast([128, 4, 128]), mul=30000.0), [pmt], [maskT4])
                run_items([('s', kc) for kc in range(nk)])
                back_T(pnT)
                finish_branch(pn, 1, False)
                back_T(pnT2)
                finish_branch(pn, 2, False)
                cp('act', onsa, onsa[:], oacc, oacc[:].rearrange("p h d -> p (h d)"))
                dma(d_omix, d_omix[jj * 128:(jj + 1) * 128, 512 + g * 256:512 + (g + 1) * 256], onsa, onsa[:])
        P.pop()

    if upto >= 3:
        P.push()
        woutb = P.sb([128, 8, D], BF16, "woutb"); wqb = P.sb([128, 8, D], BF16, "wqb"); wob = P.sb([128, 8, D], BF16, "wob")
        pwqb = P.sb([128, 8, D], BF16, "pwqb")
        skb = P.sb([128, 8, 256], BF16, "skb")
        kTm = P.sb([128, 8, 256], BF16, "kTm")
        vm = P.sb([128, 2, 4, 257], BF16, "vm")
        xt = [P.sb([128, D], F32, "x3_%d" % i) for i in range(2)]
        junk = P.sb([128, D], BF16, "junk3"); ss = P.sb([128, 1], F32, "ss3"); rstd = P.sb([128, 1], F32, "rstd3")
        hn = P.sb([128, D], BF16, "hn3"); hnT = P.sb([128, 8, 128], BF16, "hnT3")
        pt = P.ps([128, 8, 128], BF16, "pt3")
        pa = [P.ps([128, 512], F32, "pa%d" % i) for i in range(4)]
        pxa = P.ps([128, 2, 512], F32, "pxa")
        P.push()
        wst = [P.sb([128, D], F32, "wst3_%d" % i) for i in range(2)]
        wtmp = P.sb([128, 8, D], BF16, "wtmp"); skf = P.sb([128, 8, 256], F32, "skf")
        memT = P.sb([128, 8, 256], BF16, "memT")
        k = [0]

        def load_w(dst, src_t, src_ap3):
            for c in range(8):
                a = wst[k[0] % 2]
                dma(a, a[:], src_t, src_ap3[c])
                cp(['dve', 'pool'][k[0] % 2], dst, dst[:, c, :], a, a[:]); k[0] += 1
        load_w(woutb, w_out, w_out[:].rearrange("(c p) n -> c p n", p=128))
        load_w(wqb, xa_w, xa_w[0].rearrange("(c p) n -> c p n", p=128))
        load_w(wob, xa_w, xa_w[3].rearrange("(c p) n -> c p n", p=128))
        load_w(pwqb, pwq, pwq[:].rearrange("(c p) n -> c p n", p=128))
        dma(skf, skf[:], skd, skd[:].rearrange("c p n -> p c n"))
        cp('dve', skb, skb[:], skf, skf[:])
        memset('dve', vm, vm[:, :, :, 256:257], 1.0)
        for mc in range(2):
            x_t = xt[mc % 2]
            dma(x_t, x_t[:], memb, memb[mc * 128:(mc + 1) * 128, :])
            rmsnorm(x_t, x_t[:], 2, hn, hn[:], junk, ss, rstd)
            for c in range(8):
                tr(pt, pt[:, c, :], hn, hn[:, c * 128:(c + 1) * 128], ident)
            cp('act', memT, memT[:, :, mc * 128:(mc + 1) * 128], pt, pt[:])
        load_w(wtmp, xa_w, xa_w[1].rearrange("(c p) n -> c p n", p=128))
        for oc in range(8):
            for c in range(8):
                mm(pa[0], pa[0][:, 0:256], wtmp, wtmp[:, c, oc * 128:(oc + 1) * 128], memT, memT[:, c, :], start=(c == 0), stop=(c == 7))
            cp('act', kTm, kTm[:, oc, :], pa[0], pa[0][:, 0:256])
        load_w(wtmp, xa_w, xa_w[2].rearrange("(c p) n -> c p n", p=128))
        for mc in range(2):
            for half in range(2):
                for c in range(8):
                    mm(pa[half], pa[half][:], memT, memT[:, c, mc * 128:(mc + 1) * 128], wtmp, wtmp[:, c, half * 512:(half + 1) * 512], start=(c == 0), stop=(c == 7))
                cp('act', vm, vm[:, mc, half * 2:half * 2 + 2, 0:256], pa[half], pa[half][:].rearrange("p (h d) -> p h d", d=256))
        P.pop()
        og2 = P.sb([128, 2, 512], BF16, "og2"); omx = P.sb([128, D], BF16, "omx"); otm = P.sb([128, 512], F32, "otm")
        h1 = P.sb([128, D], F32, "h1"); qTx = P.sb([128, 8, 128], BF16, "qTx")
        Ex = P.sb([128, 2, 4, 128], BF16, "Ex"); rdx = P.sb([128, 4], F32, "rdx")
        oxa = P.sb([128, D], BF16, "oxa")
        hn3T = P.sb([128, 8, 128], BF16, "hn3Ts"); qpT = P.sb([128, 8, 128], BF16, "qpT")
        sc_ = [P.sb([128, 16, 128], F32, "scr%d" % i) for i in range(2)]; ab_ = [P.sb([128, 16, 128], F32, "ab%d" % i) for i in range(2)]
        negm = P.sb([128, 16], F32, "negm"); t16 = P.sb([128, 16, 16], F32, "t16"); scr2_ = [P.sb([128, 128], F32, "scr2_%d" % i) for i in range(4)]
        candall = P.sb([128, 8, 256], F32, "candall"); cand2_ = [P.sb([128, 256], F32, "cand2_%d" % i) for i in range(4)]; c16 = P.sb([128, 8, 16], F32, "c16")
        route = P.sb([128, 16], F32, "route"); zs = P.sb([128, 8], F32, "zs")
        memset('dve', route, route[:], 0.0)
        def Xgen(jj):
            sc = sc_[jj % 2]
            x_t = xt[jj % 2]
            dma(x_t, x_t[:], xo, xo[jj * 128:(jj + 1) * 128, :])
            dma(og2, og2[:], d_ogla, d_ogla[jj * 256:(jj + 1) * 256, :].rearrange("(u p) c -> p u c", p=128))
            dma(omx, omx[:, 512:1024], d_omix, d_omix[jj * 128:(jj + 1) * 128, 512:1024])
            ts('dve', otm, otm[:], og2, og2[:, 0, :], pv[:, 0:1], None, ALU.mult, rd=[pv])
            stt('dve', omx, omx[:, 0:512], og2, og2[:, 1, :], pv[:, 1:2], otm, otm[:], ALU.mult, ALU.add, rd=[pv])
            for c in range(8):
                tr(pt, pt[:, c, :], omx, omx[:, c * 128:(c + 1) * 128], ident)
            cp('act', hnT, hnT[:], pt, pt[:])
            for half in range(2):
                for c in range(8):
                    mm(pa[half], pa[half][:], hnT, hnT[:, c, :], woutb, woutb[:, c, half * 512:(half + 1) * 512], start=(c == 0), stop=(c == 7))
                tt('dve', h1, h1[:, half * 512:(half + 1) * 512], pa[half], pa[half][:], x_t, x_t[:, half * 512:(half + 1) * 512], ALU.add)
            yield
            rmsnorm(h1, h1[:], 1, hn, hn[:], junk, ss, rstd)
            for c in range(8):
                tr(pt, pt[:, c, :], hn, hn[:, c * 128:(c + 1) * 128], ident)
            cp('act', hnT, hnT[:], pt, pt[:])
            for oc in range(8):
                pq_ = pa[2 + oc % 2]
                for c in range(8):
                    mm(pq_, pq_[:, 0:128], wqb, wqb[:, c, oc * 128:(oc + 1) * 128], hnT, hnT[:, c, :], start=(c == 0), stop=(c == 7))
                cp(['act', 'dve'][oc % 2], qTx, qTx[:, oc, :], pq_, pq_[:, 0:128])
                yield
            yield
            for mc in range(2):
                for h in range(4):
                    for dc in range(2):
                        mm(pxa, pxa[:, mc, h * 128:(h + 1) * 128], kTm, kTm[:, h * 2 + dc, mc * 128:(mc + 1) * 128], qTx, qTx[:, h * 2 + dc, :], start=(dc == 0), stop=(dc == 1))
            act(Ex, Ex[:].rearrange("p a h q -> p a (h q)"), pxa, pxa[:], AF.Exp, scale=1.0 / 16)
            for h in range(4):
                o_ap = pxa[:, h // 2, (h % 2) * 256:(h % 2) * 256 + 256]
                for mc in range(2):
                    mm(pa[h % 2], pa[h % 2][:, 0:257], Ex, Ex[:, mc, h, :], vm, vm[:, mc, h, :], start=(mc == 0), stop=(mc == 1))
                ts('dve', rdx, rdx[:, h:h + 1], pa[h % 2], pa[h % 2][:, 256:257], 1e-30, None, ALU.max)
                recip(rdx, rdx[:, h:h + 1], rdx, rdx[:, h:h + 1])
                ts('dve', oxa, oxa[:, h * 256:(h + 1) * 256], pa[h % 2], pa[h % 2][:, 0:256], rdx[:, h:h + 1], None, ALU.mult, rd=[rdx])
            yield
            for c in range(8):
                tr(pt, pt[:, c, :], oxa, oxa[:, c * 128:(c + 1) * 128], ident)
            cp('act', hnT, hnT[:], pt, pt[:])
            for half in range(2):
                for c in range(8):
                    mm(pa[half], pa[half][:], hnT, hnT[:, c, :], wob, wob[:, c, half * 512:(half + 1) * 512], start=(c == 0), stop=(c == 7))
                tt('dve', h1, h1[:, half * 512:(half + 1) * 512], pa[half], pa[half][:], h1, h1[:, half * 512:(half + 1) * 512], ALU.add)
            dma(d_h, d_h[jj * 128:(jj + 1) * 128, :], h1, h1[:])
            yield
            rmsnorm(h1, h1[:], 3, hn, hn[:], junk, ss, rstd)
            for c in range(8):
                tr(pt, pt[:, c, :], hn, hn[:, c * 128:(c + 1) * 128], ident)
            cp('act', hn3T, hn3T[:], pt, pt[:])
            dma(d_hn3T, d_hn3T[:, :, jj * 128:(jj + 1) * 128], hn3T, hn3T[:])
            for oc in range(8):
                pq_ = pa[2 + oc % 2]
                for c in range(8):
                    mm(pq_, pq_[:, 0:128], pwqb, pwqb[:, c, oc * 128:(oc + 1) * 128], hn3T, hn3T[:, c, :], start=(c == 0), stop=(c == 7))
                cp(['act', 'dve'][oc % 2], qpT, qpT[:, oc, :], pq_, pq_[:, 0:128])
                yield
            yield
            for oc in range(8):
                pq_ = pa[oc % 2]
                mm(pq_, pq_[:, 0:256], qpT, qpT[:, oc, :], skb, skb[:, oc, :])
                cp(['act', 'dve'][oc % 2], sc, sc[:, 2 * oc:2 * oc + 2, :], pq_, pq_[:, 0:256].rearrange("p (a k) -> p a k", a=2))
            yield

        def Rgen(jj):
            sc = sc_[jj % 2]; ab = ab_[jj % 2]
            P.op('dve', lambda e: e.tensor_reduce(out=negm[:], in_=sc[:], axis=AX.X, op=ALU.max), [sc], [negm])
            ts('dve', negm, negm[:], negm, negm[:], -1.0, None, ALU.mult)
            for r in range(16):
                act(ab, ab[:, r, :], sc, sc[:, r, :], AF.Exp, bias=negm[:, r:r + 1], rd=[negm])
            yield
            for r0 in range(0, 16, 4):
                for r in range(r0, r0 + 4):
                    P.op('dve', lambda e, r=r: e.max(out=t16[:, r, 0:8], in_=ab[:, r, :]), [ab], [t16])
                for r in range(r0, r0 + 4):
                    P.op('dve', lambda e, r=r: e.match_replace(out=scr2_[r % 4][:], in_to_replace=t16[:, r, 0:8], in_values=ab[:, r, :], imm_value=-1.0), [ab, t16], [scr2_[r % 4]])
                for r in range(r0, r0 + 4):
                    P.op('dve', lambda e, r=r: e.max(out=t16[:, r, 8:16], in_=scr2_[r % 4][:]), [scr2_[r % 4]], [t16])
                yield
            t16v = t16[:].rearrange("p (h a) k -> p h a k", a=2)
            abv = ab[:].rearrange("p (h a) k -> p h a k", a=2)

            def cand_top16():
                yield
                tt('dve', candall, candall[:].rearrange("p h (a b) -> p h a b", a=16),
                   t16, t16v[:, :, 0, :].unsqueeze(3).to_broadcast([128, 8, 16, 16]),
                   t16, t16v[:, :, 1, :].unsqueeze(2).to_broadcast([128, 8, 16, 16]), ALU.mult)
                for h0 in range(0, 8, 4):
                    for h in range(h0, h0 + 4):
                        P.op('dve', lambda e, h=h: e.max(out=c16[:, h, 0:8], in_=candall[:, h, :]), [candall], [c16])
                    for h in range(h0, h0 + 4):
                        P.op('dve', lambda e, h=h: e.match_replace(out=cand2_[h % 4][:], in_to_replace=c16[:, h, 0:8], in_values=candall[:, h, :], imm_value=-1.0), [candall, c16], [cand2_[h % 4]])
                    for h in range(h0, h0 + 4):
                        P.op('dve', lambda e, h=h: e.max(out=c16[:, h, 8:16], in_=cand2_[h % 4][:]), [cand2_[h % 4]], [c16])
                    yield
            yield from cand_top16()
            P.op('dve', lambda e: e.tensor_reduce(out=zs[:], in_=c16[:], axis=AX.X, op=ALU.add), [c16], [zs])
            recip(zs, zs[:], zs, zs[:])
            tt('dve', ab, abv[:, :, 1, :], ab, abv[:, :, 1, :], zs, zs[:].unsqueeze(2).to_broadcast([128, 8, 128]), ALU.mult)
            stt('dve', route, route[:, 0:8], c16, c16[:, :, 15], 1.0 - 1e-6, zs, zs[:], ALU.mult, ALU.mult)
            dma(d_route, d_route[jj * 128:(jj + 1) * 128, 0:2048], ab, ab[:].rearrange("p r k -> p (r k)"))
            dma(d_route, d_route[jj * 128:(jj + 1) * 128, 2048:2064], route, route[:])
            yield

        def drain(g):
            for _ in g:
                pass
        drain(Xgen(0))
        for jj in range(NO):
            gr = Rgen(jj)
            gx = Xgen(jj + 1) if jj + 1 < NO else iter(())
            ra = xa_ = True
            while ra or xa_:
                if xa_:
                    xa_ = next(gx, 'END') != 'END'
                if ra:
                    ra = next(gr, 'END') != 'END'
        P.pop()

    if upto >= 4:
        P.push()
        TG = 2
        IC = 16
        NCH = 128 // IC
        ACT_HEADS = (2, 4, 6)
        hT = P.sb([128, 8, TG * 128], BF16, "hT")
        ab = [P.sb([128, 16, 128], F32, "ab4_%d" % u) for u in range(TG)]
        rt = [P.sb([128, 16], F32, "rt%d" % u) for u in range(TG)]
        Wc = [[P.sb([128, IC * 128], BF16, "Wc%d_%d" % (i, u)) for u in range(TG)] for i in range(2)]
        et = [P.sb([128, IC, 128], F32, "et%d" % i) for i in range(4)]
        mt = [P.sb([128, IC, 128], BF16, "mt%d" % i) for i in range(2)]
        dnb = [P.sb([128, 8, 512], BF16, "dnb%d" % i) for i in range(3)]
        upb = [P.sb([128, 4, D], BF16, "upb%d" % i) for i in range(3)]
        Gs = [P.sb([128, 512], BF16, "G%d" % i) for i in range(2)]
        GT = [P.sb([128, 4, 128], BF16, "GT%d" % i) for i in range(2)]
        py = [P.ps([128, 2, 512], F32, "py%d" % u) for u in range(TG)]
        pd = [P.ps([128, 512], F32, "pd%d" % i) for i in range(2)]
        ptg = [P.ps([128, 8, 128], BF16, "ptg%d" % i) for i in range(2)]
        h2 = P.sb([128, D], F32, "h2"); yo = P.sb([128, D], F32, "yo")
        junk = P.sb([128, D], BF16, "junk4"); ss = P.sb([128, 1], F32, "ss4"); rstd = P.sb([128, 1], F32, "rstd4")
        qn = [0]
        for tg in range(NO // TG):
            dma(hT, hT[:], d_hn3T, d_hn3T[:, :, tg * TG * 128:(tg + 1) * TG * 128])
            for u in range(TG):
                j = tg * TG + u
                dma(ab[u], ab[u][:].rearrange("p r k -> p (r k)"), d_route, d_route[j * 128:(j + 1) * 128, 0:2048])
                dma(rt[u], rt[u][:], d_route, d_route[j * 128:(j + 1) * 128, 2048:2064])

            def wgen(c):
                its = [(u, h) for u in range(TG) for h in range(8)]
                K = len(its)
                eb_ = {}; mb_ = {}

                def E_(n):
                    u, h = its[n]
                    e_ = et[qn[0] % 4]; qn[0] += 1
                    eb_[n] = e_
                    if h in ACT_HEADS:
                        for i_ in range(IC):
                            P.op('act', lambda e, e_=e_, i_=i_, u=u, h=h: e.activation(out=e_[:, i_, :], in_=ab[u][:, 2 * h + 1, :], func=AF.Copy,
                                                                                  scale=ab[u][:, 2 * h, c * IC + i_:c * IC + i_ + 1]), [ab[u]], [e_])
                    else:
                        tt('dve', e_, e_[:], ab[u], ab[u][:, 2 * h, c * IC:(c + 1) * IC].unsqueeze(2).to_broadcast([128, IC, 128]),
                           ab[u], ab[u][:, 2 * h + 1, :].unsqueeze(1).to_broadcast([128, IC, 128]), ALU.mult)

                def S_(n):
                    u, h = its[n]
                    e_ = eb_.pop(n)
                    w_ap = Wc[c % 2][u][:].rearrange("p (a b) -> p a b", a=IC)
                    if h == 0:
                        stt('dve', Wc[c % 2][u], w_ap, e_, e_[:], rt[u][:, h:h + 1], e_, e_[:], ALU.is_ge, ALU.mult, rd=[rt[u]])
                    else:
                        m_ = mt[n % 2]
                        mb_[n] = m_
                        stt('dve', m_, m_[:], e_, e_[:], rt[u][:, h:h + 1], e_, e_[:], ALU.is_ge, ALU.mult, rd=[rt[u]])

                def A_(n):
                    u, h = its[n]
                    if h == 0:
                        return
                    m_ = mb_.pop(n)
                    w_ap = Wc[c % 2][u][:].rearrange("p (a b) -> p a b", a=IC)
                    tt('dve', Wc[c % 2][u], w_ap, Wc[c % 2][u], w_ap, m_, m_[:], ALU.add)
                for n in range(K + 2):
                    if n < K:
                        E_(n)
                    if 1 <= n <= K:
                        S_(n - 1)
                    if n >= 2:
                        A_(n - 2)
                    yield

            def load_w(ecx):
                dn, ub = dnb[ecx % 3], upb[ecx % 3]
                dma(dn, dn[:], d_downT, d_downT[:, ecx * 512:(ecx + 1) * 512].rearrange("(c p) e -> p c e", p=128))
                dma(ub, ub[:], d_up, d_up[ecx * 512:(ecx + 1) * 512, :].rearrange("(s p) d -> p s d", p=128))

            items = [(ecx, u) for ecx in range(32) for u in range(TG)]
            N = len(items)

            def stA(n):
                ecx, u = items[n]
                if u == 0:
                    if ecx == 0:
                        load_w(0)
                    if ecx + 1 < 32:
                        load_w(ecx + 1)
                    if ecx % 4 == 0:
                        for _ in wg[0]:
                            pass
                        wg[0] = wgen(ecx // 4 + 1) if ecx // 4 + 1 < NCH else iter(())
                for _ in range(3):
                    next(wg[0], None)
                dn = dnb[ecx % 3]
                pdt = pd[n % 2]; G = Gs[n % 2]
                for c in range(8):
                    mm(pdt, pdt[:], hT, hT[:, c, u * 128:(u + 1) * 128], dn, dn[:, c, :], start=(c == 0), stop=(c == 7))
                act(G, G[:], pdt, pdt[:], AF.Gelu_apprx_tanh)
                wch = Wc[(ecx // 4) % 2][u]
                tt('dve', G, G[:], G, G[:], wch, wch[:, (ecx % 4) * 512:(ecx % 4 + 1) * 512], ALU.mult)

            def stB(n):
                G = Gs[n % 2]; gt_ = GT[n % 2]; pt_ = ptg[n % 2]
                for s_ in range(4):
                    tr(pt_, pt_[:, s_, :], G, G[:, s_ * 128:(s_ + 1) * 128], ident)
                cp('act', gt_, gt_[:], pt_, pt_[:, 0:4, :])

            def stC(n):
                ecx, u = items[n]
                gt_ = GT[n % 2]; ub = upb[ecx % 3]
                for half in range(2):
                    for s_ in range(4):
                        mm(py[u], py[u][:, half, :], gt_, gt_[:, s_, :], ub, ub[:, s_, half * 512:(half + 1) * 512],
                           start=(ecx == 0 and s_ == 0), stop=(ecx == 31 and s_ == 3))

            wg = [iter(())]
            for _ in wgen(0):
                pass
            for n in range(N + 2):
                if n < N:
                    stA(n)
                if 1 <= n <= N:
                    stB(n - 1)
                if n >= 2:
                    stC(n - 2)
            for _ in wg[0]:
                pass
            for u in range(TG):
                j = tg * TG + u
                dma(h2, h2[:], d_h, d_h[j * 128:(j + 1) * 128, :])
                tt('dve', h2, h2[:], h2, h2[:], py[u], py[u][:].rearrange("p a b -> p (a b)"), ALU.add)
                rmsnorm(h2, h2[:], 4, yo, yo[:], junk, ss, rstd)
                dma(out, out[j * 128:(j + 1) * 128, :], yo, yo[:])
        P.pop()

    P.finish()
    return nc, dict(npair=npair, pair_off=pair_off, NCC=NCC, NCMP=NCMP, ninst=P.ninst)


def _t5_bucket_np(dist):
    n = np.maximum(dist, 0)
    nf = np.maximum(n, 1).astype(np.float32)
    log_ratio = (np.log(nf / np.float32(16)) / np.float32(math.log(2048 / 16))).astype(np.float32)
    large = 16 + (log_ratio * np.float32(16)).astype(np.int32)
    large = np.minimum(large, 31)
    return np.where(n < 16, n, large).astype(np.int64)


def _bias_tile(rel_bias, g, dist, valid):
    bk = _t5_bucket_np(dist)
    outt = np.empty((128, 4, 128), np.float32)
    for h in range(4):
        outt[:, h, :] = np.where(valid, rel_bias[bk, g * 4 + h], np.float32(NEG))
    return outt.reshape(128, 512)


def make_core_inputs(inputs, T, b, p, meta):
    NT = T // 128; NO = NT // 2; NS = T // 64; NCMP = meta['NCMP']; NCC = meta['NCC']
    f = lambda a: np.ascontiguousarray(np.asarray(a, dtype=np.float32))
    x = f(inputs['x'][b]); rel_bias = f(inputs['rel_bias'])
    m = {}
    m['xb'] = x
    m['xo'] = np.ascontiguousarray(x.reshape(NO, 2, 128, D)[:, p].reshape(NO * 128, D))
    m['mem'] = f(inputs['mem'][b])
    w_in = f(inputs['w_in'][0])
    m['w_in'] = np.ascontiguousarray(np.concatenate([w_in[:, ORIG[k][0]:ORIG[k][1]] for k in PERM_ORDER], axis=1))
    m['gw2'] = f(inputs['gla_gate_w2'][0]); m['gb'] = f(inputs['gla_gate_b'][0]).reshape(1, 256)
    m['gnorm'] = f(inputs['gla_out_norm'][0]).reshape(1, 128)
    m['norms'] = np.stack([f(inputs['norm_mix'][0]), f(inputs['norm_xattn'][0]), f(inputs['norm_mem'][0]),
                           f(inputs['norm_ffn'][0]), f(inputs['norm_final'])], 0)
    cw1 = np.stack([f(inputs['cmp_k_w1'][0]), f(inputs['cmp_v_w1'][0])], 0)
    m['cw1'] = np.ascontiguousarray(cw1.reshape(2, 32, 64, 128).transpose(0, 2, 1, 3))
    cpos = np.stack([f(inputs['cmp_pos_k'][0]), f(inputs['cmp_pos_v'][0])], 0)
    m['cpos'] = np.ascontiguousarray(cpos.transpose(0, 2, 1))
    m['cw2'] = np.stack([f(inputs['cmp_k_w2'][0]), f(inputs['cmp_v_w2'][0])], 0)
    m['w_out'] = f(inputs['w_out'][0])
    m['xa_w'] = np.stack([f(inputs['xa_wq'][0]), f(inputs['xa_wk'][0]), f(inputs['xa_wv'][0]), f(inputs['xa_wo'][0])], 0)
    m['pwq'] = f(inputs['peer_wq'][0])
    sk = f(inputs['peer_subkeys'][0])
    skd = np.zeros((8, 128, 256), np.float32)
    for h in range(8):
        for pp in range(2):
            skd[h, pp * 64:(pp + 1) * 64, pp * 128:(pp + 1) * 128] = sk[h, pp].T
    m['skd'] = skd
    m['downT'] = np.ascontiguousarray(f(inputs['peer_down'][0]).T)
    m['up'] = f(inputs['peer_up'][0])
    m['c_ident'] = np.eye(128, dtype=np.float32)
    s_ = np.arange(128)[:, None]; t_ = np.arange(128)[None, :]
    m['c_ucs'] = np.where(s_ <= t_, -1.0 / 16, 0.0).astype(np.float32)
    m['c_urev'] = np.where(s_ > t_, -1.0 / 16, 0.0).astype(np.float32)
    m['c_causal'] = (s_ <= t_).astype(np.float32)
    m['c_selu'] = np.stack([np.eye(128) * (1 - p), np.eye(128) * p], 0).astype(np.float32)
    m['c_pv'] = np.tile(np.array([[1.0 - p, float(p)]], np.float32), (128, 1))
    kk = np.arange(128)[:, None]; qq = np.arange(128)[None, :]
    selb = np.empty((2, 128, 15, 512), np.float32); winb = np.empty((2, 128, 6, 512), np.float32)
    for g in range(2):
        for mm_ in range(15):
            j = mm_ - 1 + p
            dist = 128 * j + qq - kk
            selb[g, :, mm_, :] = _bias_tile(rel_bias, g, dist, (dist >= 0) & (j >= 0))
        for mm_ in range(6):
            j = mm_ - 1 + p
            dist = 128 * j + qq - kk
            winb[g, :, mm_, :] = _bias_tile(rel_bias, g, dist, (dist >= 0) & (dist < 512) & (j >= 0))
    m['c_selb'] = selb; m['c_winb'] = winb
    cmpb = np.empty((2, meta['npair'], 128, 512), np.float32)
    for jj in range(NO):
        for c in range(min(NCC, cmp_nchunks(jj))):
            n = 128 * c + kk
            t = (2 * jj + p) * 128 + qq
            dist = t - (16 * n + 31)
            for g in range(2):
                cmpb[g, meta['pair_off'][jj] + c] = _bias_tile(rel_bias, g, dist, (dist >= 0) & (n < NCMP))
    m['c_cmpb'] = cmpb
    far = np.empty((2, 128, 4, 128), np.float32)
    for g in range(2):
        for h in range(4):
            far[g, :, h, :] = rel_bias[31, g * 4 + h]
    m['c_far'] = far.reshape(2, 128, 512)
    wimp = np.zeros((NCC * 128, 128), np.float32)
    for s in range(NS):
        for r in range(-1, 4):
            n = 4 * s + r
            lo = 16 * r
            ov = min(lo + 32, 64) - max(lo, 0)
            if 0 <= n < NCMP:
                wimp[n, s] += ov / 32.0
    m['c_wimp'] = wimp.reshape(NCC, 128, 128)
    m12 = np.zeros((NO, 128, 2, 128), np.float32)
    sid = np.arange(128)[None, :]
    for jj in range(NO):
        t = (2 * jj + p) * 128 + np.arange(128)[:, None]
        cur = t // 64
        visible = (sid * 64 <= t) & (sid < NS)
        f0 = (sid == 0); f1 = (sid == cur); f2 = (sid == cur - 1)
        forced = f0 | f1 | f2
        m12[jj, :, 0, :] = (visible & ~forced)
        add = np.where(~visible, -100.0 - sid, 0.0)
        add = np.where(f2, 100.0, add); add = np.where(f1, 101.0, add); add = np.where(f0 & (sid < NS), 102.0, add)
        m12[jj, :, 1, :] = add
    m['c_m12'] = m12
    ex = np.zeros((128, T), np.float32)
    ex[np.arange(T) // 64, np.arange(T)] = 1.0
    m['c_ex'] = ex.astype(NPBF)
    return m


_CACHE = {}


def kernel(**inputs):
    T = inputs['x'].shape[1]
    B = inputs['x'].shape[0]
    if T not in _CACHE:
        _CACHE[T] = build(T)
    nc, meta = _CACHE[T]
    in_maps = []
    for c in range(2 * B):
        in_maps.append(make_core_inputs(inputs, T, c // 2, c % 2, meta))
    res = run_bass_kernel_spmd(nc, in_maps, core_ids=list(range(2 * B)))
    NO = T // 256
    outp = np.empty((B, T // 128, 128, D), np.float32)
    for c in range(2 * B):
        o = np.asarray(res.results[c]["out"], dtype=np.float32).reshape(NO, 128, D)
        outp[c // 2, (c % 2)::2] = o
    return outp.reshape(B, T, D)
```

```python
import math
import numpy as np
import ml_dtypes
import concourse.bass as bass
import concourse.mybir as mybir
from concourse.bass_utils import run_bass_kernel_spmd
from contextlib import ExitStack

F32 = mybir.dt.float32
BF16 = mybir.dt.bfloat16
AF = mybir.ActivationFunctionType
ALU = mybir.AluOpType
AX = mybir.AxisListType
NPBF = ml_dtypes.bfloat16

D = 1024
NEG = -30000.0


class T:
    def __init__(self, h, name):
        self.h = h
        self.name = name
        self.w = None
        self.r = []
        self.psum = False

    def __getitem__(self, k):
        return self.h[k]


class Prog:
    def __init__(self, nc, n_dma_sems=48):
        self.nc = nc
        self.es = ExitStack()
        self.scopes = []
        self.eng = {'pe': nc.tensor, 'dve': nc.vector, 'act': nc.scalar,
                    'pool': nc.gpsimd, 'sp': nc.sync}
        self.sems = {}
        for k in self.eng:
            self.sems['e_' + k] = self.es.enter_context(nc.semaphore('e_' + k))
        self.cnt = {k: 0 for k in self.sems}
        self.ndma = n_dma_sems
        for i in range(n_dma_sems):
            key = 'd_%d' % i
            self.sems[key] = self.es.enter_context(nc.semaphore(key))
            self.cnt[key] = 0
        self.dma_rr = 0
        self.known = {k: {} for k in self.eng}
        self.ntile = 0
        self.ninst = 0

    def push(self):
        self.scopes.append(ExitStack())

    def pop(self):
        self.barrier()
        self.scopes.pop().close()

    def _stack(self):
        return self.scopes[-1] if self.scopes else self.es

    def sb(self, shape, dt, name=None):
        self.ntile += 1
        name = (name or 't') + '_%d' % self.ntile
        h = self._stack().enter_context(self.nc.sbuf_tensor(name, list(shape), dt))
        return T(h, name)

    def ps(self, shape, dt, name=None):
        self.ntile += 1
        name = (name or 'p') + '_%d' % self.ntile
        h = self._stack().enter_context(self.nc.psum_tensor(name, list(shape), dt))
        t = T(h, name)
        t.psum = True
        return t

    def dram(self, name, shape, dt, kind="Internal"):
        h = self.nc.dram_tensor(name, list(shape), dt, kind=kind).ap()
        return T(h, name)

    def _deps(self, reads, writes, e=None):
        deps = []
        for t in reads:
            if t.w is not None:
                deps.append(t.w)
            if t.psum:
                deps.extend([tok for tok in t.r if tok[0] != 'e_' + str(e)])
        for t in writes:
            if t.w is not None:
                deps.append(t.w)
            deps.extend(t.r)
        return deps

    def _waits(self, e, deps):
        kn = self.known[e]
        need = {}
        for (k, v) in deps:
            if kn.get(k, 0) >= v:
                continue
            if need.get(k, 0) < v:
                need[k] = v
        for k, v in need.items():
            kn[k] = v
        return list(need.items())

    def _emit(self, e, waits, fn, inc):
        eng = self.eng[e]
        for (k, v) in waits:
            eng.wait_ge(self.sems[k], v)
        ins = fn(eng)
        ins.then_inc(self.sems[inc[0]], inc[1])
        self.ninst += 1

    def op(self, e, fn, reads=(), writes=()):
        deps = self._deps(reads, writes, e)
        key = 'e_' + e
        if e == 'pe':
            deps = [d for d in deps if d[0] != key]
        waits = self._waits(e, deps)
        self.cnt[key] += 1
        tok = (key, self.cnt[key])
        self._emit(e, waits, fn, (key, 1))
        for t in reads:
            t.r.append(tok)
        for t in writes:
            t.w = tok
            t.r = []
        return tok

    def dma(self, e, out_t, out_ap, in_t, in_ap, **kw):
        reads = [in_t]
        writes = [out_t]
        deps = self._deps(reads, writes)
        key = 'd_%d' % self.dma_rr
        self.dma_rr = (self.dma_rr + 1) % self.ndma
        if self.cnt[key] > 0:
            deps.append((key, self.cnt[key]))
        waits = self._waits(e, deps)
        self.cnt[key] += 16
        tok = (key, self.cnt[key])
        self._emit(e, waits, lambda eng: eng.dma_start(out=out_ap, in_=in_ap, **kw), (key, 16))
        in_t.r.append(tok)
        out_t.w = tok
        out_t.r = []
        return tok

    def barrier(self):
        allt = [(k, v) for k, v in self.cnt.items() if v > 0]
        for e in self.eng:
            for (k, v) in self._waits(e, allt):
                self.eng[e].wait_ge(self.sems[k], v)

    def finish(self):
        self.barrier()
        while self.scopes:
            self.scopes.pop().close()
        self.es.close()


ORIG = dict(gq=(0, 256), gk=(256, 512), gv=(512, 1024), gr=(1024, 1536), glr=(1536, 1552),
            nq=(1552, 2064), kc=(2064, 2192), vc=(2192, 2320), ks=(2320, 2448), vs=(2448, 2576),
            kw=(2576, 2704), vw=(2704, 2832), ng=(2832, 2856))
PERM_ORDER = ['gq', 'gk', 'gv', 'gr', 'nq', 'kc', 'vc', 'ks', 'vs', 'kw', 'vw', 'ng', 'glr']
NCOL = 2856


def cmp_nchunks(jj):
    return min(4, (16 * jj + 15 + 127) // 128)


def build(T, debug=False, upto=9, cut=99):
    NT = T // 128
    NO = NT // 2
    NS = T // 64
    TO = T // 2
    NCMP = (T - 32) // 16 + 1
    NCC = (NCMP + 127) // 128
    pair_off = []
    npair = 0
    for jj in range(NO):
        pair_off.append(npair)
        npair += min(NCC, cmp_nchunks(jj))

    nc = bass.Bass("TRN2", target_bir_lowering=False)
    P = Prog(nc)
    SK = "ExternalOutput" if debug else "Internal"

    def inp(name, shape, dt=F32):
        return P.dram(name, shape, dt, kind="ExternalInput")

    xb = inp("xb", [T, D]); xo = inp("xo", [TO, D]); memb = inp("mem", [256, D])
    w_in = inp("w_in", [D, NCOL]); gw2 = inp("gw2", [16, 256]); gb = inp("gb", [1, 256])
    gnorm = inp("gnorm", [1, 128])
    norms = inp("norms", [5, D])
    cw1 = inp("cw1", [2, 64, 32, 128]); cpos = inp("cpos", [2, 64, 32]); cw2 = inp("cw2", [2, 128, 64])
    w_out = inp("w_out", [D, D]); xa_w = inp("xa_w", [4, D, D])
    pwq = inp("pwq", [D, D]); skd = inp("skd", [8, 128, 256])
    downT = inp("downT", [D, 16384]); up = inp("up", [16384, D])
    c_ident = inp("c_ident", [128, 128]); c_ucs = inp("c_ucs", [128, 128]); c_urev = inp("c_urev", [128, 128])
    c_causal = inp("c_causal", [128, 128]); c_selu = inp("c_selu", [2, 128, 128]); c_pv = inp("c_pv", [128, 2])
    c_selb = inp("c_selb", [2, 128, 15, 512]); c_winb = inp("c_winb", [2, 128, 6, 512])
    c_cmpb = inp("c_cmpb", [2, npair, 128, 512])
    c_far = inp("c_far", [2, 128, 512])
    c_wimp = inp("c_wimp", [NCC, 128, 128]); c_m12 = inp("c_m12", [NO, 128, 2, 128])
    c_ex = inp("c_ex", [128, T], BF16)
    out = P.dram("out", [TO, D], F32, kind="ExternalOutput")

    d_nq = P.dram("d_nq", [T, 512], BF16, kind=SK)
    d_ng = P.dram("d_ng", [T, 24], F32, kind=SK)
    d_ogla = P.dram("d_ogla", [T, 512], BF16, kind=SK)
    d_kT = P.dram("d_kT", [4, 128, T], BF16, kind=SK)
    d_vs = P.dram("d_vs", [T, 128], BF16, kind=SK)
    d_vw = P.dram("d_vw", [T, 128], BF16, kind=SK)
    d_omix = P.dram("d_omix", [TO, D], BF16, kind=SK)
    d_h = P.dram("d_h", [TO, D], F32, kind=SK)
    d_hn3T = P.dram("d_hn3T", [128, 8, TO], BF16, kind=SK)
    d_route = P.dram("d_route", [TO, 2064], F32, kind=SK)
    d_downT = P.dram("d_downT", [D, 16384], BF16, kind="Internal")
    d_up = P.dram("d_up", [16384, D], BF16, kind="Internal")
    d_cmp = P.dram("d_cmp", [2, 64, 512], BF16, kind=SK)
    d_dbg = P.dram("d_dbg", [3, TO, 512], F32, kind=SK)
    d_imp = P.dram("d_imp", [2, TO, 128], F32, kind=SK)

    def mm(ot, o_ap, lt, l_ap, rt, r_ap, start=True, stop=True):
        P.op('pe', lambda e: e.matmul(o_ap, lhsT=l_ap, rhs=r_ap, start=start, stop=stop), [lt, rt], [ot])

    def tr(ot, o_ap, it, i_ap, idt):
        P.op('pe', lambda e: e.transpose(out=o_ap, in_=i_ap, identity=idt[:]), [it, idt], [ot])

    def act(ot, o_ap, it, i_ap, func, bias=None, scale=None, accum=None, rd=(), wr=()):
        kw = {}
        if bias is not None:
            kw['bias'] = bias
        if scale is not None:
            kw['scale'] = scale
        if accum is not None:
            kw['accum_out'] = accum
        P.op('act', lambda e: e.activation(out=o_ap, in_=i_ap, func=func, **kw), [it] + list(rd), [ot] + list(wr))

    def cp(eng, ot, o_ap, it, i_ap):
        if eng == 'act':
            P.op('act', lambda e: e.copy(out=o_ap, in_=i_ap), [it], [ot])
        else:
            P.op(eng, lambda e: e.tensor_copy(out=o_ap, in_=i_ap), [it], [ot])

    def tt(eng, ot, o_ap, at, a_ap, bt, b_ap, op):
        P.op(eng, lambda e: e.tensor_tensor(out=o_ap, in0=a_ap, in1=b_ap, op=op), [at, bt], [ot])

    def ts(eng, ot, o_ap, at, a_ap, s1, s2, op0, op1=None, rd=()):
        if op1 is None:
            P.op(eng, lambda e: e.tensor_scalar(out=o_ap, in0=a_ap, scalar1=s1, scalar2=None, op0=op0), [at] + list(rd), [ot])
        else:
            P.op(eng, lambda e: e.tensor_scalar(out=o_ap, in0=a_ap, scalar1=s1, scalar2=s2, op0=op0, op1=op1), [at] + list(rd), [ot])

    def stt(eng, ot, o_ap, at, a_ap, sc, bt, b_ap, op0, op1, rd=()):
        P.op(eng, lambda e: e.scalar_tensor_tensor(out=o_ap, in0=a_ap, scalar=sc, in1=b_ap, op0=op0, op1=op1),
             [at, bt] + list(rd), [ot])

    def memset(eng, t, ap, v):
        P.op(eng, lambda e: e.memset(ap, v), [], [t])

    def recip(ot, o_ap, it, i_ap):
        P.op('dve', lambda e: e.reciprocal(out=o_ap, in_=i_ap), [it], [ot])

    dmaq = ['sp', 'act', 'pool']
    dq = [0]

    def dma(ot, o_ap, it, i_ap, q=None, **kw):
        if q is None:
            q = dmaq[dq[0] % 2]
            dq[0] += 1
        return P.dma(q, ot, o_ap, it, i_ap, **kw)

    ident = P.sb([128, 128], BF16, "ident"); identf = P.sb([128, 128], F32, "identf")
    ones = P.sb([128, 128], BF16, "ones")
    pv = P.sb([128, 2], F32, "pv")
    nrm = P.sb([128, 5, D], F32, "nrm")
    dma(identf, identf[:], c_ident, c_ident[:])
    dma(pv, pv[:], c_pv, c_pv[:])
    for k in range(5):
        dma(nrm, nrm[:, k, :], norms, norms[k:k + 1, :].partition_broadcast(128))
    cp('dve', ident, ident[:], identf, identf[:])
    memset('pool', ones, ones[:], 1.0)
    eps_t = P.sb([128, 1], F32, "eps_t")
    memset('dve', eps_t, eps_t[:], 1e-6)
    kcmpT = [P.sb([128, 512], BF16, "kcmpT%d" % g) for g in range(2)]
    vcmp = [P.sb([128, 4, 65], BF16, "vcmp%d" % g) for g in range(2)]

    def rmsnorm(xt, x_ap, gain_k, hn, hn_ap, junk, ss, rstd, n=D, eps=1e-6):
        memset('dve', ss, ss[:], 0.0)
        act(junk, junk[:, 0:n], xt, x_ap, AF.Square, accum=ss[:], rd=[ss], wr=[ss])
        act(rstd, rstd[:], ss, ss[:], AF.Ln, scale=1.0 / n, bias=eps_t[:, 0:1], rd=[eps_t])
        act(rstd, rstd[:], rstd, rstd[:], AF.Exp, scale=-0.5)
        stt('dve', hn, hn_ap, xt, x_ap, rstd[:, 0:1], nrm, nrm[:, gain_k, 0:n], ALU.mult, ALU.mult, rd=[rstd])

    if upto >= 1:
        P.push()
        winb = P.sb([128, 8, NCOL], BF16, "winb")
        wst = [P.sb([128, NCOL], F32, "wst%d" % i) for i in range(2)]
        w_in_v = w_in[:].rearrange("(c p) n -> c p n", p=128)
        for c in range(8):
            dma(wst[c % 2], wst[c % 2][:], w_in, w_in_v[c])
            cp(['dve', 'pool'][c % 2], winb, winb[:, c, :], wst[c % 2], wst[c % 2][:])
        gw2t = P.sb([16, 256], F32, "gw2t"); gbt = P.sb([1, 256], F32, "gbt"); onesrow = P.sb([1, 128], F32, "onesrow")
        gnt = P.sb([128, 128], F32, "gnt")
        ucs = P.sb([128, 128], F32, "ucs"); urev = P.sb([128, 128], F32, "urev"); causal = P.sb([128, 128], F32, "causal")
        m16 = P.sb([128, 1], F32, "m16")
        dma(gw2t, gw2t[:], gw2, gw2[:]); dma(gbt, gbt[:], gb, gb[:])
        dma(gnt, gnt[:], gnorm, gnorm[0:1, :].partition_broadcast(128))
        dma(ucs, ucs[:], c_ucs, c_ucs[:]); dma(urev, urev[:], c_urev, c_urev[:]); dma(causal, causal[:], c_causal, c_causal[:])
        memset('dve', onesrow, onesrow[:], 1.0)
        memset('dve', m16, m16[:], -1.0 / 16)
        S = P.sb([128, 2, 128], F32, "S"); Sb = P.sb([128, 2, 128], BF16, "Sb")
        memset('dve', S, S[:], 0.0); memset('pool', Sb, Sb[:], 0.0)

        xt = [P.sb([128, D], F32, "xt%d" % i) for i in range(2)]
        junk = P.sb([128, D], BF16, "junk"); ss = P.sb([128, 1], F32, "ss"); rstd = P.sb([128, 1], F32, "rstd")
        hn = P.sb([128, D], BF16, "hn"); hnT = P.sb([128, 8, 128], BF16, "hnT")
        pt = P.ps([128, 8, 128], BF16, "pt")
        pz = [P.ps([128, 512], F32, "pz%d" % i) for i in range(3)]
        pg1 = P.ps([128, 512], F32, "pg1"); pg2 = P.ps([128, 512], F32, "pg2")
        po = P.ps([128, 512], F32, "po"); ptb = P.ps([128, 8, 128], BF16, "ptb")
        glr = P.sb([128, 16], F32, "glr"); glrT = P.sb([16, 128], F32, "glrT")
        e1 = P.sb([128, 256], F32, "e1"); L = P.sb([128, 256], F32, "L")
        eb = P.sb([128, 256], F32, "eb"); enb = P.sb([128, 256], F32, "enb"); ec = P.sb([128, 256], F32, "ec")
        ebl = P.sb([128, 2], F32, "ebl")
        qk = P.sb([128, 3, 256], BF16, "qk")
        qkT = P.sb([128, 4, 128], BF16, "qkT")
        vv = P.sb([128, 512], BF16, "vv"); sg = P.sb([128, 512], F32, "sg")
        AT = P.sb([128, 128], BF16, "AT")
        ssq = P.sb([128, 4], F32, "ssq"); rs4 = P.sb([128, 4], F32, "rs4"); tmpn = P.sb([128, 128], F32, "tmpn")
        junk2 = P.sb([128, 128], F32, "junk2")
        og = P.sb([128, 512], BF16, "og"); otmp4 = P.sb([128, 512], F32, "otmp4")
        nqs = P.sb([128, 512], BF16, "nqs"); ngs = P.sb([128, 24], F32, "ngs")
        kk = P.sb([128, 4, 128], BF16, "kk"); kkT = P.sb([128, 4, 128], BF16, "kkT")
        vsw = P.sb([128, 2, 128], BF16, "vsw")
        cast_jobs = []
        if upto >= 4:
            stg = [P.sb([128, 4096], F32, "stg%d" % i) for i in range(2)]
            stb = [P.sb([128, 4096], BF16, "stb%d" % i) for i in range(2)]
            for r in range(8):
                for c in range(4):
                    cast_jobs.append((downT, downT[r * 128:(r + 1) * 128, c * 4096:(c + 1) * 4096],
                                      d_downT, d_downT[r * 128:(r + 1) * 128, c * 4096:(c + 1) * 4096]))
            sv = up[:].rearrange("(a p f) d -> a p (f d)", p=128, f=4)
            dv = d_up[:].rearrange("(a p f) d -> a p (f d)", p=128, f=4)
            for a in range(32):
                cast_jobs.append((up, sv[a], d_up, dv[a]))
        cj = [0]

        def cast_job():
            if cj[0] >= len(cast_jobs):
                return
            src_t, s_ap, dst_t, d_ap = cast_jobs[cj[0]]
            a, b_ = stg[cj[0] % 2], stb[cj[0] % 2]
            P.dma('pool', a, a[:], src_t, s_ap)
            for q_ in range(4):
                cp('act', b_, b_[:, q_ * 1024:(q_ + 1) * 1024], a, a[:, q_ * 1024:(q_ + 1) * 1024])
            P.dma('pool', dst_t, d_ap, b_, b_[:])
            cj[0] += 1
        cA, cB, cC, cD, cE, cF = 0, 512, 1024, 1536, 2048, 2560
        for i in range(NT):
            x_t = xt[i % 2]
            dma(x_t, x_t[:], xb, xb[i * 128:(i + 1) * 128, :])
            rmsnorm(x_t, x_t[:], 0, hn, hn[:], junk, ss, rstd)
            for c in range(8):
                tr(pt, pt[:, c, :], hn, hn[:, c * 128:(c + 1) * 128], ident)
            cp('act', hnT, hnT[:], pt, pt[:])

            def zgroup(pz_t, c0, n):
                for c in range(8):
                    mm(pz_t, pz_t[:, 0:n], hnT, hnT[:, c, :], winb, winb[:, c, c0:c0 + n], start=(c == 0), stop=(c == 7))
            zgroup(pz[0], cF, 296)
            cp('act', kk, kk[:, 3, :], pz[0], pz[0][:, 0:128])
            cp('act', vsw, vsw[:, 1, :], pz[0], pz[0][:, 128:256])
            cp('act', glr, glr[:], pz[0], pz[0][:, 280:296])
            act(ngs, ngs[:], pz[0], pz[0][:, 256:280], AF.Exp, scale=-1.0)
            ts('dve', ngs, ngs[:], ngs, ngs[:], 1.0, None, ALU.add)
            recip(ngs, ngs[:], ngs, ngs[:])
            dma(d_ng, d_ng[i * 128:(i + 1) * 128, :], ngs, ngs[:])
            zgroup(pz[1], cE, 512)
            cp('act', kk, kk[:, 0:3, :], pz[1], pz[1][:, 0:384].rearrange("p (a b) -> p a b", a=3))
            cp('dve', vsw, vsw[:, 0, :], pz[1], pz[1][:, 384:512])
            dma(d_vs, d_vs[i * 128:(i + 1) * 128, :], vsw, vsw[:, 0, :])
            dma(d_vw, d_vw[i * 128:(i + 1) * 128, :], vsw, vsw[:, 1, :])
            for a in range(4):
                tr(ptb, ptb[:, a, :], kk, kk[:, a, :], ident)
            cp('act', kkT, kkT[:], ptb, ptb[:, 0:4, :])
            dma(d_kT, d_kT[:, :, i * 128:(i + 1) * 128].rearrange("a p t -> p a t"), kkT, kkT[:])
            zgroup(pz[2], cD, 512)
            P.op('act', lambda e, pzt=pz[2]: e.mul(out=nqs[:], in_=pzt[:], mul=0.125), [pz[2]], [nqs])
            dma(d_nq, d_nq[i * 128:(i + 1) * 128, :], nqs, nqs[:])
            tr(pg1, pg1[0:16, 0:128], glr, glr[:], identf)
            cp('dve', glrT, glrT[:], pg1, pg1[0:16, 0:128])
            mm(pg1, pg1[:, 256:512], glrT, glrT[:], gw2t, gw2t[:], start=True, stop=False)
            mm(pg1, pg1[:, 256:512], onesrow, onesrow[:], gbt, gbt[:], start=False, stop=True)
            zgroup(pz[0], cA, 512)
            zgroup(pz[1], cB, 512)
            zgroup(pz[2], cC, 512)
            act(e1, e1[:], pg1, pg1[:, 256:512], AF.Exp, scale=-1.0)
            act(L, L[:], e1, e1[:], AF.Ln, bias=1.0)
            cp('act', vv, vv[:], pz[1], pz[1][:])
            act(sg, sg[:], pz[2], pz[2][:], AF.Exp, scale=-1.0)
            ts('dve', sg, sg[:], sg, sg[:], 1.0, None, ALU.add)
            recip(sg, sg[:], sg, sg[:])
            tt('dve', sg, sg[:], sg, sg[:], pz[2], pz[2][:], ALU.mult)
            tt('pool', sg, sg[:].rearrange("p (h d) -> p h d", h=4), sg, sg[:].rearrange("p (h d) -> p h d", h=4),
               gnt, gnt[:].unsqueeze(1).to_broadcast([128, 4, 128]), ALU.mult)
            mm(pg2, pg2[:, 0:256], ucs, ucs[:], L, L[:])
            mm(pg2, pg2[:, 256:512], urev, urev[:], L, L[:])
            for hp in range(2):
                mm(pg1, pg1[:, hp:hp + 1], L, L[:, hp * 128:(hp + 1) * 128], m16, m16[:])
            act(eb, eb[:], pg2, pg2[:, 0:256], AF.Exp)
            act(enb, enb[:], pg2, pg2[:, 0:256], AF.Exp, scale=-1.0)
            act(ec, ec[:], pg2, pg2[:, 256:512], AF.Exp)
            act(ebl, ebl[:], pg1, pg1[:, 0:2], AF.Exp)
            stt('dve', qk, qk[:, 0, :], pz[0], pz[0][:, 0:256], 0.125, eb, eb[:], ALU.mult, ALU.mult)
            tt('dve', qk, qk[:, 1, :], pz[0], pz[0][:, 256:512], enb, enb[:], ALU.mult)
            tt('dve', qk, qk[:, 2, :], pz[0], pz[0][:, 256:512], ec, ec[:], ALU.mult)
            for a in range(4):
                tr(ptb, ptb[:, 4 + a, :], qk, qk[:, a // 2, (a % 2) * 128:(a % 2 + 1) * 128], ident)
            cp('act', qkT, qkT[:], ptb, ptb[:, 4:8, :])
            memset('dve', ssq, ssq[:], 0.0)
            for h in range(4):
                hp, hh = h // 2, h % 2
                pr = slice(hh * 64, hh * 64 + 64)
                mm(pg2, pg2[:, 0:128], qkT, qkT[pr, 2 + hp, :], qkT, qkT[pr, hp, :])
                tt('dve', AT, AT[:], pg2, pg2[:, 0:128], causal, causal[:], ALU.mult)
                o_ap = po[:, h * 128:(h + 1) * 128]
                mm(po, o_ap, AT, AT[:], vv, vv[:, h * 128:(h + 1) * 128], start=True, stop=False)
                mm(po, o_ap, qkT, qkT[pr, hp, :], Sb, Sb[pr, hp, :], start=False, stop=True)
                mm(pg2, pg2[:, 128:256], qk, qk[:, 2, hp * 128:(hp + 1) * 128], vv, vv[:, h * 128:(h + 1) * 128])
                stt('dve', Sb, Sb[pr, hp, :], S, S[pr, hp, :], ebl[pr, hp:hp + 1], pg2, pg2[pr, 128:256], ALU.mult, ALU.add, rd=[ebl])
                stt('dve', S, S[pr, hp, :], S, S[pr, hp, :], ebl[pr, hp:hp + 1], pg2, pg2[pr, 128:256], ALU.mult, ALU.add, rd=[ebl])
                act(junk2, junk2[:], po, o_ap, AF.Square, accum=ssq[:, h:h + 1], rd=[ssq], wr=[ssq])
            act(rs4, rs4[:], ssq, ssq[:], AF.Ln, scale=1.0 / 128, bias=eps_t[:, 0:1], rd=[eps_t])
            act(rs4, rs4[:], rs4, rs4[:], AF.Exp, scale=-0.5)
            tt('dve', otmp4, otmp4[:], po, po[:], sg, sg[:], ALU.mult)
            tt('dve', og, og[:].rearrange("p (h d) -> p h d", h=4), otmp4, otmp4[:].rearrange("p (h d) -> p h d", h=4),
               rs4, rs4[:].unsqueeze(2).to_broadcast([128, 4, 128]), ALU.mult)
            dma(d_ogla, d_ogla[i * 128:(i + 1) * 128, :], og, og[:])
            for _ in range((len(cast_jobs) + NT - 1) // NT):
                cast_job()
        while cj[0] < len(cast_jobs):
            cast_job()
        P.pop()

        P.push()
        w1f = P.sb([64, 32, 128], F32, "w1f"); w1b = P.sb([64, 32, 128], BF16, "w1b")
        posf = P.sb([64, 32], F32, "posf"); posb = P.sb([64, 32], BF16, "posb")
        w2f = P.sb([128, 64], F32, "w2f"); w2b = P.sb([128, 64], BF16, "w2b")
        kTg = P.sb([64, T], BF16, "kTg")
        ph = P.ps([128, 512], F32, "ph"); pb = P.ps([128, 512], F32, "pb"); pc = P.ps([128, 512], F32, "pc")
        bias_h = P.sb([128, 1], F32, "bias_h")
        H = P.sb([128, 512], BF16, "H")
        for g in range(2):
            memset('dve', kcmpT[g], kcmpT[g][:], 0.0)
            memset('dve', vcmp[g], vcmp[g][:], 0.0)
            memset('dve', vcmp[g], vcmp[g][:, :, 64:65], 1.0)
        for kv in range(2):
            dma(w1f, w1f[:], cw1, cw1[kv]); dma(posf, posf[:], cpos, cpos[kv]); dma(w2f, w2f[:], cw2, cw2[kv])
            cp('dve', w1b, w1b[:], w1f, w1f[:]); cp('dve', posb, posb[:], posf, posf[:]); cp('dve', w2b, w2b[:], w2f, w2f[:])
            for l in range(32):
                mm(pb, pb[:, 0:1], w1b, w1b[:, l, :], posb, posb[:, l:l + 1], start=(l == 0), stop=(l == 31))
            cp('dve', bias_h, bias_h[:], pb, pb[:, 0:1])
            for g in range(2):
                dma(kTg, kTg[:], d_kT, d_kT[kv, g * 64:(g + 1) * 64, :])
                for l in range(32):
                    mm(ph, ph[:, 0:NCMP], w1b, w1b[:, l, :], kTg, kTg[:, l:l + 16 * (NCMP - 1) + 1:16],
                       start=(l == 0), stop=(l == 31))
                memset('dve', H, H[:], 0.0)
                act(H, H[:, 0:NCMP], ph, ph[:, 0:NCMP], AF.Gelu_apprx_tanh, bias=bias_h[:, 0:1], rd=[bias_h])
                if kv == 0:
                    mm(pc, pc[0:64, 0:NCMP], w2b, w2b[:], H, H[:, 0:NCMP])
                    cp('act', kcmpT[g], kcmpT[g][0:64, 0:NCMP], pc, pc[0:64, 0:NCMP])
                    if debug:
                        dma(d_cmp, d_cmp[g], kcmpT[g], kcmpT[g][0:64, :])
                else:
                    for c in range(NCC):
                        mm(pc, pc[:, c * 64:(c + 1) * 64], H, H[:, c * 128:(c + 1) * 128], w2b, w2b[:])
                    cp('act', vcmp[g], vcmp[g][:, 0:NCC, 0:64], pc, pc[:, 0:NCC * 64].rearrange("p (c d) -> p c d", d=64))
        P.pop()

    if upto >= 2:
        P.push()
        selu = P.sb([128, 2, 128], BF16, "selu"); seluf = P.sb([128, 2, 128], F32, "seluf")
        dma(seluf, seluf[:], c_selu, c_selu[:].rearrange("a p t -> p a t"))
        cp('dve', selu, selu[:], seluf, seluf[:])
        exm = P.sb([128, T], BF16, "exm")
        dma(exm, exm[:], c_ex, c_ex[:])
        wimpf = P.sb([128, NCC, 128], F32, "wimpf"); wimp = P.sb([128, NCC, 128], BF16, "wimp")
        dma(wimpf, wimpf[:], c_wimp, c_wimp[:].rearrange("c p s -> p c s"))
        cp('dve', wimp, wimp[:], wimpf, wimpf[:])
        ksT = P.sb([128, T], BF16, "ksT"); kwT = P.sb([128, T], BF16, "kwT")
        memset('dve', ksT, ksT[64:128, :], 0.0)
        memset('pool', kwT, kwT[64:128, :], 0.0)
        memset('dve', ksT, ksT[64:65, :], 1.0)
        farf = P.sb([128, 512], F32, "farf")
        vs = P.sb([128, NT, 65], BF16, "vs"); vw = P.sb([128, NT, 65], BF16, "vw")
        selb = P.sb([128, 15, 512], BF16, "selb"); winb2 = P.sb([128, 6, 512], BF16, "winb2")
        bst = [P.sb([128, 512], F32, "bst%d" % i) for i in range(2)]
        cbt = [P.sb([128, 512], BF16, "cbt%d" % i) for i in range(2)]
        qrows_ = [P.sb([128, 2, 256], BF16, "qrows%d" % i) for i in range(2)]; grows_ = [P.sb([128, 2, 24], F32, "grows%d" % i) for i in range(2)]
        gown_ = [P.sb([128, 12], F32, "gown%d" % i) for i in range(2)]; gtmp = P.sb([128, 12], F32, "gtmp")
        qT_ = [P.sb([128, 4, 128], BF16, "qT%d" % i) for i in range(2)]
        for i_ in range(2):
            memset('dve', qT_[i_], qT_[i_][64:128, :, :], 0.0)
        Ec = [P.sb([128, 4, 128], BF16, "Ec%d" % i) for i in range(4)]
        Eb = [P.sb([128, 4, 128], BF16, "Eb%d" % i) for i in range(3)]
        m12_ = [P.sb([128, 2, 128], F32, "m12_%d" % i) for i in range(2)]
        imp = P.sb([128, 128], F32, "imp"); imp2 = P.sb([128, 128], F32, "imp2"); m8 = P.sb([128, 16], F32, "m8")
        mk = P.sb([128, 128], BF16, "mk"); maskT4 = P.sb([128, 4, 128], BF16, "maskT4")
        rden = P.sb([128, 4], F32, "rden"); coef = P.sb([128, 4], F32, "coef")
        oacc = P.sb([128, 4, 64], F32, "oacc"); otmp = P.sb([128, 4, 64], F32, "otmp"); onsa = P.sb([128, 256], BF16, "onsa")
        pq = P.ps([64, 4, 128], F32, "pq")
        psc = [P.ps([128, 4, 128], F32, "psc%d" % i) for i in range(2)]
        pn = P.ps([128, 4, 128], F32, "pn")
        pnT = P.ps([65, 4, 128], F32, "pnT")
        pnT2 = P.ps([65, 4, 128], F32, "pnT2")
        numTs = P.sb([65, 4, 128], F32, "numTs")
        pimp = P.ps([128, 4, 128], F32, "pimp")
        pmt = P.ps([128, 128], BF16, "pmt")
        for g in range(2):
            dma(ksT, ksT[0:64, :], d_kT, d_kT[2, g * 64:(g + 1) * 64, :])
            dma(farf, farf[:], c_far, c_far[g])
            for i_ in range(2):
                cp('dve', qT_[i_], qT_[i_][64:65, :, :], farf, farf[64:65, :].rearrange("p (h q) -> p h q", h=4))
            dma(kwT, kwT[0:64, :], d_kT, d_kT[3, g * 64:(g + 1) * 64, :])
            for n0 in range(0, NT, 16):
                n1 = min(NT, n0 + 16)
                dma(vs, vs[:, n0:n1, 0:64], d_vs, d_vs[n0 * 128:n1 * 128, g * 64:(g + 1) * 64].rearrange("(n p) c -> p n c", p=128))
                dma(vw, vw[:, n0:n1, 0:64], d_vw, d_vw[n0 * 128:n1 * 128, g * 64:(g + 1) * 64].rearrange("(n p) c -> p n c", p=128))
            memset('dve', vs, vs[:, :, 64:65], 1.0)
            memset('dve', vw, vw[:, :, 64:65], 1.0)
            k = 0
            for m in range(15):
                dma(bst[k % 2], bst[k % 2][:], c_selb, c_selb[g, :, m, :])
                tt('pool', selb, selb[:, m, :], bst[k % 2], bst[k % 2][:], farf, farf[:], ALU.subtract); k += 1
            for m in range(6):
                dma(bst[k % 2], bst[k % 2][:], c_winb, c_winb[g, :, m, :])
                cp('pool', winb2, winb2[:, m, :], bst[k % 2], bst[k % 2][:]); k += 1
            ne = 0
            ne_ = [0]
            for jj in range(NO):
                qrows, grows, gown, qT, m12 = qrows_[jj % 2], grows_[jj % 2], gown_[jj % 2], qT_[jj % 2], m12_[jj % 2]
                dma(qrows, qrows[:], d_nq, d_nq[jj * 256:(jj + 1) * 256, g * 256:(g + 1) * 256].rearrange("(u p) c -> p u c", p=128))
                dma(grows, grows[:], d_ng, d_ng[jj * 256:(jj + 1) * 256, :].rearrange("(u p) c -> p u c", p=128))
                dma(m12, m12[:], c_m12, c_m12[jj])
                for h in range(4):
                    for u in range(2):
                        mm(pq, pq[:, h, :], qrows, qrows[:, u, h * 64:(h + 1) * 64], selu, selu[:, u, :], start=(u == 0), stop=(u == 1))
                cp('act', qT, qT[0:64], pq, pq[:])
                ts('dve', gtmp, gtmp[:], grows, grows[:, 0, g * 12:(g + 1) * 12], pv[:, 0:1], None, ALU.mult, rd=[pv])
                stt('dve', gown, gown[:], grows, grows[:, 1, g * 12:(g + 1) * 12], pv[:, 1:2], gtmp, gtmp[:], ALU.mult, ALU.add, rd=[pv])
                gv3 = gown[:].rearrange("p (h k) -> p h k", k=3)
                qT_all = qT[:].rearrange("p h q -> p (h q)")
                qT_aug = qT_all

                def finish_branch(pn, br, first):
                    ts('dve', rden, rden[:], pn, pn[:, :, 64], 1e-30, None, ALU.max)
                    recip(rden, rden[:], rden, rden[:])
                    tt('dve', coef, coef[:], rden, rden[:], gown, gv3[:, :, br], ALU.mult)
                    dst = oacc if first else otmp
                    tt('dve', dst, dst[:], pn, pn[:, :, 0:64], coef, coef[:].unsqueeze(2).to_broadcast([128, 4, 64]), ALU.mult)
                    if not first:
                        tt('pool', oacc, oacc[:], oacc, oacc[:], otmp, otmp[:], ALU.add)
                    if debug:
                        dma(d_dbg, d_dbg[br, jj * 128:(jj + 1) * 128, g * 256:(g + 1) * 256], oacc, oacc[:].rearrange("p h d -> p (h d)"))

                def back_T(src):
                    cp('act', numTs, numTs[:], src, src[:])
                    for h in range(4):
                        P.op('pe', lambda e, h=h: e.transpose(out=pn[:, h, 0:65], in_=numTs[:, h, :], identity=identf[0:65, 0:65]), [numTs, identf], [pn])

                ncc = min(NCC, cmp_nchunks(jj))
                for c in range(ncc):
                    pi = pair_off[jj] + c
                    dma(bst[k % 2], bst[k % 2][:], c_cmpb, c_cmpb[g, pi])
                    cp('pool', cbt[k % 2], cbt[k % 2][:], bst[k % 2], bst[k % 2][:])
                    sc = psc[ne % 2]; ne += 1
                    sc_all = sc[:].rearrange("p h q -> p (h q)")
                    mm(sc, sc_all, kcmpT[g], kcmpT[g][:, c * 128:(c + 1) * 128], qT, qT_all, start=True, stop=False)
                    mm(sc, sc_all, ident, ident[:], cbt[k % 2], cbt[k % 2][:], start=False, stop=True)
                    k += 1
                    act(Ec[c], Ec[c][:], sc, sc[:], AF.Exp)
                for h in range(4):
                    for c in range(ncc):
                        mm(pn, pn[:, h, 0:65], Ec[c], Ec[c][:, h, :], vcmp[g], vcmp[g][:, c, :], start=(c == 0), stop=(c == ncc - 1))
                    for c in range(ncc):
                        mm(pimp, pimp[:, h, :], Ec[c], Ec[c][:, h, :], wimp, wimp[:, c, :], start=(c == 0), stop=(c == ncc - 1))
                nk = 2 * jj + 2
                k0 = max(0, 2 * jj - 4)
                bufs = {}

                def score(kind, kc, n):
                    sc = psc[ne_[0] % 2]; E = Eb[ne_[0] % 3]; ne_[0] += 1
                    bufs[n] = E
                    sc_all = sc[:].rearrange("p h q -> p (h q)")
                    if kind == 's':
                        m = 2 * jj + 1 - kc
                        mm(sc, sc_all, ksT, ksT[:, kc * 128:(kc + 1) * 128], qT, qT_aug, start=True, stop=False)
                        if m < 14:
                            mm(sc, sc_all, ident, ident[:], selb, selb[:, m, :], start=False, stop=False)
                        mm(sc, sc_all, exm, exm[:, kc * 128:(kc + 1) * 128], maskT4, mT_all, start=False, stop=True)
                    else:
                        m = 2 * jj + 1 - kc
                        mm(sc, sc_all, kwT, kwT[:, kc * 128:(kc + 1) * 128], qT, qT_all, start=True, stop=False)
                        mm(sc, sc_all, ident, ident[:], winb2, winb2[:, m, :], start=False, stop=True)
                    act(E, E[:], sc, sc[:], AF.Exp)

                def pvs(kind, kc, n):
                    E = bufs.pop(n)
                    if kind == 's':
                        mm(pnT, pnT[:].rearrange("p h q -> p (h q)"), vs, vs[:, kc, :], E, E[:].rearrange("p h q -> p (h q)"), start=(kc == 0), stop=(kc == nk - 1))
                    else:
                        mm(pnT2, pnT2[:].rearrange("p h q -> p (h q)"), vw, vw[:, kc, :], E, E[:].rearrange("p h q -> p (h q)"), start=(kc == k0), stop=(kc == nk - 1))

                def run_items(items):
                    NI = len(items)
                    for n in range(NI + 1):
                        if n < NI:
                            score(items[n][0], items[n][1], n)
                        if n >= 1:
                            pvs(items[n - 1][0], items[n - 1][1], n - 1)
                mT_all = maskT4[:].rearrange("p h q -> p (h q)")
                run_items([('w', kc) for kc in range(k0, nk)])
                finish_branch(pn, 0, True)
                for h in range(4):
                    if h == 0:
                        ts('dve', imp, imp[:], pimp, pimp[:, 0, :], rden[:, 0:1], None, ALU.mult, rd=[rden])
                    else:
                        stt('dve', imp, imp[:], pimp, pimp[:, h, :], rden[:, h:h + 1], imp, imp[:], ALU.mult, ALU.add, rd=[rden])
                tt('dve', imp, imp[:], imp, imp[:], m12, m12[:, 0, :], ALU.mult)
                tt('dve', imp, imp[:], imp, imp[:], m12, m12[:, 1, :], ALU.add)
                if debug:
                    dma(d_imp, d_imp[g, jj * 128:(jj + 1) * 128, :], imp, imp[:])
                P.op('dve', lambda e: e.max(out=m8[:, 0:8], in_=imp[:]), [imp], [m8])
                P.op('dve', lambda e: e.match_replace(out=imp2[:], in_to_replace=m8[:, 0:8], in_values=imp[:], imm_value=-1e30), [imp, m8], [imp2])
                P.op('dve', lambda e: e.max(out=m8[:, 8:16], in_=imp2[:]), [imp2], [m8])
                ts('dve', mk, mk[:], imp, imp[:], m8[:, 15:16], 1.0, ALU.is_ge, ALU.subtract, rd=[m8])
                tr(pmt, pmt[:], mk, mk[:], ident)
                P.op('act', lambda e: e.mul(out=maskT4[:], in_=pmt[:].unsqueeze(1).to_broadcast([128, 4, 128]), mul=30000.0), [pmt], [maskT4])
                run_items([('s', kc) for kc in range(nk)])
                back_T(pnT)
                finish_branch(pn, 1, False)
                back_T(pnT2)
                finish_branch(pn, 2, False)
                cp('act', onsa, onsa[:], oacc, oacc[:].rearrange("p h d -> p (h d)"))
                dma(d_omix, d_omix[jj * 128:(jj + 1) * 128, 512 + g * 256:512 + (g + 1) * 256], onsa, onsa[:])
        P.pop()

    if upto >= 3:
        P.push()
        woutb = P.sb([128, 8, D], BF16, "woutb"); wqb = P.sb([128, 8, D], BF16, "wqb"); wob = P.sb([128, 8, D], BF16, "wob")
        pwqb = P.sb([128, 8, D], BF16, "pwqb")
        skb = P.sb([128, 8, 256], BF16, "skb")
        kTm = P.sb([128, 8, 256], BF16, "kTm")
        vm = P.sb([128, 2, 4, 257], BF16, "vm")
        xt = [P.sb([128, D], F32, "x3_%d" % i) for i in range(2)]
        junk = P.sb([128, D], BF16, "junk3"); ss = P.sb([128, 1], F32, "ss3"); rstd = P.sb([128, 1], F32, "rstd3")
        hn = P.sb([128, D], BF16, "hn3"); hnT = P.sb([128, 8, 128], BF16, "hnT3")
        pt = P.ps([128, 8, 128], BF16, "pt3")
        pa = [P.ps([128, 512], F32, "pa%d" % i) for i in range(4)]
        pxa = P.ps([128, 2, 512], F32, "pxa")
        P.push()
        wst = [P.sb([128, D], F32, "wst3_%d" % i) for i in range(2)]
        wtmp = P.sb([128, 8, D], BF16, "wtmp"); skf = P.sb([128, 8, 256], F32, "skf")
        memT = P.sb([128, 8, 256], BF16, "memT")
        k = [0]

        def load_w(dst, src_t, src_ap3):
            for c in range(8):
                a = wst[k[0] % 2]
                dma(a, a[:], src_t, src_ap3[c])
                cp(['dve', 'pool'][k[0] % 2], dst, dst[:, c, :], a, a[:]); k[0] += 1
        load_w(woutb, w_out, w_out[:].rearrange("(c p) n -> c p n", p=128))
        load_w(wqb, xa_w, xa_w[0].rearrange("(c p) n -> c p n", p=128))
        load_w(wob, xa_w, xa_w[3].rearrange("(c p) n -> c p n", p=128))
        load_w(pwqb, pwq, pwq[:].rearrange("(c p) n -> c p n", p=128))
        dma(skf, skf[:], skd, skd[:].rearrange("c p n -> p c n"))
        cp('dve', skb, skb[:], skf, skf[:])
        memset('dve', vm, vm[:, :, :, 256:257], 1.0)
        for mc in range(2):
            x_t = xt[mc % 2]
            dma(x_t, x_t[:], memb, memb[mc * 128:(mc + 1) * 128, :])
            rmsnorm(x_t, x_t[:], 2, hn, hn[:], junk, ss, rstd)
            for c in range(8):
                tr(pt, pt[:, c, :], hn, hn[:, c * 128:(c + 1) * 128], ident)
            cp('act', memT, memT[:, :, mc * 128:(mc + 1) * 128], pt, pt[:])
        load_w(wtmp, xa_w, xa_w[1].rearrange("(c p) n -> c p n", p=128))
        for oc in range(8):
            for c in range(8):
                mm(pa[0], pa[0][:, 0:256], wtmp, wtmp[:, c, oc * 128:(oc + 1) * 128], memT, memT[:, c, :], start=(c == 0), stop=(c == 7))
            cp('act', kTm, kTm[:, oc, :], pa[0], pa[0][:, 0:256])
        load_w(wtmp, xa_w, xa_w[2].rearrange("(c p) n -> c p n", p=128))
        for mc in range(2):
            for half in range(2):
                for c in range(8):
                    mm(pa[half], pa[half][:], memT, memT[:, c, mc * 128:(mc + 1) * 128], wtmp, wtmp[:, c, half * 512:(half + 1) * 512], start=(c == 0), stop=(c == 7))
                cp('act', vm, vm[:, mc, half * 2:half * 2 + 2, 0:256], pa[half], pa[half][:].rearrange("p (h d) -> p h d", d=256))
        P.pop()
        og2 = P.sb([128, 2, 512], BF16, "og2"); omx = P.sb([128, D], BF16, "omx"); otm = P.sb([128, 512], F32, "otm")
        h1 = P.sb([128, D], F32, "h1"); qTx = P.sb([128, 8, 128], BF16, "qTx")
        Ex = P.sb([128, 2, 4, 128], BF16, "Ex"); rdx = P.sb([128, 4], F32, "rdx")
        oxa = P.sb([128, D], BF16, "oxa")
        hn3T = P.sb([128, 8, 128], BF16, "hn3Ts"); qpT = P.sb([128, 8, 128], BF16, "qpT")
        sc_ = [P.sb([128, 16, 128], F32, "scr%d" % i) for i in range(2)]; ab_ = [P.sb([128, 16, 128], F32, "ab%d" % i) for i in range(2)]
        negm = P.sb([128, 16], F32, "negm"); t16 = P.sb([128, 16, 16], F32, "t16"); scr2_ = [P.sb([128, 128], F32, "scr2_%d" % i) for i in range(4)]
        candall = P.sb([128, 8, 256], F32, "candall"); cand2_ = [P.sb([128, 256], F32, "cand2_%d" % i) for i in range(4)]; c16 = P.sb([128, 8, 16], F32, "c16")
        route = P.sb([128, 16], F32, "route"); zs = P.sb([128, 8], F32, "zs")
        memset('dve', route, route[:], 0.0)
        def Xgen(jj):
            sc = sc_[jj % 2]
            x_t = xt[jj % 2]
            dma(x_t, x_t[:], xo, xo[jj * 128:(jj + 1) * 128, :])
            dma(og2, og2[:], d_ogla, d_ogla[jj * 256:(jj + 1) * 256, :].rearrange("(u p) c -> p u c", p=128))
            dma(omx, omx[:, 512:1024], d_omix, d_omix[jj * 128:(jj + 1) * 128, 512:1024])
            ts('dve', otm, otm[:], og2, og2[:, 0, :], pv[:, 0:1], None, ALU.mult, rd=[pv])
            stt('dve', omx, omx[:, 0:512], og2, og2[:, 1, :], pv[:, 1:2], otm, otm[:], ALU.mult, ALU.add, rd=[pv])
            for c in range(8):
                tr(pt, pt[:, c, :], omx, omx[:, c * 128:(c + 1) * 128], ident)
            cp('act', hnT, hnT[:], pt, pt[:])
            for half in range(2):
                for c in range(8):
                    mm(pa[half], pa[half][:], hnT, hnT[:, c, :], woutb, woutb[:, c, half * 512:(half + 1) * 512], start=(c == 0), stop=(c == 7))
                tt('dve', h1, h1[:, half * 512:(half + 1) * 512], pa[half], pa[half][:], x_t, x_t[:, half * 512:(half + 1) * 512], ALU.add)
            yield
            rmsnorm(h1, h1[:], 1, hn, hn[:], junk, ss, rstd)
            for c in range(8):
                tr(pt, pt[:, c, :], hn, hn[:, c * 128:(c + 1) * 128], ident)
            cp('act', hnT, hnT[:], pt, pt[:])
            for oc in range(8):
                pq_ = pa[2 + oc % 2]
                for c in range(8):
                    mm(pq_, pq_[:, 0:128], wqb, wqb[:, c, oc * 128:(oc + 1) * 128], hnT, hnT[:, c, :], start=(c == 0), stop=(c == 7))
                cp(['act', 'dve'][oc % 2], qTx, qTx[:, oc, :], pq_, pq_[:, 0:128])
                yield
            yield
            for mc in range(2):
                for h in range(4):
                    for dc in range(2):
                        mm(pxa, pxa[:, mc, h * 128:(h + 1) * 128], kTm, kTm[:, h * 2 + dc, mc * 128:(mc + 1) * 128], qTx, qTx[:, h * 2 + dc, :], start=(dc == 0), stop=(dc == 1))
            act(Ex, Ex[:].rearrange("p a h q -> p a (h q)"), pxa, pxa[:], AF.Exp, scale=1.0 / 16)
            for h in range(4):
                o_ap = pxa[:, h // 2, (h % 2) * 256:(h % 2) * 256 + 256]
                for mc in range(2):
                    mm(pa[h % 2], pa[h % 2][:, 0:257], Ex, Ex[:, mc, h, :], vm, vm[:, mc, h, :], start=(mc == 0), stop=(mc == 1))
                ts('dve', rdx, rdx[:, h:h + 1], pa[h % 2], pa[h % 2][:, 256:257], 1e-30, None, ALU.max)
                recip(rdx, rdx[:, h:h + 1], rdx, rdx[:, h:h + 1])
                ts('dve', oxa, oxa[:, h * 256:(h + 1) * 256], pa[h % 2], pa[h % 2][:, 0:256], rdx[:, h:h + 1], None, ALU.mult, rd=[rdx])
            yield
            for c in range(8):
                tr(pt, pt[:, c, :], oxa, oxa[:, c * 128:(c + 1) * 128], ident)
            cp('act', hnT, hnT[:], pt, pt[:])
            for half in range(2):
                for c in range(8):
                    mm(pa[half], pa[half][:], hnT, hnT[:, c, :], wob, wob[:, c, half * 512:(half + 1) * 512], start=(c == 0), stop=(c == 7))
                tt('dve', h1, h1[:, half * 512:(half + 1) * 512], pa[half], pa[half][:], h1, h1[:, half * 512:(half + 1) * 512], ALU.add)
            dma(d_h, d_h[jj * 128:(jj + 1) * 128, :], h1, h1[:])
            yield
            rmsnorm(h1, h1[:], 3, hn, hn[:], junk, ss, rstd)
            for c in range(8):
                tr(pt, pt[:, c, :], hn, hn[:, c * 128:(c + 1) * 128], ident)
            cp('act', hn3T, hn3T[:], pt, pt[:])
            dma(d_hn3T, d_hn3T[:, :, jj * 128:(jj + 1) * 128], hn3T, hn3T[:])
            for oc in range(8):
                pq_ = pa[2 + oc % 2]
                for c in range(8):
                    mm(pq_, pq_[:, 0:128], pwqb, pwqb[:, c, oc * 128:(oc + 1) * 128], hn3T, hn3T[:, c, :], start=(c == 0), stop=(c == 7))
                cp(['act', 'dve'][oc % 2], qpT, qpT[:, oc, :], pq_, pq_[:, 0:128])
                yield
            yield
            for oc in range(8):
                pq_ = pa[oc % 2]
                mm(pq_, pq_[:, 0:256], qpT, qpT[:, oc, :], skb, skb[:, oc, :])
                cp(['act', 'dve'][oc % 2], sc, sc[:, 2 * oc:2 * oc + 2, :], pq_, pq_[:, 0:256].rearrange("p (a k) -> p a k", a=2))
            yield

        def Rgen(jj):
            sc = sc_[jj % 2]; ab = ab_[jj % 2]
            P.op('dve', lambda e: e.tensor_reduce(out=negm[:], in_=sc[:], axis=AX.X, op=ALU.max), [sc], [negm])
            ts('dve', negm, negm[:], negm, negm[:], -1.0, None, ALU.mult)
            for r in range(16):
                act(ab, ab[:, r, :], sc, sc[:, r, :], AF.Exp, bias=negm[:, r:r + 1], rd=[negm])
            yield
            for r0 in range(0, 16, 4):
                for r in range(r0, r0 + 4):
                    P.op('dve', lambda e, r=r: e.max(out=t16[:, r, 0:8], in_=ab[:, r, :]), [ab], [t16])
                for r in range(r0, r0 + 4):
                    P.op('dve', lambda e, r=r: e.match_replace(out=scr2_[r % 4][:], in_to_replace=t16[:, r, 0:8], in_values=ab[:, r, :], imm_value=-1.0), [ab, t16], [scr2_[r % 4]])
                for r in range(r0, r0 + 4):
                    P.op('dve', lambda e, r=r: e.max(out=t16[:, r, 8:16], in_=scr2_[r % 4][:]), [scr2_[r % 4]], [t16])
                yield
            t16v = t16[:].rearrange("p (h a) k -> p h a k", a=2)
            abv = ab[:].rearrange("p (h a) k -> p h a k", a=2)

            def cand_top16():
                yield
                tt('dve', candall, candall[:].rearrange("p h (a b) -> p h a b", a=16),
                   t16, t16v[:, :, 0, :].unsqueeze(3).to_broadcast([128, 8, 16, 16]),
                   t16, t16v[:, :, 1, :].unsqueeze(2).to_broadcast([128, 8, 16, 16]), ALU.mult)
                for h0 in range(0, 8, 4):
                    for h in range(h0, h0 + 4):
                        P.op('dve', lambda e, h=h: e.max(out=c16[:, h, 0:8], in_=candall[:, h, :]), [candall], [c16])
                    for h in range(h0, h0 + 4):
                        P.op('dve', lambda e, h=h: e.match_replace(out=cand2_[h % 4][:], in_to_replace=c16[:, h, 0:8], in_values=candall[:, h, :], imm_value=-1.0), [candall, c16], [cand2_[h % 4]])
                    for h in range(h0, h0 + 4):
                        P.op('dve', lambda e, h=h: e.max(out=c16[:, h, 8:16], in_=cand2_[h % 4][:]), [cand2_[h % 4]], [c16])
                    yield
            yield from cand_top16()
            P.op('dve', lambda e: e.tensor_reduce(out=zs[:], in_=c16[:], axis=AX.X, op=ALU.add), [c16], [zs])
            recip(zs, zs[:], zs, zs[:])
            tt('dve', ab, abv[:, :, 1, :], ab, abv[:, :, 1, :], zs, zs[:].unsqueeze(2).to_broadcast([128, 8, 128]), ALU.mult)
            stt('dve', route, route[:, 0:8], c16, c16[:, :, 15], 1.0 - 1e-6, zs, zs[:], ALU.mult, ALU.mult)
            dma(d_route, d_route[jj * 128:(jj + 1) * 128, 0:2048], ab, ab[:].rearrange("p r k -> p (r k)"))
            dma(d_route, d_route[jj * 128:(jj + 1) * 128, 2048:2064], route, route[:])
            yield

        def drain(g):
            for _ in g:
                pass
        drain(Xgen(0))
        for jj in range(NO):
            gr = Rgen(jj)
            gx = Xgen(jj + 1) if jj + 1 < NO else iter(())
            ra = xa_ = True
            while ra or xa_:
                if xa_:
                    xa_ = next(gx, 'END') != 'END'
                if ra:
                    ra = next(gr, 'END') != 'END'
        P.pop()

    if upto >= 4:
        P.push()
        TG = 2
        IC = 16
        NCH = 128 // IC
        ACT_HEADS = (1, 3, 5, 7)
        hT = P.sb([128, 8, TG * 128], BF16, "hT")
        ab = [P.sb([128, 16, 128], F32, "ab4_%d" % u) for u in range(TG)]
        rt = [P.sb([128, 16], F32, "rt%d" % u) for u in range(TG)]
        Wc = [[P.sb([128, IC * 128], BF16, "Wc%d_%d" % (i, u)) for u in range(TG)] for i in range(2)]
        et = [P.sb([128, IC, 128], F32, "et%d" % i) for i in range(4)]
        mt = [P.sb([128, IC, 128], BF16, "mt%d" % i) for i in range(2)]
        dnb = [P.sb([128, 8, 512], BF16, "dnb%d" % i) for i in range(3)]
        upb = [P.sb([128, 4, D], BF16, "upb%d" % i) for i in range(3)]
        Gs = [P.sb([128, 512], BF16, "G%d" % i) for i in range(2)]
        GT = [P.sb([128, 4, 128], BF16, "GT%d" % i) for i in range(2)]
        py = [P.ps([128, 2, 512], F32, "py%d" % u) for u in range(TG)]
        pd = [P.ps([128, 512], F32, "pd%d" % i) for i in range(2)]
        ptg = [P.ps([128, 8, 128], BF16, "ptg%d" % i) for i in range(2)]
        h2 = P.sb([128, D], F32, "h2"); yo = P.sb([128, D], F32, "yo")
        junk = P.sb([128, D], BF16, "junk4"); ss = P.sb([128, 1], F32, "ss4"); rstd = P.sb([128, 1], F32, "rstd4")
        qn = [0]
        for tg in range(NO // TG):
            dma(hT, hT[:], d_hn3T, d_hn3T[:, :, tg * TG * 128:(tg + 1) * TG * 128])
            for u in range(TG):
                j = tg * TG + u
                dma(ab[u], ab[u][:].rearrange("p r k -> p (r k)"), d_route, d_route[j * 128:(j + 1) * 128, 0:2048])
                dma(rt[u], rt[u][:], d_route, d_route[j * 128:(j + 1) * 128, 2048:2064])

            def wgen(c):
                its = [(u, h) for u in range(TG) for h in range(8)]
                K = len(its)
                eb_ = {}; mb_ = {}

                def E_(n):
                    u, h = its[n]
                    e_ = et[qn[0] % 4]; qn[0] += 1
                    eb_[n] = e_
                    if h in ACT_HEADS:
                        for i_ in range(IC):
                            P.op('act', lambda e, e_=e_, i_=i_, u=u, h=h: e.activation(out=e_[:, i_, :], in_=ab[u][:, 2 * h + 1, :], func=AF.Copy,
                                                                                  scale=ab[u][:, 2 * h, c * IC + i_:c * IC + i_ + 1]),
                                 [ab[u]], [e_] if i_ in (0, IC - 1) else [])
                    else:
                        tt('dve', e_, e_[:], ab[u], ab[u][:, 2 * h, c * IC:(c + 1) * IC].unsqueeze(2).to_broadcast([128, IC, 128]),
                           ab[u], ab[u][:, 2 * h + 1, :].unsqueeze(1).to_broadcast([128, IC, 128]), ALU.mult)

                def S_(n):
                    u, h = its[n]
                    e_ = eb_.pop(n)
                    w_ap = Wc[c % 2][u][:].rearrange("p (a b) -> p a b", a=IC)
                    if h == 0:
                        stt('dve', Wc[c % 2][u], w_ap, e_, e_[:], rt[u][:, h:h + 1], e_, e_[:], ALU.is_ge, ALU.mult, rd=[rt[u]])
                    else:
                        m_ = mt[n % 2]
                        mb_[n] = m_
                        stt('dve', m_, m_[:], e_, e_[:], rt[u][:, h:h + 1], e_, e_[:], ALU.is_ge, ALU.mult, rd=[rt[u]])

                def A_(n):
                    u, h = its[n]
                    if h == 0:
                        return
                    m_ = mb_.pop(n)
                    w_ap = Wc[c % 2][u][:].rearrange("p (a b) -> p a b", a=IC)
                    tt('dve', Wc[c % 2][u], w_ap, Wc[c % 2][u], w_ap, m_, m_[:], ALU.add)
                for n in range(K + 2):
                    if n < K:
                        E_(n)
                    if 1 <= n <= K:
                        S_(n - 1)
                    if n >= 2:
                        A_(n - 2)
                    yield

            def load_w(ecx):
                dn, ub = dnb[ecx % 3], upb[ecx % 3]
                dma(dn, dn[:], d_downT, d_downT[:, ecx * 512:(ecx + 1) * 512].rearrange("(c p) e -> p c e", p=128))
                dma(ub, ub[:], d_up, d_up[ecx * 512:(ecx + 1) * 512, :].rearrange("(s p) d -> p s d", p=128))

            items = [(ecx, u) for ecx in range(32) for u in range(TG)]
            N = len(items)

            def stA(n):
                ecx, u = items[n]
                if u == 0:
                    if ecx == 0:
                        load_w(0)
                    if ecx + 1 < 32:
                        load_w(ecx + 1)
                    if ecx % 4 == 0:
                        for _ in wg[0]:
                            pass
                        wg[0] = wgen(ecx // 4 + 1) if ecx // 4 + 1 < NCH else iter(())
                for _ in range(3):
                    next(wg[0], None)
                dn = dnb[ecx % 3]
                pdt = pd[n % 2]; G = Gs[n % 2]
                for c in range(8):
                    mm(pdt, pdt[:], hT, hT[:, c, u * 128:(u + 1) * 128], dn, dn[:, c, :], start=(c == 0), stop=(c == 7))
                act(G, G[:], pdt, pdt[:], AF.Gelu_apprx_tanh)
                wch = Wc[(ecx // 4) % 2][u]
                tt('dve', G, G[:], G, G[:], wch, wch[:, (ecx % 4) * 512:(ecx % 4 + 1) * 512], ALU.mult)

            def stB(n):
                G = Gs[n % 2]; gt_ = GT[n % 2]; pt_ = ptg[n % 2]
                for s_ in range(4):
                    tr(pt_, pt_[:, s_, :], G, G[:, s_ * 128:(s_ + 1) * 128], ident)
                cp('act', gt_, gt_[:], pt_, pt_[:, 0:4, :])

            def stC(n):
                ecx, u = items[n]
                gt_ = GT[n % 2]; ub = upb[ecx % 3]
                for half in range(2):
                    for s_ in range(4):
                        mm(py[u], py[u][:, half, :], gt_, gt_[:, s_, :], ub, ub[:, s_, half * 512:(half + 1) * 512],
                           start=(ecx == 0 and s_ == 0), stop=(ecx == 31 and s_ == 3))

            wg = [iter(())]
            for _ in wgen(0):
                pass
            for n in range(N + 2):
                if n < N:
                    stA(n)
                if 1 <= n <= N:
                    stB(n - 1)
                if n >= 2:
                    stC(n - 2)
            for _ in wg[0]:
                pass
            for u in range(TG):
                j = tg * TG + u
                dma(h2, h2[:], d_h, d_h[j * 128:(j + 1) * 128, :])
                tt('dve', h2, h2[:], h2, h2[:], py[u], py[u][:].rearrange("p a b -> p (a b)"), ALU.add)
                rmsnorm(h2, h2[:], 4, yo, yo[:], junk, ss, rstd)
                dma(out, out[j * 128:(j + 1) * 128, :], yo, yo[:])
        P.pop()

    P.finish()
    return nc, dict(npair=npair, pair_off=pair_off, NCC=NCC, NCMP=NCMP, ninst=P.ninst)


def _t5_bucket_np(dist):
    n = np.maximum(dist, 0)
    nf = np.maximum(n, 1).astype(np.float32)
    log_ratio = (np.log(nf / np.float32(16)) / np.float32(math.log(2048 / 16))).astype(np.float32)
    large = 16 + (log_ratio * np.float32(16)).astype(np.int32)
    large = np.minimum(large, 31)
    return np.where(n < 16, n, large).astype(np.int64)


def _bias_tile(rel_bias, g, dist, valid):
    bk = _t5_bucket_np(dist)
    outt = np.empty((128, 4, 128), np.float32)
    for h in range(4):
        outt[:, h, :] = np.where(valid, rel_bias[bk, g * 4 + h], np.float32(NEG))
    return outt.reshape(128, 512)


def make_core_inputs(inputs, T, b, p, meta):
    NT = T // 128; NO = NT // 2; NS = T // 64; NCMP = meta['NCMP']; NCC = meta['NCC']
    f = lambda a: np.ascontiguousarray(np.asarray(a, dtype=np.float32))
    x = f(inputs['x'][b]); rel_bias = f(inputs['rel_bias'])
    m = {}
    m['xb'] = x
    m['xo'] = np.ascontiguousarray(x.reshape(NO, 2, 128, D)[:, p].reshape(NO * 128, D))
    m['mem'] = f(inputs['mem'][b])
    w_in = f(inputs['w_in'][0])
    m['w_in'] = np.ascontiguousarray(np.concatenate([w_in[:, ORIG[k][0]:ORIG[k][1]] for k in PERM_ORDER], axis=1))
    m['gw2'] = f(inputs['gla_gate_w2'][0]); m['gb'] = f(inputs['gla_gate_b'][0]).reshape(1, 256)
    m['gnorm'] = f(inputs['gla_out_norm'][0]).reshape(1, 128)
    m['norms'] = np.stack([f(inputs['norm_mix'][0]), f(inputs['norm_xattn'][0]), f(inputs['norm_mem'][0]),
                           f(inputs['norm_ffn'][0]), f(inputs['norm_final'])], 0)
    cw1 = np.stack([f(inputs['cmp_k_w1'][0]), f(inputs['cmp_v_w1'][0])], 0)
    m['cw1'] = np.ascontiguousarray(cw1.reshape(2, 32, 64, 128).transpose(0, 2, 1, 3))
    cpos = np.stack([f(inputs['cmp_pos_k'][0]), f(inputs['cmp_pos_v'][0])], 0)
    m['cpos'] = np.ascontiguousarray(cpos.transpose(0, 2, 1))
    m['cw2'] = np.stack([f(inputs['cmp_k_w2'][0]), f(inputs['cmp_v_w2'][0])], 0)
    m['w_out'] = f(inputs['w_out'][0])
    m['xa_w'] = np.stack([f(inputs['xa_wq'][0]), f(inputs['xa_wk'][0]), f(inputs['xa_wv'][0]), f(inputs['xa_wo'][0])], 0)
    m['pwq'] = f(inputs['peer_wq'][0])
    sk = f(inputs['peer_subkeys'][0])
    skd = np.zeros((8, 128, 256), np.float32)
    for h in range(8):
        for pp in range(2):
            skd[h, pp * 64:(pp + 1) * 64, pp * 128:(pp + 1) * 128] = sk[h, pp].T
    m['skd'] = skd
    m['downT'] = np.ascontiguousarray(f(inputs['peer_down'][0]).T)
    m['up'] = f(inputs['peer_up'][0])
    m['c_ident'] = np.eye(128, dtype=np.float32)
    s_ = np.arange(128)[:, None]; t_ = np.arange(128)[None, :]
    m['c_ucs'] = np.where(s_ <= t_, -1.0 / 16, 0.0).astype(np.float32)
    m['c_urev'] = np.where(s_ > t_, -1.0 / 16, 0.0).astype(np.float32)
    m['c_causal'] = (s_ <= t_).astype(np.float32)
    m['c_selu'] = np.stack([np.eye(128) * (1 - p), np.eye(128) * p], 0).astype(np.float32)
    m['c_pv'] = np.tile(np.array([[1.0 - p, float(p)]], np.float32), (128, 1))
    kk = np.arange(128)[:, None]; qq = np.arange(128)[None, :]
    selb = np.empty((2, 128, 15, 512), np.float32); winb = np.empty((2, 128, 6, 512), np.float32)
    for g in range(2):
        for mm_ in range(15):
            j = mm_ - 1 + p
            dist = 128 * j + qq - kk
            selb[g, :, mm_, :] = _bias_tile(rel_bias, g, dist, (dist >= 0) & (j >= 0))
        for mm_ in range(6):
            j = mm_ - 1 + p
            dist = 128 * j + qq - kk
            winb[g, :, mm_, :] = _bias_tile(rel_bias, g, dist, (dist >= 0) & (dist < 512) & (j >= 0))
    m['c_selb'] = selb; m['c_winb'] = winb
    cmpb = np.empty((2, meta['npair'], 128, 512), np.float32)
    for jj in range(NO):
        for c in range(min(NCC, cmp_nchunks(jj))):
            n = 128 * c + kk
            t = (2 * jj + p) * 128 + qq
            dist = t - (16 * n + 31)
            for g in range(2):
                cmpb[g, meta['pair_off'][jj] + c] = _bias_tile(rel_bias, g, dist, (dist >= 0) & (n < NCMP))
    m['c_cmpb'] = cmpb
    far = np.empty((2, 128, 4, 128), np.float32)
    for g in range(2):
        for h in range(4):
            far[g, :, h, :] = rel_bias[31, g * 4 + h]
    m['c_far'] = far.reshape(2, 128, 512)
    wimp = np.zeros((NCC * 128, 128), np.float32)
    for s in range(NS):
        for r in range(-1, 4):
            n = 4 * s + r
            lo = 16 * r
            ov = min(lo + 32, 64) - max(lo, 0)
            if 0 <= n < NCMP:
                wimp[n, s] += ov / 32.0
    m['c_wimp'] = wimp.reshape(NCC, 128, 128)
    m12 = np.zeros((NO, 128, 2, 128), np.float32)
    sid = np.arange(128)[None, :]
    for jj in range(NO):
        t = (2 * jj + p) * 128 + np.arange(128)[:, None]
        cur = t // 64
        visible = (sid * 64 <= t) & (sid < NS)
        f0 = (sid == 0); f1 = (sid == cur); f2 = (sid == cur - 1)
        forced = f0 | f1 | f2
        m12[jj, :, 0, :] = (visible & ~forced)
        add = np.where(~visible, -100.0 - sid, 0.0)
        add = np.where(f2, 100.0, add); add = np.where(f1, 101.0, add); add = np.where(f0 & (sid < NS), 102.0, add)
        m12[jj, :, 1, :] = add
    m['c_m12'] = m12
    ex = np.zeros((128, T), np.float32)
    ex[np.arange(T) // 64, np.arange(T)] = 1.0
    m['c_ex'] = ex.astype(NPBF)
    return m


_CACHE = {}


def kernel(**inputs):
    T = inputs['x'].shape[1]
    B = inputs['x'].shape[0]
    if T not in _CACHE:
        _CACHE[T] = build(T)
    nc, meta = _CACHE[T]
    in_maps = []
    for c in range(2 * B):
        in_maps.append(make_core_inputs(inputs, T, c // 2, c % 2, meta))
    res = run_bass_kernel_spmd(nc, in_maps, core_ids=list(range(2 * B)))
    NO = T // 256
    outp = np.empty((B, T // 128, 128, D), np.float32)
    for c in range(2 * B):
        o = np.asarray(res.results[c]["out"], dtype=np.float32).reshape(NO, 128, D)
        outp[c // 2, (c % 2)::2] = o
    return outp.reshape(B, T, D)
```

```python
import math
import numpy as np
import ml_dtypes
import concourse.bass as bass
import concourse.mybir as mybir
from concourse.bass_utils import run_bass_kernel_spmd
from contextlib import ExitStack

F32 = mybir.dt.float32
BF16 = mybir.dt.bfloat16
AF = mybir.ActivationFunctionType
ALU = mybir.AluOpType
AX = mybir.AxisListType
NPBF = ml_dtypes.bfloat16

D = 1024
NEG = -30000.0


class T:
    def __init__(self, h, name):
        self.h = h
        self.name = name
        self.w = None
        self.r = []
        self.psum = False

    def __getitem__(self, k):
        return self.h[k]


class Prog:
    def __init__(self, nc, n_dma_sems=48):
        self.nc = nc
        self.es = ExitStack()
        self.scopes = []
        self.eng = {'pe': nc.tensor, 'dve': nc.vector, 'act': nc.scalar,
                    'pool': nc.gpsimd, 'sp': nc.sync}
        self.sems = {}
        for k in self.eng:
            self.sems['e_' + k] = self.es.enter_context(nc.semaphore('e_' + k))
        self.cnt = {k: 0 for k in self.sems}
        self.ndma = n_dma_sems
        for i in range(n_dma_sems):
            key = 'd_%d' % i
            self.sems[key] = self.es.enter_context(nc.semaphore(key))
            self.cnt[key] = 0
        self.dma_rr = 0
        self.known = {k: {} for k in self.eng}
        self.ntile = 0
        self.ninst = 0

    def push(self):
        self.scopes.append(ExitStack())

    def pop(self):
        self.barrier()
        self.scopes.pop().close()

    def _stack(self):
        return self.scopes[-1] if self.scopes else self.es

    def sb(self, shape, dt, name=None):
        self.ntile += 1
        name = (name or 't') + '_%d' % self.ntile
        h = self._stack().enter_context(self.nc.sbuf_tensor(name, list(shape), dt))
        return T(h, name)

    def ps(self, shape, dt, name=None):
        self.ntile += 1
        name = (name or 'p') + '_%d' % self.ntile
        h = self._stack().enter_context(self.nc.psum_tensor(name, list(shape), dt))
        t = T(h, name)
        t.psum = True
        return t

    def dram(self, name, shape, dt, kind="Internal"):
        h = self.nc.dram_tensor(name, list(shape), dt, kind=kind).ap()
        return T(h, name)

    def _deps(self, reads, writes, e=None):
        deps = []
        for t in reads:
            if t.w is not None:
                deps.append(t.w)
            if t.psum:
                deps.extend([tok for tok in t.r if tok[0] != 'e_' + str(e)])
        for t in writes:
            if t.w is not None:
                deps.append(t.w)
            deps.extend(t.r)
        return deps

    def _waits(self, e, deps):
        kn = self.known[e]
        need = {}
        for (k, v) in deps:
            if kn.get(k, 0) >= v:
                continue
            if need.get(k, 0) < v:
                need[k] = v
        for k, v in need.items():
            kn[k] = v
        return list(need.items())

    def _emit(self, e, waits, fn, inc):
        eng = self.eng[e]
        for (k, v) in waits:
            eng.wait_ge(self.sems[k], v)
        ins = fn(eng)
        ins.then_inc(self.sems[inc[0]], inc[1])
        self.ninst += 1

    def op(self, e, fn, reads=(), writes=()):
        deps = self._deps(reads, writes, e)
        key = 'e_' + e
        if e == 'pe':
            deps = [d for d in deps if d[0] != key]
        waits = self._waits(e, deps)
        self.cnt[key] += 1
        tok = (key, self.cnt[key])
        self._emit(e, waits, fn, (key, 1))
        for t in reads:
            t.r.append(tok)
        for t in writes:
            t.w = tok
            t.r = []
        return tok

    def dma(self, e, out_t, out_ap, in_t, in_ap, **kw):
        reads = [in_t]
        writes = [out_t]
        deps = self._deps(reads, writes)
        key = 'd_%d' % self.dma_rr
        self.dma_rr = (self.dma_rr + 1) % self.ndma
        if self.cnt[key] > 0:
            deps.append((key, self.cnt[key]))
        waits = self._waits(e, deps)
        self.cnt[key] += 16
        tok = (key, self.cnt[key])
        self._emit(e, waits, lambda eng: eng.dma_start(out=out_ap, in_=in_ap, **kw), (key, 16))
        in_t.r.append(tok)
        out_t.w = tok
        out_t.r = []
        return tok

    def barrier(self):
        allt = [(k, v) for k, v in self.cnt.items() if v > 0]
        for e in self.eng:
            for (k, v) in self._waits(e, allt):
                self.eng[e].wait_ge(self.sems[k], v)

    def finish(self):
        self.barrier()
        while self.scopes:
            self.scopes.pop().close()
        self.es.close()


ORIG = dict(gq=(0, 256), gk=(256, 512), gv=(512, 1024), gr=(1024, 1536), glr=(1536, 1552),
            nq=(1552, 2064), kc=(2064, 2192), vc=(2192, 2320), ks=(2320, 2448), vs=(2448, 2576),
            kw=(2576, 2704), vw=(2704, 2832), ng=(2832, 2856))
PERM_ORDER = ['gq', 'gk', 'gv', 'gr', 'nq', 'kc', 'vc', 'ks', 'vs', 'kw', 'vw', 'ng', 'glr']
NCOL = 2856


def cmp_nchunks(jj):
    return min(4, (16 * jj + 15 + 127) // 128)


def build(T, debug=False, upto=9, cut=99):
    NT = T // 128
    NO = NT // 2
    NS = T // 64
    TO = T // 2
    NCMP = (T - 32) // 16 + 1
    NCC = (NCMP + 127) // 128
    pair_off = []
    npair = 0
    for jj in range(NO):
        pair_off.append(npair)
        npair += min(NCC, cmp_nchunks(jj))

    nc = bass.Bass("TRN2", target_bir_lowering=False)
    P = Prog(nc)
    SK = "ExternalOutput" if debug else "Internal"

    def inp(name, shape, dt=F32):
        return P.dram(name, shape, dt, kind="ExternalInput")

    xb = inp("xb", [T, D]); xo = inp("xo", [TO, D]); memb = inp("mem", [256, D])
    w_in = inp("w_in", [D, NCOL]); gw2 = inp("gw2", [16, 256]); gb = inp("gb", [1, 256])
    gnorm = inp("gnorm", [1, 128])
    norms = inp("norms", [5, D])
    cw1 = inp("cw1", [2, 64, 32, 128]); cpos = inp("cpos", [2, 64, 32]); cw2 = inp("cw2", [2, 128, 64])
    w_out = inp("w_out", [D, D]); xa_w = inp("xa_w", [4, D, D])
    pwq = inp("pwq", [D, D]); skd = inp("skd", [8, 128, 256])
    downT = inp("downT", [D, 16384]); up = inp("up", [16384, D])
    c_ident = inp("c_ident", [128, 128]); c_ucs = inp("c_ucs", [128, 128]); c_urev = inp("c_urev", [128, 128])
    c_causal = inp("c_causal", [128, 128]); c_selu = inp("c_selu", [2, 128, 128]); c_pv = inp("c_pv", [128, 2])
    c_selb = inp("c_selb", [2, 128, 15, 512]); c_winb = inp("c_winb", [2, 128, 6, 512])
    c_cmpb = inp("c_cmpb", [2, npair, 128, 512])
    c_far = inp("c_far", [2, 128, 512])
    c_wimp = inp("c_wimp", [NCC, 128, 128]); c_m12 = inp("c_m12", [NO, 128, 2, 128])
    c_ex = inp("c_ex", [128, T], BF16)
    out = P.dram("out", [TO, D], F32, kind="ExternalOutput")

    d_nq = P.dram("d_nq", [T, 512], BF16, kind=SK)
    d_ng = P.dram("d_ng", [T, 24], F32, kind=SK)
    d_ogla = P.dram("d_ogla", [T, 512], BF16, kind=SK)
    d_kT = P.dram("d_kT", [4, 128, T], BF16, kind=SK)
    d_vs = P.dram("d_vs", [T, 128], BF16, kind=SK)
    d_vw = P.dram("d_vw", [T, 128], BF16, kind=SK)
    d_omix = P.dram("d_omix", [TO, D], BF16, kind=SK)
    d_h = P.dram("d_h", [TO, D], F32, kind=SK)
    d_hn3T = P.dram("d_hn3T", [128, 8, TO], BF16, kind=SK)
    d_route = P.dram("d_route", [TO, 2064], F32, kind=SK)
    d_downT = P.dram("d_downT", [D, 16384], BF16, kind="Internal")
    d_up = P.dram("d_up", [16384, D], BF16, kind="Internal")
    d_cmp = P.dram("d_cmp", [2, 64, 512], BF16, kind=SK)
    d_dbg = P.dram("d_dbg", [3, TO, 512], F32, kind=SK)
    d_imp = P.dram("d_imp", [2, TO, 128], F32, kind=SK)

    def mm(ot, o_ap, lt, l_ap, rt, r_ap, start=True, stop=True):
        P.op('pe', lambda e: e.matmul(o_ap, lhsT=l_ap, rhs=r_ap, start=start, stop=stop), [lt, rt], [ot])

    def tr(ot, o_ap, it, i_ap, idt):
        P.op('pe', lambda e: e.transpose(out=o_ap, in_=i_ap, identity=idt[:]), [it, idt], [ot])

    def act(ot, o_ap, it, i_ap, func, bias=None, scale=None, accum=None, rd=(), wr=()):
        kw = {}
        if bias is not None:
            kw['bias'] = bias
        if scale is not None:
            kw['scale'] = scale
        if accum is not None:
            kw['accum_out'] = accum
        P.op('act', lambda e: e.activation(out=o_ap, in_=i_ap, func=func, **kw), [it] + list(rd), [ot] + list(wr))

    def cp(eng, ot, o_ap, it, i_ap):
        if eng == 'act':
            P.op('act', lambda e: e.copy(out=o_ap, in_=i_ap), [it], [ot])
        else:
            P.op(eng, lambda e: e.tensor_copy(out=o_ap, in_=i_ap), [it], [ot])

    def tt(eng, ot, o_ap, at, a_ap, bt, b_ap, op):
        P.op(eng, lambda e: e.tensor_tensor(out=o_ap, in0=a_ap, in1=b_ap, op=op), [at, bt], [ot])

    def ts(eng, ot, o_ap, at, a_ap, s1, s2, op0, op1=None, rd=()):
        if op1 is None:
            P.op(eng, lambda e: e.tensor_scalar(out=o_ap, in0=a_ap, scalar1=s1, scalar2=None, op0=op0), [at] + list(rd), [ot])
        else:
            P.op(eng, lambda e: e.tensor_scalar(out=o_ap, in0=a_ap, scalar1=s1, scalar2=s2, op0=op0, op1=op1), [at] + list(rd), [ot])

    def stt(eng, ot, o_ap, at, a_ap, sc, bt, b_ap, op0, op1, rd=()):
        P.op(eng, lambda e: e.scalar_tensor_tensor(out=o_ap, in0=a_ap, scalar=sc, in1=b_ap, op0=op0, op1=op1),
             [at, bt] + list(rd), [ot])

    def memset(eng, t, ap, v):
        P.op(eng, lambda e: e.memset(ap, v), [], [t])

    def recip(ot, o_ap, it, i_ap):
        P.op('dve', lambda e: e.reciprocal(out=o_ap, in_=i_ap), [it], [ot])

    dmaq = ['sp', 'act', 'pool']
    dq = [0]

    def dma(ot, o_ap, it, i_ap, q=None, **kw):
        if q is None:
            q = dmaq[dq[0] % 2]
            dq[0] += 1
        return P.dma(q, ot, o_ap, it, i_ap, **kw)

    ident = P.sb([128, 128], BF16, "ident"); identf = P.sb([128, 128], F32, "identf")
    ones = P.sb([128, 128], BF16, "ones")
    pv = P.sb([128, 2], F32, "pv")
    nrm = P.sb([128, 5, D], F32, "nrm")
    dma(identf, identf[:], c_ident, c_ident[:])
    dma(pv, pv[:], c_pv, c_pv[:])
    for k in range(5):
        dma(nrm, nrm[:, k, :], norms, norms[k:k + 1, :].partition_broadcast(128))
    cp('dve', ident, ident[:], identf, identf[:])
    memset('pool', ones, ones[:], 1.0)
    eps_t = P.sb([128, 1], F32, "eps_t")
    memset('dve', eps_t, eps_t[:], 1e-6)
    kcmpT = [P.sb([128, 512], BF16, "kcmpT%d" % g) for g in range(2)]
    vcmp = [P.sb([128, 4, 65], BF16, "vcmp%d" % g) for g in range(2)]

    def rmsnorm(xt, x_ap, gain_k, hn, hn_ap, junk, ss, rstd, n=D, eps=1e-6):
        memset('dve', ss, ss[:], 0.0)
        act(junk, junk[:, 0:n], xt, x_ap, AF.Square, accum=ss[:], rd=[ss], wr=[ss])
        act(rstd, rstd[:], ss, ss[:], AF.Ln, scale=1.0 / n, bias=eps_t[:, 0:1], rd=[eps_t])
        act(rstd, rstd[:], rstd, rstd[:], AF.Exp, scale=-0.5)
        stt('dve', hn, hn_ap, xt, x_ap, rstd[:, 0:1], nrm, nrm[:, gain_k, 0:n], ALU.mult, ALU.mult, rd=[rstd])

    if upto >= 1:
        P.push()
        winb = P.sb([128, 8, NCOL], BF16, "winb")
        wst = [P.sb([128, NCOL], F32, "wst%d" % i) for i in range(2)]
        w_in_v = w_in[:].rearrange("(c p) n -> c p n", p=128)
        for c in range(8):
            dma(wst[c % 2], wst[c % 2][:], w_in, w_in_v[c])
            cp(['dve', 'pool'][c % 2], winb, winb[:, c, :], wst[c % 2], wst[c % 2][:])
        gw2t = P.sb([16, 256], F32, "gw2t"); gbt = P.sb([1, 256], F32, "gbt"); onesrow = P.sb([1, 128], F32, "onesrow")
        gnt = P.sb([128, 128], F32, "gnt")
        ucs = P.sb([128, 128], F32, "ucs"); urev = P.sb([128, 128], F32, "urev"); causal = P.sb([128, 128], F32, "causal")
        m16 = P.sb([128, 1], F32, "m16")
        dma(gw2t, gw2t[:], gw2, gw2[:]); dma(gbt, gbt[:], gb, gb[:])
        dma(gnt, gnt[:], gnorm, gnorm[0:1, :].partition_broadcast(128))
        dma(ucs, ucs[:], c_ucs, c_ucs[:]); dma(urev, urev[:], c_urev, c_urev[:]); dma(causal, causal[:], c_causal, c_causal[:])
        memset('dve', onesrow, onesrow[:], 1.0)
        memset('dve', m16, m16[:], -1.0 / 16)
        S = P.sb([128, 2, 128], F32, "S"); Sb = P.sb([128, 2, 128], BF16, "Sb")
        memset('dve', S, S[:], 0.0); memset('pool', Sb, Sb[:], 0.0)

        xt = [P.sb([128, D], F32, "xt%d" % i) for i in range(2)]
        junk = P.sb([128, D], BF16, "junk"); ss = P.sb([128, 1], F32, "ss"); rstd = P.sb([128, 1], F32, "rstd")
        hn = P.sb([128, D], BF16, "hn"); hnT = P.sb([128, 8, 128], BF16, "hnT")
        pt = P.ps([128, 8, 128], BF16, "pt")
        pz = [P.ps([128, 512], F32, "pz%d" % i) for i in range(3)]
        pg1 = P.ps([128, 512], F32, "pg1"); pg2 = P.ps([128, 512], F32, "pg2")
        po = P.ps([128, 512], F32, "po"); ptb = P.ps([128, 8, 128], BF16, "ptb")
        glr = P.sb([128, 16], F32, "glr"); glrT = P.sb([16, 128], F32, "glrT")
        e1 = P.sb([128, 256], F32, "e1"); L = P.sb([128, 256], F32, "L")
        eb = P.sb([128, 256], F32, "eb"); enb = P.sb([128, 256], F32, "enb"); ec = P.sb([128, 256], F32, "ec")
        ebl = P.sb([128, 2], F32, "ebl")
        qk = P.sb([128, 3, 256], BF16, "qk")
        qkT = P.sb([128, 4, 128], BF16, "qkT")
        vv = P.sb([128, 512], BF16, "vv"); sg = P.sb([128, 512], F32, "sg")
        AT = P.sb([128, 128], BF16, "AT")
        ssq = P.sb([128, 4], F32, "ssq"); rs4 = P.sb([128, 4], F32, "rs4"); tmpn = P.sb([128, 128], F32, "tmpn")
        junk2 = P.sb([128, 128], F32, "junk2")
        og = P.sb([128, 512], BF16, "og"); otmp4 = P.sb([128, 512], F32, "otmp4")
        nqs = P.sb([128, 512], BF16, "nqs"); ngs = P.sb([128, 24], F32, "ngs")
        kk = P.sb([128, 4, 128], BF16, "kk"); kkT = P.sb([128, 4, 128], BF16, "kkT")
        vsw = P.sb([128, 2, 128], BF16, "vsw")
        cast_jobs = []
        if upto >= 4:
            stg = [P.sb([128, 4096], F32, "stg%d" % i) for i in range(2)]
            stb = [P.sb([128, 4096], BF16, "stb%d" % i) for i in range(2)]
            for r in range(8):
                for c in range(4):
                    cast_jobs.append((downT, downT[r * 128:(r + 1) * 128, c * 4096:(c + 1) * 4096],
                                      d_downT, d_downT[r * 128:(r + 1) * 128, c * 4096:(c + 1) * 4096]))
            sv = up[:].rearrange("(a p f) d -> a p (f d)", p=128, f=4)
            dv = d_up[:].rearrange("(a p f) d -> a p (f d)", p=128, f=4)
            for a in range(32):
                cast_jobs.append((up, sv[a], d_up, dv[a]))
        cj = [0]

        def cast_job():
            if cj[0] >= len(cast_jobs):
                return
            src_t, s_ap, dst_t, d_ap = cast_jobs[cj[0]]
            a, b_ = stg[cj[0] % 2], stb[cj[0] % 2]
            P.dma('pool', a, a[:], src_t, s_ap)
            for q_ in range(4):
                cp('act', b_, b_[:, q_ * 1024:(q_ + 1) * 1024], a, a[:, q_ * 1024:(q_ + 1) * 1024])
            P.dma('pool', dst_t, d_ap, b_, b_[:])
            cj[0] += 1
        cA, cB, cC, cD, cE, cF = 0, 512, 1024, 1536, 2048, 2560
        for i in range(NT):
            x_t = xt[i % 2]
            dma(x_t, x_t[:], xb, xb[i * 128:(i + 1) * 128, :])
            rmsnorm(x_t, x_t[:], 0, hn, hn[:], junk, ss, rstd)
            for c in range(8):
                tr(pt, pt[:, c, :], hn, hn[:, c * 128:(c + 1) * 128], ident)
            cp('act', hnT, hnT[:], pt, pt[:])

            def zgroup(pz_t, c0, n):
                for c in range(8):
                    mm(pz_t, pz_t[:, 0:n], hnT, hnT[:, c, :], winb, winb[:, c, c0:c0 + n], start=(c == 0), stop=(c == 7))
            zgroup(pz[0], cF, 296)
            cp('act', kk, kk[:, 3, :], pz[0], pz[0][:, 0:128])
            cp('act', vsw, vsw[:, 1, :], pz[0], pz[0][:, 128:256])
            cp('act', glr, glr[:], pz[0], pz[0][:, 280:296])
            act(ngs, ngs[:], pz[0], pz[0][:, 256:280], AF.Exp, scale=-1.0)
            ts('dve', ngs, ngs[:], ngs, ngs[:], 1.0, None, ALU.add)
            recip(ngs, ngs[:], ngs, ngs[:])
            dma(d_ng, d_ng[i * 128:(i + 1) * 128, :], ngs, ngs[:])
            zgroup(pz[1], cE, 512)
            cp('act', kk, kk[:, 0:3, :], pz[1], pz[1][:, 0:384].rearrange("p (a b) -> p a b", a=3))
            cp('dve', vsw, vsw[:, 0, :], pz[1], pz[1][:, 384:512])
            dma(d_vs, d_vs[i * 128:(i + 1) * 128, :], vsw, vsw[:, 0, :])
            dma(d_vw, d_vw[i * 128:(i + 1) * 128, :], vsw, vsw[:, 1, :])
            for a in range(4):
                tr(ptb, ptb[:, a, :], kk, kk[:, a, :], ident)
            cp('act', kkT, kkT[:], ptb, ptb[:, 0:4, :])
            dma(d_kT, d_kT[:, :, i * 128:(i + 1) * 128].rearrange("a p t -> p a t"), kkT, kkT[:])
            zgroup(pz[2], cD, 512)
            P.op('act', lambda e, pzt=pz[2]: e.mul(out=nqs[:], in_=pzt[:], mul=0.125), [pz[2]], [nqs])
            dma(d_nq, d_nq[i * 128:(i + 1) * 128, :], nqs, nqs[:])
            tr(pg1, pg1[0:16, 0:128], glr, glr[:], identf)
            cp('dve', glrT, glrT[:], pg1, pg1[0:16, 0:128])
            mm(pg1, pg1[:, 256:512], glrT, glrT[:], gw2t, gw2t[:], start=True, stop=False)
            mm(pg1, pg1[:, 256:512], onesrow, onesrow[:], gbt, gbt[:], start=False, stop=True)
            zgroup(pz[0], cA, 512)
            zgroup(pz[1], cB, 512)
            zgroup(pz[2], cC, 512)
            act(e1, e1[:], pg1, pg1[:, 256:512], AF.Exp, scale=-1.0)
            act(L, L[:], e1, e1[:], AF.Ln, bias=1.0)
            cp('act', vv, vv[:], pz[1], pz[1][:])
            act(sg, sg[:], pz[2], pz[2][:], AF.Exp, scale=-1.0)
            ts('dve', sg, sg[:], sg, sg[:], 1.0, None, ALU.add)
            recip(sg, sg[:], sg, sg[:])
            tt('dve', sg, sg[:], sg, sg[:], pz[2], pz[2][:], ALU.mult)
            tt('pool', sg, sg[:].rearrange("p (h d) -> p h d", h=4), sg, sg[:].rearrange("p (h d) -> p h d", h=4),
               gnt, gnt[:].unsqueeze(1).to_broadcast([128, 4, 128]), ALU.mult)
            mm(pg2, pg2[:, 0:256], ucs, ucs[:], L, L[:])
            mm(pg2, pg2[:, 256:512], urev, urev[:], L, L[:])
            for hp in range(2):
                mm(pg1, pg1[:, hp:hp + 1], L, L[:, hp * 128:(hp + 1) * 128], m16, m16[:])
            act(eb, eb[:], pg2, pg2[:, 0:256], AF.Exp)
            act(enb, enb[:], pg2, pg2[:, 0:256], AF.Exp, scale=-1.0)
            act(ec, ec[:], pg2, pg2[:, 256:512], AF.Exp)
            act(ebl, ebl[:], pg1, pg1[:, 0:2], AF.Exp)
            stt('dve', qk, qk[:, 0, :], pz[0], pz[0][:, 0:256], 0.125, eb, eb[:], ALU.mult, ALU.mult)
            tt('dve', qk, qk[:, 1, :], pz[0], pz[0][:, 256:512], enb, enb[:], ALU.mult)
            tt('dve', qk, qk[:, 2, :], pz[0], pz[0][:, 256:512], ec, ec[:], ALU.mult)
            for a in range(4):
                tr(ptb, ptb[:, 4 + a, :], qk, qk[:, a // 2, (a % 2) * 128:(a % 2 + 1) * 128], ident)
            cp('act', qkT, qkT[:], ptb, ptb[:, 4:8, :])
            memset('dve', ssq, ssq[:], 0.0)
            for h in range(4):
                hp, hh = h // 2, h % 2
                pr = slice(hh * 64, hh * 64 + 64)
                mm(pg2, pg2[:, 0:128], qkT, qkT[pr, 2 + hp, :], qkT, qkT[pr, hp, :])
                tt('dve', AT, AT[:], pg2, pg2[:, 0:128], causal, causal[:], ALU.mult)
                o_ap = po[:, h * 128:(h + 1) * 128]
                mm(po, o_ap, AT, AT[:], vv, vv[:, h * 128:(h + 1) * 128], start=True, stop=False)
                mm(po, o_ap, qkT, qkT[pr, hp, :], Sb, Sb[pr, hp, :], start=False, stop=True)
                mm(pg2, pg2[:, 128:256], qk, qk[:, 2, hp * 128:(hp + 1) * 128], vv, vv[:, h * 128:(h + 1) * 128])
                stt('dve', Sb, Sb[pr, hp, :], S, S[pr, hp, :], ebl[pr, hp:hp + 1], pg2, pg2[pr, 128:256], ALU.mult, ALU.add, rd=[ebl])
                stt('dve', S, S[pr, hp, :], S, S[pr, hp, :], ebl[pr, hp:hp + 1], pg2, pg2[pr, 128:256], ALU.mult, ALU.add, rd=[ebl])
                act(junk2, junk2[:], po, o_ap, AF.Square, accum=ssq[:, h:h + 1], rd=[ssq], wr=[ssq])
            act(rs4, rs4[:], ssq, ssq[:], AF.Ln, scale=1.0 / 128, bias=eps_t[:, 0:1], rd=[eps_t])
            act(rs4, rs4[:], rs4, rs4[:], AF.Exp, scale=-0.5)
            tt('dve', otmp4, otmp4[:], po, po[:], sg, sg[:], ALU.mult)
            tt('dve', og, og[:].rearrange("p (h d) -> p h d", h=4), otmp4, otmp4[:].rearrange("p (h d) -> p h d", h=4),
               rs4, rs4[:].unsqueeze(2).to_broadcast([128, 4, 128]), ALU.mult)
            dma(d_ogla, d_ogla[i * 128:(i + 1) * 128, :], og, og[:])
            for _ in range((len(cast_jobs) + NT - 1) // NT):
                cast_job()
        while cj[0] < len(cast_jobs):
            cast_job()
        P.pop()

        P.push()
        w1f = P.sb([64, 32, 128], F32, "w1f"); w1b = P.sb([64, 32, 128], BF16, "w1b")
        posf = P.sb([64, 32], F32, "posf"); posb = P.sb([64, 32], BF16, "posb")
        w2f = P.sb([128, 64], F32, "w2f"); w2b = P.sb([128, 64], BF16, "w2b")
        kTg = P.sb([64, T], BF16, "kTg")
        ph = P.ps([128, 512], F32, "ph"); pb = P.ps([128, 512], F32, "pb"); pc = P.ps([128, 512], F32, "pc")
        bias_h = P.sb([128, 1], F32, "bias_h")
        H = P.sb([128, 512], BF16, "H")
        for g in range(2):
            memset('dve', kcmpT[g], kcmpT[g][:], 0.0)
            memset('dve', vcmp[g], vcmp[g][:], 0.0)
            memset('dve', vcmp[g], vcmp[g][:, :, 64:65], 1.0)
        for kv in range(2):
            dma(w1f, w1f[:], cw1, cw1[kv]); dma(posf, posf[:], cpos, cpos[kv]); dma(w2f, w2f[:], cw2, cw2[kv])
            cp('dve', w1b, w1b[:], w1f, w1f[:]); cp('dve', posb, posb[:], posf, posf[:]); cp('dve', w2b, w2b[:], w2f, w2f[:])
            for l in range(32):
                mm(pb, pb[:, 0:1], w1b, w1b[:, l, :], posb, posb[:, l:l + 1], start=(l == 0), stop=(l == 31))
            cp('dve', bias_h, bias_h[:], pb, pb[:, 0:1])
            for g in range(2):
                dma(kTg, kTg[:], d_kT, d_kT[kv, g * 64:(g + 1) * 64, :])
                for l in range(32):
                    mm(ph, ph[:, 0:NCMP], w1b, w1b[:, l, :], kTg, kTg[:, l:l + 16 * (NCMP - 1) + 1:16],
                       start=(l == 0), stop=(l == 31))
                memset('dve', H, H[:], 0.0)
                act(H, H[:, 0:NCMP], ph, ph[:, 0:NCMP], AF.Gelu_apprx_tanh, bias=bias_h[:, 0:1], rd=[bias_h])
                if kv == 0:
                    mm(pc, pc[0:64, 0:NCMP], w2b, w2b[:], H, H[:, 0:NCMP])
                    cp('act', kcmpT[g], kcmpT[g][0:64, 0:NCMP], pc, pc[0:64, 0:NCMP])
                    if debug:
                        dma(d_cmp, d_cmp[g], kcmpT[g], kcmpT[g][0:64, :])
                else:
                    for c in range(NCC):
                        mm(pc, pc[:, c * 64:(c + 1) * 64], H, H[:, c * 128:(c + 1) * 128], w2b, w2b[:])
                    cp('act', vcmp[g], vcmp[g][:, 0:NCC, 0:64], pc, pc[:, 0:NCC * 64].rearrange("p (c d) -> p c d", d=64))
        P.pop()

    if upto >= 2:
        P.push()
        selu = P.sb([128, 2, 128], BF16, "selu"); seluf = P.sb([128, 2, 128], F32, "seluf")
        dma(seluf, seluf[:], c_selu, c_selu[:].rearrange("a p t -> p a t"))
        cp('dve', selu, selu[:], seluf, seluf[:])
        exm = P.sb([128, T], BF16, "exm")
        dma(exm, exm[:], c_ex, c_ex[:])
        wimpf = P.sb([128, NCC, 128], F32, "wimpf"); wimp = P.sb([128, NCC, 128], BF16, "wimp")
        dma(wimpf, wimpf[:], c_wimp, c_wimp[:].rearrange("c p s -> p c s"))
        cp('dve', wimp, wimp[:], wimpf, wimpf[:])
        ksT = P.sb([128, T], BF16, "ksT"); kwT = P.sb([128, T], BF16, "kwT")
        memset('dve', ksT, ksT[64:128, :], 0.0)
        memset('pool', kwT, kwT[64:128, :], 0.0)
        memset('dve', ksT, ksT[64:65, :], 1.0)
        farf = P.sb([128, 512], F32, "farf")
        vs = P.sb([128, NT, 65], BF16, "vs"); vw = P.sb([128, NT, 65], BF16, "vw")
        selb = P.sb([128, 15, 512], BF16, "selb"); winb2 = P.sb([128, 6, 512], BF16, "winb2")
        bst = [P.sb([128, 512], F32, "bst%d" % i) for i in range(2)]
        cbt = [P.sb([128, 512], BF16, "cbt%d" % i) for i in range(2)]
        qrows_ = [P.sb([128, 2, 256], BF16, "qrows%d" % i) for i in range(2)]; grows_ = [P.sb([128, 2, 24], F32, "grows%d" % i) for i in range(2)]
        gown_ = [P.sb([128, 12], F32, "gown%d" % i) for i in range(2)]; gtmp = P.sb([128, 12], F32, "gtmp")
        qT_ = [P.sb([128, 4, 128], BF16, "qT%d" % i) for i in range(2)]
        for i_ in range(2):
            memset('dve', qT_[i_], qT_[i_][64:128, :, :], 0.0)
        Ec = [P.sb([128, 4, 128], BF16, "Ec%d" % i) for i in range(4)]
        Eb = [P.sb([128, 4, 128], BF16, "Eb%d" % i) for i in range(3)]
        m12_ = [P.sb([128, 2, 128], F32, "m12_%d" % i) for i in range(2)]
        imp = P.sb([128, 128], F32, "imp"); imp2 = P.sb([128, 128], F32, "imp2"); m8 = P.sb([128, 16], F32, "m8")
        mk = P.sb([128, 128], BF16, "mk"); maskT4 = P.sb([128, 4, 128], BF16, "maskT4")
        rden = P.sb([128, 4], F32, "rden"); coef = P.sb([128, 4], F32, "coef")
        oacc = P.sb([128, 4, 64], F32, "oacc"); otmp = P.sb([128, 4, 64], F32, "otmp"); onsa = P.sb([128, 256], BF16, "onsa")
        pq = P.ps([64, 4, 128], F32, "pq")
        psc = [P.ps([128, 4, 128], F32, "psc%d" % i) for i in range(2)]
        pn = P.ps([128, 4, 128], F32, "pn")
        pnT = P.ps([65, 4, 128], F32, "pnT")
        pnT2 = P.ps([65, 4, 128], F32, "pnT2")
        numTs = P.sb([65, 4, 128], F32, "numTs")
        pimp = P.ps([128, 4, 128], F32, "pimp")
        pmt = P.ps([128, 128], BF16, "pmt")
        for g in range(2):
            dma(ksT, ksT[0:64, :], d_kT, d_kT[2, g * 64:(g + 1) * 64, :])
            dma(farf, farf[:], c_far, c_far[g])
            for i_ in range(2):
                cp('dve', qT_[i_], qT_[i_][64:65, :, :], farf, farf[64:65, :].rearrange("p (h q) -> p h q", h=4))
            dma(kwT, kwT[0:64, :], d_kT, d_kT[3, g * 64:(g + 1) * 64, :])
            for n0 in range(0, NT, 16):
                n1 = min(NT, n0 + 16)
                dma(vs, vs[:, n0:n1, 0:64], d_vs, d_vs[n0 * 128:n1 * 128, g * 64:(g + 1) * 64].rearrange("(n p) c -> p n c", p=128))
                dma(vw, vw[:, n0:n1, 0:64], d_vw, d_vw[n0 * 128:n1 * 128, g * 64:(g + 1) * 64].rearrange("(n p) c -> p n c", p=128))
            memset('dve', vs, vs[:, :, 64:65], 1.0)
            memset('dve', vw, vw[:, :, 64:65], 1.0)
            k = 0
            for m in range(15):
                dma(bst[k % 2], bst[k % 2][:], c_selb, c_selb[g, :, m, :])
                tt('pool', selb, selb[:, m, :], bst[k % 2], bst[k % 2][:], farf, farf[:], ALU.subtract); k += 1
            for m in range(6):
                dma(bst[k % 2], bst[k % 2][:], c_winb, c_winb[g, :, m, :])
                cp('pool', winb2, winb2[:, m, :], bst[k % 2], bst[k % 2][:]); k += 1
            ne = 0
            ne_ = [0]
            for jj in range(NO):
                qrows, grows, gown, qT, m12 = qrows_[jj % 2], grows_[jj % 2], gown_[jj % 2], qT_[jj % 2], m12_[jj % 2]
                dma(qrows, qrows[:], d_nq, d_nq[jj * 256:(jj + 1) * 256, g * 256:(g + 1) * 256].rearrange("(u p) c -> p u c", p=128))
                dma(grows, grows[:], d_ng, d_ng[jj * 256:(jj + 1) * 256, :].rearrange("(u p) c -> p u c", p=128))
                dma(m12, m12[:], c_m12, c_m12[jj])
                for h in range(4):
                    for u in range(2):
                        mm(pq, pq[:, h, :], qrows, qrows[:, u, h * 64:(h + 1) * 64], selu, selu[:, u, :], start=(u == 0), stop=(u == 1))
                cp('act', qT, qT[0:64], pq, pq[:])
                ts('dve', gtmp, gtmp[:], grows, grows[:, 0, g * 12:(g + 1) * 12], pv[:, 0:1], None, ALU.mult, rd=[pv])
                stt('dve', gown, gown[:], grows, grows[:, 1, g * 12:(g + 1) * 12], pv[:, 1:2], gtmp, gtmp[:], ALU.mult, ALU.add, rd=[pv])
                gv3 = gown[:].rearrange("p (h k) -> p h k", k=3)
                qT_all = qT[:].rearrange("p h q -> p (h q)")
                qT_aug = qT_all

                def finish_branch(pn, br, first):
                    ts('dve', rden, rden[:], pn, pn[:, :, 64], 1e-30, None, ALU.max)
                    recip(rden, rden[:], rden, rden[:])
                    tt('dve', coef, coef[:], rden, rden[:], gown, gv3[:, :, br], ALU.mult)
                    dst = oacc if first else otmp
                    tt('dve', dst, dst[:], pn, pn[:, :, 0:64], coef, coef[:].unsqueeze(2).to_broadcast([128, 4, 64]), ALU.mult)
                    if not first:
                        tt('pool', oacc, oacc[:], oacc, oacc[:], otmp, otmp[:], ALU.add)
                    if debug:
                        dma(d_dbg, d_dbg[br, jj * 128:(jj + 1) * 128, g * 256:(g + 1) * 256], oacc, oacc[:].rearrange("p h d -> p (h d)"))

                def back_T(src):
                    cp('act', numTs, numTs[:], src, src[:])
                    for h in range(4):
                        P.op('pe', lambda e, h=h: e.transpose(out=pn[:, h, 0:65], in_=numTs[:, h, :], identity=identf[0:65, 0:65]), [numTs, identf], [pn])

                ncc = min(NCC, cmp_nchunks(jj))
                for c in range(ncc):
                    pi = pair_off[jj] + c
                    dma(bst[k % 2], bst[k % 2][:], c_cmpb, c_cmpb[g, pi])
                    cp('pool', cbt[k % 2], cbt[k % 2][:], bst[k % 2], bst[k % 2][:])
                    sc = psc[ne % 2]; ne += 1
                    sc_all = sc[:].rearrange("p h q -> p (h q)")
                    mm(sc, sc_all, kcmpT[g], kcmpT[g][:, c * 128:(c + 1) * 128], qT, qT_all, start=True, stop=False)
                    mm(sc, sc_all, ident, ident[:], cbt[k % 2], cbt[k % 2][:], start=False, stop=True)
                    k += 1
                    act(Ec[c], Ec[c][:], sc, sc[:], AF.Exp)
                for h in range(4):
                    for c in range(ncc):
                        mm(pn, pn[:, h, 0:65], Ec[c], Ec[c][:, h, :], vcmp[g], vcmp[g][:, c, :], start=(c == 0), stop=(c == ncc - 1))
                    for c in range(ncc):
                        mm(pimp, pimp[:, h, :], Ec[c], Ec[c][:, h, :], wimp, wimp[:, c, :], start=(c == 0), stop=(c == ncc - 1))
                nk = 2 * jj + 2
                k0 = max(0, 2 * jj - 4)
                bufs = {}

                def score(kind, kc, n):
                    sc = psc[ne_[0] % 2]; E = Eb[ne_[0] % 3]; ne_[0] += 1
                    bufs[n] = E
                    sc_all = sc[:].rearrange("p h q -> p (h q)")
                    if kind == 's':
                        m = 2 * jj + 1 - kc
                        mm(sc, sc_all, ksT, ksT[:, kc * 128:(kc + 1) * 128], qT, qT_aug, start=True, stop=False)
                        if m < 14:
                            mm(sc, sc_all, ident, ident[:], selb, selb[:, m, :], start=False, stop=False)
                        mm(sc, sc_all, exm, exm[:, kc * 128:(kc + 1) * 128], maskT4, mT_all, start=False, stop=True)
                    else:
                        m = 2 * jj + 1 - kc
                        mm(sc, sc_all, kwT, kwT[:, kc * 128:(kc + 1) * 128], qT, qT_all, start=True, stop=False)
                        mm(sc, sc_all, ident, ident[:], winb2, winb2[:, m, :], start=False, stop=True)
                    act(E, E[:], sc, sc[:], AF.Exp)

                def pvs(kind, kc, n):
                    E = bufs.pop(n)
                    if kind == 's':
                        mm(pnT, pnT[:].rearrange("p h q -> p (h q)"), vs, vs[:, kc, :], E, E[:].rearrange("p h q -> p (h q)"), start=(kc == 0), stop=(kc == nk - 1))
                    else:
                        mm(pnT2, pnT2[:].rearrange("p h q -> p (h q)"), vw, vw[:, kc, :], E, E[:].rearrange("p h q -> p (h q)"), start=(kc == k0), stop=(kc == nk - 1))

                def run_items(items):
                    NI = len(items)
                    for n in range(NI + 1):
                        if n < NI:
                            score(items[n][0], items[n][1], n)
                        if n >= 1:
                            pvs(items[n - 1][0], items[n - 1][1], n - 1)
                mT_all = maskT4[:].rearrange("p h q -> p (h q)")
                run_items([('w', kc) for kc in range(k0, nk)])
                finish_branch(pn, 0, True)
                for h in range(4):
                    if h == 0:
                        ts('dve', imp, imp[:], pimp, pimp[:, 0, :], rden[:, 0:1], None, ALU.mult, rd=[rden])
                    else:
                        stt('dve', imp, imp[:], pimp, pimp[:, h, :], rden[:, h:h + 1], imp, imp[:], ALU.mult, ALU.add, rd=[rden])
                tt('dve', imp, imp[:], imp, imp[:], m12, m12[:, 0, :], ALU.mult)
                tt('dve', imp, imp[:], imp, imp[:], m12, m12[:, 1, :], ALU.add)
                if debug:
                    dma(d_imp, d_imp[g, jj * 128:(jj + 1) * 128, :], imp, imp[:])
                P.op('dve', lambda e: e.max(out=m8[:, 0:8], in_=imp[:]), [imp], [m8])
                P.op('dve', lambda e: e.match_replace(out=imp2[:], in_to_replace=m8[:, 0:8], in_values=imp[:], imm_value=-1e30), [imp, m8], [imp2])
                P.op('dve', lambda e: e.max(out=m8[:, 8:16], in_=imp2[:]), [imp2], [m8])
                ts('dve', mk, mk[:], imp, imp[:], m8[:, 15:16], 1.0, ALU.is_ge, ALU.subtract, rd=[m8])
                tr(pmt, pmt[:], mk, mk[:], ident)
                P.op('act', lambda e: e.mul(out=maskT4[:], in_=pmt[:].unsqueeze(1).to_broadcast([128, 4, 128]), mul=30000.0), [pmt], [maskT4])
                run_items([('s', kc) for kc in range(nk)])
                back_T(pnT)
                finish_branch(pn, 1, False)
                back_T(pnT2)
                finish_branch(pn, 2, False)
                cp('act', onsa, onsa[:], oacc, oacc[:].rearrange("p h d -> p (h d)"))
                dma(d_omix, d_omix[jj * 128:(jj + 1) * 128, 512 + g * 256:512 + (g + 1) * 256], onsa, onsa[:])
        P.pop()

    if upto >= 3:
        P.push()
        woutb = P.sb([128, 8, D], BF16, "woutb"); wqb = P.sb([128, 8, D], BF16, "wqb"); wob = P.sb([128, 8, D], BF16, "wob")
        pwqb = P.sb([128, 8, D], BF16, "pwqb")
        skb = P.sb([128, 8, 256], BF16, "skb")
        kTm = P.sb([128, 8, 256], BF16, "kTm")
        vm = P.sb([128, 2, 4, 257], BF16, "vm")
        xt = [P.sb([128, D], F32, "x3_%d" % i) for i in range(2)]
        junk = P.sb([128, D], BF16, "junk3"); ss = P.sb([128, 1], F32, "ss3"); rstd = P.sb([128, 1], F32, "rstd3")
        hn = P.sb([128, D], BF16, "hn3"); hnT = P.sb([128, 8, 128], BF16, "hnT3")
        pt = P.ps([128, 8, 128], BF16, "pt3")
        pa = [P.ps([128, 512], F32, "pa%d" % i) for i in range(4)]
        pxa = P.ps([128, 2, 512], F32, "pxa")
        P.push()
        wst = [P.sb([128, D], F32, "wst3_%d" % i) for i in range(2)]
        wtmp = P.sb([128, 8, D], BF16, "wtmp"); skf = P.sb([128, 8, 256], F32, "skf")
        memT = P.sb([128, 8, 256], BF16, "memT")
        k = [0]

        def load_w(dst, src_t, src_ap3):
            for c in range(8):
                a = wst[k[0] % 2]
                dma(a, a[:], src_t, src_ap3[c])
                cp(['dve', 'pool'][k[0] % 2], dst, dst[:, c, :], a, a[:]); k[0] += 1
        load_w(woutb, w_out, w_out[:].rearrange("(c p) n -> c p n", p=128))
        load_w(wqb, xa_w, xa_w[0].rearrange("(c p) n -> c p n", p=128))
        load_w(wob, xa_w, xa_w[3].rearrange("(c p) n -> c p n", p=128))
        load_w(pwqb, pwq, pwq[:].rearrange("(c p) n -> c p n", p=128))
        dma(skf, skf[:], skd, skd[:].rearrange("c p n -> p c n"))
        cp('dve', skb, skb[:], skf, skf[:])
        memset('dve', vm, vm[:, :, :, 256:257], 1.0)
        for mc in range(2):
            x_t = xt[mc % 2]
            dma(x_t, x_t[:], memb, memb[mc * 128:(mc + 1) * 128, :])
            rmsnorm(x_t, x_t[:], 2, hn, hn[:], junk, ss, rstd)
            for c in range(8):
                tr(pt, pt[:, c, :], hn, hn[:, c * 128:(c + 1) * 128], ident)
            cp('act', memT, memT[:, :, mc * 128:(mc + 1) * 128], pt, pt[:])
        load_w(wtmp, xa_w, xa_w[1].rearrange("(c p) n -> c p n", p=128))
        for oc in range(8):
            for c in range(8):
                mm(pa[0], pa[0][:, 0:256], wtmp, wtmp[:, c, oc * 128:(oc + 1) * 128], memT, memT[:, c, :], start=(c == 0), stop=(c == 7))
            cp('act', kTm, kTm[:, oc, :], pa[0], pa[0][:, 0:256])
        load_w(wtmp, xa_w, xa_w[2].rearrange("(c p) n -> c p n", p=128))
        for mc in range(2):
            for half in range(2):
                for c in range(8):
                    mm(pa[half], pa[half][:], memT, memT[:, c, mc * 128:(mc + 1) * 128], wtmp, wtmp[:, c, half * 512:(half + 1) * 512], start=(c == 0), stop=(c == 7))
                cp('act', vm, vm[:, mc, half * 2:half * 2 + 2, 0:256], pa[half], pa[half][:].rearrange("p (h d) -> p h d", d=256))
        P.pop()
        og2 = P.sb([128, 2, 512], BF16, "og2"); omx = P.sb([128, D], BF16, "omx"); otm = P.sb([128, 512], F32, "otm")
        h1 = P.sb([128, D], F32, "h1"); qTx = P.sb([128, 8, 128], BF16, "qTx")
        Ex = P.sb([128, 2, 4, 128], BF16, "Ex"); rdx = P.sb([128, 4], F32, "rdx")
        oxa = P.sb([128, D], BF16, "oxa")
        hn3T = P.sb([128, 8, 128], BF16, "hn3Ts"); qpT = P.sb([128, 8, 128], BF16, "qpT")
        sc_ = [P.sb([128, 16, 128], F32, "scr%d" % i) for i in range(2)]; ab_ = [P.sb([128, 16, 128], F32, "ab%d" % i) for i in range(2)]
        negm = P.sb([128, 16], F32, "negm"); t16 = P.sb([128, 16, 16], F32, "t16"); scr2_ = [P.sb([128, 128], F32, "scr2_%d" % i) for i in range(4)]
        candall = P.sb([128, 8, 256], F32, "candall"); cand2_ = [P.sb([128, 256], F32, "cand2_%d" % i) for i in range(4)]; c16 = P.sb([128, 8, 16], F32, "c16")
        route = P.sb([128, 16], F32, "route"); zs = P.sb([128, 8], F32, "zs")
        memset('dve', route, route[:], 0.0)
        def Xgen(jj):
            sc = sc_[jj % 2]
            x_t = xt[jj % 2]
            dma(x_t, x_t[:], xo, xo[jj * 128:(jj + 1) * 128, :])
            dma(og2, og2[:], d_ogla, d_ogla[jj * 256:(jj + 1) * 256, :].rearrange("(u p) c -> p u c", p=128))
            dma(omx, omx[:, 512:1024], d_omix, d_omix[jj * 128:(jj + 1) * 128, 512:1024])
            ts('dve', otm, otm[:], og2, og2[:, 0, :], pv[:, 0:1], None, ALU.mult, rd=[pv])
            stt('dve', omx, omx[:, 0:512], og2, og2[:, 1, :], pv[:, 1:2], otm, otm[:], ALU.mult, ALU.add, rd=[pv])
            for c in range(8):
                tr(pt, pt[:, c, :], omx, omx[:, c * 128:(c + 1) * 128], ident)
            cp('act', hnT, hnT[:], pt, pt[:])
            for half in range(2):
                for c in range(8):
                    mm(pa[half], pa[half][:], hnT, hnT[:, c, :], woutb, woutb[:, c, half * 512:(half + 1) * 512], start=(c == 0), stop=(c == 7))
                tt('dve', h1, h1[:, half * 512:(half + 1) * 512], pa[half], pa[half][:], x_t, x_t[:, half * 512:(half + 1) * 512], ALU.add)
            yield
            rmsnorm(h1, h1[:], 1, hn, hn[:], junk, ss, rstd)
            for c in range(8):
                tr(pt, pt[:, c, :], hn, hn[:, c * 128:(c + 1) * 128], ident)
            cp('act', hnT, hnT[:], pt, pt[:])
            for oc in range(8):
                pq_ = pa[2 + oc % 2]
                for c in range(8):
                    mm(pq_, pq_[:, 0:128], wqb, wqb[:, c, oc * 128:(oc + 1) * 128], hnT, hnT[:, c, :], start=(c == 0), stop=(c == 7))
                cp(['act', 'dve'][oc % 2], qTx, qTx[:, oc, :], pq_, pq_[:, 0:128])
                yield
            yield
            for mc in range(2):
                for h in range(4):
                    for dc in range(2):
                        mm(pxa, pxa[:, mc, h * 128:(h + 1) * 128], kTm, kTm[:, h * 2 + dc, mc * 128:(mc + 1) * 128], qTx, qTx[:, h * 2 + dc, :], start=(dc == 0), stop=(dc == 1))
            act(Ex, Ex[:].rearrange("p a h q -> p a (h q)"), pxa, pxa[:], AF.Exp, scale=1.0 / 16)
            for h in range(4):
                o_ap = pxa[:, h // 2, (h % 2) * 256:(h % 2) * 256 + 256]
                for mc in range(2):
                    mm(pa[h % 2], pa[h % 2][:, 0:257], Ex, Ex[:, mc, h, :], vm, vm[:, mc, h, :], start=(mc == 0), stop=(mc == 1))
                ts('dve', rdx, rdx[:, h:h + 1], pa[h % 2], pa[h % 2][:, 256:257], 1e-30, None, ALU.max)
                recip(rdx, rdx[:, h:h + 1], rdx, rdx[:, h:h + 1])
                ts('dve', oxa, oxa[:, h * 256:(h + 1) * 256], pa[h % 2], pa[h % 2][:, 0:256], rdx[:, h:h + 1], None, ALU.mult, rd=[rdx])
            yield
            for c in range(8):
                tr(pt, pt[:, c, :], oxa, oxa[:, c * 128:(c + 1) * 128], ident)
            cp('act', hnT, hnT[:], pt, pt[:])
            for half in range(2):
                for c in range(8):
                    mm(pa[half], pa[half][:], hnT, hnT[:, c, :], wob, wob[:, c, half * 512:(half + 1) * 512], start=(c == 0), stop=(c == 7))
                tt('dve', h1, h1[:, half * 512:(half + 1) * 512], pa[half], pa[half][:], h1, h1[:, half * 512:(half + 1) * 512], ALU.add)
            dma(d_h, d_h[jj * 128:(jj + 1) * 128, :], h1, h1[:])
            yield
            rmsnorm(h1, h1[:], 3, hn, hn[:], junk, ss, rstd)
            for c in range(8):
                tr(pt, pt[:, c, :], hn, hn[:, c * 128:(c + 1) * 128], ident)
            cp('act', hn3T, hn3T[:], pt, pt[:])
            dma(d_hn3T, d_hn3T[:, :, jj * 128:(jj + 1) * 128], hn3T, hn3T[:])
            for oc in range(8):
                pq_ = pa[2 + oc % 2]
                for c in range(8):
                    mm(pq_, pq_[:, 0:128], pwqb, pwqb[:, c, oc * 128:(oc + 1) * 128], hn3T, hn3T[:, c, :], start=(c == 0), stop=(c == 7))
                cp(['act', 'dve'][oc % 2], qpT, qpT[:, oc, :], pq_, pq_[:, 0:128])
                yield
            yield
            for oc in range(8):
                pq_ = pa[oc % 2]
                mm(pq_, pq_[:, 0:256], qpT, qpT[:, oc, :], skb, skb[:, oc, :])
                cp(['act', 'dve'][oc % 2], sc, sc[:, 2 * oc:2 * oc + 2, :], pq_, pq_[:, 0:256].rearrange("p (a k) -> p a k", a=2))
            yield

        def Rgen(jj):
            sc = sc_[jj % 2]; ab = ab_[jj % 2]
            P.op('dve', lambda e: e.tensor_reduce(out=negm[:], in_=sc[:], axis=AX.X, op=ALU.max), [sc], [negm])
            ts('dve', negm, negm[:], negm, negm[:], -1.0, None, ALU.mult)
            for r in range(16):
                act(ab, ab[:, r, :], sc, sc[:, r, :], AF.Exp, bias=negm[:, r:r + 1], rd=[negm])
            yield
            for r0 in range(0, 16, 4):
                for r in range(r0, r0 + 4):
                    P.op('dve', lambda e, r=r: e.max(out=t16[:, r, 0:8], in_=ab[:, r, :]), [ab], [t16])
                for r in range(r0, r0 + 4):
                    P.op('dve', lambda e, r=r: e.match_replace(out=scr2_[r % 4][:], in_to_replace=t16[:, r, 0:8], in_values=ab[:, r, :], imm_value=-1.0), [ab, t16], [scr2_[r % 4]])
                for r in range(r0, r0 + 4):
                    P.op('dve', lambda e, r=r: e.max(out=t16[:, r, 8:16], in_=scr2_[r % 4][:]), [scr2_[r % 4]], [t16])
                yield
            t16v = t16[:].rearrange("p (h a) k -> p h a k", a=2)
            abv = ab[:].rearrange("p (h a) k -> p h a k", a=2)

            def cand_top16():
                yield
                tt('dve', candall, candall[:].rearrange("p h (a b) -> p h a b", a=16),
                   t16, t16v[:, :, 0, :].unsqueeze(3).to_broadcast([128, 8, 16, 16]),
                   t16, t16v[:, :, 1, :].unsqueeze(2).to_broadcast([128, 8, 16, 16]), ALU.mult)
                for h0 in range(0, 8, 4):
                    for h in range(h0, h0 + 4):
                        P.op('dve', lambda e, h=h: e.max(out=c16[:, h, 0:8], in_=candall[:, h, :]), [candall], [c16])
                    for h in range(h0, h0 + 4):
                        P.op('dve', lambda e, h=h: e.match_replace(out=cand2_[h % 4][:], in_to_replace=c16[:, h, 0:8], in_values=candall[:, h, :], imm_value=-1.0), [candall, c16], [cand2_[h % 4]])
                    for h in range(h0, h0 + 4):
                        P.op('dve', lambda e, h=h: e.max(out=c16[:, h, 8:16], in_=cand2_[h % 4][:]), [cand2_[h % 4]], [c16])
                    yield
            yield from cand_top16()
            P.op('dve', lambda e: e.tensor_reduce(out=zs[:], in_=c16[:], axis=AX.X, op=ALU.add), [c16], [zs])
            recip(zs, zs[:], zs, zs[:])
            tt('dve', ab, abv[:, :, 1, :], ab, abv[:, :, 1, :], zs, zs[:].unsqueeze(2).to_broadcast([128, 8, 128]), ALU.mult)
            stt('dve', route, route[:, 0:8], c16, c16[:, :, 15], 1.0 - 1e-6, zs, zs[:], ALU.mult, ALU.mult)
            dma(d_route, d_route[jj * 128:(jj + 1) * 128, 0:2048], ab, ab[:].rearrange("p r k -> p (r k)"))
            dma(d_route, d_route[jj * 128:(jj + 1) * 128, 2048:2064], route, route[:])
            yield

        def drain(g):
            for _ in g:
                pass
        drain(Xgen(0))
        for jj in range(NO):
            gr = Rgen(jj)
            gx = Xgen(jj + 1) if jj + 1 < NO else iter(())
            ra = xa_ = True
            while ra or xa_:
                if xa_:
                    xa_ = next(gx, 'END') != 'END'
                if ra:
                    ra = next(gr, 'END') != 'END'
        P.pop()

    if upto >= 4:
        P.push()
        TG = 2
        IC = 16
        NCH = 128 // IC
        ACT_HEADS = (1, 3, 4, 6, 7)
        hT = P.sb([128, 8, TG * 128], BF16, "hT")
        ab = [P.sb([128, 16, 128], F32, "ab4_%d" % u) for u in range(TG)]
        rt = [P.sb([128, 16], F32, "rt%d" % u) for u in range(TG)]
        Wc = [[P.sb([128, IC * 128], BF16, "Wc%d_%d" % (i, u)) for u in range(TG)] for i in range(2)]
        et = [P.sb([128, IC, 128], F32, "et%d" % i) for i in range(4)]
        mt = [P.sb([128, IC, 128], BF16, "mt%d" % i) for i in range(2)]
        dnb = [P.sb([128, 8, 512], BF16, "dnb%d" % i) for i in range(3)]
        upb = [P.sb([128, 4, D], BF16, "upb%d" % i) for i in range(3)]
        Gs = [P.sb([128, 512], BF16, "G%d" % i) for i in range(2)]
        GT = [P.sb([128, 4, 128], BF16, "GT%d" % i) for i in range(2)]
        py = [P.ps([128, 2, 512], F32, "py%d" % u) for u in range(TG)]
        pd = [P.ps([128, 512], F32, "pd%d" % i) for i in range(2)]
        ptg = [P.ps([128, 8, 128], BF16, "ptg%d" % i) for i in range(2)]
        h2 = P.sb([128, D], F32, "h2"); yo = P.sb([128, D], F32, "yo")
        junk = P.sb([128, D], BF16, "junk4"); ss = P.sb([128, 1], F32, "ss4"); rstd = P.sb([128, 1], F32, "rstd4")
        qn = [0]
        for tg in range(NO // TG):
            dma(hT, hT[:], d_hn3T, d_hn3T[:, :, tg * TG * 128:(tg + 1) * TG * 128])
            for u in range(TG):
                j = tg * TG + u
                dma(ab[u], ab[u][:].rearrange("p r k -> p (r k)"), d_route, d_route[j * 128:(j + 1) * 128, 0:2048])
                dma(rt[u], rt[u][:], d_route, d_route[j * 128:(j + 1) * 128, 2048:2064])

            def wgen(c):
                its = [(u, h) for u in range(TG) for h in range(8)]
                K = len(its)
                eb_ = {}; mb_ = {}

                def E_(n):
                    u, h = its[n]
                    e_ = et[qn[0] % 4]; qn[0] += 1
                    eb_[n] = e_
                    if h in ACT_HEADS:
                        for i_ in range(IC):
                            P.op('act', lambda e, e_=e_, i_=i_, u=u, h=h: e.activation(out=e_[:, i_, :], in_=ab[u][:, 2 * h + 1, :], func=AF.Copy,
                                                                                  scale=ab[u][:, 2 * h, c * IC + i_:c * IC + i_ + 1]),
                                 [ab[u]], [e_] if i_ in (0, IC - 1) else [])
                    else:
                        tt('dve', e_, e_[:], ab[u], ab[u][:, 2 * h, c * IC:(c + 1) * IC].unsqueeze(2).to_broadcast([128, IC, 128]),
                           ab[u], ab[u][:, 2 * h + 1, :].unsqueeze(1).to_broadcast([128, IC, 128]), ALU.mult)

                def S_(n):
                    u, h = its[n]
                    e_ = eb_.pop(n)
                    w_ap = Wc[c % 2][u][:].rearrange("p (a b) -> p a b", a=IC)
                    if h == 0:
                        stt('dve', Wc[c % 2][u], w_ap, e_, e_[:], rt[u][:, h:h + 1], e_, e_[:], ALU.is_ge, ALU.mult, rd=[rt[u]])
                    else:
                        m_ = mt[n % 2]
                        mb_[n] = m_
                        stt('dve', m_, m_[:], e_, e_[:], rt[u][:, h:h + 1], e_, e_[:], ALU.is_ge, ALU.mult, rd=[rt[u]])

                def A_(n):
                    u, h = its[n]
                    if h == 0:
                        return
                    m_ = mb_.pop(n)
                    w_ap = Wc[c % 2][u][:].rearrange("p (a b) -> p a b", a=IC)
                    tt('dve', Wc[c % 2][u], w_ap, Wc[c % 2][u], w_ap, m_, m_[:], ALU.add)
                for n in range(K + 2):
                    if n < K:
                        E_(n)
                    if 1 <= n <= K:
                        S_(n - 1)
                    if n >= 2:
                        A_(n - 2)
                    yield

            def load_w(ecx):
                dn, ub = dnb[ecx % 3], upb[ecx % 3]
                dma(dn, dn[:], d_downT, d_downT[:, ecx * 512:(ecx + 1) * 512].rearrange("(c p) e -> p c e", p=128))
                dma(ub, ub[:], d_up, d_up[ecx * 512:(ecx + 1) * 512, :].rearrange("(s p) d -> p s d", p=128))

            items = [(ecx, u) for ecx in range(32) for u in range(TG)]
            N = len(items)

            def stA(n):
                ecx, u = items[n]
                if u == 0:
                    if ecx == 0:
                        load_w(0)
                    if ecx + 1 < 32:
                        load_w(ecx + 1)
                    if ecx % 4 == 0:
                        for _ in wg[0]:
                            pass
                        wg[0] = wgen(ecx // 4 + 1) if ecx // 4 + 1 < NCH else iter(())
                for _ in range(3):
                    next(wg[0], None)
                dn = dnb[ecx % 3]
                pdt = pd[n % 2]; G = Gs[n % 2]
                for c in range(8):
                    mm(pdt, pdt[:], hT, hT[:, c, u * 128:(u + 1) * 128], dn, dn[:, c, :], start=(c == 0), stop=(c == 7))
                act(G, G[:], pdt, pdt[:], AF.Gelu_apprx_tanh)
                wch = Wc[(ecx // 4) % 2][u]
                tt('dve', G, G[:], G, G[:], wch, wch[:, (ecx % 4) * 512:(ecx % 4 + 1) * 512], ALU.mult)

            def stB(n):
                G = Gs[n % 2]; gt_ = GT[n % 2]; pt_ = ptg[n % 2]
                for s_ in range(4):
                    tr(pt_, pt_[:, s_, :], G, G[:, s_ * 128:(s_ + 1) * 128], ident)
                cp('act', gt_, gt_[:], pt_, pt_[:, 0:4, :])

            def stC(n):
                ecx, u = items[n]
                gt_ = GT[n % 2]; ub = upb[ecx % 3]
                for half in range(2):
                    for s_ in range(4):
                        mm(py[u], py[u][:, half, :], gt_, gt_[:, s_, :], ub, ub[:, s_, half * 512:(half + 1) * 512],
                           start=(ecx == 0 and s_ == 0), stop=(ecx == 31 and s_ == 3))

            wg = [iter(())]
            for _ in wgen(0):
                pass
            for n in range(N + 2):
                if n < N:
                    stA(n)
                if 1 <= n <= N:
                    stB(n - 1)
                if n >= 2:
                    stC(n - 2)
            for _ in wg[0]:
                pass
            for u in range(TG):
                j = tg * TG + u
                dma(h2, h2[:], d_h, d_h[j * 128:(j + 1) * 128, :])
                tt('dve', h2, h2[:], h2, h2[:], py[u], py[u][:].rearrange("p a b -> p (a b)"), ALU.add)
                rmsnorm(h2, h2[:], 4, yo, yo[:], junk, ss, rstd)
                dma(out, out[j * 128:(j + 1) * 128, :], yo, yo[:])
        P.pop()

    P.finish()
    return nc, dict(npair=npair, pair_off=pair_off, NCC=NCC, NCMP=NCMP, ninst=P.ninst)


def _t5_bucket_np(dist):
    n = np.maximum(dist, 0)
    nf = np.maximum(n, 1).astype(np.float32)
    log_ratio = (np.log(nf / np.float32(16)) / np.float32(math.log(2048 / 16))).astype(np.float32)
    large = 16 + (log_ratio * np.float32(16)).astype(np.int32)
    large = np.minimum(large, 31)
    return np.where(n < 16, n, large).astype(np.int64)


def _bias_tile(rel_bias, g, dist, valid):
    bk = _t5_bucket_np(dist)
    outt = np.empty((128, 4, 128), np.float32)
    for h in range(4):
        outt[:, h, :] = np.where(valid, rel_bias[bk, g * 4 + h], np.float32(NEG))
    return outt.reshape(128, 512)


def make_core_inputs(inputs, T, b, p, meta):
    NT = T // 128; NO = NT // 2; NS = T // 64; NCMP = meta['NCMP']; NCC = meta['NCC']
    f = lambda a: np.ascontiguousarray(np.asarray(a, dtype=np.float32))
    x = f(inputs['x'][b]); rel_bias = f(inputs['rel_bias'])
    m = {}
    m['xb'] = x
    m['xo'] = np.ascontiguousarray(x.reshape(NO, 2, 128, D)[:, p].reshape(NO * 128, D))
    m['mem'] = f(inputs['mem'][b])
    w_in = f(inputs['w_in'][0])
    m['w_in'] = np.ascontiguousarray(np.concatenate([w_in[:, ORIG[k][0]:ORIG[k][1]] for k in PERM_ORDER], axis=1))
    m['gw2'] = f(inputs['gla_gate_w2'][0]); m['gb'] = f(inputs['gla_gate_b'][0]).reshape(1, 256)
    m['gnorm'] = f(inputs['gla_out_norm'][0]).reshape(1, 128)
    m['norms'] = np.stack([f(inputs['norm_mix'][0]), f(inputs['norm_xattn'][0]), f(inputs['norm_mem'][0]),
                           f(inputs['norm_ffn'][0]), f(inputs['norm_final'])], 0)
    cw1 = np.stack([f(inputs['cmp_k_w1'][0]), f(inputs['cmp_v_w1'][0])], 0)
    m['cw1'] = np.ascontiguousarray(cw1.reshape(2, 32, 64, 128).transpose(0, 2, 1, 3))
    cpos = np.stack([f(inputs['cmp_pos_k'][0]), f(inputs['cmp_pos_v'][0])], 0)
    m['cpos'] = np.ascontiguousarray(cpos.transpose(0, 2, 1))
    m['cw2'] = np.stack([f(inputs['cmp_k_w2'][0]), f(inputs['cmp_v_w2'][0])], 0)
    m['w_out'] = f(inputs['w_out'][0])
    m['xa_w'] = np.stack([f(inputs['xa_wq'][0]), f(inputs['xa_wk'][0]), f(inputs['xa_wv'][0]), f(inputs['xa_wo'][0])], 0)
    m['pwq'] = f(inputs['peer_wq'][0])
    sk = f(inputs['peer_subkeys'][0])
    skd = np.zeros((8, 128, 256), np.float32)
    for h in range(8):
        for pp in range(2):
            skd[h, pp * 64:(pp + 1) * 64, pp * 128:(pp + 1) * 128] = sk[h, pp].T
    m['skd'] = skd
    m['downT'] = np.ascontiguousarray(f(inputs['peer_down'][0]).T)
    m['up'] = f(inputs['peer_up'][0])
    m['c_ident'] = np.eye(128, dtype=np.float32)
    s_ = np.arange(128)[:, None]; t_ = np.arange(128)[None, :]
    m['c_ucs'] = np.where(s_ <= t_, -1.0 / 16, 0.0).astype(np.float32)
    m['c_urev'] = np.where(s_ > t_, -1.0 / 16, 0.0).astype(np.float32)
    m['c_causal'] = (s_ <= t_).astype(np.float32)
    m['c_selu'] = np.stack([np.eye(128) * (1 - p), np.eye(128) * p], 0).astype(np.float32)
    m['c_pv'] = np.tile(np.array([[1.0 - p, float(p)]], np.float32), (128, 1))
    kk = np.arange(128)[:, None]; qq = np.arange(128)[None, :]
    selb = np.empty((2, 128, 15, 512), np.float32); winb = np.empty((2, 128, 6, 512), np.float32)
    for g in range(2):
        for mm_ in range(15):
            j = mm_ - 1 + p
            dist = 128 * j + qq - kk
            selb[g, :, mm_, :] = _bias_tile(rel_bias, g, dist, (dist >= 0) & (j >= 0))
        for mm_ in range(6):
            j = mm_ - 1 + p
            dist = 128 * j + qq - kk
            winb[g, :, mm_, :] = _bias_tile(rel_bias, g, dist, (dist >= 0) & (dist < 512) & (j >= 0))
    m['c_selb'] = selb; m['c_winb'] = winb
    cmpb = np.empty((2, meta['npair'], 128, 512), np.float32)
    for jj in range(NO):
        for c in range(min(NCC, cmp_nchunks(jj))):
            n = 128 * c + kk
            t = (2 * jj + p) * 128 + qq
            dist = t - (16 * n + 31)
            for g in range(2):
                cmpb[g, meta['pair_off'][jj] + c] = _bias_tile(rel_bias, g, dist, (dist >= 0) & (n < NCMP))
    m['c_cmpb'] = cmpb
    far = np.empty((2, 128, 4, 128), np.float32)
    for g in range(2):
        for h in range(4):
            far[g, :, h, :] = rel_bias[31, g * 4 + h]
    m['c_far'] = far.reshape(2, 128, 512)
    wimp = np.zeros((NCC * 128, 128), np.float32)
    for s in range(NS):
        for r in range(-1, 4):
            n = 4 * s + r
            lo = 16 * r
            ov = min(lo + 32, 64) - max(lo, 0)
            if 0 <= n < NCMP:
                wimp[n, s] += ov / 32.0
    m['c_wimp'] = wimp.reshape(NCC, 128, 128)
    m12 = np.zeros((NO, 128, 2, 128), np.float32)
    sid = np.arange(128)[None, :]
    for jj in range(NO):
        t = (2 * jj + p) * 128 + np.arange(128)[:, None]
        cur = t // 64
        visible = (sid * 64 <= t) & (sid < NS)
        f0 = (sid == 0); f1 = (sid == cur); f2 = (sid == cur - 1)
        forced = f0 | f1 | f2
        m12[jj, :, 0, :] = (visible & ~forced)
        add = np.where(~visible, -100.0 - sid, 0.0)
        add = np.where(f2, 100.0, add); add = np.where(f1, 101.0, add); add = np.where(f0 & (sid < NS), 102.0, add)
        m12[jj, :, 1, :] = add
    m['c_m12'] = m12
    ex = np.zeros((128, T), np.float32)
    ex[np.arange(T) // 64, np.arange(T)] = 1.0
    m['c_ex'] = ex.astype(NPBF)
    return m


_CACHE = {}


def kernel(**inputs):
    T = inputs['x'].shape[1]
    B = inputs['x'].shape[0]
    if T not in _CACHE:
        _CACHE[T] = build(T)
    nc, meta = _CACHE[T]
    in_maps = []
    for c in range(2 * B):
        in_maps.append(make_core_inputs(inputs, T, c // 2, c % 2, meta))
    res = run_bass_kernel_spmd(nc, in_maps, core_ids=list(range(2 * B)))
    NO = T // 256
    outp = np.empty((B, T // 128, 128, D), np.float32)
    for c in range(2 * B):
        o = np.asarray(res.results[c]["out"], dtype=np.float32).reshape(NO, 128, D)
        outp[c // 2, (c % 2)::2] = o
    return outp.reshape(B, T, D)
```

```python
import math
import numpy as np
import ml_dtypes
import concourse.bass as bass
import concourse.mybir as mybir
from concourse.bass_utils import run_bass_kernel_spmd
from contextlib import ExitStack

F32 = mybir.dt.float32
BF16 = mybir.dt.bfloat16
AF = mybir.ActivationFunctionType
ALU = mybir.AluOpType
AX = mybir.AxisListType
NPBF = ml_dtypes.bfloat16

D = 1024
NEG = -30000.0


class T:
    def __init__(self, h, name):
        self.h = h
        self.name = name
        self.w = None
        self.r = []
        self.psum = False
        self.dram = False

    def __getitem__(self, k):
        return self.h[k]


class Prog:
    def __init__(self, nc, n_dma_sems=48):
        self.nc = nc
        self.es = ExitStack()
        self.scopes = []
        self.eng = {'pe': nc.tensor, 'dve': nc.vector, 'act': nc.scalar,
                    'pool': nc.gpsimd, 'sp': nc.sync}
        self.sems = {}
        for k in self.eng:
            self.sems['e_' + k] = self.es.enter_context(nc.semaphore('e_' + k))
        self.cnt = {k: 0 for k in self.sems}
        self.ndma = n_dma_sems
        for i in range(n_dma_sems):
            key = 'd_%d' % i
            self.sems[key] = self.es.enter_context(nc.semaphore(key))
            self.cnt[key] = 0
        self.dma_rr = 0
        self.known = {k: {} for k in self.eng}
        self.ntile = 0
        self.ninst = 0

    def push(self):
        self.scopes.append(ExitStack())

    def pop(self):
        self.barrier()
        self.scopes.pop().close()

    def _stack(self):
        return self.scopes[-1] if self.scopes else self.es

    def sb(self, shape, dt, name=None):
        self.ntile += 1
        name = (name or 't') + '_%d' % self.ntile
        h = self._stack().enter_context(self.nc.sbuf_tensor(name, list(shape), dt))
        return T(h, name)

    def ps(self, shape, dt, name=None):
        self.ntile += 1
        name = (name or 'p') + '_%d' % self.ntile
        h = self._stack().enter_context(self.nc.psum_tensor(name, list(shape), dt))
        t = T(h, name)
        t.psum = True
        return t

    def dram(self, name, shape, dt, kind="Internal"):
        h = self.nc.dram_tensor(name, list(shape), dt, kind=kind).ap()
        t = T(h, name)
        t.dram = True
        return t

    def _deps(self, reads, writes, e=None):
        deps = []
        for t in reads:
            if t.w is not None:
                deps.append(t.w)
            if t.psum:
                deps.extend([tok for tok in t.r if tok[0] != 'e_' + str(e)])
        for t in writes:
            if t.w is not None:
                deps.append(t.w)
            deps.extend(t.r)
        return deps

    def _waits(self, e, deps):
        kn = self.known[e]
        need = {}
        for (k, v) in deps:
            if kn.get(k, 0) >= v:
                continue
            if need.get(k, 0) < v:
                need[k] = v
        for k, v in need.items():
            kn[k] = v
        return list(need.items())

    def _emit(self, e, waits, fn, inc):
        eng = self.eng[e]
        for (k, v) in waits:
            eng.wait_ge(self.sems[k], v)
        ins = fn(eng)
        ins.then_inc(self.sems[inc[0]], inc[1])
        self.ninst += 1

    def op(self, e, fn, reads=(), writes=()):
        deps = self._deps(reads, writes, e)
        key = 'e_' + e
        if e == 'pe':
            deps = [d for d in deps if d[0] != key]
        waits = self._waits(e, deps)
        self.cnt[key] += 1
        tok = (key, self.cnt[key])
        self._emit(e, waits, fn, (key, 1))
        for t in reads:
            t.r.append(tok)
        for t in writes:
            t.w = tok
            t.r = []
        return tok

    def dma(self, e, out_t, out_ap, in_t, in_ap, **kw):
        reads = [in_t]
        writes = [out_t]
        deps = self._deps(reads, writes)
        key = 'd_%d' % self.dma_rr
        self.dma_rr = (self.dma_rr + 1) % self.ndma
        if self.cnt[key] > 0:
            deps.append((key, self.cnt[key]))
        waits = self._waits(e, deps)
        self.cnt[key] += 16
        tok = (key, self.cnt[key])
        self._emit(e, waits, lambda eng: eng.dma_start(out=out_ap, in_=in_ap, **kw), (key, 16))
        in_t.r.append(tok)
        out_t.w = tok
        out_t.r = []
        return tok

    def barrier(self):
        allt = [(k, v) for k, v in self.cnt.items() if v > 0]
        for e in self.eng:
            for (k, v) in self._waits(e, allt):
                self.eng[e].wait_ge(self.sems[k], v)

    def finish(self):
        self.barrier()
        while self.scopes:
            self.scopes.pop().close()
        self.es.close()


ORIG = dict(gq=(0, 256), gk=(256, 512), gv=(512, 1024), gr=(1024, 1536), glr=(1536, 1552),
            nq=(1552, 2064), kc=(2064, 2192), vc=(2192, 2320), ks=(2320, 2448), vs=(2448, 2576),
            kw=(2576, 2704), vw=(2704, 2832), ng=(2832, 2856))
PERM_ORDER = ['gq', 'gk', 'gv', 'gr', 'nq', 'kc', 'vc', 'ks', 'vs', 'kw', 'vw', 'ng', 'glr']
NCOL = 2856


def cmp_nchunks(jj):
    return min(4, (16 * jj + 15 + 127) // 128)


def build(T, debug=False, upto=9, cut=99):
    NT = T // 128
    NO = NT // 2
    NS = T // 64
    TO = T // 2
    NCMP = (T - 32) // 16 + 1
    NCC = (NCMP + 127) // 128
    pair_off = []
    npair = 0
    for jj in range(NO):
        pair_off.append(npair)
        npair += min(NCC, cmp_nchunks(jj))

    nc = bass.Bass("TRN2", target_bir_lowering=False)
    P = Prog(nc)
    SK = "ExternalOutput" if debug else "Internal"

    def inp(name, shape, dt=F32):
        return P.dram(name, shape, dt, kind="ExternalInput")

    xb = inp("xb", [T, D]); xo = inp("xo", [TO, D]); memb = inp("mem", [256, D])
    w_in = inp("w_in", [D, NCOL]); gw2 = inp("gw2", [16, 256]); gb = inp("gb", [1, 256])
    gnorm = inp("gnorm", [1, 128])
    norms = inp("norms", [5, D])
    cw1 = inp("cw1", [2, 64, 32, 128]); cpos = inp("cpos", [2, 64, 32]); cw2 = inp("cw2", [2, 128, 64])
    w_out = inp("w_out", [D, D]); xa_w = inp("xa_w", [4, D, D])
    pwq = inp("pwq", [D, D]); skd = inp("skd", [8, 128, 256])
    downT = inp("downT", [D, 16384]); up = inp("up", [16384, D])
    c_ident = inp("c_ident", [128, 128]); c_ucs = inp("c_ucs", [128, 128]); c_urev = inp("c_urev", [128, 128])
    c_causal = inp("c_causal", [128, 128]); c_selu = inp("c_selu", [2, 128, 128]); c_pv = inp("c_pv", [128, 2])
    c_selb = inp("c_selb", [2, 128, 15, 512]); c_winb = inp("c_winb", [2, 128, 6, 512])
    c_cmpb = inp("c_cmpb", [2, npair, 128, 512])
    c_far = inp("c_far", [2, 128, 512])
    c_wimp = inp("c_wimp", [NCC, 128, 128]); c_m12 = inp("c_m12", [NO, 128, 2, 128])
    c_ex = inp("c_ex", [128, T], BF16)
    out = P.dram("out", [TO, D], F32, kind="ExternalOutput")

    d_nq = P.dram("d_nq", [T, 512], BF16, kind=SK)
    d_ng = P.dram("d_ng", [T, 24], F32, kind=SK)
    d_ogla = P.dram("d_ogla", [T, 512], BF16, kind=SK)
    d_kT = P.dram("d_kT", [4, 128, T], BF16, kind=SK)
    d_vs = P.dram("d_vs", [T, 128], BF16, kind=SK)
    d_vw = P.dram("d_vw", [T, 128], BF16, kind=SK)
    d_omix = P.dram("d_omix", [TO, D], BF16, kind=SK)
    d_h = P.dram("d_h", [TO, D], F32, kind=SK)
    d_hn3T = P.dram("d_hn3T", [128, 8, TO], BF16, kind=SK)
    d_route = P.dram("d_route", [TO, 2064], F32, kind=SK)
    d_downT = P.dram("d_downT", [D, 16384], BF16, kind="Internal")
    d_up = P.dram("d_up", [16384, D], BF16, kind="Internal")
    d_cmp = P.dram("d_cmp", [2, 64, 512], BF16, kind=SK)
    d_dbg = P.dram("d_dbg", [3, TO, 512], F32, kind=SK)
    d_imp = P.dram("d_imp", [2, TO, 128], F32, kind=SK)

    def mm(ot, o_ap, lt, l_ap, rt, r_ap, start=True, stop=True):
        P.op('pe', lambda e: e.matmul(o_ap, lhsT=l_ap, rhs=r_ap, start=start, stop=stop), [lt, rt], [ot])

    def tr(ot, o_ap, it, i_ap, idt):
        P.op('pe', lambda e: e.transpose(out=o_ap, in_=i_ap, identity=idt[:]), [it, idt], [ot])

    def act(ot, o_ap, it, i_ap, func, bias=None, scale=None, accum=None, rd=(), wr=()):
        kw = {}
        if bias is not None:
            kw['bias'] = bias
        if scale is not None:
            kw['scale'] = scale
        if accum is not None:
            kw['accum_out'] = accum
        P.op('act', lambda e: e.activation(out=o_ap, in_=i_ap, func=func, **kw), [it] + list(rd), [ot] + list(wr))

    def cp(eng, ot, o_ap, it, i_ap):
        if eng == 'act':
            P.op('act', lambda e: e.copy(out=o_ap, in_=i_ap), [it], [ot])
        else:
            P.op(eng, lambda e: e.tensor_copy(out=o_ap, in_=i_ap), [it], [ot])

    def tt(eng, ot, o_ap, at, a_ap, bt, b_ap, op):
        P.op(eng, lambda e: e.tensor_tensor(out=o_ap, in0=a_ap, in1=b_ap, op=op), [at, bt], [ot])

    def ts(eng, ot, o_ap, at, a_ap, s1, s2, op0, op1=None, rd=()):
        if op1 is None:
            P.op(eng, lambda e: e.tensor_scalar(out=o_ap, in0=a_ap, scalar1=s1, scalar2=None, op0=op0), [at] + list(rd), [ot])
        else:
            P.op(eng, lambda e: e.tensor_scalar(out=o_ap, in0=a_ap, scalar1=s1, scalar2=s2, op0=op0, op1=op1), [at] + list(rd), [ot])

    def stt(eng, ot, o_ap, at, a_ap, sc, bt, b_ap, op0, op1, rd=()):
        P.op(eng, lambda e: e.scalar_tensor_tensor(out=o_ap, in0=a_ap, scalar=sc, in1=b_ap, op0=op0, op1=op1),
             [at, bt] + list(rd), [ot])

    def memset(eng, t, ap, v):
        P.op(eng, lambda e: e.memset(ap, v), [], [t])

    def recip(ot, o_ap, it, i_ap):
        P.op('dve', lambda e: e.reciprocal(out=o_ap, in_=i_ap), [it], [ot])

    dmaq = ['sp', 'act', 'pool']
    dq = [0]

    dma_pol = {'load': ['sp'], 'store': ['pool']}

    def dma(ot, o_ap, it, i_ap, q=None, **kw):
        if q is None:
            qs = dma_pol['store'] if ot.dram else dma_pol['load']
            q = qs[dq[0] % len(qs)]
            dq[0] += 1
        return P.dma(q, ot, o_ap, it, i_ap, **kw)

    ident = P.sb([128, 128], BF16, "ident"); identf = P.sb([128, 128], F32, "identf")
    ones = P.sb([128, 128], BF16, "ones")
    pv = P.sb([128, 2], F32, "pv")
    nrm = P.sb([128, 5, D], F32, "nrm")
    dma(identf, identf[:], c_ident, c_ident[:])
    dma(pv, pv[:], c_pv, c_pv[:])
    for k in range(5):
        dma(nrm, nrm[:, k, :], norms, norms[k:k + 1, :].partition_broadcast(128))
    cp('dve', ident, ident[:], identf, identf[:])
    memset('pool', ones, ones[:], 1.0)
    eps_t = P.sb([128, 1], F32, "eps_t")
    memset('dve', eps_t, eps_t[:], 1e-6)
    kcmpT = [P.sb([128, 512], BF16, "kcmpT%d" % g) for g in range(2)]
    vcmp = [P.sb([128, 4, 65], BF16, "vcmp%d" % g) for g in range(2)]

    def rmsnorm(xt, x_ap, gain_k, hn, hn_ap, junk, ss, rstd, n=D, eps=1e-6):
        memset('dve', ss, ss[:], 0.0)
        act(junk, junk[:, 0:n], xt, x_ap, AF.Square, accum=ss[:], rd=[ss], wr=[ss])
        act(rstd, rstd[:], ss, ss[:], AF.Ln, scale=1.0 / n, bias=eps_t[:, 0:1], rd=[eps_t])
        act(rstd, rstd[:], rstd, rstd[:], AF.Exp, scale=-0.5)
        stt('dve', hn, hn_ap, xt, x_ap, rstd[:, 0:1], nrm, nrm[:, gain_k, 0:n], ALU.mult, ALU.mult, rd=[rstd])

    if upto >= 1:
        P.push()
        winb = P.sb([128, 8, NCOL], BF16, "winb")
        wst = [P.sb([128, NCOL], F32, "wst%d" % i) for i in range(2)]
        w_in_v = w_in[:].rearrange("(c p) n -> c p n", p=128)
        for c in range(8):
            dma(wst[c % 2], wst[c % 2][:], w_in, w_in_v[c])
            cp(['dve', 'pool'][c % 2], winb, winb[:, c, :], wst[c % 2], wst[c % 2][:])
        gw2t = P.sb([16, 256], F32, "gw2t"); gbt = P.sb([1, 256], F32, "gbt"); onesrow = P.sb([1, 128], F32, "onesrow")
        gnt = P.sb([128, 128], F32, "gnt")
        ucs = P.sb([128, 128], F32, "ucs"); urev = P.sb([128, 128], F32, "urev"); causal = P.sb([128, 128], F32, "causal")
        m16 = P.sb([128, 1], F32, "m16")
        dma(gw2t, gw2t[:], gw2, gw2[:]); dma(gbt, gbt[:], gb, gb[:])
        dma(gnt, gnt[:], gnorm, gnorm[0:1, :].partition_broadcast(128))
        dma(ucs, ucs[:], c_ucs, c_ucs[:]); dma(urev, urev[:], c_urev, c_urev[:]); dma(causal, causal[:], c_causal, c_causal[:])
        memset('dve', onesrow, onesrow[:], 1.0)
        memset('dve', m16, m16[:], -1.0 / 16)
        S = P.sb([128, 2, 128], F32, "S"); Sb = P.sb([128, 2, 128], BF16, "Sb")
        memset('dve', S, S[:], 0.0); memset('pool', Sb, Sb[:], 0.0)

        xt = [P.sb([128, D], F32, "xt%d" % i) for i in range(2)]
        junk = P.sb([128, D], BF16, "junk"); ss = P.sb([128, 1], F32, "ss"); rstd = P.sb([128, 1], F32, "rstd")
        hn = P.sb([128, D], BF16, "hn"); hnT = P.sb([128, 8, 128], BF16, "hnT")
        pt = P.ps([128, 8, 128], BF16, "pt")
        pz = [P.ps([128, 512], F32, "pz%d" % i) for i in range(3)]
        pg1 = P.ps([128, 512], F32, "pg1"); pg2 = P.ps([128, 512], F32, "pg2")
        po = P.ps([128, 512], F32, "po"); ptb = P.ps([128, 8, 128], BF16, "ptb")
        glr = P.sb([128, 16], F32, "glr"); glrT = P.sb([16, 128], F32, "glrT")
        e1 = P.sb([128, 256], F32, "e1"); L = P.sb([128, 256], F32, "L")
        eb = P.sb([128, 256], F32, "eb"); enb = P.sb([128, 256], F32, "enb"); ec = P.sb([128, 256], F32, "ec")
        ebl = P.sb([128, 2], F32, "ebl")
        qk = P.sb([128, 3, 256], BF16, "qk")
        qkT = P.sb([128, 4, 128], BF16, "qkT")
        vv = P.sb([128, 512], BF16, "vv"); sg = P.sb([128, 512], F32, "sg")
        AT = P.sb([128, 128], BF16, "AT")
        ssq = P.sb([128, 4], F32, "ssq"); rs4 = P.sb([128, 4], F32, "rs4"); tmpn = P.sb([128, 128], F32, "tmpn")
        junk2 = P.sb([128, 128], F32, "junk2")
        og = P.sb([128, 512], BF16, "og"); otmp4 = P.sb([128, 512], F32, "otmp4")
        nqs = P.sb([128, 512], BF16, "nqs"); ngs = P.sb([128, 24], F32, "ngs")
        kk = P.sb([128, 4, 128], BF16, "kk"); kkT = P.sb([128, 4, 128], BF16, "kkT")
        vsw = P.sb([128, 2, 128], BF16, "vsw")
        cast_jobs = []
        if upto >= 4:
            stg = [P.sb([128, 4096], F32, "stg%d" % i) for i in range(2)]
            stb = [P.sb([128, 4096], BF16, "stb%d" % i) for i in range(2)]
            for r in range(8):
                for c in range(4):
                    cast_jobs.append((downT, downT[r * 128:(r + 1) * 128, c * 4096:(c + 1) * 4096],
                                      d_downT, d_downT[r * 128:(r + 1) * 128, c * 4096:(c + 1) * 4096]))
            sv = up[:].rearrange("(a p f) d -> a p (f d)", p=128, f=4)
            dv = d_up[:].rearrange("(a p f) d -> a p (f d)", p=128, f=4)
            for a in range(32):
                cast_jobs.append((up, sv[a], d_up, dv[a]))
        cj = [0]

        def cast_job():
            if cj[0] >= len(cast_jobs):
                return
            src_t, s_ap, dst_t, d_ap = cast_jobs[cj[0]]
            a, b_ = stg[cj[0] % 2], stb[cj[0] % 2]
            P.dma('pool', a, a[:], src_t, s_ap)
            for q_ in range(4):
                cp('act', b_, b_[:, q_ * 1024:(q_ + 1) * 1024], a, a[:, q_ * 1024:(q_ + 1) * 1024])
            P.dma('pool', dst_t, d_ap, b_, b_[:])
            cj[0] += 1
        cA, cB, cC, cD, cE, cF = 0, 512, 1024, 1536, 2048, 2560
        hn2_ = [hn, P.sb([128, D], BF16, "hn_b")]; hnT2_ = [hnT, P.sb([128, 8, 128], BF16, "hnT_b")]
        ss2_ = [ss, P.sb([128, 1], F32, "ss_b")]; rstd2_ = [rstd, P.sb([128, 1], F32, "rstd_b")]

        def front(i):
            x_t = xt[i % 2]
            dma(x_t, x_t[:], xb, xb[i * 128:(i + 1) * 128, :])
            rmsnorm(x_t, x_t[:], 0, hn2_[i % 2], hn2_[i % 2][:], junk, ss2_[i % 2], rstd2_[i % 2])
            for c in range(8):
                tr(pt, pt[:, c, :], hn2_[i % 2], hn2_[i % 2][:, c * 128:(c + 1) * 128], ident)
            cp('act', hnT2_[i % 2], hnT2_[i % 2][:], pt, pt[:])
        front(0)
        for i in range(NT):
            hnT = hnT2_[i % 2]

            def zgroup(pz_t, c0, n):
                for c in range(8):
                    mm(pz_t, pz_t[:, 0:n], hnT, hnT[:, c, :], winb, winb[:, c, c0:c0 + n], start=(c == 0), stop=(c == 7))
            zgroup(pz[0], cF, 296)
            cp('act', kk, kk[:, 3, :], pz[0], pz[0][:, 0:128])
            cp('act', vsw, vsw[:, 1, :], pz[0], pz[0][:, 128:256])
            cp('act', glr, glr[:], pz[0], pz[0][:, 280:296])
            act(ngs, ngs[:], pz[0], pz[0][:, 256:280], AF.Exp, scale=-1.0)
            ts('dve', ngs, ngs[:], ngs, ngs[:], 1.0, None, ALU.add)
            recip(ngs, ngs[:], ngs, ngs[:])
            dma(d_ng, d_ng[i * 128:(i + 1) * 128, :], ngs, ngs[:])
            zgroup(pz[1], cE, 512)
            cp('act', kk, kk[:, 0:3, :], pz[1], pz[1][:, 0:384].rearrange("p (a b) -> p a b", a=3))
            cp('dve', vsw, vsw[:, 0, :], pz[1], pz[1][:, 384:512])
            dma(d_vs, d_vs[i * 128:(i + 1) * 128, :], vsw, vsw[:, 0, :])
            dma(d_vw, d_vw[i * 128:(i + 1) * 128, :], vsw, vsw[:, 1, :])
            for a in range(4):
                tr(ptb, ptb[:, a, :], kk, kk[:, a, :], ident)
            cp('act', kkT, kkT[:], ptb, ptb[:, 0:4, :])
            dma(d_kT, d_kT[:, :, i * 128:(i + 1) * 128].rearrange("a p t -> p a t"), kkT, kkT[:])
            zgroup(pz[2], cD, 512)
            P.op('act', lambda e, pzt=pz[2]: e.mul(out=nqs[:], in_=pzt[:], mul=0.125), [pz[2]], [nqs])
            dma(d_nq, d_nq[i * 128:(i + 1) * 128, :], nqs, nqs[:])
            tr(pg1, pg1[0:16, 0:128], glr, glr[:], identf)
            cp('dve', glrT, glrT[:], pg1, pg1[0:16, 0:128])
            mm(pg1, pg1[:, 256:512], glrT, glrT[:], gw2t, gw2t[:], start=True, stop=False)
            mm(pg1, pg1[:, 256:512], onesrow, onesrow[:], gbt, gbt[:], start=False, stop=True)
            zgroup(pz[0], cA, 512)
            zgroup(pz[1], cB, 512)
            zgroup(pz[2], cC, 512)
            act(e1, e1[:], pg1, pg1[:, 256:512], AF.Exp, scale=-1.0)
            act(L, L[:], e1, e1[:], AF.Ln, bias=1.0)
            cp('act', vv, vv[:], pz[1], pz[1][:])
            act(sg, sg[:], pz[2], pz[2][:], AF.Exp, scale=-1.0)
            act(sg, sg[:], sg, sg[:], AF.Ln, bias=1.0)
            act(sg, sg[:], sg, sg[:], AF.Exp, scale=-1.0)
            tt('dve', sg, sg[:], sg, sg[:], pz[2], pz[2][:], ALU.mult)
            tt('pool', sg, sg[:].rearrange("p (h d) -> p h d", h=4), sg, sg[:].rearrange("p (h d) -> p h d", h=4),
               gnt, gnt[:].unsqueeze(1).to_broadcast([128, 4, 128]), ALU.mult)
            mm(pg2, pg2[:, 0:256], ucs, ucs[:], L, L[:])
            mm(pg2, pg2[:, 256:512], urev, urev[:], L, L[:])
            for hp in range(2):
                mm(pg1, pg1[:, hp:hp + 1], L, L[:, hp * 128:(hp + 1) * 128], m16, m16[:])
            act(eb, eb[:], pg2, pg2[:, 0:256], AF.Exp)
            act(enb, enb[:], pg2, pg2[:, 0:256], AF.Exp, scale=-1.0)
            act(ec, ec[:], pg2, pg2[:, 256:512], AF.Exp)
            act(ebl, ebl[:], pg1, pg1[:, 0:2], AF.Exp)
            stt('dve', qk, qk[:, 0, :], pz[0], pz[0][:, 0:256], 0.125, eb, eb[:], ALU.mult, ALU.mult)
            tt('dve', qk, qk[:, 1, :], pz[0], pz[0][:, 256:512], enb, enb[:], ALU.mult)
            tt('dve', qk, qk[:, 2, :], pz[0], pz[0][:, 256:512], ec, ec[:], ALU.mult)
            for a in range(4):
                tr(ptb, ptb[:, 4 + a, :], qk, qk[:, a // 2, (a % 2) * 128:(a % 2 + 1) * 128], ident)
            cp('act', qkT, qkT[:], ptb, ptb[:, 4:8, :])
            if i + 1 < NT:
                front(i + 1)
            memset('dve', ssq, ssq[:], 0.0)
            for h in range(4):
                hp, hh = h // 2, h % 2
                pr = slice(hh * 64, hh * 64 + 64)
                mm(pg2, pg2[:, 0:128], qkT, qkT[pr, 2 + hp, :], qkT, qkT[pr, hp, :])
                tt('dve', AT, AT[:], pg2, pg2[:, 0:128], causal, causal[:], ALU.mult)
                o_ap = po[:, h * 128:(h + 1) * 128]
                mm(po, o_ap, AT, AT[:], vv, vv[:, h * 128:(h + 1) * 128], start=True, stop=False)
                mm(po, o_ap, qkT, qkT[pr, hp, :], Sb, Sb[pr, hp, :], start=False, stop=True)
                mm(pg2, pg2[:, 128:256], qk, qk[:, 2, hp * 128:(hp + 1) * 128], vv, vv[:, h * 128:(h + 1) * 128])
                stt('dve', Sb, Sb[pr, hp, :], S, S[pr, hp, :], ebl[pr, hp:hp + 1], pg2, pg2[pr, 128:256], ALU.mult, ALU.add, rd=[ebl])
                stt('dve', S, S[pr, hp, :], S, S[pr, hp, :], ebl[pr, hp:hp + 1], pg2, pg2[pr, 128:256], ALU.mult, ALU.add, rd=[ebl])
                act(junk2, junk2[:], po, o_ap, AF.Square, accum=ssq[:, h:h + 1], rd=[ssq], wr=[ssq])
            act(rs4, rs4[:], ssq, ssq[:], AF.Ln, scale=1.0 / 128, bias=eps_t[:, 0:1], rd=[eps_t])
            act(rs4, rs4[:], rs4, rs4[:], AF.Exp, scale=-0.5)
            tt('dve', otmp4, otmp4[:], po, po[:], sg, sg[:], ALU.mult)
            tt('dve', og, og[:].rearrange("p (h d) -> p h d", h=4), otmp4, otmp4[:].rearrange("p (h d) -> p h d", h=4),
               rs4, rs4[:].unsqueeze(2).to_broadcast([128, 4, 128]), ALU.mult)
            dma(d_ogla, d_ogla[i * 128:(i + 1) * 128, :], og, og[:])
            for _ in range((len(cast_jobs) + NT - 1) // NT):
                cast_job()
        while cj[0] < len(cast_jobs):
            cast_job()
        P.pop()

        P.push()
        w1f = P.sb([64, 32, 128], F32, "w1f"); w1b = P.sb([64, 32, 128], BF16, "w1b")
        posf = P.sb([64, 32], F32, "posf"); posb = P.sb([64, 32], BF16, "posb")
        w2f = P.sb([128, 64], F32, "w2f"); w2b = P.sb([128, 64], BF16, "w2b")
        kTg = P.sb([64, T], BF16, "kTg")
        ph = P.ps([128, 512], F32, "ph"); pb = P.ps([128, 512], F32, "pb"); pc = P.ps([128, 512], F32, "pc")
        bias_h = P.sb([128, 1], F32, "bias_h")
        H = P.sb([128, 512], BF16, "H")
        for g in range(2):
            memset('dve', kcmpT[g], kcmpT[g][:], 0.0)
            memset('dve', vcmp[g], vcmp[g][:], 0.0)
            memset('dve', vcmp[g], vcmp[g][:, :, 64:65], 1.0)
        for kv in range(2):
            dma(w1f, w1f[:], cw1, cw1[kv]); dma(posf, posf[:], cpos, cpos[kv]); dma(w2f, w2f[:], cw2, cw2[kv])
            cp('dve', w1b, w1b[:], w1f, w1f[:]); cp('dve', posb, posb[:], posf, posf[:]); cp('dve', w2b, w2b[:], w2f, w2f[:])
            for l in range(32):
                mm(pb, pb[:, 0:1], w1b, w1b[:, l, :], posb, posb[:, l:l + 1], start=(l == 0), stop=(l == 31))
            cp('dve', bias_h, bias_h[:], pb, pb[:, 0:1])
            for g in range(2):
                dma(kTg, kTg[:], d_kT, d_kT[kv, g * 64:(g + 1) * 64, :])
                for l in range(32):
                    mm(ph, ph[:, 0:NCMP], w1b, w1b[:, l, :], kTg, kTg[:, l:l + 16 * (NCMP - 1) + 1:16],
                       start=(l == 0), stop=(l == 31))
                memset('dve', H, H[:], 0.0)
                act(H, H[:, 0:NCMP], ph, ph[:, 0:NCMP], AF.Gelu_apprx_tanh, bias=bias_h[:, 0:1], rd=[bias_h])
                if kv == 0:
                    mm(pc, pc[0:64, 0:NCMP], w2b, w2b[:], H, H[:, 0:NCMP])
                    cp('act', kcmpT[g], kcmpT[g][0:64, 0:NCMP], pc, pc[0:64, 0:NCMP])
                    if debug:
                        dma(d_cmp, d_cmp[g], kcmpT[g], kcmpT[g][0:64, :])
                else:
                    for c in range(NCC):
                        mm(pc, pc[:, c * 64:(c + 1) * 64], H, H[:, c * 128:(c + 1) * 128], w2b, w2b[:])
                    cp('act', vcmp[g], vcmp[g][:, 0:NCC, 0:64], pc, pc[:, 0:NCC * 64].rearrange("p (c d) -> p c d", d=64))
        P.pop()

    if upto >= 2:
        P.push()
        dma_pol['load'] = ['sp']; dma_pol['store'] = ['sp']
        selu = P.sb([128, 2, 128], BF16, "selu"); seluf = P.sb([128, 2, 128], F32, "seluf")
        dma(seluf, seluf[:], c_selu, c_selu[:].rearrange("a p t -> p a t"))
        cp('dve', selu, selu[:], seluf, seluf[:])
        exm = P.sb([128, T], BF16, "exm")
        dma(exm, exm[:], c_ex, c_ex[:])
        wimpf = P.sb([128, NCC, 128], F32, "wimpf"); wimp = P.sb([128, NCC, 128], BF16, "wimp")
        dma(wimpf, wimpf[:], c_wimp, c_wimp[:].rearrange("c p s -> p c s"))
        cp('dve', wimp, wimp[:], wimpf, wimpf[:])
        ksT = P.sb([128, T], BF16, "ksT"); kwT = P.sb([128, T], BF16, "kwT")
        memset('dve', ksT, ksT[64:128, :], 0.0)
        memset('pool', kwT, kwT[64:128, :], 0.0)
        memset('dve', ksT, ksT[64:65, :], 1.0)
        farf = P.sb([128, 512], F32, "farf")
        vs = P.sb([128, NT, 65], BF16, "vs"); vw = P.sb([128, NT, 65], BF16, "vw")
        selb = P.sb([128, 15, 512], BF16, "selb"); winb2 = P.sb([128, 6, 512], BF16, "winb2")
        bst = [P.sb([128, 512], F32, "bst%d" % i) for i in range(2)]
        cbt = [P.sb([128, 512], BF16, "cbt%d" % i) for i in range(2)]
        qrows_ = [P.sb([128, 2, 256], BF16, "qrows%d" % i) for i in range(2)]; grows_ = [P.sb([128, 2, 24], F32, "grows%d" % i) for i in range(2)]
        gown_ = [P.sb([128, 12], F32, "gown%d" % i) for i in range(2)]; gtmp = P.sb([128, 12], F32, "gtmp")
        qT_ = [P.sb([128, 4, 128], BF16, "qT%d" % i) for i in range(2)]
        for i_ in range(2):
            memset('dve', qT_[i_], qT_[i_][64:128, :, :], 0.0)
        Ec = [P.sb([128, 4, 128], BF16, "Ec%d" % i) for i in range(4)]
        Eb = [P.sb([128, 4, 128], BF16, "Eb%d" % i) for i in range(3)]
        m12_ = [P.sb([128, 2, 128], F32, "m12_%d" % i) for i in range(2)]
        imp = P.sb([128, 128], F32, "imp"); imp2 = P.sb([128, 128], F32, "imp2"); m8 = P.sb([128, 16], F32, "m8")
        mk = P.sb([128, 128], BF16, "mk"); maskT4 = P.sb([128, 4, 128], BF16, "maskT4")
        rden = P.sb([128, 4], F32, "rden"); coef = P.sb([128, 4], F32, "coef")
        oacc = P.sb([128, 4, 64], F32, "oacc"); otmp = P.sb([128, 4, 64], F32, "otmp"); onsa = P.sb([128, 256], BF16, "onsa")
        pq = P.ps([64, 4, 128], F32, "pq")
        psc = [P.ps([128, 4, 128], F32, "psc%d" % i) for i in range(2)]
        pn = P.ps([128, 4, 128], F32, "pn")
        pnT = P.ps([65, 4, 128], F32, "pnT")
        pnT2 = P.ps([65, 4, 128], F32, "pnT2")
        numTs = P.sb([65, 4, 128], F32, "numTs")
        pimp = P.ps([128, 4, 128], F32, "pimp")
        pmt = P.ps([128, 128], BF16, "pmt")
        for g in range(2):
            dma(ksT, ksT[0:64, :], d_kT, d_kT[2, g * 64:(g + 1) * 64, :])
            dma(farf, farf[:], c_far, c_far[g])
            for i_ in range(2):
                cp('dve', qT_[i_], qT_[i_][64:65, :, :], farf, farf[64:65, :].rearrange("p (h q) -> p h q", h=4))
            dma(kwT, kwT[0:64, :], d_kT, d_kT[3, g * 64:(g + 1) * 64, :])
            for n0 in range(0, NT, 16):
                n1 = min(NT, n0 + 16)
                dma(vs, vs[:, n0:n1, 0:64], d_vs, d_vs[n0 * 128:n1 * 128, g * 64:(g + 1) * 64].rearrange("(n p) c -> p n c", p=128))
                dma(vw, vw[:, n0:n1, 0:64], d_vw, d_vw[n0 * 128:n1 * 128, g * 64:(g + 1) * 64].rearrange("(n p) c -> p n c", p=128))
            memset('dve', vs, vs[:, :, 64:65], 1.0)
            memset('dve', vw, vw[:, :, 64:65], 1.0)
            k = 0
            for m in range(15):
                dma(bst[k % 2], bst[k % 2][:], c_selb, c_selb[g, :, m, :])
                tt('pool', selb, selb[:, m, :], bst[k % 2], bst[k % 2][:], farf, farf[:], ALU.subtract); k += 1
            for m in range(6):
                dma(bst[k % 2], bst[k % 2][:], c_winb, c_winb[g, :, m, :])
                cp('pool', winb2, winb2[:, m, :], bst[k % 2], bst[k % 2][:]); k += 1
            ne = 0
            ne_ = [0]
            for jj in range(NO):
                qrows, grows, gown, qT, m12 = qrows_[jj % 2], grows_[jj % 2], gown_[jj % 2], qT_[jj % 2], m12_[jj % 2]
                dma(qrows, qrows[:], d_nq, d_nq[jj * 256:(jj + 1) * 256, g * 256:(g + 1) * 256].rearrange("(u p) c -> p u c", p=128))
                dma(grows, grows[:], d_ng, d_ng[jj * 256:(jj + 1) * 256, :].rearrange("(u p) c -> p u c", p=128))
                dma(m12, m12[:], c_m12, c_m12[jj])
                for h in range(4):
                    for u in range(2):
                        mm(pq, pq[:, h, :], qrows, qrows[:, u, h * 64:(h + 1) * 64], selu, selu[:, u, :], start=(u == 0), stop=(u == 1))
                cp('act', qT, qT[0:64], pq, pq[:])
                ts('dve', gtmp, gtmp[:], grows, grows[:, 0, g * 12:(g + 1) * 12], pv[:, 0:1], None, ALU.mult, rd=[pv])
                stt('dve', gown, gown[:], grows, grows[:, 1, g * 12:(g + 1) * 12], pv[:, 1:2], gtmp, gtmp[:], ALU.mult, ALU.add, rd=[pv])
                gv3 = gown[:].rearrange("p (h k) -> p h k", k=3)
                qT_all = qT[:].rearrange("p h q -> p (h q)")
                qT_aug = qT_all

                def finish_branch(pn, br, first):
                    ts('dve', rden, rden[:], pn, pn[:, :, 64], 1e-30, None, ALU.max)
                    recip(rden, rden[:], rden, rden[:])
                    tt('dve', coef, coef[:], rden, rden[:], gown, gv3[:, :, br], ALU.mult)
                    dst = oacc if first else otmp
                    tt('dve', dst, dst[:], pn, pn[:, :, 0:64], coef, coef[:].unsqueeze(2).to_broadcast([128, 4, 64]), ALU.mult)
                    if not first:
                        tt('pool', oacc, oacc[:], oacc, oacc[:], otmp, otmp[:], ALU.add)
                    if debug:
                        dma(d_dbg, d_dbg[br, jj * 128:(jj + 1) * 128, g * 256:(g + 1) * 256], oacc, oacc[:].rearrange("p h d -> p (h d)"))

                def back_T(src):
                    cp('act', numTs, numTs[:], src, src[:])
                    for h in range(4):
                        P.op('pe', lambda e, h=h: e.transpose(out=pn[:, h, 0:65], in_=numTs[:, h, :], identity=identf[0:65, 0:65]), [numTs, identf], [pn])

                ncc = min(NCC, cmp_nchunks(jj))
                for c in range(ncc):
                    pi = pair_off[jj] + c
                    dma(bst[k % 2], bst[k % 2][:], c_cmpb, c_cmpb[g, pi])
                    cp('pool', cbt[k % 2], cbt[k % 2][:], bst[k % 2], bst[k % 2][:])
                    sc = psc[ne % 2]; ne += 1
                    sc_all = sc[:].rearrange("p h q -> p (h q)")
                    mm(sc, sc_all, kcmpT[g], kcmpT[g][:, c * 128:(c + 1) * 128], qT, qT_all, start=True, stop=False)
                    mm(sc, sc_all, ident, ident[:], cbt[k % 2], cbt[k % 2][:], start=False, stop=True)
                    k += 1
                    act(Ec[c], Ec[c][:], sc, sc[:], AF.Exp)
                for h in range(4):
                    for c in range(ncc):
                        mm(pn, pn[:, h, 0:65], Ec[c], Ec[c][:, h, :], vcmp[g], vcmp[g][:, c, :], start=(c == 0), stop=(c == ncc - 1))
                    for c in range(ncc):
                        mm(pimp, pimp[:, h, :], Ec[c], Ec[c][:, h, :], wimp, wimp[:, c, :], start=(c == 0), stop=(c == ncc - 1))
                nk = 2 * jj + 2
                k0 = max(0, 2 * jj - 4)
                bufs = {}

                def score(kind, kc, n):
                    sc = psc[ne_[0] % 2]; E = Eb[ne_[0] % 3]; ne_[0] += 1
                    bufs[n] = E
                    sc_all = sc[:].rearrange("p h q -> p (h q)")
                    if kind == 's':
                        m = 2 * jj + 1 - kc
                        mm(sc, sc_all, ksT, ksT[:, kc * 128:(kc + 1) * 128], qT, qT_aug, start=True, stop=False)
                        if m < 14:
                            mm(sc, sc_all, ident, ident[:], selb, selb[:, m, :], start=False, stop=False)
                        mm(sc, sc_all, exm, exm[:, kc * 128:(kc + 1) * 128], maskT4, mT_all, start=False, stop=True)
                    else:
                        m = 2 * jj + 1 - kc
                        mm(sc, sc_all, kwT, kwT[:, kc * 128:(kc + 1) * 128], qT, qT_all, start=True, stop=False)
                        mm(sc, sc_all, ident, ident[:], winb2, winb2[:, m, :], start=False, stop=True)
                    act(E, E[:], sc, sc[:], AF.Exp)

                def pvs(kind, kc, n):
                    E = bufs.pop(n)
                    if kind == 's':
                        mm(pnT, pnT[:].rearrange("p h q -> p (h q)"), vs, vs[:, kc, :], E, E[:].rearrange("p h q -> p (h q)"), start=(kc == 0), stop=(kc == nk - 1))
                    else:
                        mm(pnT2, pnT2[:].rearrange("p h q -> p (h q)"), vw, vw[:, kc, :], E, E[:].rearrange("p h q -> p (h q)"), start=(kc == k0), stop=(kc == nk - 1))

                def run_items(items):
                    NI = len(items)
                    for n in range(NI + 1):
                        if n < NI:
                            score(items[n][0], items[n][1], n)
                        if n >= 1:
                            pvs(items[n - 1][0], items[n - 1][1], n - 1)
                mT_all = maskT4[:].rearrange("p h q -> p (h q)")
                run_items([('w', kc) for kc in range(k0, nk)])
                finish_branch(pn, 0, True)
                for h in range(4):
                    if h == 0:
                        ts('dve', imp, imp[:], pimp, pimp[:, 0, :], rden[:, 0:1], None, ALU.mult, rd=[rden])
                    else:
                        stt('dve', imp, imp[:], pimp, pimp[:, h, :], rden[:, h:h + 1], imp, imp[:], ALU.mult, ALU.add, rd=[rden])
                tt('dve', imp, imp[:], imp, imp[:], m12, m12[:, 0, :], ALU.mult)
                tt('dve', imp, imp[:], imp, imp[:], m12, m12[:, 1, :], ALU.add)
                if debug:
                    dma(d_imp, d_imp[g, jj * 128:(jj + 1) * 128, :], imp, imp[:])
                P.op('dve', lambda e: e.max(out=m8[:, 0:8], in_=imp[:]), [imp], [m8])
                P.op('dve', lambda e: e.match_replace(out=imp2[:], in_to_replace=m8[:, 0:8], in_values=imp[:], imm_value=-1e30), [imp, m8], [imp2])
                P.op('dve', lambda e: e.max(out=m8[:, 8:16], in_=imp2[:]), [imp2], [m8])
                ts('dve', mk, mk[:], imp, imp[:], m8[:, 15:16], 1.0, ALU.is_ge, ALU.subtract, rd=[m8])
                tr(pmt, pmt[:], mk, mk[:], ident)
                P.op('act', lambda e: e.mul(out=maskT4[:], in_=pmt[:].unsqueeze(1).to_broadcast([128, 4, 128]), mul=30000.0), [pmt], [maskT4])
                run_items([('s', kc) for kc in range(nk)])
                back_T(pnT)
                finish_branch(pn, 1, False)
                back_T(pnT2)
                finish_branch(pn, 2, False)
                cp('act', onsa, onsa[:], oacc, oacc[:].rearrange("p h d -> p (h d)"))
                dma(d_omix, d_omix[jj * 128:(jj + 1) * 128, 512 + g * 256:512 + (g + 1) * 256], onsa, onsa[:])
        P.pop()

    if upto >= 3:
        P.push()
        dma_pol['load'] = ['sp']; dma_pol['store'] = ['pool']
        woutb = P.sb([128, 8, D], BF16, "woutb"); wqb = P.sb([128, 8, D], BF16, "wqb"); wob = P.sb([128, 8, D], BF16, "wob")
        pwqb = P.sb([128, 8, D], BF16, "pwqb")
        skb = P.sb([128, 8, 256], BF16, "skb")
        kTm = P.sb([128, 8, 256], BF16, "kTm")
        vm = P.sb([128, 2, 4, 257], BF16, "vm")
        xt = [P.sb([128, D], F32, "x3_%d" % i) for i in range(2)]
        junk = P.sb([128, D], BF16, "junk3"); ss = P.sb([128, 1], F32, "ss3"); rstd = P.sb([128, 1], F32, "rstd3")
        hn = P.sb([128, D], BF16, "hn3"); hnT = P.sb([128, 8, 128], BF16, "hnT3")
        pt = P.ps([128, 8, 128], BF16, "pt3")
        pa = [P.ps([128, 512], F32, "pa%d" % i) for i in range(4)]
        pxa = P.ps([128, 2, 512], F32, "pxa")
        P.push()
        wst = [P.sb([128, D], F32, "wst3_%d" % i) for i in range(2)]
        wtmp = P.sb([128, 8, D], BF16, "wtmp"); skf = P.sb([128, 8, 256], F32, "skf")
        memT = P.sb([128, 8, 256], BF16, "memT")
        k = [0]

        def load_w(dst, src_t, src_ap3):
            for c in range(8):
                a = wst[k[0] % 2]
                dma(a, a[:], src_t, src_ap3[c])
                cp(['dve', 'pool'][k[0] % 2], dst, dst[:, c, :], a, a[:]); k[0] += 1
        load_w(woutb, w_out, w_out[:].rearrange("(c p) n -> c p n", p=128))
        load_w(wqb, xa_w, xa_w[0].rearrange("(c p) n -> c p n", p=128))
        load_w(wob, xa_w, xa_w[3].rearrange("(c p) n -> c p n", p=128))
        load_w(pwqb, pwq, pwq[:].rearrange("(c p) n -> c p n", p=128))
        dma(skf, skf[:], skd, skd[:].rearrange("c p n -> p c n"))
        cp('dve', skb, skb[:], skf, skf[:])
        memset('dve', vm, vm[:, :, :, 256:257], 1.0)
        for mc in range(2):
            x_t = xt[mc % 2]
            dma(x_t, x_t[:], memb, memb[mc * 128:(mc + 1) * 128, :])
            rmsnorm(x_t, x_t[:], 2, hn, hn[:], junk, ss, rstd)
            for c in range(8):
                tr(pt, pt[:, c, :], hn, hn[:, c * 128:(c + 1) * 128], ident)
            cp('act', memT, memT[:, :, mc * 128:(mc + 1) * 128], pt, pt[:])
        load_w(wtmp, xa_w, xa_w[1].rearrange("(c p) n -> c p n", p=128))
        for oc in range(8):
            for c in range(8):
                mm(pa[0], pa[0][:, 0:256], wtmp, wtmp[:, c, oc * 128:(oc + 1) * 128], memT, memT[:, c, :], start=(c == 0), stop=(c == 7))
            cp('act', kTm, kTm[:, oc, :], pa[0], pa[0][:, 0:256])
        load_w(wtmp, xa_w, xa_w[2].rearrange("(c p) n -> c p n", p=128))
        for mc in range(2):
            for half in range(2):
                for c in range(8):
                    mm(pa[half], pa[half][:], memT, memT[:, c, mc * 128:(mc + 1) * 128], wtmp, wtmp[:, c, half * 512:(half + 1) * 512], start=(c == 0), stop=(c == 7))
                cp('act', vm, vm[:, mc, half * 2:half * 2 + 2, 0:256], pa[half], pa[half][:].rearrange("p (h d) -> p h d", d=256))
        P.pop()
        og2 = P.sb([128, 2, 512], BF16, "og2"); omx = P.sb([128, D], BF16, "omx"); otm = P.sb([128, 512], F32, "otm")
        h1 = P.sb([128, D], F32, "h1"); qTx = P.sb([128, 8, 128], BF16, "qTx")
        Ex = P.sb([128, 2, 4, 128], BF16, "Ex"); rdx = P.sb([128, 4], F32, "rdx")
        oxa = P.sb([128, D], BF16, "oxa")
        hn3T = P.sb([128, 8, 128], BF16, "hn3Ts"); qpT = P.sb([128, 8, 128], BF16, "qpT")
        sc_ = [P.sb([128, 16, 128], F32, "scr%d" % i) for i in range(2)]; ab_ = [P.sb([128, 16, 128], F32, "ab%d" % i) for i in range(2)]
        negm = P.sb([128, 16], F32, "negm"); t16 = P.sb([128, 16, 16], F32, "t16"); scr2_ = [P.sb([128, 128], F32, "scr2_%d" % i) for i in range(4)]
        candall = P.sb([128, 8, 256], F32, "candall"); cand2_ = [P.sb([128, 256], F32, "cand2_%d" % i) for i in range(4)]; c16 = P.sb([128, 8, 16], F32, "c16")
        route = P.sb([128, 16], F32, "route"); zs = P.sb([128, 8], F32, "zs")
        memset('dve', route, route[:], 0.0)
        def Xgen(jj):
            sc = sc_[jj % 2]
            x_t = xt[jj % 2]
            dma(x_t, x_t[:], xo, xo[jj * 128:(jj + 1) * 128, :])
            dma(og2, og2[:], d_ogla, d_ogla[jj * 256:(jj + 1) * 256, :].rearrange("(u p) c -> p u c", p=128))
            dma(omx, omx[:, 512:1024], d_omix, d_omix[jj * 128:(jj + 1) * 128, 512:1024])
            ts('dve', otm, otm[:], og2, og2[:, 0, :], pv[:, 0:1], None, ALU.mult, rd=[pv])
            stt('dve', omx, omx[:, 0:512], og2, og2[:, 1, :], pv[:, 1:2], otm, otm[:], ALU.mult, ALU.add, rd=[pv])
            for c in range(8):
                tr(pt, pt[:, c, :], omx, omx[:, c * 128:(c + 1) * 128], ident)
            cp('act', hnT, hnT[:], pt, pt[:])
            for half in range(2):
                for c in range(8):
                    mm(pa[half], pa[half][:], hnT, hnT[:, c, :], woutb, woutb[:, c, half * 512:(half + 1) * 512], start=(c == 0), stop=(c == 7))
                tt('dve', h1, h1[:, half * 512:(half + 1) * 512], pa[half], pa[half][:], x_t, x_t[:, half * 512:(half + 1) * 512], ALU.add)
            yield
            rmsnorm(h1, h1[:], 1, hn, hn[:], junk, ss, rstd)
            for c in range(8):
                tr(pt, pt[:, c, :], hn, hn[:, c * 128:(c + 1) * 128], ident)
            cp('act', hnT, hnT[:], pt, pt[:])
            for oc in range(8):
                pq_ = pa[2 + oc % 2]
                for c in range(8):
                    mm(pq_, pq_[:, 0:128], wqb, wqb[:, c, oc * 128:(oc + 1) * 128], hnT, hnT[:, c, :], start=(c == 0), stop=(c == 7))
                cp(['act', 'dve'][oc % 2], qTx, qTx[:, oc, :], pq_, pq_[:, 0:128])
                yield
            yield
            for mc in range(2):
                for h in range(4):
                    for dc in range(2):
                        mm(pxa, pxa[:, mc, h * 128:(h + 1) * 128], kTm, kTm[:, h * 2 + dc, mc * 128:(mc + 1) * 128], qTx, qTx[:, h * 2 + dc, :], start=(dc == 0), stop=(dc == 1))
            act(Ex, Ex[:].rearrange("p a h q -> p a (h q)"), pxa, pxa[:], AF.Exp, scale=1.0 / 16)
            for h in range(4):
                o_ap = pxa[:, h // 2, (h % 2) * 256:(h % 2) * 256 + 256]
                for mc in range(2):
                    mm(pa[h % 2], pa[h % 2][:, 0:257], Ex, Ex[:, mc, h, :], vm, vm[:, mc, h, :], start=(mc == 0), stop=(mc == 1))
                ts('dve', rdx, rdx[:, h:h + 1], pa[h % 2], pa[h % 2][:, 256:257], 1e-30, None, ALU.max)
                recip(rdx, rdx[:, h:h + 1], rdx, rdx[:, h:h + 1])
                ts('dve', oxa, oxa[:, h * 256:(h + 1) * 256], pa[h % 2], pa[h % 2][:, 0:256], rdx[:, h:h + 1], None, ALU.mult, rd=[rdx])
            yield
            for c in range(8):
                tr(pt, pt[:, c, :], oxa, oxa[:, c * 128:(c + 1) * 128], ident)
            cp('act', hnT, hnT[:], pt, pt[:])
            for half in range(2):
                for c in range(8):
                    mm(pa[half], pa[half][:], hnT, hnT[:, c, :], wob, wob[:, c, half * 512:(half + 1) * 512], start=(c == 0), stop=(c == 7))
                tt('dve', h1, h1[:, half * 512:(half + 1) * 512], pa[half], pa[half][:], h1, h1[:, half * 512:(half + 1) * 512], ALU.add)
            dma(d_h, d_h[jj * 128:(jj + 1) * 128, :], h1, h1[:])
            yield
            rmsnorm(h1, h1[:], 3, hn, hn[:], junk, ss, rstd)
            for c in range(8):
                tr(pt, pt[:, c, :], hn, hn[:, c * 128:(c + 1) * 128], ident)
            cp('act', hn3T, hn3T[:], pt, pt[:])
            dma(d_hn3T, d_hn3T[:, :, jj * 128:(jj + 1) * 128], hn3T, hn3T[:])
            for oc in range(8):
                pq_ = pa[2 + oc % 2]
                for c in range(8):
                    mm(pq_, pq_[:, 0:128], pwqb, pwqb[:, c, oc * 128:(oc + 1) * 128], hn3T, hn3T[:, c, :], start=(c == 0), stop=(c == 7))
                cp(['act', 'dve'][oc % 2], qpT, qpT[:, oc, :], pq_, pq_[:, 0:128])
                yield
            yield
            for oc in range(8):
                pq_ = pa[oc % 2]
                mm(pq_, pq_[:, 0:256], qpT, qpT[:, oc, :], skb, skb[:, oc, :])
                cp(['act', 'dve'][oc % 2], sc, sc[:, 2 * oc:2 * oc + 2, :], pq_, pq_[:, 0:256].rearrange("p (a k) -> p a k", a=2))
            yield

        def Rgen(jj):
            sc = sc_[jj % 2]; ab = ab_[jj % 2]
            P.op('dve', lambda e: e.tensor_reduce(out=negm[:], in_=sc[:], axis=AX.X, op=ALU.max), [sc], [negm])
            ts('dve', negm, negm[:], negm, negm[:], -1.0, None, ALU.mult)
            for r in range(16):
                act(ab, ab[:, r, :], sc, sc[:, r, :], AF.Exp, bias=negm[:, r:r + 1], rd=[negm])
            yield
            for r0 in range(0, 16, 4):
                for r in range(r0, r0 + 4):
                    P.op('dve', lambda e, r=r: e.max(out=t16[:, r, 0:8], in_=ab[:, r, :]), [ab], [t16])
                for r in range(r0, r0 + 4):
                    P.op('dve', lambda e, r=r: e.match_replace(out=scr2_[r % 4][:], in_to_replace=t16[:, r, 0:8], in_values=ab[:, r, :], imm_value=-1.0), [ab, t16], [scr2_[r % 4]])
                for r in range(r0, r0 + 4):
                    P.op('dve', lambda e, r=r: e.max(out=t16[:, r, 8:16], in_=scr2_[r % 4][:]), [scr2_[r % 4]], [t16])
                yield
            t16v = t16[:].rearrange("p (h a) k -> p h a k", a=2)
            abv = ab[:].rearrange("p (h a) k -> p h a k", a=2)

            def cand_top16():
                yield
                tt('dve', candall, candall[:].rearrange("p h (a b) -> p h a b", a=16),
                   t16, t16v[:, :, 0, :].unsqueeze(3).to_broadcast([128, 8, 16, 16]),
                   t16, t16v[:, :, 1, :].unsqueeze(2).to_broadcast([128, 8, 16, 16]), ALU.mult)
                for h0 in range(0, 8, 4):
                    for h in range(h0, h0 + 4):
                        P.op('dve', lambda e, h=h: e.max(out=c16[:, h, 0:8], in_=candall[:, h, :]), [candall], [c16])
                    for h in range(h0, h0 + 4):
                        P.op('dve', lambda e, h=h: e.match_replace(out=cand2_[h % 4][:], in_to_replace=c16[:, h, 0:8], in_values=candall[:, h, :], imm_value=-1.0), [candall, c16], [cand2_[h % 4]])
                    for h in range(h0, h0 + 4):
                        P.op('dve', lambda e, h=h: e.max(out=c16[:, h, 8:16], in_=cand2_[h % 4][:]), [cand2_[h % 4]], [c16])
                    yield
            yield from cand_top16()
            P.op('dve', lambda e: e.tensor_reduce(out=zs[:], in_=c16[:], axis=AX.X, op=ALU.add), [c16], [zs])
            recip(zs, zs[:], zs, zs[:])
            tt('dve', ab, abv[:, :, 1, :], ab, abv[:, :, 1, :], zs, zs[:].unsqueeze(2).to_broadcast([128, 8, 128]), ALU.mult)
            stt('dve', route, route[:, 0:8], c16, c16[:, :, 15], 1.0 - 1e-6, zs, zs[:], ALU.mult, ALU.mult)
            dma(d_route, d_route[jj * 128:(jj + 1) * 128, 0:2048], ab, ab[:].rearrange("p r k -> p (r k)"))
            dma(d_route, d_route[jj * 128:(jj + 1) * 128, 2048:2064], route, route[:])
            yield

        def drain(g):
            for _ in g:
                pass
        drain(Xgen(0))
        for jj in range(NO):
            gr = Rgen(jj)
            gx = Xgen(jj + 1) if jj + 1 < NO else iter(())
            ra = xa_ = True
            while ra or xa_:
                if xa_:
                    xa_ = next(gx, 'END') != 'END'
                if ra:
                    ra = next(gr, 'END') != 'END'
        P.pop()

    if upto >= 4:
        P.push()
        dma_pol['load'] = ['sp', 'pool']; dma_pol['store'] = ['sp']
        TG = 2
        IC = 16
        NCH = 128 // IC
        ACT_HEADS = (1, 3, 4, 6, 7)
        hT = P.sb([128, 8, TG * 128], BF16, "hT")
        ab = [P.sb([128, 16, 128], F32, "ab4_%d" % u) for u in range(TG)]
        rt = [P.sb([128, 16], F32, "rt%d" % u) for u in range(TG)]
        Wc = [[P.sb([128, IC * 128], BF16, "Wc%d_%d" % (i, u)) for u in range(TG)] for i in range(2)]
        et = [P.sb([128, IC, 128], F32, "et%d" % i) for i in range(4)]
        mt = [P.sb([128, IC, 128], BF16, "mt%d" % i) for i in range(2)]
        dnb = [P.sb([128, 8, 512], BF16, "dnb%d" % i) for i in range(3)]
        upb = [P.sb([128, 4, D], BF16, "upb%d" % i) for i in range(3)]
        Gs = [P.sb([128, 512], BF16, "G%d" % i) for i in range(2)]
        GT = [P.sb([128, 4, 128], BF16, "GT%d" % i) for i in range(2)]
        py = [P.ps([128, 2, 512], F32, "py%d" % u) for u in range(TG)]
        pd = [P.ps([128, 512], F32, "pd%d" % i) for i in range(2)]
        ptg = [P.ps([128, 8, 128], BF16, "ptg%d" % i) for i in range(2)]
        h2 = P.sb([128, D], F32, "h2"); yo = P.sb([128, D], F32, "yo")
        junk = P.sb([128, D], BF16, "junk4"); ss = P.sb([128, 1], F32, "ss4"); rstd = P.sb([128, 1], F32, "rstd4")
        qn = [0]
        for tg in range(NO // TG):
            dma(hT, hT[:], d_hn3T, d_hn3T[:, :, tg * TG * 128:(tg + 1) * TG * 128])
            for u in range(TG):
                j = tg * TG + u
                dma(ab[u], ab[u][:].rearrange("p r k -> p (r k)"), d_route, d_route[j * 128:(j + 1) * 128, 0:2048])
                dma(rt[u], rt[u][:], d_route, d_route[j * 128:(j + 1) * 128, 2048:2064])

            def wgen(c):
                its = [(u, h) for u in range(TG) for h in range(8)]
                K = len(its)
                eb_ = {}; mb_ = {}

                def E_(n):
                    u, h = its[n]
                    e_ = et[qn[0] % 4]; qn[0] += 1
                    eb_[n] = e_
                    if h in ACT_HEADS:
                        for i_ in range(IC):
                            P.op('act', lambda e, e_=e_, i_=i_, u=u, h=h: e.activation(out=e_[:, i_, :], in_=ab[u][:, 2 * h + 1, :], func=AF.Copy,
                                                                                  scale=ab[u][:, 2 * h, c * IC + i_:c * IC + i_ + 1]),
                                 [ab[u]], [e_] if i_ in (0, IC - 1) else [])
                    else:
                        tt('dve', e_, e_[:], ab[u], ab[u][:, 2 * h, c * IC:(c + 1) * IC].unsqueeze(2).to_broadcast([128, IC, 128]),
                           ab[u], ab[u][:, 2 * h + 1, :].unsqueeze(1).to_broadcast([128, IC, 128]), ALU.mult)

                def S_(n):
                    u, h = its[n]
                    e_ = eb_.pop(n)
                    w_ap = Wc[c % 2][u][:].rearrange("p (a b) -> p a b", a=IC)
                    if h == 0:
                        stt('dve', Wc[c % 2][u], w_ap, e_, e_[:], rt[u][:, h:h + 1], e_, e_[:], ALU.is_ge, ALU.mult, rd=[rt[u]])
                    else:
                        m_ = mt[n % 2]
                        mb_[n] = m_
                        stt('dve', m_, m_[:], e_, e_[:], rt[u][:, h:h + 1], e_, e_[:], ALU.is_ge, ALU.mult, rd=[rt[u]])

                def A_(n):
                    u, h = its[n]
                    if h == 0:
                        return
                    m_ = mb_.pop(n)
                    w_ap = Wc[c % 2][u][:].rearrange("p (a b) -> p a b", a=IC)
                    tt('dve', Wc[c % 2][u], w_ap, Wc[c % 2][u], w_ap, m_, m_[:], ALU.add)
                for n in range(K + 2):
                    if n < K:
                        E_(n)
                    if 1 <= n <= K:
                        S_(n - 1)
                    if n >= 2:
                        A_(n - 2)
                    yield

            def load_w(ecx):
                dn, ub = dnb[ecx % 3], upb[ecx % 3]
                dma(dn, dn[:], d_downT, d_downT[:, ecx * 512:(ecx + 1) * 512].rearrange("(c p) e -> p c e", p=128))
                dma(ub, ub[:], d_up, d_up[ecx * 512:(ecx + 1) * 512, :].rearrange("(s p) d -> p s d", p=128))

            items = [(ecx, u) for ecx in range(32) for u in range(TG)]
            N = len(items)

            def stA(n):
                ecx, u = items[n]
                if u == 0:
                    if ecx == 0:
                        load_w(0)
                    if ecx + 1 < 32:
                        load_w(ecx + 1)
                    if ecx % 4 == 0:
                        for _ in wg[0]:
                            pass
                        wg[0] = wgen(ecx // 4 + 1) if ecx // 4 + 1 < NCH else iter(())
                for _ in range(3):
                    next(wg[0], None)
                dn = dnb[ecx % 3]
                pdt = pd[n % 2]; G = Gs[n % 2]
                for c in range(8):
                    mm(pdt, pdt[:], hT, hT[:, c, u * 128:(u + 1) * 128], dn, dn[:, c, :], start=(c == 0), stop=(c == 7))
                act(G, G[:], pdt, pdt[:], AF.Gelu_apprx_tanh)
                wch = Wc[(ecx // 4) % 2][u]
                tt('dve', G, G[:], G, G[:], wch, wch[:, (ecx % 4) * 512:(ecx % 4 + 1) * 512], ALU.mult)

            def stB(n):
                G = Gs[n % 2]; gt_ = GT[n % 2]; pt_ = ptg[n % 2]
                for s_ in range(4):
                    tr(pt_, pt_[:, s_, :], G, G[:, s_ * 128:(s_ + 1) * 128], ident)
                cp('act', gt_, gt_[:], pt_, pt_[:, 0:4, :])

            def stC(n):
                ecx, u = items[n]
                gt_ = GT[n % 2]; ub = upb[ecx % 3]
                for half in range(2):
                    for s_ in range(4):
                        mm(py[u], py[u][:, half, :], gt_, gt_[:, s_, :], ub, ub[:, s_, half * 512:(half + 1) * 512],
                           start=(ecx == 0 and s_ == 0), stop=(ecx == 31 and s_ == 3))

            wg = [iter(())]
            for _ in wgen(0):
                pass
            for n in range(N + 2):
                if n < N:
                    stA(n)
                if 1 <= n <= N:
                    stB(n - 1)
                if n >= 2:
                    stC(n - 2)
            for _ in wg[0]:
                pass
            for u in range(TG):
                j = tg * TG + u
                dma(h2, h2[:], d_h, d_h[j * 128:(j + 1) * 128, :])
                tt('dve', h2, h2[:], h2, h2[:], py[u], py[u][:].rearrange("p a b -> p (a b)"), ALU.add)
                rmsnorm(h2, h2[:], 4, yo, yo[:], junk, ss, rstd)
                dma(out, out[j * 128:(j + 1) * 128, :], yo, yo[:])
        P.pop()

    P.finish()
    return nc, dict(npair=npair, pair_off=pair_off, NCC=NCC, NCMP=NCMP, ninst=P.ninst)


def _t5_bucket_np(dist):
    n = np.maximum(dist, 0)
    nf = np.maximum(n, 1).astype(np.float32)
    log_ratio = (np.log(nf / np.float32(16)) / np.float32(math.log(2048 / 16))).astype(np.float32)
    large = 16 + (log_ratio * np.float32(16)).astype(np.int32)
    large = np.minimum(large, 31)
    return np.where(n < 16, n, large).astype(np.int64)


def _bias_tile(rel_bias, g, dist, valid):
    bk = _t5_bucket_np(dist)
    outt = np.empty((128, 4, 128), np.float32)
    for h in range(4):
        outt[:, h, :] = np.where(valid, rel_bias[bk, g * 4 + h], np.float32(NEG))
    return outt.reshape(128, 512)


def make_core_inputs(inputs, T, b, p, meta):
    NT = T // 128; NO = NT // 2; NS = T // 64; NCMP = meta['NCMP']; NCC = meta['NCC']
    f = lambda a: np.ascontiguousarray(np.asarray(a, dtype=np.float32))
    x = f(inputs['x'][b]); rel_bias = f(inputs['rel_bias'])
    m = {}
    m['xb'] = x
    m['xo'] = np.ascontiguousarray(x.reshape(NO, 2, 128, D)[:, p].reshape(NO * 128, D))
    m['mem'] = f(inputs['mem'][b])
    w_in = f(inputs['w_in'][0])
    m['w_in'] = np.ascontiguousarray(np.concatenate([w_in[:, ORIG[k][0]:ORIG[k][1]] for k in PERM_ORDER], axis=1))
    m['gw2'] = f(inputs['gla_gate_w2'][0]); m['gb'] = f(inputs['gla_gate_b'][0]).reshape(1, 256)
    m['gnorm'] = f(inputs['gla_out_norm'][0]).reshape(1, 128)
    m['norms'] = np.stack([f(inputs['norm_mix'][0]), f(inputs['norm_xattn'][0]), f(inputs['norm_mem'][0]),
                           f(inputs['norm_ffn'][0]), f(inputs['norm_final'])], 0)
    cw1 = np.stack([f(inputs['cmp_k_w1'][0]), f(inputs['cmp_v_w1'][0])], 0)
    m['cw1'] = np.ascontiguousarray(cw1.reshape(2, 32, 64, 128).transpose(0, 2, 1, 3))
    cpos = np.stack([f(inputs['cmp_pos_k'][0]), f(inputs['cmp_pos_v'][0])], 0)
    m['cpos'] = np.ascontiguousarray(cpos.transpose(0, 2, 1))
    m['cw2'] = np.stack([f(inputs['cmp_k_w2'][0]), f(inputs['cmp_v_w2'][0])], 0)
    m['w_out'] = f(inputs['w_out'][0])
    m['xa_w'] = np.stack([f(inputs['xa_wq'][0]), f(inputs['xa_wk'][0]), f(inputs['xa_wv'][0]), f(inputs['xa_wo'][0])], 0)
    m['pwq'] = f(inputs['peer_wq'][0])
    sk = f(inputs['peer_subkeys'][0])
    skd = np.zeros((8, 128, 256), np.float32)
    for h in range(8):
        for pp in range(2):
            skd[h, pp * 64:(pp + 1) * 64, pp * 128:(pp + 1) * 128] = sk[h, pp].T
    m['skd'] = skd
    m['downT'] = np.ascontiguousarray(f(inputs['peer_down'][0]).T)
    m['up'] = f(inputs['peer_up'][0])
    m['c_ident'] = np.eye(128, dtype=np.float32)
    s_ = np.arange(128)[:, None]; t_ = np.arange(128)[None, :]
    m['c_ucs'] = np.where(s_ <= t_, -1.0 / 16, 0.0).astype(np.float32)
    m['c_urev'] = np.where(s_ > t_, -1.0 / 16, 0.0).astype(np.float32)
    m['c_causal'] = (s_ <= t_).astype(np.float32)
    m['c_selu'] = np.stack([np.eye(128) * (1 - p), np.eye(128) * p], 0).astype(np.float32)
    m['c_pv'] = np.tile(np.array([[1.0 - p, float(p)]], np.float32), (128, 1))
    kk = np.arange(128)[:, None]; qq = np.arange(128)[None, :]
    selb = np.empty((2, 128, 15, 512), np.float32); winb = np.empty((2, 128, 6, 512), np.float32)
    for g in range(2):
        for mm_ in range(15):
            j = mm_ - 1 + p
            dist = 128 * j + qq - kk
            selb[g, :, mm_, :] = _bias_tile(rel_bias, g, dist, (dist >= 0) & (j >= 0))
        for mm_ in range(6):
            j = mm_ - 1 + p
            dist = 128 * j + qq - kk
            winb[g, :, mm_, :] = _bias_tile(rel_bias, g, dist, (dist >= 0) & (dist < 512) & (j >= 0))
    m['c_selb'] = selb; m['c_winb'] = winb
    cmpb = np.empty((2, meta['npair'], 128, 512), np.float32)
    for jj in range(NO):
        for c in range(min(NCC, cmp_nchunks(jj))):
            n = 128 * c + kk
            t = (2 * jj + p) * 128 + qq
            dist = t - (16 * n + 31)
            for g in range(2):
                cmpb[g, meta['pair_off'][jj] + c] = _bias_tile(rel_bias, g, dist, (dist >= 0) & (n < NCMP))
    m['c_cmpb'] = cmpb
    far = np.empty((2, 128, 4, 128), np.float32)
    for g in range(2):
        for h in range(4):
            far[g, :, h, :] = rel_bias[31, g * 4 + h]
    m['c_far'] = far.reshape(2, 128, 512)
    wimp = np.zeros((NCC * 128, 128), np.float32)
    for s in range(NS):
        for r in range(-1, 4):
            n = 4 * s + r
            lo = 16 * r
            ov = min(lo + 32, 64) - max(lo, 0)
            if 0 <= n < NCMP:
                wimp[n, s] += ov / 32.0
    m['c_wimp'] = wimp.reshape(NCC, 128, 128)
    m12 = np.zeros((NO, 128, 2, 128), np.float32)
    sid = np.arange(128)[None, :]
    for jj in range(NO):
        t = (2 * jj + p) * 128 + np.arange(128)[:, None]
        cur = t // 64
        visible = (sid * 64 <= t) & (sid < NS)
        f0 = (sid == 0); f1 = (sid == cur); f2 = (sid == cur - 1)
        forced = f0 | f1 | f2
        m12[jj, :, 0, :] = (visible & ~forced)
        add = np.where(~visible, -100.0 - sid, 0.0)
        add = np.where(f2, 100.0, add); add = np.where(f1, 101.0, add); add = np.where(f0 & (sid < NS), 102.0, add)
        m12[jj, :, 1, :] = add
    m['c_m12'] = m12
    ex = np.zeros((128, T), np.float32)
    ex[np.arange(T) // 64, np.arange(T)] = 1.0
    m['c_ex'] = ex.astype(NPBF)
    return m


_CACHE = {}


def kernel(**inputs):
    T = inputs['x'].shape[1]
    B = inputs['x'].shape[0]
    if T not in _CACHE:
        _CACHE[T] = build(T)
    nc, meta = _CACHE[T]
    in_maps = []
    for c in range(2 * B):
        in_maps.append(make_core_inputs(inputs, T, c // 2, c % 2, meta))
    res = run_bass_kernel_spmd(nc, in_maps, core_ids=list(range(2 * B)))
    NO = T // 256
    outp = np.empty((B, T // 128, 128, D), np.float32)
    for c in range(2 * B):
        o = np.asarray(res.results[c]["out"], dtype=np.float32).reshape(NO, 128, D)
        outp[c // 2, (c % 2)::2] = o
    return outp.reshape(B, T, D)
```

```python
import math
import numpy as np
import ml_dtypes
import concourse.bass as bass
import concourse.mybir as mybir
from concourse.bass_utils import run_bass_kernel_spmd
from contextlib import ExitStack

F32 = mybir.dt.float32
BF16 = mybir.dt.bfloat16
AF = mybir.ActivationFunctionType
ALU = mybir.AluOpType
AX = mybir.AxisListType
NPBF = ml_dtypes.bfloat16

D = 1024
NEG = -30000.0


class T:
    def __init__(self, h, name):
        self.h = h
        self.name = name
        self.w = None
        self.r = []
        self.psum = False
        self.dram = False

    def __getitem__(self, k):
        return self.h[k]


class Prog:
    def __init__(self, nc, n_dma_sems=48):
        self.nc = nc
        self.es = ExitStack()
        self.scopes = []
        self.eng = {'pe': nc.tensor, 'dve': nc.vector, 'act': nc.scalar,
                    'pool': nc.gpsimd, 'sp': nc.sync}
        self.sems = {}
        for k in self.eng:
            self.sems['e_' + k] = self.es.enter_context(nc.semaphore('e_' + k))
        self.cnt = {k: 0 for k in self.sems}
        self.ndma = n_dma_sems
        for i in range(n_dma_sems):
            key = 'd_%d' % i
            self.sems[key] = self.es.enter_context(nc.semaphore(key))
            self.cnt[key] = 0
        self.dma_rr = 0
        self.known = {k: {} for k in self.eng}
        self.ntile = 0
        self.ninst = 0

    def push(self):
        self.scopes.append(ExitStack())

    def pop(self):
        self.barrier()
        self.scopes.pop().close()

    def _stack(self):
        return self.scopes[-1] if self.scopes else self.es

    def sb(self, shape, dt, name=None):
        self.ntile += 1
        name = (name or 't') + '_%d' % self.ntile
        h = self._stack().enter_context(self.nc.sbuf_tensor(name, list(shape), dt))
        return T(h, name)

    def ps(self, shape, dt, name=None):
        self.ntile += 1
        name = (name or 'p') + '_%d' % self.ntile
        h = self._stack().enter_context(self.nc.psum_tensor(name, list(shape), dt))
        t = T(h, name)
        t.psum = True
        return t

    def dram(self, name, shape, dt, kind="Internal"):
        h = self.nc.dram_tensor(name, list(shape), dt, kind=kind).ap()
        t = T(h, name)
        t.dram = True
        return t

    def _deps(self, reads, writes, e=None):
        deps = []
        for t in reads:
            if t.w is not None:
                deps.append(t.w)
            if t.psum:
                deps.extend([tok for tok in t.r if tok[0] != 'e_' + str(e)])
        for t in writes:
            if t.w is not None:
                deps.append(t.w)
            deps.extend(t.r)
        return deps

    def _waits(self, e, deps):
        kn = self.known[e]
        need = {}
        for (k, v) in deps:
            if kn.get(k, 0) >= v:
                continue
            if need.get(k, 0) < v:
                need[k] = v
        for k, v in need.items():
            kn[k] = v
        return list(need.items())

    def _emit(self, e, waits, fn, inc):
        eng = self.eng[e]
        for (k, v) in waits:
            eng.wait_ge(self.sems[k], v)
        ins = fn(eng)
        ins.then_inc(self.sems[inc[0]], inc[1])
        self.ninst += 1

    def op(self, e, fn, reads=(), writes=()):
        deps = self._deps(reads, writes, e)
        key = 'e_' + e
        if e == 'pe':
            deps = [d for d in deps if d[0] != key]
        waits = self._waits(e, deps)
        self.cnt[key] += 1
        tok = (key, self.cnt[key])
        self._emit(e, waits, fn, (key, 1))
        for t in reads:
            t.r.append(tok)
        for t in writes:
            t.w = tok
            t.r = []
        return tok

    def dma(self, e, out_t, out_ap, in_t, in_ap, **kw):
        reads = [in_t]
        writes = [out_t]
        deps = self._deps(reads, writes)
        key = 'd_%d' % self.dma_rr
        self.dma_rr = (self.dma_rr + 1) % self.ndma
        if self.cnt[key] > 0:
            deps.append((key, self.cnt[key]))
        waits = self._waits(e, deps)
        self.cnt[key] += 16
        tok = (key, self.cnt[key])
        self._emit(e, waits, lambda eng: eng.dma_start(out=out_ap, in_=in_ap, **kw), (key, 16))
        in_t.r.append(tok)
        out_t.w = tok
        out_t.r = []
        return tok

    def barrier(self):
        allt = [(k, v) for k, v in self.cnt.items() if v > 0]
        for e in self.eng:
            for (k, v) in self._waits(e, allt):
                self.eng[e].wait_ge(self.sems[k], v)

    def finish(self):
        self.barrier()
        while self.scopes:
            self.scopes.pop().close()
        self.es.close()


ORIG = dict(gq=(0, 256), gk=(256, 512), gv=(512, 1024), gr=(1024, 1536), glr=(1536, 1552),
            nq=(1552, 2064), kc=(2064, 2192), vc=(2192, 2320), ks=(2320, 2448), vs=(2448, 2576),
            kw=(2576, 2704), vw=(2704, 2832), ng=(2832, 2856))
PERM_ORDER = ['gq', 'gk', 'gv', 'gr', 'nq', 'kc', 'vc', 'ks', 'vs', 'kw', 'vw', 'ng', 'glr']
NCOL = 2856


def cmp_nchunks(jj):
    return min(4, (16 * jj + 15 + 127) // 128)


def build(T, debug=False, upto=9, cut=99):
    NT = T // 128
    NO = NT // 2
    NS = T // 64
    TO = T // 2
    NCMP = (T - 32) // 16 + 1
    NCC = (NCMP + 127) // 128
    pair_off = []
    npair = 0
    for jj in range(NO):
        pair_off.append(npair)
        npair += min(NCC, cmp_nchunks(jj))

    nc = bass.Bass("TRN2", target_bir_lowering=False)
    P = Prog(nc)
    SK = "ExternalOutput" if debug else "Internal"

    def inp(name, shape, dt=F32):
        return P.dram(name, shape, dt, kind="ExternalInput")

    xb = inp("xb", [T, D]); xo = inp("xo", [TO, D]); memb = inp("mem", [256, D])
    w_in = inp("w_in", [D, NCOL]); gw2 = inp("gw2", [16, 256]); gb = inp("gb", [1, 256])
    gnorm = inp("gnorm", [1, 128])
    norms = inp("norms", [5, D])
    cw1 = inp("cw1", [2, 64, 32, 128]); cpos = inp("cpos", [2, 64, 32]); cw2 = inp("cw2", [2, 128, 64])
    w_out = inp("w_out", [D, D]); xa_w = inp("xa_w", [4, D, D])
    pwq = inp("pwq", [D, D]); skd = inp("skd", [8, 128, 256])
    downT = inp("downT", [D, 16384]); up = inp("up", [16384, D])
    c_ident = inp("c_ident", [128, 128]); c_ucs = inp("c_ucs", [128, 128]); c_urev = inp("c_urev", [128, 128])
    c_causal = inp("c_causal", [128, 128]); c_selu = inp("c_selu", [2, 128, 128]); c_pv = inp("c_pv", [128, 2])
    c_selb = inp("c_selb", [2, 128, 15, 512]); c_winb = inp("c_winb", [2, 128, 6, 512])
    c_cmpb = inp("c_cmpb", [2, npair, 128, 512])
    c_far = inp("c_far", [2, 128, 512])
    c_wimp = inp("c_wimp", [NCC, 128, 128]); c_m12 = inp("c_m12", [NO, 128, 2, 128])
    c_ex = inp("c_ex", [128, T], BF16)
    out = P.dram("out", [TO, D], F32, kind="ExternalOutput")

    d_nq = P.dram("d_nq", [T, 512], BF16, kind=SK)
    d_ng = P.dram("d_ng", [T, 24], F32, kind=SK)
    d_ogla = P.dram("d_ogla", [T, 512], BF16, kind=SK)
    d_kT = P.dram("d_kT", [4, 128, T], BF16, kind=SK)
    d_vs = P.dram("d_vs", [T, 128], BF16, kind=SK)
    d_vw = P.dram("d_vw", [T, 128], BF16, kind=SK)
    d_omix = P.dram("d_omix", [TO, D], BF16, kind=SK)
    d_h = P.dram("d_h", [TO, D], F32, kind=SK)
    d_hn3T = P.dram("d_hn3T", [128, 8, TO], BF16, kind=SK)
    d_route = P.dram("d_route", [TO, 2064], F32, kind=SK)
    d_downT = P.dram("d_downT", [D, 16384], BF16, kind="Internal")
    d_up = P.dram("d_up", [16384, D], BF16, kind="Internal")
    d_cmp = P.dram("d_cmp", [2, 64, 512], BF16, kind=SK)
    d_dbg = P.dram("d_dbg", [3, TO, 512], F32, kind=SK)
    d_imp = P.dram("d_imp", [2, TO, 128], F32, kind=SK)

    def mm(ot, o_ap, lt, l_ap, rt, r_ap, start=True, stop=True):
        P.op('pe', lambda e: e.matmul(o_ap, lhsT=l_ap, rhs=r_ap, start=start, stop=stop), [lt, rt], [ot])

    def tr(ot, o_ap, it, i_ap, idt):
        P.op('pe', lambda e: e.transpose(out=o_ap, in_=i_ap, identity=idt[:]), [it, idt], [ot])

    def act(ot, o_ap, it, i_ap, func, bias=None, scale=None, accum=None, rd=(), wr=()):
        kw = {}
        if bias is not None:
            kw['bias'] = bias
        if scale is not None:
            kw['scale'] = scale
        if accum is not None:
            kw['accum_out'] = accum
        P.op('act', lambda e: e.activation(out=o_ap, in_=i_ap, func=func, **kw), [it] + list(rd), [ot] + list(wr))

    def cp(eng, ot, o_ap, it, i_ap):
        if eng == 'act':
            P.op('act', lambda e: e.copy(out=o_ap, in_=i_ap), [it], [ot])
        else:
            P.op(eng, lambda e: e.tensor_copy(out=o_ap, in_=i_ap), [it], [ot])

    def tt(eng, ot, o_ap, at, a_ap, bt, b_ap, op):
        P.op(eng, lambda e: e.tensor_tensor(out=o_ap, in0=a_ap, in1=b_ap, op=op), [at, bt], [ot])

    def ts(eng, ot, o_ap, at, a_ap, s1, s2, op0, op1=None, rd=()):
        if op1 is None:
            P.op(eng, lambda e: e.tensor_scalar(out=o_ap, in0=a_ap, scalar1=s1, scalar2=None, op0=op0), [at] + list(rd), [ot])
        else:
            P.op(eng, lambda e: e.tensor_scalar(out=o_ap, in0=a_ap, scalar1=s1, scalar2=s2, op0=op0, op1=op1), [at] + list(rd), [ot])

    def stt(eng, ot, o_ap, at, a_ap, sc, bt, b_ap, op0, op1, rd=()):
        P.op(eng, lambda e: e.scalar_tensor_tensor(out=o_ap, in0=a_ap, scalar=sc, in1=b_ap, op0=op0, op1=op1),
             [at, bt] + list(rd), [ot])

    def memset(eng, t, ap, v):
        P.op(eng, lambda e: e.memset(ap, v), [], [t])

    def recip(ot, o_ap, it, i_ap):
        P.op('dve', lambda e: e.reciprocal(out=o_ap, in_=i_ap), [it], [ot])

    dmaq = ['sp', 'act', 'pool']
    dq = [0]

    dma_pol = {'load': ['sp'], 'store': ['pool']}

    def dma(ot, o_ap, it, i_ap, q=None, **kw):
        if q is None:
            qs = dma_pol['store'] if ot.dram else dma_pol['load']
            q = qs[dq[0] % len(qs)]
            dq[0] += 1
        return P.dma(q, ot, o_ap, it, i_ap, **kw)

    ident = P.sb([128, 128], BF16, "ident"); identf = P.sb([128, 128], F32, "identf")
    ones = P.sb([128, 128], BF16, "ones")
    pv = P.sb([128, 2], F32, "pv")
    nrm = P.sb([128, 5, D], F32, "nrm")
    dma(identf, identf[:], c_ident, c_ident[:])
    dma(pv, pv[:], c_pv, c_pv[:])
    for k in range(5):
        dma(nrm, nrm[:, k, :], norms, norms[k:k + 1, :].partition_broadcast(128))
    cp('dve', ident, ident[:], identf, identf[:])
    memset('pool', ones, ones[:], 1.0)
    eps_t = P.sb([128, 1], F32, "eps_t")
    memset('dve', eps_t, eps_t[:], 1e-6)
    kcmpT = [P.sb([128, 512], BF16, "kcmpT%d" % g) for g in range(2)]
    vcmp = [P.sb([128, 4, 65], BF16, "vcmp%d" % g) for g in range(2)]

    def rmsnorm(xt, x_ap, gain_k, hn, hn_ap, junk, ss, rstd, n=D, eps=1e-6):
        memset('dve', ss, ss[:], 0.0)
        act(junk, junk[:, 0:n], xt, x_ap, AF.Square, accum=ss[:], rd=[ss], wr=[ss])
        act(rstd, rstd[:], ss, ss[:], AF.Ln, scale=1.0 / n, bias=eps_t[:, 0:1], rd=[eps_t])
        act(rstd, rstd[:], rstd, rstd[:], AF.Exp, scale=-0.5)
        stt('dve', hn, hn_ap, xt, x_ap, rstd[:, 0:1], nrm, nrm[:, gain_k, 0:n], ALU.mult, ALU.mult, rd=[rstd])

    if upto >= 1:
        P.push()
        winb = P.sb([128, 8, NCOL], BF16, "winb")
        wst = [P.sb([128, NCOL], F32, "wst%d" % i) for i in range(2)]
        w_in_v = w_in[:].rearrange("(c p) n -> c p n", p=128)
        for c in range(8):
            dma(wst[c % 2], wst[c % 2][:], w_in, w_in_v[c])
            cp(['dve', 'pool'][c % 2], winb, winb[:, c, :], wst[c % 2], wst[c % 2][:])
        gw2t = P.sb([16, 256], F32, "gw2t"); gbt = P.sb([1, 256], F32, "gbt"); onesrow = P.sb([1, 128], F32, "onesrow")
        gnt = P.sb([128, 128], F32, "gnt")
        ucs = P.sb([128, 128], F32, "ucs"); urev = P.sb([128, 128], F32, "urev"); causal = P.sb([128, 128], F32, "causal")
        m16 = P.sb([128, 1], F32, "m16")
        dma(gw2t, gw2t[:], gw2, gw2[:]); dma(gbt, gbt[:], gb, gb[:])
        dma(gnt, gnt[:], gnorm, gnorm[0:1, :].partition_broadcast(128))
        dma(ucs, ucs[:], c_ucs, c_ucs[:]); dma(urev, urev[:], c_urev, c_urev[:]); dma(causal, causal[:], c_causal, c_causal[:])
        memset('dve', onesrow, onesrow[:], 1.0)
        memset('dve', m16, m16[:], -1.0 / 16)
        S = P.sb([128, 2, 128], F32, "S"); Sb = P.sb([128, 2, 128], BF16, "Sb")
        memset('dve', S, S[:], 0.0); memset('pool', Sb, Sb[:], 0.0)

        xt = [P.sb([128, D], F32, "xt%d" % i) for i in range(2)]
        junk = P.sb([128, D], BF16, "junk"); ss = P.sb([128, 1], F32, "ss"); rstd = P.sb([128, 1], F32, "rstd")
        hn = P.sb([128, D], BF16, "hn"); hnT = P.sb([128, 8, 128], BF16, "hnT")
        pt = P.ps([128, 8, 128], BF16, "pt")
        pz = [P.ps([128, 512], F32, "pz%d" % i) for i in range(3)]
        pg1 = P.ps([128, 512], F32, "pg1"); pg2 = P.ps([128, 512], F32, "pg2")
        po = P.ps([128, 512], F32, "po"); ptb = P.ps([128, 8, 128], BF16, "ptb")
        glr = P.sb([128, 16], F32, "glr"); glrT = P.sb([16, 128], F32, "glrT")
        e1 = P.sb([128, 256], F32, "e1"); L = P.sb([128, 256], F32, "L")
        eb = P.sb([128, 256], F32, "eb"); enb = P.sb([128, 256], F32, "enb"); ec = P.sb([128, 256], F32, "ec")
        ebl = P.sb([128, 2], F32, "ebl")
        qk = P.sb([128, 3, 256], BF16, "qk")
        qkT = P.sb([128, 4, 128], BF16, "qkT")
        vv = P.sb([128, 512], BF16, "vv"); sg = P.sb([128, 512], F32, "sg")
        AT = P.sb([128, 128], BF16, "AT")
        ssq = P.sb([128, 4], F32, "ssq"); rs4 = P.sb([128, 4], F32, "rs4"); tmpn = P.sb([128, 128], F32, "tmpn")
        junk2 = P.sb([128, 128], F32, "junk2")
        og = P.sb([128, 512], BF16, "og"); otmp4 = P.sb([128, 512], F32, "otmp4")
        nqs = P.sb([128, 512], BF16, "nqs"); ngs = P.sb([128, 24], F32, "ngs")
        kk = P.sb([128, 4, 128], BF16, "kk"); kkT = P.sb([128, 4, 128], BF16, "kkT")
        vsw = P.sb([128, 2, 128], BF16, "vsw")
        cast_jobs = []
        if upto >= 4:
            stg = [P.sb([128, 4096], F32, "stg%d" % i) for i in range(2)]
            stb = [P.sb([128, 4096], BF16, "stb%d" % i) for i in range(2)]
            for r in range(8):
                for c in range(4):
                    cast_jobs.append((downT, downT[r * 128:(r + 1) * 128, c * 4096:(c + 1) * 4096],
                                      d_downT, d_downT[r * 128:(r + 1) * 128, c * 4096:(c + 1) * 4096]))
            sv = up[:].rearrange("(a p f) d -> a p (f d)", p=128, f=4)
            dv = d_up[:].rearrange("(a p f) d -> a p (f d)", p=128, f=4)
            for a in range(32):
                cast_jobs.append((up, sv[a], d_up, dv[a]))
        cj = [0]

        def cast_job():
            k_ = cj[0]
            if k_ < len(cast_jobs):
                src_t, s_ap, dst_t, d_ap = cast_jobs[k_]
                P.dma('pool', stg[k_ % 2], stg[k_ % 2][:], src_t, s_ap)
            if 1 <= k_ <= len(cast_jobs):
                src_t, s_ap, dst_t, d_ap = cast_jobs[k_ - 1]
                a, b_ = stg[(k_ - 1) % 2], stb[(k_ - 1) % 2]
                for q_ in range(4):
                    cp('act', b_, b_[:, q_ * 1024:(q_ + 1) * 1024], a, a[:, q_ * 1024:(q_ + 1) * 1024])
                P.dma('pool', dst_t, d_ap, b_, b_[:])
            cj[0] += 1
        cA, cB, cC, cD, cE, cF = 0, 512, 1024, 1536, 2048, 2560
        hn2_ = [hn, P.sb([128, D], BF16, "hn_b")]; hnT2_ = [hnT, P.sb([128, 8, 128], BF16, "hnT_b")]
        ss2_ = [ss, P.sb([128, 1], F32, "ss_b")]; rstd2_ = [rstd, P.sb([128, 1], F32, "rstd_b")]

        def front(i):
            x_t = xt[i % 2]
            dma(x_t, x_t[:], xb, xb[i * 128:(i + 1) * 128, :])
            rmsnorm(x_t, x_t[:], 0, hn2_[i % 2], hn2_[i % 2][:], junk, ss2_[i % 2], rstd2_[i % 2])
            for c in range(8):
                tr(pt, pt[:, c, :], hn2_[i % 2], hn2_[i % 2][:, c * 128:(c + 1) * 128], ident)
            cp('act', hnT2_[i % 2], hnT2_[i % 2][:], pt, pt[:])
        front(0)
        for i in range(NT):
            hnT = hnT2_[i % 2]

            def zgroup(pz_t, c0, n):
                for c in range(8):
                    mm(pz_t, pz_t[:, 0:n], hnT, hnT[:, c, :], winb, winb[:, c, c0:c0 + n], start=(c == 0), stop=(c == 7))
            zgroup(pz[0], cF, 296)
            cp('act', kk, kk[:, 3, :], pz[0], pz[0][:, 0:128])
            cp('act', vsw, vsw[:, 1, :], pz[0], pz[0][:, 128:256])
            cp('act', glr, glr[:], pz[0], pz[0][:, 280:296])
            act(ngs, ngs[:], pz[0], pz[0][:, 256:280], AF.Exp, scale=-1.0)
            ts('dve', ngs, ngs[:], ngs, ngs[:], 1.0, None, ALU.add)
            recip(ngs, ngs[:], ngs, ngs[:])
            dma(d_ng, d_ng[i * 128:(i + 1) * 128, :], ngs, ngs[:])
            zgroup(pz[1], cE, 512)
            cp('act', kk, kk[:, 0:3, :], pz[1], pz[1][:, 0:384].rearrange("p (a b) -> p a b", a=3))
            cp('dve', vsw, vsw[:, 0, :], pz[1], pz[1][:, 384:512])
            dma(d_vs, d_vs[i * 128:(i + 1) * 128, :], vsw, vsw[:, 0, :])
            dma(d_vw, d_vw[i * 128:(i + 1) * 128, :], vsw, vsw[:, 1, :])
            for a in range(4):
                tr(ptb, ptb[:, a, :], kk, kk[:, a, :], ident)
            cp('act', kkT, kkT[:], ptb, ptb[:, 0:4, :])
            dma(d_kT, d_kT[:, :, i * 128:(i + 1) * 128].rearrange("a p t -> p a t"), kkT, kkT[:])
            zgroup(pz[2], cD, 512)
            P.op('act', lambda e, pzt=pz[2]: e.mul(out=nqs[:], in_=pzt[:], mul=0.125), [pz[2]], [nqs])
            dma(d_nq, d_nq[i * 128:(i + 1) * 128, :], nqs, nqs[:])
            tr(pg1, pg1[0:16, 0:128], glr, glr[:], identf)
            cp('dve', glrT, glrT[:], pg1, pg1[0:16, 0:128])
            mm(pg1, pg1[:, 256:512], glrT, glrT[:], gw2t, gw2t[:], start=True, stop=False)
            mm(pg1, pg1[:, 256:512], onesrow, onesrow[:], gbt, gbt[:], start=False, stop=True)
            zgroup(pz[0], cA, 512)
            zgroup(pz[1], cB, 512)
            zgroup(pz[2], cC, 512)
            act(e1, e1[:], pg1, pg1[:, 256:512], AF.Exp, scale=-1.0)
            act(L, L[:], e1, e1[:], AF.Ln, bias=1.0)
            cp('act', vv, vv[:], pz[1], pz[1][:])
            act(sg, sg[:], pz[2], pz[2][:], AF.Exp, scale=-1.0)
            act(sg, sg[:], sg, sg[:], AF.Ln, bias=1.0)
            act(sg, sg[:], sg, sg[:], AF.Exp, scale=-1.0)
            tt('dve', sg, sg[:], sg, sg[:], pz[2], pz[2][:], ALU.mult)
            tt('pool', sg, sg[:].rearrange("p (h d) -> p h d", h=4), sg, sg[:].rearrange("p (h d) -> p h d", h=4),
               gnt, gnt[:].unsqueeze(1).to_broadcast([128, 4, 128]), ALU.mult)
            mm(pg2, pg2[:, 0:256], ucs, ucs[:], L, L[:])
            mm(pg2, pg2[:, 256:512], urev, urev[:], L, L[:])
            for hp in range(2):
                mm(pg1, pg1[:, hp:hp + 1], L, L[:, hp * 128:(hp + 1) * 128], m16, m16[:])
            act(eb, eb[:], pg2, pg2[:, 0:256], AF.Exp)
            act(enb, enb[:], pg2, pg2[:, 0:256], AF.Exp, scale=-1.0)
            act(ec, ec[:], pg2, pg2[:, 256:512], AF.Exp)
            act(ebl, ebl[:], pg1, pg1[:, 0:2], AF.Exp)
            stt('dve', qk, qk[:, 0, :], pz[0], pz[0][:, 0:256], 0.125, eb, eb[:], ALU.mult, ALU.mult)
            tt('dve', qk, qk[:, 1, :], pz[0], pz[0][:, 256:512], enb, enb[:], ALU.mult)
            tt('dve', qk, qk[:, 2, :], pz[0], pz[0][:, 256:512], ec, ec[:], ALU.mult)
            for a in range(4):
                tr(ptb, ptb[:, 4 + a, :], qk, qk[:, a // 2, (a % 2) * 128:(a % 2 + 1) * 128], ident)
            cp('act', qkT, qkT[:], ptb, ptb[:, 4:8, :])
            if i + 1 < NT:
                front(i + 1)
            memset('dve', ssq, ssq[:], 0.0)
            for h in range(4):
                hp, hh = h // 2, h % 2
                pr = slice(hh * 64, hh * 64 + 64)
                mm(pg2, pg2[:, 0:128], qkT, qkT[pr, 2 + hp, :], qkT, qkT[pr, hp, :])
                tt('dve', AT, AT[:], pg2, pg2[:, 0:128], causal, causal[:], ALU.mult)
                o_ap = po[:, h * 128:(h + 1) * 128]
                mm(po, o_ap, AT, AT[:], vv, vv[:, h * 128:(h + 1) * 128], start=True, stop=False)
                mm(po, o_ap, qkT, qkT[pr, hp, :], Sb, Sb[pr, hp, :], start=False, stop=True)
                mm(pg2, pg2[:, 128:256], qk, qk[:, 2, hp * 128:(hp + 1) * 128], vv, vv[:, h * 128:(h + 1) * 128])
                stt('dve', Sb, Sb[pr, hp, :], S, S[pr, hp, :], ebl[pr, hp:hp + 1], pg2, pg2[pr, 128:256], ALU.mult, ALU.add, rd=[ebl])
                stt('dve', S, S[pr, hp, :], S, S[pr, hp, :], ebl[pr, hp:hp + 1], pg2, pg2[pr, 128:256], ALU.mult, ALU.add, rd=[ebl])
                act(junk2, junk2[:], po, o_ap, AF.Square, accum=ssq[:, h:h + 1], rd=[ssq], wr=[ssq])
            act(rs4, rs4[:], ssq, ssq[:], AF.Ln, scale=1.0 / 128, bias=eps_t[:, 0:1], rd=[eps_t])
            act(rs4, rs4[:], rs4, rs4[:], AF.Exp, scale=-0.5)
            tt('dve', otmp4, otmp4[:], po, po[:], sg, sg[:], ALU.mult)
            tt('dve', og, og[:].rearrange("p (h d) -> p h d", h=4), otmp4, otmp4[:].rearrange("p (h d) -> p h d", h=4),
               rs4, rs4[:].unsqueeze(2).to_broadcast([128, 4, 128]), ALU.mult)
            dma(d_ogla, d_ogla[i * 128:(i + 1) * 128, :], og, og[:])
            for _ in range((len(cast_jobs) + NT - 1) // NT):
                cast_job()
        while cast_jobs and cj[0] <= len(cast_jobs):
            cast_job()
        P.pop()

        P.push()
        w1f = P.sb([64, 32, 128], F32, "w1f"); w1b = P.sb([64, 32, 128], BF16, "w1b")
        posf = P.sb([64, 32], F32, "posf"); posb = P.sb([64, 32], BF16, "posb")
        w2f = P.sb([128, 64], F32, "w2f"); w2b = P.sb([128, 64], BF16, "w2b")
        kTg = P.sb([64, T], BF16, "kTg")
        ph = P.ps([128, 512], F32, "ph"); pb = P.ps([128, 512], F32, "pb"); pc = P.ps([128, 512], F32, "pc")
        bias_h = P.sb([128, 1], F32, "bias_h")
        H = P.sb([128, 512], BF16, "H")
        for g in range(2):
            memset('dve', kcmpT[g], kcmpT[g][:], 0.0)
            memset('dve', vcmp[g], vcmp[g][:], 0.0)
            memset('dve', vcmp[g], vcmp[g][:, :, 64:65], 1.0)
        for kv in range(2):
            dma(w1f, w1f[:], cw1, cw1[kv]); dma(posf, posf[:], cpos, cpos[kv]); dma(w2f, w2f[:], cw2, cw2[kv])
            cp('dve', w1b, w1b[:], w1f, w1f[:]); cp('dve', posb, posb[:], posf, posf[:]); cp('dve', w2b, w2b[:], w2f, w2f[:])
            for l in range(32):
                mm(pb, pb[:, 0:1], w1b, w1b[:, l, :], posb, posb[:, l:l + 1], start=(l == 0), stop=(l == 31))
            cp('dve', bias_h, bias_h[:], pb, pb[:, 0:1])
            for g in range(2):
                dma(kTg, kTg[:], d_kT, d_kT[kv, g * 64:(g + 1) * 64, :])
                for l in range(32):
                    mm(ph, ph[:, 0:NCMP], w1b, w1b[:, l, :], kTg, kTg[:, l:l + 16 * (NCMP - 1) + 1:16],
                       start=(l == 0), stop=(l == 31))
                memset('dve', H, H[:], 0.0)
                act(H, H[:, 0:NCMP], ph, ph[:, 0:NCMP], AF.Gelu_apprx_tanh, bias=bias_h[:, 0:1], rd=[bias_h])
                if kv == 0:
                    mm(pc, pc[0:64, 0:NCMP], w2b, w2b[:], H, H[:, 0:NCMP])
                    cp('act', kcmpT[g], kcmpT[g][0:64, 0:NCMP], pc, pc[0:64, 0:NCMP])
                    if debug:
                        dma(d_cmp, d_cmp[g], kcmpT[g], kcmpT[g][0:64, :])
                else:
                    for c in range(NCC):
                        mm(pc, pc[:, c * 64:(c + 1) * 64], H, H[:, c * 128:(c + 1) * 128], w2b, w2b[:])
                    cp('act', vcmp[g], vcmp[g][:, 0:NCC, 0:64], pc, pc[:, 0:NCC * 64].rearrange("p (c d) -> p c d", d=64))
        P.pop()

    if upto >= 2:
        P.push()
        dma_pol['load'] = ['sp']; dma_pol['store'] = ['sp']
        selu = P.sb([128, 2, 128], BF16, "selu"); seluf = P.sb([128, 2, 128], F32, "seluf")
        dma(seluf, seluf[:], c_selu, c_selu[:].rearrange("a p t -> p a t"))
        cp('dve', selu, selu[:], seluf, seluf[:])
        exm = P.sb([128, T], BF16, "exm")
        dma(exm, exm[:], c_ex, c_ex[:])
        wimpf = P.sb([128, NCC, 128], F32, "wimpf"); wimp = P.sb([128, NCC, 128], BF16, "wimp")
        dma(wimpf, wimpf[:], c_wimp, c_wimp[:].rearrange("c p s -> p c s"))
        cp('dve', wimp, wimp[:], wimpf, wimpf[:])
        ksT = P.sb([128, T], BF16, "ksT"); kwT = P.sb([128, T], BF16, "kwT")
        memset('dve', ksT, ksT[64:128, :], 0.0)
        memset('pool', kwT, kwT[64:128, :], 0.0)
        memset('dve', ksT, ksT[64:65, :], 1.0)
        farf = P.sb([128, 512], F32, "farf")
        vs = P.sb([128, NT, 65], BF16, "vs"); vw = P.sb([128, NT, 65], BF16, "vw")
        selb = P.sb([128, 15, 512], BF16, "selb"); winb2 = P.sb([128, 6, 512], BF16, "winb2")
        bst = [P.sb([128, 512], F32, "bst%d" % i) for i in range(2)]
        cbt = [P.sb([128, 512], BF16, "cbt%d" % i) for i in range(2)]
        qrows_ = [P.sb([128, 2, 256], BF16, "qrows%d" % i) for i in range(2)]; grows_ = [P.sb([128, 2, 24], F32, "grows%d" % i) for i in range(2)]
        gown_ = [P.sb([128, 12], F32, "gown%d" % i) for i in range(2)]; gtmp = P.sb([128, 12], F32, "gtmp")
        qT_ = [P.sb([128, 4, 128], BF16, "qT%d" % i) for i in range(2)]
        for i_ in range(2):
            memset('dve', qT_[i_], qT_[i_][64:128, :, :], 0.0)
        Ec = [P.sb([128, 4, 128], BF16, "Ec%d" % i) for i in range(4)]
        Eb = [P.sb([128, 4, 128], BF16, "Eb%d" % i) for i in range(3)]
        m12_ = [P.sb([128, 2, 128], F32, "m12_%d" % i) for i in range(2)]
        imp = P.sb([128, 128], F32, "imp"); imp2 = P.sb([128, 128], F32, "imp2"); m8 = P.sb([128, 16], F32, "m8")
        mk = P.sb([128, 128], BF16, "mk"); maskT4 = P.sb([128, 4, 128], BF16, "maskT4")
        rden = P.sb([128, 4], F32, "rden"); coef = P.sb([128, 4], F32, "coef")
        oacc = P.sb([128, 4, 64], F32, "oacc"); otmp = P.sb([128, 4, 64], F32, "otmp"); onsa = P.sb([128, 256], BF16, "onsa")
        pq = P.ps([64, 4, 128], F32, "pq")
        psc = [P.ps([128, 4, 128], F32, "psc%d" % i) for i in range(2)]
        pn = P.ps([128, 4, 128], F32, "pn")
        pnT = P.ps([65, 4, 128], F32, "pnT")
        pnT2 = P.ps([65, 4, 128], F32, "pnT2")
        numTs = P.sb([65, 4, 128], F32, "numTs")
        pimp = P.ps([128, 4, 128], F32, "pimp")
        pmt = P.ps([128, 128], BF16, "pmt")
        for g in range(2):
            dma(ksT, ksT[0:64, :], d_kT, d_kT[2, g * 64:(g + 1) * 64, :])
            dma(farf, farf[:], c_far, c_far[g])
            for i_ in range(2):
                cp('dve', qT_[i_], qT_[i_][64:65, :, :], farf, farf[64:65, :].rearrange("p (h q) -> p h q", h=4))
            dma(kwT, kwT[0:64, :], d_kT, d_kT[3, g * 64:(g + 1) * 64, :])
            for n0 in range(0, NT, 16):
                n1 = min(NT, n0 + 16)
                dma(vs, vs[:, n0:n1, 0:64], d_vs, d_vs[n0 * 128:n1 * 128, g * 64:(g + 1) * 64].rearrange("(n p) c -> p n c", p=128))
                dma(vw, vw[:, n0:n1, 0:64], d_vw, d_vw[n0 * 128:n1 * 128, g * 64:(g + 1) * 64].rearrange("(n p) c -> p n c", p=128))
            memset('dve', vs, vs[:, :, 64:65], 1.0)
            memset('dve', vw, vw[:, :, 64:65], 1.0)
            k = 0
            for m in range(15):
                dma(bst[k % 2], bst[k % 2][:], c_selb, c_selb[g, :, m, :])
                tt('pool', selb, selb[:, m, :], bst[k % 2], bst[k % 2][:], farf, farf[:], ALU.subtract); k += 1
            for m in range(6):
                dma(bst[k % 2], bst[k % 2][:], c_winb, c_winb[g, :, m, :])
                cp('pool', winb2, winb2[:, m, :], bst[k % 2], bst[k % 2][:]); k += 1
            ne = 0
            ne_ = [0]
            for jj in range(NO):
                qrows, grows, gown, qT, m12 = qrows_[jj % 2], grows_[jj % 2], gown_[jj % 2], qT_[jj % 2], m12_[jj % 2]
                dma(qrows, qrows[:], d_nq, d_nq[jj * 256:(jj + 1) * 256, g * 256:(g + 1) * 256].rearrange("(u p) c -> p u c", p=128))
                dma(grows, grows[:], d_ng, d_ng[jj * 256:(jj + 1) * 256, :].rearrange("(u p) c -> p u c", p=128))
                dma(m12, m12[:], c_m12, c_m12[jj])
                for h in range(4):
                    for u in range(2):
                        mm(pq, pq[:, h, :], qrows, qrows[:, u, h * 64:(h + 1) * 64], selu, selu[:, u, :], start=(u == 0), stop=(u == 1))
                cp('act', qT, qT[0:64], pq, pq[:])
                ts('dve', gtmp, gtmp[:], grows, grows[:, 0, g * 12:(g + 1) * 12], pv[:, 0:1], None, ALU.mult, rd=[pv])
                stt('dve', gown, gown[:], grows, grows[:, 1, g * 12:(g + 1) * 12], pv[:, 1:2], gtmp, gtmp[:], ALU.mult, ALU.add, rd=[pv])
                gv3 = gown[:].rearrange("p (h k) -> p h k", k=3)
                qT_all = qT[:].rearrange("p h q -> p (h q)")
                qT_aug = qT_all

                def finish_branch(pn, br, first):
                    ts('dve', rden, rden[:], pn, pn[:, :, 64], 1e-30, None, ALU.max)
                    recip(rden, rden[:], rden, rden[:])
                    tt('dve', coef, coef[:], rden, rden[:], gown, gv3[:, :, br], ALU.mult)
                    dst = oacc if first else otmp
                    tt('dve', dst, dst[:], pn, pn[:, :, 0:64], coef, coef[:].unsqueeze(2).to_broadcast([128, 4, 64]), ALU.mult)
                    if not first:
                        tt('pool', oacc, oacc[:], oacc, oacc[:], otmp, otmp[:], ALU.add)
                    if debug:
                        dma(d_dbg, d_dbg[br, jj * 128:(jj + 1) * 128, g * 256:(g + 1) * 256], oacc, oacc[:].rearrange("p h d -> p (h d)"))

                def back_T(src):
                    cp('act', numTs, numTs[:], src, src[:])
                    for h in range(4):
                        P.op('pe', lambda e, h=h: e.transpose(out=pn[:, h, 0:65], in_=numTs[:, h, :], identity=identf[0:65, 0:65]), [numTs, identf], [pn])

                ncc = min(NCC, cmp_nchunks(jj))
                for c in range(ncc):
                    pi = pair_off[jj] + c
                    dma(bst[k % 2], bst[k % 2][:], c_cmpb, c_cmpb[g, pi])
                    cp('pool', cbt[k % 2], cbt[k % 2][:], bst[k % 2], bst[k % 2][:])
                    sc = psc[ne % 2]; ne += 1
                    sc_all = sc[:].rearrange("p h q -> p (h q)")
                    mm(sc, sc_all, kcmpT[g], kcmpT[g][:, c * 128:(c + 1) * 128], qT, qT_all, start=True, stop=False)
                    mm(sc, sc_all, ident, ident[:], cbt[k % 2], cbt[k % 2][:], start=False, stop=True)
                    k += 1
                    act(Ec[c], Ec[c][:], sc, sc[:], AF.Exp)
                for h in range(4):
                    for c in range(ncc):
                        mm(pn, pn[:, h, 0:65], Ec[c], Ec[c][:, h, :], vcmp[g], vcmp[g][:, c, :], start=(c == 0), stop=(c == ncc - 1))
                    for c in range(ncc):
                        mm(pimp, pimp[:, h, :], Ec[c], Ec[c][:, h, :], wimp, wimp[:, c, :], start=(c == 0), stop=(c == ncc - 1))
                nk = 2 * jj + 2
                k0 = max(0, 2 * jj - 4)
                bufs = {}

                def score(kind, kc, n):
                    sc = psc[ne_[0] % 2]; E = Eb[ne_[0] % 3]; ne_[0] += 1
                    bufs[n] = E
                    sc_all = sc[:].rearrange("p h q -> p (h q)")
                    if kind == 's':
                        m = 2 * jj + 1 - kc
                        mm(sc, sc_all, ksT, ksT[:, kc * 128:(kc + 1) * 128], qT, qT_aug, start=True, stop=False)
                        if m < 14:
                            mm(sc, sc_all, ident, ident[:], selb, selb[:, m, :], start=False, stop=False)
                        mm(sc, sc_all, exm, exm[:, kc * 128:(kc + 1) * 128], maskT4, mT_all, start=False, stop=True)
                    else:
                        m = 2 * jj + 1 - kc
                        mm(sc, sc_all, kwT, kwT[:, kc * 128:(kc + 1) * 128], qT, qT_all, start=True, stop=False)
                        mm(sc, sc_all, ident, ident[:], winb2, winb2[:, m, :], start=False, stop=True)
                    act(E, E[:], sc, sc[:], AF.Exp)

                def pvs(kind, kc, n):
                    E = bufs.pop(n)
                    if kind == 's':
                        mm(pnT, pnT[:].rearrange("p h q -> p (h q)"), vs, vs[:, kc, :], E, E[:].rearrange("p h q -> p (h q)"), start=(kc == 0), stop=(kc == nk - 1))
                    else:
                        mm(pnT2, pnT2[:].rearrange("p h q -> p (h q)"), vw, vw[:, kc, :], E, E[:].rearrange("p h q -> p (h q)"), start=(kc == k0), stop=(kc == nk - 1))

                def run_items(items):
                    NI = len(items)
                    for n in range(NI + 1):
                        if n < NI:
                            score(items[n][0], items[n][1], n)
                        if n >= 1:
                            pvs(items[n - 1][0], items[n - 1][1], n - 1)
                mT_all = maskT4[:].rearrange("p h q -> p (h q)")
                run_items([('w', kc) for kc in range(k0, nk)])
                finish_branch(pn, 0, True)
                for h in range(4):
                    if h == 0:
                        ts('dve', imp, imp[:], pimp, pimp[:, 0, :], rden[:, 0:1], None, ALU.mult, rd=[rden])
                    else:
                        stt('dve', imp, imp[:], pimp, pimp[:, h, :], rden[:, h:h + 1], imp, imp[:], ALU.mult, ALU.add, rd=[rden])
                tt('dve', imp, imp[:], imp, imp[:], m12, m12[:, 0, :], ALU.mult)
                tt('dve', imp, imp[:], imp, imp[:], m12, m12[:, 1, :], ALU.add)
                if debug:
                    dma(d_imp, d_imp[g, jj * 128:(jj + 1) * 128, :], imp, imp[:])
                P.op('dve', lambda e: e.max(out=m8[:, 0:8], in_=imp[:]), [imp], [m8])
                P.op('dve', lambda e: e.match_replace(out=imp2[:], in_to_replace=m8[:, 0:8], in_values=imp[:], imm_value=-1e30), [imp, m8], [imp2])
                P.op('dve', lambda e: e.max(out=m8[:, 8:16], in_=imp2[:]), [imp2], [m8])
                ts('dve', mk, mk[:], imp, imp[:], m8[:, 15:16], 1.0, ALU.is_ge, ALU.subtract, rd=[m8])
                tr(pmt, pmt[:], mk, mk[:], ident)
                P.op('act', lambda e: e.mul(out=maskT4[:], in_=pmt[:].unsqueeze(1).to_broadcast([128, 4, 128]), mul=30000.0), [pmt], [maskT4])
                run_items([('s', kc) for kc in range(nk)])
                back_T(pnT)
                finish_branch(pn, 1, False)
                back_T(pnT2)
                finish_branch(pn, 2, False)
                cp('act', onsa, onsa[:], oacc, oacc[:].rearrange("p h d -> p (h d)"))
                dma(d_omix, d_omix[jj * 128:(jj + 1) * 128, 512 + g * 256:512 + (g + 1) * 256], onsa, onsa[:])
        P.pop()

    if upto >= 3:
        P.push()
        dma_pol['load'] = ['sp']; dma_pol['store'] = ['pool']
        woutb = P.sb([128, 8, D], BF16, "woutb"); wqb = P.sb([128, 8, D], BF16, "wqb"); wob = P.sb([128, 8, D], BF16, "wob")
        pwqb = P.sb([128, 8, D], BF16, "pwqb")
        skb = P.sb([128, 8, 256], BF16, "skb")
        kTm = P.sb([128, 8, 256], BF16, "kTm")
        vm = P.sb([128, 2, 4, 257], BF16, "vm")
        xt = [P.sb([128, D], F32, "x3_%d" % i) for i in range(2)]
        junk = P.sb([128, D], BF16, "junk3"); ss = P.sb([128, 1], F32, "ss3"); rstd = P.sb([128, 1], F32, "rstd3")
        hn = P.sb([128, D], BF16, "hn3"); hnT = P.sb([128, 8, 128], BF16, "hnT3")
        pt = P.ps([128, 8, 128], BF16, "pt3")
        pa = [P.ps([128, 512], F32, "pa%d" % i) for i in range(4)]
        pxa = P.ps([128, 2, 512], F32, "pxa")
        P.push()
        wst = [P.sb([128, D], F32, "wst3_%d" % i) for i in range(2)]
        wtmp = P.sb([128, 8, D], BF16, "wtmp"); skf = P.sb([128, 8, 256], F32, "skf")
        memT = P.sb([128, 8, 256], BF16, "memT")
        k = [0]

        def load_w(dst, src_t, src_ap3):
            for c in range(8):
                a = wst[k[0] % 2]
                dma(a, a[:], src_t, src_ap3[c])
                cp(['dve', 'pool'][k[0] % 2], dst, dst[:, c, :], a, a[:]); k[0] += 1
        load_w(woutb, w_out, w_out[:].rearrange("(c p) n -> c p n", p=128))
        load_w(wqb, xa_w, xa_w[0].rearrange("(c p) n -> c p n", p=128))
        load_w(wob, xa_w, xa_w[3].rearrange("(c p) n -> c p n", p=128))
        load_w(pwqb, pwq, pwq[:].rearrange("(c p) n -> c p n", p=128))
        dma(skf, skf[:], skd, skd[:].rearrange("c p n -> p c n"))
        cp('dve', skb, skb[:], skf, skf[:])
        memset('dve', vm, vm[:, :, :, 256:257], 1.0)
        for mc in range(2):
            x_t = xt[mc % 2]
            dma(x_t, x_t[:], memb, memb[mc * 128:(mc + 1) * 128, :])
            rmsnorm(x_t, x_t[:], 2, hn, hn[:], junk, ss, rstd)
            for c in range(8):
                tr(pt, pt[:, c, :], hn, hn[:, c * 128:(c + 1) * 128], ident)
            cp('act', memT, memT[:, :, mc * 128:(mc + 1) * 128], pt, pt[:])
        load_w(wtmp, xa_w, xa_w[1].rearrange("(c p) n -> c p n", p=128))
        for oc in range(8):
            for c in range(8):
                mm(pa[0], pa[0][:, 0:256], wtmp, wtmp[:, c, oc * 128:(oc + 1) * 128], memT, memT[:, c, :], start=(c == 0), stop=(c == 7))
            cp('act', kTm, kTm[:, oc, :], pa[0], pa[0][:, 0:256])
        load_w(wtmp, xa_w, xa_w[2].rearrange("(c p) n -> c p n", p=128))
        for mc in range(2):
            for half in range(2):
                for c in range(8):
                    mm(pa[half], pa[half][:], memT, memT[:, c, mc * 128:(mc + 1) * 128], wtmp, wtmp[:, c, half * 512:(half + 1) * 512], start=(c == 0), stop=(c == 7))
                cp('act', vm, vm[:, mc, half * 2:half * 2 + 2, 0:256], pa[half], pa[half][:].rearrange("p (h d) -> p h d", d=256))
        P.pop()
        og2 = P.sb([128, 2, 512], BF16, "og2"); omx = P.sb([128, D], BF16, "omx"); otm = P.sb([128, 512], F32, "otm")
        h1 = P.sb([128, D], F32, "h1"); qTx = P.sb([128, 8, 128], BF16, "qTx")
        Ex = P.sb([128, 2, 4, 128], BF16, "Ex"); rdx = P.sb([128, 4], F32, "rdx")
        oxa = P.sb([128, D], BF16, "oxa")
        hn3T = P.sb([128, 8, 128], BF16, "hn3Ts"); qpT = P.sb([128, 8, 128], BF16, "qpT")
        sc_ = [P.sb([128, 16, 128], F32, "scr%d" % i) for i in range(2)]; ab_ = [P.sb([128, 16, 128], F32, "ab%d" % i) for i in range(2)]
        negm = P.sb([128, 16], F32, "negm"); t16 = P.sb([128, 16, 16], F32, "t16"); scr2_ = [P.sb([128, 128], F32, "scr2_%d" % i) for i in range(4)]
        candall = P.sb([128, 8, 256], F32, "candall"); cand2_ = [P.sb([128, 256], F32, "cand2_%d" % i) for i in range(4)]; c16 = P.sb([128, 8, 16], F32, "c16")
        route = P.sb([128, 16], F32, "route"); zs = P.sb([128, 8], F32, "zs")
        memset('dve', route, route[:], 0.0)
        def Xgen(jj):
            sc = sc_[jj % 2]
            x_t = xt[jj % 2]
            dma(x_t, x_t[:], xo, xo[jj * 128:(jj + 1) * 128, :])
            dma(og2, og2[:], d_ogla, d_ogla[jj * 256:(jj + 1) * 256, :].rearrange("(u p) c -> p u c", p=128))
            dma(omx, omx[:, 512:1024], d_omix, d_omix[jj * 128:(jj + 1) * 128, 512:1024])
            ts('dve', otm, otm[:], og2, og2[:, 0, :], pv[:, 0:1], None, ALU.mult, rd=[pv])
            stt('dve', omx, omx[:, 0:512], og2, og2[:, 1, :], pv[:, 1:2], otm, otm[:], ALU.mult, ALU.add, rd=[pv])
            for c in range(8):
                tr(pt, pt[:, c, :], omx, omx[:, c * 128:(c + 1) * 128], ident)
            cp('act', hnT, hnT[:], pt, pt[:])
            for half in range(2):
                for c in range(8):
                    mm(pa[half], pa[half][:], hnT, hnT[:, c, :], woutb, woutb[:, c, half * 512:(half + 1) * 512], start=(c == 0), stop=(c == 7))
                tt('dve', h1, h1[:, half * 512:(half + 1) * 512], pa[half], pa[half][:], x_t, x_t[:, half * 512:(half + 1) * 512], ALU.add)
            yield
            rmsnorm(h1, h1[:], 1, hn, hn[:], junk, ss, rstd)
            for c in range(8):
                tr(pt, pt[:, c, :], hn, hn[:, c * 128:(c + 1) * 128], ident)
            cp('act', hnT, hnT[:], pt, pt[:])
            for oc in range(8):
                pq_ = pa[2 + oc % 2]
                for c in range(8):
                    mm(pq_, pq_[:, 0:128], wqb, wqb[:, c, oc * 128:(oc + 1) * 128], hnT, hnT[:, c, :], start=(c == 0), stop=(c == 7))
                cp(['act', 'dve'][oc % 2], qTx, qTx[:, oc, :], pq_, pq_[:, 0:128])
                yield
            yield
            for mc in range(2):
                for h in range(4):
                    for dc in range(2):
                        mm(pxa, pxa[:, mc, h * 128:(h + 1) * 128], kTm, kTm[:, h * 2 + dc, mc * 128:(mc + 1) * 128], qTx, qTx[:, h * 2 + dc, :], start=(dc == 0), stop=(dc == 1))
            act(Ex, Ex[:].rearrange("p a h q -> p a (h q)"), pxa, pxa[:], AF.Exp, scale=1.0 / 16)
            for h in range(4):
                o_ap = pxa[:, h // 2, (h % 2) * 256:(h % 2) * 256 + 256]
                for mc in range(2):
                    mm(pa[h % 2], pa[h % 2][:, 0:257], Ex, Ex[:, mc, h, :], vm, vm[:, mc, h, :], start=(mc == 0), stop=(mc == 1))
                ts('dve', rdx, rdx[:, h:h + 1], pa[h % 2], pa[h % 2][:, 256:257], 1e-30, None, ALU.max)
                recip(rdx, rdx[:, h:h + 1], rdx, rdx[:, h:h + 1])
                ts('dve', oxa, oxa[:, h * 256:(h + 1) * 256], pa[h % 2], pa[h % 2][:, 0:256], rdx[:, h:h + 1], None, ALU.mult, rd=[rdx])
            yield
            for c in range(8):
                tr(pt, pt[:, c, :], oxa, oxa[:, c * 128:(c + 1) * 128], ident)
            cp('act', hnT, hnT[:], pt, pt[:])
            for half in range(2):
                for c in range(8):
                    mm(pa[half], pa[half][:], hnT, hnT[:, c, :], wob, wob[:, c, half * 512:(half + 1) * 512], start=(c == 0), stop=(c == 7))
                tt('dve', h1, h1[:, half * 512:(half + 1) * 512], pa[half], pa[half][:], h1, h1[:, half * 512:(half + 1) * 512], ALU.add)
            dma(d_h, d_h[jj * 128:(jj + 1) * 128, :], h1, h1[:])
            yield
            rmsnorm(h1, h1[:], 3, hn, hn[:], junk, ss, rstd)
            for c in range(8):
                tr(pt, pt[:, c, :], hn, hn[:, c * 128:(c + 1) * 128], ident)
            cp('act', hn3T, hn3T[:], pt, pt[:])
            dma(d_hn3T, d_hn3T[:, :, jj * 128:(jj + 1) * 128], hn3T, hn3T[:])
            for oc in range(8):
                pq_ = pa[2 + oc % 2]
                for c in range(8):
                    mm(pq_, pq_[:, 0:128], pwqb, pwqb[:, c, oc * 128:(oc + 1) * 128], hn3T, hn3T[:, c, :], start=(c == 0), stop=(c == 7))
                cp(['act', 'dve'][oc % 2], qpT, qpT[:, oc, :], pq_, pq_[:, 0:128])
                yield
            yield
            for oc in range(8):
                pq_ = pa[oc % 2]
                mm(pq_, pq_[:, 0:256], qpT, qpT[:, oc, :], skb, skb[:, oc, :])
                cp(['act', 'dve'][oc % 2], sc, sc[:, 2 * oc:2 * oc + 2, :], pq_, pq_[:, 0:256].rearrange("p (a k) -> p a k", a=2))
            yield

        def Rgen(jj):
            sc = sc_[jj % 2]; ab = ab_[jj % 2]
            P.op('dve', lambda e: e.tensor_reduce(out=negm[:], in_=sc[:], axis=AX.X, op=ALU.max), [sc], [negm])
            ts('dve', negm, negm[:], negm, negm[:], -1.0, None, ALU.mult)
            for r in range(16):
                act(ab, ab[:, r, :], sc, sc[:, r, :], AF.Exp, bias=negm[:, r:r + 1], rd=[negm])
            yield
            for r0 in range(0, 16, 4):
                for r in range(r0, r0 + 4):
                    P.op('dve', lambda e, r=r: e.max(out=t16[:, r, 0:8], in_=ab[:, r, :]), [ab], [t16])
                for r in range(r0, r0 + 4):
                    P.op('dve', lambda e, r=r: e.match_replace(out=scr2_[r % 4][:], in_to_replace=t16[:, r, 0:8], in_values=ab[:, r, :], imm_value=-1.0), [ab, t16], [scr2_[r % 4]])
                for r in range(r0, r0 + 4):
                    P.op('dve', lambda e, r=r: e.max(out=t16[:, r, 8:16], in_=scr2_[r % 4][:]), [scr2_[r % 4]], [t16])
                yield
            t16v = t16[:].rearrange("p (h a) k -> p h a k", a=2)
            abv = ab[:].rearrange("p (h a) k -> p h a k", a=2)

            def cand_top16():
                yield
                tt('dve', candall, candall[:].rearrange("p h (a b) -> p h a b", a=16),
                   t16, t16v[:, :, 0, :].unsqueeze(3).to_broadcast([128, 8, 16, 16]),
                   t16, t16v[:, :, 1, :].unsqueeze(2).to_broadcast([128, 8, 16, 16]), ALU.mult)
                for h0 in range(0, 8, 4):
                    for h in range(h0, h0 + 4):
                        P.op('dve', lambda e, h=h: e.max(out=c16[:, h, 0:8], in_=candall[:, h, :]), [candall], [c16])
                    for h in range(h0, h0 + 4):
                        P.op('dve', lambda e, h=h: e.match_replace(out=cand2_[h % 4][:], in_to_replace=c16[:, h, 0:8], in_values=candall[:, h, :], imm_value=-1.0), [candall, c16], [cand2_[h % 4]])
                    for h in range(h0, h0 + 4):
                        P.op('dve', lambda e, h=h: e.max(out=c16[:, h, 8:16], in_=cand2_[h % 4][:]), [cand2_[h % 4]], [c16])
                    yield
            yield from cand_top16()
            P.op('dve', lambda e: e.tensor_reduce(out=zs[:], in_=c16[:], axis=AX.X, op=ALU.add), [c16], [zs])
            recip(zs, zs[:], zs, zs[:])
            tt('dve', ab, abv[:, :, 1, :], ab, abv[:, :, 1, :], zs, zs[:].unsqueeze(2).to_broadcast([128, 8, 128]), ALU.mult)
            stt('dve', route, route[:, 0:8], c16, c16[:, :, 15], 1.0 - 1e-6, zs, zs[:], ALU.mult, ALU.mult)
            dma(d_route, d_route[jj * 128:(jj + 1) * 128, 0:2048], ab, ab[:].rearrange("p r k -> p (r k)"))
            dma(d_route, d_route[jj * 128:(jj + 1) * 128, 2048:2064], route, route[:])
            yield

        def drain(g):
            for _ in g:
                pass
        drain(Xgen(0))
        for jj in range(NO):
            gr = Rgen(jj)
            gx = Xgen(jj + 1) if jj + 1 < NO else iter(())
            ra = xa_ = True
            while ra or xa_:
                if xa_:
                    xa_ = next(gx, 'END') != 'END'
                if ra:
                    ra = next(gr, 'END') != 'END'
        P.pop()

    if upto >= 4:
        P.push()
        dma_pol['load'] = ['sp', 'pool']; dma_pol['store'] = ['sp']
        TG = 2
        IC = 16
        NCH = 128 // IC
        ACT_HEADS = (1, 3, 4, 6, 7)
        hT = P.sb([128, 8, TG * 128], BF16, "hT")
        ab = [P.sb([128, 16, 128], F32, "ab4_%d" % u) for u in range(TG)]
        rt = [P.sb([128, 16], F32, "rt%d" % u) for u in range(TG)]
        Wc = [[P.sb([128, IC * 128], BF16, "Wc%d_%d" % (i, u)) for u in range(TG)] for i in range(2)]
        et = [P.sb([128, IC, 128], F32, "et%d" % i) for i in range(4)]
        mt = [P.sb([128, IC, 128], BF16, "mt%d" % i) for i in range(2)]
        dnb = [P.sb([128, 8, 512], BF16, "dnb%d" % i) for i in range(3)]
        upb = [P.sb([128, 4, D], BF16, "upb%d" % i) for i in range(3)]
        Gs = [P.sb([128, 512], BF16, "G%d" % i) for i in range(2)]
        GT = [P.sb([128, 4, 128], BF16, "GT%d" % i) for i in range(2)]
        py = [P.ps([128, 2, 512], F32, "py%d" % u) for u in range(TG)]
        pd = [P.ps([128, 512], F32, "pd%d" % i) for i in range(2)]
        ptg = [P.ps([128, 8, 128], BF16, "ptg%d" % i) for i in range(2)]
        h2 = P.sb([128, D], F32, "h2"); yo = P.sb([128, D], F32, "yo")
        junk = P.sb([128, D], BF16, "junk4"); ss = P.sb([128, 1], F32, "ss4"); rstd = P.sb([128, 1], F32, "rstd4")
        qn = [0]
        for tg in range(NO // TG):
            dma(hT, hT[:], d_hn3T, d_hn3T[:, :, tg * TG * 128:(tg + 1) * TG * 128])
            for u in range(TG):
                j = tg * TG + u
                dma(ab[u], ab[u][:].rearrange("p r k -> p (r k)"), d_route, d_route[j * 128:(j + 1) * 128, 0:2048])
                dma(rt[u], rt[u][:], d_route, d_route[j * 128:(j + 1) * 128, 2048:2064])

            def wgen(c):
                its = [(u, h) for u in range(TG) for h in range(8)]
                K = len(its)
                eb_ = {}; mb_ = {}

                def E_(n):
                    u, h = its[n]
                    e_ = et[qn[0] % 4]; qn[0] += 1
                    eb_[n] = e_
                    if h in ACT_HEADS:
                        for i_ in range(IC):
                            P.op('act', lambda e, e_=e_, i_=i_, u=u, h=h: e.activation(out=e_[:, i_, :], in_=ab[u][:, 2 * h + 1, :], func=AF.Copy,
                                                                                  scale=ab[u][:, 2 * h, c * IC + i_:c * IC + i_ + 1]),
                                 [ab[u]], [e_] if i_ in (0, IC - 1) else [])
                    else:
                        tt('dve', e_, e_[:], ab[u], ab[u][:, 2 * h, c * IC:(c + 1) * IC].unsqueeze(2).to_broadcast([128, IC, 128]),
                           ab[u], ab[u][:, 2 * h + 1, :].unsqueeze(1).to_broadcast([128, IC, 128]), ALU.mult)

                def S_(n):
                    u, h = its[n]
                    e_ = eb_.pop(n)
                    w_ap = Wc[c % 2][u][:].rearrange("p (a b) -> p a b", a=IC)
                    if h == 0:
                        stt('dve', Wc[c % 2][u], w_ap, e_, e_[:], rt[u][:, h:h + 1], e_, e_[:], ALU.is_ge, ALU.mult, rd=[rt[u]])
                    else:
                        m_ = mt[n % 2]
                        mb_[n] = m_
                        stt('dve', m_, m_[:], e_, e_[:], rt[u][:, h:h + 1], e_, e_[:], ALU.is_ge, ALU.mult, rd=[rt[u]])

                def A_(n):
                    u, h = its[n]
                    if h == 0:
                        return
                    m_ = mb_.pop(n)
                    w_ap = Wc[c % 2][u][:].rearrange("p (a b) -> p a b", a=IC)
                    tt('dve', Wc[c % 2][u], w_ap, Wc[c % 2][u], w_ap, m_, m_[:], ALU.add)
                for n in range(K + 2):
                    if n < K:
                        E_(n)
                    if 1 <= n <= K:
                        S_(n - 1)
                    if n >= 2:
                        A_(n - 2)
                    yield

            def load_w(ecx):
                dn, ub = dnb[ecx % 3], upb[ecx % 3]
                dma(dn, dn[:], d_downT, d_downT[:, ecx * 512:(ecx + 1) * 512].rearrange("(c p) e -> p c e", p=128))
                dma(ub, ub[:], d_up, d_up[ecx * 512:(ecx + 1) * 512, :].rearrange("(s p) d -> p s d", p=128))

            items = [(ecx, u) for ecx in range(32) for u in range(TG)]
            N = len(items)

            def stA(n):
                ecx, u = items[n]
                if u == 0:
                    if ecx == 0:
                        load_w(0)
                    if ecx + 1 < 32:
                        load_w(ecx + 1)
                    if ecx % 4 == 0:
                        for _ in wg[0]:
                            pass
                        wg[0] = wgen(ecx // 4 + 1) if ecx // 4 + 1 < NCH else iter(())
                for _ in range(3):
                    next(wg[0], None)
                dn = dnb[ecx % 3]
                pdt = pd[n % 2]; G = Gs[n % 2]
                for c in range(8):
                    mm(pdt, pdt[:], hT, hT[:, c, u * 128:(u + 1) * 128], dn, dn[:, c, :], start=(c == 0), stop=(c == 7))
                act(G, G[:], pdt, pdt[:], AF.Gelu_apprx_tanh)
                wch = Wc[(ecx // 4) % 2][u]
                tt('dve', G, G[:], G, G[:], wch, wch[:, (ecx % 4) * 512:(ecx % 4 + 1) * 512], ALU.mult)

            def stB(n):
                G = Gs[n % 2]; gt_ = GT[n % 2]; pt_ = ptg[n % 2]
                for s_ in range(4):
                    tr(pt_, pt_[:, s_, :], G, G[:, s_ * 128:(s_ + 1) * 128], ident)
                cp('act', gt_, gt_[:], pt_, pt_[:, 0:4, :])

            def stC(n):
                ecx, u = items[n]
                gt_ = GT[n % 2]; ub = upb[ecx % 3]
                for half in range(2):
                    for s_ in range(4):
                        mm(py[u], py[u][:, half, :], gt_, gt_[:, s_, :], ub, ub[:, s_, half * 512:(half + 1) * 512],
                           start=(ecx == 0 and s_ == 0), stop=(ecx == 31 and s_ == 3))

            wg = [iter(())]
            for _ in wgen(0):
                pass
            for n in range(N + 2):
                if n < N:
                    stA(n)
                if 1 <= n <= N:
                    stB(n - 1)
                if n >= 2:
                    stC(n - 2)
            for _ in wg[0]:
                pass
            for u in range(TG):
                j = tg * TG + u
                dma(h2, h2[:], d_h, d_h[j * 128:(j + 1) * 128, :])
                tt('dve', h2, h2[:], h2, h2[:], py[u], py[u][:].rearrange("p a b -> p (a b)"), ALU.add)
                rmsnorm(h2, h2[:], 4, yo, yo[:], junk, ss, rstd)
                dma(out, out[j * 128:(j + 1) * 128, :], yo, yo[:])
        P.pop()

    P.finish()
    return nc, dict(npair=npair, pair_off=pair_off, NCC=NCC, NCMP=NCMP, ninst=P.ninst)


def _t5_bucket_np(dist):
    n = np.maximum(dist, 0)
    nf = np.maximum(n, 1).astype(np.float32)
    log_ratio = (np.log(nf / np.float32(16)) / np.float32(math.log(2048 / 16))).astype(np.float32)
    large = 16 + (log_ratio * np.float32(16)).astype(np.int32)
    large = np.minimum(large, 31)
    return np.where(n < 16, n, large).astype(np.int64)


def _bias_tile(rel_bias, g, dist, valid):
    bk = _t5_bucket_np(dist)
    outt = np.empty((128, 4, 128), np.float32)
    for h in range(4):
        outt[:, h, :] = np.where(valid, rel_bias[bk, g * 4 + h], np.float32(NEG))
    return outt.reshape(128, 512)


def make_core_inputs(inputs, T, b, p, meta):
    NT = T // 128; NO = NT // 2; NS = T // 64; NCMP = meta['NCMP']; NCC = meta['NCC']
    f = lambda a: np.ascontiguousarray(np.asarray(a, dtype=np.float32))
    x = f(inputs['x'][b]); rel_bias = f(inputs['rel_bias'])
    m = {}
    m['xb'] = x
    m['xo'] = np.ascontiguousarray(x.reshape(NO, 2, 128, D)[:, p].reshape(NO * 128, D))
    m['mem'] = f(inputs['mem'][b])
    w_in = f(inputs['w_in'][0])
    m['w_in'] = np.ascontiguousarray(np.concatenate([w_in[:, ORIG[k][0]:ORIG[k][1]] for k in PERM_ORDER], axis=1))
    m['gw2'] = f(inputs['gla_gate_w2'][0]); m['gb'] = f(inputs['gla_gate_b'][0]).reshape(1, 256)
    m['gnorm'] = f(inputs['gla_out_norm'][0]).reshape(1, 128)
    m['norms'] = np.stack([f(inputs['norm_mix'][0]), f(inputs['norm_xattn'][0]), f(inputs['norm_mem'][0]),
                           f(inputs['norm_ffn'][0]), f(inputs['norm_final'])], 0)
    cw1 = np.stack([f(inputs['cmp_k_w1'][0]), f(inputs['cmp_v_w1'][0])], 0)
    m['cw1'] = np.ascontiguousarray(cw1.reshape(2, 32, 64, 128).transpose(0, 2, 1, 3))
    cpos = np.stack([f(inputs['cmp_pos_k'][0]), f(inputs['cmp_pos_v'][0])], 0)
    m['cpos'] = np.ascontiguousarray(cpos.transpose(0, 2, 1))
    m['cw2'] = np.stack([f(inputs['cmp_k_w2'][0]), f(inputs['cmp_v_w2'][0])], 0)
    m['w_out'] = f(inputs['w_out'][0])
    m['xa_w'] = np.stack([f(inputs['xa_wq'][0]), f(inputs['xa_wk'][0]), f(inputs['xa_wv'][0]), f(inputs['xa_wo'][0])], 0)
    m['pwq'] = f(inputs['peer_wq'][0])
    sk = f(inputs['peer_subkeys'][0])
    skd = np.zeros((8, 128, 256), np.float32)
    for h in range(8):
        for pp in range(2):
            skd[h, pp * 64:(pp + 1) * 64, pp * 128:(pp + 1) * 128] = sk[h, pp].T
    m['skd'] = skd
    m['downT'] = np.ascontiguousarray(f(inputs['peer_down'][0]).T)
    m['up'] = f(inputs['peer_up'][0])
    m['c_ident'] = np.eye(128, dtype=np.float32)
    s_ = np.arange(128)[:, None]; t_ = np.arange(128)[None, :]
    m['c_ucs'] = np.where(s_ <= t_, -1.0 / 16, 0.0).astype(np.float32)
    m['c_urev'] = np.where(s_ > t_, -1.0 / 16, 0.0).astype(np.float32)
    m['c_causal'] = (s_ <= t_).astype(np.float32)
    m['c_selu'] = np.stack([np.eye(128) * (1 - p), np.eye(128) * p], 0).astype(np.float32)
    m['c_pv'] = np.tile(np.array([[1.0 - p, float(p)]], np.float32), (128, 1))
    kk = np.arange(128)[:, None]; qq = np.arange(128)[None, :]
    selb = np.empty((2, 128, 15, 512), np.float32); winb = np.empty((2, 128, 6, 512), np.float32)
    for g in range(2):
        for mm_ in range(15):
            j = mm_ - 1 + p
            dist = 128 * j + qq - kk
            selb[g, :, mm_, :] = _bias_tile(rel_bias, g, dist, (dist >= 0) & (j >= 0))
        for mm_ in range(6):
            j = mm_ - 1 + p
            dist = 128 * j + qq - kk
            winb[g, :, mm_, :] = _bias_tile(rel_bias, g, dist, (dist >= 0) & (dist < 512) & (j >= 0))
    m['c_selb'] = selb; m['c_winb'] = winb
    cmpb = np.empty((2, meta['npair'], 128, 512), np.float32)
    for jj in range(NO):
        for c in range(min(NCC, cmp_nchunks(jj))):
            n = 128 * c + kk
            t = (2 * jj + p) * 128 + qq
            dist = t - (16 * n + 31)
            for g in range(2):
                cmpb[g, meta['pair_off'][jj] + c] = _bias_tile(rel_bias, g, dist, (dist >= 0) & (n < NCMP))
    m['c_cmpb'] = cmpb
    far = np.empty((2, 128, 4, 128), np.float32)
    for g in range(2):
        for h in range(4):
            far[g, :, h, :] = rel_bias[31, g * 4 + h]
    m['c_far'] = far.reshape(2, 128, 512)
    wimp = np.zeros((NCC * 128, 128), np.float32)
    for s in range(NS):
        for r in range(-1, 4):
            n = 4 * s + r
            lo = 16 * r
            ov = min(lo + 32, 64) - max(lo, 0)
            if 0 <= n < NCMP:
                wimp[n, s] += ov / 32.0
    m['c_wimp'] = wimp.reshape(NCC, 128, 128)
    m12 = np.zeros((NO, 128, 2, 128), np.float32)
    sid = np.arange(128)[None, :]
    for jj in range(NO):
        t = (2 * jj + p) * 128 + np.arange(128)[:, None]
        cur = t // 64
        visible = (sid * 64 <= t) & (sid < NS)
        f0 = (sid == 0); f1 = (sid == cur); f2 = (sid == cur - 1)
        forced = f0 | f1 | f2
        m12[jj, :, 0, :] = (visible & ~forced)
        add = np.where(~visible, -100.0 - sid, 0.0)
        add = np.where(f2, 100.0, add); add = np.where(f1, 101.0, add); add = np.where(f0 & (sid < NS), 102.0, add)
        m12[jj, :, 1, :] = add
    m['c_m12'] = m12
    ex = np.zeros((128, T), np.float32)
    ex[np.arange(T) // 64, np.arange(T)] = 1.0
    m['c_ex'] = ex.astype(NPBF)
    return m


_CACHE = {}


def kernel(**inputs):
    T = inputs['x'].shape[1]
    B = inputs['x'].shape[0]
    if T not in _CACHE:
        _CACHE[T] = build(T)
    nc, meta = _CACHE[T]
    in_maps = []
    for c in range(2 * B):
        in_maps.append(make_core_inputs(inputs, T, c // 2, c % 2, meta))
    res = run_bass_kernel_spmd(nc, in_maps, core_ids=list(range(2 * B)))
    NO = T // 256
    outp = np.empty((B, T // 128, 128, D), np.float32)
    for c in range(2 * B):
        o = np.asarray(res.results[c]["out"], dtype=np.float32).reshape(NO, 128, D)
        outp[c // 2, (c % 2)::2] = o
    return outp.reshape(B, T, D)
```

```python
import math
import numpy as np
import ml_dtypes
import concourse.bass as bass
import concourse.mybir as mybir
from concourse.bass_utils import run_bass_kernel_spmd
from contextlib import ExitStack

F32 = mybir.dt.float32
BF16 = mybir.dt.bfloat16
AF = mybir.ActivationFunctionType
ALU = mybir.AluOpType
AX = mybir.AxisListType
NPBF = ml_dtypes.bfloat16

D = 1024
NEG = -30000.0


class T:
    def __init__(self, h, name):
        self.h = h
        self.name = name
        self.w = None
        self.r = []
        self.psum = False
        self.dram = False

    def __getitem__(self, k):
        return self.h[k]


class Prog:
    def __init__(self, nc, n_dma_sems=48):
        self.nc = nc
        self.es = ExitStack()
        self.scopes = []
        self.eng = {'pe': nc.tensor, 'dve': nc.vector, 'act': nc.scalar,
                    'pool': nc.gpsimd, 'sp': nc.sync}
        self.sems = {}
        for k in self.eng:
            self.sems['e_' + k] = self.es.enter_context(nc.semaphore('e_' + k))
        self.cnt = {k: 0 for k in self.sems}
        self.ndma = n_dma_sems
        for i in range(n_dma_sems):
            key = 'd_%d' % i
            self.sems[key] = self.es.enter_context(nc.semaphore(key))
            self.cnt[key] = 0
        self.dma_rr = 0
        self.known = {k: {} for k in self.eng}
        self.ntile = 0
        self.ninst = 0

    def push(self):
        self.scopes.append(ExitStack())

    def pop(self):
        self.barrier()
        self.scopes.pop().close()

    def _stack(self):
        return self.scopes[-1] if self.scopes else self.es

    def sb(self, shape, dt, name=None):
        self.ntile += 1
        name = (name or 't') + '_%d' % self.ntile
        h = self._stack().enter_context(self.nc.sbuf_tensor(name, list(shape), dt))
        return T(h, name)

    def ps(self, shape, dt, name=None):
        self.ntile += 1
        name = (name or 'p') + '_%d' % self.ntile
        h = self._stack().enter_context(self.nc.psum_tensor(name, list(shape), dt))
        t = T(h, name)
        t.psum = True
        return t

    def dram(self, name, shape, dt, kind="Internal"):
        h = self.nc.dram_tensor(name, list(shape), dt, kind=kind).ap()
        t = T(h, name)
        t.dram = True
        return t

    def _deps(self, reads, writes, e=None):
        deps = []
        for t in reads:
            if t.w is not None:
                deps.append(t.w)
            if t.psum:
                deps.extend([tok for tok in t.r if tok[0] != 'e_' + str(e)])
        for t in writes:
            if t.w is not None:
                deps.append(t.w)
            deps.extend(t.r)
        return deps

    def _waits(self, e, deps):
        kn = self.known[e]
        need = {}
        for (k, v) in deps:
            if kn.get(k, 0) >= v:
                continue
            if need.get(k, 0) < v:
                need[k] = v
        for k, v in need.items():
            kn[k] = v
        return list(need.items())

    def _emit(self, e, waits, fn, inc):
        eng = self.eng[e]
        for (k, v) in waits:
            eng.wait_ge(self.sems[k], v)
        ins = fn(eng)
        ins.then_inc(self.sems[inc[0]], inc[1])
        self.ninst += 1

    def op(self, e, fn, reads=(), writes=()):
        deps = self._deps(reads, writes, e)
        key = 'e_' + e
        if e == 'pe':
            deps = [d for d in deps if d[0] != key]
        waits = self._waits(e, deps)
        self.cnt[key] += 1
        tok = (key, self.cnt[key])
        self._emit(e, waits, fn, (key, 1))
        for t in reads:
            t.r.append(tok)
        for t in writes:
            t.w = tok
            t.r = []
        return tok

    def dma(self, e, out_t, out_ap, in_t, in_ap, **kw):
        reads = [in_t]
        writes = [out_t]
        deps = self._deps(reads, writes)
        key = 'd_%d' % self.dma_rr
        self.dma_rr = (self.dma_rr + 1) % self.ndma
        if self.cnt[key] > 0:
            deps.append((key, self.cnt[key]))
        waits = self._waits(e, deps)
        self.cnt[key] += 16
        tok = (key, self.cnt[key])
        self._emit(e, waits, lambda eng: eng.dma_start(out=out_ap, in_=in_ap, **kw), (key, 16))
        in_t.r.append(tok)
        out_t.w = tok
        out_t.r = []
        return tok

    def barrier(self):
        allt = [(k, v) for k, v in self.cnt.items() if v > 0]
        for e in self.eng:
            for (k, v) in self._waits(e, allt):
                self.eng[e].wait_ge(self.sems[k], v)

    def finish(self):
        self.barrier()
        while self.scopes:
            self.scopes.pop().close()
        self.es.close()


ORIG = dict(gq=(0, 256), gk=(256, 512), gv=(512, 1024), gr=(1024, 1536), glr=(1536, 1552),
            nq=(1552, 2064), kc=(2064, 2192), vc=(2192, 2320), ks=(2320, 2448), vs=(2448, 2576),
            kw=(2576, 2704), vw=(2704, 2832), ng=(2832, 2856))
PERM_ORDER = ['gq', 'gk', 'gv', 'gr', 'nq', 'kc', 'vc', 'ks', 'vs', 'kw', 'vw', 'ng', 'glr']
NCOL = 2856


def cmp_nchunks(jj):
    return min(4, (16 * jj + 15 + 127) // 128)


def build(T, debug=False, upto=9, cut=99):
    NT = T // 128
    NO = NT // 2
    NS = T // 64
    TO = T // 2
    NCMP = (T - 32) // 16 + 1
    NCC = (NCMP + 127) // 128
    pair_off = []
    npair = 0
    for jj in range(NO):
        pair_off.append(npair)
        npair += min(NCC, cmp_nchunks(jj))

    nc = bass.Bass("TRN2", target_bir_lowering=False)
    P = Prog(nc)
    SK = "ExternalOutput" if debug else "Internal"

    def inp(name, shape, dt=F32):
        return P.dram(name, shape, dt, kind="ExternalInput")

    xb = inp("xb", [T, D]); xo = inp("xo", [TO, D]); memb = inp("mem", [256, D])
    w_in = inp("w_in", [D, NCOL]); gw2 = inp("gw2", [16, 256]); gb = inp("gb", [1, 256])
    gnorm = inp("gnorm", [1, 128])
    norms = inp("norms", [5, D])
    cw1 = inp("cw1", [2, 64, 32, 128]); cpos = inp("cpos", [2, 64, 32]); cw2 = inp("cw2", [2, 128, 64])
    w_out = inp("w_out", [D, D]); xa_w = inp("xa_w", [4, D, D])
    pwq = inp("pwq", [D, D]); skd = inp("skd", [8, 128, 256])
    downT = inp("downT", [D, 16384]); up = inp("up", [16384, D])
    c_ident = inp("c_ident", [128, 128]); c_ucs = inp("c_ucs", [128, 128]); c_urev = inp("c_urev", [128, 128])
    c_causal = inp("c_causal", [128, 128]); c_selu = inp("c_selu", [2, 128, 128]); c_pv = inp("c_pv", [128, 2])
    c_selb = inp("c_selb", [2, 128, 15, 512]); c_winb = inp("c_winb", [2, 128, 6, 512])
    c_cmpb = inp("c_cmpb", [2, npair, 128, 512])
    c_far = inp("c_far", [2, 128, 512])
    c_wimp = inp("c_wimp", [NCC, 128, 128]); c_m12 = inp("c_m12", [NO, 128, 2, 128])
    c_ex = inp("c_ex", [128, T], BF16)
    out = P.dram("out", [TO, D], F32, kind="ExternalOutput")

    d_nq = P.dram("d_nq", [T, 512], BF16, kind=SK)
    d_ng = P.dram("d_ng", [T, 24], F32, kind=SK)
    d_ogla = P.dram("d_ogla", [T, 512], BF16, kind=SK)
    d_kT = P.dram("d_kT", [4, 128, T], BF16, kind=SK)
    d_vs = P.dram("d_vs", [T, 128], BF16, kind=SK)
    d_vw = P.dram("d_vw", [T, 128], BF16, kind=SK)
    d_omix = P.dram("d_omix", [TO, D], BF16, kind=SK)
    d_h = P.dram("d_h", [TO, D], F32, kind=SK)
    d_hn3T = P.dram("d_hn3T", [128, 8, TO], BF16, kind=SK)
    d_route = P.dram("d_route", [TO, 2064], F32, kind=SK)
    d_downT = P.dram("d_downT", [D, 16384], BF16, kind="Internal")
    d_up = P.dram("d_up", [16384, D], BF16, kind="Internal")
    d_cmp = P.dram("d_cmp", [2, 64, 512], BF16, kind=SK)
    d_dbg = P.dram("d_dbg", [3, TO, 512], F32, kind=SK)
    d_imp = P.dram("d_imp", [2, TO, 128], F32, kind=SK)

    def mm(ot, o_ap, lt, l_ap, rt, r_ap, start=True, stop=True):
        P.op('pe', lambda e: e.matmul(o_ap, lhsT=l_ap, rhs=r_ap, start=start, stop=stop), [lt, rt], [ot])

    def tr(ot, o_ap, it, i_ap, idt):
        P.op('pe', lambda e: e.transpose(out=o_ap, in_=i_ap, identity=idt[:]), [it, idt], [ot])

    def act(ot, o_ap, it, i_ap, func, bias=None, scale=None, accum=None, rd=(), wr=()):
        kw = {}
        if bias is not None:
            kw['bias'] = bias
        if scale is not None:
            kw['scale'] = scale
        if accum is not None:
            kw['accum_out'] = accum
        P.op('act', lambda e: e.activation(out=o_ap, in_=i_ap, func=func, **kw), [it] + list(rd), [ot] + list(wr))

    def cp(eng, ot, o_ap, it, i_ap):
        if eng == 'act':
            P.op('act', lambda e: e.copy(out=o_ap, in_=i_ap), [it], [ot])
        else:
            P.op(eng, lambda e: e.tensor_copy(out=o_ap, in_=i_ap), [it], [ot])

    def tt(eng, ot, o_ap, at, a_ap, bt, b_ap, op):
        P.op(eng, lambda e: e.tensor_tensor(out=o_ap, in0=a_ap, in1=b_ap, op=op), [at, bt], [ot])

    def ts(eng, ot, o_ap, at, a_ap, s1, s2, op0, op1=None, rd=()):
        if op1 is None:
            P.op(eng, lambda e: e.tensor_scalar(out=o_ap, in0=a_ap, scalar1=s1, scalar2=None, op0=op0), [at] + list(rd), [ot])
        else:
            P.op(eng, lambda e: e.tensor_scalar(out=o_ap, in0=a_ap, scalar1=s1, scalar2=s2, op0=op0, op1=op1), [at] + list(rd), [ot])

    def stt(eng, ot, o_ap, at, a_ap, sc, bt, b_ap, op0, op1, rd=()):
        P.op(eng, lambda e: e.scalar_tensor_tensor(out=o_ap, in0=a_ap, scalar=sc, in1=b_ap, op0=op0, op1=op1),
             [at, bt] + list(rd), [ot])

    def memset(eng, t, ap, v):
        P.op(eng, lambda e: e.memset(ap, v), [], [t])

    def recip(ot, o_ap, it, i_ap):
        P.op('dve', lambda e: e.reciprocal(out=o_ap, in_=i_ap), [it], [ot])

    dmaq = ['sp', 'act', 'pool']
    dq = [0]

    dma_pol = {'load': ['sp'], 'store': ['pool']}

    def dma(ot, o_ap, it, i_ap, q=None, **kw):
        if q is None:
            qs = dma_pol['store'] if ot.dram else dma_pol['load']
            q = qs[dq[0] % len(qs)]
            dq[0] += 1
        return P.dma(q, ot, o_ap, it, i_ap, **kw)

    ident = P.sb([128, 128], BF16, "ident"); identf = P.sb([128, 128], F32, "identf")
    ones = P.sb([128, 128], BF16, "ones")
    pv = P.sb([128, 2], F32, "pv")
    nrm = P.sb([128, 5, D], F32, "nrm")
    dma(identf, identf[:], c_ident, c_ident[:])
    dma(pv, pv[:], c_pv, c_pv[:])
    for k in range(5):
        dma(nrm, nrm[:, k, :], norms, norms[k:k + 1, :].partition_broadcast(128))
    cp('dve', ident, ident[:], identf, identf[:])
    memset('pool', ones, ones[:], 1.0)
    eps_t = P.sb([128, 1], F32, "eps_t")
    memset('dve', eps_t, eps_t[:], 1e-6)
    kcmpT = [P.sb([128, 512], BF16, "kcmpT%d" % g) for g in range(2)]
    vcmp = [P.sb([128, 4, 65], BF16, "vcmp%d" % g) for g in range(2)]

    def rmsnorm(xt, x_ap, gain_k, hn, hn_ap, junk, ss, rstd, n=D, eps=1e-6):
        memset('dve', ss, ss[:], 0.0)
        act(junk, junk[:, 0:n], xt, x_ap, AF.Square, accum=ss[:], rd=[ss], wr=[ss])
        act(rstd, rstd[:], ss, ss[:], AF.Ln, scale=1.0 / n, bias=eps_t[:, 0:1], rd=[eps_t])
        act(rstd, rstd[:], rstd, rstd[:], AF.Exp, scale=-0.5)
        stt('dve', hn, hn_ap, xt, x_ap, rstd[:, 0:1], nrm, nrm[:, gain_k, 0:n], ALU.mult, ALU.mult, rd=[rstd])

    if upto >= 1:
        P.push()
        winb = P.sb([128, 8, NCOL], BF16, "winb")
        wst = [P.sb([128, NCOL], F32, "wst%d" % i) for i in range(2)]
        w_in_v = w_in[:].rearrange("(c p) n -> c p n", p=128)
        for c in range(8):
            dma(wst[c % 2], wst[c % 2][:], w_in, w_in_v[c])
            cp(['dve', 'pool'][c % 2], winb, winb[:, c, :], wst[c % 2], wst[c % 2][:])
        gw2t = P.sb([16, 256], F32, "gw2t"); gbt = P.sb([1, 256], F32, "gbt"); onesrow = P.sb([1, 128], F32, "onesrow")
        gnt = P.sb([128, 128], F32, "gnt")
        ucs = P.sb([128, 128], F32, "ucs"); urev = P.sb([128, 128], F32, "urev"); causal = P.sb([128, 128], F32, "causal")
        m16 = P.sb([128, 1], F32, "m16")
        dma(gw2t, gw2t[:], gw2, gw2[:]); dma(gbt, gbt[:], gb, gb[:])
        dma(gnt, gnt[:], gnorm, gnorm[0:1, :].partition_broadcast(128))
        dma(ucs, ucs[:], c_ucs, c_ucs[:]); dma(urev, urev[:], c_urev, c_urev[:]); dma(causal, causal[:], c_causal, c_causal[:])
        memset('dve', onesrow, onesrow[:], 1.0)
        memset('dve', m16, m16[:], -1.0 / 16)
        S = P.sb([128, 2, 128], F32, "S"); Sb = P.sb([128, 2, 128], BF16, "Sb")
        memset('dve', S, S[:], 0.0); memset('pool', Sb, Sb[:], 0.0)

        xt = [P.sb([128, D], F32, "xt%d" % i) for i in range(2)]
        junk = P.sb([128, D], BF16, "junk"); ss = P.sb([128, 1], F32, "ss"); rstd = P.sb([128, 1], F32, "rstd")
        hn = P.sb([128, D], BF16, "hn"); hnT = P.sb([128, 8, 128], BF16, "hnT")
        pt = P.ps([128, 8, 128], BF16, "pt")
        pz = [P.ps([128, 512], F32, "pz%d" % i) for i in range(3)]
        pg1 = P.ps([128, 512], F32, "pg1"); pg2 = P.ps([128, 512], F32, "pg2")
        po = P.ps([128, 512], F32, "po"); ptb = P.ps([128, 8, 128], BF16, "ptb")
        glr = P.sb([128, 16], F32, "glr"); glrT = P.sb([16, 128], F32, "glrT")
        e1 = P.sb([128, 256], F32, "e1"); L = P.sb([128, 256], F32, "L")
        eb = P.sb([128, 256], F32, "eb"); enb = P.sb([128, 256], F32, "enb"); ec = P.sb([128, 256], F32, "ec")
        ebl = P.sb([128, 2], F32, "ebl")
        qk = P.sb([128, 3, 256], BF16, "qk")
        qkT = P.sb([128, 4, 128], BF16, "qkT")
        vv = P.sb([128, 512], BF16, "vv"); sg = P.sb([128, 512], F32, "sg")
        AT = P.sb([128, 128], BF16, "AT")
        ssq = P.sb([128, 4], F32, "ssq"); rs4 = P.sb([128, 4], F32, "rs4"); tmpn = P.sb([128, 128], F32, "tmpn")
        junk2 = P.sb([128, 128], F32, "junk2")
        og = P.sb([128, 512], BF16, "og"); otmp4 = P.sb([128, 512], F32, "otmp4")
        nqs = P.sb([128, 512], BF16, "nqs"); ngs = P.sb([128, 24], F32, "ngs")
        kk = P.sb([128, 4, 128], BF16, "kk"); kkT = P.sb([128, 4, 128], BF16, "kkT")
        vsw = P.sb([128, 2, 128], BF16, "vsw")
        cast_jobs = []
        if upto >= 4:
            stg = [P.sb([128, 4096], F32, "stg%d" % i) for i in range(2)]
            stb = [P.sb([128, 4096], BF16, "stb%d" % i) for i in range(2)]
            for r in range(8):
                for c in range(4):
                    cast_jobs.append((downT, downT[r * 128:(r + 1) * 128, c * 4096:(c + 1) * 4096],
                                      d_downT, d_downT[r * 128:(r + 1) * 128, c * 4096:(c + 1) * 4096]))
            sv = up[:].rearrange("(a p f) d -> a p (f d)", p=128, f=4)
            dv = d_up[:].rearrange("(a p f) d -> a p (f d)", p=128, f=4)
            for a in range(32):
                cast_jobs.append((up, sv[a], d_up, dv[a]))
        cj = [0]

        def cast_job():
            k_ = cj[0]
            if k_ < len(cast_jobs):
                src_t, s_ap, dst_t, d_ap = cast_jobs[k_]
                P.dma('pool', stg[k_ % 2], stg[k_ % 2][:], src_t, s_ap)
            if 1 <= k_ <= len(cast_jobs):
                src_t, s_ap, dst_t, d_ap = cast_jobs[k_ - 1]
                a, b_ = stg[(k_ - 1) % 2], stb[(k_ - 1) % 2]
                for q_ in range(4):
                    cp('act', b_, b_[:, q_ * 1024:(q_ + 1) * 1024], a, a[:, q_ * 1024:(q_ + 1) * 1024])
                P.dma('pool', dst_t, d_ap, b_, b_[:])
            cj[0] += 1
        cA, cB, cC, cD, cE, cF = 0, 512, 1024, 1536, 2048, 2560
        hn2_ = [hn, P.sb([128, D], BF16, "hn_b")]; hnT2_ = [hnT, P.sb([128, 8, 128], BF16, "hnT_b")]
        ss2_ = [ss, P.sb([128, 1], F32, "ss_b")]; rstd2_ = [rstd, P.sb([128, 1], F32, "rstd_b")]

        def front(i):
            x_t = xt[i % 2]
            dma(x_t, x_t[:], xb, xb[i * 128:(i + 1) * 128, :])
            rmsnorm(x_t, x_t[:], 0, hn2_[i % 2], hn2_[i % 2][:], junk, ss2_[i % 2], rstd2_[i % 2])
            for c in range(8):
                tr(pt, pt[:, c, :], hn2_[i % 2], hn2_[i % 2][:, c * 128:(c + 1) * 128], ident)
            cp('act', hnT2_[i % 2], hnT2_[i % 2][:], pt, pt[:])
        front(0)
        for i in range(NT):
            hnT = hnT2_[i % 2]

            def zgroup(pz_t, c0, n):
                for c in range(8):
                    mm(pz_t, pz_t[:, 0:n], hnT, hnT[:, c, :], winb, winb[:, c, c0:c0 + n], start=(c == 0), stop=(c == 7))
            zgroup(pz[0], cF, 296)
            cp('act', kk, kk[:, 3, :], pz[0], pz[0][:, 0:128])
            cp('act', vsw, vsw[:, 1, :], pz[0], pz[0][:, 128:256])
            cp('act', glr, glr[:], pz[0], pz[0][:, 280:296])
            act(ngs, ngs[:], pz[0], pz[0][:, 256:280], AF.Exp, scale=-1.0)
            ts('dve', ngs, ngs[:], ngs, ngs[:], 1.0, None, ALU.add)
            recip(ngs, ngs[:], ngs, ngs[:])
            dma(d_ng, d_ng[i * 128:(i + 1) * 128, :], ngs, ngs[:])
            zgroup(pz[1], cE, 512)
            cp('act', kk, kk[:, 0:3, :], pz[1], pz[1][:, 0:384].rearrange("p (a b) -> p a b", a=3))
            cp('dve', vsw, vsw[:, 0, :], pz[1], pz[1][:, 384:512])
            dma(d_vs, d_vs[i * 128:(i + 1) * 128, :], vsw, vsw[:, 0, :])
            dma(d_vw, d_vw[i * 128:(i + 1) * 128, :], vsw, vsw[:, 1, :])
            for a in range(4):
                tr(ptb, ptb[:, a, :], kk, kk[:, a, :], ident)
            cp('act', kkT, kkT[:], ptb, ptb[:, 0:4, :])
            dma(d_kT, d_kT[:, :, i * 128:(i + 1) * 128].rearrange("a p t -> p a t"), kkT, kkT[:])
            zgroup(pz[2], cD, 512)
            P.op('act', lambda e, pzt=pz[2]: e.mul(out=nqs[:], in_=pzt[:], mul=0.125), [pz[2]], [nqs])
            dma(d_nq, d_nq[i * 128:(i + 1) * 128, :], nqs, nqs[:])
            tr(pg1, pg1[0:16, 0:128], glr, glr[:], identf)
            cp('dve', glrT, glrT[:], pg1, pg1[0:16, 0:128])
            mm(pg1, pg1[:, 256:512], glrT, glrT[:], gw2t, gw2t[:], start=True, stop=False)
            mm(pg1, pg1[:, 256:512], onesrow, onesrow[:], gbt, gbt[:], start=False, stop=True)
            zgroup(pz[0], cA, 512)
            zgroup(pz[1], cB, 512)
            zgroup(pz[2], cC, 512)
            act(e1, e1[:], pg1, pg1[:, 256:512], AF.Exp, scale=-1.0)
            act(L, L[:], e1, e1[:], AF.Ln, bias=1.0)
            cp('act', vv, vv[:], pz[1], pz[1][:])
            act(sg, sg[:], pz[2], pz[2][:], AF.Exp, scale=-1.0)
            act(sg, sg[:], sg, sg[:], AF.Ln, bias=1.0)
            act(sg, sg[:], sg, sg[:], AF.Exp, scale=-1.0)
            tt('dve', sg, sg[:], sg, sg[:], pz[2], pz[2][:], ALU.mult)
            tt('pool', sg, sg[:].rearrange("p (h d) -> p h d", h=4), sg, sg[:].rearrange("p (h d) -> p h d", h=4),
               gnt, gnt[:].unsqueeze(1).to_broadcast([128, 4, 128]), ALU.mult)
            mm(pg2, pg2[:, 0:256], ucs, ucs[:], L, L[:])
            mm(pg2, pg2[:, 256:512], urev, urev[:], L, L[:])
            for hp in range(2):
                mm(pg1, pg1[:, hp:hp + 1], L, L[:, hp * 128:(hp + 1) * 128], m16, m16[:])
            act(eb, eb[:], pg2, pg2[:, 0:256], AF.Exp)
            act(enb, enb[:], pg2, pg2[:, 0:256], AF.Exp, scale=-1.0)
            act(ec, ec[:], pg2, pg2[:, 256:512], AF.Exp)
            act(ebl, ebl[:], pg1, pg1[:, 0:2], AF.Exp)
            stt('dve', qk, qk[:, 0, :], pz[0], pz[0][:, 0:256], 0.125, eb, eb[:], ALU.mult, ALU.mult)
            tt('dve', qk, qk[:, 1, :], pz[0], pz[0][:, 256:512], enb, enb[:], ALU.mult)
            tt('dve', qk, qk[:, 2, :], pz[0], pz[0][:, 256:512], ec, ec[:], ALU.mult)
            for a in range(4):
                tr(ptb, ptb[:, 4 + a, :], qk, qk[:, a // 2, (a % 2) * 128:(a % 2 + 1) * 128], ident)
            cp('act', qkT, qkT[:], ptb, ptb[:, 4:8, :])
            if i + 1 < NT:
                front(i + 1)
            memset('dve', ssq, ssq[:], 0.0)
            for h in range(4):
                hp, hh = h // 2, h % 2
                pr = slice(hh * 64, hh * 64 + 64)
                mm(pg2, pg2[:, 0:128], qkT, qkT[pr, 2 + hp, :], qkT, qkT[pr, hp, :])
                tt('dve', AT, AT[:], pg2, pg2[:, 0:128], causal, causal[:], ALU.mult)
                o_ap = po[:, h * 128:(h + 1) * 128]
                mm(po, o_ap, AT, AT[:], vv, vv[:, h * 128:(h + 1) * 128], start=True, stop=False)
                mm(po, o_ap, qkT, qkT[pr, hp, :], Sb, Sb[pr, hp, :], start=False, stop=True)
                mm(pg2, pg2[:, 128:256], qk, qk[:, 2, hp * 128:(hp + 1) * 128], vv, vv[:, h * 128:(h + 1) * 128])
                stt('dve', Sb, Sb[pr, hp, :], S, S[pr, hp, :], ebl[pr, hp:hp + 1], pg2, pg2[pr, 128:256], ALU.mult, ALU.add, rd=[ebl])
                stt('dve', S, S[pr, hp, :], S, S[pr, hp, :], ebl[pr, hp:hp + 1], pg2, pg2[pr, 128:256], ALU.mult, ALU.add, rd=[ebl])
                act(junk2, junk2[:], po, o_ap, AF.Square, accum=ssq[:, h:h + 1], rd=[ssq], wr=[ssq])
            act(rs4, rs4[:], ssq, ssq[:], AF.Ln, scale=1.0 / 128, bias=eps_t[:, 0:1], rd=[eps_t])
            act(rs4, rs4[:], rs4, rs4[:], AF.Exp, scale=-0.5)
            tt('dve', otmp4, otmp4[:], po, po[:], sg, sg[:], ALU.mult)
            tt('dve', og, og[:].rearrange("p (h d) -> p h d", h=4), otmp4, otmp4[:].rearrange("p (h d) -> p h d", h=4),
               rs4, rs4[:].unsqueeze(2).to_broadcast([128, 4, 128]), ALU.mult)
            dma(d_ogla, d_ogla[i * 128:(i + 1) * 128, :], og, og[:])
            for _ in range((len(cast_jobs) + NT - 1) // NT):
                cast_job()
        while cast_jobs and cj[0] <= len(cast_jobs):
            cast_job()
        P.pop()

        P.push()
        w1f = P.sb([64, 32, 128], F32, "w1f"); w1b = P.sb([64, 32, 128], BF16, "w1b")
        posf = P.sb([64, 32], F32, "posf"); posb = P.sb([64, 32], BF16, "posb")
        w2f = P.sb([128, 64], F32, "w2f"); w2b = P.sb([128, 64], BF16, "w2b")
        kTg = P.sb([64, T], BF16, "kTg")
        ph = P.ps([128, 512], F32, "ph"); pb = P.ps([128, 512], F32, "pb"); pc = P.ps([128, 512], F32, "pc")
        bias_h = P.sb([128, 1], F32, "bias_h")
        H = P.sb([128, 512], BF16, "H")
        for g in range(2):
            memset('dve', kcmpT[g], kcmpT[g][:], 0.0)
            memset('dve', vcmp[g], vcmp[g][:], 0.0)
            memset('dve', vcmp[g], vcmp[g][:, :, 64:65], 1.0)
        for kv in range(2):
            dma(w1f, w1f[:], cw1, cw1[kv]); dma(posf, posf[:], cpos, cpos[kv]); dma(w2f, w2f[:], cw2, cw2[kv])
            cp('dve', w1b, w1b[:], w1f, w1f[:]); cp('dve', posb, posb[:], posf, posf[:]); cp('dve', w2b, w2b[:], w2f, w2f[:])
            for l in range(32):
                mm(pb, pb[:, 0:1], w1b, w1b[:, l, :], posb, posb[:, l:l + 1], start=(l == 0), stop=(l == 31))
            cp('dve', bias_h, bias_h[:], pb, pb[:, 0:1])
            for g in range(2):
                dma(kTg, kTg[:], d_kT, d_kT[kv, g * 64:(g + 1) * 64, :])
                for l in range(32):
                    mm(ph, ph[:, 0:NCMP], w1b, w1b[:, l, :], kTg, kTg[:, l:l + 16 * (NCMP - 1) + 1:16],
                       start=(l == 0), stop=(l == 31))
                memset('dve', H, H[:], 0.0)
                act(H, H[:, 0:NCMP], ph, ph[:, 0:NCMP], AF.Gelu_apprx_tanh, bias=bias_h[:, 0:1], rd=[bias_h])
                if kv == 0:
                    mm(pc, pc[0:64, 0:NCMP], w2b, w2b[:], H, H[:, 0:NCMP])
                    cp('act', kcmpT[g], kcmpT[g][0:64, 0:NCMP], pc, pc[0:64, 0:NCMP])
                    if debug:
                        dma(d_cmp, d_cmp[g], kcmpT[g], kcmpT[g][0:64, :])
                else:
                    for c in range(NCC):
                        mm(pc, pc[:, c * 64:(c + 1) * 64], H, H[:, c * 128:(c + 1) * 128], w2b, w2b[:])
                    cp('act', vcmp[g], vcmp[g][:, 0:NCC, 0:64], pc, pc[:, 0:NCC * 64].rearrange("p (c d) -> p c d", d=64))
        P.pop()

    if upto >= 2:
        P.push()
        dma_pol['load'] = ['sp']; dma_pol['store'] = ['sp']
        selu = P.sb([128, 2, 128], BF16, "selu"); seluf = P.sb([128, 2, 128], F32, "seluf")
        dma(seluf, seluf[:], c_selu, c_selu[:].rearrange("a p t -> p a t"))
        cp('dve', selu, selu[:], seluf, seluf[:])
        exm = P.sb([128, T], BF16, "exm")
        dma(exm, exm[:], c_ex, c_ex[:])
        wimpf = P.sb([128, NCC, 128], F32, "wimpf"); wimp = P.sb([128, NCC, 128], BF16, "wimp")
        dma(wimpf, wimpf[:], c_wimp, c_wimp[:].rearrange("c p s -> p c s"))
        cp('dve', wimp, wimp[:], wimpf, wimpf[:])
        ksT = P.sb([128, T], BF16, "ksT"); kwT = P.sb([128, T], BF16, "kwT")
        memset('dve', ksT, ksT[64:128, :], 0.0)
        memset('pool', kwT, kwT[64:128, :], 0.0)
        memset('dve', ksT, ksT[64:65, :], 1.0)
        farf = P.sb([128, 512], F32, "farf")
        vs = P.sb([128, NT, 65], BF16, "vs"); vw = P.sb([128, NT, 65], BF16, "vw")
        selb = P.sb([128, 15, 512], BF16, "selb"); winb2 = P.sb([128, 6, 512], BF16, "winb2")
        bst = [P.sb([128, 512], F32, "bst%d" % i) for i in range(2)]
        cbt = [P.sb([128, 512], BF16, "cbt%d" % i) for i in range(2)]
        qrows_ = [P.sb([128, 2, 256], BF16, "qrows%d" % i) for i in range(2)]; grows_ = [P.sb([128, 2, 24], F32, "grows%d" % i) for i in range(2)]
        gown_ = [P.sb([128, 12], F32, "gown%d" % i) for i in range(2)]; gtmp = P.sb([128, 12], F32, "gtmp")
        qT_ = [P.sb([128, 4, 128], BF16, "qT%d" % i) for i in range(2)]
        for i_ in range(2):
            memset('dve', qT_[i_], qT_[i_][64:128, :, :], 0.0)
        Ec = [P.sb([128, 4, 128], BF16, "Ec%d" % i) for i in range(4)]
        Eb = [P.sb([128, 4, 128], BF16, "Eb%d" % i) for i in range(3)]
        m12_ = [P.sb([128, 2, 128], F32, "m12_%d" % i) for i in range(2)]
        imp = P.sb([128, 128], F32, "imp"); imp2 = P.sb([128, 128], F32, "imp2"); m8 = P.sb([128, 16], F32, "m8")
        mk = P.sb([128, 128], BF16, "mk"); maskT4 = P.sb([128, 4, 128], BF16, "maskT4")
        rden = P.sb([128, 4], F32, "rden"); coef = P.sb([128, 4], F32, "coef")
        oacc = P.sb([128, 4, 64], F32, "oacc"); otmp = P.sb([128, 4, 64], F32, "otmp"); onsa = P.sb([128, 256], BF16, "onsa")
        pq = P.ps([64, 4, 128], F32, "pq")
        psc = [P.ps([128, 4, 128], F32, "psc%d" % i) for i in range(2)]
        pn = P.ps([128, 4, 128], F32, "pn")
        pnT = P.ps([65, 4, 128], F32, "pnT")
        pnT2 = P.ps([65, 4, 128], F32, "pnT2")
        numTs = P.sb([65, 4, 128], F32, "numTs")
        pimp = P.ps([128, 4, 128], F32, "pimp")
        pmt = P.ps([128, 128], BF16, "pmt")
        for g in range(2):
            dma(ksT, ksT[0:64, :], d_kT, d_kT[2, g * 64:(g + 1) * 64, :])
            dma(farf, farf[:], c_far, c_far[g])
            for i_ in range(2):
                cp('dve', qT_[i_], qT_[i_][64:65, :, :], farf, farf[64:65, :].rearrange("p (h q) -> p h q", h=4))
            dma(kwT, kwT[0:64, :], d_kT, d_kT[3, g * 64:(g + 1) * 64, :])
            for n0 in range(0, NT, 16):
                n1 = min(NT, n0 + 16)
                dma(vs, vs[:, n0:n1, 0:64], d_vs, d_vs[n0 * 128:n1 * 128, g * 64:(g + 1) * 64].rearrange("(n p) c -> p n c", p=128))
                dma(vw, vw[:, n0:n1, 0:64], d_vw, d_vw[n0 * 128:n1 * 128, g * 64:(g + 1) * 64].rearrange("(n p) c -> p n c", p=128))
            memset('dve', vs, vs[:, :, 64:65], 1.0)
            memset('dve', vw, vw[:, :, 64:65], 1.0)
            k = 0
            for m in range(15):
                dma(bst[k % 2], bst[k % 2][:], c_selb, c_selb[g, :, m, :])
                tt('pool', selb, selb[:, m, :], bst[k % 2], bst[k % 2][:], farf, farf[:], ALU.subtract); k += 1
            for m in range(6):
                dma(bst[k % 2], bst[k % 2][:], c_winb, c_winb[g, :, m, :])
                cp('pool', winb2, winb2[:, m, :], bst[k % 2], bst[k % 2][:]); k += 1
            ne = 0
            ne_ = [0]
            for jj in range(NO):
                qrows, grows, gown, qT, m12 = qrows_[jj % 2], grows_[jj % 2], gown_[jj % 2], qT_[jj % 2], m12_[jj % 2]

                def load_q(j2):
                    dma(qrows_[j2 % 2], qrows_[j2 % 2][:], d_nq, d_nq[j2 * 256:(j2 + 1) * 256, g * 256:(g + 1) * 256].rearrange("(u p) c -> p u c", p=128))
                    dma(grows_[j2 % 2], grows_[j2 % 2][:], d_ng, d_ng[j2 * 256:(j2 + 1) * 256, :].rearrange("(u p) c -> p u c", p=128))
                    dma(m12_[j2 % 2], m12_[j2 % 2][:], c_m12, c_m12[j2])
                if jj == 0:
                    load_q(0)
                if jj + 1 < NO:
                    load_q(jj + 1)
                for h in range(4):
                    for u in range(2):
                        mm(pq, pq[:, h, :], qrows, qrows[:, u, h * 64:(h + 1) * 64], selu, selu[:, u, :], start=(u == 0), stop=(u == 1))
                cp('act', qT, qT[0:64], pq, pq[:])
                ts('dve', gtmp, gtmp[:], grows, grows[:, 0, g * 12:(g + 1) * 12], pv[:, 0:1], None, ALU.mult, rd=[pv])
                stt('dve', gown, gown[:], grows, grows[:, 1, g * 12:(g + 1) * 12], pv[:, 1:2], gtmp, gtmp[:], ALU.mult, ALU.add, rd=[pv])
                gv3 = gown[:].rearrange("p (h k) -> p h k", k=3)
                qT_all = qT[:].rearrange("p h q -> p (h q)")
                qT_aug = qT_all

                def finish_branch(pn, br, first):
                    ts('dve', rden, rden[:], pn, pn[:, :, 64], 1e-30, None, ALU.max)
                    recip(rden, rden[:], rden, rden[:])
                    tt('dve', coef, coef[:], rden, rden[:], gown, gv3[:, :, br], ALU.mult)
                    dst = oacc if first else otmp
                    tt('dve', dst, dst[:], pn, pn[:, :, 0:64], coef, coef[:].unsqueeze(2).to_broadcast([128, 4, 64]), ALU.mult)
                    if not first:
                        tt('pool', oacc, oacc[:], oacc, oacc[:], otmp, otmp[:], ALU.add)
                    if debug:
                        dma(d_dbg, d_dbg[br, jj * 128:(jj + 1) * 128, g * 256:(g + 1) * 256], oacc, oacc[:].rearrange("p h d -> p (h d)"))

                def back_T(src):
                    cp('act', numTs, numTs[:], src, src[:])
                    for h in range(4):
                        P.op('pe', lambda e, h=h: e.transpose(out=pn[:, h, 0:65], in_=numTs[:, h, :], identity=identf[0:65, 0:65]), [numTs, identf], [pn])

                ncc = min(NCC, cmp_nchunks(jj))
                for c in range(ncc):
                    pi = pair_off[jj] + c
                    dma(bst[k % 2], bst[k % 2][:], c_cmpb, c_cmpb[g, pi])
                    cp('pool', cbt[k % 2], cbt[k % 2][:], bst[k % 2], bst[k % 2][:])
                    sc = psc[ne % 2]; ne += 1
                    sc_all = sc[:].rearrange("p h q -> p (h q)")
                    mm(sc, sc_all, kcmpT[g], kcmpT[g][:, c * 128:(c + 1) * 128], qT, qT_all, start=True, stop=False)
                    mm(sc, sc_all, ident, ident[:], cbt[k % 2], cbt[k % 2][:], start=False, stop=True)
                    k += 1
                    act(Ec[c], Ec[c][:], sc, sc[:], AF.Exp)
                for h in range(4):
                    for c in range(ncc):
                        mm(pn, pn[:, h, 0:65], Ec[c], Ec[c][:, h, :], vcmp[g], vcmp[g][:, c, :], start=(c == 0), stop=(c == ncc - 1))
                    for c in range(ncc):
                        mm(pimp, pimp[:, h, :], Ec[c], Ec[c][:, h, :], wimp, wimp[:, c, :], start=(c == 0), stop=(c == ncc - 1))
                nk = 2 * jj + 2
                k0 = max(0, 2 * jj - 4)
                bufs = {}

                def score(kind, kc, n):
                    sc = psc[ne_[0] % 2]; E = Eb[ne_[0] % 3]; ne_[0] += 1
                    bufs[n] = E
                    sc_all = sc[:].rearrange("p h q -> p (h q)")
                    if kind == 's':
                        m = 2 * jj + 1 - kc
                        mm(sc, sc_all, ksT, ksT[:, kc * 128:(kc + 1) * 128], qT, qT_aug, start=True, stop=False)
                        if m < 14:
                            mm(sc, sc_all, ident, ident[:], selb, selb[:, m, :], start=False, stop=False)
                        mm(sc, sc_all, exm, exm[:, kc * 128:(kc + 1) * 128], maskT4, mT_all, start=False, stop=True)
                    else:
                        m = 2 * jj + 1 - kc
                        mm(sc, sc_all, kwT, kwT[:, kc * 128:(kc + 1) * 128], qT, qT_all, start=True, stop=False)
                        mm(sc, sc_all, ident, ident[:], winb2, winb2[:, m, :], start=False, stop=True)
                    act(E, E[:], sc, sc[:], AF.Exp)

                def pvs(kind, kc, n):
                    E = bufs.pop(n)
                    if kind == 's':
                        mm(pnT, pnT[:].rearrange("p h q -> p (h q)"), vs, vs[:, kc, :], E, E[:].rearrange("p h q -> p (h q)"), start=(kc == 0), stop=(kc == nk - 1))
                    else:
                        mm(pnT2, pnT2[:].rearrange("p h q -> p (h q)"), vw, vw[:, kc, :], E, E[:].rearrange("p h q -> p (h q)"), start=(kc == k0), stop=(kc == nk - 1))

                def run_items(items):
                    NI = len(items)
                    for n in range(NI + 1):
                        if n < NI:
                            score(items[n][0], items[n][1], n)
                        if n >= 1:
                            pvs(items[n - 1][0], items[n - 1][1], n - 1)
                mT_all = maskT4[:].rearrange("p h q -> p (h q)")
                run_items([('w', kc) for kc in range(k0, nk)])
                finish_branch(pn, 0, True)
                for h in range(4):
                    if h == 0:
                        ts('dve', imp, imp[:], pimp, pimp[:, 0, :], rden[:, 0:1], None, ALU.mult, rd=[rden])
                    else:
                        stt('dve', imp, imp[:], pimp, pimp[:, h, :], rden[:, h:h + 1], imp, imp[:], ALU.mult, ALU.add, rd=[rden])
                tt('dve', imp, imp[:], imp, imp[:], m12, m12[:, 0, :], ALU.mult)
                tt('dve', imp, imp[:], imp, imp[:], m12, m12[:, 1, :], ALU.add)
                if debug:
                    dma(d_imp, d_imp[g, jj * 128:(jj + 1) * 128, :], imp, imp[:])
                P.op('dve', lambda e: e.max(out=m8[:, 0:8], in_=imp[:]), [imp], [m8])
                P.op('dve', lambda e: e.match_replace(out=imp2[:], in_to_replace=m8[:, 0:8], in_values=imp[:], imm_value=-1e30), [imp, m8], [imp2])
                P.op('dve', lambda e: e.max(out=m8[:, 8:16], in_=imp2[:]), [imp2], [m8])
                ts('dve', mk, mk[:], imp, imp[:], m8[:, 15:16], 1.0, ALU.is_ge, ALU.subtract, rd=[m8])
                tr(pmt, pmt[:], mk, mk[:], ident)
                P.op('act', lambda e: e.mul(out=maskT4[:], in_=pmt[:].unsqueeze(1).to_broadcast([128, 4, 128]), mul=30000.0), [pmt], [maskT4])
                run_items([('s', kc) for kc in range(nk)])
                back_T(pnT)
                finish_branch(pn, 1, False)
                back_T(pnT2)
                finish_branch(pn, 2, False)
                cp('act', onsa, onsa[:], oacc, oacc[:].rearrange("p h d -> p (h d)"))
                dma(d_omix, d_omix[jj * 128:(jj + 1) * 128, 512 + g * 256:512 + (g + 1) * 256], onsa, onsa[:])
        P.pop()

    if upto >= 3:
        P.push()
        dma_pol['load'] = ['sp']; dma_pol['store'] = ['pool']
        woutb = P.sb([128, 8, D], BF16, "woutb"); wqb = P.sb([128, 8, D], BF16, "wqb"); wob = P.sb([128, 8, D], BF16, "wob")
        pwqb = P.sb([128, 8, D], BF16, "pwqb")
        skb = P.sb([128, 8, 256], BF16, "skb")
        kTm = P.sb([128, 8, 256], BF16, "kTm")
        vm = P.sb([128, 2, 4, 257], BF16, "vm")
        xt = [P.sb([128, D], F32, "x3_%d" % i) for i in range(2)]
        junk = P.sb([128, D], BF16, "junk3"); ss = P.sb([128, 1], F32, "ss3"); rstd = P.sb([128, 1], F32, "rstd3")
        hn = P.sb([128, D], BF16, "hn3"); hnT = P.sb([128, 8, 128], BF16, "hnT3")
        pt = P.ps([128, 8, 128], BF16, "pt3")
        pa = [P.ps([128, 512], F32, "pa%d" % i) for i in range(4)]
        pxa = P.ps([128, 2, 512], F32, "pxa")
        P.push()
        wst = [P.sb([128, D], F32, "wst3_%d" % i) for i in range(2)]
        wtmp = P.sb([128, 8, D], BF16, "wtmp"); skf = P.sb([128, 8, 256], F32, "skf")
        memT = P.sb([128, 8, 256], BF16, "memT")
        k = [0]

        def load_w(dst, src_t, src_ap3):
            for c in range(8):
                a = wst[k[0] % 2]
                dma(a, a[:], src_t, src_ap3[c])
                cp(['dve', 'pool'][k[0] % 2], dst, dst[:, c, :], a, a[:]); k[0] += 1
        load_w(woutb, w_out, w_out[:].rearrange("(c p) n -> c p n", p=128))
        load_w(wqb, xa_w, xa_w[0].rearrange("(c p) n -> c p n", p=128))
        load_w(wob, xa_w, xa_w[3].rearrange("(c p) n -> c p n", p=128))
        load_w(pwqb, pwq, pwq[:].rearrange("(c p) n -> c p n", p=128))
        dma(skf, skf[:], skd, skd[:].rearrange("c p n -> p c n"))
        cp('dve', skb, skb[:], skf, skf[:])
        memset('dve', vm, vm[:, :, :, 256:257], 1.0)
        for mc in range(2):
            x_t = xt[mc % 2]
            dma(x_t, x_t[:], memb, memb[mc * 128:(mc + 1) * 128, :])
            rmsnorm(x_t, x_t[:], 2, hn, hn[:], junk, ss, rstd)
            for c in range(8):
                tr(pt, pt[:, c, :], hn, hn[:, c * 128:(c + 1) * 128], ident)
            cp('act', memT, memT[:, :, mc * 128:(mc + 1) * 128], pt, pt[:])
        load_w(wtmp, xa_w, xa_w[1].rearrange("(c p) n -> c p n", p=128))
        for oc in range(8):
            for c in range(8):
                mm(pa[0], pa[0][:, 0:256], wtmp, wtmp[:, c, oc * 128:(oc + 1) * 128], memT, memT[:, c, :], start=(c == 0), stop=(c == 7))
            cp('act', kTm, kTm[:, oc, :], pa[0], pa[0][:, 0:256])
        load_w(wtmp, xa_w, xa_w[2].rearrange("(c p) n -> c p n", p=128))
        for mc in range(2):
            for half in range(2):
                for c in range(8):
                    mm(pa[half], pa[half][:], memT, memT[:, c, mc * 128:(mc + 1) * 128], wtmp, wtmp[:, c, half * 512:(half + 1) * 512], start=(c == 0), stop=(c == 7))
                cp('act', vm, vm[:, mc, half * 2:half * 2 + 2, 0:256], pa[half], pa[half][:].rearrange("p (h d) -> p h d", d=256))
        P.pop()
        og2 = P.sb([128, 2, 512], BF16, "og2"); omx = P.sb([128, D], BF16, "omx"); otm = P.sb([128, 512], F32, "otm")
        h1 = P.sb([128, D], F32, "h1"); qTx = P.sb([128, 8, 128], BF16, "qTx")
        Ex = P.sb([128, 2, 4, 128], BF16, "Ex"); rdx = P.sb([128, 4], F32, "rdx")
        oxa = P.sb([128, D], BF16, "oxa")
        hn3T = P.sb([128, 8, 128], BF16, "hn3Ts"); qpT = P.sb([128, 8, 128], BF16, "qpT")
        sc_ = [P.sb([128, 16, 128], F32, "scr%d" % i) for i in range(2)]; ab_ = [P.sb([128, 16, 128], F32, "ab%d" % i) for i in range(2)]
        negm = P.sb([128, 16], F32, "negm"); t16 = P.sb([128, 16, 16], F32, "t16"); scr2_ = [P.sb([128, 128], F32, "scr2_%d" % i) for i in range(4)]
        candall = P.sb([128, 8, 256], F32, "candall"); cand2_ = [P.sb([128, 256], F32, "cand2_%d" % i) for i in range(4)]; c16 = P.sb([128, 8, 16], F32, "c16")
        route = P.sb([128, 16], F32, "route"); zs = P.sb([128, 8], F32, "zs")
        memset('dve', route, route[:], 0.0)
        def Xgen(jj):
            sc = sc_[jj % 2]
            x_t = xt[jj % 2]
            dma(x_t, x_t[:], xo, xo[jj * 128:(jj + 1) * 128, :])
            dma(og2, og2[:], d_ogla, d_ogla[jj * 256:(jj + 1) * 256, :].rearrange("(u p) c -> p u c", p=128))
            dma(omx, omx[:, 512:1024], d_omix, d_omix[jj * 128:(jj + 1) * 128, 512:1024])
            ts('dve', otm, otm[:], og2, og2[:, 0, :], pv[:, 0:1], None, ALU.mult, rd=[pv])
            stt('dve', omx, omx[:, 0:512], og2, og2[:, 1, :], pv[:, 1:2], otm, otm[:], ALU.mult, ALU.add, rd=[pv])
            for c in range(8):
                tr(pt, pt[:, c, :], omx, omx[:, c * 128:(c + 1) * 128], ident)
            cp('act', hnT, hnT[:], pt, pt[:])
            for half in range(2):
                for c in range(8):
                    mm(pa[half], pa[half][:], hnT, hnT[:, c, :], woutb, woutb[:, c, half * 512:(half + 1) * 512], start=(c == 0), stop=(c == 7))
                tt('dve', h1, h1[:, half * 512:(half + 1) * 512], pa[half], pa[half][:], x_t, x_t[:, half * 512:(half + 1) * 512], ALU.add)
            yield
            rmsnorm(h1, h1[:], 1, hn, hn[:], junk, ss, rstd)
            for c in range(8):
                tr(pt, pt[:, c, :], hn, hn[:, c * 128:(c + 1) * 128], ident)
            cp('act', hnT, hnT[:], pt, pt[:])
            for oc in range(8):
                pq_ = pa[2 + oc % 2]
                for c in range(8):
                    mm(pq_, pq_[:, 0:128], wqb, wqb[:, c, oc * 128:(oc + 1) * 128], hnT, hnT[:, c, :], start=(c == 0), stop=(c == 7))
                cp(['act', 'dve'][oc % 2], qTx, qTx[:, oc, :], pq_, pq_[:, 0:128])
                yield
            yield
            for mc in range(2):
                for h in range(4):
                    for dc in range(2):
                        mm(pxa, pxa[:, mc, h * 128:(h + 1) * 128], kTm, kTm[:, h * 2 + dc, mc * 128:(mc + 1) * 128], qTx, qTx[:, h * 2 + dc, :], start=(dc == 0), stop=(dc == 1))
            act(Ex, Ex[:].rearrange("p a h q -> p a (h q)"), pxa, pxa[:], AF.Exp, scale=1.0 / 16)
            for h in range(4):
                o_ap = pxa[:, h // 2, (h % 2) * 256:(h % 2) * 256 + 256]
                for mc in range(2):
                    mm(pa[h % 2], pa[h % 2][:, 0:257], Ex, Ex[:, mc, h, :], vm, vm[:, mc, h, :], start=(mc == 0), stop=(mc == 1))
                ts('dve', rdx, rdx[:, h:h + 1], pa[h % 2], pa[h % 2][:, 256:257], 1e-30, None, ALU.max)
                recip(rdx, rdx[:, h:h + 1], rdx, rdx[:, h:h + 1])
                ts('dve', oxa, oxa[:, h * 256:(h + 1) * 256], pa[h % 2], pa[h % 2][:, 0:256], rdx[:, h:h + 1], None, ALU.mult, rd=[rdx])
            yield
            for c in range(8):
                tr(pt, pt[:, c, :], oxa, oxa[:, c * 128:(c + 1) * 128], ident)
            cp('act', hnT, hnT[:], pt, pt[:])
            for half in range(2):
                for c in range(8):
                    mm(pa[half], pa[half][:], hnT, hnT[:, c, :], wob, wob[:, c, half * 512:(half + 1) * 512], start=(c == 0), stop=(c == 7))
                tt('dve', h1, h1[:, half * 512:(half + 1) * 512], pa[half], pa[half][:], h1, h1[:, half * 512:(half + 1) * 512], ALU.add)
            dma(d_h, d_h[jj * 128:(jj + 1) * 128, :], h1, h1[:])
            yield
            rmsnorm(h1, h1[:], 3, hn, hn[:], junk, ss, rstd)
            for c in range(8):
                tr(pt, pt[:, c, :], hn, hn[:, c * 128:(c + 1) * 128], ident)
            cp('act', hn3T, hn3T[:], pt, pt[:])
            dma(d_hn3T, d_hn3T[:, :, jj * 128:(jj + 1) * 128], hn3T, hn3T[:])
            for oc in range(8):
                pq_ = pa[2 + oc % 2]
                for c in range(8):
                    mm(pq_, pq_[:, 0:128], pwqb, pwqb[:, c, oc * 128:(oc + 1) * 128], hn3T, hn3T[:, c, :], start=(c == 0), stop=(c == 7))
                cp(['act', 'dve'][oc % 2], qpT, qpT[:, oc, :], pq_, pq_[:, 0:128])
                yield
            yield
            for oc in range(8):
                pq_ = pa[oc % 2]
                mm(pq_, pq_[:, 0:256], qpT, qpT[:, oc, :], skb, skb[:, oc, :])
                cp(['act', 'dve'][oc % 2], sc, sc[:, 2 * oc:2 * oc + 2, :], pq_, pq_[:, 0:256].rearrange("p (a k) -> p a k", a=2))
            yield

        def Rgen(jj):
            sc = sc_[jj % 2]; ab = ab_[jj % 2]
            P.op('dve', lambda e: e.tensor_reduce(out=negm[:], in_=sc[:], axis=AX.X, op=ALU.max), [sc], [negm])
            ts('dve', negm, negm[:], negm, negm[:], -1.0, None, ALU.mult)
            for r in range(16):
                act(ab, ab[:, r, :], sc, sc[:, r, :], AF.Exp, bias=negm[:, r:r + 1], rd=[negm])
            yield
            for r0 in range(0, 16, 4):
                for r in range(r0, r0 + 4):
                    P.op('dve', lambda e, r=r: e.max(out=t16[:, r, 0:8], in_=ab[:, r, :]), [ab], [t16])
                for r in range(r0, r0 + 4):
                    P.op('dve', lambda e, r=r: e.match_replace(out=scr2_[r % 4][:], in_to_replace=t16[:, r, 0:8], in_values=ab[:, r, :], imm_value=-1.0), [ab, t16], [scr2_[r % 4]])
                for r in range(r0, r0 + 4):
                    P.op('dve', lambda e, r=r: e.max(out=t16[:, r, 8:16], in_=scr2_[r % 4][:]), [scr2_[r % 4]], [t16])
                yield
            t16v = t16[:].rearrange("p (h a) k -> p h a k", a=2)
            abv = ab[:].rearrange("p (h a) k -> p h a k", a=2)

            def cand_top16():
                yield
                tt('dve', candall, candall[:].rearrange("p h (a b) -> p h a b", a=16),
                   t16, t16v[:, :, 0, :].unsqueeze(3).to_broadcast([128, 8, 16, 16]),
                   t16, t16v[:, :, 1, :].unsqueeze(2).to_broadcast([128, 8, 16, 16]), ALU.mult)
                for h0 in range(0, 8, 4):
                    for h in range(h0, h0 + 4):
                        P.op('dve', lambda e, h=h: e.max(out=c16[:, h, 0:8], in_=candall[:, h, :]), [candall], [c16])
                    for h in range(h0, h0 + 4):
                        P.op('dve', lambda e, h=h: e.match_replace(out=cand2_[h % 4][:], in_to_replace=c16[:, h, 0:8], in_values=candall[:, h, :], imm_value=-1.0), [candall, c16], [cand2_[h % 4]])
                    for h in range(h0, h0 + 4):
                        P.op('dve', lambda e, h=h: e.max(out=c16[:, h, 8:16], in_=cand2_[h % 4][:]), [cand2_[h % 4]], [c16])
                    yield
            yield from cand_top16()
            P.op('dve', lambda e: e.tensor_reduce(out=zs[:], in_=c16[:], axis=AX.X, op=ALU.add), [c16], [zs])
            recip(zs, zs[:], zs, zs[:])
            tt('dve', ab, abv[:, :, 1, :], ab, abv[:, :, 1, :], zs, zs[:].unsqueeze(2).to_broadcast([128, 8, 128]), ALU.mult)
            stt('dve', route, route[:, 0:8], c16, c16[:, :, 15], 1.0 - 1e-6, zs, zs[:], ALU.mult, ALU.mult)
            dma(d_route, d_route[jj * 128:(jj + 1) * 128, 0:2048], ab, ab[:].rearrange("p r k -> p (r k)"))
            dma(d_route, d_route[jj * 128:(jj + 1) * 128, 2048:2064], route, route[:])
            yield

        def drain(g):
            for _ in g:
                pass
        drain(Xgen(0))
        for jj in range(NO):
            gr = Rgen(jj)
            gx = Xgen(jj + 1) if jj + 1 < NO else iter(())
            ra = xa_ = True
            while ra or xa_:
                if xa_:
                    xa_ = next(gx, 'END') != 'END'
                if ra:
                    ra = next(gr, 'END') != 'END'
        P.pop()

    if upto >= 4:
        P.push()
        dma_pol['load'] = ['sp', 'pool']; dma_pol['store'] = ['sp']
        TG = 2
        IC = 16
        NCH = 128 // IC
        ACT_HEADS = (1, 3, 4, 6, 7)
        hT_ = [P.sb([128, 8, TG * 128], BF16, "hT%d" % i) for i in range(2)]
        ab_ = [[P.sb([128, 16, 128], F32, "ab4_%d_%d" % (i, u)) for u in range(TG)] for i in range(2)]
        rt_ = [[P.sb([128, 16], F32, "rt%d_%d" % (i, u)) for u in range(TG)] for i in range(2)]
        Wc = [[P.sb([128, IC * 128], BF16, "Wc%d_%d" % (i, u)) for u in range(TG)] for i in range(2)]
        et = [P.sb([128, IC, 128], F32, "et%d" % i) for i in range(4)]
        mt = [P.sb([128, IC, 128], BF16, "mt%d" % i) for i in range(2)]
        dnb = [P.sb([128, 8, 512], BF16, "dnb%d" % i) for i in range(3)]
        upb = [P.sb([128, 4, D], BF16, "upb%d" % i) for i in range(3)]
        Gs = [P.sb([128, 512], BF16, "G%d" % i) for i in range(2)]
        GT = [P.sb([128, 4, 128], BF16, "GT%d" % i) for i in range(2)]
        py = [P.ps([128, 2, 512], F32, "py%d" % u) for u in range(TG)]
        pd = [P.ps([128, 512], F32, "pd%d" % i) for i in range(2)]
        ptg = [P.ps([128, 8, 128], BF16, "ptg%d" % i) for i in range(2)]
        h2 = P.sb([128, D], F32, "h2"); yo = P.sb([128, D], F32, "yo")
        junk = P.sb([128, D], BF16, "junk4"); ss = P.sb([128, 1], F32, "ss4"); rstd = P.sb([128, 1], F32, "rstd4")
        qn = [0]
        def load_group(t2):
            hT2, ab2, rt2 = hT_[t2 % 2], ab_[t2 % 2], rt_[t2 % 2]
            dma(hT2, hT2[:], d_hn3T, d_hn3T[:, :, t2 * TG * 128:(t2 + 1) * TG * 128])
            for u in range(TG):
                j = t2 * TG + u
                dma(ab2[u], ab2[u][:].rearrange("p r k -> p (r k)"), d_route, d_route[j * 128:(j + 1) * 128, 0:2048])
                dma(rt2[u], rt2[u][:], d_route, d_route[j * 128:(j + 1) * 128, 2048:2064])
        load_group(0)
        for tg in range(NO // TG):
            hT, ab, rt = hT_[tg % 2], ab_[tg % 2], rt_[tg % 2]
            if tg + 1 < NO // TG:
                load_group(tg + 1)

            def wgen(c):
                its = [(u, h) for u in range(TG) for h in range(8)]
                K = len(its)
                eb_ = {}; mb_ = {}

                def E_(n):
                    u, h = its[n]
                    e_ = et[qn[0] % 4]; qn[0] += 1
                    eb_[n] = e_
                    if h in ACT_HEADS:
                        for i_ in range(IC):
                            P.op('act', lambda e, e_=e_, i_=i_, u=u, h=h: e.activation(out=e_[:, i_, :], in_=ab[u][:, 2 * h + 1, :], func=AF.Copy,
                                                                                  scale=ab[u][:, 2 * h, c * IC + i_:c * IC + i_ + 1]),
                                 [ab[u]], [e_] if i_ in (0, IC - 1) else [])
                    else:
                        tt('dve', e_, e_[:], ab[u], ab[u][:, 2 * h, c * IC:(c + 1) * IC].unsqueeze(2).to_broadcast([128, IC, 128]),
                           ab[u], ab[u][:, 2 * h + 1, :].unsqueeze(1).to_broadcast([128, IC, 128]), ALU.mult)

                def S_(n):
                    u, h = its[n]
                    e_ = eb_.pop(n)
                    w_ap = Wc[c % 2][u][:].rearrange("p (a b) -> p a b", a=IC)
                    if h == 0:
                        stt('dve', Wc[c % 2][u], w_ap, e_, e_[:], rt[u][:, h:h + 1], e_, e_[:], ALU.is_ge, ALU.mult, rd=[rt[u]])
                    else:
                        m_ = mt[n % 2]
                        mb_[n] = m_
                        stt('dve', m_, m_[:], e_, e_[:], rt[u][:, h:h + 1], e_, e_[:], ALU.is_ge, ALU.mult, rd=[rt[u]])

                def A_(n):
                    u, h = its[n]
                    if h == 0:
                        return
                    m_ = mb_.pop(n)
                    w_ap = Wc[c % 2][u][:].rearrange("p (a b) -> p a b", a=IC)
                    tt('dve', Wc[c % 2][u], w_ap, Wc[c % 2][u], w_ap, m_, m_[:], ALU.add)
                for n in range(K + 2):
                    if n < K:
                        E_(n)
                    if 1 <= n <= K:
                        S_(n - 1)
                    if n >= 2:
                        A_(n - 2)
                    yield

            def load_w(ecx):
                dn, ub = dnb[ecx % 3], upb[ecx % 3]
                dma(dn, dn[:], d_downT, d_downT[:, ecx * 512:(ecx + 1) * 512].rearrange("(c p) e -> p c e", p=128))
                dma(ub, ub[:], d_up, d_up[ecx * 512:(ecx + 1) * 512, :].rearrange("(s p) d -> p s d", p=128))

            items = [(ecx, u) for ecx in range(32) for u in range(TG)]
            N = len(items)

            def stA(n):
                ecx, u = items[n]
                if u == 0:
                    if ecx == 0:
                        load_w(0)
                    if ecx + 1 < 32:
                        load_w(ecx + 1)
                    if ecx % 4 == 0:
                        for _ in wg[0]:
                            pass
                        wg[0] = wgen(ecx // 4 + 1) if ecx // 4 + 1 < NCH else iter(())
                for _ in range(3):
                    next(wg[0], None)
                dn = dnb[ecx % 3]
                pdt = pd[n % 2]; G = Gs[n % 2]
                for c in range(8):
                    mm(pdt, pdt[:], hT, hT[:, c, u * 128:(u + 1) * 128], dn, dn[:, c, :], start=(c == 0), stop=(c == 7))
                act(G, G[:], pdt, pdt[:], AF.Gelu_apprx_tanh)
                wch = Wc[(ecx // 4) % 2][u]
                tt('dve', G, G[:], G, G[:], wch, wch[:, (ecx % 4) * 512:(ecx % 4 + 1) * 512], ALU.mult)

            def stB(n):
                G = Gs[n % 2]; gt_ = GT[n % 2]; pt_ = ptg[n % 2]
                for s_ in range(4):
                    tr(pt_, pt_[:, s_, :], G, G[:, s_ * 128:(s_ + 1) * 128], ident)
                cp('act', gt_, gt_[:], pt_, pt_[:, 0:4, :])

            def stC(n):
                ecx, u = items[n]
                gt_ = GT[n % 2]; ub = upb[ecx % 3]
                for half in range(2):
                    for s_ in range(4):
                        mm(py[u], py[u][:, half, :], gt_, gt_[:, s_, :], ub, ub[:, s_, half * 512:(half + 1) * 512],
                           start=(ecx == 0 and s_ == 0), stop=(ecx == 31 and s_ == 3))

            wg = [iter(())]
            for _ in wgen(0):
                pass
            for n in range(N + 2):
                if n < N:
                    stA(n)
                if 1 <= n <= N:
                    stB(n - 1)
                if n >= 2:
                    stC(n - 2)
            for _ in wg[0]:
                pass
            for u in range(TG):
                j = tg * TG + u
                dma(h2, h2[:], d_h, d_h[j * 128:(j + 1) * 128, :])
                tt('dve', h2, h2[:], h2, h2[:], py[u], py[u][:].rearrange("p a b -> p (a b)"), ALU.add)
                rmsnorm(h2, h2[:], 4, yo, yo[:], junk, ss, rstd)
                dma(out, out[j * 128:(j + 1) * 128, :], yo, yo[:])
        P.pop()

    P.finish()
    return nc, dict(npair=npair, pair_off=pair_off, NCC=NCC, NCMP=NCMP, ninst=P.ninst)


def _t5_bucket_np(dist):
    n = np.maximum(dist, 0)
    nf = np.maximum(n, 1).astype(np.float32)
    log_ratio = (np.log(nf / np.float32(16)) / np.float32(math.log(2048 / 16))).astype(np.float32)
    large = 16 + (log_ratio * np.float32(16)).astype(np.int32)
    large = np.minimum(large, 31)
    return np.where(n < 16, n, large).astype(np.int64)


def _bias_tile(rel_bias, g, dist, valid):
    bk = _t5_bucket_np(dist)
    outt = np.empty((128, 4, 128), np.float32)
    for h in range(4):
        outt[:, h, :] = np.where(valid, rel_bias[bk, g * 4 + h], np.float32(NEG))
    return outt.reshape(128, 512)


def make_core_inputs(inputs, T, b, p, meta):
    NT = T // 128; NO = NT // 2; NS = T // 64; NCMP = meta['NCMP']; NCC = meta['NCC']
    f = lambda a: np.ascontiguousarray(np.asarray(a, dtype=np.float32))
    x = f(inputs['x'][b]); rel_bias = f(inputs['rel_bias'])
    m = {}
    m['xb'] = x
    m['xo'] = np.ascontiguousarray(x.reshape(NO, 2, 128, D)[:, p].reshape(NO * 128, D))
    m['mem'] = f(inputs['mem'][b])
    w_in = f(inputs['w_in'][0])
    m['w_in'] = np.ascontiguousarray(np.concatenate([w_in[:, ORIG[k][0]:ORIG[k][1]] for k in PERM_ORDER], axis=1))
    m['gw2'] = f(inputs['gla_gate_w2'][0]); m['gb'] = f(inputs['gla_gate_b'][0]).reshape(1, 256)
    m['gnorm'] = f(inputs['gla_out_norm'][0]).reshape(1, 128)
    m['norms'] = np.stack([f(inputs['norm_mix'][0]), f(inputs['norm_xattn'][0]), f(inputs['norm_mem'][0]),
                           f(inputs['norm_ffn'][0]), f(inputs['norm_final'])], 0)
    cw1 = np.stack([f(inputs['cmp_k_w1'][0]), f(inputs['cmp_v_w1'][0])], 0)
    m['cw1'] = np.ascontiguousarray(cw1.reshape(2, 32, 64, 128).transpose(0, 2, 1, 3))
    cpos = np.stack([f(inputs['cmp_pos_k'][0]), f(inputs['cmp_pos_v'][0])], 0)
    m['cpos'] = np.ascontiguousarray(cpos.transpose(0, 2, 1))
    m['cw2'] = np.stack([f(inputs['cmp_k_w2'][0]), f(inputs['cmp_v_w2'][0])], 0)
    m['w_out'] = f(inputs['w_out'][0])
    m['xa_w'] = np.stack([f(inputs['xa_wq'][0]), f(inputs['xa_wk'][0]), f(inputs['xa_wv'][0]), f(inputs['xa_wo'][0])], 0)
    m['pwq'] = f(inputs['peer_wq'][0])
    sk = f(inputs['peer_subkeys'][0])
    skd = np.zeros((8, 128, 256), np.float32)
    for h in range(8):
        for pp in range(2):
            skd[h, pp * 64:(pp + 1) * 64, pp * 128:(pp + 1) * 128] = sk[h, pp].T
    m['skd'] = skd
    m['downT'] = np.ascontiguousarray(f(inputs['peer_down'][0]).T)
    m['up'] = f(inputs['peer_up'][0])
    m['c_ident'] = np.eye(128, dtype=np.float32)
    s_ = np.arange(128)[:, None]; t_ = np.arange(128)[None, :]
    m['c_ucs'] = np.where(s_ <= t_, -1.0 / 16, 0.0).astype(np.float32)
    m['c_urev'] = np.where(s_ > t_, -1.0 / 16, 0.0).astype(np.float32)
    m['c_causal'] = (s_ <= t_).astype(np.float32)
    m['c_selu'] = np.stack([np.eye(128) * (1 - p), np.eye(128) * p], 0).astype(np.float32)
    m['c_pv'] = np.tile(np.array([[1.0 - p, float(p)]], np.float32), (128, 1))
    kk = np.arange(128)[:, None]; qq = np.arange(128)[None, :]
    selb = np.empty((2, 128, 15, 512), np.float32); winb = np.empty((2, 128, 6, 512), np.float32)
    for g in range(2):
        for mm_ in range(15):
            j = mm_ - 1 + p
            dist = 128 * j + qq - kk
            selb[g, :, mm_, :] = _bias_tile(rel_bias, g, dist, (dist >= 0) & (j >= 0))
        for mm_ in range(6):
            j = mm_ - 1 + p
            dist = 128 * j + qq - kk
            winb[g, :, mm_, :] = _bias_tile(rel_bias, g, dist, (dist >= 0) & (dist < 512) & (j >= 0))
    m['c_selb'] = selb; m['c_winb'] = winb
    cmpb = np.empty((2, meta['npair'], 128, 512), np.float32)
    for jj in range(NO):
        for c in range(min(NCC, cmp_nchunks(jj))):
            n = 128 * c + kk
            t = (2 * jj + p) * 128 + qq
            dist = t - (16 * n + 31)
            for g in range(2):
                cmpb[g, meta['pair_off'][jj] + c] = _bias_tile(rel_bias, g, dist, (dist >= 0) & (n < NCMP))
    m['c_cmpb'] = cmpb
    far = np.empty((2, 128, 4, 128), np.float32)
    for g in range(2):
        for h in range(4):
            far[g, :, h, :] = rel_bias[31, g * 4 + h]
    m['c_far'] = far.reshape(2, 128, 512)
    wimp = np.zeros((NCC * 128, 128), np.float32)
    for s in range(NS):
        for r in range(-1, 4):
            n = 4 * s + r
            lo = 16 * r
            ov = min(lo + 32, 64) - max(lo, 0)
            if 0 <= n < NCMP:
                wimp[n, s] += ov / 32.0
    m['c_wimp'] = wimp.reshape(NCC, 128, 128)
    m12 = np.zeros((NO, 128, 2, 128), np.float32)
    sid = np.arange(128)[None, :]
    for jj in range(NO):
        t = (2 * jj + p) * 128 + np.arange(128)[:, None]
        cur = t // 64
        visible = (sid * 64 <= t) & (sid < NS)
        f0 = (sid == 0); f1 = (sid == cur); f2 = (sid == cur - 1)
        forced = f0 | f1 | f2
        m12[jj, :, 0, :] = (visible & ~forced)
        add = np.where(~visible, -100.0 - sid, 0.0)
        add = np.where(f2, 100.0, add); add = np.where(f1, 101.0, add); add = np.where(f0 & (sid < NS), 102.0, add)
        m12[jj, :, 1, :] = add
    m['c_m12'] = m12
    ex = np.zeros((128, T), np.float32)
    ex[np.arange(T) // 64, np.arange(T)] = 1.0
    m['c_ex'] = ex.astype(NPBF)
    return m


_CACHE = {}


def kernel(**inputs):
    T = inputs['x'].shape[1]
    B = inputs['x'].shape[0]
    if T not in _CACHE:
        _CACHE[T] = build(T)
    nc, meta = _CACHE[T]
    in_maps = []
    for c in range(2 * B):
        in_maps.append(make_core_inputs(inputs, T, c // 2, c % 2, meta))
    res = run_bass_kernel_spmd(nc, in_maps, core_ids=list(range(2 * B)))
    NO = T // 256
    outp = np.empty((B, T // 128, 128, D), np.float32)
    for c in range(2 * B):
        o = np.asarray(res.results[c]["out"], dtype=np.float32).reshape(NO, 128, D)
        outp[c // 2, (c % 2)::2] = o
    return outp.reshape(B, T, D)
```

```python
import math
import numpy as np
import ml_dtypes
import concourse.bass as bass
import concourse.mybir as mybir
from concourse.bass_utils import run_bass_kernel_spmd
from contextlib import ExitStack

F32 = mybir.dt.float32
BF16 = mybir.dt.bfloat16
AF = mybir.ActivationFunctionType
ALU = mybir.AluOpType
AX = mybir.AxisListType
NPBF = ml_dtypes.bfloat16

D = 1024
NEG = -30000.0


class T:
    def __init__(self, h, name):
        self.h = h
        self.name = name
        self.w = None
        self.r = []
        self.psum = False
        self.dram = False

    def __getitem__(self, k):
        return self.h[k]


class Prog:
    def __init__(self, nc, n_dma_sems=48):
        self.nc = nc
        self.es = ExitStack()
        self.scopes = []
        self.eng = {'pe': nc.tensor, 'dve': nc.vector, 'act': nc.scalar,
                    'pool': nc.gpsimd, 'sp': nc.sync}
        self.sems = {}
        for k in self.eng:
            self.sems['e_' + k] = self.es.enter_context(nc.semaphore('e_' + k))
        self.cnt = {k: 0 for k in self.sems}
        self.ndma = n_dma_sems
        for i in range(n_dma_sems):
            key = 'd_%d' % i
            self.sems[key] = self.es.enter_context(nc.semaphore(key))
            self.cnt[key] = 0
        self.dma_rr = 0
        self.known = {k: {} for k in self.eng}
        self.ntile = 0
        self.ninst = 0

    def push(self):
        self.scopes.append(ExitStack())

    def pop(self):
        self.barrier()
        self.scopes.pop().close()

    def _stack(self):
        return self.scopes[-1] if self.scopes else self.es

    def sb(self, shape, dt, name=None):
        self.ntile += 1
        name = (name or 't') + '_%d' % self.ntile
        h = self._stack().enter_context(self.nc.sbuf_tensor(name, list(shape), dt))
        return T(h, name)

    def ps(self, shape, dt, name=None):
        self.ntile += 1
        name = (name or 'p') + '_%d' % self.ntile
        h = self._stack().enter_context(self.nc.psum_tensor(name, list(shape), dt))
        t = T(h, name)
        t.psum = True
        return t

    def dram(self, name, shape, dt, kind="Internal"):
        h = self.nc.dram_tensor(name, list(shape), dt, kind=kind).ap()
        t = T(h, name)
        t.dram = True
        return t

    def _deps(self, reads, writes, e=None):
        deps = []
        for t in reads:
            if t.w is not None:
                deps.append(t.w)
            if t.psum:
                deps.extend([tok for tok in t.r if tok[0] != 'e_' + str(e)])
        for t in writes:
            if t.w is not None:
                deps.append(t.w)
            deps.extend(t.r)
        return deps

    def _waits(self, e, deps):
        kn = self.known[e]
        need = {}
        for (k, v) in deps:
            if kn.get(k, 0) >= v:
                continue
            if need.get(k, 0) < v:
                need[k] = v
        for k, v in need.items():
            kn[k] = v
        return list(need.items())

    def _emit(self, e, waits, fn, inc):
        eng = self.eng[e]
        for (k, v) in waits:
            eng.wait_ge(self.sems[k], v)
        ins = fn(eng)
        ins.then_inc(self.sems[inc[0]], inc[1])
        self.ninst += 1

    def op(self, e, fn, reads=(), writes=()):
        deps = self._deps(reads, writes, e)
        key = 'e_' + e
        if e == 'pe':
            deps = [d for d in deps if d[0] != key]
        waits = self._waits(e, deps)
        self.cnt[key] += 1
        tok = (key, self.cnt[key])
        self._emit(e, waits, fn, (key, 1))
        for t in reads:
            t.r.append(tok)
        for t in writes:
            t.w = tok
            t.r = []
        return tok

    def dma(self, e, out_t, out_ap, in_t, in_ap, **kw):
        reads = [in_t]
        writes = [out_t]
        deps = self._deps(reads, writes)
        key = 'd_%d' % self.dma_rr
        self.dma_rr = (self.dma_rr + 1) % self.ndma
        if self.cnt[key] > 0:
            deps.append((key, self.cnt[key]))
        waits = self._waits(e, deps)
        self.cnt[key] += 16
        tok = (key, self.cnt[key])
        self._emit(e, waits, lambda eng: eng.dma_start(out=out_ap, in_=in_ap, **kw), (key, 16))
        in_t.r.append(tok)
        out_t.w = tok
        out_t.r = []
        return tok

    def barrier(self):
        allt = [(k, v) for k, v in self.cnt.items() if v > 0]
        for e in self.eng:
            for (k, v) in self._waits(e, allt):
                self.eng[e].wait_ge(self.sems[k], v)

    def finish(self):
        self.barrier()
        while self.scopes:
            self.scopes.pop().close()
        self.es.close()


ORIG = dict(gq=(0, 256), gk=(256, 512), gv=(512, 1024), gr=(1024, 1536), glr=(1536, 1552),
            nq=(1552, 2064), kc=(2064, 2192), vc=(2192, 2320), ks=(2320, 2448), vs=(2448, 2576),
            kw=(2576, 2704), vw=(2704, 2832), ng=(2832, 2856))
PERM_ORDER = ['gq', 'gk', 'gv', 'gr', 'nq', 'kc', 'vc', 'ks', 'vs', 'kw', 'vw', 'ng', 'glr']
NCOL = 2856


def cmp_nchunks(jj):
    return min(4, (16 * jj + 15 + 127) // 128)


def build(T, debug=False, upto=9, cut=99):
    NT = T // 128
    NO = NT // 2
    NS = T // 64
    TO = T // 2
    NCMP = (T - 32) // 16 + 1
    NCC = (NCMP + 127) // 128
    pair_off = []
    npair = 0
    for jj in range(NO):
        pair_off.append(npair)
        npair += min(NCC, cmp_nchunks(jj))

    nc = bass.Bass("TRN2", target_bir_lowering=False)
    P = Prog(nc)
    SK = "ExternalOutput" if debug else "Internal"

    def inp(name, shape, dt=F32):
        return P.dram(name, shape, dt, kind="ExternalInput")

    xb = inp("xb", [T, D]); xo = inp("xo", [TO, D]); memb = inp("mem", [256, D])
    w_in = inp("w_in", [D, NCOL]); gw2 = inp("gw2", [16, 256]); gb = inp("gb", [1, 256])
    gnorm = inp("gnorm", [1, 128])
    norms = inp("norms", [5, D])
    cw1 = inp("cw1", [2, 64, 32, 128]); cpos = inp("cpos", [2, 64, 32]); cw2 = inp("cw2", [2, 128, 64])
    w_out = inp("w_out", [D, D]); xa_w = inp("xa_w", [4, D, D])
    pwq = inp("pwq", [D, D]); skd = inp("skd", [8, 128, 256])
    downT = inp("downT", [D, 16384]); up = inp("up", [16384, D])
    c_ident = inp("c_ident", [128, 128]); c_ucs = inp("c_ucs", [128, 128]); c_urev = inp("c_urev", [128, 128])
    c_causal = inp("c_causal", [128, 128]); c_selu = inp("c_selu", [2, 128, 128]); c_pv = inp("c_pv", [128, 2])
    c_selb = inp("c_selb", [2, 128, 15, 512]); c_winb = inp("c_winb", [2, 128, 6, 512])
    c_cmpb = inp("c_cmpb", [2, npair, 128, 512])
    c_far = inp("c_far", [2, 128, 512])
    c_wimp = inp("c_wimp", [NCC, 128, 128]); c_m12 = inp("c_m12", [NO, 128, 2, 128])
    c_ex = inp("c_ex", [128, T], BF16)
    out = P.dram("out", [TO, D], F32, kind="ExternalOutput")

    d_nq = P.dram("d_nq", [T, 512], BF16, kind=SK)
    d_ng = P.dram("d_ng", [T, 24], F32, kind=SK)
    d_ogla = P.dram("d_ogla", [T, 512], BF16, kind=SK)
    d_kT = P.dram("d_kT", [4, 128, T], BF16, kind=SK)
    d_vs = P.dram("d_vs", [T, 128], BF16, kind=SK)
    d_vw = P.dram("d_vw", [T, 128], BF16, kind=SK)
    d_omix = P.dram("d_omix", [TO, D], BF16, kind=SK)
    d_h = P.dram("d_h", [TO, D], F32, kind=SK)
    d_hn3T = P.dram("d_hn3T", [128, 8, TO], BF16, kind=SK)
    d_route = P.dram("d_route", [TO, 2064], F32, kind=SK)
    d_downT = P.dram("d_downT", [D, 16384], BF16, kind="Internal")
    d_up = P.dram("d_up", [16384, D], BF16, kind="Internal")
    d_cmp = P.dram("d_cmp", [2, 64, 512], BF16, kind=SK)
    d_dbg = P.dram("d_dbg", [3, TO, 512], F32, kind=SK)
    d_imp = P.dram("d_imp", [2, TO, 128], F32, kind=SK)

    def mm(ot, o_ap, lt, l_ap, rt, r_ap, start=True, stop=True):
        P.op('pe', lambda e: e.matmul(o_ap, lhsT=l_ap, rhs=r_ap, start=start, stop=stop), [lt, rt], [ot])

    def tr(ot, o_ap, it, i_ap, idt):
        P.op('pe', lambda e: e.transpose(out=o_ap, in_=i_ap, identity=idt[:]), [it, idt], [ot])

    def act(ot, o_ap, it, i_ap, func, bias=None, scale=None, accum=None, rd=(), wr=()):
        kw = {}
        if bias is not None:
            kw['bias'] = bias
        if scale is not None:
            kw['scale'] = scale
        if accum is not None:
            kw['accum_out'] = accum
        P.op('act', lambda e: e.activation(out=o_ap, in_=i_ap, func=func, **kw), [it] + list(rd), [ot] + list(wr))

    def cp(eng, ot, o_ap, it, i_ap):
        if eng == 'act':
            P.op('act', lambda e: e.copy(out=o_ap, in_=i_ap), [it], [ot])
        else:
            P.op(eng, lambda e: e.tensor_copy(out=o_ap, in_=i_ap), [it], [ot])

    def tt(eng, ot, o_ap, at, a_ap, bt, b_ap, op):
        P.op(eng, lambda e: e.tensor_tensor(out=o_ap, in0=a_ap, in1=b_ap, op=op), [at, bt], [ot])

    def ts(eng, ot, o_ap, at, a_ap, s1, s2, op0, op1=None, rd=()):
        if op1 is None:
            P.op(eng, lambda e: e.tensor_scalar(out=o_ap, in0=a_ap, scalar1=s1, scalar2=None, op0=op0), [at] + list(rd), [ot])
        else:
            P.op(eng, lambda e: e.tensor_scalar(out=o_ap, in0=a_ap, scalar1=s1, scalar2=s2, op0=op0, op1=op1), [at] + list(rd), [ot])

    def stt(eng, ot, o_ap, at, a_ap, sc, bt, b_ap, op0, op1, rd=()):
        P.op(eng, lambda e: e.scalar_tensor_tensor(out=o_ap, in0=a_ap, scalar=sc, in1=b_ap, op0=op0, op1=op1),
             [at, bt] + list(rd), [ot])

    def memset(eng, t, ap, v):
        P.op(eng, lambda e: e.memset(ap, v), [], [t])

    def recip(ot, o_ap, it, i_ap):
        P.op('dve', lambda e: e.reciprocal(out=o_ap, in_=i_ap), [it], [ot])

    dmaq = ['sp', 'act', 'pool']
    dq = [0]

    dma_pol = {'load': ['sp'], 'store': ['pool']}

    def dma(ot, o_ap, it, i_ap, q=None, **kw):
        if q is None:
            qs = dma_pol['store'] if ot.dram else dma_pol['load']
            q = qs[dq[0] % len(qs)]
            dq[0] += 1
        return P.dma(q, ot, o_ap, it, i_ap, **kw)

    ident = P.sb([128, 128], BF16, "ident"); identf = P.sb([128, 128], F32, "identf")
    ones = P.sb([128, 128], BF16, "ones")
    pv = P.sb([128, 2], F32, "pv")
    nrm = P.sb([128, 5, D], F32, "nrm")
    dma(identf, identf[:], c_ident, c_ident[:])
    dma(pv, pv[:], c_pv, c_pv[:])
    for k in range(5):
        dma(nrm, nrm[:, k, :], norms, norms[k:k + 1, :].partition_broadcast(128))
    cp('dve', ident, ident[:], identf, identf[:])
    memset('pool', ones, ones[:], 1.0)
    eps_t = P.sb([128, 1], F32, "eps_t")
    memset('dve', eps_t, eps_t[:], 1e-6)
    kcmpT = [P.sb([128, 512], BF16, "kcmpT%d" % g) for g in range(2)]
    vcmp = [P.sb([128, 4, 65], BF16, "vcmp%d" % g) for g in range(2)]

    def rmsnorm(xt, x_ap, gain_k, hn, hn_ap, junk, ss, rstd, n=D, eps=1e-6):
        memset('dve', ss, ss[:], 0.0)
        act(junk, junk[:, 0:n], xt, x_ap, AF.Square, accum=ss[:], rd=[ss], wr=[ss])
        act(rstd, rstd[:], ss, ss[:], AF.Ln, scale=1.0 / n, bias=eps_t[:, 0:1], rd=[eps_t])
        act(rstd, rstd[:], rstd, rstd[:], AF.Exp, scale=-0.5)
        stt('dve', hn, hn_ap, xt, x_ap, rstd[:, 0:1], nrm, nrm[:, gain_k, 0:n], ALU.mult, ALU.mult, rd=[rstd])

    if upto >= 1:
        P.push()
        winb = P.sb([128, 8, NCOL], BF16, "winb")
        wst = [P.sb([128, NCOL], F32, "wst%d" % i) for i in range(2)]
        w_in_v = w_in[:].rearrange("(c p) n -> c p n", p=128)
        for c in range(8):
            dma(wst[c % 2], wst[c % 2][:], w_in, w_in_v[c])
            cp(['dve', 'pool'][c % 2], winb, winb[:, c, :], wst[c % 2], wst[c % 2][:])
        gw2t = P.sb([16, 256], F32, "gw2t"); gbt = P.sb([1, 256], F32, "gbt"); onesrow = P.sb([1, 128], F32, "onesrow")
        gnt = P.sb([128, 128], F32, "gnt")
        ucs = P.sb([128, 128], F32, "ucs"); urev = P.sb([128, 128], F32, "urev"); causal = P.sb([128, 128], F32, "causal")
        m16 = P.sb([128, 1], F32, "m16")
        dma(gw2t, gw2t[:], gw2, gw2[:]); dma(gbt, gbt[:], gb, gb[:])
        dma(gnt, gnt[:], gnorm, gnorm[0:1, :].partition_broadcast(128))
        dma(ucs, ucs[:], c_ucs, c_ucs[:]); dma(urev, urev[:], c_urev, c_urev[:]); dma(causal, causal[:], c_causal, c_causal[:])
        memset('dve', onesrow, onesrow[:], 1.0)
        memset('dve', m16, m16[:], -1.0 / 16)
        S_ = [P.sb([128, 128], F32, "S%d" % h) for h in range(4)]; Sb_ = [P.sb([128, 128], BF16, "Sb%d" % h) for h in range(4)]
        for h in range(4):
            memset('dve', S_[h], S_[h][:], 0.0); memset('pool', Sb_[h], Sb_[h][:], 0.0)

        xt = [P.sb([128, D], F32, "xt%d" % i) for i in range(2)]
        junk = P.sb([128, D], BF16, "junk"); ss = P.sb([128, 1], F32, "ss"); rstd = P.sb([128, 1], F32, "rstd")
        hn = P.sb([128, D], BF16, "hn"); hnT = P.sb([128, 8, 128], BF16, "hnT")
        pt = P.ps([128, 8, 128], BF16, "pt")
        pz = [P.ps([128, 512], F32, "pz%d" % i) for i in range(3)]
        pg1 = P.ps([128, 512], F32, "pg1"); pg2 = P.ps([128, 512], F32, "pg2")
        po = P.ps([128, 512], F32, "po"); ptb = P.ps([128, 8, 128], BF16, "ptb")
        glr = P.sb([128, 16], F32, "glr"); glrT = P.sb([16, 128], F32, "glrT")
        e1 = P.sb([128, 256], F32, "e1"); L = P.sb([128, 256], F32, "L")
        eb = P.sb([128, 256], F32, "eb"); enb = P.sb([128, 256], F32, "enb"); ec = P.sb([128, 256], F32, "ec")
        ebl = P.sb([128, 2], F32, "ebl")
        qk = P.sb([128, 3, 256], BF16, "qk")
        qkT = P.sb([128, 4, 128], BF16, "qkT")
        vv = P.sb([128, 512], BF16, "vv"); sg = P.sb([128, 512], F32, "sg")
        AT_ = [P.sb([128, 128], BF16, "AT%d" % i) for i in range(2)]
        ssq = P.sb([128, 4], F32, "ssq"); rs4 = P.sb([128, 4], F32, "rs4"); tmpn = P.sb([128, 128], F32, "tmpn")
        junk2 = P.sb([128, 128], F32, "junk2")
        og = P.sb([128, 512], BF16, "og"); otmp4 = P.sb([128, 512], F32, "otmp4")
        nqs = P.sb([128, 512], BF16, "nqs"); ngs = P.sb([128, 24], F32, "ngs")
        kk = P.sb([128, 4, 128], BF16, "kk"); kkT = P.sb([128, 4, 128], BF16, "kkT")
        vsw = P.sb([128, 2, 128], BF16, "vsw")
        cast_jobs = []
        if upto >= 4:
            stg = [P.sb([128, 4096], F32, "stg%d" % i) for i in range(2)]
            stb = [P.sb([128, 4096], BF16, "stb%d" % i) for i in range(2)]
            for r in range(8):
                for c in range(4):
                    cast_jobs.append((downT, downT[r * 128:(r + 1) * 128, c * 4096:(c + 1) * 4096],
                                      d_downT, d_downT[r * 128:(r + 1) * 128, c * 4096:(c + 1) * 4096]))
            sv = up[:].rearrange("(a p f) d -> a p (f d)", p=128, f=4)
            dv = d_up[:].rearrange("(a p f) d -> a p (f d)", p=128, f=4)
            for a in range(32):
                cast_jobs.append((up, sv[a], d_up, dv[a]))
        cj = [0]

        def cast_job():
            k_ = cj[0]
            if k_ < len(cast_jobs):
                src_t, s_ap, dst_t, d_ap = cast_jobs[k_]
                P.dma('pool', stg[k_ % 2], stg[k_ % 2][:], src_t, s_ap)
            if 1 <= k_ <= len(cast_jobs):
                src_t, s_ap, dst_t, d_ap = cast_jobs[k_ - 1]
                a, b_ = stg[(k_ - 1) % 2], stb[(k_ - 1) % 2]
                for q_ in range(4):
                    cp('act', b_, b_[:, q_ * 1024:(q_ + 1) * 1024], a, a[:, q_ * 1024:(q_ + 1) * 1024])
                P.dma('pool', dst_t, d_ap, b_, b_[:])
            cj[0] += 1
        cA, cB, cC, cD, cE, cF = 0, 512, 1024, 1536, 2048, 2560
        hn2_ = [hn, P.sb([128, D], BF16, "hn_b")]; hnT2_ = [hnT, P.sb([128, 8, 128], BF16, "hnT_b")]
        ss2_ = [ss, P.sb([128, 1], F32, "ss_b")]; rstd2_ = [rstd, P.sb([128, 1], F32, "rstd_b")]

        def front(i):
            x_t = xt[i % 2]
            dma(x_t, x_t[:], xb, xb[i * 128:(i + 1) * 128, :])
            rmsnorm(x_t, x_t[:], 0, hn2_[i % 2], hn2_[i % 2][:], junk, ss2_[i % 2], rstd2_[i % 2])
            for c in range(8):
                tr(pt, pt[:, c, :], hn2_[i % 2], hn2_[i % 2][:, c * 128:(c + 1) * 128], ident)
            cp('act', hnT2_[i % 2], hnT2_[i % 2][:], pt, pt[:])
        front(0)
        for i in range(NT):
            hnT = hnT2_[i % 2]

            def zgroup(pz_t, c0, n):
                for c in range(8):
                    mm(pz_t, pz_t[:, 0:n], hnT, hnT[:, c, :], winb, winb[:, c, c0:c0 + n], start=(c == 0), stop=(c == 7))
            zgroup(pz[0], cF, 296)
            cp('act', kk, kk[:, 3, :], pz[0], pz[0][:, 0:128])
            cp('act', vsw, vsw[:, 1, :], pz[0], pz[0][:, 128:256])
            cp('act', glr, glr[:], pz[0], pz[0][:, 280:296])
            act(ngs, ngs[:], pz[0], pz[0][:, 256:280], AF.Exp, scale=-1.0)
            ts('dve', ngs, ngs[:], ngs, ngs[:], 1.0, None, ALU.add)
            recip(ngs, ngs[:], ngs, ngs[:])
            dma(d_ng, d_ng[i * 128:(i + 1) * 128, :], ngs, ngs[:])
            zgroup(pz[1], cE, 512)
            cp('act', kk, kk[:, 0:3, :], pz[1], pz[1][:, 0:384].rearrange("p (a b) -> p a b", a=3))
            cp('dve', vsw, vsw[:, 0, :], pz[1], pz[1][:, 384:512])
            dma(d_vs, d_vs[i * 128:(i + 1) * 128, :], vsw, vsw[:, 0, :])
            dma(d_vw, d_vw[i * 128:(i + 1) * 128, :], vsw, vsw[:, 1, :])
            for a in range(4):
                tr(ptb, ptb[:, a, :], kk, kk[:, a, :], ident)
            cp('act', kkT, kkT[:], ptb, ptb[:, 0:4, :])
            dma(d_kT, d_kT[:, :, i * 128:(i + 1) * 128].rearrange("a p t -> p a t"), kkT, kkT[:])
            zgroup(pz[2], cD, 512)
            P.op('act', lambda e, pzt=pz[2]: e.mul(out=nqs[:], in_=pzt[:], mul=0.125), [pz[2]], [nqs])
            dma(d_nq, d_nq[i * 128:(i + 1) * 128, :], nqs, nqs[:])
            tr(pg1, pg1[0:16, 0:128], glr, glr[:], identf)
            cp('dve', glrT, glrT[:], pg1, pg1[0:16, 0:128])
            mm(pg1, pg1[:, 256:512], glrT, glrT[:], gw2t, gw2t[:], start=True, stop=False)
            mm(pg1, pg1[:, 256:512], onesrow, onesrow[:], gbt, gbt[:], start=False, stop=True)
            zgroup(pz[0], cA, 512)
            zgroup(pz[1], cB, 512)
            zgroup(pz[2], cC, 512)
            act(e1, e1[:], pg1, pg1[:, 256:512], AF.Exp, scale=-1.0)
            act(L, L[:], e1, e1[:], AF.Ln, bias=1.0)
            cp('act', vv, vv[:], pz[1], pz[1][:])
            act(sg, sg[:], pz[2], pz[2][:], AF.Exp, scale=-1.0)
            act(sg, sg[:], sg, sg[:], AF.Ln, bias=1.0)
            act(sg, sg[:], sg, sg[:], AF.Exp, scale=-1.0)
            tt('dve', sg, sg[:], sg, sg[:], pz[2], pz[2][:], ALU.mult)
            tt('pool', sg, sg[:].rearrange("p (h d) -> p h d", h=4), sg, sg[:].rearrange("p (h d) -> p h d", h=4),
               gnt, gnt[:].unsqueeze(1).to_broadcast([128, 4, 128]), ALU.mult)
            mm(pg2, pg2[:, 0:256], ucs, ucs[:], L, L[:])
            mm(pg2, pg2[:, 256:512], urev, urev[:], L, L[:])
            for hp in range(2):
                mm(pg1, pg1[:, hp:hp + 1], L, L[:, hp * 128:(hp + 1) * 128], m16, m16[:])
            act(eb, eb[:], pg2, pg2[:, 0:256], AF.Exp)
            act(enb, enb[:], pg2, pg2[:, 0:256], AF.Exp, scale=-1.0)
            act(ec, ec[:], pg2, pg2[:, 256:512], AF.Exp)
            act(ebl, ebl[:], pg1, pg1[:, 0:2], AF.Exp)
            stt('dve', qk, qk[:, 0, :], pz[0], pz[0][:, 0:256], 0.125, eb, eb[:], ALU.mult, ALU.mult)
            tt('dve', qk, qk[:, 1, :], pz[0], pz[0][:, 256:512], enb, enb[:], ALU.mult)
            tt('dve', qk, qk[:, 2, :], pz[0], pz[0][:, 256:512], ec, ec[:], ALU.mult)
            for a in range(4):
                tr(ptb, ptb[:, 4 + a, :], qk, qk[:, a // 2, (a % 2) * 128:(a % 2 + 1) * 128], ident)
            cp('act', qkT, qkT[:], ptb, ptb[:, 4:8, :])
            if i + 1 < NT:
                front(i + 1)
            memset('dve', ssq, ssq[:], 0.0)
            for h in range(4):
                hp, hh = h // 2, h % 2
                pr = slice(hh * 64, hh * 64 + 64)
                pgh = pg2 if h % 2 == 0 else pg1
                ATh = AT_[h % 2]
                Sh, Sbh = S_[h], Sb_[h]
                mm(pgh, pgh[:, 0:128], qkT, qkT[pr, 2 + hp, :], qkT, qkT[pr, hp, :])
                tt('dve', ATh, ATh[:], pgh, pgh[:, 0:128], causal, causal[:], ALU.mult)
                o_ap = po[:, h * 128:(h + 1) * 128]
                mm(po, o_ap, ATh, ATh[:], vv, vv[:, h * 128:(h + 1) * 128], start=True, stop=False)
                mm(po, o_ap, qkT, qkT[pr, hp, :], Sbh, Sbh[pr, :], start=False, stop=True)
                mm(pgh, pgh[:, 128:256], qk, qk[:, 2, hp * 128:(hp + 1) * 128], vv, vv[:, h * 128:(h + 1) * 128])
                stt('dve', Sbh, Sbh[pr, :], Sh, Sh[pr, :], ebl[pr, hp:hp + 1], pgh, pgh[pr, 128:256], ALU.mult, ALU.add, rd=[ebl])
                stt('dve', Sh, Sh[pr, :], Sh, Sh[pr, :], ebl[pr, hp:hp + 1], pgh, pgh[pr, 128:256], ALU.mult, ALU.add, rd=[ebl])
            for h in range(4):
                act(junk2, junk2[:], po, po[:, h * 128:(h + 1) * 128], AF.Square, accum=ssq[:, h:h + 1], rd=[ssq], wr=[ssq])
            act(rs4, rs4[:], ssq, ssq[:], AF.Ln, scale=1.0 / 128, bias=eps_t[:, 0:1], rd=[eps_t])
            act(rs4, rs4[:], rs4, rs4[:], AF.Exp, scale=-0.5)
            tt('dve', otmp4, otmp4[:], po, po[:], sg, sg[:], ALU.mult)
            tt('dve', og, og[:].rearrange("p (h d) -> p h d", h=4), otmp4, otmp4[:].rearrange("p (h d) -> p h d", h=4),
               rs4, rs4[:].unsqueeze(2).to_broadcast([128, 4, 128]), ALU.mult)
            dma(d_ogla, d_ogla[i * 128:(i + 1) * 128, :], og, og[:])
            for _ in range((len(cast_jobs) + NT - 1) // NT):
                cast_job()
        while cast_jobs and cj[0] <= len(cast_jobs):
            cast_job()
        P.pop()

        P.push()
        w1f = P.sb([64, 32, 128], F32, "w1f"); w1b = P.sb([64, 32, 128], BF16, "w1b")
        posf = P.sb([64, 32], F32, "posf"); posb = P.sb([64, 32], BF16, "posb")
        w2f = P.sb([128, 64], F32, "w2f"); w2b = P.sb([128, 64], BF16, "w2b")
        kTg = P.sb([64, T], BF16, "kTg")
        ph = P.ps([128, 512], F32, "ph"); pb = P.ps([128, 512], F32, "pb"); pc = P.ps([128, 512], F32, "pc")
        bias_h = P.sb([128, 1], F32, "bias_h")
        H = P.sb([128, 512], BF16, "H")
        for g in range(2):
            memset('dve', kcmpT[g], kcmpT[g][:], 0.0)
            memset('dve', vcmp[g], vcmp[g][:], 0.0)
            memset('dve', vcmp[g], vcmp[g][:, :, 64:65], 1.0)
        for kv in range(2):
            dma(w1f, w1f[:], cw1, cw1[kv]); dma(posf, posf[:], cpos, cpos[kv]); dma(w2f, w2f[:], cw2, cw2[kv])
            cp('dve', w1b, w1b[:], w1f, w1f[:]); cp('dve', posb, posb[:], posf, posf[:]); cp('dve', w2b, w2b[:], w2f, w2f[:])
            for l in range(32):
                mm(pb, pb[:, 0:1], w1b, w1b[:, l, :], posb, posb[:, l:l + 1], start=(l == 0), stop=(l == 31))
            cp('dve', bias_h, bias_h[:], pb, pb[:, 0:1])
            for g in range(2):
                dma(kTg, kTg[:], d_kT, d_kT[kv, g * 64:(g + 1) * 64, :])
                for l in range(32):
                    mm(ph, ph[:, 0:NCMP], w1b, w1b[:, l, :], kTg, kTg[:, l:l + 16 * (NCMP - 1) + 1:16],
                       start=(l == 0), stop=(l == 31))
                memset('dve', H, H[:], 0.0)
                act(H, H[:, 0:NCMP], ph, ph[:, 0:NCMP], AF.Gelu_apprx_tanh, bias=bias_h[:, 0:1], rd=[bias_h])
                if kv == 0:
                    mm(pc, pc[0:64, 0:NCMP], w2b, w2b[:], H, H[:, 0:NCMP])
                    cp('act', kcmpT[g], kcmpT[g][0:64, 0:NCMP], pc, pc[0:64, 0:NCMP])
                    if debug:
                        dma(d_cmp, d_cmp[g], kcmpT[g], kcmpT[g][0:64, :])
                else:
                    for c in range(NCC):
                        mm(pc, pc[:, c * 64:(c + 1) * 64], H, H[:, c * 128:(c + 1) * 128], w2b, w2b[:])
                    cp('act', vcmp[g], vcmp[g][:, 0:NCC, 0:64], pc, pc[:, 0:NCC * 64].rearrange("p (c d) -> p c d", d=64))
        P.pop()

    if upto >= 2:
        P.push()
        dma_pol['load'] = ['sp']; dma_pol['store'] = ['sp']
        selu = P.sb([128, 2, 128], BF16, "selu"); seluf = P.sb([128, 2, 128], F32, "seluf")
        dma(seluf, seluf[:], c_selu, c_selu[:].rearrange("a p t -> p a t"))
        cp('dve', selu, selu[:], seluf, seluf[:])
        exm = P.sb([128, T], BF16, "exm")
        dma(exm, exm[:], c_ex, c_ex[:])
        wimpf = P.sb([128, NCC, 128], F32, "wimpf"); wimp = P.sb([128, NCC, 128], BF16, "wimp")
        dma(wimpf, wimpf[:], c_wimp, c_wimp[:].rearrange("c p s -> p c s"))
        cp('dve', wimp, wimp[:], wimpf, wimpf[:])
        ksT = P.sb([128, T], BF16, "ksT"); kwT = P.sb([128, T], BF16, "kwT")
        memset('dve', ksT, ksT[64:128, :], 0.0)
        memset('pool', kwT, kwT[64:128, :], 0.0)
        memset('dve', ksT, ksT[64:65, :], 1.0)
        farf = P.sb([128, 512], F32, "farf")
        vs = P.sb([128, NT, 65], BF16, "vs"); vw = P.sb([128, NT, 65], BF16, "vw")
        selb = P.sb([128, 15, 512], BF16, "selb"); winb2 = P.sb([128, 6, 512], BF16, "winb2")
        bst = [P.sb([128, 512], F32, "bst%d" % i) for i in range(2)]
        cbt = [P.sb([128, 512], BF16, "cbt%d" % i) for i in range(2)]
        qrows_ = [P.sb([128, 2, 256], BF16, "qrows%d" % i) for i in range(2)]; grows_ = [P.sb([128, 2, 24], F32, "grows%d" % i) for i in range(2)]
        gown_ = [P.sb([128, 12], F32, "gown%d" % i) for i in range(2)]; gtmp = P.sb([128, 12], F32, "gtmp")
        qT_ = [P.sb([128, 4, 128], BF16, "qT%d" % i) for i in range(2)]
        for i_ in range(2):
            memset('dve', qT_[i_], qT_[i_][64:128, :, :], 0.0)
        Ec = [P.sb([128, 4, 128], BF16, "Ec%d" % i) for i in range(4)]
        Eb = [P.sb([128, 4, 128], BF16, "Eb%d" % i) for i in range(3)]
        m12_ = [P.sb([128, 2, 128], F32, "m12_%d" % i) for i in range(2)]
        imp = P.sb([128, 128], F32, "imp"); imp2 = P.sb([128, 128], F32, "imp2"); m8 = P.sb([128, 16], F32, "m8")
        mk = P.sb([128, 128], BF16, "mk"); maskT4 = P.sb([128, 4, 128], BF16, "maskT4")
        rden = P.sb([128, 4], F32, "rden"); coef = P.sb([128, 4], F32, "coef")
        oacc = P.sb([128, 4, 64], F32, "oacc"); otmp = P.sb([128, 4, 64], F32, "otmp"); onsa = P.sb([128, 256], BF16, "onsa")
        pq = P.ps([64, 4, 128], F32, "pq")
        psc = [P.ps([128, 4, 128], F32, "psc%d" % i) for i in range(2)]
        pn = P.ps([128, 4, 128], F32, "pn")
        pnT = P.ps([65, 4, 128], F32, "pnT")
        pnT2 = P.ps([65, 4, 128], F32, "pnT2")
        numTs = P.sb([65, 4, 128], F32, "numTs")
        pimp = P.ps([128, 4, 128], F32, "pimp")
        pmt = P.ps([128, 128], BF16, "pmt")
        for g in range(2):
            dma(ksT, ksT[0:64, :], d_kT, d_kT[2, g * 64:(g + 1) * 64, :])
            dma(farf, farf[:], c_far, c_far[g])
            for i_ in range(2):
                cp('dve', qT_[i_], qT_[i_][64:65, :, :], farf, farf[64:65, :].rearrange("p (h q) -> p h q", h=4))
            dma(kwT, kwT[0:64, :], d_kT, d_kT[3, g * 64:(g + 1) * 64, :])
            for n0 in range(0, NT, 16):
                n1 = min(NT, n0 + 16)
                dma(vs, vs[:, n0:n1, 0:64], d_vs, d_vs[n0 * 128:n1 * 128, g * 64:(g + 1) * 64].rearrange("(n p) c -> p n c", p=128))
                dma(vw, vw[:, n0:n1, 0:64], d_vw, d_vw[n0 * 128:n1 * 128, g * 64:(g + 1) * 64].rearrange("(n p) c -> p n c", p=128))
            memset('dve', vs, vs[:, :, 64:65], 1.0)
            memset('dve', vw, vw[:, :, 64:65], 1.0)
            k = 0
            for m in range(15):
                dma(bst[k % 2], bst[k % 2][:], c_selb, c_selb[g, :, m, :])
                tt('dve', selb, selb[:, m, :], bst[k % 2], bst[k % 2][:], farf, farf[:], ALU.subtract); k += 1
            for m in range(6):
                dma(bst[k % 2], bst[k % 2][:], c_winb, c_winb[g, :, m, :])
                cp('dve', winb2, winb2[:, m, :], bst[k % 2], bst[k % 2][:]); k += 1
            ne = 0
            ne_ = [0]
            for jj in range(NO):
                qrows, grows, gown, qT, m12 = qrows_[jj % 2], grows_[jj % 2], gown_[jj % 2], qT_[jj % 2], m12_[jj % 2]

                def load_q(j2):
                    dma(qrows_[j2 % 2], qrows_[j2 % 2][:], d_nq, d_nq[j2 * 256:(j2 + 1) * 256, g * 256:(g + 1) * 256].rearrange("(u p) c -> p u c", p=128))
                    dma(grows_[j2 % 2], grows_[j2 % 2][:], d_ng, d_ng[j2 * 256:(j2 + 1) * 256, :].rearrange("(u p) c -> p u c", p=128))
                    dma(m12_[j2 % 2], m12_[j2 % 2][:], c_m12, c_m12[j2])
                if jj == 0:
                    load_q(0)
                if jj + 1 < NO:
                    load_q(jj + 1)
                for h in range(4):
                    for u in range(2):
                        mm(pq, pq[:, h, :], qrows, qrows[:, u, h * 64:(h + 1) * 64], selu, selu[:, u, :], start=(u == 0), stop=(u == 1))
                cp('act', qT, qT[0:64], pq, pq[:])
                ts('dve', gtmp, gtmp[:], grows, grows[:, 0, g * 12:(g + 1) * 12], pv[:, 0:1], None, ALU.mult, rd=[pv])
                stt('dve', gown, gown[:], grows, grows[:, 1, g * 12:(g + 1) * 12], pv[:, 1:2], gtmp, gtmp[:], ALU.mult, ALU.add, rd=[pv])
                gv3 = gown[:].rearrange("p (h k) -> p h k", k=3)
                qT_all = qT[:].rearrange("p h q -> p (h q)")
                qT_aug = qT_all

                def finish_branch(pn, br, first):
                    ts('dve', rden, rden[:], pn, pn[:, :, 64], 1e-30, None, ALU.max)
                    recip(rden, rden[:], rden, rden[:])
                    tt('dve', coef, coef[:], rden, rden[:], gown, gv3[:, :, br], ALU.mult)
                    dst = oacc if first else otmp
                    tt('dve', dst, dst[:], pn, pn[:, :, 0:64], coef, coef[:].unsqueeze(2).to_broadcast([128, 4, 64]), ALU.mult)
                    if not first:
                        tt('pool', oacc, oacc[:], oacc, oacc[:], otmp, otmp[:], ALU.add)
                    if debug:
                        dma(d_dbg, d_dbg[br, jj * 128:(jj + 1) * 128, g * 256:(g + 1) * 256], oacc, oacc[:].rearrange("p h d -> p (h d)"))

                def back_T(src):
                    cp('act', numTs, numTs[:], src, src[:])
                    for h in range(4):
                        P.op('pe', lambda e, h=h: e.transpose(out=pn[:, h, 0:65], in_=numTs[:, h, :], identity=identf[0:65, 0:65]), [numTs, identf], [pn])

                ncc = min(NCC, cmp_nchunks(jj))
                for c in range(ncc):
                    pi = pair_off[jj] + c
                    dma(bst[k % 2], bst[k % 2][:], c_cmpb, c_cmpb[g, pi])
                    cp('dve', cbt[k % 2], cbt[k % 2][:], bst[k % 2], bst[k % 2][:])
                    sc = psc[ne % 2]; ne += 1
                    sc_all = sc[:].rearrange("p h q -> p (h q)")
                    mm(sc, sc_all, kcmpT[g], kcmpT[g][:, c * 128:(c + 1) * 128], qT, qT_all, start=True, stop=False)
                    mm(sc, sc_all, ident, ident[:], cbt[k % 2], cbt[k % 2][:], start=False, stop=True)
                    k += 1
                    act(Ec[c], Ec[c][:], sc, sc[:], AF.Exp)
                for h in range(4):
                    for c in range(ncc):
                        mm(pn, pn[:, h, 0:65], Ec[c], Ec[c][:, h, :], vcmp[g], vcmp[g][:, c, :], start=(c == 0), stop=(c == ncc - 1))
                    for c in range(ncc):
                        mm(pimp, pimp[:, h, :], Ec[c], Ec[c][:, h, :], wimp, wimp[:, c, :], start=(c == 0), stop=(c == ncc - 1))
                nk = 2 * jj + 2
                k0 = max(0, 2 * jj - 4)
                bufs = {}

                def score(kind, kc, n):
                    sc = psc[ne_[0] % 2]; E = Eb[ne_[0] % 3]; ne_[0] += 1
                    bufs[n] = E
                    sc_all = sc[:].rearrange("p h q -> p (h q)")
                    if kind == 's':
                        m = 2 * jj + 1 - kc
                        mm(sc, sc_all, ksT, ksT[:, kc * 128:(kc + 1) * 128], qT, qT_aug, start=True, stop=False)
                        if m < 14:
                            mm(sc, sc_all, ident, ident[:], selb, selb[:, m, :], start=False, stop=False)
                        mm(sc, sc_all, exm, exm[:, kc * 128:(kc + 1) * 128], maskT4, mT_all, start=False, stop=True)
                    else:
                        m = 2 * jj + 1 - kc
                        mm(sc, sc_all, kwT, kwT[:, kc * 128:(kc + 1) * 128], qT, qT_all, start=True, stop=False)
                        mm(sc, sc_all, ident, ident[:], winb2, winb2[:, m, :], start=False, stop=True)
                    act(E, E[:], sc, sc[:], AF.Exp)

                def pvs(kind, kc, n):
                    E = bufs.pop(n)
                    if kind == 's':
                        mm(pnT, pnT[:].rearrange("p h q -> p (h q)"), vs, vs[:, kc, :], E, E[:].rearrange("p h q -> p (h q)"), start=(kc == 0), stop=(kc == nk - 1))
                    else:
                        mm(pnT2, pnT2[:].rearrange("p h q -> p (h q)"), vw, vw[:, kc, :], E, E[:].rearrange("p h q -> p (h q)"), start=(kc == k0), stop=(kc == nk - 1))

                def run_items(items):
                    NI = len(items)
                    for n in range(NI + 1):
                        if n < NI:
                            score(items[n][0], items[n][1], n)
                        if n >= 1:
                            pvs(items[n - 1][0], items[n - 1][1], n - 1)
                mT_all = maskT4[:].rearrange("p h q -> p (h q)")
                run_items([('w', kc) for kc in range(k0, nk)])
                finish_branch(pn, 0, True)
                for h in range(4):
                    if h == 0:
                        ts('dve', imp, imp[:], pimp, pimp[:, 0, :], rden[:, 0:1], None, ALU.mult, rd=[rden])
                    else:
                        stt('dve', imp, imp[:], pimp, pimp[:, h, :], rden[:, h:h + 1], imp, imp[:], ALU.mult, ALU.add, rd=[rden])
                tt('dve', imp, imp[:], imp, imp[:], m12, m12[:, 0, :], ALU.mult)
                tt('dve', imp, imp[:], imp, imp[:], m12, m12[:, 1, :], ALU.add)
                if debug:
                    dma(d_imp, d_imp[g, jj * 128:(jj + 1) * 128, :], imp, imp[:])
                P.op('dve', lambda e: e.max(out=m8[:, 0:8], in_=imp[:]), [imp], [m8])
                P.op('dve', lambda e: e.match_replace(out=imp2[:], in_to_replace=m8[:, 0:8], in_values=imp[:], imm_value=-1e30), [imp, m8], [imp2])
                P.op('dve', lambda e: e.max(out=m8[:, 8:16], in_=imp2[:]), [imp2], [m8])
                ts('dve', mk, mk[:], imp, imp[:], m8[:, 15:16], 1.0, ALU.is_ge, ALU.subtract, rd=[m8])
                tr(pmt, pmt[:], mk, mk[:], ident)
                P.op('act', lambda e: e.mul(out=maskT4[:], in_=pmt[:].unsqueeze(1).to_broadcast([128, 4, 128]), mul=30000.0), [pmt], [maskT4])
                run_items([('s', kc) for kc in range(nk)])
                back_T(pnT)
                finish_branch(pn, 1, False)
                back_T(pnT2)
                finish_branch(pn, 2, False)
                cp('act', onsa, onsa[:], oacc, oacc[:].rearrange("p h d -> p (h d)"))
                dma(d_omix, d_omix[jj * 128:(jj + 1) * 128, 512 + g * 256:512 + (g + 1) * 256], onsa, onsa[:])
        P.pop()

    if upto >= 3:
        P.push()
        dma_pol['load'] = ['sp']; dma_pol['store'] = ['pool']
        woutb = P.sb([128, 8, D], BF16, "woutb"); wqb = P.sb([128, 8, D], BF16, "wqb"); wob = P.sb([128, 8, D], BF16, "wob")
        pwqb = P.sb([128, 8, D], BF16, "pwqb")
        skb = P.sb([128, 8, 256], BF16, "skb")
        kTm = P.sb([128, 8, 256], BF16, "kTm")
        vm = P.sb([128, 2, 4, 257], BF16, "vm")
        xt = [P.sb([128, D], F32, "x3_%d" % i) for i in range(2)]
        junk = P.sb([128, D], BF16, "junk3"); ss = P.sb([128, 1], F32, "ss3"); rstd = P.sb([128, 1], F32, "rstd3")
        hn = P.sb([128, D], BF16, "hn3"); hnT = P.sb([128, 8, 128], BF16, "hnT3")
        pt = P.ps([128, 8, 128], BF16, "pt3")
        pa = [P.ps([128, 512], F32, "pa%d" % i) for i in range(4)]
        pxa = P.ps([128, 2, 512], F32, "pxa")
        P.push()
        wst = [P.sb([128, D], F32, "wst3_%d" % i) for i in range(2)]
        wtmp = P.sb([128, 8, D], BF16, "wtmp"); skf = P.sb([128, 8, 256], F32, "skf")
        memT = P.sb([128, 8, 256], BF16, "memT")
        k = [0]

        def load_w(dst, src_t, src_ap3):
            for c in range(8):
                a = wst[k[0] % 2]
                dma(a, a[:], src_t, src_ap3[c])
                cp(['dve', 'pool'][k[0] % 2], dst, dst[:, c, :], a, a[:]); k[0] += 1
        load_w(woutb, w_out, w_out[:].rearrange("(c p) n -> c p n", p=128))
        load_w(wqb, xa_w, xa_w[0].rearrange("(c p) n -> c p n", p=128))
        load_w(wob, xa_w, xa_w[3].rearrange("(c p) n -> c p n", p=128))
        load_w(pwqb, pwq, pwq[:].rearrange("(c p) n -> c p n", p=128))
        dma(skf, skf[:], skd, skd[:].rearrange("c p n -> p c n"))
        cp('dve', skb, skb[:], skf, skf[:])
        memset('dve', vm, vm[:, :, :, 256:257], 1.0)
        for mc in range(2):
            x_t = xt[mc % 2]
            dma(x_t, x_t[:], memb, memb[mc * 128:(mc + 1) * 128, :])
            rmsnorm(x_t, x_t[:], 2, hn, hn[:], junk, ss, rstd)
            for c in range(8):
                tr(pt, pt[:, c, :], hn, hn[:, c * 128:(c + 1) * 128], ident)
            cp('act', memT, memT[:, :, mc * 128:(mc + 1) * 128], pt, pt[:])
        load_w(wtmp, xa_w, xa_w[1].rearrange("(c p) n -> c p n", p=128))
        for oc in range(8):
            for c in range(8):
                mm(pa[0], pa[0][:, 0:256], wtmp, wtmp[:, c, oc * 128:(oc + 1) * 128], memT, memT[:, c, :], start=(c == 0), stop=(c == 7))
            cp('act', kTm, kTm[:, oc, :], pa[0], pa[0][:, 0:256])
        load_w(wtmp, xa_w, xa_w[2].rearrange("(c p) n -> c p n", p=128))
        for mc in range(2):
            for half in range(2):
                for c in range(8):
                    mm(pa[half], pa[half][:], memT, memT[:, c, mc * 128:(mc + 1) * 128], wtmp, wtmp[:, c, half * 512:(half + 1) * 512], start=(c == 0), stop=(c == 7))
                cp('act', vm, vm[:, mc, half * 2:half * 2 + 2, 0:256], pa[half], pa[half][:].rearrange("p (h d) -> p h d", d=256))
        P.pop()
        og2 = P.sb([128, 2, 512], BF16, "og2"); omx = P.sb([128, D], BF16, "omx"); otm = P.sb([128, 512], F32, "otm")
        h1 = P.sb([128, D], F32, "h1"); qTx = P.sb([128, 8, 128], BF16, "qTx")
        Ex = P.sb([128, 2, 4, 128], BF16, "Ex"); rdx = P.sb([128, 4], F32, "rdx")
        oxa = P.sb([128, D], BF16, "oxa")
        hn3T = P.sb([128, 8, 128], BF16, "hn3Ts"); qpT = P.sb([128, 8, 128], BF16, "qpT")
        sc_ = [P.sb([128, 16, 128], F32, "scr%d" % i) for i in range(2)]; ab_ = [P.sb([128, 16, 128], F32, "ab%d" % i) for i in range(2)]
        negm = P.sb([128, 16], F32, "negm"); t16 = P.sb([128, 16, 16], F32, "t16"); scr2_ = [P.sb([128, 128], F32, "scr2_%d" % i) for i in range(4)]
        candall = P.sb([128, 8, 256], F32, "candall"); cand2_ = [P.sb([128, 256], F32, "cand2_%d" % i) for i in range(4)]; c16 = P.sb([128, 8, 16], F32, "c16")
        route = P.sb([128, 16], F32, "route"); zs = P.sb([128, 8], F32, "zs")
        memset('dve', route, route[:], 0.0)
        def Xgen(jj):
            sc = sc_[jj % 2]
            x_t = xt[jj % 2]
            dma(x_t, x_t[:], xo, xo[jj * 128:(jj + 1) * 128, :])
            dma(og2, og2[:], d_ogla, d_ogla[jj * 256:(jj + 1) * 256, :].rearrange("(u p) c -> p u c", p=128))
            dma(omx, omx[:, 512:1024], d_omix, d_omix[jj * 128:(jj + 1) * 128, 512:1024])
            ts('dve', otm, otm[:], og2, og2[:, 0, :], pv[:, 0:1], None, ALU.mult, rd=[pv])
            stt('dve', omx, omx[:, 0:512], og2, og2[:, 1, :], pv[:, 1:2], otm, otm[:], ALU.mult, ALU.add, rd=[pv])
            for c in range(8):
                tr(pt, pt[:, c, :], omx, omx[:, c * 128:(c + 1) * 128], ident)
            cp('act', hnT, hnT[:], pt, pt[:])
            for half in range(2):
                for c in range(8):
                    mm(pa[half], pa[half][:], hnT, hnT[:, c, :], woutb, woutb[:, c, half * 512:(half + 1) * 512], start=(c == 0), stop=(c == 7))
                tt('dve', h1, h1[:, half * 512:(half + 1) * 512], pa[half], pa[half][:], x_t, x_t[:, half * 512:(half + 1) * 512], ALU.add)
            yield
            rmsnorm(h1, h1[:], 1, hn, hn[:], junk, ss, rstd)
            for c in range(8):
                tr(pt, pt[:, c, :], hn, hn[:, c * 128:(c + 1) * 128], ident)
            cp('act', hnT, hnT[:], pt, pt[:])
            for oc in range(8):
                pq_ = pa[2 + oc % 2]
                for c in range(8):
                    mm(pq_, pq_[:, 0:128], wqb, wqb[:, c, oc * 128:(oc + 1) * 128], hnT, hnT[:, c, :], start=(c == 0), stop=(c == 7))
                cp(['act', 'dve'][oc % 2], qTx, qTx[:, oc, :], pq_, pq_[:, 0:128])
                yield
            yield
            for mc in range(2):
                for h in range(4):
                    for dc in range(2):
                        mm(pxa, pxa[:, mc, h * 128:(h + 1) * 128], kTm, kTm[:, h * 2 + dc, mc * 128:(mc + 1) * 128], qTx, qTx[:, h * 2 + dc, :], start=(dc == 0), stop=(dc == 1))
            act(Ex, Ex[:].rearrange("p a h q -> p a (h q)"), pxa, pxa[:], AF.Exp, scale=1.0 / 16)
            for h in range(4):
                o_ap = pxa[:, h // 2, (h % 2) * 256:(h % 2) * 256 + 256]
                for mc in range(2):
                    mm(pa[h % 2], pa[h % 2][:, 0:257], Ex, Ex[:, mc, h, :], vm, vm[:, mc, h, :], start=(mc == 0), stop=(mc == 1))
                ts('dve', rdx, rdx[:, h:h + 1], pa[h % 2], pa[h % 2][:, 256:257], 1e-30, None, ALU.max)
                recip(rdx, rdx[:, h:h + 1], rdx, rdx[:, h:h + 1])
                ts('dve', oxa, oxa[:, h * 256:(h + 1) * 256], pa[h % 2], pa[h % 2][:, 0:256], rdx[:, h:h + 1], None, ALU.mult, rd=[rdx])
            yield
            for c in range(8):
                tr(pt, pt[:, c, :], oxa, oxa[:, c * 128:(c + 1) * 128], ident)
            cp('act', hnT, hnT[:], pt, pt[:])
            for half in range(2):
                for c in range(8):
                    mm(pa[half], pa[half][:], hnT, hnT[:, c, :], wob, wob[:, c, half * 512:(half + 1) * 512], start=(c == 0), stop=(c == 7))
                tt('dve', h1, h1[:, half * 512:(half + 1) * 512], pa[half], pa[half][:], h1, h1[:, half * 512:(half + 1) * 512], ALU.add)
            dma(d_h, d_h[jj * 128:(jj + 1) * 128, :], h1, h1[:])
            yield
            rmsnorm(h1, h1[:], 3, hn, hn[:], junk, ss, rstd)
            for c in range(8):
                tr(pt, pt[:, c, :], hn, hn[:, c * 128:(c + 1) * 128], ident)
            cp('act', hn3T, hn3T[:], pt, pt[:])
            dma(d_hn3T, d_hn3T[:, :, jj * 128:(jj + 1) * 128], hn3T, hn3T[:])
            for oc in range(8):
                pq_ = pa[2 + oc % 2]
                for c in range(8):
                    mm(pq_, pq_[:, 0:128], pwqb, pwqb[:, c, oc * 128:(oc + 1) * 128], hn3T, hn3T[:, c, :], start=(c == 0), stop=(c == 7))
                cp(['act', 'dve'][oc % 2], qpT, qpT[:, oc, :], pq_, pq_[:, 0:128])
                yield
            yield
            for oc in range(8):
                pq_ = pa[oc % 2]
                mm(pq_, pq_[:, 0:256], qpT, qpT[:, oc, :], skb, skb[:, oc, :])
                cp(['act', 'dve'][oc % 2], sc, sc[:, 2 * oc:2 * oc + 2, :], pq_, pq_[:, 0:256].rearrange("p (a k) -> p a k", a=2))
            yield

        def Rgen(jj):
            sc = sc_[jj % 2]; ab = ab_[jj % 2]
            P.op('dve', lambda e: e.tensor_reduce(out=negm[:], in_=sc[:], axis=AX.X, op=ALU.max), [sc], [negm])
            tt('dve', sc, sc[:], sc, sc[:], negm, negm[:].unsqueeze(2).to_broadcast([128, 16, 128]), ALU.subtract)
            act(ab, ab[:].rearrange("p r k -> p (r k)"), sc, sc[:].rearrange("p r k -> p (r k)"), AF.Exp)
            yield
            for r0 in range(0, 16, 4):
                for r in range(r0, r0 + 4):
                    P.op('dve', lambda e, r=r: e.max(out=t16[:, r, 0:8], in_=ab[:, r, :]), [ab], [t16])
                for r in range(r0, r0 + 4):
                    P.op('dve', lambda e, r=r: e.match_replace(out=scr2_[r % 4][:], in_to_replace=t16[:, r, 0:8], in_values=ab[:, r, :], imm_value=-1.0), [ab, t16], [scr2_[r % 4]])
                for r in range(r0, r0 + 4):
                    P.op('dve', lambda e, r=r: e.max(out=t16[:, r, 8:16], in_=scr2_[r % 4][:]), [scr2_[r % 4]], [t16])
                yield
            t16v = t16[:].rearrange("p (h a) k -> p h a k", a=2)
            abv = ab[:].rearrange("p (h a) k -> p h a k", a=2)

            def cand_top16():
                yield
                tt('dve', candall, candall[:].rearrange("p h (a b) -> p h a b", a=16),
                   t16, t16v[:, :, 0, :].unsqueeze(3).to_broadcast([128, 8, 16, 16]),
                   t16, t16v[:, :, 1, :].unsqueeze(2).to_broadcast([128, 8, 16, 16]), ALU.mult)
                for h0 in range(0, 8, 4):
                    for h in range(h0, h0 + 4):
                        P.op('dve', lambda e, h=h: e.max(out=c16[:, h, 0:8], in_=candall[:, h, :]), [candall], [c16])
                    for h in range(h0, h0 + 4):
                        P.op('dve', lambda e, h=h: e.match_replace(out=cand2_[h % 4][:], in_to_replace=c16[:, h, 0:8], in_values=candall[:, h, :], imm_value=-1.0), [candall, c16], [cand2_[h % 4]])
                    for h in range(h0, h0 + 4):
                        P.op('dve', lambda e, h=h: e.max(out=c16[:, h, 8:16], in_=cand2_[h % 4][:]), [cand2_[h % 4]], [c16])
                    yield
            yield from cand_top16()
            P.op('dve', lambda e: e.tensor_reduce(out=zs[:], in_=c16[:], axis=AX.X, op=ALU.add), [c16], [zs])
            recip(zs, zs[:], zs, zs[:])
            tt('dve', ab, abv[:, :, 1, :], ab, abv[:, :, 1, :], zs, zs[:].unsqueeze(2).to_broadcast([128, 8, 128]), ALU.mult)
            stt('dve', route, route[:, 0:8], c16, c16[:, :, 15], 1.0 - 1e-6, zs, zs[:], ALU.mult, ALU.mult)
            dma(d_route, d_route[jj * 128:(jj + 1) * 128, 0:2048], ab, ab[:].rearrange("p r k -> p (r k)"))
            dma(d_route, d_route[jj * 128:(jj + 1) * 128, 2048:2064], route, route[:])
            yield

        def drain(g):
            for _ in g:
                pass
        drain(Xgen(0))
        for jj in range(NO):
            gr = Rgen(jj)
            gx = Xgen(jj + 1) if jj + 1 < NO else iter(())
            ra = xa_ = True
            while ra or xa_:
                if xa_:
                    xa_ = next(gx, 'END') != 'END'
                if ra:
                    ra = next(gr, 'END') != 'END'
        P.pop()

    if upto >= 4:
        P.push()
        dma_pol['load'] = ['sp', 'pool']; dma_pol['store'] = ['sp']
        TG = 2
        IC = 16
        NCH = 128 // IC
        ACT_HEADS = (1, 3, 4, 6, 7)
        hT_ = [P.sb([128, 8, TG * 128], BF16, "hT%d" % i) for i in range(2)]
        ab_ = [[P.sb([128, 16, 128], F32, "ab4_%d_%d" % (i, u)) for u in range(TG)] for i in range(2)]
        rt_ = [[P.sb([128, 16], F32, "rt%d_%d" % (i, u)) for u in range(TG)] for i in range(2)]
        Wc = [[P.sb([128, IC * 128], BF16, "Wc%d_%d" % (i, u)) for u in range(TG)] for i in range(2)]
        et = [P.sb([128, IC, 128], F32, "et%d" % i) for i in range(4)]
        mt = [P.sb([128, IC, 128], BF16, "mt%d" % i) for i in range(2)]
        dnb = [P.sb([128, 8, 512], BF16, "dnb%d" % i) for i in range(3)]
        upb = [P.sb([128, 4, D], BF16, "upb%d" % i) for i in range(3)]
        Gs = [P.sb([128, 512], BF16, "G%d" % i) for i in range(2)]
        GT = [P.sb([128, 4, 128], BF16, "GT%d" % i) for i in range(2)]
        py = [P.ps([128, 2, 512], F32, "py%d" % u) for u in range(TG)]
        pd = [P.ps([128, 512], F32, "pd%d" % i) for i in range(2)]
        ptg = [P.ps([128, 8, 128], BF16, "ptg%d" % i) for i in range(2)]
        h2 = P.sb([128, D], F32, "h2"); yo = P.sb([128, D], F32, "yo")
        junk = P.sb([128, D], BF16, "junk4"); ss = P.sb([128, 1], F32, "ss4"); rstd = P.sb([128, 1], F32, "rstd4")
        qn = [0]
        def load_group(t2):
            hT2, ab2, rt2 = hT_[t2 % 2], ab_[t2 % 2], rt_[t2 % 2]
            dma(hT2, hT2[:], d_hn3T, d_hn3T[:, :, t2 * TG * 128:(t2 + 1) * TG * 128])
            for u in range(TG):
                j = t2 * TG + u
                dma(ab2[u], ab2[u][:].rearrange("p r k -> p (r k)"), d_route, d_route[j * 128:(j + 1) * 128, 0:2048])
                dma(rt2[u], rt2[u][:], d_route, d_route[j * 128:(j + 1) * 128, 2048:2064])
        load_group(0)
        for tg in range(NO // TG):
            hT, ab, rt = hT_[tg % 2], ab_[tg % 2], rt_[tg % 2]
            if tg + 1 < NO // TG:
                load_group(tg + 1)

            def wgen(c):
                its = [(u, h) for u in range(TG) for h in range(8)]
                K = len(its)
                eb_ = {}; mb_ = {}

                def E_(n):
                    u, h = its[n]
                    e_ = et[qn[0] % 4]; qn[0] += 1
                    eb_[n] = e_
                    if h in ACT_HEADS:
                        for i_ in range(IC):
                            P.op('act', lambda e, e_=e_, i_=i_, u=u, h=h: e.activation(out=e_[:, i_, :], in_=ab[u][:, 2 * h + 1, :], func=AF.Copy,
                                                                                  scale=ab[u][:, 2 * h, c * IC + i_:c * IC + i_ + 1]),
                                 [ab[u]], [e_] if i_ in (0, IC - 1) else [])
                    else:
                        tt('dve', e_, e_[:], ab[u], ab[u][:, 2 * h, c * IC:(c + 1) * IC].unsqueeze(2).to_broadcast([128, IC, 128]),
                           ab[u], ab[u][:, 2 * h + 1, :].unsqueeze(1).to_broadcast([128, IC, 128]), ALU.mult)

                def S_(n):
                    u, h = its[n]
                    e_ = eb_.pop(n)
                    w_ap = Wc[c % 2][u][:].rearrange("p (a b) -> p a b", a=IC)
                    if h == 0:
                        stt('dve', Wc[c % 2][u], w_ap, e_, e_[:], rt[u][:, h:h + 1], e_, e_[:], ALU.is_ge, ALU.mult, rd=[rt[u]])
                    else:
                        m_ = mt[n % 2]
                        mb_[n] = m_
                        stt('dve', m_, m_[:], e_, e_[:], rt[u][:, h:h + 1], e_, e_[:], ALU.is_ge, ALU.mult, rd=[rt[u]])

                def A_(n):
                    u, h = its[n]
                    if h == 0:
                        return
                    m_ = mb_.pop(n)
                    w_ap = Wc[c % 2][u][:].rearrange("p (a b) -> p a b", a=IC)
                    tt('dve', Wc[c % 2][u], w_ap, Wc[c % 2][u], w_ap, m_, m_[:], ALU.add)
                for n in range(K + 2):
                    if n < K:
                        E_(n)
                    if 1 <= n <= K:
                        S_(n - 1)
                    if n >= 2:
                        A_(n - 2)
                    yield

            def load_w(ecx):
                dn, ub = dnb[ecx % 3], upb[ecx % 3]
                dma(dn, dn[:], d_downT, d_downT[:, ecx * 512:(ecx + 1) * 512].rearrange("(c p) e -> p c e", p=128))
                dma(ub, ub[:], d_up, d_up[ecx * 512:(ecx + 1) * 512, :].rearrange("(s p) d -> p s d", p=128))

            items = [(ecx, u) for ecx in range(32) for u in range(TG)]
            N = len(items)

            def stA(n):
                ecx, u = items[n]
                if u == 0:
                    if ecx == 0:
                        load_w(0)
                    if ecx + 1 < 32:
                        load_w(ecx + 1)
                    if ecx % 4 == 0:
                        for _ in wg[0]:
                            pass
                        wg[0] = wgen(ecx // 4 + 1) if ecx // 4 + 1 < NCH else iter(())
                for _ in range(3):
                    next(wg[0], None)
                dn = dnb[ecx % 3]
                pdt = pd[n % 2]; G = Gs[n % 2]
                for c in range(8):
                    mm(pdt, pdt[:], hT, hT[:, c, u * 128:(u + 1) * 128], dn, dn[:, c, :], start=(c == 0), stop=(c == 7))
                act(G, G[:], pdt, pdt[:], AF.Gelu_apprx_tanh)
                wch = Wc[(ecx // 4) % 2][u]
                tt('dve', G, G[:], G, G[:], wch, wch[:, (ecx % 4) * 512:(ecx % 4 + 1) * 512], ALU.mult)

            def stB(n):
                G = Gs[n % 2]; gt_ = GT[n % 2]; pt_ = ptg[n % 2]
                for s_ in range(4):
                    tr(pt_, pt_[:, s_, :], G, G[:, s_ * 128:(s_ + 1) * 128], ident)
                cp('act', gt_, gt_[:], pt_, pt_[:, 0:4, :])

            def stC(n):
                ecx, u = items[n]
                gt_ = GT[n % 2]; ub = upb[ecx % 3]
                for half in range(2):
                    for s_ in range(4):
                        mm(py[u], py[u][:, half, :], gt_, gt_[:, s_, :], ub, ub[:, s_, half * 512:(half + 1) * 512],
                           start=(ecx == 0 and s_ == 0), stop=(ecx == 31 and s_ == 3))

            wg = [iter(())]
            for _ in wgen(0):
                pass
            for n in range(N + 2):
                if n < N:
                    stA(n)
                if 1 <= n <= N:
                    stB(n - 1)
                if n >= 2:
                    stC(n - 2)
            for _ in wg[0]:
                pass
            for u in range(TG):
                j = tg * TG + u
                dma(h2, h2[:], d_h, d_h[j * 128:(j + 1) * 128, :])
                tt('dve', h2, h2[:], h2, h2[:], py[u], py[u][:].rearrange("p a b -> p (a b)"), ALU.add)
                rmsnorm(h2, h2[:], 4, yo, yo[:], junk, ss, rstd)
                dma(out, out[j * 128:(j + 1) * 128, :], yo, yo[:])
        P.pop()

    P.finish()
    return nc, dict(npair=npair, pair_off=pair_off, NCC=NCC, NCMP=NCMP, ninst=P.ninst)


def _t5_bucket_np(dist):
    n = np.maximum(dist, 0)
    nf = np.maximum(n, 1).astype(np.float32)
    log_ratio = (np.log(nf / np.float32(16)) / np.float32(math.log(2048 / 16))).astype(np.float32)
    large = 16 + (log_ratio * np.float32(16)).astype(np.int32)
    large = np.minimum(large, 31)
    return np.where(n < 16, n, large).astype(np.int64)


def _bias_tile(rel_bias, g, dist, valid):
    bk = _t5_bucket_np(dist)
    outt = np.empty((128, 4, 128), np.float32)
    for h in range(4):
        outt[:, h, :] = np.where(valid, rel_bias[bk, g * 4 + h], np.float32(NEG))
    return outt.reshape(128, 512)


def make_core_inputs(inputs, T, b, p, meta):
    NT = T // 128; NO = NT // 2; NS = T // 64; NCMP = meta['NCMP']; NCC = meta['NCC']
    f = lambda a: np.ascontiguousarray(np.asarray(a, dtype=np.float32))
    x = f(inputs['x'][b]); rel_bias = f(inputs['rel_bias'])
    m = {}
    m['xb'] = x
    m['xo'] = np.ascontiguousarray(x.reshape(NO, 2, 128, D)[:, p].reshape(NO * 128, D))
    m['mem'] = f(inputs['mem'][b])
    w_in = f(inputs['w_in'][0])
    m['w_in'] = np.ascontiguousarray(np.concatenate([w_in[:, ORIG[k][0]:ORIG[k][1]] for k in PERM_ORDER], axis=1))
    m['gw2'] = f(inputs['gla_gate_w2'][0]); m['gb'] = f(inputs['gla_gate_b'][0]).reshape(1, 256)
    m['gnorm'] = f(inputs['gla_out_norm'][0]).reshape(1, 128)
    m['norms'] = np.stack([f(inputs['norm_mix'][0]), f(inputs['norm_xattn'][0]), f(inputs['norm_mem'][0]),
                           f(inputs['norm_ffn'][0]), f(inputs['norm_final'])], 0)
    cw1 = np.stack([f(inputs['cmp_k_w1'][0]), f(inputs['cmp_v_w1'][0])], 0)
    m['cw1'] = np.ascontiguousarray(cw1.reshape(2, 32, 64, 128).transpose(0, 2, 1, 3))
    cpos = np.stack([f(inputs['cmp_pos_k'][0]), f(inputs['cmp_pos_v'][0])], 0)
    m['cpos'] = np.ascontiguousarray(cpos.transpose(0, 2, 1))
    m['cw2'] = np.stack([f(inputs['cmp_k_w2'][0]), f(inputs['cmp_v_w2'][0])], 0)
    m['w_out'] = f(inputs['w_out'][0])
    m['xa_w'] = np.stack([f(inputs['xa_wq'][0]), f(inputs['xa_wk'][0]), f(inputs['xa_wv'][0]), f(inputs['xa_wo'][0])], 0)
    m['pwq'] = f(inputs['peer_wq'][0])
    sk = f(inputs['peer_subkeys'][0])
    skd = np.zeros((8, 128, 256), np.float32)
    for h in range(8):
        for pp in range(2):
            skd[h, pp * 64:(pp + 1) * 64, pp * 128:(pp + 1) * 128] = sk[h, pp].T
    m['skd'] = skd
    m['downT'] = np.ascontiguousarray(f(inputs['peer_down'][0]).T)
    m['up'] = f(inputs['peer_up'][0])
    m['c_ident'] = np.eye(128, dtype=np.float32)
    s_ = np.arange(128)[:, None]; t_ = np.arange(128)[None, :]
    m['c_ucs'] = np.where(s_ <= t_, -1.0 / 16, 0.0).astype(np.float32)
    m['c_urev'] = np.where(s_ > t_, -1.0 / 16, 0.0).astype(np.float32)
    m['c_causal'] = (s_ <= t_).astype(np.float32)
    m['c_selu'] = np.stack([np.eye(128) * (1 - p), np.eye(128) * p], 0).astype(np.float32)
    m['c_pv'] = np.tile(np.array([[1.0 - p, float(p)]], np.float32), (128, 1))
    kk = np.arange(128)[:, None]; qq = np.arange(128)[None, :]
    selb = np.empty((2, 128, 15, 512), np.float32); winb = np.empty((2, 128, 6, 512), np.float32)
    for g in range(2):
        for mm_ in range(15):
            j = mm_ - 1 + p
            dist = 128 * j + qq - kk
            selb[g, :, mm_, :] = _bias_tile(rel_bias, g, dist, (dist >= 0) & (j >= 0))
        for mm_ in range(6):
            j = mm_ - 1 + p
            dist = 128 * j + qq - kk
            winb[g, :, mm_, :] = _bias_tile(rel_bias, g, dist, (dist >= 0) & (dist < 512) & (j >= 0))
    m['c_selb'] = selb; m['c_winb'] = winb
    cmpb = np.empty((2, meta['npair'], 128, 512), np.float32)
    for jj in range(NO):
        for c in range(min(NCC, cmp_nchunks(jj))):
            n = 128 * c + kk
            t = (2 * jj + p) * 128 + qq
            dist = t - (16 * n + 31)
            for g in range(2):
                cmpb[g, meta['pair_off'][jj] + c] = _bias_tile(rel_bias, g, dist, (dist >= 0) & (n < NCMP))
    m['c_cmpb'] = cmpb
    far = np.empty((2, 128, 4, 128), np.float32)
    for g in range(2):
        for h in range(4):
            far[g, :, h, :] = rel_bias[31, g * 4 + h]
    m['c_far'] = far.reshape(2, 128, 512)
    wimp = np.zeros((NCC * 128, 128), np.float32)
    for s in range(NS):
        for r in range(-1, 4):
            n = 4 * s + r
            lo = 16 * r
            ov = min(lo + 32, 64) - max(lo, 0)
            if 0 <= n < NCMP:
                wimp[n, s] += ov / 32.0
    m['c_wimp'] = wimp.reshape(NCC, 128, 128)
    m12 = np.zeros((NO, 128, 2, 128), np.float32)
    sid = np.arange(128)[None, :]
    for jj in range(NO):
        t = (2 * jj + p) * 128 + np.arange(128)[:, None]
        cur = t // 64
        visible = (sid * 64 <= t) & (sid < NS)
        f0 = (sid == 0); f1 = (sid == cur); f2 = (sid == cur - 1)
        forced = f0 | f1 | f2
        m12[jj, :, 0, :] = (visible & ~forced)
        add = np.where(~visible, -100.0 - sid, 0.0)
        add = np.where(f2, 100.0, add); add = np.where(f1, 101.0, add); add = np.where(f0 & (sid < NS), 102.0, add)
        m12[jj, :, 1, :] = add
    m['c_m12'] = m12
    ex = np.zeros((128, T), np.float32)
    ex[np.arange(T) // 64, np.arange(T)] = 1.0
    m['c_ex'] = ex.astype(NPBF)
    return m


_CACHE = {}


def kernel(**inputs):
    T = inputs['x'].shape[1]
    B = inputs['x'].shape[0]
    if T not in _CACHE:
        _CACHE[T] = build(T)
    nc, meta = _CACHE[T]
    in_maps = []
    for c in range(2 * B):
        in_maps.append(make_core_inputs(inputs, T, c // 2, c % 2, meta))
    res = run_bass_kernel_spmd(nc, in_maps, core_ids=list(range(2 * B)))
    NO = T // 256
    outp = np.empty((B, T // 128, 128, D), np.float32)
    for c in range(2 * B):
        o = np.asarray(res.results[c]["out"], dtype=np.float32).reshape(NO, 128, D)
        outp[c // 2, (c % 2)::2] = o
    return outp.reshape(B, T, D)
```

```python
import math
import numpy as np
import ml_dtypes
import concourse.bass as bass
import concourse.mybir as mybir
from concourse.bass_utils import run_bass_kernel_spmd
from contextlib import ExitStack

F32 = mybir.dt.float32
BF16 = mybir.dt.bfloat16
AF = mybir.ActivationFunctionType
ALU = mybir.AluOpType
AX = mybir.AxisListType
NPBF = ml_dtypes.bfloat16

D = 1024
NEG = -30000.0


class T:
    def __init__(self, h, name):
        self.h = h
        self.name = name
        self.w = None
        self.r = []
        self.psum = False
        self.dram = False

    def __getitem__(self, k):
        return self.h[k]


class Prog:
    def __init__(self, nc, n_dma_sems=48):
        self.nc = nc
        self.es = ExitStack()
        self.scopes = []
        self.eng = {'pe': nc.tensor, 'dve': nc.vector, 'act': nc.scalar,
                    'pool': nc.gpsimd, 'sp': nc.sync}
        self.sems = {}
        for k in self.eng:
            self.sems['e_' + k] = self.es.enter_context(nc.semaphore('e_' + k))
        self.cnt = {k: 0 for k in self.sems}
        self.ndma = n_dma_sems
        for i in range(n_dma_sems):
            key = 'd_%d' % i
            self.sems[key] = self.es.enter_context(nc.semaphore(key))
            self.cnt[key] = 0
        self.dma_rr = 0
        self.known = {k: {} for k in self.eng}
        self.ntile = 0
        self.ninst = 0

    def push(self):
        self.scopes.append(ExitStack())

    def pop(self):
        self.barrier()
        self.scopes.pop().close()

    def _stack(self):
        return self.scopes[-1] if self.scopes else self.es

    def sb(self, shape, dt, name=None):
        self.ntile += 1
        name = (name or 't') + '_%d' % self.ntile
        h = self._stack().enter_context(self.nc.sbuf_tensor(name, list(shape), dt))
        return T(h, name)

    def ps(self, shape, dt, name=None):
        self.ntile += 1
        name = (name or 'p') + '_%d' % self.ntile
        h = self._stack().enter_context(self.nc.psum_tensor(name, list(shape), dt))
        t = T(h, name)
        t.psum = True
        return t

    def dram(self, name, shape, dt, kind="Internal"):
        h = self.nc.dram_tensor(name, list(shape), dt, kind=kind).ap()
        t = T(h, name)
        t.dram = True
        return t

    def _deps(self, reads, writes, e=None):
        deps = []
        for t in reads:
            if t.w is not None:
                deps.append(t.w)
            if t.psum:
                deps.extend([tok for tok in t.r if tok[0] != 'e_' + str(e)])
        for t in writes:
            if t.w is not None:
                deps.append(t.w)
            deps.extend(t.r)
        return deps

    def _waits(self, e, deps):
        kn = self.known[e]
        need = {}
        for (k, v) in deps:
            if kn.get(k, 0) >= v:
                continue
            if need.get(k, 0) < v:
                need[k] = v
        for k, v in need.items():
            kn[k] = v
        return list(need.items())

    def _emit(self, e, waits, fn, inc):
        eng = self.eng[e]
        for (k, v) in waits:
            eng.wait_ge(self.sems[k], v)
        ins = fn(eng)
        ins.then_inc(self.sems[inc[0]], inc[1])
        self.ninst += 1

    def op(self, e, fn, reads=(), writes=()):
        deps = self._deps(reads, writes, e)
        key = 'e_' + e
        if e == 'pe':
            deps = [d for d in deps if d[0] != key]
        waits = self._waits(e, deps)
        self.cnt[key] += 1
        tok = (key, self.cnt[key])
        self._emit(e, waits, fn, (key, 1))
        for t in reads:
            t.r.append(tok)
        for t in writes:
            t.w = tok
            t.r = []
        return tok

    def dma(self, e, out_t, out_ap, in_t, in_ap, **kw):
        reads = [in_t]
        writes = [out_t]
        deps = self._deps(reads, writes)
        key = 'd_%d' % self.dma_rr
        self.dma_rr = (self.dma_rr + 1) % self.ndma
        if self.cnt[key] > 0:
            deps.append((key, self.cnt[key]))
        waits = self._waits(e, deps)
        self.cnt[key] += 16
        tok = (key, self.cnt[key])
        self._emit(e, waits, lambda eng: eng.dma_start(out=out_ap, in_=in_ap, **kw), (key, 16))
        in_t.r.append(tok)
        out_t.w = tok
        out_t.r = []
        return tok

    def barrier(self):
        allt = [(k, v) for k, v in self.cnt.items() if v > 0]
        for e in self.eng:
            for (k, v) in self._waits(e, allt):
                self.eng[e].wait_ge(self.sems[k], v)

    def finish(self):
        self.barrier()
        while self.scopes:
            self.scopes.pop().close()
        self.es.close()


ORIG = dict(gq=(0, 256), gk=(256, 512), gv=(512, 1024), gr=(1024, 1536), glr=(1536, 1552),
            nq=(1552, 2064), kc=(2064, 2192), vc=(2192, 2320), ks=(2320, 2448), vs=(2448, 2576),
            kw=(2576, 2704), vw=(2704, 2832), ng=(2832, 2856))
PERM_ORDER = ['gq', 'gk', 'gv', 'gr', 'nq', 'kc', 'vc', 'ks', 'vs', 'kw', 'vw', 'ng', 'glr']
NCOL = 2856


def cmp_nchunks(jj):
    return min(4, (16 * jj + 15 + 127) // 128)


def build(T, debug=False, upto=9, cut=99):
    NT = T // 128
    NO = NT // 2
    NS = T // 64
    TO = T // 2
    NCMP = (T - 32) // 16 + 1
    NCC = (NCMP + 127) // 128
    pair_off = []
    npair = 0
    for jj in range(NO):
        pair_off.append(npair)
        npair += min(NCC, cmp_nchunks(jj))

    nc = bass.Bass("TRN2", target_bir_lowering=False)
    P = Prog(nc)
    SK = "ExternalOutput" if debug else "Internal"

    def inp(name, shape, dt=F32):
        return P.dram(name, shape, dt, kind="ExternalInput")

    xb = inp("xb", [T, D]); xo = inp("xo", [TO, D]); memb = inp("mem", [256, D])
    w_in = inp("w_in", [D, NCOL]); gw2 = inp("gw2", [16, 256]); gb = inp("gb", [1, 256])
    gnorm = inp("gnorm", [1, 128])
    norms = inp("norms", [5, D])
    cw1 = inp("cw1", [2, 64, 32, 128]); cpos = inp("cpos", [2, 64, 32]); cw2 = inp("cw2", [2, 128, 64])
    w_out = inp("w_out", [D, D]); xa_w = inp("xa_w", [4, D, D])
    pwq = inp("pwq", [D, D]); skd = inp("skd", [8, 128, 256])
    downT = inp("downT", [D, 16384]); up = inp("up", [16384, D])
    c_ident = inp("c_ident", [128, 128]); c_ucs = inp("c_ucs", [128, 128]); c_urev = inp("c_urev", [128, 128])
    c_causal = inp("c_causal", [128, 128]); c_selu = inp("c_selu", [2, 128, 128]); c_pv = inp("c_pv", [128, 2])
    c_selb = inp("c_selb", [2, 128, 15, 512]); c_winb = inp("c_winb", [2, 128, 6, 512])
    c_cmpb = inp("c_cmpb", [2, npair, 128, 512])
    c_far = inp("c_far", [2, 128, 512])
    c_wimp = inp("c_wimp", [NCC, 128, 128]); c_m12 = inp("c_m12", [NO, 128, 2, 128])
    c_ex = inp("c_ex", [128, T], BF16)
    out = P.dram("out", [TO, D], F32, kind="ExternalOutput")

    d_nq = P.dram("d_nq", [T, 512], BF16, kind=SK)
    d_ng = P.dram("d_ng", [T, 24], F32, kind=SK)
    d_ogla = P.dram("d_ogla", [T, 512], BF16, kind=SK)
    d_kT = P.dram("d_kT", [4, 128, T], BF16, kind=SK)
    d_vs = P.dram("d_vs", [T, 128], BF16, kind=SK)
    d_vw = P.dram("d_vw", [T, 128], BF16, kind=SK)
    d_omix = P.dram("d_omix", [TO, D], BF16, kind=SK)
    d_h = P.dram("d_h", [TO, D], F32, kind=SK)
    d_hn3T = P.dram("d_hn3T", [128, 8, TO], BF16, kind=SK)
    d_route = P.dram("d_route", [TO, 2064], F32, kind=SK)
    d_downT = P.dram("d_downT", [D, 16384], BF16, kind="Internal")
    d_up = P.dram("d_up", [16384, D], BF16, kind="Internal")
    d_cmp = P.dram("d_cmp", [2, 64, 512], BF16, kind=SK)
    d_dbg = P.dram("d_dbg", [3, TO, 512], F32, kind=SK)
    d_imp = P.dram("d_imp", [2, TO, 128], F32, kind=SK)

    def mm(ot, o_ap, lt, l_ap, rt, r_ap, start=True, stop=True):
        P.op('pe', lambda e: e.matmul(o_ap, lhsT=l_ap, rhs=r_ap, start=start, stop=stop), [lt, rt], [ot])

    def tr(ot, o_ap, it, i_ap, idt):
        P.op('pe', lambda e: e.transpose(out=o_ap, in_=i_ap, identity=idt[:]), [it, idt], [ot])

    def act(ot, o_ap, it, i_ap, func, bias=None, scale=None, accum=None, rd=(), wr=()):
        kw = {}
        if bias is not None:
            kw['bias'] = bias
        if scale is not None:
            kw['scale'] = scale
        if accum is not None:
            kw['accum_out'] = accum
        P.op('act', lambda e: e.activation(out=o_ap, in_=i_ap, func=func, **kw), [it] + list(rd), [ot] + list(wr))

    def cp(eng, ot, o_ap, it, i_ap):
        if eng == 'act':
            P.op('act', lambda e: e.copy(out=o_ap, in_=i_ap), [it], [ot])
        else:
            P.op(eng, lambda e: e.tensor_copy(out=o_ap, in_=i_ap), [it], [ot])

    def tt(eng, ot, o_ap, at, a_ap, bt, b_ap, op):
        P.op(eng, lambda e: e.tensor_tensor(out=o_ap, in0=a_ap, in1=b_ap, op=op), [at, bt], [ot])

    def ts(eng, ot, o_ap, at, a_ap, s1, s2, op0, op1=None, rd=()):
        if op1 is None:
            P.op(eng, lambda e: e.tensor_scalar(out=o_ap, in0=a_ap, scalar1=s1, scalar2=None, op0=op0), [at] + list(rd), [ot])
        else:
            P.op(eng, lambda e: e.tensor_scalar(out=o_ap, in0=a_ap, scalar1=s1, scalar2=s2, op0=op0, op1=op1), [at] + list(rd), [ot])

    def stt(eng, ot, o_ap, at, a_ap, sc, bt, b_ap, op0, op1, rd=()):
        P.op(eng, lambda e: e.scalar_tensor_tensor(out=o_ap, in0=a_ap, scalar=sc, in1=b_ap, op0=op0, op1=op1),
             [at, bt] + list(rd), [ot])

    def memset(eng, t, ap, v):
        P.op(eng, lambda e: e.memset(ap, v), [], [t])

    def recip(ot, o_ap, it, i_ap):
        P.op('dve', lambda e: e.reciprocal(out=o_ap, in_=i_ap), [it], [ot])

    dmaq = ['sp', 'act', 'pool']
    dq = [0]

    dma_pol = {'load': ['sp'], 'store': ['pool']}

    def dma(ot, o_ap, it, i_ap, q=None, **kw):
        if q is None:
            qs = dma_pol['store'] if ot.dram else dma_pol['load']
            q = qs[dq[0] % len(qs)]
            dq[0] += 1
        return P.dma(q, ot, o_ap, it, i_ap, **kw)

    ident = P.sb([128, 128], BF16, "ident"); identf = P.sb([128, 128], F32, "identf")
    ones = P.sb([128, 128], BF16, "ones")
    pv = P.sb([128, 2], F32, "pv")
    nrm = P.sb([128, 5, D], F32, "nrm")
    dma(identf, identf[:], c_ident, c_ident[:])
    dma(pv, pv[:], c_pv, c_pv[:])
    for k in range(5):
        dma(nrm, nrm[:, k, :], norms, norms[k:k + 1, :].partition_broadcast(128))
    cp('dve', ident, ident[:], identf, identf[:])
    memset('pool', ones, ones[:], 1.0)
    eps_t = P.sb([128, 1], F32, "eps_t")
    memset('dve', eps_t, eps_t[:], 1e-6)
    kcmpT = [P.sb([128, 512], BF16, "kcmpT%d" % g) for g in range(2)]
    vcmp = [P.sb([128, 4, 65], BF16, "vcmp%d" % g) for g in range(2)]

    def rmsnorm(xt, x_ap, gain_k, hn, hn_ap, junk, ss, rstd, n=D, eps=1e-6):
        memset('dve', ss, ss[:], 0.0)
        act(junk, junk[:, 0:n], xt, x_ap, AF.Square, accum=ss[:], rd=[ss], wr=[ss])
        act(rstd, rstd[:], ss, ss[:], AF.Ln, scale=1.0 / n, bias=eps_t[:, 0:1], rd=[eps_t])
        act(rstd, rstd[:], rstd, rstd[:], AF.Exp, scale=-0.5)
        stt('dve', hn, hn_ap, xt, x_ap, rstd[:, 0:1], nrm, nrm[:, gain_k, 0:n], ALU.mult, ALU.mult, rd=[rstd])

    if upto >= 1:
        P.push()
        winb = P.sb([128, 8, NCOL], BF16, "winb")
        wst = [P.sb([128, NCOL], F32, "wst%d" % i) for i in range(2)]
        w_in_v = w_in[:].rearrange("(c p) n -> c p n", p=128)
        for c in range(8):
            dma(wst[c % 2], wst[c % 2][:], w_in, w_in_v[c])
            cp(['dve', 'pool'][c % 2], winb, winb[:, c, :], wst[c % 2], wst[c % 2][:])
        gw2t = P.sb([16, 256], F32, "gw2t"); gbt = P.sb([1, 256], F32, "gbt"); onesrow = P.sb([1, 128], F32, "onesrow")
        gnt = P.sb([128, 128], F32, "gnt")
        ucs = P.sb([128, 128], F32, "ucs"); urev = P.sb([128, 128], F32, "urev"); causal = P.sb([128, 128], F32, "causal")
        m16 = P.sb([128, 1], F32, "m16")
        dma(gw2t, gw2t[:], gw2, gw2[:]); dma(gbt, gbt[:], gb, gb[:])
        dma(gnt, gnt[:], gnorm, gnorm[0:1, :].partition_broadcast(128))
        dma(ucs, ucs[:], c_ucs, c_ucs[:]); dma(urev, urev[:], c_urev, c_urev[:]); dma(causal, causal[:], c_causal, c_causal[:])
        memset('dve', onesrow, onesrow[:], 1.0)
        memset('dve', m16, m16[:], -1.0 / 16)
        S = P.sb([128, 2, 128], F32, "S"); Sb = P.sb([128, 2, 128], BF16, "Sb")
        memset('dve', S, S[:], 0.0); memset('pool', Sb, Sb[:], 0.0)

        xt = [P.sb([128, D], F32, "xt%d" % i) for i in range(2)]
        junk = P.sb([128, D], BF16, "junk"); ss = P.sb([128, 1], F32, "ss"); rstd = P.sb([128, 1], F32, "rstd")
        hn = P.sb([128, D], BF16, "hn"); hnT = P.sb([128, 8, 128], BF16, "hnT")
        pt = P.ps([128, 8, 128], BF16, "pt")
        pz = [P.ps([128, 512], F32, "pz%d" % i) for i in range(3)]
        pg1 = P.ps([128, 512], F32, "pg1"); pg2 = P.ps([128, 512], F32, "pg2")
        po = P.ps([128, 512], F32, "po"); ptb = P.ps([128, 8, 128], BF16, "ptb")
        glr = P.sb([128, 16], F32, "glr"); glrT = P.sb([16, 128], F32, "glrT")
        e1 = P.sb([128, 256], F32, "e1"); L = P.sb([128, 256], F32, "L")
        eb = P.sb([128, 256], F32, "eb"); enb = P.sb([128, 256], F32, "enb"); ec = P.sb([128, 256], F32, "ec")
        ebl = P.sb([128, 2], F32, "ebl")
        qk = P.sb([128, 3, 256], BF16, "qk")
        qkT = P.sb([128, 4, 128], BF16, "qkT")
        vv = P.sb([128, 512], BF16, "vv"); sg = P.sb([128, 512], F32, "sg")
        AT = P.sb([128, 128], BF16, "AT")
        ssq = P.sb([128, 4], F32, "ssq"); rs4 = P.sb([128, 4], F32, "rs4"); tmpn = P.sb([128, 128], F32, "tmpn")
        junk2 = P.sb([128, 128], F32, "junk2")
        og = P.sb([128, 512], BF16, "og"); otmp4 = P.sb([128, 512], F32, "otmp4")
        nqs = P.sb([128, 512], BF16, "nqs"); ngs = P.sb([128, 24], F32, "ngs")
        kk = P.sb([128, 4, 128], BF16, "kk"); kkT = P.sb([128, 4, 128], BF16, "kkT")
        vsw = P.sb([128, 2, 128], BF16, "vsw")
        cast_jobs = []
        if upto >= 4:
            stg = [P.sb([128, 4096], F32, "stg%d" % i) for i in range(2)]
            stb = [P.sb([128, 4096], BF16, "stb%d" % i) for i in range(2)]
            for r in range(8):
                for c in range(4):
                    cast_jobs.append((downT, downT[r * 128:(r + 1) * 128, c * 4096:(c + 1) * 4096],
                                      d_downT, d_downT[r * 128:(r + 1) * 128, c * 4096:(c + 1) * 4096]))
            sv = up[:].rearrange("(a p f) d -> a p (f d)", p=128, f=4)
            dv = d_up[:].rearrange("(a p f) d -> a p (f d)", p=128, f=4)
            for a in range(32):
                cast_jobs.append((up, sv[a], d_up, dv[a]))
        cj = [0]

        def cast_job():
            k_ = cj[0]
            if k_ < len(cast_jobs):
                src_t, s_ap, dst_t, d_ap = cast_jobs[k_]
                P.dma('pool', stg[k_ % 2], stg[k_ % 2][:], src_t, s_ap)
            if 1 <= k_ <= len(cast_jobs):
                src_t, s_ap, dst_t, d_ap = cast_jobs[k_ - 1]
                a, b_ = stg[(k_ - 1) % 2], stb[(k_ - 1) % 2]
                for q_ in range(4):
                    cp('act', b_, b_[:, q_ * 1024:(q_ + 1) * 1024], a, a[:, q_ * 1024:(q_ + 1) * 1024])
                P.dma('pool', dst_t, d_ap, b_, b_[:])
            cj[0] += 1
        cA, cB, cC, cD, cE, cF = 0, 512, 1024, 1536, 2048, 2560
        hn2_ = [hn, P.sb([128, D], BF16, "hn_b")]; hnT2_ = [hnT, P.sb([128, 8, 128], BF16, "hnT_b")]
        ss2_ = [ss, P.sb([128, 1], F32, "ss_b")]; rstd2_ = [rstd, P.sb([128, 1], F32, "rstd_b")]

        def front(i):
            x_t = xt[i % 2]
            dma(x_t, x_t[:], xb, xb[i * 128:(i + 1) * 128, :])
            rmsnorm(x_t, x_t[:], 0, hn2_[i % 2], hn2_[i % 2][:], junk, ss2_[i % 2], rstd2_[i % 2])
            for c in range(8):
                tr(pt, pt[:, c, :], hn2_[i % 2], hn2_[i % 2][:, c * 128:(c + 1) * 128], ident)
            cp('act', hnT2_[i % 2], hnT2_[i % 2][:], pt, pt[:])
        front(0)
        for i in range(NT):
            hnT = hnT2_[i % 2]

            def zgroup(pz_t, c0, n):
                for c in range(8):
                    mm(pz_t, pz_t[:, 0:n], hnT, hnT[:, c, :], winb, winb[:, c, c0:c0 + n], start=(c == 0), stop=(c == 7))
            zgroup(pz[0], cF, 296)
            cp('act', kk, kk[:, 3, :], pz[0], pz[0][:, 0:128])
            cp('act', vsw, vsw[:, 1, :], pz[0], pz[0][:, 128:256])
            cp('act', glr, glr[:], pz[0], pz[0][:, 280:296])
            act(ngs, ngs[:], pz[0], pz[0][:, 256:280], AF.Exp, scale=-1.0)
            ts('dve', ngs, ngs[:], ngs, ngs[:], 1.0, None, ALU.add)
            recip(ngs, ngs[:], ngs, ngs[:])
            dma(d_ng, d_ng[i * 128:(i + 1) * 128, :], ngs, ngs[:])
            zgroup(pz[1], cE, 512)
            cp('act', kk, kk[:, 0:3, :], pz[1], pz[1][:, 0:384].rearrange("p (a b) -> p a b", a=3))
            cp('dve', vsw, vsw[:, 0, :], pz[1], pz[1][:, 384:512])
            dma(d_vs, d_vs[i * 128:(i + 1) * 128, :], vsw, vsw[:, 0, :])
            dma(d_vw, d_vw[i * 128:(i + 1) * 128, :], vsw, vsw[:, 1, :])
            for a in range(4):
                tr(ptb, ptb[:, a, :], kk, kk[:, a, :], ident)
            cp('act', kkT, kkT[:], ptb, ptb[:, 0:4, :])
            dma(d_kT, d_kT[:, :, i * 128:(i + 1) * 128].rearrange("a p t -> p a t"), kkT, kkT[:])
            zgroup(pz[2], cD, 512)
            P.op('act', lambda e, pzt=pz[2]: e.mul(out=nqs[:], in_=pzt[:], mul=0.125), [pz[2]], [nqs])
            dma(d_nq, d_nq[i * 128:(i + 1) * 128, :], nqs, nqs[:])
            tr(pg1, pg1[0:16, 0:128], glr, glr[:], identf)
            cp('dve', glrT, glrT[:], pg1, pg1[0:16, 0:128])
            mm(pg1, pg1[:, 256:512], glrT, glrT[:], gw2t, gw2t[:], start=True, stop=False)
            mm(pg1, pg1[:, 256:512], onesrow, onesrow[:], gbt, gbt[:], start=False, stop=True)
            zgroup(pz[0], cA, 512)
            zgroup(pz[1], cB, 512)
            zgroup(pz[2], cC, 512)
            act(e1, e1[:], pg1, pg1[:, 256:512], AF.Exp, scale=-1.0)
            act(L, L[:], e1, e1[:], AF.Ln, bias=1.0)
            cp('act', vv, vv[:], pz[1], pz[1][:])
            act(sg, sg[:], pz[2], pz[2][:], AF.Exp, scale=-1.0)
            act(sg, sg[:], sg, sg[:], AF.Ln, bias=1.0)
            act(sg, sg[:], sg, sg[:], AF.Exp, scale=-1.0)
            tt('dve', sg, sg[:], sg, sg[:], pz[2], pz[2][:], ALU.mult)
            tt('pool', sg, sg[:].rearrange("p (h d) -> p h d", h=4), sg, sg[:].rearrange("p (h d) -> p h d", h=4),
               gnt, gnt[:].unsqueeze(1).to_broadcast([128, 4, 128]), ALU.mult)
            mm(pg2, pg2[:, 0:256], ucs, ucs[:], L, L[:])
            mm(pg2, pg2[:, 256:512], urev, urev[:], L, L[:])
            for hp in range(2):
                mm(pg1, pg1[:, hp:hp + 1], L, L[:, hp * 128:(hp + 1) * 128], m16, m16[:])
            act(eb, eb[:], pg2, pg2[:, 0:256], AF.Exp)
            act(enb, enb[:], pg2, pg2[:, 0:256], AF.Exp, scale=-1.0)
            act(ec, ec[:], pg2, pg2[:, 256:512], AF.Exp)
            act(ebl, ebl[:], pg1, pg1[:, 0:2], AF.Exp)
            stt('dve', qk, qk[:, 0, :], pz[0], pz[0][:, 0:256], 0.125, eb, eb[:], ALU.mult, ALU.mult)
            tt('dve', qk, qk[:, 1, :], pz[0], pz[0][:, 256:512], enb, enb[:], ALU.mult)
            tt('dve', qk, qk[:, 2, :], pz[0], pz[0][:, 256:512], ec, ec[:], ALU.mult)
            for a in range(4):
                tr(ptb, ptb[:, 4 + a, :], qk, qk[:, a // 2, (a % 2) * 128:(a % 2 + 1) * 128], ident)
            cp('act', qkT, qkT[:], ptb, ptb[:, 4:8, :])
            if i + 1 < NT:
                front(i + 1)
            memset('dve', ssq, ssq[:], 0.0)
            for h in range(4):
                hp, hh = h // 2, h % 2
                pr = slice(hh * 64, hh * 64 + 64)
                mm(pg2, pg2[:, 0:128], qkT, qkT[pr, 2 + hp, :], qkT, qkT[pr, hp, :])
                tt('dve', AT, AT[:], pg2, pg2[:, 0:128], causal, causal[:], ALU.mult)
                o_ap = po[:, h * 128:(h + 1) * 128]
                mm(po, o_ap, AT, AT[:], vv, vv[:, h * 128:(h + 1) * 128], start=True, stop=False)
                mm(po, o_ap, qkT, qkT[pr, hp, :], Sb, Sb[pr, hp, :], start=False, stop=True)
                mm(pg2, pg2[:, 128:256], qk, qk[:, 2, hp * 128:(hp + 1) * 128], vv, vv[:, h * 128:(h + 1) * 128])
                stt('dve', Sb, Sb[pr, hp, :], S, S[pr, hp, :], ebl[pr, hp:hp + 1], pg2, pg2[pr, 128:256], ALU.mult, ALU.add, rd=[ebl])
                stt('dve', S, S[pr, hp, :], S, S[pr, hp, :], ebl[pr, hp:hp + 1], pg2, pg2[pr, 128:256], ALU.mult, ALU.add, rd=[ebl])
                act(junk2, junk2[:], po, o_ap, AF.Square, accum=ssq[:, h:h + 1], rd=[ssq], wr=[ssq])
            act(rs4, rs4[:], ssq, ssq[:], AF.Ln, scale=1.0 / 128, bias=eps_t[:, 0:1], rd=[eps_t])
            act(rs4, rs4[:], rs4, rs4[:], AF.Exp, scale=-0.5)
            tt('dve', otmp4, otmp4[:], po, po[:], sg, sg[:], ALU.mult)
            tt('dve', og, og[:].rearrange("p (h d) -> p h d", h=4), otmp4, otmp4[:].rearrange("p (h d) -> p h d", h=4),
               rs4, rs4[:].unsqueeze(2).to_broadcast([128, 4, 128]), ALU.mult)
            dma(d_ogla, d_ogla[i * 128:(i + 1) * 128, :], og, og[:])
            for _ in range((len(cast_jobs) + NT - 1) // NT):
                cast_job()
        while cast_jobs and cj[0] <= len(cast_jobs):
            cast_job()
        P.pop()

        P.push()
        w1f = P.sb([64, 32, 128], F32, "w1f"); w1b = P.sb([64, 32, 128], BF16, "w1b")
        posf = P.sb([64, 32], F32, "posf"); posb = P.sb([64, 32], BF16, "posb")
        w2f = P.sb([128, 64], F32, "w2f"); w2b = P.sb([128, 64], BF16, "w2b")
        kTg = P.sb([64, T], BF16, "kTg")
        ph = P.ps([128, 512], F32, "ph"); pb = P.ps([128, 512], F32, "pb"); pc = P.ps([128, 512], F32, "pc")
        bias_h = P.sb([128, 1], F32, "bias_h")
        H = P.sb([128, 512], BF16, "H")
        for g in range(2):
            memset('dve', kcmpT[g], kcmpT[g][:], 0.0)
            memset('dve', vcmp[g], vcmp[g][:], 0.0)
            memset('dve', vcmp[g], vcmp[g][:, :, 64:65], 1.0)
        for kv in range(2):
            dma(w1f, w1f[:], cw1, cw1[kv]); dma(posf, posf[:], cpos, cpos[kv]); dma(w2f, w2f[:], cw2, cw2[kv])
            cp('dve', w1b, w1b[:], w1f, w1f[:]); cp('dve', posb, posb[:], posf, posf[:]); cp('dve', w2b, w2b[:], w2f, w2f[:])
            for l in range(32):
                mm(pb, pb[:, 0:1], w1b, w1b[:, l, :], posb, posb[:, l:l + 1], start=(l == 0), stop=(l == 31))
            cp('dve', bias_h, bias_h[:], pb, pb[:, 0:1])
            for g in range(2):
                dma(kTg, kTg[:], d_kT, d_kT[kv, g * 64:(g + 1) * 64, :])
                for l in range(32):
                    mm(ph, ph[:, 0:NCMP], w1b, w1b[:, l, :], kTg, kTg[:, l:l + 16 * (NCMP - 1) + 1:16],
                       start=(l == 0), stop=(l == 31))
                memset('dve', H, H[:], 0.0)
                act(H, H[:, 0:NCMP], ph, ph[:, 0:NCMP], AF.Gelu_apprx_tanh, bias=bias_h[:, 0:1], rd=[bias_h])
                if kv == 0:
                    mm(pc, pc[0:64, 0:NCMP], w2b, w2b[:], H, H[:, 0:NCMP])
                    cp('act', kcmpT[g], kcmpT[g][0:64, 0:NCMP], pc, pc[0:64, 0:NCMP])
                    if debug:
                        dma(d_cmp, d_cmp[g], kcmpT[g], kcmpT[g][0:64, :])
                else:
                    for c in range(NCC):
                        mm(pc, pc[:, c * 64:(c + 1) * 64], H, H[:, c * 128:(c + 1) * 128], w2b, w2b[:])
                    cp('act', vcmp[g], vcmp[g][:, 0:NCC, 0:64], pc, pc[:, 0:NCC * 64].rearrange("p (c d) -> p c d", d=64))
        P.pop()

    if upto >= 2:
        P.push()
        dma_pol['load'] = ['sp']; dma_pol['store'] = ['sp']
        selu = P.sb([128, 2, 128], BF16, "selu"); seluf = P.sb([128, 2, 128], F32, "seluf")
        dma(seluf, seluf[:], c_selu, c_selu[:].rearrange("a p t -> p a t"))
        cp('dve', selu, selu[:], seluf, seluf[:])
        exm = P.sb([128, T], BF16, "exm")
        dma(exm, exm[:], c_ex, c_ex[:])
        wimpf = P.sb([128, NCC, 128], F32, "wimpf"); wimp = P.sb([128, NCC, 128], BF16, "wimp")
        dma(wimpf, wimpf[:], c_wimp, c_wimp[:].rearrange("c p s -> p c s"))
        cp('dve', wimp, wimp[:], wimpf, wimpf[:])
        ksT = P.sb([128, T], BF16, "ksT"); kwT = P.sb([128, T], BF16, "kwT")
        memset('dve', ksT, ksT[64:128, :], 0.0)
        memset('pool', kwT, kwT[64:128, :], 0.0)
        memset('dve', ksT, ksT[64:65, :], 1.0)
        farf = P.sb([128, 512], F32, "farf")
        vs = P.sb([128, NT, 65], BF16, "vs"); vw = P.sb([128, NT, 65], BF16, "vw")
        selb = P.sb([128, 15, 512], BF16, "selb"); winb2 = P.sb([128, 6, 512], BF16, "winb2")
        bst = [P.sb([128, 512], F32, "bst%d" % i) for i in range(2)]
        cbt = [P.sb([128, 512], BF16, "cbt%d" % i) for i in range(2)]
        qrows_ = [P.sb([128, 2, 256], BF16, "qrows%d" % i) for i in range(2)]; grows_ = [P.sb([128, 2, 24], F32, "grows%d" % i) for i in range(2)]
        gown_ = [P.sb([128, 12], F32, "gown%d" % i) for i in range(2)]; gtmp = P.sb([128, 12], F32, "gtmp")
        qT_ = [P.sb([128, 4, 128], BF16, "qT%d" % i) for i in range(2)]
        for i_ in range(2):
            memset('dve', qT_[i_], qT_[i_][64:128, :, :], 0.0)
        Ec = [P.sb([128, 4, 128], BF16, "Ec%d" % i) for i in range(4)]
        Eb = [P.sb([128, 4, 128], BF16, "Eb%d" % i) for i in range(3)]
        m12_ = [P.sb([128, 2, 128], F32, "m12_%d" % i) for i in range(2)]
        imp = P.sb([128, 128], F32, "imp"); imp2 = P.sb([128, 128], F32, "imp2"); m8 = P.sb([128, 16], F32, "m8")
        mk = P.sb([128, 128], BF16, "mk"); maskT4 = P.sb([128, 4, 128], BF16, "maskT4")
        rden = P.sb([128, 4], F32, "rden"); coef = P.sb([128, 4], F32, "coef")
        oacc = P.sb([128, 4, 64], F32, "oacc"); otmp = P.sb([128, 4, 64], F32, "otmp"); onsa = P.sb([128, 256], BF16, "onsa")
        pq = P.ps([64, 4, 128], F32, "pq")
        psc = [P.ps([128, 4, 128], F32, "psc%d" % i) for i in range(2)]
        pn = P.ps([128, 4, 128], F32, "pn")
        pnT = P.ps([65, 4, 128], F32, "pnT")
        pnT2 = P.ps([65, 4, 128], F32, "pnT2")
        numTs = P.sb([65, 4, 128], F32, "numTs")
        pimp = P.ps([128, 4, 128], F32, "pimp")
        pmt = P.ps([128, 128], BF16, "pmt")
        for g in range(2):
            dma(ksT, ksT[0:64, :], d_kT, d_kT[2, g * 64:(g + 1) * 64, :])
            dma(farf, farf[:], c_far, c_far[g])
            for i_ in range(2):
                cp('dve', qT_[i_], qT_[i_][64:65, :, :], farf, farf[64:65, :].rearrange("p (h q) -> p h q", h=4))
            dma(kwT, kwT[0:64, :], d_kT, d_kT[3, g * 64:(g + 1) * 64, :])
            for n0 in range(0, NT, 16):
                n1 = min(NT, n0 + 16)
                dma(vs, vs[:, n0:n1, 0:64], d_vs, d_vs[n0 * 128:n1 * 128, g * 64:(g + 1) * 64].rearrange("(n p) c -> p n c", p=128))
                dma(vw, vw[:, n0:n1, 0:64], d_vw, d_vw[n0 * 128:n1 * 128, g * 64:(g + 1) * 64].rearrange("(n p) c -> p n c", p=128))
            memset('dve', vs, vs[:, :, 64:65], 1.0)
            memset('dve', vw, vw[:, :, 64:65], 1.0)
            k = 0
            for m in range(15):
                dma(bst[k % 2], bst[k % 2][:], c_selb, c_selb[g, :, m, :])
                tt('dve', selb, selb[:, m, :], bst[k % 2], bst[k % 2][:], farf, farf[:], ALU.subtract); k += 1
            for m in range(6):
                dma(bst[k % 2], bst[k % 2][:], c_winb, c_winb[g, :, m, :])
                cp('dve', winb2, winb2[:, m, :], bst[k % 2], bst[k % 2][:]); k += 1
            ne = 0
            ne_ = [0]
            for jj in range(NO):
                qrows, grows, gown, qT, m12 = qrows_[jj % 2], grows_[jj % 2], gown_[jj % 2], qT_[jj % 2], m12_[jj % 2]

                def load_q(j2):
                    dma(qrows_[j2 % 2], qrows_[j2 % 2][:], d_nq, d_nq[j2 * 256:(j2 + 1) * 256, g * 256:(g + 1) * 256].rearrange("(u p) c -> p u c", p=128))
                    dma(grows_[j2 % 2], grows_[j2 % 2][:], d_ng, d_ng[j2 * 256:(j2 + 1) * 256, :].rearrange("(u p) c -> p u c", p=128))
                    dma(m12_[j2 % 2], m12_[j2 % 2][:], c_m12, c_m12[j2])
                if jj == 0:
                    load_q(0)
                if jj + 1 < NO:
                    load_q(jj + 1)
                for h in range(4):
                    for u in range(2):
                        mm(pq, pq[:, h, :], qrows, qrows[:, u, h * 64:(h + 1) * 64], selu, selu[:, u, :], start=(u == 0), stop=(u == 1))
                cp('act', qT, qT[0:64], pq, pq[:])
                ts('dve', gtmp, gtmp[:], grows, grows[:, 0, g * 12:(g + 1) * 12], pv[:, 0:1], None, ALU.mult, rd=[pv])
                stt('dve', gown, gown[:], grows, grows[:, 1, g * 12:(g + 1) * 12], pv[:, 1:2], gtmp, gtmp[:], ALU.mult, ALU.add, rd=[pv])
                gv3 = gown[:].rearrange("p (h k) -> p h k", k=3)
                qT_all = qT[:].rearrange("p h q -> p (h q)")
                qT_aug = qT_all

                def finish_branch(pn, br, first):
                    ts('dve', rden, rden[:], pn, pn[:, :, 64], 1e-30, None, ALU.max)
                    recip(rden, rden[:], rden, rden[:])
                    tt('dve', coef, coef[:], rden, rden[:], gown, gv3[:, :, br], ALU.mult)
                    dst = oacc if first else otmp
                    tt('dve', dst, dst[:], pn, pn[:, :, 0:64], coef, coef[:].unsqueeze(2).to_broadcast([128, 4, 64]), ALU.mult)
                    if not first:
                        tt('dve', oacc, oacc[:], oacc, oacc[:], otmp, otmp[:], ALU.add)
                    if debug:
                        dma(d_dbg, d_dbg[br, jj * 128:(jj + 1) * 128, g * 256:(g + 1) * 256], oacc, oacc[:].rearrange("p h d -> p (h d)"))

                def back_T(src):
                    cp('act', numTs, numTs[:], src, src[:])
                    for h in range(4):
                        P.op('pe', lambda e, h=h: e.transpose(out=pn[:, h, 0:65], in_=numTs[:, h, :], identity=identf[0:65, 0:65]), [numTs, identf], [pn])

                ncc = min(NCC, cmp_nchunks(jj))
                for c in range(ncc):
                    pi = pair_off[jj] + c
                    dma(bst[k % 2], bst[k % 2][:], c_cmpb, c_cmpb[g, pi])
                    cp('dve', cbt[k % 2], cbt[k % 2][:], bst[k % 2], bst[k % 2][:])
                    sc = psc[ne % 2]; ne += 1
                    sc_all = sc[:].rearrange("p h q -> p (h q)")
                    mm(sc, sc_all, kcmpT[g], kcmpT[g][:, c * 128:(c + 1) * 128], qT, qT_all, start=True, stop=False)
                    mm(sc, sc_all, ident, ident[:], cbt[k % 2], cbt[k % 2][:], start=False, stop=True)
                    k += 1
                    act(Ec[c], Ec[c][:], sc, sc[:], AF.Exp)
                for h in range(4):
                    for c in range(ncc):
                        mm(pn, pn[:, h, 0:65], Ec[c], Ec[c][:, h, :], vcmp[g], vcmp[g][:, c, :], start=(c == 0), stop=(c == ncc - 1))
                    for c in range(ncc):
                        mm(pimp, pimp[:, h, :], Ec[c], Ec[c][:, h, :], wimp, wimp[:, c, :], start=(c == 0), stop=(c == ncc - 1))
                nk = 2 * jj + 2
                k0 = max(0, 2 * jj - 4)
                bufs = {}

                def score(kind, kc, n):
                    sc = psc[ne_[0] % 2]; E = Eb[ne_[0] % 3]; ne_[0] += 1
                    bufs[n] = E
                    sc_all = sc[:].rearrange("p h q -> p (h q)")
                    if kind == 's':
                        m = 2 * jj + 1 - kc
                        mm(sc, sc_all, ksT, ksT[:, kc * 128:(kc + 1) * 128], qT, qT_aug, start=True, stop=False)
                        if m < 14:
                            mm(sc, sc_all, ident, ident[:], selb, selb[:, m, :], start=False, stop=False)
                        mm(sc, sc_all, exm, exm[:, kc * 128:(kc + 1) * 128], maskT4, mT_all, start=False, stop=True)
                    else:
                        m = 2 * jj + 1 - kc
                        mm(sc, sc_all, kwT, kwT[:, kc * 128:(kc + 1) * 128], qT, qT_all, start=True, stop=False)
                        mm(sc, sc_all, ident, ident[:], winb2, winb2[:, m, :], start=False, stop=True)
                    act(E, E[:], sc, sc[:], AF.Exp)

                def pvs(kind, kc, n):
                    E = bufs.pop(n)
                    if kind == 's':
                        mm(pnT, pnT[:].rearrange("p h q -> p (h q)"), vs, vs[:, kc, :], E, E[:].rearrange("p h q -> p (h q)"), start=(kc == 0), stop=(kc == nk - 1))
                    else:
                        mm(pnT2, pnT2[:].rearrange("p h q -> p (h q)"), vw, vw[:, kc, :], E, E[:].rearrange("p h q -> p (h q)"), start=(kc == k0), stop=(kc == nk - 1))

                def run_items(items):
                    NI = len(items)
                    for n in range(NI + 1):
                        if n < NI:
                            score(items[n][0], items[n][1], n)
                        if n >= 1:
                            pvs(items[n - 1][0], items[n - 1][1], n - 1)
                mT_all = maskT4[:].rearrange("p h q -> p (h q)")
                run_items([('w', kc) for kc in range(k0, nk)])
                finish_branch(pn, 0, True)
                for h in range(4):
                    if h == 0:
                        ts('dve', imp, imp[:], pimp, pimp[:, 0, :], rden[:, 0:1], None, ALU.mult, rd=[rden])
                    else:
                        stt('dve', imp, imp[:], pimp, pimp[:, h, :], rden[:, h:h + 1], imp, imp[:], ALU.mult, ALU.add, rd=[rden])
                tt('dve', imp, imp[:], imp, imp[:], m12, m12[:, 0, :], ALU.mult)
                tt('dve', imp, imp[:], imp, imp[:], m12, m12[:, 1, :], ALU.add)
                if debug:
                    dma(d_imp, d_imp[g, jj * 128:(jj + 1) * 128, :], imp, imp[:])
                P.op('dve', lambda e: e.max(out=m8[:, 0:8], in_=imp[:]), [imp], [m8])
                P.op('dve', lambda e: e.match_replace(out=imp2[:], in_to_replace=m8[:, 0:8], in_values=imp[:], imm_value=-1e30), [imp, m8], [imp2])
                P.op('dve', lambda e: e.max(out=m8[:, 8:16], in_=imp2[:]), [imp2], [m8])
                ts('dve', mk, mk[:], imp, imp[:], m8[:, 15:16], 1.0, ALU.is_ge, ALU.subtract, rd=[m8])
                tr(pmt, pmt[:], mk, mk[:], ident)
                P.op('act', lambda e: e.mul(out=maskT4[:], in_=pmt[:].unsqueeze(1).to_broadcast([128, 4, 128]), mul=30000.0), [pmt], [maskT4])
                run_items([('s', kc) for kc in range(nk)])
                back_T(pnT)
                finish_branch(pn, 1, False)
                back_T(pnT2)
                finish_branch(pn, 2, False)
                cp('act', onsa, onsa[:], oacc, oacc[:].rearrange("p h d -> p (h d)"))
                dma(d_omix, d_omix[jj * 128:(jj + 1) * 128, 512 + g * 256:512 + (g + 1) * 256], onsa, onsa[:])
        P.pop()

    if upto >= 3:
        P.push()
        dma_pol['load'] = ['sp']; dma_pol['store'] = ['pool']
        woutb = P.sb([128, 8, D], BF16, "woutb"); wqb = P.sb([128, 8, D], BF16, "wqb"); wob = P.sb([128, 8, D], BF16, "wob")
        pwqb = P.sb([128, 8, D], BF16, "pwqb")
        skb = P.sb([128, 8, 256], BF16, "skb")
        kTm = P.sb([128, 8, 256], BF16, "kTm")
        vm = P.sb([128, 2, 4, 257], BF16, "vm")
        xt = [P.sb([128, D], F32, "x3_%d" % i) for i in range(2)]
        junk = P.sb([128, D], BF16, "junk3"); ss = P.sb([128, 1], F32, "ss3"); rstd = P.sb([128, 1], F32, "rstd3")
        hn = P.sb([128, D], BF16, "hn3"); hnT = P.sb([128, 8, 128], BF16, "hnT3")
        pt = P.ps([128, 8, 128], BF16, "pt3")
        pa = [P.ps([128, 512], F32, "pa%d" % i) for i in range(4)]
        pxa = P.ps([128, 2, 512], F32, "pxa")
        P.push()
        wst = [P.sb([128, D], F32, "wst3_%d" % i) for i in range(2)]
        wtmp = P.sb([128, 8, D], BF16, "wtmp"); skf = P.sb([128, 8, 256], F32, "skf")
        memT = P.sb([128, 8, 256], BF16, "memT")
        k = [0]

        def load_w(dst, src_t, src_ap3):
            for c in range(8):
                a = wst[k[0] % 2]
                dma(a, a[:], src_t, src_ap3[c])
                cp(['dve', 'pool'][k[0] % 2], dst, dst[:, c, :], a, a[:]); k[0] += 1
        load_w(woutb, w_out, w_out[:].rearrange("(c p) n -> c p n", p=128))
        load_w(wqb, xa_w, xa_w[0].rearrange("(c p) n -> c p n", p=128))
        load_w(wob, xa_w, xa_w[3].rearrange("(c p) n -> c p n", p=128))
        load_w(pwqb, pwq, pwq[:].rearrange("(c p) n -> c p n", p=128))
        dma(skf, skf[:], skd, skd[:].rearrange("c p n -> p c n"))
        cp('dve', skb, skb[:], skf, skf[:])
        memset('dve', vm, vm[:, :, :, 256:257], 1.0)
        for mc in range(2):
            x_t = xt[mc % 2]
            dma(x_t, x_t[:], memb, memb[mc * 128:(mc + 1) * 128, :])
            rmsnorm(x_t, x_t[:], 2, hn, hn[:], junk, ss, rstd)
            for c in range(8):
                tr(pt, pt[:, c, :], hn, hn[:, c * 128:(c + 1) * 128], ident)
            cp('act', memT, memT[:, :, mc * 128:(mc + 1) * 128], pt, pt[:])
        load_w(wtmp, xa_w, xa_w[1].rearrange("(c p) n -> c p n", p=128))
        for oc in range(8):
            for c in range(8):
                mm(pa[0], pa[0][:, 0:256], wtmp, wtmp[:, c, oc * 128:(oc + 1) * 128], memT, memT[:, c, :], start=(c == 0), stop=(c == 7))
            cp('act', kTm, kTm[:, oc, :], pa[0], pa[0][:, 0:256])
        load_w(wtmp, xa_w, xa_w[2].rearrange("(c p) n -> c p n", p=128))
        for mc in range(2):
            for half in range(2):
                for c in range(8):
                    mm(pa[half], pa[half][:], memT, memT[:, c, mc * 128:(mc + 1) * 128], wtmp, wtmp[:, c, half * 512:(half + 1) * 512], start=(c == 0), stop=(c == 7))
                cp('act', vm, vm[:, mc, half * 2:half * 2 + 2, 0:256], pa[half], pa[half][:].rearrange("p (h d) -> p h d", d=256))
        P.pop()
        og2 = P.sb([128, 2, 512], BF16, "og2"); omx = P.sb([128, D], BF16, "omx"); otm = P.sb([128, 512], F32, "otm")
        h1 = P.sb([128, D], F32, "h1"); qTx = P.sb([128, 8, 128], BF16, "qTx")
        Ex = P.sb([128, 2, 4, 128], BF16, "Ex"); rdx = P.sb([128, 4], F32, "rdx")
        oxa = P.sb([128, D], BF16, "oxa")
        hn3T = P.sb([128, 8, 128], BF16, "hn3Ts"); qpT = P.sb([128, 8, 128], BF16, "qpT")
        sc_ = [P.sb([128, 16, 128], F32, "scr%d" % i) for i in range(2)]; ab_ = [P.sb([128, 16, 128], F32, "ab%d" % i) for i in range(2)]
        negm = P.sb([128, 16], F32, "negm"); t16 = P.sb([128, 16, 16], F32, "t16"); scr2_ = [P.sb([128, 128], F32, "scr2_%d" % i) for i in range(8)]
        candall = P.sb([128, 8, 256], F32, "candall"); cand2_ = [P.sb([128, 256], F32, "cand2_%d" % i) for i in range(4)]; c16 = P.sb([128, 8, 16], F32, "c16")
        route = P.sb([128, 16], F32, "route"); zs = P.sb([128, 8], F32, "zs")
        memset('dve', route, route[:], 0.0)
        def Xgen(jj):
            sc = sc_[jj % 2]
            x_t = xt[jj % 2]
            dma(x_t, x_t[:], xo, xo[jj * 128:(jj + 1) * 128, :])
            dma(og2, og2[:], d_ogla, d_ogla[jj * 256:(jj + 1) * 256, :].rearrange("(u p) c -> p u c", p=128))
            dma(omx, omx[:, 512:1024], d_omix, d_omix[jj * 128:(jj + 1) * 128, 512:1024])
            ts('dve', otm, otm[:], og2, og2[:, 0, :], pv[:, 0:1], None, ALU.mult, rd=[pv])
            stt('dve', omx, omx[:, 0:512], og2, og2[:, 1, :], pv[:, 1:2], otm, otm[:], ALU.mult, ALU.add, rd=[pv])
            for c in range(8):
                tr(pt, pt[:, c, :], omx, omx[:, c * 128:(c + 1) * 128], ident)
            cp('act', hnT, hnT[:], pt, pt[:])
            for half in range(2):
                for c in range(8):
                    mm(pa[half], pa[half][:], hnT, hnT[:, c, :], woutb, woutb[:, c, half * 512:(half + 1) * 512], start=(c == 0), stop=(c == 7))
                tt('dve', h1, h1[:, half * 512:(half + 1) * 512], pa[half], pa[half][:], x_t, x_t[:, half * 512:(half + 1) * 512], ALU.add)
            yield
            rmsnorm(h1, h1[:], 1, hn, hn[:], junk, ss, rstd)
            for c in range(8):
                tr(pt, pt[:, c, :], hn, hn[:, c * 128:(c + 1) * 128], ident)
            cp('act', hnT, hnT[:], pt, pt[:])
            for oc in range(8):
                pq_ = pa[2 + oc % 2]
                for c in range(8):
                    mm(pq_, pq_[:, 0:128], wqb, wqb[:, c, oc * 128:(oc + 1) * 128], hnT, hnT[:, c, :], start=(c == 0), stop=(c == 7))
                cp(['act', 'dve'][oc % 2], qTx, qTx[:, oc, :], pq_, pq_[:, 0:128])
                yield
            yield
            for mc in range(2):
                for h in range(4):
                    for dc in range(2):
                        mm(pxa, pxa[:, mc, h * 128:(h + 1) * 128], kTm, kTm[:, h * 2 + dc, mc * 128:(mc + 1) * 128], qTx, qTx[:, h * 2 + dc, :], start=(dc == 0), stop=(dc == 1))
            act(Ex, Ex[:].rearrange("p a h q -> p a (h q)"), pxa, pxa[:], AF.Exp, scale=1.0 / 16)
            for h in range(4):
                o_ap = pxa[:, h // 2, (h % 2) * 256:(h % 2) * 256 + 256]
                for mc in range(2):
                    mm(pa[h % 2], pa[h % 2][:, 0:257], Ex, Ex[:, mc, h, :], vm, vm[:, mc, h, :], start=(mc == 0), stop=(mc == 1))
                ts('dve', rdx, rdx[:, h:h + 1], pa[h % 2], pa[h % 2][:, 256:257], 1e-30, None, ALU.max)
                recip(rdx, rdx[:, h:h + 1], rdx, rdx[:, h:h + 1])
                ts('dve', oxa, oxa[:, h * 256:(h + 1) * 256], pa[h % 2], pa[h % 2][:, 0:256], rdx[:, h:h + 1], None, ALU.mult, rd=[rdx])
            yield
            for c in range(8):
                tr(pt, pt[:, c, :], oxa, oxa[:, c * 128:(c + 1) * 128], ident)
            cp('act', hnT, hnT[:], pt, pt[:])
            for half in range(2):
                for c in range(8):
                    mm(pa[half], pa[half][:], hnT, hnT[:, c, :], wob, wob[:, c, half * 512:(half + 1) * 512], start=(c == 0), stop=(c == 7))
                tt('dve', h1, h1[:, half * 512:(half + 1) * 512], pa[half], pa[half][:], h1, h1[:, half * 512:(half + 1) * 512], ALU.add)
            dma(d_h, d_h[jj * 128:(jj + 1) * 128, :], h1, h1[:])
            yield
            rmsnorm(h1, h1[:], 3, hn, hn[:], junk, ss, rstd)
            for c in range(8):
                tr(pt, pt[:, c, :], hn, hn[:, c * 128:(c + 1) * 128], ident)
            cp('act', hn3T, hn3T[:], pt, pt[:])
            dma(d_hn3T, d_hn3T[:, :, jj * 128:(jj + 1) * 128], hn3T, hn3T[:])
            for oc in range(8):
                pq_ = pa[2 + oc % 2]
                for c in range(8):
                    mm(pq_, pq_[:, 0:128], pwqb, pwqb[:, c, oc * 128:(oc + 1) * 128], hn3T, hn3T[:, c, :], start=(c == 0), stop=(c == 7))
                cp(['act', 'dve'][oc % 2], qpT, qpT[:, oc, :], pq_, pq_[:, 0:128])
                yield
            yield
            for oc in range(8):
                pq_ = pa[oc % 2]
                mm(pq_, pq_[:, 0:256], qpT, qpT[:, oc, :], skb, skb[:, oc, :])
                cp(['act', 'dve'][oc % 2], sc, sc[:, 2 * oc:2 * oc + 2, :], pq_, pq_[:, 0:256].rearrange("p (a k) -> p a k", a=2))
            yield

        def Rgen(jj):
            sc = sc_[jj % 2]; ab = ab_[jj % 2]
            P.op('dve', lambda e: e.tensor_reduce(out=negm[:], in_=sc[:], axis=AX.X, op=ALU.max), [sc], [negm])
            tt('dve', sc, sc[:], sc, sc[:], negm, negm[:].unsqueeze(2).to_broadcast([128, 16, 128]), ALU.subtract)
            act(ab, ab[:].rearrange("p r k -> p (r k)"), sc, sc[:].rearrange("p r k -> p (r k)"), AF.Exp)
            yield
            for r0 in range(0, 16, 8):
                for r in range(r0, r0 + 8):
                    P.op('dve', lambda e, r=r: e.max(out=t16[:, r, 0:8], in_=ab[:, r, :]), [ab], [t16])
                for r in range(r0, r0 + 8):
                    P.op('dve', lambda e, r=r: e.match_replace(out=scr2_[r % 8][:], in_to_replace=t16[:, r, 0:8], in_values=ab[:, r, :], imm_value=-1.0), [ab, t16], [scr2_[r % 8]])
                for r in range(r0, r0 + 8):
                    P.op('dve', lambda e, r=r: e.max(out=t16[:, r, 8:16], in_=scr2_[r % 8][:]), [scr2_[r % 8]], [t16])
                yield
            t16v = t16[:].rearrange("p (h a) k -> p h a k", a=2)
            abv = ab[:].rearrange("p (h a) k -> p h a k", a=2)

            def cand_top16():
                yield
                tt('dve', candall, candall[:].rearrange("p h (a b) -> p h a b", a=16),
                   t16, t16v[:, :, 0, :].unsqueeze(3).to_broadcast([128, 8, 16, 16]),
                   t16, t16v[:, :, 1, :].unsqueeze(2).to_broadcast([128, 8, 16, 16]), ALU.mult)
                for h0 in range(0, 8, 4):
                    for h in range(h0, h0 + 4):
                        P.op('dve', lambda e, h=h: e.max(out=c16[:, h, 0:8], in_=candall[:, h, :]), [candall], [c16])
                    for h in range(h0, h0 + 4):
                        P.op('dve', lambda e, h=h: e.match_replace(out=cand2_[h % 4][:], in_to_replace=c16[:, h, 0:8], in_values=candall[:, h, :], imm_value=-1.0), [candall, c16], [cand2_[h % 4]])
                    for h in range(h0, h0 + 4):
                        P.op('dve', lambda e, h=h: e.max(out=c16[:, h, 8:16], in_=cand2_[h % 4][:]), [cand2_[h % 4]], [c16])
                    yield
            yield from cand_top16()
            P.op('dve', lambda e: e.tensor_reduce(out=zs[:], in_=c16[:], axis=AX.X, op=ALU.add), [c16], [zs])
            recip(zs, zs[:], zs, zs[:])
            tt('dve', ab, abv[:, :, 1, :], ab, abv[:, :, 1, :], zs, zs[:].unsqueeze(2).to_broadcast([128, 8, 128]), ALU.mult)
            stt('dve', route, route[:, 0:8], c16, c16[:, :, 15], 1.0 - 1e-6, zs, zs[:], ALU.mult, ALU.mult)
            dma(d_route, d_route[jj * 128:(jj + 1) * 128, 0:2048], ab, ab[:].rearrange("p r k -> p (r k)"))
            dma(d_route, d_route[jj * 128:(jj + 1) * 128, 2048:2064], route, route[:])
            yield

        def drain(g):
            for _ in g:
                pass
        drain(Xgen(0))
        for jj in range(NO):
            gr = Rgen(jj)
            gx = Xgen(jj + 1) if jj + 1 < NO else iter(())
            ra = xa_ = True
            while ra or xa_:
                if xa_:
                    xa_ = next(gx, 'END') != 'END'
                if ra:
                    ra = next(gr, 'END') != 'END'
        P.pop()

    if upto >= 4:
        P.push()
        dma_pol['load'] = ['sp', 'pool']; dma_pol['store'] = ['sp']
        TG = 2
        IC = 16
        NCH = 128 // IC
        ACT_HEADS = (1, 3, 4, 6, 7)
        hT_ = [P.sb([128, 8, TG * 128], BF16, "hT%d" % i) for i in range(2)]
        ab_ = [[P.sb([128, 16, 128], F32, "ab4_%d_%d" % (i, u)) for u in range(TG)] for i in range(2)]
        rt_ = [[P.sb([128, 16], F32, "rt%d_%d" % (i, u)) for u in range(TG)] for i in range(2)]
        Wc = [[P.sb([128, IC * 128], BF16, "Wc%d_%d" % (i, u)) for u in range(TG)] for i in range(2)]
        et = [P.sb([128, IC, 128], F32, "et%d" % i) for i in range(4)]
        mt = [P.sb([128, IC, 128], BF16, "mt%d" % i) for i in range(2)]
        dnb = [P.sb([128, 8, 512], BF16, "dnb%d" % i) for i in range(3)]
        upb = [P.sb([128, 4, D], BF16, "upb%d" % i) for i in range(3)]
        Gs = [P.sb([128, 512], BF16, "G%d" % i) for i in range(2)]
        GT = [P.sb([128, 4, 128], BF16, "GT%d" % i) for i in range(2)]
        py = [P.ps([128, 2, 512], F32, "py%d" % u) for u in range(TG)]
        pd = [P.ps([128, 512], F32, "pd%d" % i) for i in range(2)]
        ptg = [P.ps([128, 8, 128], BF16, "ptg%d" % i) for i in range(2)]
        h2 = P.sb([128, D], F32, "h2"); yo = P.sb([128, D], F32, "yo")
        junk = P.sb([128, D], BF16, "junk4"); ss = P.sb([128, 1], F32, "ss4"); rstd = P.sb([128, 1], F32, "rstd4")
        qn = [0]
        def load_group(t2):
            hT2, ab2, rt2 = hT_[t2 % 2], ab_[t2 % 2], rt_[t2 % 2]
            dma(hT2, hT2[:], d_hn3T, d_hn3T[:, :, t2 * TG * 128:(t2 + 1) * TG * 128])
            for u in range(TG):
                j = t2 * TG + u
                dma(ab2[u], ab2[u][:].rearrange("p r k -> p (r k)"), d_route, d_route[j * 128:(j + 1) * 128, 0:2048])
                dma(rt2[u], rt2[u][:], d_route, d_route[j * 128:(j + 1) * 128, 2048:2064])
        load_group(0)
        for tg in range(NO // TG):
            hT, ab, rt = hT_[tg % 2], ab_[tg % 2], rt_[tg % 2]
            if tg + 1 < NO // TG:
                load_group(tg + 1)

            def wgen(c):
                its = [(u, h) for u in range(TG) for h in range(8)]
                K = len(its)
                eb_ = {}; mb_ = {}

                def E_(n):
                    u, h = its[n]
                    e_ = et[qn[0] % 4]; qn[0] += 1
                    eb_[n] = e_
                    if h in ACT_HEADS:
                        for i_ in range(IC):
                            P.op('act', lambda e, e_=e_, i_=i_, u=u, h=h: e.activation(out=e_[:, i_, :], in_=ab[u][:, 2 * h + 1, :], func=AF.Copy,
                                                                                  scale=ab[u][:, 2 * h, c * IC + i_:c * IC + i_ + 1]),
                                 [ab[u]], [e_] if i_ in (0, IC - 1) else [])
                    else:
                        tt('dve', e_, e_[:], ab[u], ab[u][:, 2 * h, c * IC:(c + 1) * IC].unsqueeze(2).to_broadcast([128, IC, 128]),
                           ab[u], ab[u][:, 2 * h + 1, :].unsqueeze(1).to_broadcast([128, IC, 128]), ALU.mult)

                def S_(n):
                    u, h = its[n]
                    e_ = eb_.pop(n)
                    w_ap = Wc[c % 2][u][:].rearrange("p (a b) -> p a b", a=IC)
                    if h == 0:
                        stt('dve', Wc[c % 2][u], w_ap, e_, e_[:], rt[u][:, h:h + 1], e_, e_[:], ALU.is_ge, ALU.mult, rd=[rt[u]])
                    else:
                        m_ = mt[n % 2]
                        mb_[n] = m_
                        stt('dve', m_, m_[:], e_, e_[:], rt[u][:, h:h + 1], e_, e_[:], ALU.is_ge, ALU.mult, rd=[rt[u]])

                def A_(n):
                    u, h = its[n]
                    if h == 0:
                        return
                    m_ = mb_.pop(n)
                    w_ap = Wc[c % 2][u][:].rearrange("p (a b) -> p a b", a=IC)
                    tt('dve', Wc[c % 2][u], w_ap, Wc[c % 2][u], w_ap, m_, m_[:], ALU.add)
                for n in range(K + 2):
                    if n < K:
                        E_(n)
                    if 1 <= n <= K:
                        S_(n - 1)
                    if n >= 2:
                        A_(n - 2)
                    yield

            def load_w(ecx):
                dn, ub = dnb[ecx % 3], upb[ecx % 3]
                dma(dn, dn[:], d_downT, d_downT[:, ecx * 512:(ecx + 1) * 512].rearrange("(c p) e -> p c e", p=128))
                dma(ub, ub[:], d_up, d_up[ecx * 512:(ecx + 1) * 512, :].rearrange("(s p) d -> p s d", p=128))

            items = [(ecx, u) for ecx in range(32) for u in range(TG)]
            N = len(items)

            def stA(n):
                ecx, u = items[n]
                if u == 0:
                    if ecx == 0:
                        load_w(0)
                    if ecx + 1 < 32:
                        load_w(ecx + 1)
                    if ecx % 4 == 0:
                        for _ in wg[0]:
                            pass
                        wg[0] = wgen(ecx // 4 + 1) if ecx // 4 + 1 < NCH else iter(())
                for _ in range(3):
                    next(wg[0], None)
                dn = dnb[ecx % 3]
                pdt = pd[n % 2]; G = Gs[n % 2]
                for c in range(8):
                    mm(pdt, pdt[:], hT, hT[:, c, u * 128:(u + 1) * 128], dn, dn[:, c, :], start=(c == 0), stop=(c == 7))
                act(G, G[:], pdt, pdt[:], AF.Gelu_apprx_tanh)
                wch = Wc[(ecx // 4) % 2][u]
                tt('dve', G, G[:], G, G[:], wch, wch[:, (ecx % 4) * 512:(ecx % 4 + 1) * 512], ALU.mult)

            def stB(n):
                G = Gs[n % 2]; gt_ = GT[n % 2]; pt_ = ptg[n % 2]
                for s_ in range(4):
                    tr(pt_, pt_[:, s_, :], G, G[:, s_ * 128:(s_ + 1) * 128], ident)
                cp('act', gt_, gt_[:], pt_, pt_[:, 0:4, :])

            def stC(n):
                ecx, u = items[n]
                gt_ = GT[n % 2]; ub = upb[ecx % 3]
                for half in range(2):
                    for s_ in range(4):
                        mm(py[u], py[u][:, half, :], gt_, gt_[:, s_, :], ub, ub[:, s_, half * 512:(half + 1) * 512],
                           start=(ecx == 0 and s_ == 0), stop=(ecx == 31 and s_ == 3))

            wg = [iter(())]
            for _ in wgen(0):
                pass
            for n in range(N + 2):
                if n < N:
                    stA(n)
                if 1 <= n <= N:
                    stB(n - 1)
                if n >= 2:
                    stC(n - 2)
            for _ in wg[0]:
                pass
            for u in range(TG):
                j = tg * TG + u
                dma(h2, h2[:], d_h, d_h[j * 128:(j + 1) * 128, :])
                tt('dve', h2, h2[:], h2, h2[:], py[u], py[u][:].rearrange("p a b -> p (a b)"), ALU.add)
                rmsnorm(h2, h2[:], 4, yo, yo[:], junk, ss, rstd)
                dma(out, out[j * 128:(j + 1) * 128, :], yo, yo[:])
        P.pop()

    P.finish()
    return nc, dict(npair=npair, pair_off=pair_off, NCC=NCC, NCMP=NCMP, ninst=P.ninst)


def _t5_bucket_np(dist):
    n = np.maximum(dist, 0)
    nf = np.maximum(n, 1).astype(np.float32)
    log_ratio = (np.log(nf / np.float32(16)) / np.float32(math.log(2048 / 16))).astype(np.float32)
    large = 16 + (log_ratio * np.float32(16)).astype(np.int32)
    large = np.minimum(large, 31)
    return np.where(n < 16, n, large).astype(np.int64)


def _bias_tile(rel_bias, g, dist, valid):
    bk = _t5_bucket_np(dist)
    outt = np.empty((128, 4, 128), np.float32)
    for h in range(4):
        outt[:, h, :] = np.where(valid, rel_bias[bk, g * 4 + h], np.float32(NEG))
    return outt.reshape(128, 512)


def make_core_inputs(inputs, T, b, p, meta):
    NT = T // 128; NO = NT // 2; NS = T // 64; NCMP = meta['NCMP']; NCC = meta['NCC']
    f = lambda a: np.ascontiguousarray(np.asarray(a, dtype=np.float32))
    x = f(inputs['x'][b]); rel_bias = f(inputs['rel_bias'])
    m = {}
    m['xb'] = x
    m['xo'] = np.ascontiguousarray(x.reshape(NO, 2, 128, D)[:, p].reshape(NO * 128, D))
    m['mem'] = f(inputs['mem'][b])
    w_in = f(inputs['w_in'][0])
    m['w_in'] = np.ascontiguousarray(np.concatenate([w_in[:, ORIG[k][0]:ORIG[k][1]] for k in PERM_ORDER], axis=1))
    m['gw2'] = f(inputs['gla_gate_w2'][0]); m['gb'] = f(inputs['gla_gate_b'][0]).reshape(1, 256)
    m['gnorm'] = f(inputs['gla_out_norm'][0]).reshape(1, 128)
    m['norms'] = np.stack([f(inputs['norm_mix'][0]), f(inputs['norm_xattn'][0]), f(inputs['norm_mem'][0]),
                           f(inputs['norm_ffn'][0]), f(inputs['norm_final'])], 0)
    cw1 = np.stack([f(inputs['cmp_k_w1'][0]), f(inputs['cmp_v_w1'][0])], 0)
    m['cw1'] = np.ascontiguousarray(cw1.reshape(2, 32, 64, 128).transpose(0, 2, 1, 3))
    cpos = np.stack([f(inputs['cmp_pos_k'][0]), f(inputs['cmp_pos_v'][0])], 0)
    m['cpos'] = np.ascontiguousarray(cpos.transpose(0, 2, 1))
    m['cw2'] = np.stack([f(inputs['cmp_k_w2'][0]), f(inputs['cmp_v_w2'][0])], 0)
    m['w_out'] = f(inputs['w_out'][0])
    m['xa_w'] = np.stack([f(inputs['xa_wq'][0]), f(inputs['xa_wk'][0]), f(inputs['xa_wv'][0]), f(inputs['xa_wo'][0])], 0)
    m['pwq'] = f(inputs['peer_wq'][0])
    sk = f(inputs['peer_subkeys'][0])
    skd = np.zeros((8, 128, 256), np.float32)
    for h in range(8):
        for pp in range(2):
            skd[h, pp * 64:(pp + 1) * 64, pp * 128:(pp + 1) * 128] = sk[h, pp].T
    m['skd'] = skd
    m['downT'] = np.ascontiguousarray(f(inputs['peer_down'][0]).T)
    m['up'] = f(inputs['peer_up'][0])
    m['c_ident'] = np.eye(128, dtype=np.float32)
    s_ = np.arange(128)[:, None]; t_ = np.arange(128)[None, :]
    m['c_ucs'] = np.where(s_ <= t_, -1.0 / 16, 0.0).astype(np.float32)
    m['c_urev'] = np.where(s_ > t_, -1.0 / 16, 0.0).astype(np.float32)
    m['c_causal'] = (s_ <= t_).astype(np.float32)
    m['c_selu'] = np.stack([np.eye(128) * (1 - p), np.eye(128) * p], 0).astype(np.float32)
    m['c_pv'] = np.tile(np.array([[1.0 - p, float(p)]], np.float32), (128, 1))
    kk = np.arange(128)[:, None]; qq = np.arange(128)[None, :]
    selb = np.empty((2, 128, 15, 512), np.float32); winb = np.empty((2, 128, 6, 512), np.float32)
    for g in range(2):
        for mm_ in range(15):
            j = mm_ - 1 + p
            dist = 128 * j + qq - kk
            selb[g, :, mm_, :] = _bias_tile(rel_bias, g, dist, (dist >= 0) & (j >= 0))
        for mm_ in range(6):
            j = mm_ - 1 + p
            dist = 128 * j + qq - kk
            winb[g, :, mm_, :] = _bias_tile(rel_bias, g, dist, (dist >= 0) & (dist < 512) & (j >= 0))
    m['c_selb'] = selb; m['c_winb'] = winb
    cmpb = np.empty((2, meta['npair'], 128, 512), np.float32)
    for jj in range(NO):
        for c in range(min(NCC, cmp_nchunks(jj))):
            n = 128 * c + kk
            t = (2 * jj + p) * 128 + qq
            dist = t - (16 * n + 31)
            for g in range(2):
                cmpb[g, meta['pair_off'][jj] + c] = _bias_tile(rel_bias, g, dist, (dist >= 0) & (n < NCMP))
    m['c_cmpb'] = cmpb
    far = np.empty((2, 128, 4, 128), np.float32)
    for g in range(2):
        for h in range(4):
            far[g, :, h, :] = rel_bias[31, g * 4 + h]
    m['c_far'] = far.reshape(2, 128, 512)
    wimp = np.zeros((NCC * 128, 128), np.float32)
    for s in range(NS):
        for r in range(-1, 4):
            n = 4 * s + r
            lo = 16 * r
            ov = min(lo + 32, 64) - max(lo, 0)
            if 0 <= n < NCMP:
                wimp[n, s] += ov / 32.0
    m['c_wimp'] = wimp.reshape(NCC, 128, 128)
    m12 = np.zeros((NO, 128, 2, 128), np.float32)
    sid = np.arange(128)[None, :]
    for jj in range(NO):
        t = (2 * jj + p) * 128 + np.arange(128)[:, None]
        cur = t // 64
        visible = (sid * 64 <= t) & (sid < NS)
        f0 = (sid == 0); f1 = (sid == cur); f2 = (sid == cur - 1)
        forced = f0 | f1 | f2
        m12[jj, :, 0, :] = (visible & ~forced)
        add = np.where(~visible, -100.0 - sid, 0.0)
        add = np.where(f2, 100.0, add); add = np.where(f1, 101.0, add); add = np.where(f0 & (sid < NS), 102.0, add)
        m12[jj, :, 1, :] = add
    m['c_m12'] = m12
    ex = np.zeros((128, T), np.float32)
    ex[np.arange(T) // 64, np.arange(T)] = 1.0
    m['c_ex'] = ex.astype(NPBF)
    return m


_CACHE = {}


def kernel(**inputs):
    T = inputs['x'].shape[1]
    B = inputs['x'].shape[0]
    if T not in _CACHE:
        _CACHE[T] = build(T)
    nc, meta = _CACHE[T]
    in_maps = []
    for c in range(2 * B):
        in_maps.append(make_core_inputs(inputs, T, c // 2, c % 2, meta))
    res = run_bass_kernel_spmd(nc, in_maps, core_ids=list(range(2 * B)))
    NO = T // 256
    outp = np.empty((B, T // 128, 128, D), np.float32)
    for c in range(2 * B):
        o = np.asarray(res.results[c]["out"], dtype=np.float32).reshape(NO, 128, D)
        outp[c // 2, (c % 2)::2] = o
    return outp.reshape(B, T, D)
```

```python
import math
import numpy as np
import ml_dtypes
import concourse.bass as bass
import concourse.mybir as mybir
from concourse.bass_utils import run_bass_kernel_spmd
from contextlib import ExitStack

F32 = mybir.dt.float32
BF16 = mybir.dt.bfloat16
AF = mybir.ActivationFunctionType
ALU = mybir.AluOpType
AX = mybir.AxisListType
NPBF = ml_dtypes.bfloat16

D = 1024
NEG = -30000.0


class T:
    def __init__(self, h, name):
        self.h = h
        self.name = name
        self.w = None
        self.r = []
        self.psum = False
        self.dram = False

    def __getitem__(self, k):
        return self.h[k]


class Prog:
    def __init__(self, nc, n_dma_sems=48):
        self.nc = nc
        self.es = ExitStack()
        self.scopes = []
        self.eng = {'pe': nc.tensor, 'dve': nc.vector, 'act': nc.scalar,
                    'pool': nc.gpsimd, 'sp': nc.sync}
        self.sems = {}
        for k in self.eng:
            self.sems['e_' + k] = self.es.enter_context(nc.semaphore('e_' + k))
        self.cnt = {k: 0 for k in self.sems}
        self.ndma = n_dma_sems
        for i in range(n_dma_sems):
            key = 'd_%d' % i
            self.sems[key] = self.es.enter_context(nc.semaphore(key))
            self.cnt[key] = 0
        self.dma_rr = 0
        self.known = {k: {} for k in self.eng}
        self.ntile = 0
        self.ninst = 0

    def push(self):
        self.scopes.append(ExitStack())

    def pop(self):
        self.barrier()
        self.scopes.pop().close()

    def _stack(self):
        return self.scopes[-1] if self.scopes else self.es

    def sb(self, shape, dt, name=None):
        self.ntile += 1
        name = (name or 't') + '_%d' % self.ntile
        h = self._stack().enter_context(self.nc.sbuf_tensor(name, list(shape), dt))
        return T(h, name)

    def ps(self, shape, dt, name=None):
        self.ntile += 1
        name = (name or 'p') + '_%d' % self.ntile
        h = self._stack().enter_context(self.nc.psum_tensor(name, list(shape), dt))
        t = T(h, name)
        t.psum = True
        return t

    def dram(self, name, shape, dt, kind="Internal"):
        h = self.nc.dram_tensor(name, list(shape), dt, kind=kind).ap()
        t = T(h, name)
        t.dram = True
        return t

    def _deps(self, reads, writes, e=None):
        deps = []
        for t in reads:
            if t.w is not None:
                deps.append(t.w)
            if t.psum:
                deps.extend([tok for tok in t.r if tok[0] != 'e_' + str(e)])
        for t in writes:
            if t.w is not None:
                deps.append(t.w)
            deps.extend(t.r)
        return deps

    def _waits(self, e, deps):
        kn = self.known[e]
        need = {}
        for (k, v) in deps:
            if kn.get(k, 0) >= v:
                continue
            if need.get(k, 0) < v:
                need[k] = v
        for k, v in need.items():
            kn[k] = v
        return list(need.items())

    def _emit(self, e, waits, fn, inc):
        eng = self.eng[e]
        for (k, v) in waits:
            eng.wait_ge(self.sems[k], v)
        ins = fn(eng)
        ins.then_inc(self.sems[inc[0]], inc[1])
        self.ninst += 1

    def op(self, e, fn, reads=(), writes=()):
        deps = self._deps(reads, writes, e)
        key = 'e_' + e
        if e == 'pe':
            deps = [d for d in deps if d[0] != key]
        waits = self._waits(e, deps)
        self.cnt[key] += 1
        tok = (key, self.cnt[key])
        self._emit(e, waits, fn, (key, 1))
        for t in reads:
            t.r.append(tok)
        for t in writes:
            t.w = tok
            t.r = []
        return tok

    def dma(self, e, out_t, out_ap, in_t, in_ap, **kw):
        reads = [in_t]
        writes = [out_t]
        deps = self._deps(reads, writes)
        key = 'd_%d' % self.dma_rr
        self.dma_rr = (self.dma_rr + 1) % self.ndma
        if self.cnt[key] > 0:
            deps.append((key, self.cnt[key]))
        waits = self._waits(e, deps)
        self.cnt[key] += 16
        tok = (key, self.cnt[key])
        self._emit(e, waits, lambda eng: eng.dma_start(out=out_ap, in_=in_ap, **kw), (key, 16))
        in_t.r.append(tok)
        out_t.w = tok
        out_t.r = []
        return tok

    def barrier(self):
        allt = [(k, v) for k, v in self.cnt.items() if v > 0]
        for e in self.eng:
            for (k, v) in self._waits(e, allt):
                self.eng[e].wait_ge(self.sems[k], v)

    def finish(self):
        self.barrier()
        while self.scopes:
            self.scopes.pop().close()
        self.es.close()


ORIG = dict(gq=(0, 256), gk=(256, 512), gv=(512, 1024), gr=(1024, 1536), glr=(1536, 1552),
            nq=(1552, 2064), kc=(2064, 2192), vc=(2192, 2320), ks=(2320, 2448), vs=(2448, 2576),
            kw=(2576, 2704), vw=(2704, 2832), ng=(2832, 2856))
PERM_ORDER = ['gq', 'gk', 'gv', 'gr', 'nq', 'kc', 'vc', 'ks', 'vs', 'kw', 'vw', 'ng', 'glr']
NCOL = 2856


def cmp_nchunks(jj):
    return min(4, (16 * jj + 15 + 127) // 128)


def build(T, debug=False, upto=9, cut=99):
    NT = T // 128
    NO = NT // 2
    NS = T // 64
    TO = T // 2
    NCMP = (T - 32) // 16 + 1
    NCC = (NCMP + 127) // 128
    pair_off = []
    npair = 0
    for jj in range(NO):
        pair_off.append(npair)
        npair += min(NCC, cmp_nchunks(jj))

    nc = bass.Bass("TRN2", target_bir_lowering=False)
    P = Prog(nc)
    SK = "ExternalOutput" if debug else "Internal"

    def inp(name, shape, dt=F32):
        return P.dram(name, shape, dt, kind="ExternalInput")

    xb = inp("xb", [T, D]); xo = inp("xo", [TO, D]); memb = inp("mem", [256, D])
    w_in = inp("w_in", [D, NCOL]); gw2 = inp("gw2", [16, 256]); gb = inp("gb", [1, 256])
    gnorm = inp("gnorm", [1, 128])
    norms = inp("norms", [5, D])
    cw1 = inp("cw1", [2, 64, 32, 128]); cpos = inp("cpos", [2, 64, 32]); cw2 = inp("cw2", [2, 128, 64])
    w_out = inp("w_out", [D, D]); xa_w = inp("xa_w", [4, D, D])
    pwq = inp("pwq", [D, D]); skd = inp("skd", [8, 128, 256])
    downT = inp("downT", [D, 16384]); up = inp("up", [16384, D])
    c_ident = inp("c_ident", [128, 128]); c_ucs = inp("c_ucs", [128, 128]); c_urev = inp("c_urev", [128, 128])
    c_causal = inp("c_causal", [128, 128]); c_selu = inp("c_selu", [2, 128, 128]); c_pv = inp("c_pv", [128, 2])
    c_selb = inp("c_selb", [2, 128, 15, 512]); c_winb = inp("c_winb", [2, 128, 6, 512])
    c_cmpb = inp("c_cmpb", [2, npair, 128, 512])
    c_far = inp("c_far", [2, 128, 512])
    c_wimp = inp("c_wimp", [NCC, 128, 128]); c_m12 = inp("c_m12", [NO, 128, 2, 128])
    c_ex = inp("c_ex", [128, T], BF16)
    out = P.dram("out", [TO, D], F32, kind="ExternalOutput")

    d_nq = P.dram("d_nq", [T, 512], BF16, kind=SK)
    d_ng = P.dram("d_ng", [T, 24], F32, kind=SK)
    d_ogla = P.dram("d_ogla", [T, 512], BF16, kind=SK)
    d_kT = P.dram("d_kT", [4, 128, T], BF16, kind=SK)
    d_vs = P.dram("d_vs", [T, 128], BF16, kind=SK)
    d_vw = P.dram("d_vw", [T, 128], BF16, kind=SK)
    d_omix = P.dram("d_omix", [TO, D], BF16, kind=SK)
    d_h = P.dram("d_h", [TO, D], F32, kind=SK)
    d_hn3T = P.dram("d_hn3T", [128, 8, TO], BF16, kind=SK)
    d_route = P.dram("d_route", [TO, 2064], F32, kind=SK)
    d_downT = P.dram("d_downT", [D, 16384], BF16, kind="Internal")
    d_up = P.dram("d_up", [16384, D], BF16, kind="Internal")
    d_cmp = P.dram("d_cmp", [2, 64, 512], BF16, kind=SK)
    d_dbg = P.dram("d_dbg", [3, TO, 512], F32, kind=SK)
    d_imp = P.dram("d_imp", [2, TO, 128], F32, kind=SK)

    def mm(ot, o_ap, lt, l_ap, rt, r_ap, start=True, stop=True):
        P.op('pe', lambda e: e.matmul(o_ap, lhsT=l_ap, rhs=r_ap, start=start, stop=stop), [lt, rt], [ot])

    def tr(ot, o_ap, it, i_ap, idt):
        P.op('pe', lambda e: e.transpose(out=o_ap, in_=i_ap, identity=idt[:]), [it, idt], [ot])

    def act(ot, o_ap, it, i_ap, func, bias=None, scale=None, accum=None, rd=(), wr=()):
        kw = {}
        if bias is not None:
            kw['bias'] = bias
        if scale is not None:
            kw['scale'] = scale
        if accum is not None:
            kw['accum_out'] = accum
        P.op('act', lambda e: e.activation(out=o_ap, in_=i_ap, func=func, **kw), [it] + list(rd), [ot] + list(wr))

    def cp(eng, ot, o_ap, it, i_ap):
        if eng == 'act':
            P.op('act', lambda e: e.copy(out=o_ap, in_=i_ap), [it], [ot])
        else:
            P.op(eng, lambda e: e.tensor_copy(out=o_ap, in_=i_ap), [it], [ot])

    def tt(eng, ot, o_ap, at, a_ap, bt, b_ap, op):
        P.op(eng, lambda e: e.tensor_tensor(out=o_ap, in0=a_ap, in1=b_ap, op=op), [at, bt], [ot])

    def ts(eng, ot, o_ap, at, a_ap, s1, s2, op0, op1=None, rd=()):
        if op1 is None:
            P.op(eng, lambda e: e.tensor_scalar(out=o_ap, in0=a_ap, scalar1=s1, scalar2=None, op0=op0), [at] + list(rd), [ot])
        else:
            P.op(eng, lambda e: e.tensor_scalar(out=o_ap, in0=a_ap, scalar1=s1, scalar2=s2, op0=op0, op1=op1), [at] + list(rd), [ot])

    def stt(eng, ot, o_ap, at, a_ap, sc, bt, b_ap, op0, op1, rd=()):
        P.op(eng, lambda e: e.scalar_tensor_tensor(out=o_ap, in0=a_ap, scalar=sc, in1=b_ap, op0=op0, op1=op1),
             [at, bt] + list(rd), [ot])

    def memset(eng, t, ap, v):
        P.op(eng, lambda e: e.memset(ap, v), [], [t])

    def recip(ot, o_ap, it, i_ap):
        P.op('dve', lambda e: e.reciprocal(out=o_ap, in_=i_ap), [it], [ot])

    dmaq = ['sp', 'act', 'pool']
    dq = [0]

    dma_pol = {'load': ['sp'], 'store': ['pool']}

    def dma(ot, o_ap, it, i_ap, q=None, **kw):
        if q is None:
            qs = dma_pol['store'] if ot.dram else dma_pol['load']
            q = qs[dq[0] % len(qs)]
            dq[0] += 1
        return P.dma(q, ot, o_ap, it, i_ap, **kw)

    ident = P.sb([128, 128], BF16, "ident"); identf = P.sb([128, 128], F32, "identf")
    ones = P.sb([128, 128], BF16, "ones")
    pv = P.sb([128, 2], F32, "pv")
    nrm = P.sb([128, 5, D], F32, "nrm")
    dma(identf, identf[:], c_ident, c_ident[:])
    dma(pv, pv[:], c_pv, c_pv[:])
    for k in range(5):
        dma(nrm, nrm[:, k, :], norms, norms[k:k + 1, :].partition_broadcast(128))
    cp('dve', ident, ident[:], identf, identf[:])
    memset('pool', ones, ones[:], 1.0)
    eps_t = P.sb([128, 1], F32, "eps_t")
    memset('dve', eps_t, eps_t[:], 1e-6)
    kcmpT = [P.sb([128, 512], BF16, "kcmpT%d" % g) for g in range(2)]
    vcmp = [P.sb([128, 4, 65], BF16, "vcmp%d" % g) for g in range(2)]

    def rmsnorm(xt, x_ap, gain_k, hn, hn_ap, junk, ss, rstd, n=D, eps=1e-6):
        memset('dve', ss, ss[:], 0.0)
        act(junk, junk[:, 0:n], xt, x_ap, AF.Square, accum=ss[:], rd=[ss], wr=[ss])
        act(rstd, rstd[:], ss, ss[:], AF.Ln, scale=1.0 / n, bias=eps_t[:, 0:1], rd=[eps_t])
        act(rstd, rstd[:], rstd, rstd[:], AF.Exp, scale=-0.5)
        stt('dve', hn, hn_ap, xt, x_ap, rstd[:, 0:1], nrm, nrm[:, gain_k, 0:n], ALU.mult, ALU.mult, rd=[rstd])

    if upto >= 1:
        P.push()
        winb = P.sb([128, 8, NCOL], BF16, "winb")
        wst = [P.sb([128, NCOL], F32, "wst%d" % i) for i in range(2)]
        w_in_v = w_in[:].rearrange("(c p) n -> c p n", p=128)
        for c in range(8):
            dma(wst[c % 2], wst[c % 2][:], w_in, w_in_v[c])
            cp(['dve', 'pool'][c % 2], winb, winb[:, c, :], wst[c % 2], wst[c % 2][:])
        gw2t = P.sb([16, 256], F32, "gw2t"); gbt = P.sb([1, 256], F32, "gbt"); onesrow = P.sb([1, 128], F32, "onesrow")
        gnt = P.sb([128, 128], F32, "gnt")
        ucs = P.sb([128, 128], F32, "ucs"); urev = P.sb([128, 128], F32, "urev"); causal = P.sb([128, 128], F32, "causal")
        m16 = P.sb([128, 1], F32, "m16")
        dma(gw2t, gw2t[:], gw2, gw2[:]); dma(gbt, gbt[:], gb, gb[:])
        dma(gnt, gnt[:], gnorm, gnorm[0:1, :].partition_broadcast(128))
        dma(ucs, ucs[:], c_ucs, c_ucs[:]); dma(urev, urev[:], c_urev, c_urev[:]); dma(causal, causal[:], c_causal, c_causal[:])
        memset('dve', onesrow, onesrow[:], 1.0)
        memset('dve', m16, m16[:], -1.0 / 16)
        S = P.sb([128, 2, 128], F32, "S"); Sb = P.sb([128, 2, 128], BF16, "Sb")
        memset('dve', S, S[:], 0.0); memset('pool', Sb, Sb[:], 0.0)

        xt = [P.sb([128, D], F32, "xt%d" % i) for i in range(2)]
        junk = P.sb([128, D], BF16, "junk"); ss = P.sb([128, 1], F32, "ss"); rstd = P.sb([128, 1], F32, "rstd")
        hn = P.sb([128, D], BF16, "hn"); hnT = P.sb([128, 8, 128], BF16, "hnT")
        pt = P.ps([128, 8, 128], BF16, "pt")
        pz = [P.ps([128, 512], F32, "pz%d" % i) for i in range(3)]
        pg1 = P.ps([128, 512], F32, "pg1"); pg2 = P.ps([128, 512], F32, "pg2")
        po = P.ps([128, 512], F32, "po"); ptb = P.ps([128, 8, 128], BF16, "ptb")
        glr = P.sb([128, 16], F32, "glr"); glrT = P.sb([16, 128], F32, "glrT")
        e1 = P.sb([128, 256], F32, "e1"); L = P.sb([128, 256], F32, "L")
        eb = P.sb([128, 256], F32, "eb"); enb = P.sb([128, 256], F32, "enb"); ec = P.sb([128, 256], F32, "ec")
        ebl = P.sb([128, 2], F32, "ebl")
        qk = P.sb([128, 3, 256], BF16, "qk")
        qkT = P.sb([128, 4, 128], BF16, "qkT")
        vv = P.sb([128, 512], BF16, "vv"); sg = P.sb([128, 512], F32, "sg")
        AT = P.sb([128, 128], BF16, "AT")
        ssq = P.sb([128, 4], F32, "ssq"); rs4 = P.sb([128, 4], F32, "rs4"); tmpn = P.sb([128, 128], F32, "tmpn")
        junk2 = P.sb([128, 128], F32, "junk2")
        og = P.sb([128, 512], BF16, "og"); otmp4 = P.sb([128, 512], F32, "otmp4")
        nqs = P.sb([128, 512], BF16, "nqs"); ngs = P.sb([128, 24], F32, "ngs")
        kk = P.sb([128, 4, 128], BF16, "kk"); kkT = P.sb([128, 4, 128], BF16, "kkT")
        vsw = P.sb([128, 2, 128], BF16, "vsw")
        cast_jobs = []
        if upto >= 4:
            stg = [P.sb([128, 4096], F32, "stg%d" % i) for i in range(2)]
            stb = [P.sb([128, 4096], BF16, "stb%d" % i) for i in range(2)]
            for r in range(8):
                for c in range(4):
                    cast_jobs.append((downT, downT[r * 128:(r + 1) * 128, c * 4096:(c + 1) * 4096],
                                      d_downT, d_downT[r * 128:(r + 1) * 128, c * 4096:(c + 1) * 4096]))
            sv = up[:].rearrange("(a p f) d -> a p (f d)", p=128, f=4)
            dv = d_up[:].rearrange("(a p f) d -> a p (f d)", p=128, f=4)
            for a in range(32):
                cast_jobs.append((up, sv[a], d_up, dv[a]))
        cj = [0]

        def cast_job():
            k_ = cj[0]
            if k_ < len(cast_jobs):
                src_t, s_ap, dst_t, d_ap = cast_jobs[k_]
                P.dma('pool', stg[k_ % 2], stg[k_ % 2][:], src_t, s_ap)
            if 1 <= k_ <= len(cast_jobs):
                src_t, s_ap, dst_t, d_ap = cast_jobs[k_ - 1]
                a, b_ = stg[(k_ - 1) % 2], stb[(k_ - 1) % 2]
                for q_ in range(4):
                    cp('act', b_, b_[:, q_ * 1024:(q_ + 1) * 1024], a, a[:, q_ * 1024:(q_ + 1) * 1024])
                P.dma('pool', dst_t, d_ap, b_, b_[:])
            cj[0] += 1
        cA, cB, cC, cD, cE, cF = 0, 512, 1024, 1536, 2048, 2560
        hn2_ = [hn, P.sb([128, D], BF16, "hn_b")]; hnT2_ = [hnT, P.sb([128, 8, 128], BF16, "hnT_b")]
        ss2_ = [ss, P.sb([128, 1], F32, "ss_b")]; rstd2_ = [rstd, P.sb([128, 1], F32, "rstd_b")]

        def front(i):
            x_t = xt[i % 2]
            dma(x_t, x_t[:], xb, xb[i * 128:(i + 1) * 128, :])
            rmsnorm(x_t, x_t[:], 0, hn2_[i % 2], hn2_[i % 2][:], junk, ss2_[i % 2], rstd2_[i % 2])
            for c in range(8):
                tr(pt, pt[:, c, :], hn2_[i % 2], hn2_[i % 2][:, c * 128:(c + 1) * 128], ident)
            cp('act', hnT2_[i % 2], hnT2_[i % 2][:], pt, pt[:])
        front(0)
        for i in range(NT):
            hnT = hnT2_[i % 2]

            def zgroup(pz_t, c0, n):
                for c in range(8):
                    mm(pz_t, pz_t[:, 0:n], hnT, hnT[:, c, :], winb, winb[:, c, c0:c0 + n], start=(c == 0), stop=(c == 7))
            zgroup(pz[0], cF, 296)
            cp('act', kk, kk[:, 3, :], pz[0], pz[0][:, 0:128])
            cp('act', vsw, vsw[:, 1, :], pz[0], pz[0][:, 128:256])
            cp('act', glr, glr[:], pz[0], pz[0][:, 280:296])
            act(ngs, ngs[:], pz[0], pz[0][:, 256:280], AF.Exp, scale=-1.0)
            ts('dve', ngs, ngs[:], ngs, ngs[:], 1.0, None, ALU.add)
            recip(ngs, ngs[:], ngs, ngs[:])
            dma(d_ng, d_ng[i * 128:(i + 1) * 128, :], ngs, ngs[:])
            zgroup(pz[1], cE, 512)
            cp('act', kk, kk[:, 0:3, :], pz[1], pz[1][:, 0:384].rearrange("p (a b) -> p a b", a=3))
            cp('dve', vsw, vsw[:, 0, :], pz[1], pz[1][:, 384:512])
            dma(d_vs, d_vs[i * 128:(i + 1) * 128, :], vsw, vsw[:, 0, :])
            dma(d_vw, d_vw[i * 128:(i + 1) * 128, :], vsw, vsw[:, 1, :])
            for a in range(4):
                tr(ptb, ptb[:, a, :], kk, kk[:, a, :], ident)
            cp('act', kkT, kkT[:], ptb, ptb[:, 0:4, :])
            dma(d_kT, d_kT[:, :, i * 128:(i + 1) * 128].rearrange("a p t -> p a t"), kkT, kkT[:])
            zgroup(pz[2], cD, 512)
            P.op('act', lambda e, pzt=pz[2]: e.mul(out=nqs[:], in_=pzt[:], mul=0.125), [pz[2]], [nqs])
            dma(d_nq, d_nq[i * 128:(i + 1) * 128, :], nqs, nqs[:])
            tr(pg1, pg1[0:16, 0:128], glr, glr[:], identf)
            cp('dve', glrT, glrT[:], pg1, pg1[0:16, 0:128])
            mm(pg1, pg1[:, 256:512], glrT, glrT[:], gw2t, gw2t[:], start=True, stop=False)
            mm(pg1, pg1[:, 256:512], onesrow, onesrow[:], gbt, gbt[:], start=False, stop=True)
            zgroup(pz[0], cA, 512)
            zgroup(pz[1], cB, 512)
            zgroup(pz[2], cC, 512)
            act(e1, e1[:], pg1, pg1[:, 256:512], AF.Exp, scale=-1.0)
            act(L, L[:], e1, e1[:], AF.Ln, bias=1.0)
            cp('act', vv, vv[:], pz[1], pz[1][:])
            act(sg, sg[:], pz[2], pz[2][:], AF.Exp, scale=-1.0)
            act(sg, sg[:], sg, sg[:], AF.Ln, bias=1.0)
            act(sg, sg[:], sg, sg[:], AF.Exp, scale=-1.0)
            tt('dve', sg, sg[:], sg, sg[:], pz[2], pz[2][:], ALU.mult)
            tt('dve', sg, sg[:].rearrange("p (h d) -> p h d", h=4), sg, sg[:].rearrange("p (h d) -> p h d", h=4),
               gnt, gnt[:].unsqueeze(1).to_broadcast([128, 4, 128]), ALU.mult)
            mm(pg2, pg2[:, 0:256], ucs, ucs[:], L, L[:])
            mm(pg2, pg2[:, 256:512], urev, urev[:], L, L[:])
            for hp in range(2):
                mm(pg1, pg1[:, hp:hp + 1], L, L[:, hp * 128:(hp + 1) * 128], m16, m16[:])
            act(eb, eb[:], pg2, pg2[:, 0:256], AF.Exp)
            act(enb, enb[:], pg2, pg2[:, 0:256], AF.Exp, scale=-1.0)
            act(ec, ec[:], pg2, pg2[:, 256:512], AF.Exp)
            act(ebl, ebl[:], pg1, pg1[:, 0:2], AF.Exp)
            stt('dve', qk, qk[:, 0, :], pz[0], pz[0][:, 0:256], 0.125, eb, eb[:], ALU.mult, ALU.mult)
            tt('dve', qk, qk[:, 1, :], pz[0], pz[0][:, 256:512], enb, enb[:], ALU.mult)
            tt('dve', qk, qk[:, 2, :], pz[0], pz[0][:, 256:512], ec, ec[:], ALU.mult)
            for a in range(4):
                tr(ptb, ptb[:, 4 + a, :], qk, qk[:, a // 2, (a % 2) * 128:(a % 2 + 1) * 128], ident)
            cp('act', qkT, qkT[:], ptb, ptb[:, 4:8, :])
            if i + 1 < NT:
                front(i + 1)
            memset('dve', ssq, ssq[:], 0.0)
            for h in range(4):
                hp, hh = h // 2, h % 2
                pr = slice(hh * 64, hh * 64 + 64)
                mm(pg2, pg2[:, 0:128], qkT, qkT[pr, 2 + hp, :], qkT, qkT[pr, hp, :])
                tt('dve', AT, AT[:], pg2, pg2[:, 0:128], causal, causal[:], ALU.mult)
                o_ap = po[:, h * 128:(h + 1) * 128]
                mm(po, o_ap, AT, AT[:], vv, vv[:, h * 128:(h + 1) * 128], start=True, stop=False)
                mm(po, o_ap, qkT, qkT[pr, hp, :], Sb, Sb[pr, hp, :], start=False, stop=True)
                mm(pg2, pg2[:, 128:256], qk, qk[:, 2, hp * 128:(hp + 1) * 128], vv, vv[:, h * 128:(h + 1) * 128])
                stt('dve', Sb, Sb[pr, hp, :], S, S[pr, hp, :], ebl[pr, hp:hp + 1], pg2, pg2[pr, 128:256], ALU.mult, ALU.add, rd=[ebl])
                stt('dve', S, S[pr, hp, :], S, S[pr, hp, :], ebl[pr, hp:hp + 1], pg2, pg2[pr, 128:256], ALU.mult, ALU.add, rd=[ebl])
                act(junk2, junk2[:], po, o_ap, AF.Square, accum=ssq[:, h:h + 1], rd=[ssq], wr=[ssq])
            act(rs4, rs4[:], ssq, ssq[:], AF.Ln, scale=1.0 / 128, bias=eps_t[:, 0:1], rd=[eps_t])
            act(rs4, rs4[:], rs4, rs4[:], AF.Exp, scale=-0.5)
            tt('dve', otmp4, otmp4[:], po, po[:], sg, sg[:], ALU.mult)
            tt('dve', og, og[:].rearrange("p (h d) -> p h d", h=4), otmp4, otmp4[:].rearrange("p (h d) -> p h d", h=4),
               rs4, rs4[:].unsqueeze(2).to_broadcast([128, 4, 128]), ALU.mult)
            dma(d_ogla, d_ogla[i * 128:(i + 1) * 128, :], og, og[:])
            for _ in range((len(cast_jobs) + NT - 1) // NT):
                cast_job()
        while cast_jobs and cj[0] <= len(cast_jobs):
            cast_job()
        P.pop()

        P.push()
        w1f = P.sb([64, 32, 128], F32, "w1f"); w1b = P.sb([64, 32, 128], BF16, "w1b")
        posf = P.sb([64, 32], F32, "posf"); posb = P.sb([64, 32], BF16, "posb")
        w2f = P.sb([128, 64], F32, "w2f"); w2b = P.sb([128, 64], BF16, "w2b")
        kTg = P.sb([64, T], BF16, "kTg")
        ph = P.ps([128, 512], F32, "ph"); pb = P.ps([128, 512], F32, "pb"); pc = P.ps([128, 512], F32, "pc")
        bias_h = P.sb([128, 1], F32, "bias_h")
        H = P.sb([128, 512], BF16, "H")
        for g in range(2):
            memset('dve', kcmpT[g], kcmpT[g][:], 0.0)
            memset('dve', vcmp[g], vcmp[g][:], 0.0)
            memset('dve', vcmp[g], vcmp[g][:, :, 64:65], 1.0)
        for kv in range(2):
            dma(w1f, w1f[:], cw1, cw1[kv]); dma(posf, posf[:], cpos, cpos[kv]); dma(w2f, w2f[:], cw2, cw2[kv])
            cp('dve', w1b, w1b[:], w1f, w1f[:]); cp('dve', posb, posb[:], posf, posf[:]); cp('dve', w2b, w2b[:], w2f, w2f[:])
            for l in range(32):
                mm(pb, pb[:, 0:1], w1b, w1b[:, l, :], posb, posb[:, l:l + 1], start=(l == 0), stop=(l == 31))
            cp('dve', bias_h, bias_h[:], pb, pb[:, 0:1])
            for g in range(2):
                dma(kTg, kTg[:], d_kT, d_kT[kv, g * 64:(g + 1) * 64, :])
                for l in range(32):
                    mm(ph, ph[:, 0:NCMP], w1b, w1b[:, l, :], kTg, kTg[:, l:l + 16 * (NCMP - 1) + 1:16],
                       start=(l == 0), stop=(l == 31))
                memset('dve', H, H[:], 0.0)
                act(H, H[:, 0:NCMP], ph, ph[:, 0:NCMP], AF.Gelu_apprx_tanh, bias=bias_h[:, 0:1], rd=[bias_h])
                if kv == 0:
                    mm(pc, pc[0:64, 0:NCMP], w2b, w2b[:], H, H[:, 0:NCMP])
                    cp('act', kcmpT[g], kcmpT[g][0:64, 0:NCMP], pc, pc[0:64, 0:NCMP])
                    if debug:
                        dma(d_cmp, d_cmp[g], kcmpT[g], kcmpT[g][0:64, :])
                else:
                    for c in range(NCC):
                        mm(pc, pc[:, c * 64:(c + 1) * 64], H, H[:, c * 128:(c + 1) * 128], w2b, w2b[:])
                    cp('act', vcmp[g], vcmp[g][:, 0:NCC, 0:64], pc, pc[:, 0:NCC * 64].rearrange("p (c d) -> p c d", d=64))
        P.pop()

    if upto >= 2:
        P.push()
        dma_pol['load'] = ['sp']; dma_pol['store'] = ['sp']
        selu = P.sb([128, 2, 128], BF16, "selu"); seluf = P.sb([128, 2, 128], F32, "seluf")
        dma(seluf, seluf[:], c_selu, c_selu[:].rearrange("a p t -> p a t"))
        cp('dve', selu, selu[:], seluf, seluf[:])
        exm = P.sb([128, T], BF16, "exm")
        dma(exm, exm[:], c_ex, c_ex[:])
        wimpf = P.sb([128, NCC, 128], F32, "wimpf"); wimp = P.sb([128, NCC, 128], BF16, "wimp")
        dma(wimpf, wimpf[:], c_wimp, c_wimp[:].rearrange("c p s -> p c s"))
        cp('dve', wimp, wimp[:], wimpf, wimpf[:])
        ksT = P.sb([128, T], BF16, "ksT"); kwT = P.sb([128, T], BF16, "kwT")
        memset('dve', ksT, ksT[64:128, :], 0.0)
        memset('pool', kwT, kwT[64:128, :], 0.0)
        memset('dve', ksT, ksT[64:65, :], 1.0)
        farf = P.sb([128, 512], F32, "farf")
        vs = P.sb([128, NT, 65], BF16, "vs"); vw = P.sb([128, NT, 65], BF16, "vw")
        selb = P.sb([128, 15, 512], BF16, "selb"); winb2 = P.sb([128, 6, 512], BF16, "winb2")
        bst = [P.sb([128, 512], F32, "bst%d" % i) for i in range(2)]
        cbt = [P.sb([128, 512], BF16, "cbt%d" % i) for i in range(2)]
        qrows_ = [P.sb([128, 2, 256], BF16, "qrows%d" % i) for i in range(2)]; grows_ = [P.sb([128, 2, 24], F32, "grows%d" % i) for i in range(2)]
        gown_ = [P.sb([128, 12], F32, "gown%d" % i) for i in range(2)]; gtmp = P.sb([128, 12], F32, "gtmp")
        qT_ = [P.sb([128, 4, 128], BF16, "qT%d" % i) for i in range(2)]
        for i_ in range(2):
            memset('dve', qT_[i_], qT_[i_][64:128, :, :], 0.0)
        Ec = [P.sb([128, 4, 128], BF16, "Ec%d" % i) for i in range(4)]
        Eb = [P.sb([128, 4, 128], BF16, "Eb%d" % i) for i in range(3)]
        m12_ = [P.sb([128, 2, 128], F32, "m12_%d" % i) for i in range(2)]
        imp = P.sb([128, 128], F32, "imp"); imp2 = P.sb([128, 128], F32, "imp2"); m8 = P.sb([128, 16], F32, "m8")
        mk = P.sb([128, 128], BF16, "mk"); maskT4 = P.sb([128, 4, 128], BF16, "maskT4")
        rden = P.sb([128, 4], F32, "rden"); coef = P.sb([128, 4], F32, "coef")
        oacc = P.sb([128, 4, 64], F32, "oacc"); otmp = P.sb([128, 4, 64], F32, "otmp"); onsa = P.sb([128, 256], BF16, "onsa")
        pq = P.ps([64, 4, 128], F32, "pq")
        psc = [P.ps([128, 4, 128], F32, "psc%d" % i) for i in range(2)]
        pn = P.ps([128, 4, 128], F32, "pn")
        pnT = P.ps([65, 4, 128], F32, "pnT")
        pnT2 = P.ps([65, 4, 128], F32, "pnT2")
        numTs = P.sb([65, 4, 128], F32, "numTs")
        pimp = P.ps([128, 4, 128], F32, "pimp")
        pmt = P.ps([128, 128], BF16, "pmt")
        for g in range(2):
            dma(ksT, ksT[0:64, :], d_kT, d_kT[2, g * 64:(g + 1) * 64, :])
            dma(farf, farf[:], c_far, c_far[g])
            for i_ in range(2):
                cp('dve', qT_[i_], qT_[i_][64:65, :, :], farf, farf[64:65, :].rearrange("p (h q) -> p h q", h=4))
            dma(kwT, kwT[0:64, :], d_kT, d_kT[3, g * 64:(g + 1) * 64, :])
            for n0 in range(0, NT, 16):
                n1 = min(NT, n0 + 16)
                dma(vs, vs[:, n0:n1, 0:64], d_vs, d_vs[n0 * 128:n1 * 128, g * 64:(g + 1) * 64].rearrange("(n p) c -> p n c", p=128))
                dma(vw, vw[:, n0:n1, 0:64], d_vw, d_vw[n0 * 128:n1 * 128, g * 64:(g + 1) * 64].rearrange("(n p) c -> p n c", p=128))
            memset('dve', vs, vs[:, :, 64:65], 1.0)
            memset('dve', vw, vw[:, :, 64:65], 1.0)
            k = 0
            for m in range(15):
                dma(bst[k % 2], bst[k % 2][:], c_selb, c_selb[g, :, m, :])
                tt('dve', selb, selb[:, m, :], bst[k % 2], bst[k % 2][:], farf, farf[:], ALU.subtract); k += 1
            for m in range(6):
                dma(bst[k % 2], bst[k % 2][:], c_winb, c_winb[g, :, m, :])
                cp('dve', winb2, winb2[:, m, :], bst[k % 2], bst[k % 2][:]); k += 1
            ne = 0
            ne_ = [0]
            for jj in range(NO):
                qrows, grows, gown, qT, m12 = qrows_[jj % 2], grows_[jj % 2], gown_[jj % 2], qT_[jj % 2], m12_[jj % 2]

                def load_q(j2):
                    dma(qrows_[j2 % 2], qrows_[j2 % 2][:], d_nq, d_nq[j2 * 256:(j2 + 1) * 256, g * 256:(g + 1) * 256].rearrange("(u p) c -> p u c", p=128))
                    dma(grows_[j2 % 2], grows_[j2 % 2][:], d_ng, d_ng[j2 * 256:(j2 + 1) * 256, :].rearrange("(u p) c -> p u c", p=128))
                    dma(m12_[j2 % 2], m12_[j2 % 2][:], c_m12, c_m12[j2])
                if jj == 0:
                    load_q(0)
                if jj + 1 < NO:
                    load_q(jj + 1)
                for h in range(4):
                    for u in range(2):
                        mm(pq, pq[:, h, :], qrows, qrows[:, u, h * 64:(h + 1) * 64], selu, selu[:, u, :], start=(u == 0), stop=(u == 1))
                cp('act', qT, qT[0:64], pq, pq[:])
                ts('dve', gtmp, gtmp[:], grows, grows[:, 0, g * 12:(g + 1) * 12], pv[:, 0:1], None, ALU.mult, rd=[pv])
                stt('dve', gown, gown[:], grows, grows[:, 1, g * 12:(g + 1) * 12], pv[:, 1:2], gtmp, gtmp[:], ALU.mult, ALU.add, rd=[pv])
                gv3 = gown[:].rearrange("p (h k) -> p h k", k=3)
                qT_all = qT[:].rearrange("p h q -> p (h q)")
                qT_aug = qT_all

                def finish_branch(pn, br, first):
                    ts('dve', rden, rden[:], pn, pn[:, :, 64], 1e-30, None, ALU.max)
                    recip(rden, rden[:], rden, rden[:])
                    tt('dve', coef, coef[:], rden, rden[:], gown, gv3[:, :, br], ALU.mult)
                    dst = oacc if first else otmp
                    tt('dve', dst, dst[:], pn, pn[:, :, 0:64], coef, coef[:].unsqueeze(2).to_broadcast([128, 4, 64]), ALU.mult)
                    if not first:
                        tt('dve', oacc, oacc[:], oacc, oacc[:], otmp, otmp[:], ALU.add)
                    if debug:
                        dma(d_dbg, d_dbg[br, jj * 128:(jj + 1) * 128, g * 256:(g + 1) * 256], oacc, oacc[:].rearrange("p h d -> p (h d)"))

                def back_T(src):
                    cp('act', numTs, numTs[:], src, src[:])
                    for h in range(4):
                        P.op('pe', lambda e, h=h: e.transpose(out=pn[:, h, 0:65], in_=numTs[:, h, :], identity=identf[0:65, 0:65]), [numTs, identf], [pn])

                ncc = min(NCC, cmp_nchunks(jj))
                for c in range(ncc):
                    pi = pair_off[jj] + c
                    dma(bst[k % 2], bst[k % 2][:], c_cmpb, c_cmpb[g, pi])
                    cp('dve', cbt[k % 2], cbt[k % 2][:], bst[k % 2], bst[k % 2][:])
                    sc = psc[ne % 2]; ne += 1
                    sc_all = sc[:].rearrange("p h q -> p (h q)")
                    mm(sc, sc_all, kcmpT[g], kcmpT[g][:, c * 128:(c + 1) * 128], qT, qT_all, start=True, stop=False)
                    mm(sc, sc_all, ident, ident[:], cbt[k % 2], cbt[k % 2][:], start=False, stop=True)
                    k += 1
                    act(Ec[c], Ec[c][:], sc, sc[:], AF.Exp)
                for h in range(4):
                    for c in range(ncc):
                        mm(pn, pn[:, h, 0:65], Ec[c], Ec[c][:, h, :], vcmp[g], vcmp[g][:, c, :], start=(c == 0), stop=(c == ncc - 1))
                    for c in range(ncc):
                        mm(pimp, pimp[:, h, :], Ec[c], Ec[c][:, h, :], wimp, wimp[:, c, :], start=(c == 0), stop=(c == ncc - 1))
                nk = 2 * jj + 2
                k0 = max(0, 2 * jj - 4)
                bufs = {}

                def score(kind, kc, n):
                    sc = psc[ne_[0] % 2]; E = Eb[ne_[0] % 3]; ne_[0] += 1
                    bufs[n] = E
                    sc_all = sc[:].rearrange("p h q -> p (h q)")
                    if kind == 's':
                        m = 2 * jj + 1 - kc
                        mm(sc, sc_all, ksT, ksT[:, kc * 128:(kc + 1) * 128], qT, qT_aug, start=True, stop=False)
                        if m < 14:
                            mm(sc, sc_all, ident, ident[:], selb, selb[:, m, :], start=False, stop=False)
                        mm(sc, sc_all, exm, exm[:, kc * 128:(kc + 1) * 128], maskT4, mT_all, start=False, stop=True)
                    else:
                        m = 2 * jj + 1 - kc
                        mm(sc, sc_all, kwT, kwT[:, kc * 128:(kc + 1) * 128], qT, qT_all, start=True, stop=False)
                        mm(sc, sc_all, ident, ident[:], winb2, winb2[:, m, :], start=False, stop=True)
                    act(E, E[:], sc, sc[:], AF.Exp)

                def pvs(kind, kc, n):
                    E = bufs.pop(n)
                    if kind == 's':
                        mm(pnT, pnT[:].rearrange("p h q -> p (h q)"), vs, vs[:, kc, :], E, E[:].rearrange("p h q -> p (h q)"), start=(kc == 0), stop=(kc == nk - 1))
                    else:
                        mm(pnT2, pnT2[:].rearrange("p h q -> p (h q)"), vw, vw[:, kc, :], E, E[:].rearrange("p h q -> p (h q)"), start=(kc == k0), stop=(kc == nk - 1))

                def run_items(items):
                    NI = len(items)
                    for n in range(NI + 1):
                        if n < NI:
                            score(items[n][0], items[n][1], n)
                        if n >= 1:
                            pvs(items[n - 1][0], items[n - 1][1], n - 1)
                mT_all = maskT4[:].rearrange("p h q -> p (h q)")
                run_items([('w', kc) for kc in range(k0, nk)])
                finish_branch(pn, 0, True)
                for h in range(4):
                    if h == 0:
                        ts('dve', imp, imp[:], pimp, pimp[:, 0, :], rden[:, 0:1], None, ALU.mult, rd=[rden])
                    else:
                        stt('dve', imp, imp[:], pimp, pimp[:, h, :], rden[:, h:h + 1], imp, imp[:], ALU.mult, ALU.add, rd=[rden])
                tt('dve', imp, imp[:], imp, imp[:], m12, m12[:, 0, :], ALU.mult)
                tt('dve', imp, imp[:], imp, imp[:], m12, m12[:, 1, :], ALU.add)
                if debug:
                    dma(d_imp, d_imp[g, jj * 128:(jj + 1) * 128, :], imp, imp[:])
                P.op('dve', lambda e: e.max(out=m8[:, 0:8], in_=imp[:]), [imp], [m8])
                P.op('dve', lambda e: e.match_replace(out=imp2[:], in_to_replace=m8[:, 0:8], in_values=imp[:], imm_value=-1e30), [imp, m8], [imp2])
                P.op('dve', lambda e: e.max(out=m8[:, 8:16], in_=imp2[:]), [imp2], [m8])
                ts('dve', mk, mk[:], imp, imp[:], m8[:, 15:16], 1.0, ALU.is_ge, ALU.subtract, rd=[m8])
                tr(pmt, pmt[:], mk, mk[:], ident)
                P.op('act', lambda e: e.mul(out=maskT4[:], in_=pmt[:].unsqueeze(1).to_broadcast([128, 4, 128]), mul=30000.0), [pmt], [maskT4])
                run_items([('s', kc) for kc in range(nk)])
                back_T(pnT)
                finish_branch(pn, 1, False)
                back_T(pnT2)
                finish_branch(pn, 2, False)
                cp('act', onsa, onsa[:], oacc, oacc[:].rearrange("p h d -> p (h d)"))
                dma(d_omix, d_omix[jj * 128:(jj + 1) * 128, 512 + g * 256:512 + (g + 1) * 256], onsa, onsa[:])
        P.pop()

    if upto >= 3:
        P.push()
        dma_pol['load'] = ['sp']; dma_pol['store'] = ['pool']
        woutb = P.sb([128, 8, D], BF16, "woutb"); wqb = P.sb([128, 8, D], BF16, "wqb"); wob = P.sb([128, 8, D], BF16, "wob")
        pwqb = P.sb([128, 8, D], BF16, "pwqb")
        skb = P.sb([128, 8, 256], BF16, "skb")
        kTm = P.sb([128, 8, 256], BF16, "kTm")
        vm = P.sb([128, 2, 4, 257], BF16, "vm")
        xt = [P.sb([128, D], F32, "x3_%d" % i) for i in range(2)]
        junk = P.sb([128, D], BF16, "junk3"); ss = P.sb([128, 1], F32, "ss3"); rstd = P.sb([128, 1], F32, "rstd3")
        hn = P.sb([128, D], BF16, "hn3"); hnT = P.sb([128, 8, 128], BF16, "hnT3")
        pt = P.ps([128, 8, 128], BF16, "pt3")
        pa = [P.ps([128, 512], F32, "pa%d" % i) for i in range(4)]
        pxa = P.ps([128, 2, 512], F32, "pxa")
        P.push()
        wst = [P.sb([128, D], F32, "wst3_%d" % i) for i in range(2)]
        wtmp = P.sb([128, 8, D], BF16, "wtmp"); skf = P.sb([128, 8, 256], F32, "skf")
        memT = P.sb([128, 8, 256], BF16, "memT")
        k = [0]

        def load_w(dst, src_t, src_ap3):
            for c in range(8):
                a = wst[k[0] % 2]
                dma(a, a[:], src_t, src_ap3[c])
                cp(['dve', 'pool'][k[0] % 2], dst, dst[:, c, :], a, a[:]); k[0] += 1
        load_w(woutb, w_out, w_out[:].rearrange("(c p) n -> c p n", p=128))
        load_w(wqb, xa_w, xa_w[0].rearrange("(c p) n -> c p n", p=128))
        load_w(wob, xa_w, xa_w[3].rearrange("(c p) n -> c p n", p=128))
        load_w(pwqb, pwq, pwq[:].rearrange("(c p) n -> c p n", p=128))
        dma(skf, skf[:], skd, skd[:].rearrange("c p n -> p c n"))
        cp('dve', skb, skb[:], skf, skf[:])
        memset('dve', vm, vm[:, :, :, 256:257], 1.0)
        for mc in range(2):
            x_t = xt[mc % 2]
            dma(x_t, x_t[:], memb, memb[mc * 128:(mc + 1) * 128, :])
            rmsnorm(x_t, x_t[:], 2, hn, hn[:], junk, ss, rstd)
            for c in range(8):
                tr(pt, pt[:, c, :], hn, hn[:, c * 128:(c + 1) * 128], ident)
            cp('act', memT, memT[:, :, mc * 128:(mc + 1) * 128], pt, pt[:])
        load_w(wtmp, xa_w, xa_w[1].rearrange("(c p) n -> c p n", p=128))
        for oc in range(8):
            for c in range(8):
                mm(pa[0], pa[0][:, 0:256], wtmp, wtmp[:, c, oc * 128:(oc + 1) * 128], memT, memT[:, c, :], start=(c == 0), stop=(c == 7))
            cp('act', kTm, kTm[:, oc, :], pa[0], pa[0][:, 0:256])
        load_w(wtmp, xa_w, xa_w[2].rearrange("(c p) n -> c p n", p=128))
        for mc in range(2):
            for half in range(2):
                for c in range(8):
                    mm(pa[half], pa[half][:], memT, memT[:, c, mc * 128:(mc + 1) * 128], wtmp, wtmp[:, c, half * 512:(half + 1) * 512], start=(c == 0), stop=(c == 7))
                cp('act', vm, vm[:, mc, half * 2:half * 2 + 2, 0:256], pa[half], pa[half][:].rearrange("p (h d) -> p h d", d=256))
        P.pop()
        og2 = P.sb([128, 2, 512], BF16, "og2"); omx = P.sb([128, D], BF16, "omx"); otm = P.sb([128, 512], F32, "otm")
        h1 = P.sb([128, D], F32, "h1"); qTx = P.sb([128, 8, 128], BF16, "qTx")
        Ex = P.sb([128, 2, 4, 128], BF16, "Ex"); rdx = P.sb([128, 4], F32, "rdx")
        oxa = P.sb([128, D], BF16, "oxa")
        hn3T = P.sb([128, 8, 128], BF16, "hn3Ts"); qpT = P.sb([128, 8, 128], BF16, "qpT")
        sc_ = [P.sb([128, 16, 128], F32, "scr%d" % i) for i in range(2)]; ab_ = [P.sb([128, 16, 128], F32, "ab%d" % i) for i in range(2)]
        negm = P.sb([128, 16], F32, "negm"); t16 = P.sb([128, 16, 16], F32, "t16"); scr2_ = [P.sb([128, 128], F32, "scr2_%d" % i) for i in range(8)]
        candall = P.sb([128, 8, 256], F32, "candall"); cand2_ = [P.sb([128, 256], F32, "cand2_%d" % i) for i in range(4)]; c16 = P.sb([128, 8, 16], F32, "c16")
        route = P.sb([128, 16], F32, "route"); zs = P.sb([128, 8], F32, "zs")
        memset('dve', route, route[:], 0.0)
        def Xgen(jj):
            sc = sc_[jj % 2]
            x_t = xt[jj % 2]
            dma(x_t, x_t[:], xo, xo[jj * 128:(jj + 1) * 128, :])
            dma(og2, og2[:], d_ogla, d_ogla[jj * 256:(jj + 1) * 256, :].rearrange("(u p) c -> p u c", p=128))
            dma(omx, omx[:, 512:1024], d_omix, d_omix[jj * 128:(jj + 1) * 128, 512:1024])
            ts('dve', otm, otm[:], og2, og2[:, 0, :], pv[:, 0:1], None, ALU.mult, rd=[pv])
            stt('dve', omx, omx[:, 0:512], og2, og2[:, 1, :], pv[:, 1:2], otm, otm[:], ALU.mult, ALU.add, rd=[pv])
            for c in range(8):
                tr(pt, pt[:, c, :], omx, omx[:, c * 128:(c + 1) * 128], ident)
            cp('act', hnT, hnT[:], pt, pt[:])
            for half in range(2):
                for c in range(8):
                    mm(pa[half], pa[half][:], hnT, hnT[:, c, :], woutb, woutb[:, c, half * 512:(half + 1) * 512], start=(c == 0), stop=(c == 7))
                tt('dve', h1, h1[:, half * 512:(half + 1) * 512], pa[half], pa[half][:], x_t, x_t[:, half * 512:(half + 1) * 512], ALU.add)
            yield
            rmsnorm(h1, h1[:], 1, hn, hn[:], junk, ss, rstd)
            for c in range(8):
                tr(pt, pt[:, c, :], hn, hn[:, c * 128:(c + 1) * 128], ident)
            cp('act', hnT, hnT[:], pt, pt[:])
            for oc in range(8):
                pq_ = pa[2 + oc % 2]
                for c in range(8):
                    mm(pq_, pq_[:, 0:128], wqb, wqb[:, c, oc * 128:(oc + 1) * 128], hnT, hnT[:, c, :], start=(c == 0), stop=(c == 7))
                cp(['act', 'dve'][oc % 2], qTx, qTx[:, oc, :], pq_, pq_[:, 0:128])
                yield
            yield
            for mc in range(2):
                for h in range(4):
                    for dc in range(2):
                        mm(pxa, pxa[:, mc, h * 128:(h + 1) * 128], kTm, kTm[:, h * 2 + dc, mc * 128:(mc + 1) * 128], qTx, qTx[:, h * 2 + dc, :], start=(dc == 0), stop=(dc == 1))
            act(Ex, Ex[:].rearrange("p a h q -> p a (h q)"), pxa, pxa[:], AF.Exp, scale=1.0 / 16)
            for h in range(4):
                o_ap = pxa[:, h // 2, (h % 2) * 256:(h % 2) * 256 + 256]
                for mc in range(2):
                    mm(pa[h % 2], pa[h % 2][:, 0:257], Ex, Ex[:, mc, h, :], vm, vm[:, mc, h, :], start=(mc == 0), stop=(mc == 1))
                ts('dve', rdx, rdx[:, h:h + 1], pa[h % 2], pa[h % 2][:, 256:257], 1e-30, None, ALU.max)
                recip(rdx, rdx[:, h:h + 1], rdx, rdx[:, h:h + 1])
                ts('dve', oxa, oxa[:, h * 256:(h + 1) * 256], pa[h % 2], pa[h % 2][:, 0:256], rdx[:, h:h + 1], None, ALU.mult, rd=[rdx])
            yield
            for c in range(8):
                tr(pt, pt[:, c, :], oxa, oxa[:, c * 128:(c + 1) * 128], ident)
            cp('act', hnT, hnT[:], pt, pt[:])
            for half in range(2):
                for c in range(8):
                    mm(pa[half], pa[half][:], hnT, hnT[:, c, :], wob, wob[:, c, half * 512:(half + 1) * 512], start=(c == 0), stop=(c == 7))
                tt('dve', h1, h1[:, half * 512:(half + 1) * 512], pa[half], pa[half][:], h1, h1[:, half * 512:(half + 1) * 512], ALU.add)
            dma(d_h, d_h[jj * 128:(jj + 1) * 128, :], h1, h1[:])
            yield
            rmsnorm(h1, h1[:], 3, hn, hn[:], junk, ss, rstd)
            for c in range(8):
                tr(pt, pt[:, c, :], hn, hn[:, c * 128:(c + 1) * 128], ident)
            cp('act', hn3T, hn3T[:], pt, pt[:])
            dma(d_hn3T, d_hn3T[:, :, jj * 128:(jj + 1) * 128], hn3T, hn3T[:])
            for oc in range(8):
                pq_ = pa[2 + oc % 2]
                for c in range(8):
                    mm(pq_, pq_[:, 0:128], pwqb, pwqb[:, c, oc * 128:(oc + 1) * 128], hn3T, hn3T[:, c, :], start=(c == 0), stop=(c == 7))
                cp(['act', 'dve'][oc % 2], qpT, qpT[:, oc, :], pq_, pq_[:, 0:128])
                yield
            yield
            for oc in range(8):
                pq_ = pa[oc % 2]
                mm(pq_, pq_[:, 0:256], qpT, qpT[:, oc, :], skb, skb[:, oc, :])
                cp(['act', 'dve'][oc % 2], sc, sc[:, 2 * oc:2 * oc + 2, :], pq_, pq_[:, 0:256].rearrange("p (a k) -> p a k", a=2))
            yield

        def Rgen(jj):
            sc = sc_[jj % 2]; ab = ab_[jj % 2]
            P.op('dve', lambda e: e.tensor_reduce(out=negm[:], in_=sc[:], axis=AX.X, op=ALU.max), [sc], [negm])
            tt('dve', sc, sc[:], sc, sc[:], negm, negm[:].unsqueeze(2).to_broadcast([128, 16, 128]), ALU.subtract)
            act(ab, ab[:].rearrange("p r k -> p (r k)"), sc, sc[:].rearrange("p r k -> p (r k)"), AF.Exp)
            yield
            for r0 in range(0, 16, 8):
                for r in range(r0, r0 + 8):
                    P.op('dve', lambda e, r=r: e.max(out=t16[:, r, 0:8], in_=ab[:, r, :]), [ab], [t16])
                for r in range(r0, r0 + 8):
                    P.op('dve', lambda e, r=r: e.match_replace(out=scr2_[r % 8][:], in_to_replace=t16[:, r, 0:8], in_values=ab[:, r, :], imm_value=-1.0), [ab, t16], [scr2_[r % 8]])
                for r in range(r0, r0 + 8):
                    P.op('dve', lambda e, r=r: e.max(out=t16[:, r, 8:16], in_=scr2_[r % 8][:]), [scr2_[r % 8]], [t16])
                yield
            t16v = t16[:].rearrange("p (h a) k -> p h a k", a=2)
            abv = ab[:].rearrange("p (h a) k -> p h a k", a=2)

            def cand_top16():
                yield
                tt('dve', candall, candall[:].rearrange("p h (a b) -> p h a b", a=16),
                   t16, t16v[:, :, 0, :].unsqueeze(3).to_broadcast([128, 8, 16, 16]),
                   t16, t16v[:, :, 1, :].unsqueeze(2).to_broadcast([128, 8, 16, 16]), ALU.mult)
                for h0 in range(0, 8, 4):
                    for h in range(h0, h0 + 4):
                        P.op('dve', lambda e, h=h: e.max(out=c16[:, h, 0:8], in_=candall[:, h, :]), [candall], [c16])
                    for h in range(h0, h0 + 4):
                        P.op('dve', lambda e, h=h: e.match_replace(out=cand2_[h % 4][:], in_to_replace=c16[:, h, 0:8], in_values=candall[:, h, :], imm_value=-1.0), [candall, c16], [cand2_[h % 4]])
                    for h in range(h0, h0 + 4):
                        P.op('dve', lambda e, h=h: e.max(out=c16[:, h, 8:16], in_=cand2_[h % 4][:]), [cand2_[h % 4]], [c16])
                    yield
            yield from cand_top16()
            P.op('dve', lambda e: e.tensor_reduce(out=zs[:], in_=c16[:], axis=AX.X, op=ALU.add), [c16], [zs])
            recip(zs, zs[:], zs, zs[:])
            tt('dve', ab, abv[:, :, 1, :], ab, abv[:, :, 1, :], zs, zs[:].unsqueeze(2).to_broadcast([128, 8, 128]), ALU.mult)
            stt('dve', route, route[:, 0:8], c16, c16[:, :, 15], 1.0 - 1e-6, zs, zs[:], ALU.mult, ALU.mult)
            dma(d_route, d_route[jj * 128:(jj + 1) * 128, 0:2048], ab, ab[:].rearrange("p r k -> p (r k)"))
            dma(d_route, d_route[jj * 128:(jj + 1) * 128, 2048:2064], route, route[:])
            yield

        def drain(g):
            for _ in g:
                pass
        drain(Xgen(0))
        for jj in range(NO):
            gr = Rgen(jj)
            gx = Xgen(jj + 1) if jj + 1 < NO else iter(())
            ra = xa_ = True
            while ra or xa_:
                if xa_:
                    xa_ = next(gx, 'END') != 'END'
                if ra:
                    ra = next(gr, 'END') != 'END'
        P.pop()

    if upto >= 4:
        P.push()
        dma_pol['load'] = ['sp', 'pool']; dma_pol['store'] = ['sp']
        TG = 2
        IC = 16
        NCH = 128 // IC
        ACT_HEADS = (1, 3, 4, 6, 7)
        hT_ = [P.sb([128, 8, TG * 128], BF16, "hT%d" % i) for i in range(2)]
        ab_ = [[P.sb([128, 16, 128], F32, "ab4_%d_%d" % (i, u)) for u in range(TG)] for i in range(2)]
        rt_ = [[P.sb([128, 16], F32, "rt%d_%d" % (i, u)) for u in range(TG)] for i in range(2)]
        Wc = [[P.sb([128, IC * 128], BF16, "Wc%d_%d" % (i, u)) for u in range(TG)] for i in range(2)]
        et = [P.sb([128, IC, 128], F32, "et%d" % i) for i in range(4)]
        mt = [P.sb([128, IC, 128], BF16, "mt%d" % i) for i in range(2)]
        dnb = [P.sb([128, 8, 512], BF16, "dnb%d" % i) for i in range(3)]
        upb = [P.sb([128, 4, D], BF16, "upb%d" % i) for i in range(3)]
        Gs = [P.sb([128, 512], BF16, "G%d" % i) for i in range(2)]
        GT = [P.sb([128, 4, 128], BF16, "GT%d" % i) for i in range(2)]
        py = [P.ps([128, 2, 512], F32, "py%d" % u) for u in range(TG)]
        pd = [P.ps([128, 512], F32, "pd%d" % i) for i in range(2)]
        ptg = [P.ps([128, 8, 128], BF16, "ptg%d" % i) for i in range(2)]
        h2 = P.sb([128, D], F32, "h2"); yo = P.sb([128, D], F32, "yo")
        junk = P.sb([128, D], BF16, "junk4"); ss = P.sb([128, 1], F32, "ss4"); rstd = P.sb([128, 1], F32, "rstd4")
        qn = [0]
        def load_group(t2):
            hT2, ab2, rt2 = hT_[t2 % 2], ab_[t2 % 2], rt_[t2 % 2]
            dma(hT2, hT2[:], d_hn3T, d_hn3T[:, :, t2 * TG * 128:(t2 + 1) * TG * 128])
            for u in range(TG):
                j = t2 * TG + u
                dma(ab2[u], ab2[u][:].rearrange("p r k -> p (r k)"), d_route, d_route[j * 128:(j + 1) * 128, 0:2048])
                dma(rt2[u], rt2[u][:], d_route, d_route[j * 128:(j + 1) * 128, 2048:2064])
        load_group(0)
        for tg in range(NO // TG):
            hT, ab, rt = hT_[tg % 2], ab_[tg % 2], rt_[tg % 2]
            if tg + 1 < NO // TG:
                load_group(tg + 1)

            def wgen(c):
                its = [(u, h) for u in range(TG) for h in range(8)]
                K = len(its)
                eb_ = {}; mb_ = {}

                def E_(n):
                    u, h = its[n]
                    e_ = et[qn[0] % 4]; qn[0] += 1
                    eb_[n] = e_
                    if h in ACT_HEADS:
                        for i_ in range(IC):
                            P.op('act', lambda e, e_=e_, i_=i_, u=u, h=h: e.activation(out=e_[:, i_, :], in_=ab[u][:, 2 * h + 1, :], func=AF.Copy,
                                                                                  scale=ab[u][:, 2 * h, c * IC + i_:c * IC + i_ + 1]),
                                 [ab[u]], [e_] if i_ in (0, IC - 1) else [])
                    else:
                        tt('dve', e_, e_[:], ab[u], ab[u][:, 2 * h, c * IC:(c + 1) * IC].unsqueeze(2).to_broadcast([128, IC, 128]),
                           ab[u], ab[u][:, 2 * h + 1, :].unsqueeze(1).to_broadcast([128, IC, 128]), ALU.mult)

                def S_(n):
                    u, h = its[n]
                    e_ = eb_.pop(n)
                    w_ap = Wc[c % 2][u][:].rearrange("p (a b) -> p a b", a=IC)
                    if h == 0:
                        stt('dve', Wc[c % 2][u], w_ap, e_, e_[:], rt[u][:, h:h + 1], e_, e_[:], ALU.is_ge, ALU.mult, rd=[rt[u]])
                    else:
                        m_ = mt[n % 2]
                        mb_[n] = m_
                        stt('dve', m_, m_[:], e_, e_[:], rt[u][:, h:h + 1], e_, e_[:], ALU.is_ge, ALU.mult, rd=[rt[u]])

                def A_(n):
                    u, h = its[n]
                    if h == 0:
                        return
                    m_ = mb_.pop(n)
                    w_ap = Wc[c % 2][u][:].rearrange("p (a b) -> p a b", a=IC)
                    tt('dve', Wc[c % 2][u], w_ap, Wc[c % 2][u], w_ap, m_, m_[:], ALU.add)
                for n in range(K + 2):
                    if n < K:
                        E_(n)
                    if 1 <= n <= K:
                        S_(n - 1)
                    if n >= 2:
                        A_(n - 2)
                    yield

            def load_w(ecx):
                dn, ub = dnb[ecx % 3], upb[ecx % 3]
                dma(dn, dn[:], d_downT, d_downT[:, ecx * 512:(ecx + 1) * 512].rearrange("(c p) e -> p c e", p=128))
                dma(ub, ub[:], d_up, d_up[ecx * 512:(ecx + 1) * 512, :].rearrange("(s p) d -> p s d", p=128))

            items = [(ecx, u) for ecx in range(32) for u in range(TG)]
            N = len(items)

            def stA(n):
                ecx, u = items[n]
                if u == 0:
                    if ecx == 0:
                        load_w(0)
                    if ecx + 1 < 32:
                        load_w(ecx + 1)
                    if ecx % 4 == 0:
                        for _ in wg[0]:
                            pass
                        wg[0] = wgen(ecx // 4 + 1) if ecx // 4 + 1 < NCH else iter(())
                for _ in range(3):
                    next(wg[0], None)
                dn = dnb[ecx % 3]
                pdt = pd[n % 2]; G = Gs[n % 2]
                for c in range(8):
                    mm(pdt, pdt[:], hT, hT[:, c, u * 128:(u + 1) * 128], dn, dn[:, c, :], start=(c == 0), stop=(c == 7))
                act(G, G[:], pdt, pdt[:], AF.Gelu_apprx_tanh)
                wch = Wc[(ecx // 4) % 2][u]
                tt('dve', G, G[:], G, G[:], wch, wch[:, (ecx % 4) * 512:(ecx % 4 + 1) * 512], ALU.mult)

            def stB(n):
                G = Gs[n % 2]; gt_ = GT[n % 2]; pt_ = ptg[n % 2]
                for s_ in range(4):
                    tr(pt_, pt_[:, s_, :], G, G[:, s_ * 128:(s_ + 1) * 128], ident)
                cp('act', gt_, gt_[:], pt_, pt_[:, 0:4, :])

            def stC(n):
                ecx, u = items[n]
                gt_ = GT[n % 2]; ub = upb[ecx % 3]
                for half in range(2):
                    for s_ in range(4):
                        mm(py[u], py[u][:, half, :], gt_, gt_[:, s_, :], ub, ub[:, s_, half * 512:(half + 1) * 512],
                           start=(ecx == 0 and s_ == 0), stop=(ecx == 31 and s_ == 3))

            wg = [iter(())]
            for _ in wgen(0):
                pass
            for n in range(N + 2):
                if n < N:
                    stA(n)
                if 1 <= n <= N:
                    stB(n - 1)
                if n >= 2:
                    stC(n - 2)
            for _ in wg[0]:
                pass
            for u in range(TG):
                j = tg * TG + u
                dma(h2, h2[:], d_h, d_h[j * 128:(j + 1) * 128, :])
                tt('dve', h2, h2[:], h2, h2[:], py[u], py[u][:].rearrange("p a b -> p (a b)"), ALU.add)
                rmsnorm(h2, h2[:], 4, yo, yo[:], junk, ss, rstd)
                dma(out, out[j * 128:(j + 1) * 128, :], yo, yo[:])
        P.pop()

    P.finish()
    return nc, dict(npair=npair, pair_off=pair_off, NCC=NCC, NCMP=NCMP, ninst=P.ninst)


def _t5_bucket_np(dist):
    n = np.maximum(dist, 0)
    nf = np.maximum(n, 1).astype(np.float32)
    log_ratio = (np.log(nf / np.float32(16)) / np.float32(math.log(2048 / 16))).astype(np.float32)
    large = 16 + (log_ratio * np.float32(16)).astype(np.int32)
    large = np.minimum(large, 31)
    return np.where(n < 16, n, large).astype(np.int64)


def _bias_tile(rel_bias, g, dist, valid):
    bk = _t5_bucket_np(dist)
    outt = np.empty((128, 4, 128), np.float32)
    for h in range(4):
        outt[:, h, :] = np.where(valid, rel_bias[bk, g * 4 + h], np.float32(NEG))
    return outt.reshape(128, 512)


def make_core_inputs(inputs, T, b, p, meta):
    NT = T // 128; NO = NT // 2; NS = T // 64; NCMP = meta['NCMP']; NCC = meta['NCC']
    f = lambda a: np.ascontiguousarray(np.asarray(a, dtype=np.float32))
    x = f(inputs['x'][b]); rel_bias = f(inputs['rel_bias'])
    m = {}
    m['xb'] = x
    m['xo'] = np.ascontiguousarray(x.reshape(NO, 2, 128, D)[:, p].reshape(NO * 128, D))
    m['mem'] = f(inputs['mem'][b])
    w_in = f(inputs['w_in'][0])
    m['w_in'] = np.ascontiguousarray(np.concatenate([w_in[:, ORIG[k][0]:ORIG[k][1]] for k in PERM_ORDER], axis=1))
    m['gw2'] = f(inputs['gla_gate_w2'][0]); m['gb'] = f(inputs['gla_gate_b'][0]).reshape(1, 256)
    m['gnorm'] = f(inputs['gla_out_norm'][0]).reshape(1, 128)
    m['norms'] = np.stack([f(inputs['norm_mix'][0]), f(inputs['norm_xattn'][0]), f(inputs['norm_mem'][0]),
                           f(inputs['norm_ffn'][0]), f(inputs['norm_final'])], 0)
    cw1 = np.stack([f(inputs['cmp_k_w1'][0]), f(inputs['cmp_v_w1'][0])], 0)
    m['cw1'] = np.ascontiguousarray(cw1.reshape(2, 32, 64, 128).transpose(0, 2, 1, 3))
    cpos = np.stack([f(inputs['cmp_pos_k'][0]), f(inputs['cmp_pos_v'][0])], 0)
    m['cpos'] = np.ascontiguousarray(cpos.transpose(0, 2, 1))
    m['cw2'] = np.stack([f(inputs['cmp_k_w2'][0]), f(inputs['cmp_v_w2'][0])], 0)
    m['w_out'] = f(inputs['w_out'][0])
    m['xa_w'] = np.stack([f(inputs['xa_wq'][0]), f(inputs['xa_wk'][0]), f(inputs['xa_wv'][0]), f(inputs['xa_wo'][0])], 0)
    m['pwq'] = f(inputs['peer_wq'][0])
    sk = f(inputs['peer_subkeys'][0])
    skd = np.zeros((8, 128, 256), np.float32)
    for h in range(8):
        for pp in range(2):
            skd[h, pp * 64:(pp + 1) * 64, pp * 128:(pp + 1) * 128] = sk[h, pp].T
    m['skd'] = skd
    m['downT'] = np.ascontiguousarray(f(inputs['peer_down'][0]).T)
    m['up'] = f(inputs['peer_up'][0])
    m['c_ident'] = np.eye(128, dtype=np.float32)
    s_ = np.arange(128)[:, None]; t_ = np.arange(128)[None, :]
    m['c_ucs'] = np.where(s_ <= t_, -1.0 / 16, 0.0).astype(np.float32)
    m['c_urev'] = np.where(s_ > t_, -1.0 / 16, 0.0).astype(np.float32)
    m['c_causal'] = (s_ <= t_).astype(np.float32)
    m['c_selu'] = np.stack([np.eye(128) * (1 - p), np.eye(128) * p], 0).astype(np.float32)
    m['c_pv'] = np.tile(np.array([[1.0 - p, float(p)]], np.float32), (128, 1))
    kk = np.arange(128)[:, None]; qq = np.arange(128)[None, :]
    selb = np.empty((2, 128, 15, 512), np.float32); winb = np.empty((2, 128, 6, 512), np.float32)
    for g in range(2):
        for mm_ in range(15):
            j = mm_ - 1 + p
            dist = 128 * j + qq - kk
            selb[g, :, mm_, :] = _bias_tile(rel_bias, g, dist, (dist >= 0) & (j >= 0))
        for mm_ in range(6):
            j = mm_ - 1 + p
            dist = 128 * j + qq - kk
            winb[g, :, mm_, :] = _bias_tile(rel_bias, g, dist, (dist >= 0) & (dist < 512) & (j >= 0))
    m['c_selb'] = selb; m['c_winb'] = winb
    cmpb = np.empty((2, meta['npair'], 128, 512), np.float32)
    for jj in range(NO):
        for c in range(min(NCC, cmp_nchunks(jj))):
            n = 128 * c + kk
            t = (2 * jj + p) * 128 + qq
            dist = t - (16 * n + 31)
            for g in range(2):
                cmpb[g, meta['pair_off'][jj] + c] = _bias_tile(rel_bias, g, dist, (dist >= 0) & (n < NCMP))
    m['c_cmpb'] = cmpb
    far = np.empty((2, 128, 4, 128), np.float32)
    for g in range(2):
        for h in range(4):
            far[g, :, h, :] = rel_bias[31, g * 4 + h]
    m['c_far'] = far.reshape(2, 128, 512)
    wimp = np.zeros((NCC * 128, 128), np.float32)
    for s in range(NS):
        for r in range(-1, 4):
            n = 4 * s + r
            lo = 16 * r
            ov = min(lo + 32, 64) - max(lo, 0)
            if 0 <= n < NCMP:
                wimp[n, s] += ov / 32.0
    m['c_wimp'] = wimp.reshape(NCC, 128, 128)
    m12 = np.zeros((NO, 128, 2, 128), np.float32)
    sid = np.arange(128)[None, :]
    for jj in range(NO):
        t = (2 * jj + p) * 128 + np.arange(128)[:, None]
        cur = t // 64
        visible = (sid * 64 <= t) & (sid < NS)
        f0 = (sid == 0); f1 = (sid == cur); f2 = (sid == cur - 1)
        forced = f0 | f1 | f2
        m12[jj, :, 0, :] = (visible & ~forced)
        add = np.where(~visible, -100.0 - sid, 0.0)
        add = np.where(f2, 100.0, add); add = np.where(f1, 101.0, add); add = np.where(f0 & (sid < NS), 102.0, add)
        m12[jj, :, 1, :] = add
    m['c_m12'] = m12
    ex = np.zeros((128, T), np.float32)
    ex[np.arange(T) // 64, np.arange(T)] = 1.0
    m['c_ex'] = ex.astype(NPBF)
    return m


_CACHE = {}


def kernel(**inputs):
    T = inputs['x'].shape[1]
    B = inputs['x'].shape[0]
    if T not in _CACHE:
        _CACHE[T] = build(T)
    nc, meta = _CACHE[T]
    in_maps = []
    for c in range(2 * B):
        in_maps.append(make_core_inputs(inputs, T, c // 2, c % 2, meta))
    res = run_bass_kernel_spmd(nc, in_maps, core_ids=list(range(2 * B)))
    NO = T // 256
    outp = np.empty((B, T // 128, 128, D), np.float32)
    for c in range(2 * B):
        o = np.asarray(res.results[c]["out"], dtype=np.float32).reshape(NO, 128, D)
        outp[c // 2, (c % 2)::2] = o
    return outp.reshape(B, T, D)
```
